# Optimizing a Trainium2 kernel written in Bass

```python
import math
import jax
import jax.numpy as jnp
from jax import lax
import numpy as np

D_MODEL = 1024
BATCH = 8
SEQ = 2048
DEPTH = 1

CHUNK = 64
EPS = 1e-6
ROPE_THETA = 10000.0
A_HEADS = 8
A_DK = 64
A_DV = 64
CONV_K = 4
B_HEADS = 8
B_KV_HEADS = 2
B_HD = 64
IDX_HEADS = 8
IDX_HD = 64
TOPK_MAX = 256
N_GROUPS = 4
EXPERTS_PER_GROUP = 4
N_EXPERTS = N_GROUPS * EXPERTS_PER_GROUP
TOPK_IN_GROUP = 2
D_EXPERT = 256

A_QK_W = A_HEADS * A_DK
A_V_W = A_HEADS * A_DV
B_Q_W = B_HEADS * B_HD
B_KV_W = B_KV_HEADS * B_HD
CONV_W = 2 * A_QK_W + A_V_W
IN_SPLITS = (
    ("a_q", A_QK_W), ("a_k", A_QK_W), ("a_v", A_V_W), ("a_z", A_V_W),
    ("a_beta", A_HEADS), ("a_alpha", A_HEADS),
    ("b_q", B_Q_W), ("b_k", B_KV_W), ("b_v", B_KV_W),
    ("i_q", IDX_HEADS * IDX_HD), ("i_k", IDX_HD), ("i_w", IDX_HEADS),
    ("gate_a", D_MODEL), ("gate_b", D_MODEL),
)
D_IN = sum(n for _, n in IN_SPLITS)

kernel_name = "hybrid_gdn_dsa_hmoe_block"


def rmsnorm(x, g):
    xf = x.astype(jnp.float32)
    y = xf * lax.rsqrt(jnp.mean(xf * xf, axis=-1, keepdims=True) + EPS)
    return (y * g.astype(jnp.float32)).astype(x.dtype)


def l2norm(x):
    xf = x.astype(jnp.float32)
    return xf * lax.rsqrt(jnp.sum(xf * xf, axis=-1, keepdims=True) + EPS)


def split_cols(z):
    parts, off = {}, 0
    for name, n in IN_SPLITS:
        parts[name] = z[..., off:off + n]
        off += n
    return parts


def rope(x, pos):
    half = x.shape[-1] // 2
    inv = ROPE_THETA ** (-jnp.arange(half, dtype=jnp.float32) / half)
    ang = pos.astype(jnp.float32)[..., None] * inv
    cos = jnp.cos(ang)[:, :, None, :]
    sin = jnp.sin(ang)[:, :, None, :]
    xf = x.astype(jnp.float32)
    x1, x2 = xf[..., :half], xf[..., half:]
    return jnp.concatenate([x1 * cos - x2 * sin, x2 * cos + x1 * sin], axis=-1).astype(x.dtype)


def causal_dwconv(x, w):
    c = x.shape[-1]
    return lax.conv_general_dilated(
        x, w[:, None, :].astype(x.dtype), window_strides=(1,), padding=[(CONV_K - 1, 0)],
        dimension_numbers=("NWC", "WIO", "NWC"), feature_group_count=c)


def gated_delta_rule(q, k, v, g, beta):
    b, s, h, dk = q.shape
    dv = v.shape[-1]
    n = s // CHUNK

    def chunks(t):
        return jnp.moveaxis(t.reshape((b, n, CHUNK, h) + t.shape[3:]), 3, 1)

    qc, kc, vc, gc, bc = chunks(q), chunks(k), chunks(v), chunks(g), chunks(beta)
    G = jnp.cumsum(gc, axis=-1)
    idx = jnp.arange(CHUNK)
    incl = idx[:, None] >= idx[None, :]
    strict = idx[:, None] > idx[None, :]
    decay = jnp.exp(jnp.where(incl, G[..., :, None] - G[..., None, :], -jnp.inf))
    kk = jnp.einsum("bhnid,bhnjd->bhnij", kc, kc)
    a_mat = jnp.where(strict, bc[..., :, None] * kk * decay, 0.0) + jnp.eye(CHUNK, dtype=jnp.float32)
    rhs = jnp.concatenate([vc * bc[..., None], kc * (bc * jnp.exp(G))[..., None]], axis=-1)
    sol = lax.linalg.triangular_solve(a_mat, rhs, left_side=True, lower=True, unit_diagonal=True)
    u, w = sol[..., :dv], sol[..., dv:]
    qk = jnp.einsum("bhnid,bhnjd->bhnij", qc, kc) * decay
    q_dec = qc * jnp.exp(G)[..., None]
    k_dec = kc * jnp.exp(G[..., -1:] - G)[..., None]
    g_last = jnp.exp(G[..., -1])

    def step(state, xs):
        u_n, w_n, q_n, qk_n, k_n, gl_n = xs
        v_new = u_n - jnp.einsum("bhck,bhkv->bhcv", w_n, state)
        o_n = jnp.einsum("bhck,bhkv->bhcv", q_n, state) + jnp.einsum("bhij,bhjv->bhiv", qk_n, v_new)
        state = state * gl_n[..., None, None] + jnp.einsum("bhck,bhcv->bhkv", k_n, v_new)
        return state, o_n

    xs = tuple(jnp.moveaxis(t, 2, 0) for t in (u, w, q_dec, qk, k_dec, g_last))
    s0 = jnp.zeros((b, h, dk, dv), jnp.float32)
    _, o = lax.scan(step, s0, xs)
    return jnp.transpose(o, (1, 0, 3, 2, 4)).reshape(b, s, h, dv)


def dsa_attention(q, k, v, qi, ki, wi):
    b, s, hq, d = q.shape
    hk = k.shape[2]
    grp = hq // hk
    n = s // CHUNK
    topk = min(TOPK_MAX, s // 4)
    kf = k.reshape(b, s, hk * d)
    vf = v.reshape(b, s, hk * d)
    kif = ki.astype(jnp.float32)
    key_pos = jnp.arange(s)
    gather = jax.vmap(lambda table, ids: table[ids])

    def to_blocks(t):
        return jnp.moveaxis(t.reshape((b, n, CHUNK) + t.shape[2:]), 1, 0)

    def block(args):
        q_b, qi_b, wi_b, blk = args
        limit = (blk + 1) * CHUNK
        rel = jax.nn.relu(jnp.einsum("bthd,bsd->bths", qi_b.astype(jnp.float32), kif))
        score = jnp.einsum("bth,bths->bts", wi_b.astype(jnp.float32), rel)
        score = jnp.where(key_pos < limit, score, -jnp.inf)
        _, sel = lax.top_k(score, topk)
        valid = sel < limit
        ks = gather(kf, sel).reshape(b, CHUNK, topk, hk, d)
        vs = gather(vf, sel).reshape(b, CHUNK, topk, hk, d)
        qg = q_b.reshape(b, CHUNK, hk, grp, d)
        logits = jnp.einsum("btkgd,btjkd->btkgj", qg, ks).astype(jnp.float32) * (d ** -0.5)
        logits = jnp.where(valid[:, :, None, None, :], logits, -jnp.inf)
        p = jax.nn.softmax(logits, axis=-1).astype(v.dtype)
        o = jnp.einsum("btkgj,btjkd->btkgd", p, vs)
        return o.reshape(b, CHUNK, hq * d)

    out = lax.map(block, (to_blocks(q), to_blocks(qi), to_blocks(wi), jnp.arange(n)))
    return jnp.moveaxis(out, 0, 1).reshape(b, s, hq * d)


def hier_moe(h, w_rg, b_rg, w_re, b_re, w1, w3, w2):
    bsz, s, d = h.shape
    xt = h.reshape(-1, d)
    glog = (xt @ w_rg + b_rg).astype(jnp.float32)
    gp = jax.nn.softmax(glog, axis=-1)
    gsel = jnp.argmax(glog, axis=-1)
    ggate = jnp.take_along_axis(gp, gsel[:, None], axis=-1)
    elog = (xt @ w_re + b_re).astype(jnp.float32).reshape(-1, N_GROUPS, EXPERTS_PER_GROUP)
    elog_g = jnp.take_along_axis(elog, gsel[:, None, None], axis=1)[:, 0]
    ep = jax.nn.softmax(elog_g, axis=-1)
    top_p, top_i = lax.top_k(ep, TOPK_IN_GROUP)
    top_p = top_p / jnp.sum(top_p, axis=-1, keepdims=True)
    wts = ggate * top_p
    eid = gsel[:, None] * EXPERTS_PER_GROUP + top_i
    comb = jnp.sum(jax.nn.one_hot(eid, N_EXPERTS, dtype=jnp.float32) * wts[..., None], axis=1)
    act = jax.nn.silu(jnp.einsum("nd,edf->nef", xt, w1)) * jnp.einsum("nd,edf->nef", xt, w3)
    act = act * comb[:, :, None].astype(act.dtype)
    y = jnp.einsum("nef,efd->nd", act, w2)
    return y.reshape(bsz, s, d)


def setup_inputs(seed: int = 0) -> dict:
    key = jax.random.key(seed)
    ks = jax.random.split(key, 24)
    f32 = jnp.float32

    def nrm(k, shape, fan):
        return jax.random.normal(k, shape, f32) * (fan ** -0.5)

    def gain(k, shape):
        return 1.0 + 0.02 * jax.random.normal(k, shape, f32)

    x = jax.random.normal(ks[0], (BATCH, SEQ, D_MODEL), f32)
    offs = jax.random.randint(ks[1], (BATCH, 1), 0, 64) * CHUNK
    positions = (offs + jnp.arange(SEQ, dtype=jnp.int32)[None, :]).astype(jnp.int32)
    norm1_g = gain(ks[2], (DEPTH, D_MODEL))
    w_in = nrm(ks[3], (DEPTH, D_MODEL, D_IN), D_MODEL)
    b_gate = 0.02 * jax.random.normal(ks[4], (DEPTH, 2 * D_MODEL), f32)
    conv_w = nrm(ks[5], (DEPTH, CONV_K, CONV_W), CONV_K)
    a_log = jnp.log(jax.random.uniform(ks[6], (DEPTH, A_HEADS), f32, 1.0, 16.0))
    dt = jnp.exp(jax.random.uniform(ks[7], (DEPTH, A_HEADS), f32, math.log(1e-3), math.log(1e-1)))
    dt_bias = dt + jnp.log(-jnp.expm1(-dt))
    a_norm_g = gain(ks[8], (DEPTH, A_DV))
    w_proj_a = nrm(ks[9], (DEPTH, A_V_W, D_MODEL), A_V_W)
    w_proj_b = nrm(ks[10], (DEPTH, B_Q_W, D_MODEL), B_Q_W)
    w_out = nrm(ks[11], (DEPTH, D_MODEL, D_MODEL), D_MODEL)
    norm2_g = gain(ks[12], (DEPTH, D_MODEL))
    w_router_group = nrm(ks[13], (DEPTH, D_MODEL, N_GROUPS), D_MODEL)
    b_router_group = 0.01 * jax.random.normal(ks[14], (DEPTH, N_GROUPS), f32)
    w_router_expert = nrm(ks[15], (DEPTH, D_MODEL, N_EXPERTS), D_MODEL)
    b_router_expert = 0.01 * jax.random.normal(ks[16], (DEPTH, N_EXPERTS), f32)
    w_exp_gate = nrm(ks[17], (DEPTH, N_EXPERTS, D_MODEL, D_EXPERT), D_MODEL)
    w_exp_up = nrm(ks[18], (DEPTH, N_EXPERTS, D_MODEL, D_EXPERT), D_MODEL)
    w_exp_down = nrm(ks[19], (DEPTH, N_EXPERTS, D_EXPERT, D_MODEL), D_EXPERT)
    final_norm_g = gain(ks[20], (D_MODEL,))
    return {
        "x": x, "positions": positions, "norm1_g": norm1_g, "w_in": w_in, "b_gate": b_gate,
        "conv_w": conv_w, "a_log": a_log, "dt_bias": dt_bias, "a_norm_g": a_norm_g,
        "w_proj_a": w_proj_a, "w_proj_b": w_proj_b, "w_out": w_out, "norm2_g": norm2_g,
        "w_router_group": w_router_group, "b_router_group": b_router_group,
        "w_router_expert": w_router_expert, "b_router_expert": b_router_expert,
        "w_exp_gate": w_exp_gate, "w_exp_up": w_exp_up, "w_exp_down": w_exp_down,
        "final_norm_g": final_norm_g,
    }


def reference(x, positions, norm1_g, w_in, b_gate, conv_w, a_log, dt_bias, a_norm_g,
              w_proj_a, w_proj_b, w_out, norm2_g, w_router_group, b_router_group,
              w_router_expert, b_router_expert, w_exp_gate, w_exp_up, w_exp_down, final_norm_g):
    b, s, _ = x.shape
    for l in range(DEPTH):
        h = rmsnorm(x, norm1_g[l])
        parts = split_cols(h @ w_in[l])

        qkv = jnp.concatenate([parts["a_q"], parts["a_k"], parts["a_v"]], axis=-1)
        qkv = jax.nn.silu(causal_dwconv(qkv, conv_w[l]))
        aq = l2norm(qkv[..., :A_QK_W].reshape(b, s, A_HEADS, A_DK)) * (A_DK ** -0.5)
        ak = l2norm(qkv[..., A_QK_W:2 * A_QK_W].reshape(b, s, A_HEADS, A_DK))
        av = qkv[..., 2 * A_QK_W:].reshape(b, s, A_HEADS, A_DV).astype(jnp.float32)
        beta = jax.nn.sigmoid(parts["a_beta"].astype(jnp.float32))
        g = -jnp.exp(a_log[l].astype(jnp.float32)) * jax.nn.softplus(
            parts["a_alpha"].astype(jnp.float32) + dt_bias[l].astype(jnp.float32))
        o_a = gated_delta_rule(aq, ak, av, g, beta)
        o_a = rmsnorm(o_a, a_norm_g[l]) * jax.nn.silu(
            parts["a_z"].reshape(b, s, A_HEADS, A_DV).astype(jnp.float32))
        o_a = o_a.reshape(b, s, A_V_W).astype(x.dtype)

        bq = rope(parts["b_q"].reshape(b, s, B_HEADS, B_HD), positions)
        bk = rope(parts["b_k"].reshape(b, s, B_KV_HEADS, B_HD), positions)
        bv = parts["b_v"].reshape(b, s, B_KV_HEADS, B_HD)
        iq = rope(parts["i_q"].reshape(b, s, IDX_HEADS, IDX_HD), positions)
        ik = rope(parts["i_k"][:, :, None, :], positions)[:, :, 0, :]
        iw = parts["i_w"] * ((IDX_HEADS ** -0.5) * (IDX_HD ** -0.5))
        o_b = dsa_attention(bq, bk, bv, iq, ik, iw)

        gate_a = jax.nn.sigmoid(parts["gate_a"] + b_gate[l][:D_MODEL])
        gate_b = jax.nn.sigmoid(parts["gate_b"] + b_gate[l][D_MODEL:])
        merged = gate_a * (o_a @ w_proj_a[l]) + gate_b * (o_b @ w_proj_b[l])
        x = x + merged @ w_out[l]

        x = x + hier_moe(rmsnorm(x, norm2_g[l]), w_router_group[l], b_router_group[l],
                         w_router_expert[l], b_router_expert[l],
                         w_exp_gate[l], w_exp_up[l], w_exp_down[l])
    return rmsnorm(x, final_norm_g)
```

```python
import math
from contextlib import ExitStack
import numpy as np
import concourse.bass as bass
import concourse.mybir as mybir
from concourse.bass_utils import run_bass_kernel_spmd

F32 = mybir.dt.float32
BF16 = mybir.dt.bfloat16
I32 = mybir.dt.int32
AF = mybir.ActivationFunctionType
ALU = mybir.AluOpType
AX = mybir.AxisListType

S = 2048
D = 1024
NT = S // 128
D_IN = 5464
EPS = 1e-6
N_DMA_SEMS = 24
NEG = -30000.0
NBIS = 12
TWO_PI = 2.0 * math.pi

C_AQ, C_AK, C_AV, C_AZ = 0, 512, 1024, 1536
C_BETA, C_ALPHA = 2048, 2056
C_BQ, C_BK, C_BV = 2064, 2576, 2704
C_IQ, C_IK, C_IW = 2832, 3344, 3408
C_GA, C_GB = 3416, 4440

DEBUG = {}
STRICT_SAME_ENGINE = True


class Sched:
    ENGS = ("pe", "act", "dve", "pool", "sp")

    def __init__(self):
        self.ops = {e: [] for e in self.ENGS}
        self.last_w = {}
        self.readers = {}
        self.dma_rr = 0
        self.dma_count = [0] * N_DMA_SEMS

    def op(self, eng, fn, reads=(), writes=(), dma=False, fast=False):
        deps = set()
        raw = set()
        for k in reads:
            t = self.last_w.get(k)
            if t is not None:
                deps.add(t)
                raw.add(t)
        for k in writes:
            t = self.last_w.get(k)
            if t is not None:
                deps.add(t)
            for t in self.readers.get(k, {}).values():
                deps.add(t)
        idx = len(self.ops[eng])
        if dma:
            si = self.dma_rr
            self.dma_rr = (self.dma_rr + 1) % N_DMA_SEMS
            prev = self.dma_count[si]
            if prev > 0:
                deps.add(("dma", si, prev))
            self.dma_count[si] = prev + 1
            tok = ("dma", si, prev + 1)
            rkey = ("dma", si)
        else:
            tok = ("eng", eng, idx)
            rkey = eng
            if STRICT_SAME_ENGINE:
                deps = {t for t in deps if not (t[0] == "eng" and t[1] == eng) or eng != "pe"}
            else:
                deps = {t for t in deps if not (t[0] == "eng" and t[1] == eng)
                        or (t in raw and eng != "pe" and not fast and idx - t[2] <= 8)}
        self.ops[eng].append(dict(fn=fn, deps=deps, signal=False, dma=(tok if dma else None)))
        for k in writes:
            self.last_w[k] = tok
            self.readers[k] = {}
        for k in reads:
            if k in writes:
                continue
            self.readers.setdefault(k, {})[rkey] = tok
        return tok

    def barrier(self):
        toks = set()
        for e in self.ENGS:
            j = len(self.ops[e]) - 1
            while j >= 0 and (self.ops[e][j]["fn"] is None or self.ops[e][j]["dma"] is not None):
                j -= 1
            if j >= 0:
                toks.add(("eng", e, j))
        for si in range(N_DMA_SEMS):
            if self.dma_count[si] > 0:
                toks.add(("dma", si, self.dma_count[si]))
        for e in self.ENGS:
            deps = {t for t in toks if not (t[0] == "eng" and t[1] == e and (e == "pe" or not STRICT_SAME_ENGINE))}
            self.ops[e].append(dict(fn=None, deps=deps, signal=False, dma=None))
        self.last_w = {}
        self.readers = {}

    def emit(self, nc, block, engsem, dmasem):
        for e in self.ENGS:
            for o in self.ops[e]:
                for t in o["deps"]:
                    if t[0] == "eng":
                        self.ops[t[1]][t[2]]["signal"] = True
        sigcount = {}
        for e in self.ENGS:
            c = 0
            lst = []
            for o in self.ops[e]:
                if o["signal"]:
                    c += 1
                lst.append(c)
            sigcount[e] = lst
        self.stats = {e: (len(self.ops[e]), sigcount[e][-1] if sigcount[e] else 0) for e in self.ENGS}

        def run(e, eng):
            waited = {}
            for o in self.ops[e]:
                need = {}
                for t in o["deps"]:
                    if t[0] == "eng":
                        key = ("eng", t[1])
                        val = sigcount[t[1]][t[2]]
                    else:
                        key = ("dma", t[1])
                        val = 16 * t[2]
                    if val > need.get(key, 0):
                        need[key] = val
                for key, val in need.items():
                    if waited.get(key, 0) >= val:
                        continue
                    waited[key] = val
                    sem = engsem[key[1]] if key[0] == "eng" else dmasem[key[1]]
                    eng.wait_ge(sem, val)
                if o["fn"] is None:
                    continue
                inst = o["fn"](eng)
                if o["dma"] is not None:
                    inst.then_inc(dmasem[o["dma"][1]], 16)
                elif o["signal"]:
                    inst.then_inc(engsem[e], 1)

        @block.tensor
        def _(eng):
            run("pe", eng)

        @block.scalar
        def _(eng):
            run("act", eng)

        @block.vector
        def _(eng):
            run("dve", eng)

        @block.gpsimd
        def _(eng):
            run("pool", eng)

        @block.sync
        def _(eng):
            run("sp", eng)


DT_SIZE = {F32: 4, BF16: 2, I32: 4}


def build_nc(debug=(), stop_after=None):
    nc = bass.Bass("TRN2", target_bir_lowering=False)

    def din(name, shape, dt=F32):
        return nc.dram_tensor(name, list(shape), dt, kind="ExternalInput").ap()

    x_d = din("x", [S, D])
    pos_d = din("positions", [128, NT], I32)
    g1_d = din("norm1_g", [128, 8])
    w_in_d = din("w_in", [D, D_IN])
    convw_d = din("conv_w", [128, 48])
    invf_d = din("inv_freq", [1, 32])
    alog_d = din("a_log", [1, 8])
    dtb_d = din("dt_bias", [1, 8])
    ang_d = din("a_norm_g", [1, 64])
    bgate_d = din("b_gate", [128, 16])
    g2_d = din("norm2_g", [128, 8])
    fng_d = din("final_norm_g", [1, D])
    wpa_d = din("w_proj_a", [512, D])
    wpb_d = din("w_proj_b", [512, D])
    wout_d = din("w_out", [D, D])
    wr_d = din("w_router", [D, 20])
    br_d = din("b_router", [1, 20])
    w1_d = din("w_exp_gate", [16, D, 256])
    w3_d = din("w_exp_up", [16, D, 256])
    w2_d = din("w_exp_down", [16, 256, D])
    out_d = nc.dram_tensor("out", [S, D], F32, kind="ExternalOutput").ap()
    dbg = {}
    for name, shape, dt in debug:
        dbg[name] = nc.dram_tensor("dbg_" + name, list(shape), dt, kind="ExternalOutput").ap()
    w_in_v = w_in_d.rearrange("(k p) c -> p k c", p=128)

    sch = Sched()
    op = sch.op
    es = ExitStack()
    with es:
        ARENA_BYTES = 207 * 1024
        arena = es.enter_context(nc.sbuf_tensor("arena", [128, ARENA_BYTES // 4], F32))

        def view(off, shape, dt):
            n = 1
            for s_ in shape[1:]:
                n *= s_
            size = n * DT_SIZE[dt]
            assert off % 4 == 0 and size % 4 == 0 and off + size <= ARENA_BYTES, (off, size)
            ap = arena[:, off // 4:(off + size) // 4]
            if dt != F32:
                ap = ap.bitcast(dt)
            if len(shape) == 3:
                ap = ap.rearrange("p (a b) -> p a b", a=shape[1])
            elif len(shape) == 4:
                ap = ap.rearrange("p (a b c) -> p a b c", a=shape[1], b=shape[2])
            return ap

        class Alloc:
            def __init__(self, base, limit):
                self.off = base
                self.limit = limit

            def __call__(self, shape, dt):
                n = 1
                for s_ in shape[1:]:
                    n *= s_
                size = (n * DT_SIZE[dt] + 63) // 64 * 64
                self.off = (self.off + 63) // 64 * 64
                v = view(self.off, shape, dt)
                self.off += size
                assert self.off <= self.limit, (self.off, self.limit)
                return v

        pbank = [es.enter_context(nc.psum_tensor("pb%d" % i, [128, 512], F32)) for i in range(8)]
        engsem = {e: es.enter_context(nc.semaphore("sem_" + e)) for e in Sched.ENGS}
        dmasem = [es.enter_context(nc.semaphore("dsem%d" % i)) for i in range(N_DMA_SEMS)]

        def PB(i):
            return pbank[i][:]

        def PBb(i):
            return pbank[i][:].bitcast(BF16)

        def body():
            K = 1024
            ca = Alloc(0, 9 * K)
            ident_f = ca([128, 128], F32)
            ident_b = ca([128, 128], BF16)
            ucs_f = ca([128, 128], F32)
            mc0_f = ca([128, 128], F32)
            mc1_f = ca([128, 128], F32)
            maskneg_f = ca([128, 128], F32)
            strict_b = ca([128, 128], BF16)
            g1col = ca([128, 8], F32)
            epsc = ca([128, 1], F32)
            cw = ca([128, 48], F32)
            invf = ca([128, 32], F32)
            dtb = ca([128, 8], F32)
            negA = ca([128, 8], F32)
            angb = ca([128, 64], F32)
            posi = ca([128, NT], I32)
            posf = ca([128, NT], F32)
            cs = ca([128, NT, 64], F32)
            assert ca.off <= 9 * K, ca.off

            op("pool", lambda e: e.memset(ident_f, 1.0), writes=["ident_f"])
            op("pool", lambda e: e.affine_select(out=ident_f, in_=ident_f, pattern=[[-1, 128]], compare_op=ALU.is_equal,
                                                 fill=0.0, base=0, channel_multiplier=1), writes=["ident_f"])
            op("pool", lambda e: e.tensor_copy(out=ident_b, in_=ident_f), reads=["ident_f"], writes=["ident_b"])
            op("pool", lambda e: e.memset(ucs_f, 1.0), writes=["ucs_f"])
            op("pool", lambda e: e.affine_select(out=ucs_f, in_=ucs_f, pattern=[[1, 128]], compare_op=ALU.is_ge,
                                                 fill=0.0, base=0, channel_multiplier=-1), writes=["ucs_f"])
            op("pool", lambda e: e.memset(ucs_f[0:64, 64:128], 0.0), writes=["ucs_f"])
            op("pool", lambda e: e.memset(mc0_f, 0.0), writes=["mc0_f"])
            op("pool", lambda e: e.memset(mc0_f[0:64, :], 1.0), writes=["mc0_f"])
            op("pool", lambda e: e.memset(mc1_f, 0.0), writes=["mc1_f"])
            op("pool", lambda e: e.memset(mc1_f[64:128, :], 1.0), writes=["mc1_f"])
            op("pool", lambda e: e.memset(maskneg_f, 0.0), writes=["maskneg_f"])
            op("pool", lambda e: e.affine_select(out=maskneg_f, in_=maskneg_f, pattern=[[-1, 128]], compare_op=ALU.is_ge,
                                                 fill=NEG, base=0, channel_multiplier=1), writes=["maskneg_f"])
            op("pool", lambda e: e.memset(maskneg_f[64:128, 0:64], NEG), writes=["maskneg_f"])
            op("pool", lambda e: e.memset(strict_b, 1.0), writes=["strict_b"])
            op("pool", lambda e: e.affine_select(out=strict_b, in_=strict_b, pattern=[[-1, 128]], compare_op=ALU.is_gt,
                                                 fill=0.0, base=0, channel_multiplier=1), writes=["strict_b"])
            op("pool", lambda e: e.memset(strict_b[64:128, 0:64], 0.0), writes=["strict_b"])
            op("dve", lambda e: e.memset(epsc, EPS), writes=["epsc"])
            op("sp", lambda e: e.dma_start(out=g1col, in_=g1_d), writes=["g1col"], dma=True)
            op("sp", lambda e: e.dma_start(out=cw, in_=convw_d), writes=["cw"], dma=True)
            op("sp", lambda e: e.dma_start(out=invf, in_=invf_d.partition_broadcast(128)), writes=["invf"], dma=True)
            op("sp", lambda e: e.dma_start(out=dtb, in_=dtb_d.partition_broadcast(128)), writes=["dtb"], dma=True)
            op("sp", lambda e: e.dma_start(out=negA, in_=alog_d.partition_broadcast(128)), writes=["negA"], dma=True)
            op("sp", lambda e: e.dma_start(out=angb, in_=ang_d.partition_broadcast(128)), writes=["angb"], dma=True)
            op("sp", lambda e: e.dma_start(out=posi, in_=pos_d), writes=["posi"], dma=True)
            op("act", lambda e: e.activation(out=negA, in_=negA, func=AF.Exp), reads=["negA"], writes=["negA"])
            op("dve", lambda e: e.tensor_scalar(out=negA, in0=negA, scalar1=-1.0, scalar2=None, op0=ALU.mult),
               reads=["negA"], writes=["negA"])

            R_A = 9 * K
            R_W = R_A + 32 * K
            R_Z = R_W + 16 * K
            R_Q = R_Z + 50 * K
            R_S = R_Q + 48 * K
            hT = view(R_A, [128, 8, S], BF16)
            wstage = view(R_W, [128, 8, 256], F32)
            wbf = [view(R_W + 8 * K + i * 4 * K, [128, 8, 256], BF16) for i in range(2)]
            zqkvT = view(R_Z, [128, 12, S + 4], BF16)
            bqT = view(R_Z, [128, 4, S], BF16)
            iqT = view(R_Z + 16 * K, [128, 4, S], BF16)
            azs = view(R_Z + 32 * K, [128, NT, 512], BF16)
            qkv_tok = view(R_Q, [128, NT, 1536], BF16)
            sa = Alloc(R_S, ARENA_BYTES)
            kz = [[sa([128, S], BF16) for _ in range(2)] for _ in range(2)]
            ikT2 = sa([128, S], BF16)
            bv_tok = sa([128, NT, 130], BF16)
            ab_tok = sa([128, NT, 16], F32)
            iw_tok = sa([128, NT, 8], F32)
            diagw = sa([128, 48, 128], BF16)
            R_WORK = sa.off

            def rope_tables():
                wa = Alloc(R_Q, R_Q + 48 * K)
                ang = wa([128, NT, 32], F32)
                tmp = wa([128, NT, 32], F32)
                ki = wa([128, NT, 32], I32)
                op("dve", lambda e: e.tensor_copy(out=posf, in_=posi), reads=["posi"], writes=["posf"])
                op("dve", lambda e: e.tensor_tensor(out=ang, in0=posf.unsqueeze(2).to_broadcast([128, NT, 32]),
                                                    in1=invf.unsqueeze(1).to_broadcast([128, NT, 32]), op=ALU.mult),
                   reads=["posf", "invf"], writes=["ang"])
                for which, shift in ((1, 0.0), (0, math.pi / 2.0)):
                    dst = cs[:, :, which * 32:(which + 1) * 32]
                    op("dve", lambda e, shift=shift: e.tensor_scalar(out=tmp, in0=ang, scalar1=shift, scalar2=None, op0=ALU.add),
                       reads=["ang"], writes=["rt_tmp"])
                    op("dve", lambda e: e.tensor_scalar(out=ki, in0=tmp, scalar1=1.0 / TWO_PI, scalar2=None, op0=ALU.mult),
                       reads=["rt_tmp"], writes=["rt_ki"])
                    op("dve", lambda e, dst=dst: e.tensor_copy(out=dst, in_=ki), reads=["rt_ki"], writes=["cs"])
                    op("dve", lambda e, dst=dst: e.scalar_tensor_tensor(out=dst, in0=dst, scalar=-TWO_PI, in1=tmp,
                                                                       op0=ALU.mult, op1=ALU.add),
                       reads=["cs", "rt_tmp"], writes=["cs"])
                    op("dve", lambda e, dst=dst: e.tensor_scalar(out=dst, in0=dst, scalar1=math.pi, scalar2=-math.pi,
                                                                op0=ALU.min, op1=ALU.max), reads=["cs"], writes=["cs"])
                    op("act", lambda e, dst=dst: e.activation(out=dst, in_=dst, func=AF.Sin), reads=["cs"], writes=["cs"])

            rope_tables()

            def phase1(hT_dst, keyp):
                wa = Alloc(R_Q + 16 * K, R_Q + 48 * K)
                xt = [wa([128, D], F32) for _ in range(2)]
                hb = [wa([128, D], BF16) for _ in range(2)]
                junk = wa([128, D], BF16)
                ss1 = wa([128, NT], F32)
                rstd1 = wa([128, NT], F32)
                op("dve", lambda e: e.memset(ss1, 0.0), writes=[keyp + "ss1"])
                for tt in range(NT):
                    b = tt % 2
                    op("sp", lambda e, tt=tt, b=b: e.dma_start(out=xt[b], in_=x_d[tt * 128:(tt + 1) * 128, :]),
                       writes=[(keyp + "xt", b)], dma=True)
                    op("act", lambda e, tt=tt, b=b: e.activation(out=junk, in_=xt[b], func=AF.Square,
                                                                 accum_out=ss1[:, tt:tt + 1]),
                       reads=[(keyp + "xt", b), keyp + "ss1"], writes=[keyp + "junk", (keyp + "ss1", tt)])
                    op("act", lambda e, tt=tt: e.activation(out=rstd1[:, tt:tt + 1], in_=ss1[:, tt:tt + 1], func=AF.Sqrt,
                                                            bias=epsc, scale=1.0 / D),
                       reads=[(keyp + "ss1", tt), "epsc"], writes=[(keyp + "rstd1", tt)])
                    op("dve", lambda e, tt=tt: e.reciprocal(out=rstd1[:, tt:tt + 1], in_=rstd1[:, tt:tt + 1]),
                       reads=[(keyp + "rstd1", tt)], writes=[(keyp + "rstd1", tt)])
                    op("dve", lambda e, tt=tt, b=b: e.tensor_scalar(out=hb[b], in0=xt[b], scalar1=rstd1[:, tt:tt + 1],
                                                                    scalar2=None, op0=ALU.mult),
                       reads=[(keyp + "xt", b), (keyp + "rstd1", tt)], writes=[(keyp + "hb", b)])
                    pbv = PBb(tt % 2)
                    for k in range(8):
                        op("pe", lambda e, k=k, b=b, pbv=pbv: e.transpose(out=pbv[:, k * 128:(k + 1) * 128],
                                                                          in_=hb[b][:, k * 128:(k + 1) * 128], identity=ident_b),
                           reads=[(keyp + "hb", b), "ident_b"], writes=[("pb", tt % 2)])
                    op("act", lambda e, tt=tt, pbv=pbv: e.copy(out=hT_dst[:, :, tt * 128:(tt + 1) * 128],
                                                               in_=pbv.rearrange("p (k t) -> p k t", k=8)),
                       reads=[("pb", tt % 2)], writes=[("hT", tt)])

            phase1(hT, "p1")
            if stop_after == "p1":
                sch.barrier()
                return
            ALL_HT = [("hT", tt) for tt in range(NT)]

            wchunk_i = [0]

            def load_w(ranges):
                i = wchunk_i[0]
                wchunk_i[0] += 1
                b = i % 2
                off = 0
                for (c0, w) in ranges:
                    op("sp", lambda e, c0=c0, w=w, off=off: e.dma_start(out=wstage[:, :, off:off + w],
                                                                        in_=w_in_v[:, :, c0:c0 + w]),
                       writes=["wstage"], dma=True)
                    off += w
                tot = off
                op("pool", lambda e, b=b, tot=tot: e.tensor_tensor(out=wbf[b][:, :, 0:tot], in0=wstage[:, :, 0:tot],
                                                                   in1=g1col.unsqueeze(2).to_broadcast([128, 8, tot]),
                                                                   op=ALU.mult),
                   reads=["wstage", "g1col"], writes=[("wbf", b)])
                return wbf[b], ("wbf", b), tot

            for ci in range(48):
                op("pool", lambda e, ci=ci: e.tensor_scalar(out=diagw[:, ci, :], in0=ident_f, scalar1=cw[:, ci:ci + 1],
                                                            scalar2=None, op0=ALU.mult),
                   reads=["ident_f", "cw"], writes=[("diagw", ci)])
            op("pool", lambda e: e.memset(zqkvT[:, :, 0:4], 0.0), writes=["zpad"])

            cva = Alloc(R_WORK, ARENA_BYTES)
            convtmp = [cva([128, 512], BF16) for _ in range(2)]
            evq = [0]

            def evac_copy(out, in_, reads, writes):
                evq[0] += 1
                if evq[0] % 2 == 0:
                    op("act", lambda e: e.copy(out=out, in_=in_), reads=reads, writes=writes)
                else:
                    op("dve", lambda e: e.tensor_copy(out=out, in_=in_), reads=reads, writes=writes)

            pbi = [0]

            def g1_proj(c, wt, wkey, ct):
                for tb in range(4):
                    bk = 2 + (pbi[0] % 2)
                    pbi[0] += 1
                    for k in range(8):
                        op("pe", lambda e, k=k, tb=tb, bk=bk: e.matmul(
                            PB(bk), lhsT=wt[:, k, ct * 128:(ct + 1) * 128], rhs=hT[:, k, tb * 512:(tb + 1) * 512],
                            start=(k == 0), stop=(k == 7)),
                           reads=[wkey] + ALL_HT[tb * 4:tb * 4 + 4], writes=[("pb", bk)])
                    evac_copy(zqkvT[:, c, 4 + tb * 512:4 + (tb + 1) * 512], PB(bk), [("pb", bk)], [("zq", c, tb)])

            def g1_conv(c):
                for tb in range(4):
                    bk = 4 + (tb % 2)
                    for j in range(4):
                        op("pe", lambda e, tb=tb, j=j, bk=bk: e.matmul(
                            PB(bk), lhsT=diagw[:, c * 4 + j, :], rhs=zqkvT[:, c, tb * 512 + j + 1:tb * 512 + j + 1 + 512],
                            start=(j == 0), stop=(j == 3)),
                           reads=[("diagw", c * 4 + j), ("zq", c, tb), "zpad"] + ([("zq", c, tb - 1)] if tb > 0 else []),
                           writes=[("pb", bk)])
                    ctb = tb % 2
                    op("act", lambda e, bk=bk, ctb=ctb: e.activation(out=convtmp[ctb], in_=PB(bk), func=AF.Silu),
                       reads=[("pb", bk)], writes=[("convtmp", ctb)])
                    tbk = 6 + (tb % 2)
                    for q in range(4):
                        op("pe", lambda e, q=q, ctb=ctb, tbk=tbk: e.transpose(out=PBb(tbk)[:, q * 128:(q + 1) * 128],
                                                                              in_=convtmp[ctb][:, q * 128:(q + 1) * 128],
                                                                              identity=ident_b),
                           reads=[("convtmp", ctb), "ident_b"], writes=[("pb", tbk)])
                    op("dve", lambda e, tb=tb, tbk=tbk: e.tensor_copy(
                        out=qkv_tok[:, tb * 4:(tb + 1) * 4, c * 128:(c + 1) * 128],
                        in_=PBb(tbk)[:, 0:512].rearrange("p (q t) -> p q t", q=4)),
                       reads=[("pb", tbk)], writes=[("qkv_tok", tb * 4 + q, c) for q in range(4)])

            prev_c = None
            nxt_w = load_w([(0, 256)])
            for chunk in range(6):
                wt, wkey, _ = nxt_w
                for ct in range(2):
                    c = chunk * 2 + ct
                    g1_proj(c, wt, wkey, ct)
                    if ct == 0:
                        nxt_w = load_w([((chunk + 1) * 256, 256)]) if chunk + 1 < 6 else load_w([(C_AZ, 256)])
                    if prev_c is not None:
                        g1_conv(prev_c)
                    prev_c = c
            g1_conv(prev_c)
            pending_w = [nxt_w]

            if "qkv_tok" in dbg:
                op("sp", lambda e: e.dma_start(out=dbg["qkv_tok"].rearrange("(t p) c -> p t c", p=128), in_=qkv_tok),
                   reads=[("qkv_tok", tt, c) for tt in range(NT) for c in range(12)], writes=["dbg_qkv_tok"], dma=True)
                op("sp", None, reads=["dbg_qkv_tok"])
            sch.barrier()
            if stop_after == "g1":
                return

            rwa = Alloc(cva.off, ARENA_BYTES)
            zr = [rwa([128, 256], F32) for _ in range(2)]
            rt = [rwa([128, 4, 32], F32) for _ in range(4)]
            roped = [rwa([128, 256], BF16) for _ in range(2)]
            op("pool", lambda e: e.memset(bv_tok, 1.0), writes=["bv_ones"])
            for a_ in range(2):
                for b_ in range(2):
                    op("pool", lambda e, a_=a_, b_=b_: e.memset(kz[a_][b_], 0.0), writes=["kz0"])

            def rope_ops(src, nh, dst_views, tt, rkey, wkeys, b):
                sv = src.rearrange("p (h d) -> p h d", h=nh)
                x1 = sv[:, :, 0:32]
                x2 = sv[:, :, 32:64]
                cc = cs[:, tt, 0:32].unsqueeze(1).to_broadcast([128, nh, 32])
                sn = cs[:, tt, 32:64].unsqueeze(1).to_broadcast([128, nh, 32])
                t = [r[:, 0:nh, :] for r in rt]
                op("dve", lambda e: e.tensor_tensor(out=t[0], in0=x1, in1=cc, op=ALU.mult), reads=[rkey, "cs"], writes=[("rt", 0)])
                op("pool", lambda e: e.tensor_tensor(out=t[1], in0=x2, in1=sn, op=ALU.mult), reads=[rkey, "cs"], writes=[("rt", 1)])
                op("pool", lambda e: e.tensor_tensor(out=t[2], in0=x2, in1=cc, op=ALU.mult), reads=[rkey, "cs"], writes=[("rt", 2)])
                op("dve", lambda e: e.tensor_tensor(out=t[3], in0=x1, in1=sn, op=ALU.mult), reads=[rkey, "cs"], writes=[("rt", 3)])
                for i, dv in enumerate(dst_views):
                    eng = "dve" if i % 2 == 0 else "pool"
                    op(eng, lambda e, dv=dv: e.tensor_tensor(out=dv[:, :, 0:32], in0=t[0], in1=t[1], op=ALU.subtract),
                       reads=[("rt", 0), ("rt", 1)], writes=wkeys)
                    op(eng, lambda e, dv=dv: e.tensor_tensor(out=dv[:, :, 32:64], in0=t[2], in1=t[3], op=ALU.add),
                       reads=[("rt", 2), ("rt", 3)], writes=wkeys)

            def tok_chunk(ranges, handler, sel=None, next_ranges=None):
                if pending_w[0] is not None:
                    wt, wkey, tot = pending_w[0]
                    pending_w[0] = None
                else:
                    wt, wkey, tot = load_w(ranges)
                lo, hi = (0, tot) if sel is None else sel
                pend = []
                for tt in range(NT):
                    if tt == 6 and next_ranges is not None:
                        pending_w[0] = load_w(next_ranges)
                    bk = 2 + (tt % 2)
                    for k in range(8):
                        op("pe", lambda e, k=k, tt=tt, bk=bk, wt=wt: e.matmul(
                            PB(bk)[:, 0:hi - lo], lhsT=hT[:, k, tt * 128:(tt + 1) * 128], rhs=wt[:, k, lo:hi],
                            start=(k == 0), stop=(k == 7)),
                           reads=[wkey, ("hT", tt)], writes=[("pb", bk)])
                    if tt >= 1:
                        pend.append(handler(tt - 1, 2 + ((tt - 1) % 2)))
                    if len(pend) >= 2:
                        p2 = pend.pop(0)
                        if p2 is not None:
                            p2()
                pend.append(handler(NT - 1, 2 + ((NT - 1) % 2)))
                for p2 in pend:
                    if p2 is not None:
                        p2()

            for j in range(2):
                def h_az(tt, bk, j=j):
                    op("act", lambda e: e.activation(out=azs[:, tt, j * 256:(j + 1) * 256], in_=PB(bk)[:, 0:256], func=AF.Silu),
                       reads=[("pb", bk)], writes=[("azs", tt, j)])
                tok_chunk([(C_AZ + j * 256, 256)], h_az, next_ranges=[(C_AZ + 256, 256)] if j == 0 else [(C_BQ, 256)])

            if stop_after == "u1":
                return
            for (c0, dstT, nm) in ((C_BQ, bqT, "bqT"), (C_IQ, iqT, "iqT")):
                for j in range(2):
                    def h_q(tt, bk, j=j, dstT=dstT, nm=nm):
                        b = tt % 2
                        op("act", lambda e: e.copy(out=zr[b], in_=PB(bk)[:, 0:256]), reads=[("pb", bk)], writes=[("zr", b)])
                        rope_ops(zr[b], 4, [roped[b].rearrange("p (h d) -> p h d", h=4)], tt, ("zr", b), [("roped", b)], b)
                        tbk = 6 + b

                        def part2():
                            for q in range(2):
                                op("pe", lambda e, q=q: e.transpose(out=PBb(tbk)[:, q * 128:(q + 1) * 128],
                                                                    in_=roped[b][:, q * 128:(q + 1) * 128], identity=ident_b),
                                   reads=[("roped", b), "ident_b"], writes=[("pb", tbk)])
                            op("act", lambda e: e.copy(out=dstT[:, 2 * j:2 * j + 2, tt * 128:(tt + 1) * 128],
                                                       in_=PBb(tbk)[:, 0:256].rearrange("p (q t) -> p q t", q=2)),
                               reads=[("pb", tbk)], writes=[(nm, tt, j)])
                        return part2
                    nr = [(c0 + 256, 256)] if j == 0 else ([(C_IQ, 256)] if c0 == C_BQ else [(C_BK, 256)])
                    tok_chunk([(c0 + j * 256, 256)], h_q, next_ranges=nr)

            if stop_after == "u23":
                return
            def h_kv(tt, bk):
                b = tt % 2
                op("act", lambda e: e.copy(out=zr[b], in_=PB(bk)[:, 0:256]), reads=[("pb", bk)], writes=[("zr", b)])
                rv = roped[b].rearrange("p (h d) -> p h d", h=4)
                rope_ops(zr[b][:, 0:128], 2, [rv[:, 0:2, :]], tt, ("zr", b), [("roped", b)], b)
                op("pool", lambda e: e.tensor_copy(out=rv[:, 2, :], in_=rv[:, 1, :]), reads=[("roped", b)], writes=[("roped", b)])
                op("pool", lambda e: e.tensor_copy(out=rv[:, 3, :], in_=rv[:, 0, :]), reads=[("roped", b)], writes=[("roped", b)])
                def part2():
                    tbk = 6 + b
                    for q in range(2):
                        op("pe", lambda e, q=q: e.transpose(out=PBb(tbk)[:, q * 128:(q + 1) * 128],
                                                            in_=roped[b][:, q * 128:(q + 1) * 128], identity=ident_b),
                           reads=[("roped", b), "ident_b"], writes=[("pb", tbk)])
                    ts_ = slice(tt * 128, (tt + 1) * 128)
                    op("act", lambda e: e.copy(out=kz[0][0][0:64, ts_], in_=PBb(tbk)[0:64, 0:128]), reads=[("pb", tbk), "kz0"],
                       writes=[("bkT", tt)])
                    op("act", lambda e: e.copy(out=kz[1][1][64:128, ts_], in_=PBb(tbk)[64:128, 0:128]), reads=[("pb", tbk)],
                       writes=[("bkT", tt)])
                    op("act", lambda e: e.copy(out=kz[1][0][0:64, ts_], in_=PBb(tbk)[0:64, 128:256]), reads=[("pb", tbk)],
                       writes=[("bkT", tt)])
                    op("act", lambda e: e.copy(out=kz[0][1][64:128, ts_], in_=PBb(tbk)[64:128, 128:256]), reads=[("pb", tbk)],
                       writes=[("bkT", tt)])

                op("dve", lambda e: e.tensor_copy(out=bv_tok[:, tt, 0:64], in_=zr[b][:, 128:192]), reads=[("zr", b), "bv_ones"],
                   writes=[("bv", tt)])
                op("dve", lambda e: e.tensor_copy(out=bv_tok[:, tt, 65:129], in_=zr[b][:, 192:256]), reads=[("zr", b)],
                   writes=[("bv", tt)])
                return part2
            tok_chunk([(C_BK, 256)], h_kv, next_ranges=[(C_IW + 8 - 256, 256)])

            if stop_after == "u4a":
                return
            IW_SCALE = (8 ** -0.5) * (64 ** -0.5)

            def h_small(tt, bk):
                b = tt % 2
                op("act", lambda e: e.copy(out=zr[b][:, 0:72], in_=PB(bk)[:, 0:72]), reads=[("pb", bk)], writes=[("zr", b)])
                rv = roped[b].rearrange("p (h d) -> p h d", h=4)
                rope_ops(zr[b][:, 0:64], 1, [rv[:, 0:1, :], rv[:, 1:2, :]], tt, ("zr", b), [("roped", b)], b)
                def part2():
                    tbk = 6 + b
                    op("pe", lambda e: e.transpose(out=PBb(tbk)[:, 0:128], in_=roped[b][:, 0:128], identity=ident_b),
                       reads=[("roped", b), "ident_b"], writes=[("pb", tbk)])
                    op("act", lambda e: e.copy(out=ikT2[:, tt * 128:(tt + 1) * 128], in_=PBb(tbk)[:, 0:128]),
                       reads=[("pb", tbk)], writes=[("ikT", tt)])

                op("dve", lambda e: e.tensor_scalar(out=iw_tok[:, tt, :], in0=zr[b][:, 64:72], scalar1=IW_SCALE, scalar2=None,
                                                    op0=ALU.mult), reads=[("zr", b)], writes=[("iw", tt)])
                return part2
            tok_chunk([(C_IW + 8 - 256, 256)], h_small, sel=(184, 256), next_ranges=[(C_BETA, 256)])

            def h_ab(tt, bk):
                op("act", lambda e: e.copy(out=ab_tok[:, tt, :], in_=PB(bk)[:, 0:16]), reads=[("pb", bk)], writes=[("ab", tt)])
            tok_chunk([(C_BETA, 256)], h_ab, sel=(0, 16))

            for nm, t_, shape in (("bqT", bqT, None), ("iqT", iqT, None)):
                if nm in dbg:
                    op("sp", lambda e, nm=nm, t_=t_: e.dma_start(out=dbg[nm].rearrange("(a p) t -> p a t", p=128), in_=t_),
                       reads=[(nm, tt, j) for tt in range(NT) for j in range(2)], writes=["dbg_" + nm], dma=True)
                    op("sp", None, reads=["dbg_" + nm])
            if "misc" in dbg:
                sch.barrier()
                mt = view(R_W, [128, NT, 154], F32)
                op("dve", lambda e: e.tensor_copy(out=mt[:, :, 0:16], in_=ab_tok), reads=[("ab", tt) for tt in range(NT)], writes=["mt"])
                op("dve", lambda e: e.tensor_copy(out=mt[:, :, 16:24], in_=iw_tok), reads=[("iw", tt) for tt in range(NT)], writes=["mt"])
                op("dve", lambda e: e.tensor_copy(out=mt[:, :, 24:154], in_=bv_tok), reads=[("bv", tt) for tt in range(NT)], writes=["mt"])
                op("sp", lambda e: e.dma_start(out=dbg["misc"].rearrange("(t p) c -> p t c", p=128), in_=mt), reads=["mt"],
                   writes=["dbg_misc"], dma=True)
                op("sp", None, reads=["dbg_misc"])
            sch.barrier()
            if stop_after == "p2":
                return

            class MultiAlloc:
                def __init__(self, regions):
                    self.regs = [[a, b] for a, b in regions]

                def __call__(self, shape, dt):
                    n = 1
                    for s_ in shape[1:]:
                        n *= s_
                    size = (n * DT_SIZE[dt] + 63) // 64 * 64
                    for r in self.regs:
                        r[0] = (r[0] + 63) // 64 * 64
                        if r[0] + size <= r[1]:
                            v = view(r[0], shape, dt)
                            r[0] += size
                            return v
                    raise AssertionError(("MultiAlloc out of space", shape, self.regs))

            def dump(name, ap, reads):
                if name in dbg:
                    op("sp", lambda e: e.dma_start(out=dbg[name], in_=ap), reads=reads, writes=["dbg_" + name], dma=True)
                    op("sp", None, reads=["dbg_" + name])

            o_aT = view(R_A, [128, 4, S], BF16)
            diagw_off = R_WORK - 12 * K
            ga = MultiAlloc([(R_W, R_W + 16 * K), (R_A + 16 * K, R_A + 32 * K), (diagw_off, ARENA_BYTES)])
            g_all = ga([128, NT, 8], F32)
            bet = ga([128, NT, 8], F32)
            gs = ga([128, 24], F32)
            eG = ga([128, 8], F32)
            eGlmG = ga([128, 8], F32)
            scs = [ga([128, 4], F32) for _ in range(2)]
            g_bc = ga([128, 8, 128], F32)
            sq = ga([128, 1024], F32)
            ssn = ga([128, 16], F32)
            rn = ga([128, 16], F32)
            cq = ga([128, 8], F32)
            cqd = ga([128, 8], F32)
            cbk = ga([128, 8], F32)
            ckd = ga([128, 8], F32)
            negbeta = ga([128, 8], F32)
            qn = ga([128, 512], BF16)
            qd = ga([128, 512], BF16)
            kn = ga([128, 512], BF16)
            rhsk = ga([128, 512], BF16)
            kdec = ga([128, 512], BF16)
            rhsv = ga([128, 512], BF16)
            qnT = ga([128, 4, 128], BF16)
            qdT = ga([128, 4, 128], BF16)
            knT = ga([128, 4, 128], BF16)
            Dm = ga([128, 8, 128], BF16)
            Ds = ga([128, 8, 128], BF16)
            Mm = [ga([128, 8, 128], BF16) for _ in range(2)]
            Nm = [ga([128, 8, 128], BF16) for _ in range(2)]
            Pm = [ga([128, 8, 128], BF16) for _ in range(2)]
            qkm = ga([128, 8, 128], BF16)
            qkT_sb = ga([128, 8, 128], BF16)
            u_c = ga([128, 2, 512], F32)
            w_tok = ga([128, 512], BF16)
            wT_sb = ga([128, 4, 128], BF16)
            vn_b = ga([128, 512], BF16)
            Sst = ga([128, 4, 128], F32)
            Stmp = ga([128, 4, 128], F32)
            S_bd = ga([128, 4, 128], BF16)
            bdmask = ga([128, 4, 128], BF16)
            o_c = ga([128, 512], F32)
            qkT_c1 = ga([128, 8, 64], BF16)
            kdec_c1 = ga([128, 512], BF16)
            az_c = ga([128, 512], BF16)
            ss2 = ga([128, 8], F32)
            r2 = ga([128, 8], F32)
            oa_b = ga([128, 512], BF16)

            def bc8(v):
                return v.unsqueeze(2).to_broadcast([128, 8, 64])

            ABK = [("ab", tt) for tt in range(NT)]
            op("act", lambda e: e.activation(out=bet, in_=ab_tok[:, :, 0:8], func=AF.Sigmoid), reads=["ab_all"], writes=["bet"])
            op("dve", lambda e: e.tensor_tensor(out=g_all, in0=ab_tok[:, :, 8:16], in1=dtb.unsqueeze(1).to_broadcast([128, NT, 8]),
                                                op=ALU.add), reads=["ab_all", "dtb"], writes=["g_all"])
            op("act", lambda e: e.activation(out=g_all, in_=g_all, func=AF.Exp), reads=["g_all"], writes=["g_all"])
            op("act", lambda e: e.activation(out=g_all, in_=g_all, func=AF.Ln, bias=1.0), reads=["g_all"], writes=["g_all"])
            op("dve", lambda e: e.tensor_tensor(out=g_all, in0=g_all, in1=negA.unsqueeze(1).to_broadcast([128, NT, 8]),
                                                op=ALU.mult), reads=["g_all", "negA"], writes=["g_all"])
            op("dve", lambda e: e.memset(Sst, 0.0), writes=["S"])
            op("dve", lambda e: e.memset(S_bd, 0.0), writes=["S_bd"])
            op("pool", lambda e: e.memset(bdmask, 0.0), writes=["bdmask"])
            op("pool", lambda e: e.memset(bdmask[0:64, :, 0:64], 1.0), writes=["bdmask"])
            op("pool", lambda e: e.memset(bdmask[64:128, :, 64:128], 1.0), writes=["bdmask"])
            if "g" in dbg:
                op("sp", lambda e: e.dma_start(out=dbg["g"].rearrange("(t p) c -> p t c", p=128), in_=g_all), reads=["g_all"],
                   writes=["dbg_g"], dma=True)
                op("sp", None, reads=["dbg_g"])

            if stop_after == "gdn_pre":
                return
            for tt in range(NT):
                op("pe", lambda e, tt=tt: e.matmul(PB(0)[:, 0:8], lhsT=ucs_f, rhs=g_all[:, tt, :], start=True, stop=True),
                   reads=["g_all"], writes=[("pb", 0)])
                op("pe", lambda e, tt=tt: e.matmul(PB(0)[:, 8:16], lhsT=mc0_f, rhs=g_all[:, tt, :], start=True, stop=True),
                   reads=["g_all"], writes=[("pb", 0)])
                op("pe", lambda e, tt=tt: e.matmul(PB(0)[:, 16:24], lhsT=mc1_f, rhs=g_all[:, tt, :], start=True, stop=True),
                   reads=["g_all"], writes=[("pb", 0)])
                op("act", lambda e: e.copy(out=gs, in_=PB(0)[:, 0:24]), reads=[("pb", 0)], writes=["gs"])
                op("act", lambda e: e.activation(out=eG, in_=gs[:, 0:8], func=AF.Exp), reads=["gs"], writes=["eG"])
                op("dve", lambda e: e.tensor_tensor(out=eGlmG[0:64, :], in0=gs[0:64, 8:16], in1=gs[0:64, 0:8], op=ALU.subtract),
                   reads=["gs"], writes=["eGlmG"])
                op("dve", lambda e: e.tensor_tensor(out=eGlmG[64:128, :], in0=gs[64:128, 16:24], in1=gs[64:128, 0:8],
                                                    op=ALU.subtract), reads=["gs"], writes=["eGlmG"])
                op("act", lambda e: e.activation(out=eGlmG, in_=eGlmG, func=AF.Exp), reads=["eGlmG"], writes=["eGlmG"])
                for hf in range(2):
                    c0 = 8 + 8 * hf
                    op("act", lambda e, hf=hf, c0=c0: e.activation(out=scs[hf][0:64, :], in_=gs[0:64, c0:c0 + 8:2], func=AF.Exp),
                       reads=["gs"], writes=[("scs", hf)])
                    op("act", lambda e, hf=hf, c0=c0: e.activation(out=scs[hf][64:128, :], in_=gs[64:128, c0 + 1:c0 + 8:2],
                                                                   func=AF.Exp), reads=["gs"], writes=[("scs", hf)])
                op("dve", lambda e, tt=tt: e.tensor_scalar(out=g_bc, in0=g_all[:, tt, :].unsqueeze(2).to_broadcast([128, 8, 128]),
                                                           scalar1=-1.0, scalar2=None, op0=ALU.mult),
                   reads=["g_all"], writes=["g_bc"])
                if stop_after == "gdn_a":
                    return
                QK = [("qkv_tok", tt, c) for c in range(8)]
                VV = [("qkv_tok", tt, c) for c in range(8, 12)]
                op("dve", lambda e, tt=tt: e.tensor_tensor(out=sq, in0=qkv_tok[:, tt, 0:1024], in1=qkv_tok[:, tt, 0:1024],
                                                           op=ALU.mult), reads=["qkv_all"], writes=["sq"])
                op("dve", lambda e: e.tensor_reduce(out=ssn, in_=sq.rearrange("p (h d) -> p h d", h=16), axis=AX.X, op=ALU.add),
                   reads=["sq"], writes=["ssn"])
                op("act", lambda e: e.activation(out=rn, in_=ssn, func=AF.Sqrt, bias=epsc, scale=1.0), reads=["ssn", "epsc"],
                   writes=["rn"])
                op("dve", lambda e: e.reciprocal(out=rn, in_=rn), reads=["rn"], writes=["rn"])
                op("dve", lambda e: e.tensor_scalar(out=cq, in0=rn[:, 0:8], scalar1=0.125, scalar2=None, op0=ALU.mult),
                   reads=["rn"], writes=["cq"])
                op("dve", lambda e: e.tensor_tensor(out=cqd, in0=cq, in1=eG, op=ALU.mult), reads=["cq", "eG"], writes=["cqd"])
                op("dve", lambda e, tt=tt: e.tensor_tensor(out=cbk, in0=rn[:, 8:16], in1=bet[:, tt, :], op=ALU.mult),
                   reads=["rn", "bet"], writes=["cbk"])
                op("dve", lambda e: e.tensor_tensor(out=cbk, in0=cbk, in1=eG, op=ALU.mult), reads=["cbk", "eG"], writes=["cbk"])
                op("dve", lambda e: e.tensor_tensor(out=ckd, in0=rn[:, 8:16], in1=eGlmG, op=ALU.mult), reads=["rn", "eGlmG"],
                   writes=["ckd"])
                op("dve", lambda e, tt=tt: e.tensor_scalar(out=negbeta, in0=bet[:, tt, :], scalar1=-1.0, scalar2=None,
                                                           op0=ALU.mult), reads=["bet"], writes=["negbeta"])
                qv = qkv_tok[:, tt, 0:512].rearrange("p (h d) -> p h d", h=8)
                kv = qkv_tok[:, tt, 512:1024].rearrange("p (h d) -> p h d", h=8)
                vv = qkv_tok[:, tt, 1024:1536].rearrange("p (h d) -> p h d", h=8)

                def v3(t_):
                    return t_.rearrange("p (h d) -> p h d", h=8)
                op("dve", lambda e, qv=qv: e.tensor_tensor(out=v3(qn), in0=qv, in1=bc8(cq), op=ALU.mult),
                   reads=["qkv_all", "cq"], writes=["qn"])
                op("pool", lambda e, qv=qv: e.tensor_tensor(out=v3(qd), in0=qv, in1=bc8(cqd), op=ALU.mult),
                   reads=["qkv_all", "cqd"], writes=["qd"])
                op("dve", lambda e, kv=kv: e.tensor_tensor(out=v3(kn), in0=kv, in1=bc8(rn[:, 8:16]), op=ALU.mult),
                   reads=["qkv_all", "rn"], writes=["kn"])
                op("pool", lambda e, kv=kv: e.tensor_tensor(out=v3(rhsk), in0=kv, in1=bc8(cbk), op=ALU.mult),
                   reads=["qkv_all", "cbk"], writes=["rhsk"])
                op("pool", lambda e, kv=kv: e.tensor_tensor(out=v3(kdec), in0=kv, in1=bc8(ckd), op=ALU.mult),
                   reads=["qkv_all", "ckd"], writes=["kdec"])
                op("dve", lambda e, vv=vv, tt=tt: e.tensor_tensor(out=v3(rhsv), in0=vv, in1=bc8(bet[:, tt, :]), op=ALU.mult),
                   reads=["qkv_all", "bet"], writes=["rhsv"])
                if stop_after == "gdn_b":
                    return
                for (src, skey, dst, dkey, bank, coff, eng) in ((qn, "qn", qnT, "qnT", 6, 0, "act"), (qd, "qd", qdT, "qdT", 7, 0, "dve"),
                                                                (kn, "kn", knT, "knT", 0, 0, "act")):
                    for q in range(4):
                        op("pe", lambda e, src=src, bank=bank, coff=coff, q=q: e.transpose(
                            out=PBb(bank)[:, coff + q * 128:coff + (q + 1) * 128], in_=src[:, q * 128:(q + 1) * 128], identity=ident_b),
                           reads=[skey, "ident_b"], writes=[("pb", bank)])
                    if eng == "act":
                        op("act", lambda e, dst=dst, bank=bank, coff=coff: e.copy(
                            out=dst, in_=PBb(bank)[:, coff:coff + 512].rearrange("p (q t) -> p q t", q=4)),
                           reads=[("pb", bank)], writes=[dkey])
                    else:
                        op("dve", lambda e, dst=dst, bank=bank, coff=coff: e.tensor_copy(
                            out=dst, in_=PBb(bank)[:, coff:coff + 512].rearrange("p (q t) -> p q t", q=4)),
                           reads=[("pb", bank)], writes=[dkey])
                    if stop_after == "gdn_c_" + skey:
                        return
                if tt == 0:
                    dump("qn0", qn, ["qn"]); dump("kn0", kn, ["kn"]); dump("rhsv0", rhsv, ["rhsv"]); dump("rhsk0", rhsk, ["rhsk"])
                    dump("kdec0", kdec, ["kdec"]); dump("qd0", qd, ["qd"]); dump("gs0", gs, ["gs"])
                    dump("knT0", knT.rearrange("p a t -> p (a t)"), ["knT"])
                    dump("rn0", rn, ["rn"]); dump("cq0", cq, ["cq"]); dump("cqd0", cqd, ["cqd"]); dump("cbk0", cbk, ["cbk"])
                    dump("ckd0", ckd, ["ckd"]); dump("eG0", eG, ["eG"]); dump("ssn0", ssn, ["ssn"])
                if stop_after == "gdn_c":
                    return
                def group_gen(hg, bA, bB, bC, bT):
                    hs_list = list(range(4))
                    grp = slice(4 * hg, 4 * hg + 4)
                    for hs in hs_list:
                        h = 4 * hg + hs
                        hp, par = h // 2, h % 2
                        rows = slice(par * 64, par * 64 + 64)
                        cs_ = slice(hs * 128, hs * 128 + 128)
                        op("pe", lambda e, hp=hp, rows=rows, cs_=cs_, par=par: e.matmul(
                            PB(bA)[:, cs_], lhsT=knT[rows, hp, :], rhs=knT[rows, hp, :], start=True, stop=True,
                            tile_position=(par * 64, 0)), reads=["knT"], writes=[("pb", bA)])
                        op("pe", lambda e, hp=hp, rows=rows, cs_=cs_, par=par: e.matmul(
                            PB(bB)[:, cs_], lhsT=qnT[rows, hp, :], rhs=knT[rows, hp, :], start=True, stop=True,
                            tile_position=(par * 64, 0)), reads=["knT", "qnT"], writes=[("pb", bB)])
                        op("pe", lambda e, h=h, cs_=cs_: e.matmul(PB(bC)[:, cs_], lhsT=g_bc[:, h, :], rhs=ucs_f, start=True, stop=False),
                           reads=["g_bc", "ucs_f"], writes=[("pb", bC)])
                        op("pe", lambda e, cs_=cs_: e.matmul(PB(bC)[:, cs_], lhsT=ident_f, rhs=maskneg_f, start=False, stop=True),
                           reads=["ident_f", "maskneg_f"], writes=[("pb", bC)])
                    yield
                    for hs in hs_list:
                        h = 4 * hg + hs
                        cs_ = slice(hs * 128, hs * 128 + 128)
                        op("act", lambda e, h=h, cs_=cs_: e.activation(out=Dm[:, h, :], in_=PB(bC)[:, cs_], func=AF.Exp,
                                                                       bias=gs[:, h:h + 1], scale=1.0),
                           reads=[("pb", bC), "gs"], writes=[("Dm", h)])
                        op("pool", lambda e, h=h: e.tensor_tensor(out=Ds[:, h, :], in0=Dm[:, h, :], in1=strict_b, op=ALU.mult),
                           reads=[("Dm", h), "strict_b"], writes=[("Ds", h)])
                        op("dve", lambda e, h=h, cs_=cs_: e.scalar_tensor_tensor(out=Mm[0][:, h, :], in0=PB(bA)[:, cs_],
                                                                                 scalar=negbeta[:, h:h + 1], in1=Ds[:, h, :],
                                                                                 op0=ALU.mult, op1=ALU.mult),
                           reads=[("pb", bA), "negbeta", ("Ds", h)], writes=[("M", 0, hg)])
                        op("dve", lambda e, h=h, cs_=cs_: e.tensor_tensor(out=qkm[:, h, :], in0=PB(bB)[:, cs_], in1=Dm[:, h, :],
                                                                          op=ALU.mult),
                           reads=[("pb", bB), ("Dm", h)], writes=[("qkm", hg)])
                    yield
                    for hs in hs_list:
                        h = 4 * hg + hs
                        cs_ = slice(hs * 128, hs * 128 + 128)
                        op("pe", lambda e, h=h, cs_=cs_: e.transpose(out=PBb(bT)[:, cs_], in_=Mm[0][:, h, :], identity=ident_b),
                           reads=[("M", 0, hg), "ident_b"], writes=[("pb", bT)])
                    op("act", lambda e: e.copy(out=Nm[0][:, grp, :], in_=PBb(bT)[:, 0:512].rearrange("p (q t) -> p q t", q=4)),
                       reads=[("pb", bT)], writes=[("N", 0, hg)])
                    op("pool", lambda e: e.tensor_tensor(out=Pm[0][:, grp, :], in0=Nm[0][:, grp, :],
                                                         in1=ident_b.unsqueeze(1).to_broadcast([128, 4, 128]), op=ALU.add),
                       reads=[("N", 0, hg), "ident_b"], writes=[("P", 0, hg)])
                    yield
                    for hs in hs_list:
                        h = 4 * hg + hs
                        cs2 = slice(hs * 128, hs * 128 + 128)
                        op("pe", lambda e, h=h, cs2=cs2: e.transpose(out=PBb(bT)[:, cs2], in_=qkm[:, h, :], identity=ident_b),
                           reads=[("qkm", hg), "ident_b"], writes=[("pb", bT)])
                    op("dve", lambda e: e.tensor_copy(out=qkT_sb[:, grp, :], in_=PBb(bT)[:, 0:512].rearrange("p (q t) -> p q t", q=4)),
                       reads=[("pb", bT)], writes=[("qkT", hg)])
                    yield
                    for lv in range(1, 6):
                        cur, nxt = (lv - 1) % 2, lv % 2
                        for hs in hs_list:
                            h = 4 * hg + hs
                            cs_ = slice(hs * 128, hs * 128 + 128)
                            op("pe", lambda e, h=h, cs_=cs_, cur=cur: e.matmul(PB(bA)[:, cs_], lhsT=Nm[cur][:, h, :], rhs=Mm[cur][:, h, :],
                                                                               start=True, stop=True),
                               reads=[("N", cur, hg), ("M", cur, hg)], writes=[("pb", bA)])
                        if lv < 5:
                            for hs in hs_list:
                                h = 4 * hg + hs
                                cs_ = slice(hs * 128, hs * 128 + 128)
                                op("pe", lambda e, h=h, cs_=cs_, cur=cur: e.matmul(PB(bB)[:, cs_], lhsT=Mm[cur][:, h, :],
                                                                                   rhs=Nm[cur][:, h, :], start=True, stop=True),
                                   reads=[("N", cur, hg), ("M", cur, hg)], writes=[("pb", bB)])
                        yield
                        op("act", lambda e, nxt=nxt: e.copy(out=Mm[nxt][:, grp, :], in_=PB(bA).rearrange("p (q t) -> p q t", q=4)),
                           reads=[("pb", bA)], writes=[("M", nxt, hg)])
                        if lv < 5:
                            op("dve", lambda e, nxt=nxt: e.tensor_copy(out=Nm[nxt][:, grp, :],
                                                                       in_=PB(bB).rearrange("p (q t) -> p q t", q=4)),
                               reads=[("pb", bB)], writes=[("N", nxt, hg)])
                        for hs in hs_list:
                            h = 4 * hg + hs
                            cs_ = slice(hs * 128, hs * 128 + 128)
                            op("pe", lambda e, h=h, cs_=cs_, cur=cur, nxt=nxt: e.matmul(PB(bC)[:, cs_], lhsT=Mm[nxt][:, h, :],
                                                                                        rhs=Pm[cur][:, h, :], start=True, stop=True),
                               reads=[("M", nxt, hg), ("P", cur, hg)], writes=[("pb", bC)])
                        yield
                        op("dve", lambda e, cur=cur, nxt=nxt: e.tensor_tensor(
                            out=Pm[nxt][:, grp, :], in0=Pm[cur][:, grp, :], in1=PB(bC).rearrange("p (q t) -> p q t", q=4), op=ALU.add),
                           reads=[("pb", bC), ("P", cur, hg)], writes=[("P", nxt, hg)])

                gens = [group_gen(0, 3, 4, 5, 6), group_gen(1, 0, 1, 2, 7)]
                while gens:
                    for g_ in list(gens):
                        try:
                            next(g_)
                        except StopIteration:
                            gens.remove(g_)
                Pf = Pm[1]
                PK = [("P", 1, 0), ("P", 1, 1)]
                if tt == 0:
                    dump("D0", Dm.rearrange("p a t -> p (a t)"), [("Dm", h) for h in range(8)])
                    dump("M0", Mm[0].rearrange("p a t -> p (a t)"), [("M", 0, 0), ("M", 0, 1)])
                    dump("N0", Nm[0].rearrange("p a t -> p (a t)"), [("N", 0, 0), ("N", 0, 1)])
                    dump("P0", Pm[1].rearrange("p a t -> p (a t)"), [("P", 1, 0), ("P", 1, 1)])
                    dump("qkT0", qkT_sb.rearrange("p a t -> p (a t)"), [("qkT", 0), ("qkT", 1)])
                if stop_after == "gdn_e":
                    return
                for hf in range(2):
                    ub = 7 if hf == 0 else 0
                    for h in range(8):
                        op("pe", lambda e, h=h, hf=hf, ub=ub: e.matmul(PB(ub)[0:64, h * 64:(h + 1) * 64],
                                                                       lhsT=Pf[:, h, hf * 64:(hf + 1) * 64],
                                                                       rhs=rhsv[:, h * 64:(h + 1) * 64], start=True, stop=True),
                           reads=PK + ["rhsv"], writes=[("pb", ub)])
                    op("act", lambda e, hf=hf, ub=ub: e.copy(out=u_c[0:64, hf, :], in_=PB(ub)[0:64, :]), reads=[("pb", ub)],
                       writes=[("u_c", hf)])
                for h in range(8):
                    op("pe", lambda e, h=h: e.matmul(PB(1)[:, h * 64:(h + 1) * 64], lhsT=Pf[:, h, :], rhs=rhsk[:, h * 64:(h + 1) * 64],
                                                     start=True, stop=True), reads=PK + ["rhsk"], writes=[("pb", 1)])
                op("act", lambda e: e.copy(out=w_tok, in_=PB(1)), reads=[("pb", 1)], writes=["w_tok"])
                for q in range(4):
                    op("pe", lambda e, q=q: e.transpose(out=PBb(6)[:, q * 128:(q + 1) * 128], in_=w_tok[:, q * 128:(q + 1) * 128],
                                                        identity=ident_b), reads=["w_tok", "ident_b"], writes=[("pb", 6)])
                op("dve", lambda e: e.tensor_copy(out=wT_sb, in_=PBb(6)[:, 0:512].rearrange("p (q t) -> p q t", q=4)),
                   reads=[("pb", 6)], writes=["wT_sb"])
                op("sp", lambda e: e.dma_start(out=qkT_c1[0:64, :, :], in_=qkT_sb[64:128, :, 64:128]),
                   reads=[("qkT", 0), ("qkT", 1)], writes=["qkT_c1"], dma=True)
                op("sp", lambda e: e.dma_start(out=kdec_c1[0:64, :], in_=kdec[64:128, :]), reads=["kdec"], writes=["kdec_c1"],
                   dma=True)
                op("sp", lambda e, tt=tt: e.dma_start(out=az_c[0:64, :], in_=azs[64:128, tt, :]), reads=["azs_all"], writes=["az_c"],
                   dma=True)
                if tt == 0:
                    dump("u0", u_c.rearrange("p a t -> p (a t)"), [("u_c", 0), ("u_c", 1)])
                    dump("wT0", wT_sb.rearrange("p a t -> p (a t)"), ["wT_sb"])
                if stop_after == "gdn_f":
                    return
                for hf in range(2):
                    tcs = slice(hf * 64, hf * 64 + 64)
                    ck = 2 * tt + hf
                    if hf == 0:
                        qk_x, qk_keys = qkT_sb[0:64, :, 0:64], [("qkT", 0), ("qkT", 1)]
                        kd_x, kd_keys = kdec[0:64, :], ["kdec"]
                    else:
                        qk_x, qk_keys = qkT_c1[0:64, :, :], ["qkT_c1"]
                        kd_x, kd_keys = kdec_c1[0:64, :], ["kdec_c1"]
                    for hp in range(4):
                        op("pe", lambda e, hp=hp, tcs=tcs: e.matmul(PB(1)[0:64, hp * 128:(hp + 1) * 128], lhsT=wT_sb[:, hp, tcs],
                                                                    rhs=S_bd[:, hp, :], start=True, stop=True),
                           reads=["wT_sb", "S_bd"], writes=[("pb", 1)])
                    op("dve", lambda e, hf=hf: e.tensor_tensor(out=vn_b[0:64, :], in0=u_c[0:64, hf, :], in1=PB(1)[0:64, :],
                                                               op=ALU.subtract), reads=[("u_c", hf), ("pb", 1)], writes=["vn_b"])
                    for h in range(8):
                        hp, par = h // 2, h % 2
                        op("pe", lambda e, h=h, hp=hp, par=par, tcs=tcs: e.matmul(
                            PB(2)[0:64, h * 64:(h + 1) * 64], lhsT=qdT[:, hp, tcs], rhs=S_bd[:, hp, par * 64:(par + 1) * 64],
                            start=True, stop=False), reads=["qdT", "S_bd"], writes=[("pb", 2)])
                        op("pe", lambda e, h=h, qk_x=qk_x: e.matmul(
                            PB(2)[0:64, h * 64:(h + 1) * 64], lhsT=qk_x[:, h, :], rhs=vn_b[0:64, h * 64:(h + 1) * 64],
                            start=False, stop=True), reads=qk_keys + ["vn_b"], writes=[("pb", 2)])
                    for hp in range(4):
                        op("pe", lambda e, hp=hp, kd_x=kd_x: e.matmul(PB(7)[:, hp * 128:(hp + 1) * 128],
                                                                      lhsT=kd_x[:, hp * 128:(hp + 1) * 128],
                                                                      rhs=vn_b[0:64, hp * 128:(hp + 1) * 128], start=True, stop=True),
                           reads=kd_keys + ["vn_b"], writes=[("pb", 7)])
                    op("act", lambda e: e.copy(out=o_c[0:64, :], in_=PB(2)[0:64, :]), reads=[("pb", 2)], writes=["o_c"])
                    for hp in range(4):
                        op("act", lambda e, hf=hf, hp=hp: e.activation(out=Stmp[:, hp, :], in_=Sst[:, hp, :], func=AF.Identity,
                                                                       scale=scs[hf][:, hp:hp + 1]),
                           reads=["S", ("scs", hf)], writes=["Stmp"])
                    op("dve", lambda e: e.tensor_tensor(out=Sst, in0=Stmp, in1=PB(7).rearrange("p (a d) -> p a d", a=4), op=ALU.add),
                       reads=["Stmp", ("pb", 7)], writes=["S"])
                    op("dve", lambda e: e.tensor_tensor(out=S_bd, in0=Sst, in1=bdmask, op=ALU.mult), reads=["S", "bdmask"],
                       writes=["S_bd"])
                    if "o_raw" in dbg:
                        op("sp", lambda e, ck=ck: e.dma_start(out=dbg["o_raw"][ck * 64:(ck + 1) * 64, :], in_=o_c[0:64, :]),
                           reads=["o_c"], writes=["dbg_o_raw"], dma=True)
                    sqh = sq[0:64, 0:512]
                    op("dve", lambda e: e.tensor_tensor(out=sqh, in0=o_c[0:64, :], in1=o_c[0:64, :], op=ALU.mult), reads=["o_c"],
                       writes=["sq"])
                    op("dve", lambda e: e.tensor_reduce(out=ss2[0:64, :], in_=sqh.rearrange("p (h d) -> p h d", h=8), axis=AX.X,
                                                        op=ALU.add), reads=["sq"], writes=["ss2"])
                    op("act", lambda e: e.activation(out=r2[0:64, :], in_=ss2[0:64, :], func=AF.Sqrt, bias=epsc[0:64, :],
                                                     scale=1.0 / 64), reads=["ss2", "epsc"], writes=["r2"])
                    op("dve", lambda e: e.reciprocal(out=r2[0:64, :], in_=r2[0:64, :]), reads=["r2"], writes=["r2"])
                    op("dve", lambda e: e.tensor_tensor(out=v3(sqh), in0=v3(o_c[0:64, :]),
                                                        in1=r2[0:64, :].unsqueeze(2).to_broadcast([64, 8, 64]), op=ALU.mult),
                       reads=["o_c", "r2"], writes=["sq"])
                    op("pool", lambda e: e.tensor_tensor(out=v3(sqh), in0=v3(sqh),
                                                         in1=angb[0:64, :].unsqueeze(1).to_broadcast([64, 8, 64]), op=ALU.mult),
                       reads=["sq", "angb"], writes=["sq"])
                    az_x = azs[0:64, tt, :] if hf == 0 else az_c[0:64, :]
                    op("pool", lambda e, az_x=az_x: e.tensor_tensor(out=oa_b[0:64, :], in0=sqh, in1=az_x, op=ALU.mult),
                       reads=["sq", "az_c"], writes=["oa_b"])
                    for q in range(4):
                        op("pe", lambda e, q=q: e.transpose(out=PBb(6)[:, q * 64:(q + 1) * 64], in_=oa_b[0:64, q * 128:(q + 1) * 128],
                                                            identity=ident_b[0:64, 0:64]), reads=["oa_b", "ident_b"],
                           writes=[("pb", 6)])
                    op("act", lambda e, ck=ck: e.copy(out=o_aT[:, :, ck * 64:(ck + 1) * 64],
                                                      in_=PBb(6)[:, 0:256].rearrange("p (q t) -> p q t", q=4)),
                       reads=[("pb", 6)], writes=[("o_aT", ck)])
            if "o_raw" in dbg:
                op("sp", None, reads=["dbg_o_raw"])
            if "o_aT" in dbg:
                op("sp", lambda e: e.dma_start(out=dbg["o_aT"].rearrange("(a p) t -> p a t", p=128), in_=o_aT),
                   reads=[("o_aT", ck) for ck in range(2 * NT)], writes=["dbg_o_aT"], dma=True)
                op("sp", None, reads=["dbg_o_aT"])

            sch.barrier()
            if stop_after == "gdn":
                return

            o_bT = view(R_A + 16 * K, [128, 4, S], BF16)
            da = MultiAlloc([(R_Q, R_Q + 48 * K), (R_W, R_W + 16 * K)])
            scoreb = [da([128, S], F32) for _ in range(2)]
            rl = [da([128, 512], F32) for _ in range(2)]
            maskbb = [da([128, S], BF16) for _ in range(2)]
            thr_t = [da([128, 1], F32) for _ in range(2)]
            PTt = [[da([128, 512], BF16) for _ in range(2)] for _ in range(2)]
            I4 = da([128, 512], BF16)
            lo_t = da([128, 1], F32)
            hi_t = da([128, 1], F32)
            W0 = da([128, 1], F32)
            mid_t = da([128, 1], F32)
            tsel = da([128, 1], F32)
            Wk = da([128, NBIS], F32)
            cnt = da([128, NBIS], F32)
            pow2 = da([128, NBIS], F32)
            ob = da([128, 520], F32)
            rden = da([128, 8], F32)
            ob_b = da([128, 512], BF16)
            for q in range(4):
                op("pool", lambda e, q=q: e.tensor_copy(out=I4[:, q * 128:(q + 1) * 128], in_=ident_b), reads=["ident_b"], writes=["I4"])
            for k in range(NBIS):
                op("pool", lambda e, k=k: e.memset(pow2[:, k:k + 1], 2.0 ** (-(k + 1))), writes=["pow2"])

            def scores_part(tt, sb):
                L = (tt + 1) * 128
                nkb = (L + 511) // 512
                qs = slice(tt * 128, (tt + 1) * 128)
                score = scoreb[sb]
                maskb = maskbb[sb]
                for h in range(8):
                    hp, par = h // 2, h % 2
                    rows = slice(par * 64, par * 64 + 64)
                    for kb in range(nkb):
                        w = min(512, L - kb * 512)
                        bank = kb % 2
                        ks = slice(kb * 512, kb * 512 + w)
                        op("pe", lambda e, hp=hp, par=par, rows=rows, w=w, bank=bank, ks=ks: e.matmul(
                            PB(bank)[:, 0:w], lhsT=iqT[rows, hp, qs], rhs=ikT2[rows, ks], start=True, stop=True,
                            tile_position=(par * 64, 0)), reads=["iqT", "ikT2"], writes=[("pb", bank)])
                        op("act", lambda e, w=w, bank=bank: e.activation(out=rl[bank][:, 0:w], in_=PB(bank)[:, 0:w], func=AF.Relu),
                           reads=[("pb", bank)], writes=[("rl", bank)])
                        if h == 0:
                            op("dve", lambda e, w=w, bank=bank, ks=ks: e.tensor_scalar(
                                out=score[:, ks], in0=rl[bank][:, 0:w], scalar1=iw_tok[:, tt, 0:1], scalar2=None, op0=ALU.mult),
                               reads=[("rl", bank), "iw_tok"], writes=[("score", sb, kb)])
                        else:
                            op("dve", lambda e, w=w, bank=bank, ks=ks, h=h: e.scalar_tensor_tensor(
                                out=score[:, ks], in0=rl[bank][:, 0:w], scalar=iw_tok[:, tt, h:h + 1], in1=score[:, ks],
                                op0=ALU.mult, op1=ALU.add), reads=[("rl", bank), "iw_tok", ("score", sb, kb)],
                               writes=[("score", sb, kb)], fast=(w >= 256))
                SK = [("score", sb, kb) for kb in range(nkb)]
                if tt >= 2:
                    op("dve", lambda e: e.tensor_reduce(out=hi_t, in_=score[:, 0:L], axis=AX.X, op=ALU.max), reads=SK, writes=["hi"])
                    op("dve", lambda e: e.tensor_reduce(out=lo_t, in_=score[:, 0:L], axis=AX.X, op=ALU.min), reads=SK, writes=["lo"])
                op("dve", lambda e: e.memset(score[0:64, L - 64:L], -1.0e30), reads=SK, writes=SK)
                if tt >= 2:
                    op("dve", lambda e: e.tensor_tensor(out=W0, in0=hi_t, in1=lo_t, op=ALU.subtract), reads=["hi", "lo"], writes=["W0"])
                    op("dve", lambda e: e.tensor_scalar(out=Wk, in0=pow2, scalar1=W0[:, 0:1], scalar2=None, op0=ALU.mult),
                       reads=["W0", "pow2"], writes=["Wk"])
                    op("dve", lambda e: e.memset(cnt, 0.0), writes=["cnt"])
                    op("dve", lambda e: e.tensor_tensor(out=mid_t, in0=lo_t, in1=Wk[:, 0:1], op=ALU.add), reads=["lo", "Wk"], writes=["mid"])
                    for k in range(NBIS):
                        op("dve", lambda e, k=k: e.tensor_scalar(out=maskb[:, 0:L], in0=score[:, 0:L], scalar1=mid_t[:, 0:1],
                                                                 scalar2=0.0, op0=ALU.is_gt, op1=ALU.add, accum_out=cnt[:, k:k + 1]),
                           reads=SK + ["mid", "cnt"], writes=[("maskb", sb), ("cntk", k)])
                        op("dve", lambda e, k=k: e.tensor_scalar(out=tsel, in0=cnt[:, k:k + 1], scalar1=255.5, scalar2=0.5,
                                                                 op0=ALU.is_gt, op1=ALU.subtract), reads=[("cntk", k)], writes=["tsel"])
                        op("dve", lambda e, k=k: e.scalar_tensor_tensor(out=mid_t, in0=tsel, scalar=Wk[:, k:k + 1], in1=mid_t,
                                                                        op0=ALU.mult, op1=ALU.add),
                           reads=["tsel", "Wk", "mid"], writes=["mid"])
                    op("dve", lambda e: e.scalar_tensor_tensor(out=thr_t[sb], in0=Wk[:, NBIS - 1:NBIS], scalar=-0.5, in1=mid_t,
                                                               op0=ALU.mult, op1=ALU.add), reads=["Wk", "mid"], writes=[("thr", sb)])
                else:
                    op("dve", lambda e: e.memset(thr_t[sb], -1.0e29), writes=[("thr", sb)])
                op("dve", lambda e: e.tensor_scalar(out=maskb[:, 0:L], in0=score[:, 0:L], scalar1=thr_t[sb][:, 0:1], scalar2=NEG,
                                                    op0=ALU.is_le, op1=ALU.mult), reads=SK + [("thr", sb)], writes=[("maskb", sb)])
                if "thr" in dbg:
                    op("sp", lambda e: e.dma_start(out=dbg["thr"][tt * 128:(tt + 1) * 128, :], in_=thr_t[sb]), reads=[("thr", sb)],
                       writes=["dbg_thr"], dma=True)
                if "score" in dbg and tt == NT - 1:
                    op("sp", lambda e: e.dma_start(out=dbg["score"], in_=score), reads=SK, writes=["dbg_score"], dma=True)

            def attn_part(tt, sb):
                qs = slice(tt * 128, (tt + 1) * 128)
                maskb = maskbb[sb]
                for kb in range(tt + 1):
                    kcs = slice(kb * 128, (kb + 1) * 128)
                    for g2 in range(2):
                        bank = 2 + g2 + 2 * (kb % 2)
                        pt = PTt[g2][kb % 2]
                        op("pe", lambda e, bank=bank, kcs=kcs: e.matmul(PB(bank), lhsT=maskb[:, kcs], rhs=I4, start=True, stop=False),
                           reads=[("maskb", sb), "I4"], writes=[("pb", bank)])
                        for s_ in range(4):
                            h = 4 * g2 + s_
                            hp, par = h // 2, h % 2
                            kT = kz[g2][par]
                            op("pe", lambda e, bank=bank, s_=s_, kT=kT, kcs=kcs, hp=hp: e.matmul(
                                PB(bank)[:, s_ * 128:(s_ + 1) * 128], lhsT=kT[:, kcs], rhs=bqT[:, hp, qs], start=False, stop=(s_ == 3)),
                               reads=["bkT", "bqT"], writes=[("pb", bank)])
                        op("act", lambda e, bank=bank, pt=pt: e.activation(out=pt, in_=PB(bank), func=AF.Exp, scale=0.125),
                           reads=[("pb", bank)], writes=[("PT", g2, kb % 2)])
                        for s_ in range(4):
                            op("pe", lambda e, g2=g2, s_=s_, pt=pt, kb=kb: e.matmul(
                                PB(6 + g2)[:, s_ * 65:(s_ + 1) * 65], lhsT=pt[:, s_ * 128:(s_ + 1) * 128],
                                rhs=bv_tok[:, kb, g2 * 65:(g2 + 1) * 65], start=(kb == 0 and s_ == 0), stop=(kb == tt and s_ == 3)),
                               reads=[("PT", g2, kb % 2), "bv_tok"], writes=[("pb", 6 + g2)])
                op("act", lambda e: e.copy(out=ob[:, 0:260], in_=PB(6)[:, 0:260]), reads=[("pb", 6)], writes=["ob"])
                op("act", lambda e: e.copy(out=ob[:, 260:520], in_=PB(7)[:, 0:260]), reads=[("pb", 7)], writes=["ob"])
                obv = ob.rearrange("p (s e) -> p s e", e=65)
                op("dve", lambda e: e.reciprocal(out=rden, in_=obv[:, :, 64]), reads=["ob"], writes=["rden"])
                op("dve", lambda e: e.tensor_tensor(out=ob_b.rearrange("p (h d) -> p h d", h=8), in0=obv[:, :, 0:64],
                                                    in1=rden.unsqueeze(2).to_broadcast([128, 8, 64]), op=ALU.mult),
                   reads=["ob", "rden"], writes=["ob_b"])
                for q in range(4):
                    op("pe", lambda e, q=q: e.transpose(out=PBb(0)[:, q * 128:(q + 1) * 128], in_=ob_b[:, q * 128:(q + 1) * 128],
                                                        identity=ident_b), reads=["ob_b", "ident_b"], writes=[("pb", 0)])
                op("act", lambda e: e.copy(out=o_bT[:, :, qs], in_=PBb(0)[:, 0:512].rearrange("p (q t) -> p q t", q=4)),
                   reads=[("pb", 0)], writes=[("o_bT", tt)])

            scores_part(0, 0)
            for tt in range(NT):
                if tt + 1 < NT:
                    scores_part(tt + 1, (tt + 1) % 2)
                attn_part(tt, tt % 2)
            if "thr" in dbg:
                op("sp", None, reads=["dbg_thr"])
            if "score" in dbg:
                op("sp", None, reads=["dbg_score"])
            if "o_bT" in dbg:
                op("sp", lambda e: e.dma_start(out=dbg["o_bT"].rearrange("(a p) t -> p a t", p=128), in_=o_bT),
                   reads=[("o_bT", tt) for tt in range(NT)], writes=["dbg_o_bT"], dma=True)
                op("sp", None, reads=["dbg_o_bT"])

            sch.barrier()
            if stop_after == "dsa":
                return

            pa = MultiAlloc([(R_W, ARENA_BYTES)])
            hT2 = pa([128, 8, S], BF16)
            mergedT = pa([128, 8, S], BF16)
            x1 = pa([128, NT, D], F32)
            bgate = pa([128, 16], F32)
            g2col = pa([128, 8], F32)
            fng = pa([128, D], F32)
            wst2 = pa([128, 8, 256], F32)
            wg_bf = [pa([128, 8, 256], BF16) for _ in range(2)]
            wp_bf = [pa([128, 4, 256], BF16) for _ in range(2)]
            ga_s = pa([128, 512], BF16)
            gb_s = pa([128, 512], BF16)
            t1 = pa([128, 512], BF16)
            t2 = pa([128, 512], BF16)
            xt4 = [pa([128, D], F32) for _ in range(2)]
            op("sp", lambda e: e.dma_start(out=bgate, in_=bgate_d), writes=["bgate"], dma=True)
            op("sp", lambda e: e.dma_start(out=g2col, in_=g2_d), writes=["g2col"], dma=True)
            op("sp", lambda e: e.dma_start(out=fng, in_=fng_d.partition_broadcast(128)), writes=["fng"], dma=True)

            def phase1b():
                xa = Alloc(R_W + 64 * K, R_W + 128 * K)
                xt = [xa([128, D], F32) for _ in range(2)]
                hb = [xa([128, D], BF16) for _ in range(2)]
                junk = xa([128, D], BF16)
                ssx = xa([128, NT], F32)
                rsx = xa([128, NT], F32)
                op("dve", lambda e: e.memset(ssx, 0.0), writes=["ssx"])
                for tt in range(NT):
                    b = tt % 2
                    op("sp", lambda e, tt=tt, b=b: e.dma_start(out=xt[b], in_=x_d[tt * 128:(tt + 1) * 128, :]), writes=[("xt", b)], dma=True)
                    op("act", lambda e, tt=tt, b=b: e.activation(out=junk, in_=xt[b], func=AF.Square, accum_out=ssx[:, tt:tt + 1]),
                       reads=[("xt", b), "ssx"], writes=["junk", ("ssx", tt)])
                    op("act", lambda e, tt=tt: e.activation(out=rsx[:, tt:tt + 1], in_=ssx[:, tt:tt + 1], func=AF.Sqrt, bias=epsc,
                                                            scale=1.0 / D), reads=[("ssx", tt), "epsc"], writes=[("rsx", tt)])
                    op("dve", lambda e, tt=tt: e.reciprocal(out=rsx[:, tt:tt + 1], in_=rsx[:, tt:tt + 1]), reads=[("rsx", tt)],
                       writes=[("rsx", tt)])
                    op("dve", lambda e, tt=tt, b=b: e.tensor_scalar(out=hb[b], in0=xt[b], scalar1=rsx[:, tt:tt + 1], scalar2=None,
                                                                    op0=ALU.mult), reads=[("xt", b), ("rsx", tt)], writes=[("hb", b)])
                    bk = tt % 2
                    for k in range(8):
                        op("pe", lambda e, k=k, b=b, bk=bk: e.transpose(out=PBb(bk)[:, k * 128:(k + 1) * 128],
                                                                        in_=hb[b][:, k * 128:(k + 1) * 128], identity=ident_b),
                           reads=[("hb", b), "ident_b"], writes=[("pb", bk)])
                    op("act", lambda e, tt=tt, bk=bk: e.copy(out=hT2[:, :, tt * 128:(tt + 1) * 128],
                                                             in_=PBb(bk).rearrange("p (k t) -> p k t", k=8)),
                       reads=[("pb", bk)], writes=[("hT2", tt)])
            phase1b()
            sch.barrier()
            HT2 = [("hT2", tt) for tt in range(NT)]

            wpa_v = wpa_d.rearrange("(k p) c -> p k c", p=128)
            wpb_v = wpb_d.rearrange("(k p) c -> p k c", p=128)
            wout_v = wout_d.rearrange("(k p) c -> p k c", p=128)
            wi4 = [0]

            wst2b = xt4[0].rearrange("p (k c) -> p k c", k=4)
            wst2c = xt4[1].rearrange("p (k c) -> p k c", k=4)
            wi5 = [0]

            def load_gate(c0):
                i = wi4[0]
                wi4[0] += 1
                b = i % 2
                op("sp", lambda e: e.dma_start(out=wst2, in_=w_in_v[:, :, c0:c0 + 256]), writes=["wst2"], dma=True)
                op("pool", lambda e, b=b: e.tensor_tensor(out=wg_bf[b], in0=wst2, in1=g1col.unsqueeze(2).to_broadcast([128, 8, 256]),
                                                          op=ALU.mult), reads=["wst2", "g1col"], writes=[("wg", b)])
                return wg_bf[b], ("wg", b)

            def load_proj(src_v, c0):
                i = wi5[0]
                wi5[0] += 1
                b = i % 2
                st = wst2b if b == 0 else wst2c
                op("sp", lambda e: e.dma_start(out=st, in_=src_v[:, :, c0:c0 + 256]), writes=[("wstp", b)], dma=True)
                op("pool", lambda e, b=b: e.tensor_copy(out=wp_bf[b], in_=st), reads=[("wstp", b)], writes=[("wp", b)])
                return wp_bf[b], ("wp", b)

            p4u = [0]
            for j in range(4):
                wga, kga = load_gate(C_GA + j * 256)
                wgb, kgb = load_gate(C_GB + j * 256)
                wpa, kpa = load_proj(wpa_v, j * 256)
                wpb, kpb = load_proj(wpb_v, j * 256)
                for ct in range(2):
                    c = 2 * j + ct
                    ccs = slice(ct * 128, (ct + 1) * 128)
                    for tb in range(4):
                        tcs = slice(tb * 512, (tb + 1) * 512)
                        hk = HT2[tb * 4:tb * 4 + 4]
                        bA, bB, bC, bD = (2, 3, 4, 5) if (p4u[0] % 2 == 0) else (0, 1, 6, 7)
                        p4u[0] += 1
                        for k in range(8):
                            op("pe", lambda e, k=k, ccs=ccs, tcs=tcs, wga=wga, bA=bA: e.matmul(PB(bA), lhsT=wga[:, k, ccs], rhs=hT2[:, k, tcs],
                                                                                         start=(k == 0), stop=(k == 7)),
                               reads=[kga] + hk, writes=[("pb", bA)])
                        op("act", lambda e, c=c, bA=bA: e.activation(out=ga_s, in_=PB(bA), func=AF.Sigmoid, bias=bgate[:, c:c + 1], scale=1.0),
                           reads=[("pb", bA), "bgate"], writes=["ga_s"])
                        for k in range(8):
                            op("pe", lambda e, k=k, ccs=ccs, tcs=tcs, wgb=wgb, bB=bB: e.matmul(PB(bB), lhsT=wgb[:, k, ccs], rhs=hT2[:, k, tcs],
                                                                                         start=(k == 0), stop=(k == 7)),
                               reads=[kgb] + hk, writes=[("pb", bB)])
                        op("act", lambda e, c=c, bB=bB: e.activation(out=gb_s, in_=PB(bB), func=AF.Sigmoid, bias=bgate[:, 8 + c:9 + c], scale=1.0),
                           reads=[("pb", bB), "bgate"], writes=["gb_s"])
                        for hp in range(4):
                            op("pe", lambda e, hp=hp, ccs=ccs, tcs=tcs, wpa=wpa, bC=bC: e.matmul(PB(bC), lhsT=wpa[:, hp, ccs], rhs=o_aT[:, hp, tcs],
                                                                                           start=(hp == 0), stop=(hp == 3)),
                               reads=[kpa, "o_aT"], writes=[("pb", bC)])
                        for hp in range(4):
                            op("pe", lambda e, hp=hp, ccs=ccs, tcs=tcs, wpb=wpb, bD=bD: e.matmul(PB(bD), lhsT=wpb[:, hp, ccs], rhs=o_bT[:, hp, tcs],
                                                                                           start=(hp == 0), stop=(hp == 3)),
                               reads=[kpb, "o_bT"], writes=[("pb", bD)])
                        op("dve", lambda e, bC=bC: e.tensor_tensor(out=t1, in0=PB(bC), in1=ga_s, op=ALU.mult), reads=[("pb", bC), "ga_s"],
                           writes=["t1"])
                        op("dve", lambda e, bD=bD: e.tensor_tensor(out=t2, in0=PB(bD), in1=gb_s, op=ALU.mult), reads=[("pb", bD), "gb_s"],
                           writes=["t2"])
                        op("pool", lambda e, c=c, tcs=tcs: e.tensor_tensor(out=mergedT[:, c, tcs], in0=t1, in1=t2, op=ALU.add),
                           reads=["t1", "t2"], writes=[("mergedT", c, tb)])
            if "mergedT" in dbg:
                op("sp", lambda e: e.dma_start(out=dbg["mergedT"].rearrange("(a p) t -> p a t", p=128), in_=mergedT),
                   reads=[("mergedT", c, tb) for c in range(8) for tb in range(4)], writes=["dbg_mergedT"], dma=True)
                op("sp", None, reads=["dbg_mergedT"])
            sch.barrier()
            wout_bf = view(R_A, [128, 8, D], BF16)
            for j in range(4):
                op("sp", lambda e, j=j: e.dma_start(out=wst2, in_=wout_v[:, :, j * 256:(j + 1) * 256]), writes=["wst2"], dma=True)
                op("pool", lambda e, j=j: e.tensor_copy(out=wout_bf[:, :, j * 256:(j + 1) * 256], in_=wst2), reads=["wst2"],
                   writes=[("wout", j)])
            WOUT = [("wout", j) for j in range(4)]
            for tt in range(NT):
                b = tt % 2
                op("sp", lambda e, tt=tt, b=b: e.dma_start(out=xt4[b], in_=x_d[tt * 128:(tt + 1) * 128, :]), writes=[("xt4", b)], dma=True)
                for nb in range(2):
                    bk = 2 + 2 * b + nb
                    for c in range(8):
                        op("pe", lambda e, c=c, tt=tt, nb=nb, bk=bk: e.matmul(PB(bk), lhsT=mergedT[:, c, tt * 128:(tt + 1) * 128],
                                                                               rhs=wout_bf[:, c, nb * 512:(nb + 1) * 512],
                                                                               start=(c == 0), stop=(c == 7)),
                           reads=WOUT + ["mergedT_all"], writes=[("pb", bk)])
                    op("dve", lambda e, tt=tt, nb=nb, bk=bk, b=b: e.tensor_tensor(out=x1[:, tt, nb * 512:(nb + 1) * 512], in0=PB(bk),
                                                                                   in1=xt4[b][:, nb * 512:(nb + 1) * 512], op=ALU.add),
                       reads=[("pb", bk), ("xt4", b)], writes=[("x1", tt)])
            if "x1" in dbg:
                op("sp", lambda e: e.dma_start(out=dbg["x1"].rearrange("(t p) c -> p t c", p=128), in_=x1),
                   reads=[("x1", tt) for tt in range(NT)], writes=["dbg_x1"], dma=True)
                op("sp", None, reads=["dbg_x1"])
            sch.barrier()
            if stop_after == "p4":
                return

            h2T = view(R_W, [128, 8, S], BF16)
            ma = MultiAlloc([(R_W + 32 * K, R_W + 64 * K), (R_A, R_A + 32 * K)])
            tail_off = None
            hb2 = [ma([128, D], BF16) for _ in range(2)]
            junk2 = ma([128, D], BF16)
            ss5 = ma([128, NT], F32)
            rs5 = ma([128, NT], F32)
            wr_st = ma([128, 8, 20], F32)
            wr_bf = ma([128, 8, 20], BF16)
            brow = ma([128, 20], F32)
            lg = ma([128, 20], F32)
            sm = {n_: ma([128, 4], F32) for n_ in ("goh", "gex", "elg", "oh1", "msk", "oh2", "wsel")}
            sc1 = {n_: ma([128, 1], F32) for n_ in ("gmax", "ngmax", "gsum", "ggate", "m1", "m2", "d21", "e21", "den", "w1", "w2")}
            tmp44 = ma([128, 4, 4], F32)
            comb_b = ma([128, 16], BF16)
            combT = ma([128, S], BF16)
            sel16 = ma([128, 16, 128], BF16)
            est = ma([128, 8, 256], F32)
            w1b = [ma([128, 8, 256], BF16) for _ in range(2)]
            w3b = [ma([128, 8, 256], BF16) for _ in range(2)]
            w2b = [ma([128, 2, D], BF16) for _ in range(2)]
            sg = [[ma([128, 512], BF16) for _ in range(2)] for _ in range(2)]
            cbt = [ma([128, 512], BF16) for _ in range(2)]
            tu = [[ma([128, 512], BF16) for _ in range(2)] for _ in range(2)]
            actT = [[ma([128, 512], BF16) for _ in range(2)] for _ in range(2)]
            op("sp", lambda e: e.dma_start(out=wr_st, in_=wr_d.rearrange("(k p) c -> p k c", p=128)), writes=["wr_st"], dma=True)
            op("sp", lambda e: e.dma_start(out=brow, in_=br_d.partition_broadcast(128)), writes=["brow"], dma=True)
            op("pool", lambda e: e.tensor_tensor(out=wr_bf, in0=wr_st, in1=g2col.unsqueeze(2).to_broadcast([128, 8, 20]), op=ALU.mult),
               reads=["wr_st", "g2col"], writes=["wr_bf"])
            op("pool", lambda e: e.memset(sel16[0:16, :, :], 1.0), writes=["sel16"])
            op("pool", lambda e: e.affine_select(out=sel16[0:16, :, :], in_=sel16[0:16, :, :], pattern=[[-1, 16], [0, 128]],
                                                 compare_op=ALU.is_equal, fill=0.0, base=0, channel_multiplier=1), writes=["sel16"])
            op("dve", lambda e: e.memset(ss5, 0.0), writes=["ss5"])
            def prep_tile(tt):
                b = tt % 2
                bk = tt % 2
                op("act", lambda e, tt=tt: e.activation(out=junk2, in_=x1[:, tt, :], func=AF.Square, accum_out=ss5[:, tt:tt + 1]),
                   reads=["x1_all", "ss5"], writes=["junk2", ("ss5", tt)])
                op("act", lambda e, tt=tt: e.activation(out=rs5[:, tt:tt + 1], in_=ss5[:, tt:tt + 1], func=AF.Sqrt, bias=epsc,
                                                        scale=1.0 / D), reads=[("ss5", tt), "epsc"], writes=[("rs5", tt)])
                op("dve", lambda e, tt=tt: e.reciprocal(out=rs5[:, tt:tt + 1], in_=rs5[:, tt:tt + 1]), reads=[("rs5", tt)],
                   writes=[("rs5", tt)])
                op("dve", lambda e, tt=tt, b=b: e.tensor_scalar(out=hb2[b], in0=x1[:, tt, :], scalar1=rs5[:, tt:tt + 1], scalar2=None,
                                                                op0=ALU.mult), reads=["x1_all", ("rs5", tt)], writes=[("hb2", b)])
                for k in range(8):
                    op("pe", lambda e, k=k, b=b, bk=bk: e.transpose(out=PBb(bk)[:, k * 128:(k + 1) * 128],
                                                                    in_=hb2[b][:, k * 128:(k + 1) * 128], identity=ident_b),
                       reads=[("hb2", b), "ident_b"], writes=[("pb", bk)])
                op("act", lambda e, tt=tt, bk=bk: e.copy(out=h2T[:, :, tt * 128:(tt + 1) * 128],
                                                         in_=PBb(bk).rearrange("p (k t) -> p k t", k=8)),
                   reads=[("pb", bk)], writes=[("h2T", tt)])
                for k in range(8):
                    op("pe", lambda e, k=k, tt=tt: e.matmul(PB(2)[:, 0:20], lhsT=h2T[:, k, tt * 128:(tt + 1) * 128], rhs=wr_bf[:, k, :],
                                                            start=(k == 0), stop=(k == 7)),
                       reads=[("h2T", tt), "wr_bf"], writes=[("pb", 2)])
                R = []

                def rop(fn, rd, wr):
                    op("dve", fn, reads=rd, writes=wr)
                rop(lambda e: e.tensor_tensor(out=lg, in0=PB(2)[:, 0:20], in1=brow, op=ALU.add), [("pb", 2), "brow"], ["lg"])
                elv = lg[:, 4:20].rearrange("p (g x) -> p g x", g=4)
                rop(lambda e: e.tensor_reduce(out=sc1["gmax"], in_=lg[:, 0:4], axis=AX.X, op=ALU.max), ["lg"], ["gmax"])
                rop(lambda e: e.tensor_scalar(out=sm["goh"], in0=lg[:, 0:4], scalar1=sc1["gmax"][:, 0:1], scalar2=None,
                                              op0=ALU.is_equal), ["lg", "gmax"], ["goh"])
                rop(lambda e: e.tensor_scalar(out=sc1["ngmax"], in0=sc1["gmax"], scalar1=-1.0, scalar2=None, op0=ALU.mult),
                    ["gmax"], ["ngmax"])
                op("act", lambda e: e.activation(out=sm["gex"], in_=lg[:, 0:4], func=AF.Exp, bias=sc1["ngmax"][:, 0:1], scale=1.0),
                   reads=["lg", "ngmax"], writes=["gex"])
                rop(lambda e: e.tensor_reduce(out=sc1["gsum"], in_=sm["gex"], axis=AX.X, op=ALU.add), ["gex"], ["gsum"])
                rop(lambda e: e.reciprocal(out=sc1["ggate"], in_=sc1["gsum"]), ["gsum"], ["ggate"])
                rop(lambda e: e.tensor_tensor(out=tmp44, in0=elv, in1=sm["goh"].unsqueeze(2).to_broadcast([128, 4, 4]), op=ALU.mult),
                    ["lg", "goh"], ["tmp44"])
                rop(lambda e: e.tensor_reduce(out=sm["elg"], in_=tmp44.rearrange("p g x -> p x g"), axis=AX.X, op=ALU.add),
                    ["tmp44"], ["elg"])
                rop(lambda e: e.tensor_reduce(out=sc1["m1"], in_=sm["elg"], axis=AX.X, op=ALU.max), ["elg"], ["m1"])
                rop(lambda e: e.tensor_scalar(out=sm["oh1"], in0=sm["elg"], scalar1=sc1["m1"][:, 0:1], scalar2=None, op0=ALU.is_equal),
                    ["elg", "m1"], ["oh1"])
                rop(lambda e: e.scalar_tensor_tensor(out=sm["msk"], in0=sm["oh1"], scalar=-1.0e30, in1=sm["elg"], op0=ALU.mult,
                                                     op1=ALU.add), ["oh1", "elg"], ["msk"])
                rop(lambda e: e.tensor_reduce(out=sc1["m2"], in_=sm["msk"], axis=AX.X, op=ALU.max), ["msk"], ["m2"])
                rop(lambda e: e.tensor_scalar(out=sm["oh2"], in0=sm["msk"], scalar1=sc1["m2"][:, 0:1], scalar2=None, op0=ALU.is_equal),
                    ["msk", "m2"], ["oh2"])
                rop(lambda e: e.tensor_tensor(out=sc1["d21"], in0=sc1["m2"], in1=sc1["m1"], op=ALU.subtract), ["m1", "m2"], ["d21"])
                op("act", lambda e: e.activation(out=sc1["e21"], in_=sc1["d21"], func=AF.Exp), reads=["d21"], writes=["e21"])
                rop(lambda e: e.tensor_scalar(out=sc1["den"], in0=sc1["e21"], scalar1=1.0, scalar2=None, op0=ALU.add), ["e21"], ["den"])
                rop(lambda e: e.reciprocal(out=sc1["den"], in_=sc1["den"]), ["den"], ["den"])
                rop(lambda e: e.tensor_tensor(out=sc1["w1"], in0=sc1["ggate"], in1=sc1["den"], op=ALU.mult), ["ggate", "den"], ["w1"])
                rop(lambda e: e.tensor_tensor(out=sc1["w2"], in0=sc1["w1"], in1=sc1["e21"], op=ALU.mult), ["w1", "e21"], ["w2"])
                rop(lambda e: e.tensor_scalar(out=sm["wsel"], in0=sm["oh1"], scalar1=sc1["w1"][:, 0:1], scalar2=None, op0=ALU.mult),
                    ["oh1", "w1"], ["wsel"])
                rop(lambda e: e.scalar_tensor_tensor(out=sm["wsel"], in0=sm["oh2"], scalar=sc1["w2"][:, 0:1], in1=sm["wsel"],
                                                     op0=ALU.mult, op1=ALU.add), ["oh2", "w2", "wsel"], ["wsel"])
                rop(lambda e: e.tensor_tensor(out=comb_b.rearrange("p (g x) -> p g x", g=4),
                                              in0=sm["goh"].unsqueeze(2).to_broadcast([128, 4, 4]),
                                              in1=sm["wsel"].unsqueeze(1).to_broadcast([128, 4, 4]), op=ALU.mult),
                    ["goh", "wsel"], ["comb_b"])
                if "comb" in dbg:
                    op("sp", lambda e, tt=tt: e.dma_start(out=dbg["comb"][tt * 128:(tt + 1) * 128, :], in_=comb_b), reads=["comb_b"],
                       writes=["dbg_comb"], dma=True)
                op("pe", lambda e: e.transpose(out=PBb(3)[0:16, 0:128], in_=comb_b, identity=ident_b), reads=["comb_b", "ident_b"],
                   writes=[("pb", 3)])
                op("act", lambda e, tt=tt: e.copy(out=combT[0:16, tt * 128:(tt + 1) * 128], in_=PBb(3)[0:16, 0:128]),
                   reads=[("pb", 3)], writes=[("combT", tt)])
            for tt in range(4):
                prep_tile(tt)
            H2T = [("h2T", tt) for tt in range(NT)]
            CT = [("combT", tt) for tt in range(NT)]

            def load_expert(e_i):
                b = e_i % 2
                for (src, dst, nm, fold) in ((w1_d, w1b[b], "w1", True), (w3_d, w3b[b], "w3", True)):
                    op("sp", lambda e, src=src: e.dma_start(out=est, in_=src[e_i].rearrange("(k p) f -> p k f", p=128)),
                       writes=["est"], dma=True)
                    op("pool", lambda e, dst=dst: e.tensor_tensor(out=dst, in0=est, in1=g2col.unsqueeze(2).to_broadcast([128, 8, 256]),
                                                                  op=ALU.mult), reads=["est", "g2col"], writes=[(nm, b)])
                op("sp", lambda e: e.dma_start(out=est.rearrange("p k f -> p (k f)").rearrange("p (a c) -> p a c", a=2),
                                               in_=w2_d[e_i].rearrange("(a p) c -> p a c", p=128)), writes=["est"], dma=True)
                op("pool", lambda e: e.tensor_copy(out=w2b[b], in_=est.rearrange("p k f -> p (k f)").rearrange("p (a c) -> p a c", a=2)),
                   reads=["est"], writes=[("w2", b)])

            def stageA(e_i, tb, sl):
                b = e_i % 2
                tcs = slice(tb * 512, (tb + 1) * 512)
                hk = H2T[tb * 4:tb * 4 + 4]
                cbk_ = 6
                op("pe", lambda e: e.matmul(PB(cbk_), lhsT=sel16[0:16, e_i, :], rhs=combT[0:16, tcs], start=True, stop=True),
                   reads=["sel16"] + CT[tb * 4:tb * 4 + 4], writes=[("pb", cbk_)])
                op("act", lambda e: e.copy(out=cbt[sl], in_=PB(cbk_)), reads=[("pb", cbk_)], writes=[("cbt", sl)])
                for ft in range(2):
                    fcs = slice(ft * 128, (ft + 1) * 128)
                    for k in range(8):
                        op("pe", lambda e, k=k, fcs=fcs, ft=ft: e.matmul(PB(2 + ft), lhsT=w1b[b][:, k, fcs], rhs=h2T[:, k, tcs],
                                                                         start=(k == 0), stop=(k == 7)),
                           reads=[("w1", b)] + hk, writes=[("pb", 2 + ft)])
                    op("act", lambda e, ft=ft: e.activation(out=sg[sl][ft], in_=PB(2 + ft), func=AF.Silu), reads=[("pb", 2 + ft)],
                       writes=[("sg", sl, ft)])
                    yield
                    for k in range(8):
                        op("pe", lambda e, k=k, fcs=fcs, ft=ft: e.matmul(PB(4 + ft), lhsT=w3b[b][:, k, fcs], rhs=h2T[:, k, tcs],
                                                                         start=(k == 0), stop=(k == 7)),
                           reads=[("w3", b)] + hk, writes=[("pb", 4 + ft)])
                    op("dve", lambda e, ft=ft: e.tensor_tensor(out=tu[sl][ft], in0=PB(4 + ft), in1=sg[sl][ft], op=ALU.mult),
                       reads=[("pb", 4 + ft), ("sg", sl, ft)], writes=[("tu", sl, ft)], fast=True)
                    op("pool", lambda e, ft=ft: e.tensor_tensor(out=actT[sl][ft], in0=tu[sl][ft], in1=cbt[sl], op=ALU.mult),
                       reads=[("tu", sl, ft), ("cbt", sl)], writes=[("actT", sl, ft)])
                    yield

            ybank = [0]

            def stageB(e_i, tb, sl):
                b = e_i % 2
                for t4 in range(4):
                    tt = tb * 4 + t4
                    for nb in range(2):
                        bk = (0, 1, 7)[ybank[0] % 3]
                        ybank[0] += 1
                        for ft in range(2):
                            op("pe", lambda e, ft=ft, t4=t4, nb=nb, bk=bk: e.matmul(
                                PB(bk), lhsT=actT[sl][ft][:, t4 * 128:(t4 + 1) * 128], rhs=w2b[b][:, ft, nb * 512:(nb + 1) * 512],
                                start=(ft == 0), stop=(ft == 1)), reads=[("actT", sl, ft), ("w2", b)], writes=[("pb", bk)])
                        op("dve", lambda e, tt=tt, nb=nb, bk=bk: e.tensor_tensor(out=x1[:, tt, nb * 512:(nb + 1) * 512],
                                                                                 in0=PB(bk), in1=x1[:, tt, nb * 512:(nb + 1) * 512],
                                                                                 op=ALU.add),
                           reads=[("pb", bk), ("x2", tt, nb)], writes=[("x2", tt, nb)], fast=True)
                        if nb == 1:
                            yield

            def drain(g_):
                for _ in g_:
                    pass

            units = [(e_i, tb) for e_i in range(16) for tb in range(4)]
            load_expert(0)
            load_expert(1)
            drain(stageA(units[0][0], units[0][1], 0))
            for u, (e_i, tb) in enumerate(units):
                gb = stageB(e_i, tb, u % 2)
                if u + 1 < len(units):
                    ne, ntb = units[u + 1]
                    if ne == 0:
                        for tt in range(4 * ntb, 4 * ntb + 4):
                            prep_tile(tt)
                    ga_ = stageA(ne, ntb, (u + 1) % 2)
                    drain(ga_)
                drain(gb)
                if tb == 3 and e_i + 2 < 16:
                    load_expert(e_i + 2)
            sch.barrier()
            if "x2" in dbg:
                op("sp", lambda e: e.dma_start(out=dbg["x2"].rearrange("(t p) c -> p t c", p=128), in_=x1), writes=["dbg_x2"], dma=True)
                op("sp", None, reads=["dbg_x2"])

            fa = MultiAlloc([(R_W, R_W + 64 * K)])
            ss6 = fa([128, NT], F32)
            rs6 = fa([128, NT], F32)
            junk6 = fa([128, D], BF16)
            yo = [fa([128, D], F32) for _ in range(2)]
            op("dve", lambda e: e.memset(ss6, 0.0), writes=["ss6"])
            for tt in range(NT):
                b = tt % 2
                op("act", lambda e, tt=tt: e.activation(out=junk6, in_=x1[:, tt, :], func=AF.Square, accum_out=ss6[:, tt:tt + 1]),
                   reads=["ss6"], writes=["junk6", ("ss6", tt)])
                op("act", lambda e, tt=tt: e.activation(out=rs6[:, tt:tt + 1], in_=ss6[:, tt:tt + 1], func=AF.Sqrt, bias=epsc,
                                                        scale=1.0 / D), reads=[("ss6", tt), "epsc"], writes=[("rs6", tt)])
                op("dve", lambda e, tt=tt: e.reciprocal(out=rs6[:, tt:tt + 1], in_=rs6[:, tt:tt + 1]), reads=[("rs6", tt)],
                   writes=[("rs6", tt)])
                op("dve", lambda e, tt=tt, b=b: e.scalar_tensor_tensor(out=yo[b], in0=x1[:, tt, :], scalar=rs6[:, tt:tt + 1], in1=fng,
                                                                       op0=ALU.mult, op1=ALU.mult),
                   reads=[("rs6", tt), "fng"], writes=[("yo", b)])
                op("sp", lambda e, tt=tt, b=b: e.dma_start(out=out_d[tt * 128:(tt + 1) * 128, :], in_=yo[b]), reads=[("yo", b)],
                   writes=[("out", tt)], dma=True)
            op("sp", None, reads=[("out", tt) for tt in range(NT)])


        body()
        sch.barrier()
        DEBUG["stats_pre"] = {e: len(sch.ops[e]) for e in Sched.ENGS}
        with nc.Block() as block:
            sch.emit(nc, block, engsem, dmasem)
        DEBUG["stats"] = sch.stats
    return nc


_NC_CACHE = {}


def kernel(**inputs):
    dbg = tuple(DEBUG.get("outputs", ()))
    key = (dbg, DEBUG.get("stop_after"))
    if key not in _NC_CACHE:
        _NC_CACHE[key] = build_nc(dbg, DEBUG.get("stop_after"))
    nc = _NC_CACHE[key]
    n = 8
    x = np.ascontiguousarray(inputs["x"], dtype=np.float32)
    posn = np.ascontiguousarray(inputs["positions"], dtype=np.int32)
    f32 = lambda a: np.ascontiguousarray(a, dtype=np.float32)
    inv = (10000.0 ** (-np.arange(32, dtype=np.float32) / np.float32(32))).astype(np.float32).reshape(1, 32)
    shared = {
        "norm1_g": f32(inputs["norm1_g"][0].reshape(8, 128).T),
        "w_in": f32(inputs["w_in"][0]),
        "conv_w": f32(inputs["conv_w"][0].reshape(4, 12, 128).transpose(2, 1, 0).reshape(128, 48)),
        "inv_freq": inv,
        "a_log": f32(inputs["a_log"][0].reshape(1, 8)),
        "dt_bias": f32(inputs["dt_bias"][0].reshape(1, 8)),
        "a_norm_g": f32(inputs["a_norm_g"][0].reshape(1, 64)),
        "b_gate": f32(inputs["b_gate"][0].reshape(16, 128).T),
        "norm2_g": f32(inputs["norm2_g"][0].reshape(8, 128).T),
        "final_norm_g": f32(inputs["final_norm_g"].reshape(1, D)),
        "w_proj_a": f32(inputs["w_proj_a"][0]),
        "w_proj_b": f32(inputs["w_proj_b"][0]),
        "w_out": f32(inputs["w_out"][0]),
        "w_router": f32(np.concatenate([inputs["w_router_group"][0], inputs["w_router_expert"][0]], axis=1)),
        "b_router": f32(np.concatenate([inputs["b_router_group"][0], inputs["b_router_expert"][0]], axis=0).reshape(1, 20)),
        "w_exp_gate": f32(inputs["w_exp_gate"][0]),
        "w_exp_up": f32(inputs["w_exp_up"][0]),
        "w_exp_down": f32(inputs["w_exp_down"][0]),
    }
    in_maps = []
    for c in range(n):
        m = dict(shared)
        m["x"] = x[c]
        m["positions"] = np.ascontiguousarray(posn[c].reshape(NT, 128).T)
        in_maps.append(m)
    res = run_bass_kernel_spmd(nc, in_maps, core_ids=list(range(n)))
    DEBUG["results"] = res.results
    return np.stack([r["out"] for r in res.results], axis=0)
```

```python
import math
from contextlib import ExitStack
import numpy as np
import concourse.bass as bass
import concourse.mybir as mybir
from concourse.bass_utils import run_bass_kernel_spmd

F32 = mybir.dt.float32
BF16 = mybir.dt.bfloat16
I32 = mybir.dt.int32
AF = mybir.ActivationFunctionType
ALU = mybir.AluOpType
AX = mybir.AxisListType

S = 2048
D = 1024
NT = S // 128
D_IN = 5464
EPS = 1e-6
N_DMA_SEMS = 24
NEG = -30000.0
NBIS = 12
TWO_PI = 2.0 * math.pi

C_AQ, C_AK, C_AV, C_AZ = 0, 512, 1024, 1536
C_BETA, C_ALPHA = 2048, 2056
C_BQ, C_BK, C_BV = 2064, 2576, 2704
C_IQ, C_IK, C_IW = 2832, 3344, 3408
C_GA, C_GB = 3416, 4440

DEBUG = {}
STRICT_SAME_ENGINE = True


class Sched:
    ENGS = ("pe", "act", "dve", "pool", "sp")

    def __init__(self):
        self.ops = {e: [] for e in self.ENGS}
        self.last_w = {}
        self.readers = {}
        self.dma_rr = 0
        self.dma_count = [0] * N_DMA_SEMS

    def op(self, eng, fn, reads=(), writes=(), dma=False, fast=False):
        deps = set()
        raw = set()
        for k in reads:
            t = self.last_w.get(k)
            if t is not None:
                deps.add(t)
                raw.add(t)
        for k in writes:
            t = self.last_w.get(k)
            if t is not None:
                deps.add(t)
            for t in self.readers.get(k, {}).values():
                deps.add(t)
        idx = len(self.ops[eng])
        if dma:
            si = self.dma_rr
            self.dma_rr = (self.dma_rr + 1) % N_DMA_SEMS
            prev = self.dma_count[si]
            if prev > 0:
                deps.add(("dma", si, prev))
            self.dma_count[si] = prev + 1
            tok = ("dma", si, prev + 1)
            rkey = ("dma", si)
        else:
            tok = ("eng", eng, idx)
            rkey = eng
            if STRICT_SAME_ENGINE:
                deps = {t for t in deps if not (t[0] == "eng" and t[1] == eng) or eng != "pe"}
            else:
                deps = {t for t in deps if not (t[0] == "eng" and t[1] == eng)
                        or (t in raw and eng != "pe" and not fast and idx - t[2] <= 8)}
        self.ops[eng].append(dict(fn=fn, deps=deps, signal=False, dma=(tok if dma else None)))
        for k in writes:
            self.last_w[k] = tok
            self.readers[k] = {}
        for k in reads:
            if k in writes:
                continue
            self.readers.setdefault(k, {})[rkey] = tok
        return tok

    def barrier(self):
        toks = set()
        for e in self.ENGS:
            j = len(self.ops[e]) - 1
            while j >= 0 and (self.ops[e][j]["fn"] is None or self.ops[e][j]["dma"] is not None):
                j -= 1
            if j >= 0:
                toks.add(("eng", e, j))
        for si in range(N_DMA_SEMS):
            if self.dma_count[si] > 0:
                toks.add(("dma", si, self.dma_count[si]))
        for e in self.ENGS:
            deps = {t for t in toks if not (t[0] == "eng" and t[1] == e and (e == "pe" or not STRICT_SAME_ENGINE))}
            self.ops[e].append(dict(fn=None, deps=deps, signal=False, dma=None))
        self.last_w = {}
        self.readers = {}

    def emit(self, nc, block, engsem, dmasem):
        for e in self.ENGS:
            for o in self.ops[e]:
                for t in o["deps"]:
                    if t[0] == "eng":
                        self.ops[t[1]][t[2]]["signal"] = True
        sigcount = {}
        for e in self.ENGS:
            c = 0
            lst = []
            for o in self.ops[e]:
                if o["signal"]:
                    c += 1
                lst.append(c)
            sigcount[e] = lst
        self.stats = {e: (len(self.ops[e]), sigcount[e][-1] if sigcount[e] else 0) for e in self.ENGS}

        def run(e, eng):
            waited = {}
            for o in self.ops[e]:
                need = {}
                for t in o["deps"]:
                    if t[0] == "eng":
                        key = ("eng", t[1])
                        val = sigcount[t[1]][t[2]]
                    else:
                        key = ("dma", t[1])
                        val = 16 * t[2]
                    if val > need.get(key, 0):
                        need[key] = val
                for key, val in need.items():
                    if waited.get(key, 0) >= val:
                        continue
                    waited[key] = val
                    sem = engsem[key[1]] if key[0] == "eng" else dmasem[key[1]]
                    eng.wait_ge(sem, val)
                if o["fn"] is None:
                    continue
                inst = o["fn"](eng)
                if o["dma"] is not None:
                    inst.then_inc(dmasem[o["dma"][1]], 16)
                elif o["signal"]:
                    inst.then_inc(engsem[e], 1)

        @block.tensor
        def _(eng):
            run("pe", eng)

        @block.scalar
        def _(eng):
            run("act", eng)

        @block.vector
        def _(eng):
            run("dve", eng)

        @block.gpsimd
        def _(eng):
            run("pool", eng)

        @block.sync
        def _(eng):
            run("sp", eng)


DT_SIZE = {F32: 4, BF16: 2, I32: 4}


def build_nc(debug=(), stop_after=None):
    nc = bass.Bass("TRN2", target_bir_lowering=False)

    def din(name, shape, dt=F32):
        return nc.dram_tensor(name, list(shape), dt, kind="ExternalInput").ap()

    x_d = din("x", [S, D])
    pos_d = din("positions", [128, NT], I32)
    g1_d = din("norm1_g", [128, 8])
    w_in_d = din("w_in", [D, D_IN])
    convw_d = din("conv_w", [128, 48])
    invf_d = din("inv_freq", [1, 32])
    alog_d = din("a_log", [1, 8])
    dtb_d = din("dt_bias", [1, 8])
    ang_d = din("a_norm_g", [1, 64])
    bgate_d = din("b_gate", [128, 16])
    g2_d = din("norm2_g", [128, 8])
    fng_d = din("final_norm_g", [1, D])
    wpa_d = din("w_proj_a", [512, D])
    wpb_d = din("w_proj_b", [512, D])
    wout_d = din("w_out", [D, D])
    wr_d = din("w_router", [D, 20])
    br_d = din("b_router", [1, 20])
    w1_d = din("w_exp_gate", [16, D, 256])
    w3_d = din("w_exp_up", [16, D, 256])
    w2_d = din("w_exp_down", [16, 256, D])
    out_d = nc.dram_tensor("out", [S, D], F32, kind="ExternalOutput").ap()
    dbg = {}
    for name, shape, dt in debug:
        dbg[name] = nc.dram_tensor("dbg_" + name, list(shape), dt, kind="ExternalOutput").ap()
    w_in_v = w_in_d.rearrange("(k p) c -> p k c", p=128)

    sch = Sched()
    op = sch.op
    es = ExitStack()
    with es:
        ARENA_BYTES = 207 * 1024
        arena = es.enter_context(nc.sbuf_tensor("arena", [128, ARENA_BYTES // 4], F32))

        def view(off, shape, dt):
            n = 1
            for s_ in shape[1:]:
                n *= s_
            size = n * DT_SIZE[dt]
            assert off % 4 == 0 and size % 4 == 0 and off + size <= ARENA_BYTES, (off, size)
            ap = arena[:, off // 4:(off + size) // 4]
            if dt != F32:
                ap = ap.bitcast(dt)
            if len(shape) == 3:
                ap = ap.rearrange("p (a b) -> p a b", a=shape[1])
            elif len(shape) == 4:
                ap = ap.rearrange("p (a b c) -> p a b c", a=shape[1], b=shape[2])
            return ap

        class Alloc:
            def __init__(self, base, limit):
                self.off = base
                self.limit = limit

            def __call__(self, shape, dt):
                n = 1
                for s_ in shape[1:]:
                    n *= s_
                size = (n * DT_SIZE[dt] + 63) // 64 * 64
                self.off = (self.off + 63) // 64 * 64
                v = view(self.off, shape, dt)
                self.off += size
                assert self.off <= self.limit, (self.off, self.limit)
                return v

        pbank = [es.enter_context(nc.psum_tensor("pb%d" % i, [128, 512], F32)) for i in range(8)]
        engsem = {e: es.enter_context(nc.semaphore("sem_" + e)) for e in Sched.ENGS}
        dmasem = [es.enter_context(nc.semaphore("dsem%d" % i)) for i in range(N_DMA_SEMS)]

        def PB(i):
            return pbank[i][:]

        def PBb(i):
            return pbank[i][:].bitcast(BF16)

        def body():
            K = 1024
            ca = Alloc(0, 9 * K)
            ident_f = ca([128, 128], F32)
            ident_b = ca([128, 128], BF16)
            ucs_f = ca([128, 128], F32)
            mc0_f = ca([128, 128], F32)
            mc1_f = ca([128, 128], F32)
            maskneg_f = ca([128, 128], F32)
            strict_b = ca([128, 128], BF16)
            g1col = ca([128, 8], F32)
            epsc = ca([128, 1], F32)
            cw = ca([128, 48], F32)
            invf = ca([128, 32], F32)
            dtb = ca([128, 8], F32)
            negA = ca([128, 8], F32)
            angb = ca([128, 64], F32)
            posi = ca([128, NT], I32)
            posf = ca([128, NT], F32)
            cs = ca([128, NT, 64], F32)
            assert ca.off <= 9 * K, ca.off

            op("pool", lambda e: e.memset(ident_f, 1.0), writes=["ident_f"])
            op("pool", lambda e: e.affine_select(out=ident_f, in_=ident_f, pattern=[[-1, 128]], compare_op=ALU.is_equal,
                                                 fill=0.0, base=0, channel_multiplier=1), writes=["ident_f"])
            op("pool", lambda e: e.tensor_copy(out=ident_b, in_=ident_f), reads=["ident_f"], writes=["ident_b"])
            op("pool", lambda e: e.memset(ucs_f, 1.0), writes=["ucs_f"])
            op("pool", lambda e: e.affine_select(out=ucs_f, in_=ucs_f, pattern=[[1, 128]], compare_op=ALU.is_ge,
                                                 fill=0.0, base=0, channel_multiplier=-1), writes=["ucs_f"])
            op("pool", lambda e: e.memset(ucs_f[0:64, 64:128], 0.0), writes=["ucs_f"])
            op("pool", lambda e: e.memset(mc0_f, 0.0), writes=["mc0_f"])
            op("pool", lambda e: e.memset(mc0_f[0:64, :], 1.0), writes=["mc0_f"])
            op("pool", lambda e: e.memset(mc1_f, 0.0), writes=["mc1_f"])
            op("pool", lambda e: e.memset(mc1_f[64:128, :], 1.0), writes=["mc1_f"])
            op("pool", lambda e: e.memset(maskneg_f, 0.0), writes=["maskneg_f"])
            op("pool", lambda e: e.affine_select(out=maskneg_f, in_=maskneg_f, pattern=[[-1, 128]], compare_op=ALU.is_ge,
                                                 fill=NEG, base=0, channel_multiplier=1), writes=["maskneg_f"])
            op("pool", lambda e: e.memset(maskneg_f[64:128, 0:64], NEG), writes=["maskneg_f"])
            op("pool", lambda e: e.memset(strict_b, 1.0), writes=["strict_b"])
            op("pool", lambda e: e.affine_select(out=strict_b, in_=strict_b, pattern=[[-1, 128]], compare_op=ALU.is_gt,
                                                 fill=0.0, base=0, channel_multiplier=1), writes=["strict_b"])
            op("pool", lambda e: e.memset(strict_b[64:128, 0:64], 0.0), writes=["strict_b"])
            op("dve", lambda e: e.memset(epsc, EPS), writes=["epsc"])
            op("sp", lambda e: e.dma_start(out=g1col, in_=g1_d), writes=["g1col"], dma=True)
            op("sp", lambda e: e.dma_start(out=cw, in_=convw_d), writes=["cw"], dma=True)
            op("sp", lambda e: e.dma_start(out=invf, in_=invf_d.partition_broadcast(128)), writes=["invf"], dma=True)
            op("sp", lambda e: e.dma_start(out=dtb, in_=dtb_d.partition_broadcast(128)), writes=["dtb"], dma=True)
            op("sp", lambda e: e.dma_start(out=negA, in_=alog_d.partition_broadcast(128)), writes=["negA"], dma=True)
            op("sp", lambda e: e.dma_start(out=angb, in_=ang_d.partition_broadcast(128)), writes=["angb"], dma=True)
            op("sp", lambda e: e.dma_start(out=posi, in_=pos_d), writes=["posi"], dma=True)
            op("act", lambda e: e.activation(out=negA, in_=negA, func=AF.Exp), reads=["negA"], writes=["negA"])
            op("dve", lambda e: e.tensor_scalar(out=negA, in0=negA, scalar1=-1.0, scalar2=None, op0=ALU.mult),
               reads=["negA"], writes=["negA"])

            R_A = 9 * K
            R_W = R_A + 32 * K
            R_Z = R_W + 16 * K
            R_Q = R_Z + 50 * K
            R_S = R_Q + 48 * K
            hT = view(R_A, [128, 8, S], BF16)
            wstage = view(R_W, [128, 8, 256], F32)
            wbf = [view(R_W + 8 * K + i * 4 * K, [128, 8, 256], BF16) for i in range(2)]
            zqkvT = view(R_Z, [128, 12, S + 4], BF16)
            bqT = view(R_Z, [128, 4, S], BF16)
            iqT = view(R_Z + 16 * K, [128, 4, S], BF16)
            azs = view(R_Z + 32 * K, [128, NT, 512], BF16)
            qkv_tok = view(R_Q, [128, NT, 1536], BF16)
            sa = Alloc(R_S, ARENA_BYTES)
            kz = [[sa([128, S], BF16) for _ in range(2)] for _ in range(2)]
            ikT2 = sa([128, S], BF16)
            bv_tok = sa([128, NT, 130], BF16)
            ab_tok = sa([128, NT, 16], F32)
            iw_tok = sa([128, NT, 8], F32)
            diagw = sa([128, 48, 128], BF16)
            R_WORK = sa.off

            def rope_tables():
                wa = Alloc(R_Q, R_Q + 48 * K)
                ang = wa([128, NT, 32], F32)
                tmp = wa([128, NT, 32], F32)
                ki = wa([128, NT, 32], I32)
                op("dve", lambda e: e.tensor_copy(out=posf, in_=posi), reads=["posi"], writes=["posf"])
                op("dve", lambda e: e.tensor_tensor(out=ang, in0=posf.unsqueeze(2).to_broadcast([128, NT, 32]),
                                                    in1=invf.unsqueeze(1).to_broadcast([128, NT, 32]), op=ALU.mult),
                   reads=["posf", "invf"], writes=["ang"])
                for which, shift in ((1, 0.0), (0, math.pi / 2.0)):
                    dst = cs[:, :, which * 32:(which + 1) * 32]
                    op("dve", lambda e, shift=shift: e.tensor_scalar(out=tmp, in0=ang, scalar1=shift, scalar2=None, op0=ALU.add),
                       reads=["ang"], writes=["rt_tmp"])
                    op("dve", lambda e: e.tensor_scalar(out=ki, in0=tmp, scalar1=1.0 / TWO_PI, scalar2=None, op0=ALU.mult),
                       reads=["rt_tmp"], writes=["rt_ki"])
                    op("dve", lambda e, dst=dst: e.tensor_copy(out=dst, in_=ki), reads=["rt_ki"], writes=["cs"])
                    op("dve", lambda e, dst=dst: e.scalar_tensor_tensor(out=dst, in0=dst, scalar=-TWO_PI, in1=tmp,
                                                                       op0=ALU.mult, op1=ALU.add),
                       reads=["cs", "rt_tmp"], writes=["cs"])
                    op("dve", lambda e, dst=dst: e.tensor_scalar(out=dst, in0=dst, scalar1=math.pi, scalar2=-math.pi,
                                                                op0=ALU.min, op1=ALU.max), reads=["cs"], writes=["cs"])
                    op("act", lambda e, dst=dst: e.activation(out=dst, in_=dst, func=AF.Sin), reads=["cs"], writes=["cs"])

            rope_tables()

            def phase1(hT_dst, keyp):
                wa = Alloc(R_Q + 16 * K, R_Q + 48 * K)
                xt = [wa([128, D], F32) for _ in range(2)]
                hb = [wa([128, D], BF16) for _ in range(2)]
                junk = wa([128, D], BF16)
                ss1 = wa([128, NT], F32)
                rstd1 = wa([128, NT], F32)
                op("dve", lambda e: e.memset(ss1, 0.0), writes=[keyp + "ss1"])
                for tt in range(NT):
                    b = tt % 2
                    op("sp", lambda e, tt=tt, b=b: e.dma_start(out=xt[b], in_=x_d[tt * 128:(tt + 1) * 128, :]),
                       writes=[(keyp + "xt", b)], dma=True)
                    op("act", lambda e, tt=tt, b=b: e.activation(out=junk, in_=xt[b], func=AF.Square,
                                                                 accum_out=ss1[:, tt:tt + 1]),
                       reads=[(keyp + "xt", b), keyp + "ss1"], writes=[keyp + "junk", (keyp + "ss1", tt)])
                    op("act", lambda e, tt=tt: e.activation(out=rstd1[:, tt:tt + 1], in_=ss1[:, tt:tt + 1], func=AF.Sqrt,
                                                            bias=epsc, scale=1.0 / D),
                       reads=[(keyp + "ss1", tt), "epsc"], writes=[(keyp + "rstd1", tt)])
                    op("dve", lambda e, tt=tt: e.reciprocal(out=rstd1[:, tt:tt + 1], in_=rstd1[:, tt:tt + 1]),
                       reads=[(keyp + "rstd1", tt)], writes=[(keyp + "rstd1", tt)])
                    op("dve", lambda e, tt=tt, b=b: e.tensor_scalar(out=hb[b], in0=xt[b], scalar1=rstd1[:, tt:tt + 1],
                                                                    scalar2=None, op0=ALU.mult),
                       reads=[(keyp + "xt", b), (keyp + "rstd1", tt)], writes=[(keyp + "hb", b)])
                    pbv = PBb(tt % 2)
                    for k in range(8):
                        op("pe", lambda e, k=k, b=b, pbv=pbv: e.transpose(out=pbv[:, k * 128:(k + 1) * 128],
                                                                          in_=hb[b][:, k * 128:(k + 1) * 128], identity=ident_b),
                           reads=[(keyp + "hb", b), "ident_b"], writes=[("pb", tt % 2)])
                    op("act", lambda e, tt=tt, pbv=pbv: e.copy(out=hT_dst[:, :, tt * 128:(tt + 1) * 128],
                                                               in_=pbv.rearrange("p (k t) -> p k t", k=8)),
                       reads=[("pb", tt % 2)], writes=[("hT", tt)])

            phase1(hT, "p1")
            if stop_after == "p1":
                sch.barrier()
                return
            ALL_HT = [("hT", tt) for tt in range(NT)]

            wchunk_i = [0]

            def load_w(ranges):
                i = wchunk_i[0]
                wchunk_i[0] += 1
                b = i % 2
                off = 0
                for (c0, w) in ranges:
                    op("sp", lambda e, c0=c0, w=w, off=off: e.dma_start(out=wstage[:, :, off:off + w],
                                                                        in_=w_in_v[:, :, c0:c0 + w]),
                       writes=["wstage"], dma=True)
                    off += w
                tot = off
                op("pool", lambda e, b=b, tot=tot: e.tensor_tensor(out=wbf[b][:, :, 0:tot], in0=wstage[:, :, 0:tot],
                                                                   in1=g1col.unsqueeze(2).to_broadcast([128, 8, tot]),
                                                                   op=ALU.mult),
                   reads=["wstage", "g1col"], writes=[("wbf", b)])
                return wbf[b], ("wbf", b), tot

            for ci in range(48):
                op("pool", lambda e, ci=ci: e.tensor_scalar(out=diagw[:, ci, :], in0=ident_f, scalar1=cw[:, ci:ci + 1],
                                                            scalar2=None, op0=ALU.mult),
                   reads=["ident_f", "cw"], writes=[("diagw", ci)])
            op("pool", lambda e: e.memset(zqkvT[:, :, 0:4], 0.0), writes=["zpad"])

            cva = Alloc(R_WORK, ARENA_BYTES)
            convtmp = [cva([128, 512], BF16) for _ in range(2)]
            evq = [0]

            def evac_copy(out, in_, reads, writes):
                evq[0] += 1
                if evq[0] % 2 == 0:
                    op("act", lambda e: e.copy(out=out, in_=in_), reads=reads, writes=writes)
                else:
                    op("dve", lambda e: e.tensor_copy(out=out, in_=in_), reads=reads, writes=writes)

            pbi = [0]

            def g1_proj(c, wt, wkey, ct):
                for tb in range(4):
                    bk = 2 + (pbi[0] % 2)
                    pbi[0] += 1
                    for k in range(8):
                        op("pe", lambda e, k=k, tb=tb, bk=bk: e.matmul(
                            PB(bk), lhsT=wt[:, k, ct * 128:(ct + 1) * 128], rhs=hT[:, k, tb * 512:(tb + 1) * 512],
                            start=(k == 0), stop=(k == 7)),
                           reads=[wkey] + ALL_HT[tb * 4:tb * 4 + 4], writes=[("pb", bk)])
                    evac_copy(zqkvT[:, c, 4 + tb * 512:4 + (tb + 1) * 512], PB(bk), [("pb", bk)], [("zq", c, tb)])

            def g1_conv(c):
                for tb in range(4):
                    bk = 4 + (tb % 2)
                    for j in range(4):
                        op("pe", lambda e, tb=tb, j=j, bk=bk: e.matmul(
                            PB(bk), lhsT=diagw[:, c * 4 + j, :], rhs=zqkvT[:, c, tb * 512 + j + 1:tb * 512 + j + 1 + 512],
                            start=(j == 0), stop=(j == 3)),
                           reads=[("diagw", c * 4 + j), ("zq", c, tb), "zpad"] + ([("zq", c, tb - 1)] if tb > 0 else []),
                           writes=[("pb", bk)])
                    ctb = tb % 2
                    op("act", lambda e, bk=bk, ctb=ctb: e.activation(out=convtmp[ctb], in_=PB(bk), func=AF.Silu),
                       reads=[("pb", bk)], writes=[("convtmp", ctb)])
                    tbk = 6 + (tb % 2)
                    for q in range(4):
                        op("pe", lambda e, q=q, ctb=ctb, tbk=tbk: e.transpose(out=PBb(tbk)[:, q * 128:(q + 1) * 128],
                                                                              in_=convtmp[ctb][:, q * 128:(q + 1) * 128],
                                                                              identity=ident_b),
                           reads=[("convtmp", ctb), "ident_b"], writes=[("pb", tbk)])
                    op("dve", lambda e, tb=tb, tbk=tbk: e.tensor_copy(
                        out=qkv_tok[:, tb * 4:(tb + 1) * 4, c * 128:(c + 1) * 128],
                        in_=PBb(tbk)[:, 0:512].rearrange("p (q t) -> p q t", q=4)),
                       reads=[("pb", tbk)], writes=[("qkv_tok", tb * 4 + q, c) for q in range(4)])

            prev_c = None
            nxt_w = load_w([(0, 256)])
            for chunk in range(6):
                wt, wkey, _ = nxt_w
                for ct in range(2):
                    c = chunk * 2 + ct
                    g1_proj(c, wt, wkey, ct)
                    if ct == 0:
                        nxt_w = load_w([((chunk + 1) * 256, 256)]) if chunk + 1 < 6 else load_w([(C_AZ, 256)])
                    if prev_c is not None:
                        g1_conv(prev_c)
                    prev_c = c
            g1_conv(prev_c)
            pending_w = [nxt_w]

            if "qkv_tok" in dbg:
                op("sp", lambda e: e.dma_start(out=dbg["qkv_tok"].rearrange("(t p) c -> p t c", p=128), in_=qkv_tok),
                   reads=[("qkv_tok", tt, c) for tt in range(NT) for c in range(12)], writes=["dbg_qkv_tok"], dma=True)
                op("sp", None, reads=["dbg_qkv_tok"])
            sch.barrier()
            if stop_after == "g1":
                return

            rwa = Alloc(cva.off, ARENA_BYTES)
            zr = [rwa([128, 256], F32) for _ in range(2)]
            rt = [rwa([128, 4, 32], F32) for _ in range(4)]
            roped = [rwa([128, 256], BF16) for _ in range(2)]
            op("pool", lambda e: e.memset(bv_tok, 1.0), writes=["bv_ones"])
            for a_ in range(2):
                for b_ in range(2):
                    op("pool", lambda e, a_=a_, b_=b_: e.memset(kz[a_][b_], 0.0), writes=["kz0"])

            def rope_ops(src, nh, dst_views, tt, rkey, wkeys, b):
                sv = src.rearrange("p (h d) -> p h d", h=nh)
                x1 = sv[:, :, 0:32]
                x2 = sv[:, :, 32:64]
                cc = cs[:, tt, 0:32].unsqueeze(1).to_broadcast([128, nh, 32])
                sn = cs[:, tt, 32:64].unsqueeze(1).to_broadcast([128, nh, 32])
                t = [r[:, 0:nh, :] for r in rt]
                op("dve", lambda e: e.tensor_tensor(out=t[0], in0=x1, in1=cc, op=ALU.mult), reads=[rkey, "cs"], writes=[("rt", 0)])
                op("pool", lambda e: e.tensor_tensor(out=t[1], in0=x2, in1=sn, op=ALU.mult), reads=[rkey, "cs"], writes=[("rt", 1)])
                op("pool", lambda e: e.tensor_tensor(out=t[2], in0=x2, in1=cc, op=ALU.mult), reads=[rkey, "cs"], writes=[("rt", 2)])
                op("dve", lambda e: e.tensor_tensor(out=t[3], in0=x1, in1=sn, op=ALU.mult), reads=[rkey, "cs"], writes=[("rt", 3)])
                for i, dv in enumerate(dst_views):
                    eng = "dve" if i % 2 == 0 else "pool"
                    op(eng, lambda e, dv=dv: e.tensor_tensor(out=dv[:, :, 0:32], in0=t[0], in1=t[1], op=ALU.subtract),
                       reads=[("rt", 0), ("rt", 1)], writes=wkeys)
                    op(eng, lambda e, dv=dv: e.tensor_tensor(out=dv[:, :, 32:64], in0=t[2], in1=t[3], op=ALU.add),
                       reads=[("rt", 2), ("rt", 3)], writes=wkeys)

            def tok_chunk(ranges, handler, sel=None, next_ranges=None):
                if pending_w[0] is not None:
                    wt, wkey, tot = pending_w[0]
                    pending_w[0] = None
                else:
                    wt, wkey, tot = load_w(ranges)
                lo, hi = (0, tot) if sel is None else sel
                pend = []
                for tt in range(NT):
                    if tt == 6 and next_ranges is not None:
                        pending_w[0] = load_w(next_ranges)
                    bk = 2 + (tt % 2)
                    for k in range(8):
                        op("pe", lambda e, k=k, tt=tt, bk=bk, wt=wt: e.matmul(
                            PB(bk)[:, 0:hi - lo], lhsT=hT[:, k, tt * 128:(tt + 1) * 128], rhs=wt[:, k, lo:hi],
                            start=(k == 0), stop=(k == 7)),
                           reads=[wkey, ("hT", tt)], writes=[("pb", bk)])
                    if tt >= 1:
                        pend.append(handler(tt - 1, 2 + ((tt - 1) % 2)))
                    if len(pend) >= 2:
                        p2 = pend.pop(0)
                        if p2 is not None:
                            p2()
                pend.append(handler(NT - 1, 2 + ((NT - 1) % 2)))
                for p2 in pend:
                    if p2 is not None:
                        p2()

            for j in range(2):
                def h_az(tt, bk, j=j):
                    op("act", lambda e: e.activation(out=azs[:, tt, j * 256:(j + 1) * 256], in_=PB(bk)[:, 0:256], func=AF.Silu),
                       reads=[("pb", bk)], writes=[("azs", tt, j)])
                tok_chunk([(C_AZ + j * 256, 256)], h_az, next_ranges=[(C_AZ + 256, 256)] if j == 0 else [(C_BQ, 256)])

            if stop_after == "u1":
                return
            for (c0, dstT, nm) in ((C_BQ, bqT, "bqT"), (C_IQ, iqT, "iqT")):
                for j in range(2):
                    def h_q(tt, bk, j=j, dstT=dstT, nm=nm):
                        b = tt % 2
                        op("act", lambda e: e.copy(out=zr[b], in_=PB(bk)[:, 0:256]), reads=[("pb", bk)], writes=[("zr", b)])
                        rope_ops(zr[b], 4, [roped[b].rearrange("p (h d) -> p h d", h=4)], tt, ("zr", b), [("roped", b)], b)
                        tbk = 6 + b

                        def part2():
                            for q in range(2):
                                op("pe", lambda e, q=q: e.transpose(out=PBb(tbk)[:, q * 128:(q + 1) * 128],
                                                                    in_=roped[b][:, q * 128:(q + 1) * 128], identity=ident_b),
                                   reads=[("roped", b), "ident_b"], writes=[("pb", tbk)])
                            op("act", lambda e: e.copy(out=dstT[:, 2 * j:2 * j + 2, tt * 128:(tt + 1) * 128],
                                                       in_=PBb(tbk)[:, 0:256].rearrange("p (q t) -> p q t", q=2)),
                               reads=[("pb", tbk)], writes=[(nm, tt, j)])
                        return part2
                    nr = [(c0 + 256, 256)] if j == 0 else ([(C_IQ, 256)] if c0 == C_BQ else [(C_BK, 256)])
                    tok_chunk([(c0 + j * 256, 256)], h_q, next_ranges=nr)

            if stop_after == "u23":
                return
            def h_kv(tt, bk):
                b = tt % 2
                op("act", lambda e: e.copy(out=zr[b], in_=PB(bk)[:, 0:256]), reads=[("pb", bk)], writes=[("zr", b)])
                rv = roped[b].rearrange("p (h d) -> p h d", h=4)
                rope_ops(zr[b][:, 0:128], 2, [rv[:, 0:2, :]], tt, ("zr", b), [("roped", b)], b)
                op("pool", lambda e: e.tensor_copy(out=rv[:, 2, :], in_=rv[:, 1, :]), reads=[("roped", b)], writes=[("roped", b)])
                op("pool", lambda e: e.tensor_copy(out=rv[:, 3, :], in_=rv[:, 0, :]), reads=[("roped", b)], writes=[("roped", b)])
                def part2():
                    tbk = 6 + b
                    for q in range(2):
                        op("pe", lambda e, q=q: e.transpose(out=PBb(tbk)[:, q * 128:(q + 1) * 128],
                                                            in_=roped[b][:, q * 128:(q + 1) * 128], identity=ident_b),
                           reads=[("roped", b), "ident_b"], writes=[("pb", tbk)])
                    ts_ = slice(tt * 128, (tt + 1) * 128)
                    op("act", lambda e: e.copy(out=kz[0][0][0:64, ts_], in_=PBb(tbk)[0:64, 0:128]), reads=[("pb", tbk), "kz0"],
                       writes=[("bkT", tt)])
                    op("act", lambda e: e.copy(out=kz[1][1][64:128, ts_], in_=PBb(tbk)[64:128, 0:128]), reads=[("pb", tbk)],
                       writes=[("bkT", tt)])
                    op("act", lambda e: e.copy(out=kz[1][0][0:64, ts_], in_=PBb(tbk)[0:64, 128:256]), reads=[("pb", tbk)],
                       writes=[("bkT", tt)])
                    op("act", lambda e: e.copy(out=kz[0][1][64:128, ts_], in_=PBb(tbk)[64:128, 128:256]), reads=[("pb", tbk)],
                       writes=[("bkT", tt)])

                op("dve", lambda e: e.tensor_copy(out=bv_tok[:, tt, 0:64], in_=zr[b][:, 128:192]), reads=[("zr", b), "bv_ones"],
                   writes=[("bv", tt)])
                op("dve", lambda e: e.tensor_copy(out=bv_tok[:, tt, 65:129], in_=zr[b][:, 192:256]), reads=[("zr", b)],
                   writes=[("bv", tt)])
                return part2
            tok_chunk([(C_BK, 256)], h_kv, next_ranges=[(C_IW + 8 - 256, 256)])

            if stop_after == "u4a":
                return
            IW_SCALE = (8 ** -0.5) * (64 ** -0.5)

            def h_small(tt, bk):
                b = tt % 2
                op("act", lambda e: e.copy(out=zr[b][:, 0:72], in_=PB(bk)[:, 0:72]), reads=[("pb", bk)], writes=[("zr", b)])
                rv = roped[b].rearrange("p (h d) -> p h d", h=4)
                rope_ops(zr[b][:, 0:64], 1, [rv[:, 0:1, :], rv[:, 1:2, :]], tt, ("zr", b), [("roped", b)], b)
                def part2():
                    tbk = 6 + b
                    op("pe", lambda e: e.transpose(out=PBb(tbk)[:, 0:128], in_=roped[b][:, 0:128], identity=ident_b),
                       reads=[("roped", b), "ident_b"], writes=[("pb", tbk)])
                    op("act", lambda e: e.copy(out=ikT2[:, tt * 128:(tt + 1) * 128], in_=PBb(tbk)[:, 0:128]),
                       reads=[("pb", tbk)], writes=[("ikT", tt)])

                op("dve", lambda e: e.tensor_scalar(out=iw_tok[:, tt, :], in0=zr[b][:, 64:72], scalar1=IW_SCALE, scalar2=None,
                                                    op0=ALU.mult), reads=[("zr", b)], writes=[("iw", tt)])
                return part2
            tok_chunk([(C_IW + 8 - 256, 256)], h_small, sel=(184, 256), next_ranges=[(C_BETA, 256)])

            def h_ab(tt, bk):
                op("act", lambda e: e.copy(out=ab_tok[:, tt, :], in_=PB(bk)[:, 0:16]), reads=[("pb", bk)], writes=[("ab", tt)])
            tok_chunk([(C_BETA, 256)], h_ab, sel=(0, 16))

            for nm, t_, shape in (("bqT", bqT, None), ("iqT", iqT, None)):
                if nm in dbg:
                    op("sp", lambda e, nm=nm, t_=t_: e.dma_start(out=dbg[nm].rearrange("(a p) t -> p a t", p=128), in_=t_),
                       reads=[(nm, tt, j) for tt in range(NT) for j in range(2)], writes=["dbg_" + nm], dma=True)
                    op("sp", None, reads=["dbg_" + nm])
            if "misc" in dbg:
                sch.barrier()
                mt = view(R_W, [128, NT, 154], F32)
                op("dve", lambda e: e.tensor_copy(out=mt[:, :, 0:16], in_=ab_tok), reads=[("ab", tt) for tt in range(NT)], writes=["mt"])
                op("dve", lambda e: e.tensor_copy(out=mt[:, :, 16:24], in_=iw_tok), reads=[("iw", tt) for tt in range(NT)], writes=["mt"])
                op("dve", lambda e: e.tensor_copy(out=mt[:, :, 24:154], in_=bv_tok), reads=[("bv", tt) for tt in range(NT)], writes=["mt"])
                op("sp", lambda e: e.dma_start(out=dbg["misc"].rearrange("(t p) c -> p t c", p=128), in_=mt), reads=["mt"],
                   writes=["dbg_misc"], dma=True)
                op("sp", None, reads=["dbg_misc"])
            sch.barrier()
            if stop_after == "p2":
                return

            class MultiAlloc:
                def __init__(self, regions):
                    self.regs = [[a, b] for a, b in regions]

                def __call__(self, shape, dt):
                    n = 1
                    for s_ in shape[1:]:
                        n *= s_
                    size = (n * DT_SIZE[dt] + 63) // 64 * 64
                    for r in self.regs:
                        r[0] = (r[0] + 63) // 64 * 64
                        if r[0] + size <= r[1]:
                            v = view(r[0], shape, dt)
                            r[0] += size
                            return v
                    raise AssertionError(("MultiAlloc out of space", shape, self.regs))

            def dump(name, ap, reads):
                if name in dbg:
                    op("sp", lambda e: e.dma_start(out=dbg[name], in_=ap), reads=reads, writes=["dbg_" + name], dma=True)
                    op("sp", None, reads=["dbg_" + name])

            o_aT = view(R_A, [128, 4, S], BF16)
            diagw_off = R_WORK - 12 * K
            ga = MultiAlloc([(R_W, R_W + 16 * K), (R_A + 16 * K, R_A + 32 * K), (diagw_off, ARENA_BYTES)])
            g_all = ga([128, NT, 8], F32)
            bet = ga([128, NT, 8], F32)
            gs = ga([128, 24], F32)
            eG = ga([128, 8], F32)
            eGlmG = ga([128, 8], F32)
            scs = [ga([128, 4], F32) for _ in range(2)]
            g_bc = ga([128, 8, 128], F32)
            sq = ga([128, 1024], F32)
            ssn = ga([128, 16], F32)
            rn = ga([128, 16], F32)
            cq = ga([128, 8], F32)
            cqd = ga([128, 8], F32)
            cbk = ga([128, 8], F32)
            ckd = ga([128, 8], F32)
            negbeta = ga([128, 8], F32)
            qn = ga([128, 512], BF16)
            qd = ga([128, 512], BF16)
            kn = ga([128, 512], BF16)
            rhsk = ga([128, 512], BF16)
            kdec = ga([128, 512], BF16)
            rhsv = ga([128, 512], BF16)
            qnT = ga([128, 4, 128], BF16)
            qdT = ga([128, 4, 128], BF16)
            knT = ga([128, 4, 128], BF16)
            Dm = ga([128, 8, 128], BF16)
            Ds = ga([128, 8, 128], BF16)
            Mm = [ga([128, 8, 128], BF16) for _ in range(2)]
            Nm = [ga([128, 8, 128], BF16) for _ in range(2)]
            Pm = [ga([128, 8, 128], BF16) for _ in range(2)]
            qkm = ga([128, 8, 128], BF16)
            qkT_sb = ga([128, 8, 128], BF16)
            u_c = ga([128, 2, 512], F32)
            w_tok = ga([128, 512], BF16)
            wT_sb = ga([128, 4, 128], BF16)
            vn_b = ga([128, 512], BF16)
            Sst = ga([128, 4, 128], F32)
            Stmp = ga([128, 4, 128], F32)
            S_bd = ga([128, 4, 128], BF16)
            bdmask = ga([128, 4, 128], BF16)
            o_c = ga([128, 512], F32)
            qkT_c1 = ga([128, 8, 64], BF16)
            kdec_c1 = ga([128, 512], BF16)
            az_c = ga([128, 512], BF16)
            ss2 = ga([128, 8], F32)
            r2 = ga([128, 8], F32)
            oa_b = ga([128, 512], BF16)

            def bc8(v):
                return v.unsqueeze(2).to_broadcast([128, 8, 64])

            ABK = [("ab", tt) for tt in range(NT)]
            op("act", lambda e: e.activation(out=bet, in_=ab_tok[:, :, 0:8], func=AF.Sigmoid), reads=["ab_all"], writes=["bet"])
            op("dve", lambda e: e.tensor_tensor(out=g_all, in0=ab_tok[:, :, 8:16], in1=dtb.unsqueeze(1).to_broadcast([128, NT, 8]),
                                                op=ALU.add), reads=["ab_all", "dtb"], writes=["g_all"])
            op("act", lambda e: e.activation(out=g_all, in_=g_all, func=AF.Exp), reads=["g_all"], writes=["g_all"])
            op("act", lambda e: e.activation(out=g_all, in_=g_all, func=AF.Ln, bias=1.0), reads=["g_all"], writes=["g_all"])
            op("dve", lambda e: e.tensor_tensor(out=g_all, in0=g_all, in1=negA.unsqueeze(1).to_broadcast([128, NT, 8]),
                                                op=ALU.mult), reads=["g_all", "negA"], writes=["g_all"])
            op("dve", lambda e: e.memset(Sst, 0.0), writes=["S"])
            op("dve", lambda e: e.memset(S_bd, 0.0), writes=["S_bd"])
            op("pool", lambda e: e.memset(bdmask, 0.0), writes=["bdmask"])
            op("pool", lambda e: e.memset(bdmask[0:64, :, 0:64], 1.0), writes=["bdmask"])
            op("pool", lambda e: e.memset(bdmask[64:128, :, 64:128], 1.0), writes=["bdmask"])
            if "g" in dbg:
                op("sp", lambda e: e.dma_start(out=dbg["g"].rearrange("(t p) c -> p t c", p=128), in_=g_all), reads=["g_all"],
                   writes=["dbg_g"], dma=True)
                op("sp", None, reads=["dbg_g"])

            if stop_after == "gdn_pre":
                return
            for tt in range(NT):
                op("pe", lambda e, tt=tt: e.matmul(PB(0)[:, 0:8], lhsT=ucs_f, rhs=g_all[:, tt, :], start=True, stop=True),
                   reads=["g_all"], writes=[("pb", 0)])
                op("pe", lambda e, tt=tt: e.matmul(PB(0)[:, 8:16], lhsT=mc0_f, rhs=g_all[:, tt, :], start=True, stop=True),
                   reads=["g_all"], writes=[("pb", 0)])
                op("pe", lambda e, tt=tt: e.matmul(PB(0)[:, 16:24], lhsT=mc1_f, rhs=g_all[:, tt, :], start=True, stop=True),
                   reads=["g_all"], writes=[("pb", 0)])
                op("act", lambda e: e.copy(out=gs, in_=PB(0)[:, 0:24]), reads=[("pb", 0)], writes=["gs"])
                op("act", lambda e: e.activation(out=eG, in_=gs[:, 0:8], func=AF.Exp), reads=["gs"], writes=["eG"])
                op("dve", lambda e: e.tensor_tensor(out=eGlmG[0:64, :], in0=gs[0:64, 8:16], in1=gs[0:64, 0:8], op=ALU.subtract),
                   reads=["gs"], writes=["eGlmG"])
                op("dve", lambda e: e.tensor_tensor(out=eGlmG[64:128, :], in0=gs[64:128, 16:24], in1=gs[64:128, 0:8],
                                                    op=ALU.subtract), reads=["gs"], writes=["eGlmG"])
                op("act", lambda e: e.activation(out=eGlmG, in_=eGlmG, func=AF.Exp), reads=["eGlmG"], writes=["eGlmG"])
                for hf in range(2):
                    c0 = 8 + 8 * hf
                    op("act", lambda e, hf=hf, c0=c0: e.activation(out=scs[hf][0:64, :], in_=gs[0:64, c0:c0 + 8:2], func=AF.Exp),
                       reads=["gs"], writes=[("scs", hf)])
                    op("act", lambda e, hf=hf, c0=c0: e.activation(out=scs[hf][64:128, :], in_=gs[64:128, c0 + 1:c0 + 8:2],
                                                                   func=AF.Exp), reads=["gs"], writes=[("scs", hf)])
                op("dve", lambda e, tt=tt: e.tensor_scalar(out=g_bc, in0=g_all[:, tt, :].unsqueeze(2).to_broadcast([128, 8, 128]),
                                                           scalar1=-1.0, scalar2=None, op0=ALU.mult),
                   reads=["g_all"], writes=["g_bc"])
                if stop_after == "gdn_a":
                    return
                QK = [("qkv_tok", tt, c) for c in range(8)]
                VV = [("qkv_tok", tt, c) for c in range(8, 12)]
                op("dve", lambda e, tt=tt: e.tensor_tensor(out=sq, in0=qkv_tok[:, tt, 0:1024], in1=qkv_tok[:, tt, 0:1024],
                                                           op=ALU.mult), reads=["qkv_all"], writes=["sq"])
                op("dve", lambda e: e.tensor_reduce(out=ssn, in_=sq.rearrange("p (h d) -> p h d", h=16), axis=AX.X, op=ALU.add),
                   reads=["sq"], writes=["ssn"])
                op("act", lambda e: e.activation(out=rn, in_=ssn, func=AF.Sqrt, bias=epsc, scale=1.0), reads=["ssn", "epsc"],
                   writes=["rn"])
                op("dve", lambda e: e.reciprocal(out=rn, in_=rn), reads=["rn"], writes=["rn"])
                op("dve", lambda e: e.tensor_scalar(out=cq, in0=rn[:, 0:8], scalar1=0.125, scalar2=None, op0=ALU.mult),
                   reads=["rn"], writes=["cq"])
                op("dve", lambda e: e.tensor_tensor(out=cqd, in0=cq, in1=eG, op=ALU.mult), reads=["cq", "eG"], writes=["cqd"])
                op("dve", lambda e, tt=tt: e.tensor_tensor(out=cbk, in0=rn[:, 8:16], in1=bet[:, tt, :], op=ALU.mult),
                   reads=["rn", "bet"], writes=["cbk"])
                op("dve", lambda e: e.tensor_tensor(out=cbk, in0=cbk, in1=eG, op=ALU.mult), reads=["cbk", "eG"], writes=["cbk"])
                op("dve", lambda e: e.tensor_tensor(out=ckd, in0=rn[:, 8:16], in1=eGlmG, op=ALU.mult), reads=["rn", "eGlmG"],
                   writes=["ckd"])
                op("dve", lambda e, tt=tt: e.tensor_scalar(out=negbeta, in0=bet[:, tt, :], scalar1=-1.0, scalar2=None,
                                                           op0=ALU.mult), reads=["bet"], writes=["negbeta"])
                qv = qkv_tok[:, tt, 0:512].rearrange("p (h d) -> p h d", h=8)
                kv = qkv_tok[:, tt, 512:1024].rearrange("p (h d) -> p h d", h=8)
                vv = qkv_tok[:, tt, 1024:1536].rearrange("p (h d) -> p h d", h=8)

                def v3(t_):
                    return t_.rearrange("p (h d) -> p h d", h=8)
                op("dve", lambda e, qv=qv: e.tensor_tensor(out=v3(qn), in0=qv, in1=bc8(cq), op=ALU.mult),
                   reads=["qkv_all", "cq"], writes=["qn"])
                op("pool", lambda e, qv=qv: e.tensor_tensor(out=v3(qd), in0=qv, in1=bc8(cqd), op=ALU.mult),
                   reads=["qkv_all", "cqd"], writes=["qd"])
                op("dve", lambda e, kv=kv: e.tensor_tensor(out=v3(kn), in0=kv, in1=bc8(rn[:, 8:16]), op=ALU.mult),
                   reads=["qkv_all", "rn"], writes=["kn"])
                op("pool", lambda e, kv=kv: e.tensor_tensor(out=v3(rhsk), in0=kv, in1=bc8(cbk), op=ALU.mult),
                   reads=["qkv_all", "cbk"], writes=["rhsk"])
                op("pool", lambda e, kv=kv: e.tensor_tensor(out=v3(kdec), in0=kv, in1=bc8(ckd), op=ALU.mult),
                   reads=["qkv_all", "ckd"], writes=["kdec"])
                op("dve", lambda e, vv=vv, tt=tt: e.tensor_tensor(out=v3(rhsv), in0=vv, in1=bc8(bet[:, tt, :]), op=ALU.mult),
                   reads=["qkv_all", "bet"], writes=["rhsv"])
                if stop_after == "gdn_b":
                    return
                for (src, skey, dst, dkey, bank, coff, eng) in ((qn, "qn", qnT, "qnT", 6, 0, "act"), (qd, "qd", qdT, "qdT", 7, 0, "dve"),
                                                                (kn, "kn", knT, "knT", 0, 0, "act")):
                    for q in range(4):
                        op("pe", lambda e, src=src, bank=bank, coff=coff, q=q: e.transpose(
                            out=PBb(bank)[:, coff + q * 128:coff + (q + 1) * 128], in_=src[:, q * 128:(q + 1) * 128], identity=ident_b),
                           reads=[skey, "ident_b"], writes=[("pb", bank)])
                    if eng == "act":
                        op("act", lambda e, dst=dst, bank=bank, coff=coff: e.copy(
                            out=dst, in_=PBb(bank)[:, coff:coff + 512].rearrange("p (q t) -> p q t", q=4)),
                           reads=[("pb", bank)], writes=[dkey])
                    else:
                        op("dve", lambda e, dst=dst, bank=bank, coff=coff: e.tensor_copy(
                            out=dst, in_=PBb(bank)[:, coff:coff + 512].rearrange("p (q t) -> p q t", q=4)),
                           reads=[("pb", bank)], writes=[dkey])
                    if stop_after == "gdn_c_" + skey:
                        return
                if tt == 0:
                    dump("qn0", qn, ["qn"]); dump("kn0", kn, ["kn"]); dump("rhsv0", rhsv, ["rhsv"]); dump("rhsk0", rhsk, ["rhsk"])
                    dump("kdec0", kdec, ["kdec"]); dump("qd0", qd, ["qd"]); dump("gs0", gs, ["gs"])
                    dump("knT0", knT.rearrange("p a t -> p (a t)"), ["knT"])
                    dump("rn0", rn, ["rn"]); dump("cq0", cq, ["cq"]); dump("cqd0", cqd, ["cqd"]); dump("cbk0", cbk, ["cbk"])
                    dump("ckd0", ckd, ["ckd"]); dump("eG0", eG, ["eG"]); dump("ssn0", ssn, ["ssn"])
                if stop_after == "gdn_c":
                    return
                def group_gen(hg, bA, bB, bC, bT):
                    hs_list = list(range(4))
                    grp = slice(4 * hg, 4 * hg + 4)
                    for hs in hs_list:
                        h = 4 * hg + hs
                        hp, par = h // 2, h % 2
                        rows = slice(par * 64, par * 64 + 64)
                        cs_ = slice(hs * 128, hs * 128 + 128)
                        op("pe", lambda e, hp=hp, rows=rows, cs_=cs_, par=par: e.matmul(
                            PB(bA)[:, cs_], lhsT=knT[rows, hp, :], rhs=knT[rows, hp, :], start=True, stop=True,
                            tile_position=(par * 64, 0)), reads=["knT"], writes=[("pb", bA)])
                        op("pe", lambda e, hp=hp, rows=rows, cs_=cs_, par=par: e.matmul(
                            PB(bB)[:, cs_], lhsT=qnT[rows, hp, :], rhs=knT[rows, hp, :], start=True, stop=True,
                            tile_position=(par * 64, 0)), reads=["knT", "qnT"], writes=[("pb", bB)])
                        op("pe", lambda e, h=h, cs_=cs_: e.matmul(PB(bC)[:, cs_], lhsT=g_bc[:, h, :], rhs=ucs_f, start=True, stop=False),
                           reads=["g_bc", "ucs_f"], writes=[("pb", bC)])
                        op("pe", lambda e, cs_=cs_: e.matmul(PB(bC)[:, cs_], lhsT=ident_f, rhs=maskneg_f, start=False, stop=True),
                           reads=["ident_f", "maskneg_f"], writes=[("pb", bC)])
                    yield
                    for hs in hs_list:
                        h = 4 * hg + hs
                        cs_ = slice(hs * 128, hs * 128 + 128)
                        op("act", lambda e, h=h, cs_=cs_: e.activation(out=Dm[:, h, :], in_=PB(bC)[:, cs_], func=AF.Exp,
                                                                       bias=gs[:, h:h + 1], scale=1.0),
                           reads=[("pb", bC), "gs"], writes=[("Dm", h)])
                        op("pool", lambda e, h=h: e.tensor_tensor(out=Ds[:, h, :], in0=Dm[:, h, :], in1=strict_b, op=ALU.mult),
                           reads=[("Dm", h), "strict_b"], writes=[("Ds", h)])
                        op("dve", lambda e, h=h, cs_=cs_: e.scalar_tensor_tensor(out=Mm[0][:, h, :], in0=PB(bA)[:, cs_],
                                                                                 scalar=negbeta[:, h:h + 1], in1=Ds[:, h, :],
                                                                                 op0=ALU.mult, op1=ALU.mult),
                           reads=[("pb", bA), "negbeta", ("Ds", h)], writes=[("M", 0, hg)])
                        op("dve", lambda e, h=h, cs_=cs_: e.tensor_tensor(out=qkm[:, h, :], in0=PB(bB)[:, cs_], in1=Dm[:, h, :],
                                                                          op=ALU.mult),
                           reads=[("pb", bB), ("Dm", h)], writes=[("qkm", hg)])
                    yield
                    for hs in hs_list:
                        h = 4 * hg + hs
                        cs_ = slice(hs * 128, hs * 128 + 128)
                        op("pe", lambda e, h=h, cs_=cs_: e.transpose(out=PBb(bT)[:, cs_], in_=Mm[0][:, h, :], identity=ident_b),
                           reads=[("M", 0, hg), "ident_b"], writes=[("pb", bT)])
                    op("act", lambda e: e.copy(out=Nm[0][:, grp, :], in_=PBb(bT)[:, 0:512].rearrange("p (q t) -> p q t", q=4)),
                       reads=[("pb", bT)], writes=[("N", 0, hg)])
                    op("pool", lambda e: e.tensor_tensor(out=Pm[0][:, grp, :], in0=Nm[0][:, grp, :],
                                                         in1=ident_b.unsqueeze(1).to_broadcast([128, 4, 128]), op=ALU.add),
                       reads=[("N", 0, hg), "ident_b"], writes=[("P", 0, hg)])
                    yield
                    for hs in hs_list:
                        h = 4 * hg + hs
                        cs2 = slice(hs * 128, hs * 128 + 128)
                        op("pe", lambda e, h=h, cs2=cs2: e.transpose(out=PBb(bT)[:, cs2], in_=qkm[:, h, :], identity=ident_b),
                           reads=[("qkm", hg), "ident_b"], writes=[("pb", bT)])
                    op("dve", lambda e: e.tensor_copy(out=qkT_sb[:, grp, :], in_=PBb(bT)[:, 0:512].rearrange("p (q t) -> p q t", q=4)),
                       reads=[("pb", bT)], writes=[("qkT", hg)])
                    yield
                    for lv in range(1, 6):
                        cur, nxt = (lv - 1) % 2, lv % 2
                        for hs in hs_list:
                            h = 4 * hg + hs
                            cs_ = slice(hs * 128, hs * 128 + 128)
                            op("pe", lambda e, h=h, cs_=cs_, cur=cur: e.matmul(PB(bA)[:, cs_], lhsT=Nm[cur][:, h, :], rhs=Mm[cur][:, h, :],
                                                                               start=True, stop=True),
                               reads=[("N", cur, hg), ("M", cur, hg)], writes=[("pb", bA)])
                        if lv < 5:
                            for hs in hs_list:
                                h = 4 * hg + hs
                                cs_ = slice(hs * 128, hs * 128 + 128)
                                op("pe", lambda e, h=h, cs_=cs_, cur=cur: e.matmul(PB(bB)[:, cs_], lhsT=Mm[cur][:, h, :],
                                                                                   rhs=Nm[cur][:, h, :], start=True, stop=True),
                                   reads=[("N", cur, hg), ("M", cur, hg)], writes=[("pb", bB)])
                        yield
                        op("act", lambda e, nxt=nxt: e.copy(out=Mm[nxt][:, grp, :], in_=PB(bA).rearrange("p (q t) -> p q t", q=4)),
                           reads=[("pb", bA)], writes=[("M", nxt, hg)])
                        if lv < 5:
                            op("dve", lambda e, nxt=nxt: e.tensor_copy(out=Nm[nxt][:, grp, :],
                                                                       in_=PB(bB).rearrange("p (q t) -> p q t", q=4)),
                               reads=[("pb", bB)], writes=[("N", nxt, hg)])
                        for hs in hs_list:
                            h = 4 * hg + hs
                            cs_ = slice(hs * 128, hs * 128 + 128)
                            op("pe", lambda e, h=h, cs_=cs_, cur=cur, nxt=nxt: e.matmul(PB(bC)[:, cs_], lhsT=Mm[nxt][:, h, :],
                                                                                        rhs=Pm[cur][:, h, :], start=True, stop=True),
                               reads=[("M", nxt, hg), ("P", cur, hg)], writes=[("pb", bC)])
                        yield
                        op("dve", lambda e, cur=cur, nxt=nxt: e.tensor_tensor(
                            out=Pm[nxt][:, grp, :], in0=Pm[cur][:, grp, :], in1=PB(bC).rearrange("p (q t) -> p q t", q=4), op=ALU.add),
                           reads=[("pb", bC), ("P", cur, hg)], writes=[("P", nxt, hg)])

                gens = [group_gen(0, 3, 4, 5, 6), group_gen(1, 0, 1, 2, 7)]
                while gens:
                    for g_ in list(gens):
                        try:
                            next(g_)
                        except StopIteration:
                            gens.remove(g_)
                Pf = Pm[1]
                PK = [("P", 1, 0), ("P", 1, 1)]
                if tt == 0:
                    dump("D0", Dm.rearrange("p a t -> p (a t)"), [("Dm", h) for h in range(8)])
                    dump("M0", Mm[0].rearrange("p a t -> p (a t)"), [("M", 0, 0), ("M", 0, 1)])
                    dump("N0", Nm[0].rearrange("p a t -> p (a t)"), [("N", 0, 0), ("N", 0, 1)])
                    dump("P0", Pm[1].rearrange("p a t -> p (a t)"), [("P", 1, 0), ("P", 1, 1)])
                    dump("qkT0", qkT_sb.rearrange("p a t -> p (a t)"), [("qkT", 0), ("qkT", 1)])
                if stop_after == "gdn_e":
                    return
                for hf in range(2):
                    ub = 7 if hf == 0 else 0
                    for h in range(8):
                        op("pe", lambda e, h=h, hf=hf, ub=ub: e.matmul(PB(ub)[0:64, h * 64:(h + 1) * 64],
                                                                       lhsT=Pf[:, h, hf * 64:(hf + 1) * 64],
                                                                       rhs=rhsv[:, h * 64:(h + 1) * 64], start=True, stop=True),
                           reads=PK + ["rhsv"], writes=[("pb", ub)])
                    op("act", lambda e, hf=hf, ub=ub: e.copy(out=u_c[0:64, hf, :], in_=PB(ub)[0:64, :]), reads=[("pb", ub)],
                       writes=[("u_c", hf)])
                for h in range(8):
                    op("pe", lambda e, h=h: e.matmul(PB(1)[:, h * 64:(h + 1) * 64], lhsT=Pf[:, h, :], rhs=rhsk[:, h * 64:(h + 1) * 64],
                                                     start=True, stop=True), reads=PK + ["rhsk"], writes=[("pb", 1)])
                op("act", lambda e: e.copy(out=w_tok, in_=PB(1)), reads=[("pb", 1)], writes=["w_tok"])
                for q in range(4):
                    op("pe", lambda e, q=q: e.transpose(out=PBb(6)[:, q * 128:(q + 1) * 128], in_=w_tok[:, q * 128:(q + 1) * 128],
                                                        identity=ident_b), reads=["w_tok", "ident_b"], writes=[("pb", 6)])
                op("dve", lambda e: e.tensor_copy(out=wT_sb, in_=PBb(6)[:, 0:512].rearrange("p (q t) -> p q t", q=4)),
                   reads=[("pb", 6)], writes=["wT_sb"])
                op("sp", lambda e: e.dma_start(out=qkT_c1[0:64, :, :], in_=qkT_sb[64:128, :, 64:128]),
                   reads=[("qkT", 0), ("qkT", 1)], writes=["qkT_c1"], dma=True)
                op("sp", lambda e: e.dma_start(out=kdec_c1[0:64, :], in_=kdec[64:128, :]), reads=["kdec"], writes=["kdec_c1"],
                   dma=True)
                op("sp", lambda e, tt=tt: e.dma_start(out=az_c[0:64, :], in_=azs[64:128, tt, :]), reads=["azs_all"], writes=["az_c"],
                   dma=True)
                if tt == 0:
                    dump("u0", u_c.rearrange("p a t -> p (a t)"), [("u_c", 0), ("u_c", 1)])
                    dump("wT0", wT_sb.rearrange("p a t -> p (a t)"), ["wT_sb"])
                if stop_after == "gdn_f":
                    return
                for hf in range(2):
                    tcs = slice(hf * 64, hf * 64 + 64)
                    ck = 2 * tt + hf
                    if hf == 0:
                        qk_x, qk_keys = qkT_sb[0:64, :, 0:64], [("qkT", 0), ("qkT", 1)]
                        kd_x, kd_keys = kdec[0:64, :], ["kdec"]
                    else:
                        qk_x, qk_keys = qkT_c1[0:64, :, :], ["qkT_c1"]
                        kd_x, kd_keys = kdec_c1[0:64, :], ["kdec_c1"]
                    for hp in range(4):
                        op("pe", lambda e, hp=hp, tcs=tcs: e.matmul(PB(1)[0:64, hp * 128:(hp + 1) * 128], lhsT=wT_sb[:, hp, tcs],
                                                                    rhs=S_bd[:, hp, :], start=True, stop=True),
                           reads=["wT_sb", "S_bd"], writes=[("pb", 1)])
                    op("dve", lambda e, hf=hf: e.tensor_tensor(out=vn_b[0:64, :], in0=u_c[0:64, hf, :], in1=PB(1)[0:64, :],
                                                               op=ALU.subtract), reads=[("u_c", hf), ("pb", 1)], writes=["vn_b"])
                    for h in range(8):
                        hp, par = h // 2, h % 2
                        op("pe", lambda e, h=h, hp=hp, par=par, tcs=tcs: e.matmul(
                            PB(2)[0:64, h * 64:(h + 1) * 64], lhsT=qdT[:, hp, tcs], rhs=S_bd[:, hp, par * 64:(par + 1) * 64],
                            start=True, stop=False), reads=["qdT", "S_bd"], writes=[("pb", 2)])
                        op("pe", lambda e, h=h, qk_x=qk_x: e.matmul(
                            PB(2)[0:64, h * 64:(h + 1) * 64], lhsT=qk_x[:, h, :], rhs=vn_b[0:64, h * 64:(h + 1) * 64],
                            start=False, stop=True), reads=qk_keys + ["vn_b"], writes=[("pb", 2)])
                    for hp in range(4):
                        op("pe", lambda e, hp=hp, kd_x=kd_x: e.matmul(PB(7)[:, hp * 128:(hp + 1) * 128],
                                                                      lhsT=kd_x[:, hp * 128:(hp + 1) * 128],
                                                                      rhs=vn_b[0:64, hp * 128:(hp + 1) * 128], start=True, stop=True),
                           reads=kd_keys + ["vn_b"], writes=[("pb", 7)])
                    op("act", lambda e: e.copy(out=o_c[0:64, :], in_=PB(2)[0:64, :]), reads=[("pb", 2)], writes=["o_c"])
                    op("dve", lambda e, hf=hf: e.tensor_tensor(out=Stmp, in0=Sst,
                                                               in1=scs[hf].unsqueeze(2).to_broadcast([128, 4, 128]), op=ALU.mult),
                       reads=["S", ("scs", hf)], writes=["Stmp"])
                    op("dve", lambda e: e.tensor_tensor(out=Sst, in0=Stmp, in1=PB(7).rearrange("p (a d) -> p a d", a=4), op=ALU.add),
                       reads=["Stmp", ("pb", 7)], writes=["S"])
                    op("dve", lambda e: e.tensor_tensor(out=S_bd, in0=Sst, in1=bdmask, op=ALU.mult), reads=["S", "bdmask"],
                       writes=["S_bd"])
                    if "o_raw" in dbg:
                        op("sp", lambda e, ck=ck: e.dma_start(out=dbg["o_raw"][ck * 64:(ck + 1) * 64, :], in_=o_c[0:64, :]),
                           reads=["o_c"], writes=["dbg_o_raw"], dma=True)
                    sqh = sq[0:64, 0:512]
                    op("dve", lambda e: e.tensor_tensor(out=sqh, in0=o_c[0:64, :], in1=o_c[0:64, :], op=ALU.mult), reads=["o_c"],
                       writes=["sq"])
                    op("dve", lambda e: e.tensor_reduce(out=ss2[0:64, :], in_=sqh.rearrange("p (h d) -> p h d", h=8), axis=AX.X,
                                                        op=ALU.add), reads=["sq"], writes=["ss2"])
                    op("act", lambda e: e.activation(out=r2[0:64, :], in_=ss2[0:64, :], func=AF.Sqrt, bias=epsc[0:64, :],
                                                     scale=1.0 / 64), reads=["ss2", "epsc"], writes=["r2"])
                    op("dve", lambda e: e.reciprocal(out=r2[0:64, :], in_=r2[0:64, :]), reads=["r2"], writes=["r2"])
                    op("dve", lambda e: e.tensor_tensor(out=v3(sqh), in0=v3(o_c[0:64, :]),
                                                        in1=r2[0:64, :].unsqueeze(2).to_broadcast([64, 8, 64]), op=ALU.mult),
                       reads=["o_c", "r2"], writes=["sq"])
                    op("pool", lambda e: e.tensor_tensor(out=v3(sqh), in0=v3(sqh),
                                                         in1=angb[0:64, :].unsqueeze(1).to_broadcast([64, 8, 64]), op=ALU.mult),
                       reads=["sq", "angb"], writes=["sq"])
                    az_x = azs[0:64, tt, :] if hf == 0 else az_c[0:64, :]
                    op("pool", lambda e, az_x=az_x: e.tensor_tensor(out=oa_b[0:64, :], in0=sqh, in1=az_x, op=ALU.mult),
                       reads=["sq", "az_c"], writes=["oa_b"])
                    for q in range(4):
                        op("pe", lambda e, q=q: e.transpose(out=PBb(6)[:, q * 64:(q + 1) * 64], in_=oa_b[0:64, q * 128:(q + 1) * 128],
                                                            identity=ident_b[0:64, 0:64]), reads=["oa_b", "ident_b"],
                           writes=[("pb", 6)])
                    op("act", lambda e, ck=ck: e.copy(out=o_aT[:, :, ck * 64:(ck + 1) * 64],
                                                      in_=PBb(6)[:, 0:256].rearrange("p (q t) -> p q t", q=4)),
                       reads=[("pb", 6)], writes=[("o_aT", ck)])
            if "o_raw" in dbg:
                op("sp", None, reads=["dbg_o_raw"])
            if "o_aT" in dbg:
                op("sp", lambda e: e.dma_start(out=dbg["o_aT"].rearrange("(a p) t -> p a t", p=128), in_=o_aT),
                   reads=[("o_aT", ck) for ck in range(2 * NT)], writes=["dbg_o_aT"], dma=True)
                op("sp", None, reads=["dbg_o_aT"])

            sch.barrier()
            if stop_after == "gdn":
                return

            o_bT = view(R_A + 16 * K, [128, 4, S], BF16)
            da = MultiAlloc([(R_Q, R_Q + 48 * K), (R_W, R_W + 16 * K)])
            scoreb = [da([128, S], F32) for _ in range(2)]
            rl = [da([128, 512], F32) for _ in range(2)]
            maskbb = [da([128, S], BF16) for _ in range(2)]
            thr_t = [da([128, 1], F32) for _ in range(2)]
            PTt = [[da([128, 512], BF16) for _ in range(2)] for _ in range(2)]
            I4 = da([128, 512], BF16)
            lo_t = da([128, 1], F32)
            hi_t = da([128, 1], F32)
            W0 = da([128, 1], F32)
            mid_t = da([128, 1], F32)
            tsel = da([128, 1], F32)
            Wk = da([128, NBIS], F32)
            cnt = da([128, NBIS], F32)
            pow2 = da([128, NBIS], F32)
            ob = da([128, 520], F32)
            rden = da([128, 8], F32)
            ob_b = da([128, 512], BF16)
            for q in range(4):
                op("pool", lambda e, q=q: e.tensor_copy(out=I4[:, q * 128:(q + 1) * 128], in_=ident_b), reads=["ident_b"], writes=["I4"])
            for k in range(NBIS):
                op("pool", lambda e, k=k: e.memset(pow2[:, k:k + 1], 2.0 ** (-(k + 1))), writes=["pow2"])

            def scores_part(tt, sb):
                L = (tt + 1) * 128
                nkb = (L + 511) // 512
                qs = slice(tt * 128, (tt + 1) * 128)
                score = scoreb[sb]
                maskb = maskbb[sb]
                for h in range(8):
                    hp, par = h // 2, h % 2
                    rows = slice(par * 64, par * 64 + 64)
                    for kb in range(nkb):
                        w = min(512, L - kb * 512)
                        bank = kb % 2
                        ks = slice(kb * 512, kb * 512 + w)
                        op("pe", lambda e, hp=hp, par=par, rows=rows, w=w, bank=bank, ks=ks: e.matmul(
                            PB(bank)[:, 0:w], lhsT=iqT[rows, hp, qs], rhs=ikT2[rows, ks], start=True, stop=True,
                            tile_position=(par * 64, 0)), reads=["iqT", "ikT2"], writes=[("pb", bank)])
                        op("act", lambda e, w=w, bank=bank: e.activation(out=rl[bank][:, 0:w], in_=PB(bank)[:, 0:w], func=AF.Relu),
                           reads=[("pb", bank)], writes=[("rl", bank)])
                        if h == 0:
                            op("dve", lambda e, w=w, bank=bank, ks=ks: e.tensor_scalar(
                                out=score[:, ks], in0=rl[bank][:, 0:w], scalar1=iw_tok[:, tt, 0:1], scalar2=None, op0=ALU.mult),
                               reads=[("rl", bank), "iw_tok"], writes=[("score", sb, kb)])
                        else:
                            op("dve", lambda e, w=w, bank=bank, ks=ks, h=h: e.scalar_tensor_tensor(
                                out=score[:, ks], in0=rl[bank][:, 0:w], scalar=iw_tok[:, tt, h:h + 1], in1=score[:, ks],
                                op0=ALU.mult, op1=ALU.add), reads=[("rl", bank), "iw_tok", ("score", sb, kb)],
                               writes=[("score", sb, kb)], fast=(w >= 256))
                SK = [("score", sb, kb) for kb in range(nkb)]
                if tt >= 2:
                    op("dve", lambda e: e.tensor_reduce(out=hi_t, in_=score[:, 0:L], axis=AX.X, op=ALU.max), reads=SK, writes=["hi"])
                    op("dve", lambda e: e.tensor_reduce(out=lo_t, in_=score[:, 0:L], axis=AX.X, op=ALU.min), reads=SK, writes=["lo"])
                op("dve", lambda e: e.memset(score[0:64, L - 64:L], -1.0e30), reads=SK, writes=SK)
                if tt >= 2:
                    op("dve", lambda e: e.tensor_tensor(out=W0, in0=hi_t, in1=lo_t, op=ALU.subtract), reads=["hi", "lo"], writes=["W0"])
                    op("dve", lambda e: e.tensor_scalar(out=Wk, in0=pow2, scalar1=W0[:, 0:1], scalar2=None, op0=ALU.mult),
                       reads=["W0", "pow2"], writes=["Wk"])
                    op("dve", lambda e: e.memset(cnt, 0.0), writes=["cnt"])
                    op("dve", lambda e: e.tensor_tensor(out=mid_t, in0=lo_t, in1=Wk[:, 0:1], op=ALU.add), reads=["lo", "Wk"], writes=["mid"])
                    for k in range(NBIS):
                        op("dve", lambda e, k=k: e.tensor_scalar(out=maskb[:, 0:L], in0=score[:, 0:L], scalar1=mid_t[:, 0:1],
                                                                 scalar2=0.0, op0=ALU.is_gt, op1=ALU.add, accum_out=cnt[:, k:k + 1]),
                           reads=SK + ["mid", "cnt"], writes=[("maskb", sb), ("cntk", k)])
                        op("dve", lambda e, k=k: e.tensor_scalar(out=tsel, in0=cnt[:, k:k + 1], scalar1=255.5, scalar2=0.5,
                                                                 op0=ALU.is_gt, op1=ALU.subtract), reads=[("cntk", k)], writes=["tsel"])
                        op("dve", lambda e, k=k: e.scalar_tensor_tensor(out=mid_t, in0=tsel, scalar=Wk[:, k:k + 1], in1=mid_t,
                                                                        op0=ALU.mult, op1=ALU.add),
                           reads=["tsel", "Wk", "mid"], writes=["mid"])
                    op("dve", lambda e: e.scalar_tensor_tensor(out=thr_t[sb], in0=Wk[:, NBIS - 1:NBIS], scalar=-0.5, in1=mid_t,
                                                               op0=ALU.mult, op1=ALU.add), reads=["Wk", "mid"], writes=[("thr", sb)])
                else:
                    op("dve", lambda e: e.memset(thr_t[sb], -1.0e29), writes=[("thr", sb)])
                op("dve", lambda e: e.tensor_scalar(out=maskb[:, 0:L], in0=score[:, 0:L], scalar1=thr_t[sb][:, 0:1], scalar2=NEG,
                                                    op0=ALU.is_le, op1=ALU.mult), reads=SK + [("thr", sb)], writes=[("maskb", sb)])
                if "thr" in dbg:
                    op("sp", lambda e: e.dma_start(out=dbg["thr"][tt * 128:(tt + 1) * 128, :], in_=thr_t[sb]), reads=[("thr", sb)],
                       writes=["dbg_thr"], dma=True)
                if "score" in dbg and tt == NT - 1:
                    op("sp", lambda e: e.dma_start(out=dbg["score"], in_=score), reads=SK, writes=["dbg_score"], dma=True)

            def attn_part(tt, sb):
                qs = slice(tt * 128, (tt + 1) * 128)
                maskb = maskbb[sb]
                for kb in range(tt + 1):
                    kcs = slice(kb * 128, (kb + 1) * 128)
                    for g2 in range(2):
                        bank = 2 + g2 + 2 * (kb % 2)
                        pt = PTt[g2][kb % 2]
                        op("pe", lambda e, bank=bank, kcs=kcs: e.matmul(PB(bank), lhsT=maskb[:, kcs], rhs=I4, start=True, stop=False),
                           reads=[("maskb", sb), "I4"], writes=[("pb", bank)])
                        for s_ in range(4):
                            h = 4 * g2 + s_
                            hp, par = h // 2, h % 2
                            kT = kz[g2][par]
                            op("pe", lambda e, bank=bank, s_=s_, kT=kT, kcs=kcs, hp=hp: e.matmul(
                                PB(bank)[:, s_ * 128:(s_ + 1) * 128], lhsT=kT[:, kcs], rhs=bqT[:, hp, qs], start=False, stop=(s_ == 3)),
                               reads=["bkT", "bqT"], writes=[("pb", bank)])
                        op("act", lambda e, bank=bank, pt=pt: e.activation(out=pt, in_=PB(bank), func=AF.Exp, scale=0.125),
                           reads=[("pb", bank)], writes=[("PT", g2, kb % 2)])
                        for s_ in range(4):
                            op("pe", lambda e, g2=g2, s_=s_, pt=pt, kb=kb: e.matmul(
                                PB(6 + g2)[:, s_ * 65:(s_ + 1) * 65], lhsT=pt[:, s_ * 128:(s_ + 1) * 128],
                                rhs=bv_tok[:, kb, g2 * 65:(g2 + 1) * 65], start=(kb == 0 and s_ == 0), stop=(kb == tt and s_ == 3)),
                               reads=[("PT", g2, kb % 2), "bv_tok"], writes=[("pb", 6 + g2)])
                op("act", lambda e: e.copy(out=ob[:, 0:260], in_=PB(6)[:, 0:260]), reads=[("pb", 6)], writes=["ob"])
                op("act", lambda e: e.copy(out=ob[:, 260:520], in_=PB(7)[:, 0:260]), reads=[("pb", 7)], writes=["ob"])
                obv = ob.rearrange("p (s e) -> p s e", e=65)
                op("dve", lambda e: e.reciprocal(out=rden, in_=obv[:, :, 64]), reads=["ob"], writes=["rden"])
                op("dve", lambda e: e.tensor_tensor(out=ob_b.rearrange("p (h d) -> p h d", h=8), in0=obv[:, :, 0:64],
                                                    in1=rden.unsqueeze(2).to_broadcast([128, 8, 64]), op=ALU.mult),
                   reads=["ob", "rden"], writes=["ob_b"])
                for q in range(4):
                    op("pe", lambda e, q=q: e.transpose(out=PBb(0)[:, q * 128:(q + 1) * 128], in_=ob_b[:, q * 128:(q + 1) * 128],
                                                        identity=ident_b), reads=["ob_b", "ident_b"], writes=[("pb", 0)])
                op("act", lambda e: e.copy(out=o_bT[:, :, qs], in_=PBb(0)[:, 0:512].rearrange("p (q t) -> p q t", q=4)),
                   reads=[("pb", 0)], writes=[("o_bT", tt)])

            scores_part(0, 0)
            for tt in range(NT):
                if tt + 1 < NT:
                    scores_part(tt + 1, (tt + 1) % 2)
                attn_part(tt, tt % 2)
            if "thr" in dbg:
                op("sp", None, reads=["dbg_thr"])
            if "score" in dbg:
                op("sp", None, reads=["dbg_score"])
            if "o_bT" in dbg:
                op("sp", lambda e: e.dma_start(out=dbg["o_bT"].rearrange("(a p) t -> p a t", p=128), in_=o_bT),
                   reads=[("o_bT", tt) for tt in range(NT)], writes=["dbg_o_bT"], dma=True)
                op("sp", None, reads=["dbg_o_bT"])

            sch.barrier()
            if stop_after == "dsa":
                return

            pa = MultiAlloc([(R_W, ARENA_BYTES)])
            hT2 = pa([128, 8, S], BF16)
            mergedT = pa([128, 8, S], BF16)
            x1 = pa([128, NT, D], F32)
            bgate = pa([128, 16], F32)
            g2col = pa([128, 8], F32)
            fng = pa([128, D], F32)
            wst2 = pa([128, 8, 256], F32)
            wg_bf = [pa([128, 8, 256], BF16) for _ in range(2)]
            wp_bf = [pa([128, 4, 256], BF16) for _ in range(2)]
            ga_s = pa([128, 512], BF16)
            gb_s = pa([128, 512], BF16)
            t1 = pa([128, 512], BF16)
            t2 = pa([128, 512], BF16)
            xt4 = [pa([128, D], F32) for _ in range(2)]
            op("sp", lambda e: e.dma_start(out=bgate, in_=bgate_d), writes=["bgate"], dma=True)
            op("sp", lambda e: e.dma_start(out=g2col, in_=g2_d), writes=["g2col"], dma=True)
            op("sp", lambda e: e.dma_start(out=fng, in_=fng_d.partition_broadcast(128)), writes=["fng"], dma=True)

            def phase1b():
                xa = Alloc(R_W + 64 * K, R_W + 128 * K)
                xt = [xa([128, D], F32) for _ in range(2)]
                hb = [xa([128, D], BF16) for _ in range(2)]
                junk = xa([128, D], BF16)
                ssx = xa([128, NT], F32)
                rsx = xa([128, NT], F32)
                op("dve", lambda e: e.memset(ssx, 0.0), writes=["ssx"])
                for tt in range(NT):
                    b = tt % 2
                    op("sp", lambda e, tt=tt, b=b: e.dma_start(out=xt[b], in_=x_d[tt * 128:(tt + 1) * 128, :]), writes=[("xt", b)], dma=True)
                    op("act", lambda e, tt=tt, b=b: e.activation(out=junk, in_=xt[b], func=AF.Square, accum_out=ssx[:, tt:tt + 1]),
                       reads=[("xt", b), "ssx"], writes=["junk", ("ssx", tt)])
                    op("act", lambda e, tt=tt: e.activation(out=rsx[:, tt:tt + 1], in_=ssx[:, tt:tt + 1], func=AF.Sqrt, bias=epsc,
                                                            scale=1.0 / D), reads=[("ssx", tt), "epsc"], writes=[("rsx", tt)])
                    op("dve", lambda e, tt=tt: e.reciprocal(out=rsx[:, tt:tt + 1], in_=rsx[:, tt:tt + 1]), reads=[("rsx", tt)],
                       writes=[("rsx", tt)])
                    op("dve", lambda e, tt=tt, b=b: e.tensor_scalar(out=hb[b], in0=xt[b], scalar1=rsx[:, tt:tt + 1], scalar2=None,
                                                                    op0=ALU.mult), reads=[("xt", b), ("rsx", tt)], writes=[("hb", b)])
                    bk = tt % 2
                    for k in range(8):
                        op("pe", lambda e, k=k, b=b, bk=bk: e.transpose(out=PBb(bk)[:, k * 128:(k + 1) * 128],
                                                                        in_=hb[b][:, k * 128:(k + 1) * 128], identity=ident_b),
                           reads=[("hb", b), "ident_b"], writes=[("pb", bk)])
                    op("act", lambda e, tt=tt, bk=bk: e.copy(out=hT2[:, :, tt * 128:(tt + 1) * 128],
                                                             in_=PBb(bk).rearrange("p (k t) -> p k t", k=8)),
                       reads=[("pb", bk)], writes=[("hT2", tt)])
            phase1b()
            sch.barrier()
            HT2 = [("hT2", tt) for tt in range(NT)]

            wpa_v = wpa_d.rearrange("(k p) c -> p k c", p=128)
            wpb_v = wpb_d.rearrange("(k p) c -> p k c", p=128)
            wout_v = wout_d.rearrange("(k p) c -> p k c", p=128)
            wi4 = [0]

            wst2b = xt4[0].rearrange("p (k c) -> p k c", k=4)
            wst2c = xt4[1].rearrange("p (k c) -> p k c", k=4)
            wi5 = [0]

            def load_gate(c0):
                i = wi4[0]
                wi4[0] += 1
                b = i % 2
                op("sp", lambda e: e.dma_start(out=wst2, in_=w_in_v[:, :, c0:c0 + 256]), writes=["wst2"], dma=True)
                op("pool", lambda e, b=b: e.tensor_tensor(out=wg_bf[b], in0=wst2, in1=g1col.unsqueeze(2).to_broadcast([128, 8, 256]),
                                                          op=ALU.mult), reads=["wst2", "g1col"], writes=[("wg", b)])
                return wg_bf[b], ("wg", b)

            def load_proj(src_v, c0):
                i = wi5[0]
                wi5[0] += 1
                b = i % 2
                st = wst2b if b == 0 else wst2c
                op("sp", lambda e: e.dma_start(out=st, in_=src_v[:, :, c0:c0 + 256]), writes=[("wstp", b)], dma=True)
                op("pool", lambda e, b=b: e.tensor_copy(out=wp_bf[b], in_=st), reads=[("wstp", b)], writes=[("wp", b)])
                return wp_bf[b], ("wp", b)

            p4u = [0]
            for j in range(4):
                wga, kga = load_gate(C_GA + j * 256)
                wgb, kgb = load_gate(C_GB + j * 256)
                wpa, kpa = load_proj(wpa_v, j * 256)
                wpb, kpb = load_proj(wpb_v, j * 256)
                for ct in range(2):
                    c = 2 * j + ct
                    ccs = slice(ct * 128, (ct + 1) * 128)
                    for tb in range(4):
                        tcs = slice(tb * 512, (tb + 1) * 512)
                        hk = HT2[tb * 4:tb * 4 + 4]
                        bA, bB, bC, bD = (2, 3, 4, 5) if (p4u[0] % 2 == 0) else (0, 1, 6, 7)
                        p4u[0] += 1
                        for k in range(8):
                            op("pe", lambda e, k=k, ccs=ccs, tcs=tcs, wga=wga, bA=bA: e.matmul(PB(bA), lhsT=wga[:, k, ccs], rhs=hT2[:, k, tcs],
                                                                                         start=(k == 0), stop=(k == 7)),
                               reads=[kga] + hk, writes=[("pb", bA)])
                        op("act", lambda e, c=c, bA=bA: e.activation(out=ga_s, in_=PB(bA), func=AF.Sigmoid, bias=bgate[:, c:c + 1], scale=1.0),
                           reads=[("pb", bA), "bgate"], writes=["ga_s"])
                        for k in range(8):
                            op("pe", lambda e, k=k, ccs=ccs, tcs=tcs, wgb=wgb, bB=bB: e.matmul(PB(bB), lhsT=wgb[:, k, ccs], rhs=hT2[:, k, tcs],
                                                                                         start=(k == 0), stop=(k == 7)),
                               reads=[kgb] + hk, writes=[("pb", bB)])
                        op("act", lambda e, c=c, bB=bB: e.activation(out=gb_s, in_=PB(bB), func=AF.Sigmoid, bias=bgate[:, 8 + c:9 + c], scale=1.0),
                           reads=[("pb", bB), "bgate"], writes=["gb_s"])
                        for hp in range(4):
                            op("pe", lambda e, hp=hp, ccs=ccs, tcs=tcs, wpa=wpa, bC=bC: e.matmul(PB(bC), lhsT=wpa[:, hp, ccs], rhs=o_aT[:, hp, tcs],
                                                                                           start=(hp == 0), stop=(hp == 3)),
                               reads=[kpa, "o_aT"], writes=[("pb", bC)])
                        for hp in range(4):
                            op("pe", lambda e, hp=hp, ccs=ccs, tcs=tcs, wpb=wpb, bD=bD: e.matmul(PB(bD), lhsT=wpb[:, hp, ccs], rhs=o_bT[:, hp, tcs],
                                                                                           start=(hp == 0), stop=(hp == 3)),
                               reads=[kpb, "o_bT"], writes=[("pb", bD)])
                        op("dve", lambda e, bC=bC: e.tensor_tensor(out=t1, in0=PB(bC), in1=ga_s, op=ALU.mult), reads=[("pb", bC), "ga_s"],
                           writes=["t1"])
                        op("dve", lambda e, bD=bD: e.tensor_tensor(out=t2, in0=PB(bD), in1=gb_s, op=ALU.mult), reads=[("pb", bD), "gb_s"],
                           writes=["t2"])
                        op("pool", lambda e, c=c, tcs=tcs: e.tensor_tensor(out=mergedT[:, c, tcs], in0=t1, in1=t2, op=ALU.add),
                           reads=["t1", "t2"], writes=[("mergedT", c, tb)])
            if "mergedT" in dbg:
                op("sp", lambda e: e.dma_start(out=dbg["mergedT"].rearrange("(a p) t -> p a t", p=128), in_=mergedT),
                   reads=[("mergedT", c, tb) for c in range(8) for tb in range(4)], writes=["dbg_mergedT"], dma=True)
                op("sp", None, reads=["dbg_mergedT"])
            sch.barrier()
            wout_bf = view(R_A, [128, 8, D], BF16)
            for j in range(4):
                op("sp", lambda e, j=j: e.dma_start(out=wst2, in_=wout_v[:, :, j * 256:(j + 1) * 256]), writes=["wst2"], dma=True)
                op("pool", lambda e, j=j: e.tensor_copy(out=wout_bf[:, :, j * 256:(j + 1) * 256], in_=wst2), reads=["wst2"],
                   writes=[("wout", j)])
            WOUT = [("wout", j) for j in range(4)]
            for tt in range(NT):
                b = tt % 2
                op("sp", lambda e, tt=tt, b=b: e.dma_start(out=xt4[b], in_=x_d[tt * 128:(tt + 1) * 128, :]), writes=[("xt4", b)], dma=True)
                for nb in range(2):
                    bk = 2 + 2 * b + nb
                    for c in range(8):
                        op("pe", lambda e, c=c, tt=tt, nb=nb, bk=bk: e.matmul(PB(bk), lhsT=mergedT[:, c, tt * 128:(tt + 1) * 128],
                                                                               rhs=wout_bf[:, c, nb * 512:(nb + 1) * 512],
                                                                               start=(c == 0), stop=(c == 7)),
                           reads=WOUT + ["mergedT_all"], writes=[("pb", bk)])
                    op("dve", lambda e, tt=tt, nb=nb, bk=bk, b=b: e.tensor_tensor(out=x1[:, tt, nb * 512:(nb + 1) * 512], in0=PB(bk),
                                                                                   in1=xt4[b][:, nb * 512:(nb + 1) * 512], op=ALU.add),
                       reads=[("pb", bk), ("xt4", b)], writes=[("x1", tt)])
            if "x1" in dbg:
                op("sp", lambda e: e.dma_start(out=dbg["x1"].rearrange("(t p) c -> p t c", p=128), in_=x1),
                   reads=[("x1", tt) for tt in range(NT)], writes=["dbg_x1"], dma=True)
                op("sp", None, reads=["dbg_x1"])
            sch.barrier()
            if stop_after == "p4":
                return

            h2T = view(R_W, [128, 8, S], BF16)
            ma = MultiAlloc([(R_W + 32 * K, R_W + 64 * K), (R_A, R_A + 32 * K)])
            tail_off = None
            hb2 = [ma([128, D], BF16) for _ in range(2)]
            junk2 = ma([128, D], BF16)
            ss5 = ma([128, NT], F32)
            rs5 = ma([128, NT], F32)
            wr_st = ma([128, 8, 20], F32)
            wr_bf = ma([128, 8, 20], BF16)
            brow = ma([128, 20], F32)
            lg = ma([128, 20], F32)
            sm = {n_: ma([128, 4], F32) for n_ in ("goh", "gex", "elg", "oh1", "msk", "oh2", "wsel")}
            sc1 = {n_: ma([128, 1], F32) for n_ in ("gmax", "ngmax", "gsum", "ggate", "m1", "m2", "d21", "e21", "den", "w1", "w2")}
            tmp44 = ma([128, 4, 4], F32)
            comb_b = ma([128, 16], BF16)
            combT = ma([128, S], BF16)
            sel16 = ma([128, 16, 128], BF16)
            est = ma([128, 8, 256], F32)
            w1b = [ma([128, 8, 256], BF16) for _ in range(2)]
            w3b = [ma([128, 8, 256], BF16) for _ in range(2)]
            w2b = [ma([128, 2, D], BF16) for _ in range(2)]
            sg = [[ma([128, 512], BF16) for _ in range(2)] for _ in range(2)]
            cbt = [ma([128, 512], BF16) for _ in range(2)]
            tu = [[ma([128, 512], BF16) for _ in range(2)] for _ in range(2)]
            actT = [[ma([128, 512], BF16) for _ in range(2)] for _ in range(2)]
            op("sp", lambda e: e.dma_start(out=wr_st, in_=wr_d.rearrange("(k p) c -> p k c", p=128)), writes=["wr_st"], dma=True)
            op("sp", lambda e: e.dma_start(out=brow, in_=br_d.partition_broadcast(128)), writes=["brow"], dma=True)
            op("pool", lambda e: e.tensor_tensor(out=wr_bf, in0=wr_st, in1=g2col.unsqueeze(2).to_broadcast([128, 8, 20]), op=ALU.mult),
               reads=["wr_st", "g2col"], writes=["wr_bf"])
            op("pool", lambda e: e.memset(sel16[0:16, :, :], 1.0), writes=["sel16"])
            op("pool", lambda e: e.affine_select(out=sel16[0:16, :, :], in_=sel16[0:16, :, :], pattern=[[-1, 16], [0, 128]],
                                                 compare_op=ALU.is_equal, fill=0.0, base=0, channel_multiplier=1), writes=["sel16"])
            op("dve", lambda e: e.memset(ss5, 0.0), writes=["ss5"])
            def prep_tile(tt):
                b = tt % 2
                bk = tt % 2
                op("act", lambda e, tt=tt: e.activation(out=junk2, in_=x1[:, tt, :], func=AF.Square, accum_out=ss5[:, tt:tt + 1]),
                   reads=["x1_all", "ss5"], writes=["junk2", ("ss5", tt)])
                op("act", lambda e, tt=tt: e.activation(out=rs5[:, tt:tt + 1], in_=ss5[:, tt:tt + 1], func=AF.Sqrt, bias=epsc,
                                                        scale=1.0 / D), reads=[("ss5", tt), "epsc"], writes=[("rs5", tt)])
                op("dve", lambda e, tt=tt: e.reciprocal(out=rs5[:, tt:tt + 1], in_=rs5[:, tt:tt + 1]), reads=[("rs5", tt)],
                   writes=[("rs5", tt)])
                op("dve", lambda e, tt=tt, b=b: e.tensor_scalar(out=hb2[b], in0=x1[:, tt, :], scalar1=rs5[:, tt:tt + 1], scalar2=None,
                                                                op0=ALU.mult), reads=["x1_all", ("rs5", tt)], writes=[("hb2", b)])
                for k in range(8):
                    op("pe", lambda e, k=k, b=b, bk=bk: e.transpose(out=PBb(bk)[:, k * 128:(k + 1) * 128],
                                                                    in_=hb2[b][:, k * 128:(k + 1) * 128], identity=ident_b),
                       reads=[("hb2", b), "ident_b"], writes=[("pb", bk)])
                op("act", lambda e, tt=tt, bk=bk: e.copy(out=h2T[:, :, tt * 128:(tt + 1) * 128],
                                                         in_=PBb(bk).rearrange("p (k t) -> p k t", k=8)),
                   reads=[("pb", bk)], writes=[("h2T", tt)])
                for k in range(8):
                    op("pe", lambda e, k=k, tt=tt: e.matmul(PB(2)[:, 0:20], lhsT=h2T[:, k, tt * 128:(tt + 1) * 128], rhs=wr_bf[:, k, :],
                                                            start=(k == 0), stop=(k == 7)),
                       reads=[("h2T", tt), "wr_bf"], writes=[("pb", 2)])
                R = []

                def rop(fn, rd, wr):
                    op("dve", fn, reads=rd, writes=wr)
                rop(lambda e: e.tensor_tensor(out=lg, in0=PB(2)[:, 0:20], in1=brow, op=ALU.add), [("pb", 2), "brow"], ["lg"])
                elv = lg[:, 4:20].rearrange("p (g x) -> p g x", g=4)
                rop(lambda e: e.tensor_reduce(out=sc1["gmax"], in_=lg[:, 0:4], axis=AX.X, op=ALU.max), ["lg"], ["gmax"])
                rop(lambda e: e.tensor_scalar(out=sm["goh"], in0=lg[:, 0:4], scalar1=sc1["gmax"][:, 0:1], scalar2=None,
                                              op0=ALU.is_equal), ["lg", "gmax"], ["goh"])
                rop(lambda e: e.tensor_scalar(out=sc1["ngmax"], in0=sc1["gmax"], scalar1=-1.0, scalar2=None, op0=ALU.mult),
                    ["gmax"], ["ngmax"])
                op("act", lambda e: e.activation(out=sm["gex"], in_=lg[:, 0:4], func=AF.Exp, bias=sc1["ngmax"][:, 0:1], scale=1.0),
                   reads=["lg", "ngmax"], writes=["gex"])
                rop(lambda e: e.tensor_reduce(out=sc1["gsum"], in_=sm["gex"], axis=AX.X, op=ALU.add), ["gex"], ["gsum"])
                rop(lambda e: e.reciprocal(out=sc1["ggate"], in_=sc1["gsum"]), ["gsum"], ["ggate"])
                rop(lambda e: e.tensor_tensor(out=tmp44, in0=elv, in1=sm["goh"].unsqueeze(2).to_broadcast([128, 4, 4]), op=ALU.mult),
                    ["lg", "goh"], ["tmp44"])
                rop(lambda e: e.tensor_reduce(out=sm["elg"], in_=tmp44.rearrange("p g x -> p x g"), axis=AX.X, op=ALU.add),
                    ["tmp44"], ["elg"])
                rop(lambda e: e.tensor_reduce(out=sc1["m1"], in_=sm["elg"], axis=AX.X, op=ALU.max), ["elg"], ["m1"])
                rop(lambda e: e.tensor_scalar(out=sm["oh1"], in0=sm["elg"], scalar1=sc1["m1"][:, 0:1], scalar2=None, op0=ALU.is_equal),
                    ["elg", "m1"], ["oh1"])
                rop(lambda e: e.scalar_tensor_tensor(out=sm["msk"], in0=sm["oh1"], scalar=-1.0e30, in1=sm["elg"], op0=ALU.mult,
                                                     op1=ALU.add), ["oh1", "elg"], ["msk"])
                rop(lambda e: e.tensor_reduce(out=sc1["m2"], in_=sm["msk"], axis=AX.X, op=ALU.max), ["msk"], ["m2"])
                rop(lambda e: e.tensor_scalar(out=sm["oh2"], in0=sm["msk"], scalar1=sc1["m2"][:, 0:1], scalar2=None, op0=ALU.is_equal),
                    ["msk", "m2"], ["oh2"])
                rop(lambda e: e.tensor_tensor(out=sc1["d21"], in0=sc1["m2"], in1=sc1["m1"], op=ALU.subtract), ["m1", "m2"], ["d21"])
                op("act", lambda e: e.activation(out=sc1["e21"], in_=sc1["d21"], func=AF.Exp), reads=["d21"], writes=["e21"])
                rop(lambda e: e.tensor_scalar(out=sc1["den"], in0=sc1["e21"], scalar1=1.0, scalar2=None, op0=ALU.add), ["e21"], ["den"])
                rop(lambda e: e.reciprocal(out=sc1["den"], in_=sc1["den"]), ["den"], ["den"])
                rop(lambda e: e.tensor_tensor(out=sc1["w1"], in0=sc1["ggate"], in1=sc1["den"], op=ALU.mult), ["ggate", "den"], ["w1"])
                rop(lambda e: e.tensor_tensor(out=sc1["w2"], in0=sc1["w1"], in1=sc1["e21"], op=ALU.mult), ["w1", "e21"], ["w2"])
                rop(lambda e: e.tensor_scalar(out=sm["wsel"], in0=sm["oh1"], scalar1=sc1["w1"][:, 0:1], scalar2=None, op0=ALU.mult),
                    ["oh1", "w1"], ["wsel"])
                rop(lambda e: e.scalar_tensor_tensor(out=sm["wsel"], in0=sm["oh2"], scalar=sc1["w2"][:, 0:1], in1=sm["wsel"],
                                                     op0=ALU.mult, op1=ALU.add), ["oh2", "w2", "wsel"], ["wsel"])
                rop(lambda e: e.tensor_tensor(out=comb_b.rearrange("p (g x) -> p g x", g=4),
                                              in0=sm["goh"].unsqueeze(2).to_broadcast([128, 4, 4]),
                                              in1=sm["wsel"].unsqueeze(1).to_broadcast([128, 4, 4]), op=ALU.mult),
                    ["goh", "wsel"], ["comb_b"])
                if "comb" in dbg:
                    op("sp", lambda e, tt=tt: e.dma_start(out=dbg["comb"][tt * 128:(tt + 1) * 128, :], in_=comb_b), reads=["comb_b"],
                       writes=["dbg_comb"], dma=True)
                op("pe", lambda e: e.transpose(out=PBb(3)[0:16, 0:128], in_=comb_b, identity=ident_b), reads=["comb_b", "ident_b"],
                   writes=[("pb", 3)])
                op("act", lambda e, tt=tt: e.copy(out=combT[0:16, tt * 128:(tt + 1) * 128], in_=PBb(3)[0:16, 0:128]),
                   reads=[("pb", 3)], writes=[("combT", tt)])
            for tt in range(4):
                prep_tile(tt)
            H2T = [("h2T", tt) for tt in range(NT)]
            CT = [("combT", tt) for tt in range(NT)]

            def load_expert(e_i):
                b = e_i % 2
                for (src, dst, nm, fold) in ((w1_d, w1b[b], "w1", True), (w3_d, w3b[b], "w3", True)):
                    op("sp", lambda e, src=src: e.dma_start(out=est, in_=src[e_i].rearrange("(k p) f -> p k f", p=128)),
                       writes=["est"], dma=True)
                    op("pool", lambda e, dst=dst: e.tensor_tensor(out=dst, in0=est, in1=g2col.unsqueeze(2).to_broadcast([128, 8, 256]),
                                                                  op=ALU.mult), reads=["est", "g2col"], writes=[(nm, b)])
                op("sp", lambda e: e.dma_start(out=est.rearrange("p k f -> p (k f)").rearrange("p (a c) -> p a c", a=2),
                                               in_=w2_d[e_i].rearrange("(a p) c -> p a c", p=128)), writes=["est"], dma=True)
                op("pool", lambda e: e.tensor_copy(out=w2b[b], in_=est.rearrange("p k f -> p (k f)").rearrange("p (a c) -> p a c", a=2)),
                   reads=["est"], writes=[("w2", b)])

            def stageA(e_i, tb, sl):
                b = e_i % 2
                tcs = slice(tb * 512, (tb + 1) * 512)
                hk = H2T[tb * 4:tb * 4 + 4]
                cbk_ = 6
                op("pe", lambda e: e.matmul(PB(cbk_), lhsT=sel16[0:16, e_i, :], rhs=combT[0:16, tcs], start=True, stop=True),
                   reads=["sel16"] + CT[tb * 4:tb * 4 + 4], writes=[("pb", cbk_)])
                op("act", lambda e: e.copy(out=cbt[sl], in_=PB(cbk_)), reads=[("pb", cbk_)], writes=[("cbt", sl)])
                for ft in range(2):
                    fcs = slice(ft * 128, (ft + 1) * 128)
                    for k in range(8):
                        op("pe", lambda e, k=k, fcs=fcs, ft=ft: e.matmul(PB(2 + ft), lhsT=w1b[b][:, k, fcs], rhs=h2T[:, k, tcs],
                                                                         start=(k == 0), stop=(k == 7)),
                           reads=[("w1", b)] + hk, writes=[("pb", 2 + ft)])
                    op("act", lambda e, ft=ft: e.activation(out=sg[sl][ft], in_=PB(2 + ft), func=AF.Silu), reads=[("pb", 2 + ft)],
                       writes=[("sg", sl, ft)])
                    yield
                    for k in range(8):
                        op("pe", lambda e, k=k, fcs=fcs, ft=ft: e.matmul(PB(4 + ft), lhsT=w3b[b][:, k, fcs], rhs=h2T[:, k, tcs],
                                                                         start=(k == 0), stop=(k == 7)),
                           reads=[("w3", b)] + hk, writes=[("pb", 4 + ft)])
                    op("dve", lambda e, ft=ft: e.tensor_tensor(out=tu[sl][ft], in0=PB(4 + ft), in1=sg[sl][ft], op=ALU.mult),
                       reads=[("pb", 4 + ft), ("sg", sl, ft)], writes=[("tu", sl, ft)], fast=True)
                    op("pool", lambda e, ft=ft: e.tensor_tensor(out=actT[sl][ft], in0=tu[sl][ft], in1=cbt[sl], op=ALU.mult),
                       reads=[("tu", sl, ft), ("cbt", sl)], writes=[("actT", sl, ft)])
                    yield

            ybank = [0]

            def stageB(e_i, tb, sl):
                b = e_i % 2
                for t4 in range(4):
                    tt = tb * 4 + t4
                    for nb in range(2):
                        bk = (0, 1, 7)[ybank[0] % 3]
                        ybank[0] += 1
                        for ft in range(2):
                            op("pe", lambda e, ft=ft, t4=t4, nb=nb, bk=bk: e.matmul(
                                PB(bk), lhsT=actT[sl][ft][:, t4 * 128:(t4 + 1) * 128], rhs=w2b[b][:, ft, nb * 512:(nb + 1) * 512],
                                start=(ft == 0), stop=(ft == 1)), reads=[("actT", sl, ft), ("w2", b)], writes=[("pb", bk)])
                        op("dve", lambda e, tt=tt, nb=nb, bk=bk: e.tensor_tensor(out=x1[:, tt, nb * 512:(nb + 1) * 512],
                                                                                 in0=PB(bk), in1=x1[:, tt, nb * 512:(nb + 1) * 512],
                                                                                 op=ALU.add),
                           reads=[("pb", bk), ("x2", tt, nb)], writes=[("x2", tt, nb)], fast=True)
                        if nb == 1:
                            yield

            def drain(g_):
                for _ in g_:
                    pass

            units = [(e_i, tb) for e_i in range(16) for tb in range(4)]
            load_expert(0)
            load_expert(1)
            drain(stageA(units[0][0], units[0][1], 0))
            for u, (e_i, tb) in enumerate(units):
                gb = stageB(e_i, tb, u % 2)
                if u + 1 < len(units):
                    ne, ntb = units[u + 1]
                    if ne == 0:
                        for tt in range(4 * ntb, 4 * ntb + 4):
                            prep_tile(tt)
                    ga_ = stageA(ne, ntb, (u + 1) % 2)
                    drain(ga_)
                drain(gb)
                if tb == 3 and e_i + 2 < 16:
                    load_expert(e_i + 2)
            sch.barrier()
            if "x2" in dbg:
                op("sp", lambda e: e.dma_start(out=dbg["x2"].rearrange("(t p) c -> p t c", p=128), in_=x1), writes=["dbg_x2"], dma=True)
                op("sp", None, reads=["dbg_x2"])

            fa = MultiAlloc([(R_W, R_W + 64 * K)])
            ss6 = fa([128, NT], F32)
            rs6 = fa([128, NT], F32)
            junk6 = fa([128, D], BF16)
            yo = [fa([128, D], F32) for _ in range(2)]
            op("dve", lambda e: e.memset(ss6, 0.0), writes=["ss6"])
            for tt in range(NT):
                b = tt % 2
                op("act", lambda e, tt=tt: e.activation(out=junk6, in_=x1[:, tt, :], func=AF.Square, accum_out=ss6[:, tt:tt + 1]),
                   reads=["ss6"], writes=["junk6", ("ss6", tt)])
                op("act", lambda e, tt=tt: e.activation(out=rs6[:, tt:tt + 1], in_=ss6[:, tt:tt + 1], func=AF.Sqrt, bias=epsc,
                                                        scale=1.0 / D), reads=[("ss6", tt), "epsc"], writes=[("rs6", tt)])
                op("dve", lambda e, tt=tt: e.reciprocal(out=rs6[:, tt:tt + 1], in_=rs6[:, tt:tt + 1]), reads=[("rs6", tt)],
                   writes=[("rs6", tt)])
                op("dve", lambda e, tt=tt, b=b: e.scalar_tensor_tensor(out=yo[b], in0=x1[:, tt, :], scalar=rs6[:, tt:tt + 1], in1=fng,
                                                                       op0=ALU.mult, op1=ALU.mult),
                   reads=[("rs6", tt), "fng"], writes=[("yo", b)])
                op("sp", lambda e, tt=tt, b=b: e.dma_start(out=out_d[tt * 128:(tt + 1) * 128, :], in_=yo[b]), reads=[("yo", b)],
                   writes=[("out", tt)], dma=True)
            op("sp", None, reads=[("out", tt) for tt in range(NT)])


        body()
        sch.barrier()
        DEBUG["stats_pre"] = {e: len(sch.ops[e]) for e in Sched.ENGS}
        with nc.Block() as block:
            sch.emit(nc, block, engsem, dmasem)
        DEBUG["stats"] = sch.stats
    return nc


_NC_CACHE = {}


def kernel(**inputs):
    dbg = tuple(DEBUG.get("outputs", ()))
    key = (dbg, DEBUG.get("stop_after"))
    if key not in _NC_CACHE:
        _NC_CACHE[key] = build_nc(dbg, DEBUG.get("stop_after"))
    nc = _NC_CACHE[key]
    n = 8
    x = np.ascontiguousarray(inputs["x"], dtype=np.float32)
    posn = np.ascontiguousarray(inputs["positions"], dtype=np.int32)
    f32 = lambda a: np.ascontiguousarray(a, dtype=np.float32)
    inv = (10000.0 ** (-np.arange(32, dtype=np.float32) / np.float32(32))).astype(np.float32).reshape(1, 32)
    shared = {
        "norm1_g": f32(inputs["norm1_g"][0].reshape(8, 128).T),
        "w_in": f32(inputs["w_in"][0]),
        "conv_w": f32(inputs["conv_w"][0].reshape(4, 12, 128).transpose(2, 1, 0).reshape(128, 48)),
        "inv_freq": inv,
        "a_log": f32(inputs["a_log"][0].reshape(1, 8)),
        "dt_bias": f32(inputs["dt_bias"][0].reshape(1, 8)),
        "a_norm_g": f32(inputs["a_norm_g"][0].reshape(1, 64)),
        "b_gate": f32(inputs["b_gate"][0].reshape(16, 128).T),
        "norm2_g": f32(inputs["norm2_g"][0].reshape(8, 128).T),
        "final_norm_g": f32(inputs["final_norm_g"].reshape(1, D)),
        "w_proj_a": f32(inputs["w_proj_a"][0]),
        "w_proj_b": f32(inputs["w_proj_b"][0]),
        "w_out": f32(inputs["w_out"][0]),
        "w_router": f32(np.concatenate([inputs["w_router_group"][0], inputs["w_router_expert"][0]], axis=1)),
        "b_router": f32(np.concatenate([inputs["b_router_group"][0], inputs["b_router_expert"][0]], axis=0).reshape(1, 20)),
        "w_exp_gate": f32(inputs["w_exp_gate"][0]),
        "w_exp_up": f32(inputs["w_exp_up"][0]),
        "w_exp_down": f32(inputs["w_exp_down"][0]),
    }
    in_maps = []
    for c in range(n):
        m = dict(shared)
        m["x"] = x[c]
        m["positions"] = np.ascontiguousarray(posn[c].reshape(NT, 128).T)
        in_maps.append(m)
    res = run_bass_kernel_spmd(nc, in_maps, core_ids=list(range(n)))
    DEBUG["results"] = res.results
    return np.stack([r["out"] for r in res.results], axis=0)
```

```python
import math
from contextlib import ExitStack
import numpy as np
import concourse.bass as bass
import concourse.mybir as mybir
from concourse.bass_utils import run_bass_kernel_spmd

F32 = mybir.dt.float32
BF16 = mybir.dt.bfloat16
I32 = mybir.dt.int32
AF = mybir.ActivationFunctionType
ALU = mybir.AluOpType
AX = mybir.AxisListType

S = 2048
D = 1024
NT = S // 128
D_IN = 5464
EPS = 1e-6
N_DMA_SEMS = 24
NEG = -30000.0
NBIS = 12
TWO_PI = 2.0 * math.pi

C_AQ, C_AK, C_AV, C_AZ = 0, 512, 1024, 1536
C_BETA, C_ALPHA = 2048, 2056
C_BQ, C_BK, C_BV = 2064, 2576, 2704
C_IQ, C_IK, C_IW = 2832, 3344, 3408
C_GA, C_GB = 3416, 4440

DEBUG = {}
STRICT_SAME_ENGINE = True


class Sched:
    ENGS = ("pe", "act", "dve", "pool", "sp")

    def __init__(self):
        self.ops = {e: [] for e in self.ENGS}
        self.last_w = {}
        self.readers = {}
        self.dma_rr = 0
        self.dma_count = [0] * N_DMA_SEMS

    def op(self, eng, fn, reads=(), writes=(), dma=False, fast=False):
        deps = set()
        raw = set()
        for k in reads:
            t = self.last_w.get(k)
            if t is not None:
                deps.add(t)
                raw.add(t)
        for k in writes:
            t = self.last_w.get(k)
            if t is not None:
                deps.add(t)
            for t in self.readers.get(k, {}).values():
                deps.add(t)
        idx = len(self.ops[eng])
        if dma:
            si = self.dma_rr
            self.dma_rr = (self.dma_rr + 1) % N_DMA_SEMS
            prev = self.dma_count[si]
            if prev > 0:
                deps.add(("dma", si, prev))
            self.dma_count[si] = prev + 1
            tok = ("dma", si, prev + 1)
            rkey = ("dma", si)
        else:
            tok = ("eng", eng, idx)
            rkey = eng
            if STRICT_SAME_ENGINE:
                deps = {t for t in deps if not (t[0] == "eng" and t[1] == eng) or eng != "pe"}
            else:
                deps = {t for t in deps if not (t[0] == "eng" and t[1] == eng)
                        or (t in raw and eng != "pe" and not fast and idx - t[2] <= 8)}
        self.ops[eng].append(dict(fn=fn, deps=deps, signal=False, dma=(tok if dma else None)))
        for k in writes:
            self.last_w[k] = tok
            self.readers[k] = {}
        for k in reads:
            if k in writes:
                continue
            self.readers.setdefault(k, {})[rkey] = tok
        return tok

    def barrier(self):
        toks = set()
        for e in self.ENGS:
            j = len(self.ops[e]) - 1
            while j >= 0 and (self.ops[e][j]["fn"] is None or self.ops[e][j]["dma"] is not None):
                j -= 1
            if j >= 0:
                toks.add(("eng", e, j))
        for si in range(N_DMA_SEMS):
            if self.dma_count[si] > 0:
                toks.add(("dma", si, self.dma_count[si]))
        for e in self.ENGS:
            deps = {t for t in toks if not (t[0] == "eng" and t[1] == e and (e == "pe" or not STRICT_SAME_ENGINE))}
            self.ops[e].append(dict(fn=None, deps=deps, signal=False, dma=None))
        self.last_w = {}
        self.readers = {}

    def emit(self, nc, block, engsem, dmasem):
        for e in self.ENGS:
            for o in self.ops[e]:
                for t in o["deps"]:
                    if t[0] == "eng":
                        self.ops[t[1]][t[2]]["signal"] = True
        sigcount = {}
        for e in self.ENGS:
            c = 0
            lst = []
            for o in self.ops[e]:
                if o["signal"]:
                    c += 1
                lst.append(c)
            sigcount[e] = lst
        self.stats = {e: (len(self.ops[e]), sigcount[e][-1] if sigcount[e] else 0) for e in self.ENGS}

        def run(e, eng):
            waited = {}
            for o in self.ops[e]:
                need = {}
                for t in o["deps"]:
                    if t[0] == "eng":
                        key = ("eng", t[1])
                        val = sigcount[t[1]][t[2]]
                    else:
                        key = ("dma", t[1])
                        val = 16 * t[2]
                    if val > need.get(key, 0):
                        need[key] = val
                for key, val in need.items():
                    if waited.get(key, 0) >= val:
                        continue
                    waited[key] = val
                    sem = engsem[key[1]] if key[0] == "eng" else dmasem[key[1]]
                    eng.wait_ge(sem, val)
                if o["fn"] is None:
                    continue
                inst = o["fn"](eng)
                if o["dma"] is not None:
                    inst.then_inc(dmasem[o["dma"][1]], 16)
                elif o["signal"]:
                    inst.then_inc(engsem[e], 1)

        @block.tensor
        def _(eng):
            run("pe", eng)

        @block.scalar
        def _(eng):
            run("act", eng)

        @block.vector
        def _(eng):
            run("dve", eng)

        @block.gpsimd
        def _(eng):
            run("pool", eng)

        @block.sync
        def _(eng):
            run("sp", eng)


DT_SIZE = {F32: 4, BF16: 2, I32: 4}


def build_nc(debug=(), stop_after=None):
    nc = bass.Bass("TRN2", target_bir_lowering=False)

    def din(name, shape, dt=F32):
        return nc.dram_tensor(name, list(shape), dt, kind="ExternalInput").ap()

    x_d = din("x", [S, D])
    pos_d = din("positions", [128, NT], I32)
    g1_d = din("norm1_g", [128, 8])
    w_in_d = din("w_in", [D, D_IN])
    convw_d = din("conv_w", [128, 48])
    invf_d = din("inv_freq", [1, 32])
    alog_d = din("a_log", [1, 8])
    dtb_d = din("dt_bias", [1, 8])
    ang_d = din("a_norm_g", [1, 64])
    bgate_d = din("b_gate", [128, 16])
    g2_d = din("norm2_g", [128, 8])
    fng_d = din("final_norm_g", [1, D])
    wpa_d = din("w_proj_a", [512, D])
    wpb_d = din("w_proj_b", [512, D])
    wout_d = din("w_out", [D, D])
    wr_d = din("w_router", [D, 20])
    br_d = din("b_router", [1, 20])
    w1_d = din("w_exp_gate", [16, D, 256])
    w3_d = din("w_exp_up", [16, D, 256])
    w2_d = din("w_exp_down", [16, 256, D])
    out_d = nc.dram_tensor("out", [S, D], F32, kind="ExternalOutput").ap()
    dbg = {}
    for name, shape, dt in debug:
        dbg[name] = nc.dram_tensor("dbg_" + name, list(shape), dt, kind="ExternalOutput").ap()
    w_in_v = w_in_d.rearrange("(k p) c -> p k c", p=128)

    sch = Sched()
    op = sch.op
    es = ExitStack()
    with es:
        ARENA_BYTES = 207 * 1024
        arena = es.enter_context(nc.sbuf_tensor("arena", [128, ARENA_BYTES // 4], F32))

        def view(off, shape, dt):
            n = 1
            for s_ in shape[1:]:
                n *= s_
            size = n * DT_SIZE[dt]
            assert off % 4 == 0 and size % 4 == 0 and off + size <= ARENA_BYTES, (off, size)
            ap = arena[:, off // 4:(off + size) // 4]
            if dt != F32:
                ap = ap.bitcast(dt)
            if len(shape) == 3:
                ap = ap.rearrange("p (a b) -> p a b", a=shape[1])
            elif len(shape) == 4:
                ap = ap.rearrange("p (a b c) -> p a b c", a=shape[1], b=shape[2])
            return ap

        class Alloc:
            def __init__(self, base, limit):
                self.off = base
                self.limit = limit

            def __call__(self, shape, dt):
                n = 1
                for s_ in shape[1:]:
                    n *= s_
                size = (n * DT_SIZE[dt] + 63) // 64 * 64
                self.off = (self.off + 63) // 64 * 64
                v = view(self.off, shape, dt)
                self.off += size
                assert self.off <= self.limit, (self.off, self.limit)
                return v

        pbank = [es.enter_context(nc.psum_tensor("pb%d" % i, [128, 512], F32)) for i in range(8)]
        engsem = {e: es.enter_context(nc.semaphore("sem_" + e)) for e in Sched.ENGS}
        dmasem = [es.enter_context(nc.semaphore("dsem%d" % i)) for i in range(N_DMA_SEMS)]

        def PB(i):
            return pbank[i][:]

        def PBb(i):
            return pbank[i][:].bitcast(BF16)

        def body():
            K = 1024
            ca = Alloc(0, 9 * K)
            ident_f = ca([128, 128], F32)
            ident_b = ca([128, 128], BF16)
            ucs_f = ca([128, 128], F32)
            mc0_f = ca([128, 128], F32)
            mc1_f = ca([128, 128], F32)
            maskneg_f = ca([128, 128], F32)
            strict_b = ca([128, 128], BF16)
            g1col = ca([128, 8], F32)
            epsc = ca([128, 1], F32)
            cw = ca([128, 48], F32)
            invf = ca([128, 32], F32)
            dtb = ca([128, 8], F32)
            negA = ca([128, 8], F32)
            angb = ca([128, 64], F32)
            posi = ca([128, NT], I32)
            posf = ca([128, NT], F32)
            cs = ca([128, NT, 64], F32)
            assert ca.off <= 9 * K, ca.off

            op("pool", lambda e: e.memset(ident_f, 1.0), writes=["ident_f"])
            op("pool", lambda e: e.affine_select(out=ident_f, in_=ident_f, pattern=[[-1, 128]], compare_op=ALU.is_equal,
                                                 fill=0.0, base=0, channel_multiplier=1), writes=["ident_f"])
            op("pool", lambda e: e.tensor_copy(out=ident_b, in_=ident_f), reads=["ident_f"], writes=["ident_b"])
            op("pool", lambda e: e.memset(ucs_f, 1.0), writes=["ucs_f"])
            op("pool", lambda e: e.affine_select(out=ucs_f, in_=ucs_f, pattern=[[1, 128]], compare_op=ALU.is_ge,
                                                 fill=0.0, base=0, channel_multiplier=-1), writes=["ucs_f"])
            op("pool", lambda e: e.memset(ucs_f[0:64, 64:128], 0.0), writes=["ucs_f"])
            op("pool", lambda e: e.memset(mc0_f, 0.0), writes=["mc0_f"])
            op("pool", lambda e: e.memset(mc0_f[0:64, :], 1.0), writes=["mc0_f"])
            op("pool", lambda e: e.memset(mc1_f, 0.0), writes=["mc1_f"])
            op("pool", lambda e: e.memset(mc1_f[64:128, :], 1.0), writes=["mc1_f"])
            op("pool", lambda e: e.memset(maskneg_f, 0.0), writes=["maskneg_f"])
            op("pool", lambda e: e.affine_select(out=maskneg_f, in_=maskneg_f, pattern=[[-1, 128]], compare_op=ALU.is_ge,
                                                 fill=NEG, base=0, channel_multiplier=1), writes=["maskneg_f"])
            op("pool", lambda e: e.memset(maskneg_f[64:128, 0:64], NEG), writes=["maskneg_f"])
            op("pool", lambda e: e.memset(strict_b, 1.0), writes=["strict_b"])
            op("pool", lambda e: e.affine_select(out=strict_b, in_=strict_b, pattern=[[-1, 128]], compare_op=ALU.is_gt,
                                                 fill=0.0, base=0, channel_multiplier=1), writes=["strict_b"])
            op("pool", lambda e: e.memset(strict_b[64:128, 0:64], 0.0), writes=["strict_b"])
            op("dve", lambda e: e.memset(epsc, EPS), writes=["epsc"])
            op("sp", lambda e: e.dma_start(out=g1col, in_=g1_d), writes=["g1col"], dma=True)
            op("sp", lambda e: e.dma_start(out=cw, in_=convw_d), writes=["cw"], dma=True)
            op("sp", lambda e: e.dma_start(out=invf, in_=invf_d.partition_broadcast(128)), writes=["invf"], dma=True)
            op("sp", lambda e: e.dma_start(out=dtb, in_=dtb_d.partition_broadcast(128)), writes=["dtb"], dma=True)
            op("sp", lambda e: e.dma_start(out=negA, in_=alog_d.partition_broadcast(128)), writes=["negA"], dma=True)
            op("sp", lambda e: e.dma_start(out=angb, in_=ang_d.partition_broadcast(128)), writes=["angb"], dma=True)
            op("sp", lambda e: e.dma_start(out=posi, in_=pos_d), writes=["posi"], dma=True)
            op("act", lambda e: e.activation(out=negA, in_=negA, func=AF.Exp), reads=["negA"], writes=["negA"])
            op("dve", lambda e: e.tensor_scalar(out=negA, in0=negA, scalar1=-1.0, scalar2=None, op0=ALU.mult),
               reads=["negA"], writes=["negA"])

            R_A = 9 * K
            R_W = R_A + 32 * K
            R_Z = R_W + 16 * K
            R_Q = R_Z + 50 * K
            R_S = R_Q + 48 * K
            hT = view(R_A, [128, 8, S], BF16)
            wstage = view(R_W, [128, 8, 256], F32)
            wbf = [view(R_W + 8 * K + i * 4 * K, [128, 8, 256], BF16) for i in range(2)]
            zqkvT = view(R_Z, [128, 12, S + 4], BF16)
            bqT = view(R_Z, [128, 4, S], BF16)
            iqT = view(R_Z + 16 * K, [128, 4, S], BF16)
            azs = view(R_Z + 32 * K, [128, NT, 512], BF16)
            qkv_tok = view(R_Q, [128, NT, 1536], BF16)
            sa = Alloc(R_S, ARENA_BYTES)
            kz = [[sa([128, S], BF16) for _ in range(2)] for _ in range(2)]
            ikT2 = sa([128, S], BF16)
            bv_tok = sa([128, NT, 130], BF16)
            ab_tok = sa([128, NT, 16], F32)
            iw_tok = sa([128, NT, 8], F32)
            diagw = sa([128, 48, 128], BF16)
            R_WORK = sa.off

            def rope_tables():
                wa = Alloc(R_Q, R_Q + 48 * K)
                ang = wa([128, NT, 32], F32)
                tmp = wa([128, NT, 32], F32)
                ki = wa([128, NT, 32], I32)
                op("dve", lambda e: e.tensor_copy(out=posf, in_=posi), reads=["posi"], writes=["posf"])
                op("dve", lambda e: e.tensor_tensor(out=ang, in0=posf.unsqueeze(2).to_broadcast([128, NT, 32]),
                                                    in1=invf.unsqueeze(1).to_broadcast([128, NT, 32]), op=ALU.mult),
                   reads=["posf", "invf"], writes=["ang"])
                for which, shift in ((1, 0.0), (0, math.pi / 2.0)):
                    dst = cs[:, :, which * 32:(which + 1) * 32]
                    op("dve", lambda e, shift=shift: e.tensor_scalar(out=tmp, in0=ang, scalar1=shift, scalar2=None, op0=ALU.add),
                       reads=["ang"], writes=["rt_tmp"])
                    op("dve", lambda e: e.tensor_scalar(out=ki, in0=tmp, scalar1=1.0 / TWO_PI, scalar2=None, op0=ALU.mult),
                       reads=["rt_tmp"], writes=["rt_ki"])
                    op("dve", lambda e, dst=dst: e.tensor_copy(out=dst, in_=ki), reads=["rt_ki"], writes=["cs"])
                    op("dve", lambda e, dst=dst: e.scalar_tensor_tensor(out=dst, in0=dst, scalar=-TWO_PI, in1=tmp,
                                                                       op0=ALU.mult, op1=ALU.add),
                       reads=["cs", "rt_tmp"], writes=["cs"])
                    op("dve", lambda e, dst=dst: e.tensor_scalar(out=dst, in0=dst, scalar1=math.pi, scalar2=-math.pi,
                                                                op0=ALU.min, op1=ALU.max), reads=["cs"], writes=["cs"])
                    op("act", lambda e, dst=dst: e.activation(out=dst, in_=dst, func=AF.Sin), reads=["cs"], writes=["cs"])

            rope_tables()

            def phase1(hT_dst, keyp):
                wa = Alloc(R_Q + 16 * K, R_Q + 48 * K)
                xt = [wa([128, D], F32) for _ in range(2)]
                hb = [wa([128, D], BF16) for _ in range(2)]
                junk = wa([128, D], BF16)
                ss1 = wa([128, NT], F32)
                rstd1 = wa([128, NT], F32)
                op("dve", lambda e: e.memset(ss1, 0.0), writes=[keyp + "ss1"])
                for tt in range(NT):
                    b = tt % 2
                    op("sp", lambda e, tt=tt, b=b: e.dma_start(out=xt[b], in_=x_d[tt * 128:(tt + 1) * 128, :]),
                       writes=[(keyp + "xt", b)], dma=True)
                    op("act", lambda e, tt=tt, b=b: e.activation(out=junk, in_=xt[b], func=AF.Square,
                                                                 accum_out=ss1[:, tt:tt + 1]),
                       reads=[(keyp + "xt", b), keyp + "ss1"], writes=[keyp + "junk", (keyp + "ss1", tt)])
                    op("act", lambda e, tt=tt: e.activation(out=rstd1[:, tt:tt + 1], in_=ss1[:, tt:tt + 1], func=AF.Sqrt,
                                                            bias=epsc, scale=1.0 / D),
                       reads=[(keyp + "ss1", tt), "epsc"], writes=[(keyp + "rstd1", tt)])
                    op("dve", lambda e, tt=tt: e.reciprocal(out=rstd1[:, tt:tt + 1], in_=rstd1[:, tt:tt + 1]),
                       reads=[(keyp + "rstd1", tt)], writes=[(keyp + "rstd1", tt)])
                    op("dve", lambda e, tt=tt, b=b: e.tensor_scalar(out=hb[b], in0=xt[b], scalar1=rstd1[:, tt:tt + 1],
                                                                    scalar2=None, op0=ALU.mult),
                       reads=[(keyp + "xt", b), (keyp + "rstd1", tt)], writes=[(keyp + "hb", b)])
                    pbv = PBb(tt % 2)
                    for k in range(8):
                        op("pe", lambda e, k=k, b=b, pbv=pbv: e.transpose(out=pbv[:, k * 128:(k + 1) * 128],
                                                                          in_=hb[b][:, k * 128:(k + 1) * 128], identity=ident_b),
                           reads=[(keyp + "hb", b), "ident_b"], writes=[("pb", tt % 2)])
                    op("act", lambda e, tt=tt, pbv=pbv: e.copy(out=hT_dst[:, :, tt * 128:(tt + 1) * 128],
                                                               in_=pbv.rearrange("p (k t) -> p k t", k=8)),
                       reads=[("pb", tt % 2)], writes=[("hT", tt)])

            phase1(hT, "p1")
            if stop_after == "p1":
                sch.barrier()
                return
            ALL_HT = [("hT", tt) for tt in range(NT)]

            wchunk_i = [0]

            def load_w(ranges):
                i = wchunk_i[0]
                wchunk_i[0] += 1
                b = i % 2
                off = 0
                for (c0, w) in ranges:
                    op("sp", lambda e, c0=c0, w=w, off=off: e.dma_start(out=wstage[:, :, off:off + w],
                                                                        in_=w_in_v[:, :, c0:c0 + w]),
                       writes=["wstage"], dma=True)
                    off += w
                tot = off
                op("pool", lambda e, b=b, tot=tot: e.tensor_tensor(out=wbf[b][:, :, 0:tot], in0=wstage[:, :, 0:tot],
                                                                   in1=g1col.unsqueeze(2).to_broadcast([128, 8, tot]),
                                                                   op=ALU.mult),
                   reads=["wstage", "g1col"], writes=[("wbf", b)])
                return wbf[b], ("wbf", b), tot

            for ci in range(48):
                op("pool", lambda e, ci=ci: e.tensor_scalar(out=diagw[:, ci, :], in0=ident_f, scalar1=cw[:, ci:ci + 1],
                                                            scalar2=None, op0=ALU.mult),
                   reads=["ident_f", "cw"], writes=[("diagw", ci)])
            op("pool", lambda e: e.memset(zqkvT[:, :, 0:4], 0.0), writes=["zpad"])

            cva = Alloc(R_WORK, ARENA_BYTES)
            convtmp = [cva([128, 512], BF16) for _ in range(2)]
            evq = [0]

            def evac_copy(out, in_, reads, writes):
                evq[0] += 1
                if evq[0] % 2 == 0:
                    op("act", lambda e: e.copy(out=out, in_=in_), reads=reads, writes=writes)
                else:
                    op("dve", lambda e: e.tensor_copy(out=out, in_=in_), reads=reads, writes=writes)

            pbi = [0]

            def g1_proj(c, wt, wkey, ct):
                for tb in range(4):
                    bk = 2 + (pbi[0] % 2)
                    pbi[0] += 1
                    for k in range(8):
                        op("pe", lambda e, k=k, tb=tb, bk=bk: e.matmul(
                            PB(bk), lhsT=wt[:, k, ct * 128:(ct + 1) * 128], rhs=hT[:, k, tb * 512:(tb + 1) * 512],
                            start=(k == 0), stop=(k == 7)),
                           reads=[wkey] + ALL_HT[tb * 4:tb * 4 + 4], writes=[("pb", bk)])
                    evac_copy(zqkvT[:, c, 4 + tb * 512:4 + (tb + 1) * 512], PB(bk), [("pb", bk)], [("zq", c, tb)])

            def g1_conv(c):
                for tb in range(4):
                    bk = 4 + (tb % 2)
                    for j in range(4):
                        op("pe", lambda e, tb=tb, j=j, bk=bk: e.matmul(
                            PB(bk), lhsT=diagw[:, c * 4 + j, :], rhs=zqkvT[:, c, tb * 512 + j + 1:tb * 512 + j + 1 + 512],
                            start=(j == 0), stop=(j == 3)),
                           reads=[("diagw", c * 4 + j), ("zq", c, tb), "zpad"] + ([("zq", c, tb - 1)] if tb > 0 else []),
                           writes=[("pb", bk)])
                    ctb = tb % 2
                    op("act", lambda e, bk=bk, ctb=ctb: e.activation(out=convtmp[ctb], in_=PB(bk), func=AF.Silu),
                       reads=[("pb", bk)], writes=[("convtmp", ctb)])
                    tbk = 6 + (tb % 2)
                    for q in range(4):
                        op("pe", lambda e, q=q, ctb=ctb, tbk=tbk: e.transpose(out=PBb(tbk)[:, q * 128:(q + 1) * 128],
                                                                              in_=convtmp[ctb][:, q * 128:(q + 1) * 128],
                                                                              identity=ident_b),
                           reads=[("convtmp", ctb), "ident_b"], writes=[("pb", tbk)])
                    op("dve", lambda e, tb=tb, tbk=tbk: e.tensor_copy(
                        out=qkv_tok[:, tb * 4:(tb + 1) * 4, c * 128:(c + 1) * 128],
                        in_=PBb(tbk)[:, 0:512].rearrange("p (q t) -> p q t", q=4)),
                       reads=[("pb", tbk)], writes=[("qkv_tok", tb * 4 + q, c) for q in range(4)])

            prev_c = None
            nxt_w = load_w([(0, 256)])
            for chunk in range(6):
                wt, wkey, _ = nxt_w
                for ct in range(2):
                    c = chunk * 2 + ct
                    g1_proj(c, wt, wkey, ct)
                    if ct == 0:
                        nxt_w = load_w([((chunk + 1) * 256, 256)]) if chunk + 1 < 6 else load_w([(C_AZ, 256)])
                    if prev_c is not None:
                        g1_conv(prev_c)
                    prev_c = c
            g1_conv(prev_c)
            pending_w = [nxt_w]

            if "qkv_tok" in dbg:
                op("sp", lambda e: e.dma_start(out=dbg["qkv_tok"].rearrange("(t p) c -> p t c", p=128), in_=qkv_tok),
                   reads=[("qkv_tok", tt, c) for tt in range(NT) for c in range(12)], writes=["dbg_qkv_tok"], dma=True)
                op("sp", None, reads=["dbg_qkv_tok"])
            sch.barrier()
            if stop_after == "g1":
                return

            rwa = Alloc(cva.off, ARENA_BYTES)
            zr = [rwa([128, 256], F32) for _ in range(2)]
            rt = [rwa([128, 4, 32], F32) for _ in range(4)]
            roped = [rwa([128, 256], BF16) for _ in range(2)]
            op("pool", lambda e: e.memset(bv_tok, 1.0), writes=["bv_ones"])
            for a_ in range(2):
                for b_ in range(2):
                    op("pool", lambda e, a_=a_, b_=b_: e.memset(kz[a_][b_], 0.0), writes=["kz0"])

            def rope_ops(src, nh, dst_views, tt, rkey, wkeys, b):
                sv = src.rearrange("p (h d) -> p h d", h=nh)
                x1 = sv[:, :, 0:32]
                x2 = sv[:, :, 32:64]
                cc = cs[:, tt, 0:32].unsqueeze(1).to_broadcast([128, nh, 32])
                sn = cs[:, tt, 32:64].unsqueeze(1).to_broadcast([128, nh, 32])
                t = [r[:, 0:nh, :] for r in rt]
                op("dve", lambda e: e.tensor_tensor(out=t[0], in0=x1, in1=cc, op=ALU.mult), reads=[rkey, "cs"], writes=[("rt", 0)])
                op("pool", lambda e: e.tensor_tensor(out=t[1], in0=x2, in1=sn, op=ALU.mult), reads=[rkey, "cs"], writes=[("rt", 1)])
                op("pool", lambda e: e.tensor_tensor(out=t[2], in0=x2, in1=cc, op=ALU.mult), reads=[rkey, "cs"], writes=[("rt", 2)])
                op("dve", lambda e: e.tensor_tensor(out=t[3], in0=x1, in1=sn, op=ALU.mult), reads=[rkey, "cs"], writes=[("rt", 3)])
                for i, dv in enumerate(dst_views):
                    eng = "dve" if i % 2 == 0 else "pool"
                    op(eng, lambda e, dv=dv: e.tensor_tensor(out=dv[:, :, 0:32], in0=t[0], in1=t[1], op=ALU.subtract),
                       reads=[("rt", 0), ("rt", 1)], writes=wkeys)
                    op(eng, lambda e, dv=dv: e.tensor_tensor(out=dv[:, :, 32:64], in0=t[2], in1=t[3], op=ALU.add),
                       reads=[("rt", 2), ("rt", 3)], writes=wkeys)

            def tok_chunk(ranges, handler, sel=None, next_ranges=None):
                if pending_w[0] is not None:
                    wt, wkey, tot = pending_w[0]
                    pending_w[0] = None
                else:
                    wt, wkey, tot = load_w(ranges)
                lo, hi = (0, tot) if sel is None else sel
                pend = []
                for tt in range(NT):
                    if tt == 6 and next_ranges is not None:
                        pending_w[0] = load_w(next_ranges)
                    bk = 2 + (tt % 2)
                    for k in range(8):
                        op("pe", lambda e, k=k, tt=tt, bk=bk, wt=wt: e.matmul(
                            PB(bk)[:, 0:hi - lo], lhsT=hT[:, k, tt * 128:(tt + 1) * 128], rhs=wt[:, k, lo:hi],
                            start=(k == 0), stop=(k == 7)),
                           reads=[wkey, ("hT", tt)], writes=[("pb", bk)])
                    if tt >= 1:
                        pend.append(handler(tt - 1, 2 + ((tt - 1) % 2)))
                    if len(pend) >= 2:
                        p2 = pend.pop(0)
                        if p2 is not None:
                            p2()
                pend.append(handler(NT - 1, 2 + ((NT - 1) % 2)))
                for p2 in pend:
                    if p2 is not None:
                        p2()

            for j in range(2):
                def h_az(tt, bk, j=j):
                    op("act", lambda e: e.activation(out=azs[:, tt, j * 256:(j + 1) * 256], in_=PB(bk)[:, 0:256], func=AF.Silu),
                       reads=[("pb", bk)], writes=[("azs", tt, j)])
                tok_chunk([(C_AZ + j * 256, 256)], h_az, next_ranges=[(C_AZ + 256, 256)] if j == 0 else [(C_BQ, 256)])

            if stop_after == "u1":
                return
            for (c0, dstT, nm) in ((C_BQ, bqT, "bqT"), (C_IQ, iqT, "iqT")):
                for j in range(2):
                    def h_q(tt, bk, j=j, dstT=dstT, nm=nm):
                        b = tt % 2
                        op("act", lambda e: e.copy(out=zr[b], in_=PB(bk)[:, 0:256]), reads=[("pb", bk)], writes=[("zr", b)])
                        rope_ops(zr[b], 4, [roped[b].rearrange("p (h d) -> p h d", h=4)], tt, ("zr", b), [("roped", b)], b)
                        tbk = 6 + b

                        def part2():
                            for q in range(2):
                                op("pe", lambda e, q=q: e.transpose(out=PBb(tbk)[:, q * 128:(q + 1) * 128],
                                                                    in_=roped[b][:, q * 128:(q + 1) * 128], identity=ident_b),
                                   reads=[("roped", b), "ident_b"], writes=[("pb", tbk)])
                            op("act", lambda e: e.copy(out=dstT[:, 2 * j:2 * j + 2, tt * 128:(tt + 1) * 128],
                                                       in_=PBb(tbk)[:, 0:256].rearrange("p (q t) -> p q t", q=2)),
                               reads=[("pb", tbk)], writes=[(nm, tt, j)])
                        return part2
                    nr = [(c0 + 256, 256)] if j == 0 else ([(C_IQ, 256)] if c0 == C_BQ else [(C_BK, 256)])
                    tok_chunk([(c0 + j * 256, 256)], h_q, next_ranges=nr)

            if stop_after == "u23":
                return
            def h_kv(tt, bk):
                b = tt % 2
                op("act", lambda e: e.copy(out=zr[b], in_=PB(bk)[:, 0:256]), reads=[("pb", bk)], writes=[("zr", b)])
                rv = roped[b].rearrange("p (h d) -> p h d", h=4)
                rope_ops(zr[b][:, 0:128], 2, [rv[:, 0:2, :]], tt, ("zr", b), [("roped", b)], b)
                op("pool", lambda e: e.tensor_copy(out=rv[:, 2, :], in_=rv[:, 1, :]), reads=[("roped", b)], writes=[("roped", b)])
                op("pool", lambda e: e.tensor_copy(out=rv[:, 3, :], in_=rv[:, 0, :]), reads=[("roped", b)], writes=[("roped", b)])
                def part2():
                    tbk = 6 + b
                    for q in range(2):
                        op("pe", lambda e, q=q: e.transpose(out=PBb(tbk)[:, q * 128:(q + 1) * 128],
                                                            in_=roped[b][:, q * 128:(q + 1) * 128], identity=ident_b),
                           reads=[("roped", b), "ident_b"], writes=[("pb", tbk)])
                    ts_ = slice(tt * 128, (tt + 1) * 128)
                    op("act", lambda e: e.copy(out=kz[0][0][0:64, ts_], in_=PBb(tbk)[0:64, 0:128]), reads=[("pb", tbk), "kz0"],
                       writes=[("bkT", tt)])
                    op("act", lambda e: e.copy(out=kz[1][1][64:128, ts_], in_=PBb(tbk)[64:128, 0:128]), reads=[("pb", tbk)],
                       writes=[("bkT", tt)])
                    op("act", lambda e: e.copy(out=kz[1][0][0:64, ts_], in_=PBb(tbk)[0:64, 128:256]), reads=[("pb", tbk)],
                       writes=[("bkT", tt)])
                    op("act", lambda e: e.copy(out=kz[0][1][64:128, ts_], in_=PBb(tbk)[64:128, 128:256]), reads=[("pb", tbk)],
                       writes=[("bkT", tt)])

                op("dve", lambda e: e.tensor_copy(out=bv_tok[:, tt, 0:64], in_=zr[b][:, 128:192]), reads=[("zr", b), "bv_ones"],
                   writes=[("bv", tt)])
                op("dve", lambda e: e.tensor_copy(out=bv_tok[:, tt, 65:129], in_=zr[b][:, 192:256]), reads=[("zr", b)],
                   writes=[("bv", tt)])
                return part2
            tok_chunk([(C_BK, 256)], h_kv, next_ranges=[(C_IW + 8 - 256, 256)])

            if stop_after == "u4a":
                return
            IW_SCALE = (8 ** -0.5) * (64 ** -0.5)

            def h_small(tt, bk):
                b = tt % 2
                op("act", lambda e: e.copy(out=zr[b][:, 0:72], in_=PB(bk)[:, 0:72]), reads=[("pb", bk)], writes=[("zr", b)])
                rv = roped[b].rearrange("p (h d) -> p h d", h=4)
                rope_ops(zr[b][:, 0:64], 1, [rv[:, 0:1, :], rv[:, 1:2, :]], tt, ("zr", b), [("roped", b)], b)
                def part2():
                    tbk = 6 + b
                    op("pe", lambda e: e.transpose(out=PBb(tbk)[:, 0:128], in_=roped[b][:, 0:128], identity=ident_b),
                       reads=[("roped", b), "ident_b"], writes=[("pb", tbk)])
                    op("act", lambda e: e.copy(out=ikT2[:, tt * 128:(tt + 1) * 128], in_=PBb(tbk)[:, 0:128]),
                       reads=[("pb", tbk)], writes=[("ikT", tt)])

                op("dve", lambda e: e.tensor_scalar(out=iw_tok[:, tt, :], in0=zr[b][:, 64:72], scalar1=IW_SCALE, scalar2=None,
                                                    op0=ALU.mult), reads=[("zr", b)], writes=[("iw", tt)])
                return part2
            tok_chunk([(C_IW + 8 - 256, 256)], h_small, sel=(184, 256), next_ranges=[(C_BETA, 256)])

            def h_ab(tt, bk):
                op("act", lambda e: e.copy(out=ab_tok[:, tt, :], in_=PB(bk)[:, 0:16]), reads=[("pb", bk)], writes=[("ab", tt)])
            tok_chunk([(C_BETA, 256)], h_ab, sel=(0, 16))

            for nm, t_, shape in (("bqT", bqT, None), ("iqT", iqT, None)):
                if nm in dbg:
                    op("sp", lambda e, nm=nm, t_=t_: e.dma_start(out=dbg[nm].rearrange("(a p) t -> p a t", p=128), in_=t_),
                       reads=[(nm, tt, j) for tt in range(NT) for j in range(2)], writes=["dbg_" + nm], dma=True)
                    op("sp", None, reads=["dbg_" + nm])
            if "misc" in dbg:
                sch.barrier()
                mt = view(R_W, [128, NT, 154], F32)
                op("dve", lambda e: e.tensor_copy(out=mt[:, :, 0:16], in_=ab_tok), reads=[("ab", tt) for tt in range(NT)], writes=["mt"])
                op("dve", lambda e: e.tensor_copy(out=mt[:, :, 16:24], in_=iw_tok), reads=[("iw", tt) for tt in range(NT)], writes=["mt"])
                op("dve", lambda e: e.tensor_copy(out=mt[:, :, 24:154], in_=bv_tok), reads=[("bv", tt) for tt in range(NT)], writes=["mt"])
                op("sp", lambda e: e.dma_start(out=dbg["misc"].rearrange("(t p) c -> p t c", p=128), in_=mt), reads=["mt"],
                   writes=["dbg_misc"], dma=True)
                op("sp", None, reads=["dbg_misc"])
            sch.barrier()
            if stop_after == "p2":
                return

            class MultiAlloc:
                def __init__(self, regions):
                    self.regs = [[a, b] for a, b in regions]

                def __call__(self, shape, dt):
                    n = 1
                    for s_ in shape[1:]:
                        n *= s_
                    size = (n * DT_SIZE[dt] + 63) // 64 * 64
                    for r in self.regs:
                        r[0] = (r[0] + 63) // 64 * 64
                        if r[0] + size <= r[1]:
                            v = view(r[0], shape, dt)
                            r[0] += size
                            return v
                    raise AssertionError(("MultiAlloc out of space", shape, self.regs))

            def dump(name, ap, reads):
                if name in dbg:
                    op("sp", lambda e: e.dma_start(out=dbg[name], in_=ap), reads=reads, writes=["dbg_" + name], dma=True)
                    op("sp", None, reads=["dbg_" + name])

            o_aT = view(R_A, [128, 4, S], BF16)
            diagw_off = R_WORK - 12 * K
            ga = MultiAlloc([(R_W, R_W + 16 * K), (R_A + 16 * K, R_A + 32 * K), (diagw_off, ARENA_BYTES)])
            g_all = ga([128, NT, 8], F32)
            bet = ga([128, NT, 8], F32)
            gs = ga([128, 24], F32)
            eG = ga([128, 8], F32)
            eGlmG = ga([128, 8], F32)
            scs = [ga([128, 4], F32) for _ in range(2)]
            g_bc = ga([128, 8, 128], F32)
            sq = ga([128, 1024], F32)
            ssn = ga([128, 16], F32)
            rn = ga([128, 16], F32)
            cq = ga([128, 8], F32)
            cqd = ga([128, 8], F32)
            cbk = ga([128, 8], F32)
            ckd = ga([128, 8], F32)
            negbeta = ga([128, 8], F32)
            qn = ga([128, 512], BF16)
            qd = ga([128, 512], BF16)
            kn = ga([128, 512], BF16)
            rhsk = ga([128, 512], BF16)
            kdec = ga([128, 512], BF16)
            rhsv = ga([128, 512], BF16)
            qnT = ga([128, 4, 128], BF16)
            qdT = ga([128, 4, 128], BF16)
            knT = ga([128, 4, 128], BF16)
            Dm = ga([128, 8, 128], BF16)
            Ds = ga([128, 8, 128], BF16)
            Mm = [ga([128, 8, 128], BF16) for _ in range(2)]
            Nm = [ga([128, 8, 128], BF16) for _ in range(2)]
            Pm = [ga([128, 8, 128], BF16) for _ in range(2)]
            qkm = ga([128, 8, 128], BF16)
            qkT_sb = ga([128, 8, 128], BF16)
            u_c = ga([128, 2, 512], F32)
            w_tok = ga([128, 512], BF16)
            wT_sb = ga([128, 4, 128], BF16)
            vn_b = ga([128, 512], BF16)
            Sst = ga([128, 4, 128], F32)
            Stmp = ga([128, 4, 128], F32)
            S_bd = ga([128, 4, 128], BF16)
            bdmask = ga([128, 4, 128], BF16)
            o_c = ga([128, 512], F32)
            qkT_c1 = ga([128, 8, 64], BF16)
            kdec_c1 = ga([128, 512], BF16)
            az_c = ga([128, 512], BF16)
            ss2 = ga([128, 8], F32)
            r2 = ga([128, 8], F32)
            oa_b = ga([128, 512], BF16)

            def bc8(v):
                return v.unsqueeze(2).to_broadcast([128, 8, 64])

            ABK = [("ab", tt) for tt in range(NT)]
            op("act", lambda e: e.activation(out=bet, in_=ab_tok[:, :, 0:8], func=AF.Sigmoid), reads=["ab_all"], writes=["bet"])
            op("dve", lambda e: e.tensor_tensor(out=g_all, in0=ab_tok[:, :, 8:16], in1=dtb.unsqueeze(1).to_broadcast([128, NT, 8]),
                                                op=ALU.add), reads=["ab_all", "dtb"], writes=["g_all"])
            op("act", lambda e: e.activation(out=g_all, in_=g_all, func=AF.Exp), reads=["g_all"], writes=["g_all"])
            op("act", lambda e: e.activation(out=g_all, in_=g_all, func=AF.Ln, bias=1.0), reads=["g_all"], writes=["g_all"])
            op("dve", lambda e: e.tensor_tensor(out=g_all, in0=g_all, in1=negA.unsqueeze(1).to_broadcast([128, NT, 8]),
                                                op=ALU.mult), reads=["g_all", "negA"], writes=["g_all"])
            op("dve", lambda e: e.memset(Sst, 0.0), writes=["S"])
            op("dve", lambda e: e.memset(S_bd, 0.0), writes=["S_bd"])
            op("pool", lambda e: e.memset(bdmask, 0.0), writes=["bdmask"])
            op("pool", lambda e: e.memset(bdmask[0:64, :, 0:64], 1.0), writes=["bdmask"])
            op("pool", lambda e: e.memset(bdmask[64:128, :, 64:128], 1.0), writes=["bdmask"])
            if "g" in dbg:
                op("sp", lambda e: e.dma_start(out=dbg["g"].rearrange("(t p) c -> p t c", p=128), in_=g_all), reads=["g_all"],
                   writes=["dbg_g"], dma=True)
                op("sp", None, reads=["dbg_g"])

            if stop_after == "gdn_pre":
                return
            for tt in range(NT):
                op("pe", lambda e, tt=tt: e.matmul(PB(0)[:, 0:8], lhsT=ucs_f, rhs=g_all[:, tt, :], start=True, stop=True),
                   reads=["g_all"], writes=[("pb", 0)])
                op("pe", lambda e, tt=tt: e.matmul(PB(0)[:, 8:16], lhsT=mc0_f, rhs=g_all[:, tt, :], start=True, stop=True),
                   reads=["g_all"], writes=[("pb", 0)])
                op("pe", lambda e, tt=tt: e.matmul(PB(0)[:, 16:24], lhsT=mc1_f, rhs=g_all[:, tt, :], start=True, stop=True),
                   reads=["g_all"], writes=[("pb", 0)])
                op("act", lambda e: e.copy(out=gs, in_=PB(0)[:, 0:24]), reads=[("pb", 0)], writes=["gs"])
                op("act", lambda e: e.activation(out=eG, in_=gs[:, 0:8], func=AF.Exp), reads=["gs"], writes=["eG"])
                op("dve", lambda e: e.tensor_tensor(out=eGlmG[0:64, :], in0=gs[0:64, 8:16], in1=gs[0:64, 0:8], op=ALU.subtract),
                   reads=["gs"], writes=["eGlmG"])
                op("dve", lambda e: e.tensor_tensor(out=eGlmG[64:128, :], in0=gs[64:128, 16:24], in1=gs[64:128, 0:8],
                                                    op=ALU.subtract), reads=["gs"], writes=["eGlmG"])
                op("act", lambda e: e.activation(out=eGlmG, in_=eGlmG, func=AF.Exp), reads=["eGlmG"], writes=["eGlmG"])
                for hf in range(2):
                    c0 = 8 + 8 * hf
                    op("act", lambda e, hf=hf, c0=c0: e.activation(out=scs[hf][0:64, :], in_=gs[0:64, c0:c0 + 8:2], func=AF.Exp),
                       reads=["gs"], writes=[("scs", hf)])
                    op("act", lambda e, hf=hf, c0=c0: e.activation(out=scs[hf][64:128, :], in_=gs[64:128, c0 + 1:c0 + 8:2],
                                                                   func=AF.Exp), reads=["gs"], writes=[("scs", hf)])
                op("dve", lambda e, tt=tt: e.tensor_scalar(out=g_bc, in0=g_all[:, tt, :].unsqueeze(2).to_broadcast([128, 8, 128]),
                                                           scalar1=-1.0, scalar2=None, op0=ALU.mult),
                   reads=["g_all"], writes=["g_bc"])
                if stop_after == "gdn_a":
                    return
                QK = [("qkv_tok", tt, c) for c in range(8)]
                VV = [("qkv_tok", tt, c) for c in range(8, 12)]
                op("dve", lambda e, tt=tt: e.tensor_tensor(out=sq, in0=qkv_tok[:, tt, 0:1024], in1=qkv_tok[:, tt, 0:1024],
                                                           op=ALU.mult), reads=["qkv_all"], writes=["sq"])
                op("dve", lambda e: e.tensor_reduce(out=ssn, in_=sq.rearrange("p (h d) -> p h d", h=16), axis=AX.X, op=ALU.add),
                   reads=["sq"], writes=["ssn"])
                op("act", lambda e: e.activation(out=rn, in_=ssn, func=AF.Ln, bias=epsc, scale=1.0), reads=["ssn", "epsc"],
                   writes=["rn"])
                op("act", lambda e: e.activation(out=rn, in_=rn, func=AF.Exp, scale=-0.5), reads=["rn"], writes=["rn"])
                op("dve", lambda e: e.tensor_scalar(out=cq, in0=rn[:, 0:8], scalar1=0.125, scalar2=None, op0=ALU.mult),
                   reads=["rn"], writes=["cq"])
                op("dve", lambda e: e.tensor_tensor(out=cqd, in0=cq, in1=eG, op=ALU.mult), reads=["cq", "eG"], writes=["cqd"])
                op("dve", lambda e, tt=tt: e.tensor_tensor(out=cbk, in0=rn[:, 8:16], in1=bet[:, tt, :], op=ALU.mult),
                   reads=["rn", "bet"], writes=["cbk"])
                op("dve", lambda e: e.tensor_tensor(out=cbk, in0=cbk, in1=eG, op=ALU.mult), reads=["cbk", "eG"], writes=["cbk"])
                op("dve", lambda e: e.tensor_tensor(out=ckd, in0=rn[:, 8:16], in1=eGlmG, op=ALU.mult), reads=["rn", "eGlmG"],
                   writes=["ckd"])
                op("dve", lambda e, tt=tt: e.tensor_scalar(out=negbeta, in0=bet[:, tt, :], scalar1=-1.0, scalar2=None,
                                                           op0=ALU.mult), reads=["bet"], writes=["negbeta"])
                qv = qkv_tok[:, tt, 0:512].rearrange("p (h d) -> p h d", h=8)
                kv = qkv_tok[:, tt, 512:1024].rearrange("p (h d) -> p h d", h=8)
                vv = qkv_tok[:, tt, 1024:1536].rearrange("p (h d) -> p h d", h=8)

                def v3(t_):
                    return t_.rearrange("p (h d) -> p h d", h=8)
                op("dve", lambda e, qv=qv: e.tensor_tensor(out=v3(qn), in0=qv, in1=bc8(cq), op=ALU.mult),
                   reads=["qkv_all", "cq"], writes=["qn"])
                op("pool", lambda e, qv=qv: e.tensor_tensor(out=v3(qd), in0=qv, in1=bc8(cqd), op=ALU.mult),
                   reads=["qkv_all", "cqd"], writes=["qd"])
                op("dve", lambda e, kv=kv: e.tensor_tensor(out=v3(kn), in0=kv, in1=bc8(rn[:, 8:16]), op=ALU.mult),
                   reads=["qkv_all", "rn"], writes=["kn"])
                op("pool", lambda e, kv=kv: e.tensor_tensor(out=v3(rhsk), in0=kv, in1=bc8(cbk), op=ALU.mult),
                   reads=["qkv_all", "cbk"], writes=["rhsk"])
                op("pool", lambda e, kv=kv: e.tensor_tensor(out=v3(kdec), in0=kv, in1=bc8(ckd), op=ALU.mult),
                   reads=["qkv_all", "ckd"], writes=["kdec"])
                op("dve", lambda e, vv=vv, tt=tt: e.tensor_tensor(out=v3(rhsv), in0=vv, in1=bc8(bet[:, tt, :]), op=ALU.mult),
                   reads=["qkv_all", "bet"], writes=["rhsv"])
                if stop_after == "gdn_b":
                    return
                for (src, skey, dst, dkey, bank, coff, eng) in ((qn, "qn", qnT, "qnT", 6, 0, "act"), (qd, "qd", qdT, "qdT", 7, 0, "dve"),
                                                                (kn, "kn", knT, "knT", 0, 0, "act")):
                    for q in range(4):
                        op("pe", lambda e, src=src, bank=bank, coff=coff, q=q: e.transpose(
                            out=PBb(bank)[:, coff + q * 128:coff + (q + 1) * 128], in_=src[:, q * 128:(q + 1) * 128], identity=ident_b),
                           reads=[skey, "ident_b"], writes=[("pb", bank)])
                    if eng == "act":
                        op("act", lambda e, dst=dst, bank=bank, coff=coff: e.copy(
                            out=dst, in_=PBb(bank)[:, coff:coff + 512].rearrange("p (q t) -> p q t", q=4)),
                           reads=[("pb", bank)], writes=[dkey])
                    else:
                        op("dve", lambda e, dst=dst, bank=bank, coff=coff: e.tensor_copy(
                            out=dst, in_=PBb(bank)[:, coff:coff + 512].rearrange("p (q t) -> p q t", q=4)),
                           reads=[("pb", bank)], writes=[dkey])
                    if stop_after == "gdn_c_" + skey:
                        return
                if tt == 0:
                    dump("qn0", qn, ["qn"]); dump("kn0", kn, ["kn"]); dump("rhsv0", rhsv, ["rhsv"]); dump("rhsk0", rhsk, ["rhsk"])
                    dump("kdec0", kdec, ["kdec"]); dump("qd0", qd, ["qd"]); dump("gs0", gs, ["gs"])
                    dump("knT0", knT.rearrange("p a t -> p (a t)"), ["knT"])
                    dump("rn0", rn, ["rn"]); dump("cq0", cq, ["cq"]); dump("cqd0", cqd, ["cqd"]); dump("cbk0", cbk, ["cbk"])
                    dump("ckd0", ckd, ["ckd"]); dump("eG0", eG, ["eG"]); dump("ssn0", ssn, ["ssn"])
                if stop_after == "gdn_c":
                    return
                def group_gen(hg, bA, bB, bC, bT):
                    hs_list = list(range(4))
                    grp = slice(4 * hg, 4 * hg + 4)
                    for hs in hs_list:
                        h = 4 * hg + hs
                        hp, par = h // 2, h % 2
                        rows = slice(par * 64, par * 64 + 64)
                        cs_ = slice(hs * 128, hs * 128 + 128)
                        op("pe", lambda e, hp=hp, rows=rows, cs_=cs_, par=par: e.matmul(
                            PB(bA)[:, cs_], lhsT=knT[rows, hp, :], rhs=knT[rows, hp, :], start=True, stop=True,
                            tile_position=(par * 64, 0)), reads=["knT"], writes=[("pb", bA)])
                        op("pe", lambda e, hp=hp, rows=rows, cs_=cs_, par=par: e.matmul(
                            PB(bB)[:, cs_], lhsT=qnT[rows, hp, :], rhs=knT[rows, hp, :], start=True, stop=True,
                            tile_position=(par * 64, 0)), reads=["knT", "qnT"], writes=[("pb", bB)])
                        op("pe", lambda e, h=h, cs_=cs_: e.matmul(PB(bC)[:, cs_], lhsT=g_bc[:, h, :], rhs=ucs_f, start=True, stop=False),
                           reads=["g_bc", "ucs_f"], writes=[("pb", bC)])
                        op("pe", lambda e, cs_=cs_: e.matmul(PB(bC)[:, cs_], lhsT=ident_f, rhs=maskneg_f, start=False, stop=True),
                           reads=["ident_f", "maskneg_f"], writes=[("pb", bC)])
                    yield
                    for hs in hs_list:
                        h = 4 * hg + hs
                        cs_ = slice(hs * 128, hs * 128 + 128)
                        op("act", lambda e, h=h, cs_=cs_: e.activation(out=Dm[:, h, :], in_=PB(bC)[:, cs_], func=AF.Exp,
                                                                       bias=gs[:, h:h + 1], scale=1.0),
                           reads=[("pb", bC), "gs"], writes=[("Dm", h)])
                        op("pool", lambda e, h=h: e.tensor_tensor(out=Ds[:, h, :], in0=Dm[:, h, :], in1=strict_b, op=ALU.mult),
                           reads=[("Dm", h), "strict_b"], writes=[("Ds", h)])
                        op("dve", lambda e, h=h, cs_=cs_: e.scalar_tensor_tensor(out=Mm[0][:, h, :], in0=PB(bA)[:, cs_],
                                                                                 scalar=negbeta[:, h:h + 1], in1=Ds[:, h, :],
                                                                                 op0=ALU.mult, op1=ALU.mult),
                           reads=[("pb", bA), "negbeta", ("Ds", h)], writes=[("M", 0, hg)])
                        op("dve", lambda e, h=h, cs_=cs_: e.tensor_tensor(out=qkm[:, h, :], in0=PB(bB)[:, cs_], in1=Dm[:, h, :],
                                                                          op=ALU.mult),
                           reads=[("pb", bB), ("Dm", h)], writes=[("qkm", hg)])
                    yield
                    for hs in hs_list:
                        h = 4 * hg + hs
                        cs_ = slice(hs * 128, hs * 128 + 128)
                        op("pe", lambda e, h=h, cs_=cs_: e.transpose(out=PBb(bT)[:, cs_], in_=Mm[0][:, h, :], identity=ident_b),
                           reads=[("M", 0, hg), "ident_b"], writes=[("pb", bT)])
                    op("act", lambda e: e.copy(out=Nm[0][:, grp, :], in_=PBb(bT)[:, 0:512].rearrange("p (q t) -> p q t", q=4)),
                       reads=[("pb", bT)], writes=[("N", 0, hg)])
                    op("pool", lambda e: e.tensor_tensor(out=Pm[0][:, grp, :], in0=Nm[0][:, grp, :],
                                                         in1=ident_b.unsqueeze(1).to_broadcast([128, 4, 128]), op=ALU.add),
                       reads=[("N", 0, hg), "ident_b"], writes=[("P", 0, hg)])
                    yield
                    for hs in hs_list:
                        h = 4 * hg + hs
                        cs2 = slice(hs * 128, hs * 128 + 128)
                        op("pe", lambda e, h=h, cs2=cs2: e.transpose(out=PBb(bT)[:, cs2], in_=qkm[:, h, :], identity=ident_b),
                           reads=[("qkm", hg), "ident_b"], writes=[("pb", bT)])
                    op("dve", lambda e: e.tensor_copy(out=qkT_sb[:, grp, :], in_=PBb(bT)[:, 0:512].rearrange("p (q t) -> p q t", q=4)),
                       reads=[("pb", bT)], writes=[("qkT", hg)])
                    yield
                    for lv in range(1, 6):
                        cur, nxt = (lv - 1) % 2, lv % 2
                        for hs in hs_list:
                            h = 4 * hg + hs
                            cs_ = slice(hs * 128, hs * 128 + 128)
                            op("pe", lambda e, h=h, cs_=cs_, cur=cur: e.matmul(PB(bA)[:, cs_], lhsT=Nm[cur][:, h, :], rhs=Mm[cur][:, h, :],
                                                                               start=True, stop=True),
                               reads=[("N", cur, hg), ("M", cur, hg)], writes=[("pb", bA)])
                        if lv < 5:
                            for hs in hs_list:
                                h = 4 * hg + hs
                                cs_ = slice(hs * 128, hs * 128 + 128)
                                op("pe", lambda e, h=h, cs_=cs_, cur=cur: e.matmul(PB(bB)[:, cs_], lhsT=Mm[cur][:, h, :],
                                                                                   rhs=Nm[cur][:, h, :], start=True, stop=True),
                                   reads=[("N", cur, hg), ("M", cur, hg)], writes=[("pb", bB)])
                        yield
                        op("act", lambda e, nxt=nxt: e.copy(out=Mm[nxt][:, grp, :], in_=PB(bA).rearrange("p (q t) -> p q t", q=4)),
                           reads=[("pb", bA)], writes=[("M", nxt, hg)])
                        if lv < 5:
                            op("dve", lambda e, nxt=nxt: e.tensor_copy(out=Nm[nxt][:, grp, :],
                                                                       in_=PB(bB).rearrange("p (q t) -> p q t", q=4)),
                               reads=[("pb", bB)], writes=[("N", nxt, hg)])
                        for hs in hs_list:
                            h = 4 * hg + hs
                            cs_ = slice(hs * 128, hs * 128 + 128)
                            op("pe", lambda e, h=h, cs_=cs_, cur=cur, nxt=nxt: e.matmul(PB(bC)[:, cs_], lhsT=Mm[nxt][:, h, :],
                                                                                        rhs=Pm[cur][:, h, :], start=True, stop=True),
                               reads=[("M", nxt, hg), ("P", cur, hg)], writes=[("pb", bC)])
                        yield
                        op("dve", lambda e, cur=cur, nxt=nxt: e.tensor_tensor(
                            out=Pm[nxt][:, grp, :], in0=Pm[cur][:, grp, :], in1=PB(bC).rearrange("p (q t) -> p q t", q=4), op=ALU.add),
                           reads=[("pb", bC), ("P", cur, hg)], writes=[("P", nxt, hg)])

                gens = [group_gen(0, 3, 4, 5, 6), group_gen(1, 0, 1, 2, 7)]
                while gens:
                    for g_ in list(gens):
                        try:
                            next(g_)
                        except StopIteration:
                            gens.remove(g_)
                Pf = Pm[1]
                PK = [("P", 1, 0), ("P", 1, 1)]
                if tt == 0:
                    dump("D0", Dm.rearrange("p a t -> p (a t)"), [("Dm", h) for h in range(8)])
                    dump("M0", Mm[0].rearrange("p a t -> p (a t)"), [("M", 0, 0), ("M", 0, 1)])
                    dump("N0", Nm[0].rearrange("p a t -> p (a t)"), [("N", 0, 0), ("N", 0, 1)])
                    dump("P0", Pm[1].rearrange("p a t -> p (a t)"), [("P", 1, 0), ("P", 1, 1)])
                    dump("qkT0", qkT_sb.rearrange("p a t -> p (a t)"), [("qkT", 0), ("qkT", 1)])
                if stop_after == "gdn_e":
                    return
                for hf in range(2):
                    ub = 7 if hf == 0 else 0
                    for h in range(8):
                        op("pe", lambda e, h=h, hf=hf, ub=ub: e.matmul(PB(ub)[0:64, h * 64:(h + 1) * 64],
                                                                       lhsT=Pf[:, h, hf * 64:(hf + 1) * 64],
                                                                       rhs=rhsv[:, h * 64:(h + 1) * 64], start=True, stop=True),
                           reads=PK + ["rhsv"], writes=[("pb", ub)])
                    op("act", lambda e, hf=hf, ub=ub: e.copy(out=u_c[0:64, hf, :], in_=PB(ub)[0:64, :]), reads=[("pb", ub)],
                       writes=[("u_c", hf)])
                for h in range(8):
                    op("pe", lambda e, h=h: e.matmul(PB(1)[:, h * 64:(h + 1) * 64], lhsT=Pf[:, h, :], rhs=rhsk[:, h * 64:(h + 1) * 64],
                                                     start=True, stop=True), reads=PK + ["rhsk"], writes=[("pb", 1)])
                op("act", lambda e: e.copy(out=w_tok, in_=PB(1)), reads=[("pb", 1)], writes=["w_tok"])
                for q in range(4):
                    op("pe", lambda e, q=q: e.transpose(out=PBb(6)[:, q * 128:(q + 1) * 128], in_=w_tok[:, q * 128:(q + 1) * 128],
                                                        identity=ident_b), reads=["w_tok", "ident_b"], writes=[("pb", 6)])
                op("dve", lambda e: e.tensor_copy(out=wT_sb, in_=PBb(6)[:, 0:512].rearrange("p (q t) -> p q t", q=4)),
                   reads=[("pb", 6)], writes=["wT_sb"])
                op("sp", lambda e: e.dma_start(out=qkT_c1[0:64, :, :], in_=qkT_sb[64:128, :, 64:128]),
                   reads=[("qkT", 0), ("qkT", 1)], writes=["qkT_c1"], dma=True)
                op("sp", lambda e: e.dma_start(out=kdec_c1[0:64, :], in_=kdec[64:128, :]), reads=["kdec"], writes=["kdec_c1"],
                   dma=True)
                op("sp", lambda e, tt=tt: e.dma_start(out=az_c[0:64, :], in_=azs[64:128, tt, :]), reads=["azs_all"], writes=["az_c"],
                   dma=True)
                if tt == 0:
                    dump("u0", u_c.rearrange("p a t -> p (a t)"), [("u_c", 0), ("u_c", 1)])
                    dump("wT0", wT_sb.rearrange("p a t -> p (a t)"), ["wT_sb"])
                if stop_after == "gdn_f":
                    return
                for hf in range(2):
                    tcs = slice(hf * 64, hf * 64 + 64)
                    ck = 2 * tt + hf
                    if hf == 0:
                        qk_x, qk_keys = qkT_sb[0:64, :, 0:64], [("qkT", 0), ("qkT", 1)]
                        kd_x, kd_keys = kdec[0:64, :], ["kdec"]
                    else:
                        qk_x, qk_keys = qkT_c1[0:64, :, :], ["qkT_c1"]
                        kd_x, kd_keys = kdec_c1[0:64, :], ["kdec_c1"]
                    for hp in range(4):
                        op("pe", lambda e, hp=hp, tcs=tcs: e.matmul(PB(1)[0:64, hp * 128:(hp + 1) * 128], lhsT=wT_sb[:, hp, tcs],
                                                                    rhs=S_bd[:, hp, :], start=True, stop=True),
                           reads=["wT_sb", "S_bd"], writes=[("pb", 1)])
                    op("dve", lambda e, hf=hf: e.tensor_tensor(out=vn_b[0:64, :], in0=u_c[0:64, hf, :], in1=PB(1)[0:64, :],
                                                               op=ALU.subtract), reads=[("u_c", hf), ("pb", 1)], writes=["vn_b"])
                    for h in range(8):
                        hp, par = h // 2, h % 2
                        op("pe", lambda e, h=h, hp=hp, par=par, tcs=tcs: e.matmul(
                            PB(2)[0:64, h * 64:(h + 1) * 64], lhsT=qdT[:, hp, tcs], rhs=S_bd[:, hp, par * 64:(par + 1) * 64],
                            start=True, stop=False), reads=["qdT", "S_bd"], writes=[("pb", 2)])
                        op("pe", lambda e, h=h, qk_x=qk_x: e.matmul(
                            PB(2)[0:64, h * 64:(h + 1) * 64], lhsT=qk_x[:, h, :], rhs=vn_b[0:64, h * 64:(h + 1) * 64],
                            start=False, stop=True), reads=qk_keys + ["vn_b"], writes=[("pb", 2)])
                    for hp in range(4):
                        op("pe", lambda e, hp=hp, kd_x=kd_x: e.matmul(PB(7)[:, hp * 128:(hp + 1) * 128],
                                                                      lhsT=kd_x[:, hp * 128:(hp + 1) * 128],
                                                                      rhs=vn_b[0:64, hp * 128:(hp + 1) * 128], start=True, stop=True),
                           reads=kd_keys + ["vn_b"], writes=[("pb", 7)])
                    op("act", lambda e: e.copy(out=o_c[0:64, :], in_=PB(2)[0:64, :]), reads=[("pb", 2)], writes=["o_c"])
                    op("pool", lambda e, hf=hf: e.tensor_tensor(out=Stmp, in0=Sst,
                                                                in1=scs[hf].unsqueeze(2).to_broadcast([128, 4, 128]), op=ALU.mult),
                       reads=["S", ("scs", hf)], writes=["Stmp"])
                    op("dve", lambda e: e.tensor_tensor(out=Sst, in0=Stmp, in1=PB(7).rearrange("p (a d) -> p a d", a=4), op=ALU.add),
                       reads=["Stmp", ("pb", 7)], writes=["S"])
                    op("pool", lambda e: e.tensor_tensor(out=S_bd, in0=Sst, in1=bdmask, op=ALU.mult), reads=["S", "bdmask"],
                       writes=["S_bd"])
                    if "o_raw" in dbg:
                        op("sp", lambda e, ck=ck: e.dma_start(out=dbg["o_raw"][ck * 64:(ck + 1) * 64, :], in_=o_c[0:64, :]),
                           reads=["o_c"], writes=["dbg_o_raw"], dma=True)
                    sqh = sq[0:64, 0:512]
                    op("dve", lambda e: e.tensor_tensor(out=sqh, in0=o_c[0:64, :], in1=o_c[0:64, :], op=ALU.mult), reads=["o_c"],
                       writes=["sq"])
                    op("dve", lambda e: e.tensor_reduce(out=ss2[0:64, :], in_=sqh.rearrange("p (h d) -> p h d", h=8), axis=AX.X,
                                                        op=ALU.add), reads=["sq"], writes=["ss2"])
                    op("act", lambda e: e.activation(out=r2[0:64, :], in_=ss2[0:64, :], func=AF.Ln, bias=epsc[0:64, :],
                                                     scale=1.0 / 64), reads=["ss2", "epsc"], writes=["r2"])
                    op("act", lambda e: e.activation(out=r2[0:64, :], in_=r2[0:64, :], func=AF.Exp, scale=-0.5), reads=["r2"],
                       writes=["r2"])
                    op("dve", lambda e: e.tensor_tensor(out=v3(sqh), in0=v3(o_c[0:64, :]),
                                                        in1=r2[0:64, :].unsqueeze(2).to_broadcast([64, 8, 64]), op=ALU.mult),
                       reads=["o_c", "r2"], writes=["sq"])
                    op("pool", lambda e: e.tensor_tensor(out=v3(sqh), in0=v3(sqh),
                                                         in1=angb[0:64, :].unsqueeze(1).to_broadcast([64, 8, 64]), op=ALU.mult),
                       reads=["sq", "angb"], writes=["sq"])
                    az_x = azs[0:64, tt, :] if hf == 0 else az_c[0:64, :]
                    op("pool", lambda e, az_x=az_x: e.tensor_tensor(out=oa_b[0:64, :], in0=sqh, in1=az_x, op=ALU.mult),
                       reads=["sq", "az_c"], writes=["oa_b"])
                    for q in range(4):
                        op("pe", lambda e, q=q: e.transpose(out=PBb(6)[:, q * 64:(q + 1) * 64], in_=oa_b[0:64, q * 128:(q + 1) * 128],
                                                            identity=ident_b[0:64, 0:64]), reads=["oa_b", "ident_b"],
                           writes=[("pb", 6)])
                    op("act", lambda e, ck=ck: e.copy(out=o_aT[:, :, ck * 64:(ck + 1) * 64],
                                                      in_=PBb(6)[:, 0:256].rearrange("p (q t) -> p q t", q=4)),
                       reads=[("pb", 6)], writes=[("o_aT", ck)])
            if "o_raw" in dbg:
                op("sp", None, reads=["dbg_o_raw"])
            if "o_aT" in dbg:
                op("sp", lambda e: e.dma_start(out=dbg["o_aT"].rearrange("(a p) t -> p a t", p=128), in_=o_aT),
                   reads=[("o_aT", ck) for ck in range(2 * NT)], writes=["dbg_o_aT"], dma=True)
                op("sp", None, reads=["dbg_o_aT"])

            sch.barrier()
            if stop_after == "gdn":
                return

            o_bT = view(R_A + 16 * K, [128, 4, S], BF16)
            da = MultiAlloc([(R_Q, R_Q + 48 * K), (R_W, R_W + 16 * K)])
            scoreb = [da([128, S], F32) for _ in range(2)]
            rl = [da([128, 512], F32) for _ in range(2)]
            maskbb = [da([128, S], BF16) for _ in range(2)]
            thr_t = [da([128, 1], F32) for _ in range(2)]
            PTt = [[da([128, 512], BF16) for _ in range(2)] for _ in range(2)]
            I4 = da([128, 512], BF16)
            lo_t = da([128, 1], F32)
            hi_t = da([128, 1], F32)
            W0 = da([128, 1], F32)
            mid_t = da([128, 1], F32)
            tsel = da([128, 1], F32)
            Wk = da([128, NBIS], F32)
            cnt = da([128, NBIS], F32)
            pow2 = da([128, NBIS], F32)
            ob = da([128, 520], F32)
            rden = da([128, 8], F32)
            ob_b = da([128, 512], BF16)
            for q in range(4):
                op("pool", lambda e, q=q: e.tensor_copy(out=I4[:, q * 128:(q + 1) * 128], in_=ident_b), reads=["ident_b"], writes=["I4"])
            for k in range(NBIS):
                op("pool", lambda e, k=k: e.memset(pow2[:, k:k + 1], 2.0 ** (-(k + 1))), writes=["pow2"])

            def scores_part(tt, sb):
                L = (tt + 1) * 128
                nkb = (L + 511) // 512
                qs = slice(tt * 128, (tt + 1) * 128)
                score = scoreb[sb]
                maskb = maskbb[sb]
                for h in range(8):
                    hp, par = h // 2, h % 2
                    rows = slice(par * 64, par * 64 + 64)
                    for kb in range(nkb):
                        w = min(512, L - kb * 512)
                        bank = kb % 2
                        ks = slice(kb * 512, kb * 512 + w)
                        op("pe", lambda e, hp=hp, par=par, rows=rows, w=w, bank=bank, ks=ks: e.matmul(
                            PB(bank)[:, 0:w], lhsT=iqT[rows, hp, qs], rhs=ikT2[rows, ks], start=True, stop=True,
                            tile_position=(par * 64, 0)), reads=["iqT", "ikT2"], writes=[("pb", bank)])
                        op("act", lambda e, w=w, bank=bank: e.activation(out=rl[bank][:, 0:w], in_=PB(bank)[:, 0:w], func=AF.Relu),
                           reads=[("pb", bank)], writes=[("rl", bank)])
                        if h == 0:
                            op("dve", lambda e, w=w, bank=bank, ks=ks: e.tensor_scalar(
                                out=score[:, ks], in0=rl[bank][:, 0:w], scalar1=iw_tok[:, tt, 0:1], scalar2=None, op0=ALU.mult),
                               reads=[("rl", bank), "iw_tok"], writes=[("score", sb, kb)])
                        else:
                            op("dve", lambda e, w=w, bank=bank, ks=ks, h=h: e.scalar_tensor_tensor(
                                out=score[:, ks], in0=rl[bank][:, 0:w], scalar=iw_tok[:, tt, h:h + 1], in1=score[:, ks],
                                op0=ALU.mult, op1=ALU.add), reads=[("rl", bank), "iw_tok", ("score", sb, kb)],
                               writes=[("score", sb, kb)], fast=(w >= 256))
                SK = [("score", sb, kb) for kb in range(nkb)]
                if tt >= 2:
                    op("dve", lambda e: e.tensor_reduce(out=hi_t, in_=score[:, 0:L], axis=AX.X, op=ALU.max), reads=SK, writes=["hi"])
                    op("dve", lambda e: e.tensor_reduce(out=lo_t, in_=score[:, 0:L], axis=AX.X, op=ALU.min), reads=SK, writes=["lo"])
                op("dve", lambda e: e.memset(score[0:64, L - 64:L], -1.0e30), reads=SK, writes=SK)
                if tt >= 2:
                    op("dve", lambda e: e.tensor_tensor(out=W0, in0=hi_t, in1=lo_t, op=ALU.subtract), reads=["hi", "lo"], writes=["W0"])
                    op("dve", lambda e: e.tensor_scalar(out=Wk, in0=pow2, scalar1=W0[:, 0:1], scalar2=None, op0=ALU.mult),
                       reads=["W0", "pow2"], writes=["Wk"])
                    op("dve", lambda e: e.memset(cnt, 0.0), writes=["cnt"])
                    op("dve", lambda e: e.tensor_tensor(out=mid_t, in0=lo_t, in1=Wk[:, 0:1], op=ALU.add), reads=["lo", "Wk"], writes=["mid"])
                    for k in range(NBIS):
                        op("dve", lambda e, k=k: e.tensor_scalar(out=maskb[:, 0:L], in0=score[:, 0:L], scalar1=mid_t[:, 0:1],
                                                                 scalar2=0.0, op0=ALU.is_gt, op1=ALU.add, accum_out=cnt[:, k:k + 1]),
                           reads=SK + ["mid", "cnt"], writes=[("maskb", sb), ("cntk", k)])
                        op("dve", lambda e, k=k: e.tensor_scalar(out=tsel, in0=cnt[:, k:k + 1], scalar1=255.5, scalar2=0.5,
                                                                 op0=ALU.is_gt, op1=ALU.subtract), reads=[("cntk", k)], writes=["tsel"])
                        op("dve", lambda e, k=k: e.scalar_tensor_tensor(out=mid_t, in0=tsel, scalar=Wk[:, k:k + 1], in1=mid_t,
                                                                        op0=ALU.mult, op1=ALU.add),
                           reads=["tsel", "Wk", "mid"], writes=["mid"])
                    op("dve", lambda e: e.scalar_tensor_tensor(out=thr_t[sb], in0=Wk[:, NBIS - 1:NBIS], scalar=-0.5, in1=mid_t,
                                                               op0=ALU.mult, op1=ALU.add), reads=["Wk", "mid"], writes=[("thr", sb)])
                else:
                    op("dve", lambda e: e.memset(thr_t[sb], -1.0e29), writes=[("thr", sb)])
                op("dve", lambda e: e.tensor_scalar(out=maskb[:, 0:L], in0=score[:, 0:L], scalar1=thr_t[sb][:, 0:1], scalar2=NEG,
                                                    op0=ALU.is_le, op1=ALU.mult), reads=SK + [("thr", sb)], writes=[("maskb", sb)])
                if "thr" in dbg:
                    op("sp", lambda e: e.dma_start(out=dbg["thr"][tt * 128:(tt + 1) * 128, :], in_=thr_t[sb]), reads=[("thr", sb)],
                       writes=["dbg_thr"], dma=True)
                if "score" in dbg and tt == NT - 1:
                    op("sp", lambda e: e.dma_start(out=dbg["score"], in_=score), reads=SK, writes=["dbg_score"], dma=True)

            def attn_part(tt, sb):
                qs = slice(tt * 128, (tt + 1) * 128)
                maskb = maskbb[sb]
                for kb in range(tt + 1):
                    kcs = slice(kb * 128, (kb + 1) * 128)
                    for g2 in range(2):
                        bank = 2 + g2 + 2 * (kb % 2)
                        pt = PTt[g2][kb % 2]
                        op("pe", lambda e, bank=bank, kcs=kcs: e.matmul(PB(bank), lhsT=maskb[:, kcs], rhs=I4, start=True, stop=False),
                           reads=[("maskb", sb), "I4"], writes=[("pb", bank)])
                        for s_ in range(4):
                            h = 4 * g2 + s_
                            hp, par = h // 2, h % 2
                            kT = kz[g2][par]
                            op("pe", lambda e, bank=bank, s_=s_, kT=kT, kcs=kcs, hp=hp: e.matmul(
                                PB(bank)[:, s_ * 128:(s_ + 1) * 128], lhsT=kT[:, kcs], rhs=bqT[:, hp, qs], start=False, stop=(s_ == 3)),
                               reads=["bkT", "bqT"], writes=[("pb", bank)])
                        op("act", lambda e, bank=bank, pt=pt: e.activation(out=pt, in_=PB(bank), func=AF.Exp, scale=0.125),
                           reads=[("pb", bank)], writes=[("PT", g2, kb % 2)])
                        for s_ in range(4):
                            op("pe", lambda e, g2=g2, s_=s_, pt=pt, kb=kb: e.matmul(
                                PB(6 + g2)[:, s_ * 65:(s_ + 1) * 65], lhsT=pt[:, s_ * 128:(s_ + 1) * 128],
                                rhs=bv_tok[:, kb, g2 * 65:(g2 + 1) * 65], start=(kb == 0 and s_ == 0), stop=(kb == tt and s_ == 3)),
                               reads=[("PT", g2, kb % 2), "bv_tok"], writes=[("pb", 6 + g2)])
                op("act", lambda e: e.copy(out=ob[:, 0:260], in_=PB(6)[:, 0:260]), reads=[("pb", 6)], writes=["ob"])
                op("act", lambda e: e.copy(out=ob[:, 260:520], in_=PB(7)[:, 0:260]), reads=[("pb", 7)], writes=["ob"])
                obv = ob.rearrange("p (s e) -> p s e", e=65)
                op("dve", lambda e: e.reciprocal(out=rden, in_=obv[:, :, 64]), reads=["ob"], writes=["rden"])
                op("dve", lambda e: e.tensor_tensor(out=ob_b.rearrange("p (h d) -> p h d", h=8), in0=obv[:, :, 0:64],
                                                    in1=rden.unsqueeze(2).to_broadcast([128, 8, 64]), op=ALU.mult),
                   reads=["ob", "rden"], writes=["ob_b"])
                for q in range(4):
                    op("pe", lambda e, q=q: e.transpose(out=PBb(0)[:, q * 128:(q + 1) * 128], in_=ob_b[:, q * 128:(q + 1) * 128],
                                                        identity=ident_b), reads=["ob_b", "ident_b"], writes=[("pb", 0)])
                op("act", lambda e: e.copy(out=o_bT[:, :, qs], in_=PBb(0)[:, 0:512].rearrange("p (q t) -> p q t", q=4)),
                   reads=[("pb", 0)], writes=[("o_bT", tt)])

            scores_part(0, 0)
            for tt in range(NT):
                if tt + 1 < NT:
                    scores_part(tt + 1, (tt + 1) % 2)
                attn_part(tt, tt % 2)
            if "thr" in dbg:
                op("sp", None, reads=["dbg_thr"])
            if "score" in dbg:
                op("sp", None, reads=["dbg_score"])
            if "o_bT" in dbg:
                op("sp", lambda e: e.dma_start(out=dbg["o_bT"].rearrange("(a p) t -> p a t", p=128), in_=o_bT),
                   reads=[("o_bT", tt) for tt in range(NT)], writes=["dbg_o_bT"], dma=True)
                op("sp", None, reads=["dbg_o_bT"])

            sch.barrier()
            if stop_after == "dsa":
                return

            pa = MultiAlloc([(R_W, ARENA_BYTES)])
            hT2 = pa([128, 8, S], BF16)
            mergedT = pa([128, 8, S], BF16)
            x1 = pa([128, NT, D], F32)
            bgate = pa([128, 16], F32)
            g2col = pa([128, 8], F32)
            fng = pa([128, D], F32)
            wst2 = pa([128, 8, 256], F32)
            wg_bf = [pa([128, 8, 256], BF16) for _ in range(2)]
            wp_bf = [pa([128, 4, 256], BF16) for _ in range(2)]
            ga_s = pa([128, 512], BF16)
            gb_s = pa([128, 512], BF16)
            t1 = pa([128, 512], BF16)
            t2 = pa([128, 512], BF16)
            xt4 = [pa([128, D], F32) for _ in range(2)]
            op("sp", lambda e: e.dma_start(out=bgate, in_=bgate_d), writes=["bgate"], dma=True)
            op("sp", lambda e: e.dma_start(out=g2col, in_=g2_d), writes=["g2col"], dma=True)
            op("sp", lambda e: e.dma_start(out=fng, in_=fng_d.partition_broadcast(128)), writes=["fng"], dma=True)

            def phase1b():
                xa = Alloc(R_W + 64 * K, R_W + 128 * K)
                xt = [xa([128, D], F32) for _ in range(2)]
                hb = [xa([128, D], BF16) for _ in range(2)]
                junk = xa([128, D], BF16)
                ssx = xa([128, NT], F32)
                rsx = xa([128, NT], F32)
                op("dve", lambda e: e.memset(ssx, 0.0), writes=["ssx"])
                for tt in range(NT):
                    b = tt % 2
                    op("sp", lambda e, tt=tt, b=b: e.dma_start(out=xt[b], in_=x_d[tt * 128:(tt + 1) * 128, :]), writes=[("xt", b)], dma=True)
                    op("act", lambda e, tt=tt, b=b: e.activation(out=junk, in_=xt[b], func=AF.Square, accum_out=ssx[:, tt:tt + 1]),
                       reads=[("xt", b), "ssx"], writes=["junk", ("ssx", tt)])
                    op("act", lambda e, tt=tt: e.activation(out=rsx[:, tt:tt + 1], in_=ssx[:, tt:tt + 1], func=AF.Sqrt, bias=epsc,
                                                            scale=1.0 / D), reads=[("ssx", tt), "epsc"], writes=[("rsx", tt)])
                    op("dve", lambda e, tt=tt: e.reciprocal(out=rsx[:, tt:tt + 1], in_=rsx[:, tt:tt + 1]), reads=[("rsx", tt)],
                       writes=[("rsx", tt)])
                    op("dve", lambda e, tt=tt, b=b: e.tensor_scalar(out=hb[b], in0=xt[b], scalar1=rsx[:, tt:tt + 1], scalar2=None,
                                                                    op0=ALU.mult), reads=[("xt", b), ("rsx", tt)], writes=[("hb", b)])
                    bk = tt % 2
                    for k in range(8):
                        op("pe", lambda e, k=k, b=b, bk=bk: e.transpose(out=PBb(bk)[:, k * 128:(k + 1) * 128],
                                                                        in_=hb[b][:, k * 128:(k + 1) * 128], identity=ident_b),
                           reads=[("hb", b), "ident_b"], writes=[("pb", bk)])
                    op("act", lambda e, tt=tt, bk=bk: e.copy(out=hT2[:, :, tt * 128:(tt + 1) * 128],
                                                             in_=PBb(bk).rearrange("p (k t) -> p k t", k=8)),
                       reads=[("pb", bk)], writes=[("hT2", tt)])
            phase1b()
            sch.barrier()
            HT2 = [("hT2", tt) for tt in range(NT)]

            wpa_v = wpa_d.rearrange("(k p) c -> p k c", p=128)
            wpb_v = wpb_d.rearrange("(k p) c -> p k c", p=128)
            wout_v = wout_d.rearrange("(k p) c -> p k c", p=128)
            wi4 = [0]

            wst2b = xt4[0].rearrange("p (k c) -> p k c", k=4)
            wst2c = xt4[1].rearrange("p (k c) -> p k c", k=4)
            wi5 = [0]

            def load_gate(c0):
                i = wi4[0]
                wi4[0] += 1
                b = i % 2
                op("sp", lambda e: e.dma_start(out=wst2, in_=w_in_v[:, :, c0:c0 + 256]), writes=["wst2"], dma=True)
                op("pool", lambda e, b=b: e.tensor_tensor(out=wg_bf[b], in0=wst2, in1=g1col.unsqueeze(2).to_broadcast([128, 8, 256]),
                                                          op=ALU.mult), reads=["wst2", "g1col"], writes=[("wg", b)])
                return wg_bf[b], ("wg", b)

            def load_proj(src_v, c0):
                i = wi5[0]
                wi5[0] += 1
                b = i % 2
                st = wst2b if b == 0 else wst2c
                op("sp", lambda e: e.dma_start(out=st, in_=src_v[:, :, c0:c0 + 256]), writes=[("wstp", b)], dma=True)
                op("pool", lambda e, b=b: e.tensor_copy(out=wp_bf[b], in_=st), reads=[("wstp", b)], writes=[("wp", b)])
                return wp_bf[b], ("wp", b)

            p4u = [0]
            for j in range(4):
                wga, kga = load_gate(C_GA + j * 256)
                wgb, kgb = load_gate(C_GB + j * 256)
                wpa, kpa = load_proj(wpa_v, j * 256)
                wpb, kpb = load_proj(wpb_v, j * 256)
                for ct in range(2):
                    c = 2 * j + ct
                    ccs = slice(ct * 128, (ct + 1) * 128)
                    for tb in range(4):
                        tcs = slice(tb * 512, (tb + 1) * 512)
                        hk = HT2[tb * 4:tb * 4 + 4]
                        bA, bB, bC, bD = (2, 3, 4, 5) if (p4u[0] % 2 == 0) else (0, 1, 6, 7)
                        p4u[0] += 1
                        for k in range(8):
                            op("pe", lambda e, k=k, ccs=ccs, tcs=tcs, wga=wga, bA=bA: e.matmul(PB(bA), lhsT=wga[:, k, ccs], rhs=hT2[:, k, tcs],
                                                                                         start=(k == 0), stop=(k == 7)),
                               reads=[kga] + hk, writes=[("pb", bA)])
                        op("act", lambda e, c=c, bA=bA: e.activation(out=ga_s, in_=PB(bA), func=AF.Sigmoid, bias=bgate[:, c:c + 1], scale=1.0),
                           reads=[("pb", bA), "bgate"], writes=["ga_s"])
                        for k in range(8):
                            op("pe", lambda e, k=k, ccs=ccs, tcs=tcs, wgb=wgb, bB=bB: e.matmul(PB(bB), lhsT=wgb[:, k, ccs], rhs=hT2[:, k, tcs],
                                                                                         start=(k == 0), stop=(k == 7)),
                               reads=[kgb] + hk, writes=[("pb", bB)])
                        op("act", lambda e, c=c, bB=bB: e.activation(out=gb_s, in_=PB(bB), func=AF.Sigmoid, bias=bgate[:, 8 + c:9 + c], scale=1.0),
                           reads=[("pb", bB), "bgate"], writes=["gb_s"])
                        for hp in range(4):
                            op("pe", lambda e, hp=hp, ccs=ccs, tcs=tcs, wpa=wpa, bC=bC: e.matmul(PB(bC), lhsT=wpa[:, hp, ccs], rhs=o_aT[:, hp, tcs],
                                                                                           start=(hp == 0), stop=(hp == 3)),
                               reads=[kpa, "o_aT"], writes=[("pb", bC)])
                        for hp in range(4):
                            op("pe", lambda e, hp=hp, ccs=ccs, tcs=tcs, wpb=wpb, bD=bD: e.matmul(PB(bD), lhsT=wpb[:, hp, ccs], rhs=o_bT[:, hp, tcs],
                                                                                           start=(hp == 0), stop=(hp == 3)),
                               reads=[kpb, "o_bT"], writes=[("pb", bD)])
                        op("dve", lambda e, bC=bC: e.tensor_tensor(out=t1, in0=PB(bC), in1=ga_s, op=ALU.mult), reads=[("pb", bC), "ga_s"],
                           writes=["t1"])
                        op("dve", lambda e, bD=bD: e.tensor_tensor(out=t2, in0=PB(bD), in1=gb_s, op=ALU.mult), reads=[("pb", bD), "gb_s"],
                           writes=["t2"])
                        op("pool", lambda e, c=c, tcs=tcs: e.tensor_tensor(out=mergedT[:, c, tcs], in0=t1, in1=t2, op=ALU.add),
                           reads=["t1", "t2"], writes=[("mergedT", c, tb)])
            if "mergedT" in dbg:
                op("sp", lambda e: e.dma_start(out=dbg["mergedT"].rearrange("(a p) t -> p a t", p=128), in_=mergedT),
                   reads=[("mergedT", c, tb) for c in range(8) for tb in range(4)], writes=["dbg_mergedT"], dma=True)
                op("sp", None, reads=["dbg_mergedT"])
            sch.barrier()
            wout_bf = view(R_A, [128, 8, D], BF16)
            for j in range(4):
                op("sp", lambda e, j=j: e.dma_start(out=wst2, in_=wout_v[:, :, j * 256:(j + 1) * 256]), writes=["wst2"], dma=True)
                op("pool", lambda e, j=j: e.tensor_copy(out=wout_bf[:, :, j * 256:(j + 1) * 256], in_=wst2), reads=["wst2"],
                   writes=[("wout", j)])
            WOUT = [("wout", j) for j in range(4)]
            for tt in range(NT):
                b = tt % 2
                op("sp", lambda e, tt=tt, b=b: e.dma_start(out=xt4[b], in_=x_d[tt * 128:(tt + 1) * 128, :]), writes=[("xt4", b)], dma=True)
                for nb in range(2):
                    bk = 2 + 2 * b + nb
                    for c in range(8):
                        op("pe", lambda e, c=c, tt=tt, nb=nb, bk=bk: e.matmul(PB(bk), lhsT=mergedT[:, c, tt * 128:(tt + 1) * 128],
                                                                               rhs=wout_bf[:, c, nb * 512:(nb + 1) * 512],
                                                                               start=(c == 0), stop=(c == 7)),
                           reads=WOUT + ["mergedT_all"], writes=[("pb", bk)])
                    op("dve", lambda e, tt=tt, nb=nb, bk=bk, b=b: e.tensor_tensor(out=x1[:, tt, nb * 512:(nb + 1) * 512], in0=PB(bk),
                                                                                   in1=xt4[b][:, nb * 512:(nb + 1) * 512], op=ALU.add),
                       reads=[("pb", bk), ("xt4", b)], writes=[("x1", tt)])
            if "x1" in dbg:
                op("sp", lambda e: e.dma_start(out=dbg["x1"].rearrange("(t p) c -> p t c", p=128), in_=x1),
                   reads=[("x1", tt) for tt in range(NT)], writes=["dbg_x1"], dma=True)
                op("sp", None, reads=["dbg_x1"])
            sch.barrier()
            if stop_after == "p4":
                return

            h2T = view(R_W, [128, 8, S], BF16)
            ma = MultiAlloc([(R_W + 32 * K, R_W + 64 * K), (R_A, R_A + 32 * K)])
            tail_off = None
            hb2 = [ma([128, D], BF16) for _ in range(2)]
            junk2 = ma([128, D], BF16)
            ss5 = ma([128, NT], F32)
            rs5 = ma([128, NT], F32)
            wr_st = ma([128, 8, 20], F32)
            wr_bf = ma([128, 8, 20], BF16)
            brow = ma([128, 20], F32)
            lg = ma([128, 20], F32)
            sm = {n_: ma([128, 4], F32) for n_ in ("goh", "gex", "elg", "oh1", "msk", "oh2", "wsel")}
            sc1 = {n_: ma([128, 1], F32) for n_ in ("gmax", "ngmax", "gsum", "ggate", "m1", "m2", "d21", "e21", "den", "w1", "w2")}
            tmp44 = ma([128, 4, 4], F32)
            comb_b = ma([128, 16], BF16)
            combT = ma([128, S], BF16)
            sel16 = ma([128, 16, 128], BF16)
            est = ma([128, 8, 256], F32)
            w1b = [ma([128, 8, 256], BF16) for _ in range(2)]
            w3b = [ma([128, 8, 256], BF16) for _ in range(2)]
            w2b = [ma([128, 2, D], BF16) for _ in range(2)]
            sg = [[ma([128, 512], BF16) for _ in range(2)] for _ in range(2)]
            cbt = [ma([128, 512], BF16) for _ in range(2)]
            tu = [[ma([128, 512], BF16) for _ in range(2)] for _ in range(2)]
            actT = [[ma([128, 512], BF16) for _ in range(2)] for _ in range(2)]
            op("sp", lambda e: e.dma_start(out=wr_st, in_=wr_d.rearrange("(k p) c -> p k c", p=128)), writes=["wr_st"], dma=True)
            op("sp", lambda e: e.dma_start(out=brow, in_=br_d.partition_broadcast(128)), writes=["brow"], dma=True)
            op("pool", lambda e: e.tensor_tensor(out=wr_bf, in0=wr_st, in1=g2col.unsqueeze(2).to_broadcast([128, 8, 20]), op=ALU.mult),
               reads=["wr_st", "g2col"], writes=["wr_bf"])
            op("pool", lambda e: e.memset(sel16[0:16, :, :], 1.0), writes=["sel16"])
            op("pool", lambda e: e.affine_select(out=sel16[0:16, :, :], in_=sel16[0:16, :, :], pattern=[[-1, 16], [0, 128]],
                                                 compare_op=ALU.is_equal, fill=0.0, base=0, channel_multiplier=1), writes=["sel16"])
            op("dve", lambda e: e.memset(ss5, 0.0), writes=["ss5"])
            def prep_tile(tt):
                b = tt % 2
                bk = tt % 2
                op("act", lambda e, tt=tt: e.activation(out=junk2, in_=x1[:, tt, :], func=AF.Square, accum_out=ss5[:, tt:tt + 1]),
                   reads=["x1_all", "ss5"], writes=["junk2", ("ss5", tt)])
                op("act", lambda e, tt=tt: e.activation(out=rs5[:, tt:tt + 1], in_=ss5[:, tt:tt + 1], func=AF.Sqrt, bias=epsc,
                                                        scale=1.0 / D), reads=[("ss5", tt), "epsc"], writes=[("rs5", tt)])
                op("dve", lambda e, tt=tt: e.reciprocal(out=rs5[:, tt:tt + 1], in_=rs5[:, tt:tt + 1]), reads=[("rs5", tt)],
                   writes=[("rs5", tt)])
                op("dve", lambda e, tt=tt, b=b: e.tensor_scalar(out=hb2[b], in0=x1[:, tt, :], scalar1=rs5[:, tt:tt + 1], scalar2=None,
                                                                op0=ALU.mult), reads=["x1_all", ("rs5", tt)], writes=[("hb2", b)])
                for k in range(8):
                    op("pe", lambda e, k=k, b=b, bk=bk: e.transpose(out=PBb(bk)[:, k * 128:(k + 1) * 128],
                                                                    in_=hb2[b][:, k * 128:(k + 1) * 128], identity=ident_b),
                       reads=[("hb2", b), "ident_b"], writes=[("pb", bk)])
                op("act", lambda e, tt=tt, bk=bk: e.copy(out=h2T[:, :, tt * 128:(tt + 1) * 128],
                                                         in_=PBb(bk).rearrange("p (k t) -> p k t", k=8)),
                   reads=[("pb", bk)], writes=[("h2T", tt)])
                for k in range(8):
                    op("pe", lambda e, k=k, tt=tt: e.matmul(PB(2)[:, 0:20], lhsT=h2T[:, k, tt * 128:(tt + 1) * 128], rhs=wr_bf[:, k, :],
                                                            start=(k == 0), stop=(k == 7)),
                       reads=[("h2T", tt), "wr_bf"], writes=[("pb", 2)])
                R = []

                def rop(fn, rd, wr):
                    op("dve", fn, reads=rd, writes=wr)
                rop(lambda e: e.tensor_tensor(out=lg, in0=PB(2)[:, 0:20], in1=brow, op=ALU.add), [("pb", 2), "brow"], ["lg"])
                elv = lg[:, 4:20].rearrange("p (g x) -> p g x", g=4)
                rop(lambda e: e.tensor_reduce(out=sc1["gmax"], in_=lg[:, 0:4], axis=AX.X, op=ALU.max), ["lg"], ["gmax"])
                rop(lambda e: e.tensor_scalar(out=sm["goh"], in0=lg[:, 0:4], scalar1=sc1["gmax"][:, 0:1], scalar2=None,
                                              op0=ALU.is_equal), ["lg", "gmax"], ["goh"])
                rop(lambda e: e.tensor_scalar(out=sc1["ngmax"], in0=sc1["gmax"], scalar1=-1.0, scalar2=None, op0=ALU.mult),
                    ["gmax"], ["ngmax"])
                op("act", lambda e: e.activation(out=sm["gex"], in_=lg[:, 0:4], func=AF.Exp, bias=sc1["ngmax"][:, 0:1], scale=1.0),
                   reads=["lg", "ngmax"], writes=["gex"])
                rop(lambda e: e.tensor_reduce(out=sc1["gsum"], in_=sm["gex"], axis=AX.X, op=ALU.add), ["gex"], ["gsum"])
                rop(lambda e: e.reciprocal(out=sc1["ggate"], in_=sc1["gsum"]), ["gsum"], ["ggate"])
                rop(lambda e: e.tensor_tensor(out=tmp44, in0=elv, in1=sm["goh"].unsqueeze(2).to_broadcast([128, 4, 4]), op=ALU.mult),
                    ["lg", "goh"], ["tmp44"])
                rop(lambda e: e.tensor_reduce(out=sm["elg"], in_=tmp44.rearrange("p g x -> p x g"), axis=AX.X, op=ALU.add),
                    ["tmp44"], ["elg"])
                rop(lambda e: e.tensor_reduce(out=sc1["m1"], in_=sm["elg"], axis=AX.X, op=ALU.max), ["elg"], ["m1"])
                rop(lambda e: e.tensor_scalar(out=sm["oh1"], in0=sm["elg"], scalar1=sc1["m1"][:, 0:1], scalar2=None, op0=ALU.is_equal),
                    ["elg", "m1"], ["oh1"])
                rop(lambda e: e.scalar_tensor_tensor(out=sm["msk"], in0=sm["oh1"], scalar=-1.0e30, in1=sm["elg"], op0=ALU.mult,
                                                     op1=ALU.add), ["oh1", "elg"], ["msk"])
                rop(lambda e: e.tensor_reduce(out=sc1["m2"], in_=sm["msk"], axis=AX.X, op=ALU.max), ["msk"], ["m2"])
                rop(lambda e: e.tensor_scalar(out=sm["oh2"], in0=sm["msk"], scalar1=sc1["m2"][:, 0:1], scalar2=None, op0=ALU.is_equal),
                    ["msk", "m2"], ["oh2"])
                rop(lambda e: e.tensor_tensor(out=sc1["d21"], in0=sc1["m2"], in1=sc1["m1"], op=ALU.subtract), ["m1", "m2"], ["d21"])
                op("act", lambda e: e.activation(out=sc1["e21"], in_=sc1["d21"], func=AF.Exp), reads=["d21"], writes=["e21"])
                rop(lambda e: e.tensor_scalar(out=sc1["den"], in0=sc1["e21"], scalar1=1.0, scalar2=None, op0=ALU.add), ["e21"], ["den"])
                rop(lambda e: e.reciprocal(out=sc1["den"], in_=sc1["den"]), ["den"], ["den"])
                rop(lambda e: e.tensor_tensor(out=sc1["w1"], in0=sc1["ggate"], in1=sc1["den"], op=ALU.mult), ["ggate", "den"], ["w1"])
                rop(lambda e: e.tensor_tensor(out=sc1["w2"], in0=sc1["w1"], in1=sc1["e21"], op=ALU.mult), ["w1", "e21"], ["w2"])
                rop(lambda e: e.tensor_scalar(out=sm["wsel"], in0=sm["oh1"], scalar1=sc1["w1"][:, 0:1], scalar2=None, op0=ALU.mult),
                    ["oh1", "w1"], ["wsel"])
                rop(lambda e: e.scalar_tensor_tensor(out=sm["wsel"], in0=sm["oh2"], scalar=sc1["w2"][:, 0:1], in1=sm["wsel"],
                                                     op0=ALU.mult, op1=ALU.add), ["oh2", "w2", "wsel"], ["wsel"])
                rop(lambda e: e.tensor_tensor(out=comb_b.rearrange("p (g x) -> p g x", g=4),
                                              in0=sm["goh"].unsqueeze(2).to_broadcast([128, 4, 4]),
                                              in1=sm["wsel"].unsqueeze(1).to_broadcast([128, 4, 4]), op=ALU.mult),
                    ["goh", "wsel"], ["comb_b"])
                if "comb" in dbg:
                    op("sp", lambda e, tt=tt: e.dma_start(out=dbg["comb"][tt * 128:(tt + 1) * 128, :], in_=comb_b), reads=["comb_b"],
                       writes=["dbg_comb"], dma=True)
                op("pe", lambda e: e.transpose(out=PBb(3)[0:16, 0:128], in_=comb_b, identity=ident_b), reads=["comb_b", "ident_b"],
                   writes=[("pb", 3)])
                op("act", lambda e, tt=tt: e.copy(out=combT[0:16, tt * 128:(tt + 1) * 128], in_=PBb(3)[0:16, 0:128]),
                   reads=[("pb", 3)], writes=[("combT", tt)])
            for tt in range(4):
                prep_tile(tt)
            H2T = [("h2T", tt) for tt in range(NT)]
            CT = [("combT", tt) for tt in range(NT)]

            def load_expert(e_i):
                b = e_i % 2
                for (src, dst, nm, fold) in ((w1_d, w1b[b], "w1", True), (w3_d, w3b[b], "w3", True)):
                    op("sp", lambda e, src=src: e.dma_start(out=est, in_=src[e_i].rearrange("(k p) f -> p k f", p=128)),
                       writes=["est"], dma=True)
                    op("pool", lambda e, dst=dst: e.tensor_tensor(out=dst, in0=est, in1=g2col.unsqueeze(2).to_broadcast([128, 8, 256]),
                                                                  op=ALU.mult), reads=["est", "g2col"], writes=[(nm, b)])
                op("sp", lambda e: e.dma_start(out=est.rearrange("p k f -> p (k f)").rearrange("p (a c) -> p a c", a=2),
                                               in_=w2_d[e_i].rearrange("(a p) c -> p a c", p=128)), writes=["est"], dma=True)
                op("pool", lambda e: e.tensor_copy(out=w2b[b], in_=est.rearrange("p k f -> p (k f)").rearrange("p (a c) -> p a c", a=2)),
                   reads=["est"], writes=[("w2", b)])

            def stageA(e_i, tb, sl):
                b = e_i % 2
                tcs = slice(tb * 512, (tb + 1) * 512)
                hk = H2T[tb * 4:tb * 4 + 4]
                cbk_ = 6
                op("pe", lambda e: e.matmul(PB(cbk_), lhsT=sel16[0:16, e_i, :], rhs=combT[0:16, tcs], start=True, stop=True),
                   reads=["sel16"] + CT[tb * 4:tb * 4 + 4], writes=[("pb", cbk_)])
                op("act", lambda e: e.copy(out=cbt[sl], in_=PB(cbk_)), reads=[("pb", cbk_)], writes=[("cbt", sl)])
                for ft in range(2):
                    fcs = slice(ft * 128, (ft + 1) * 128)
                    for k in range(8):
                        op("pe", lambda e, k=k, fcs=fcs, ft=ft: e.matmul(PB(2 + ft), lhsT=w1b[b][:, k, fcs], rhs=h2T[:, k, tcs],
                                                                         start=(k == 0), stop=(k == 7)),
                           reads=[("w1", b)] + hk, writes=[("pb", 2 + ft)])
                    op("act", lambda e, ft=ft: e.activation(out=sg[sl][ft], in_=PB(2 + ft), func=AF.Silu), reads=[("pb", 2 + ft)],
                       writes=[("sg", sl, ft)])
                    yield
                    for k in range(8):
                        op("pe", lambda e, k=k, fcs=fcs, ft=ft: e.matmul(PB(4 + ft), lhsT=w3b[b][:, k, fcs], rhs=h2T[:, k, tcs],
                                                                         start=(k == 0), stop=(k == 7)),
                           reads=[("w3", b)] + hk, writes=[("pb", 4 + ft)])
                    op("dve", lambda e, ft=ft: e.tensor_tensor(out=tu[sl][ft], in0=PB(4 + ft), in1=sg[sl][ft], op=ALU.mult),
                       reads=[("pb", 4 + ft), ("sg", sl, ft)], writes=[("tu", sl, ft)], fast=True)
                    op("pool", lambda e, ft=ft: e.tensor_tensor(out=actT[sl][ft], in0=tu[sl][ft], in1=cbt[sl], op=ALU.mult),
                       reads=[("tu", sl, ft), ("cbt", sl)], writes=[("actT", sl, ft)])
                    yield

            ybank = [0]

            def stageB(e_i, tb, sl):
                b = e_i % 2
                for t4 in range(4):
                    tt = tb * 4 + t4
                    for nb in range(2):
                        bk = (0, 1, 7)[ybank[0] % 3]
                        ybank[0] += 1
                        for ft in range(2):
                            op("pe", lambda e, ft=ft, t4=t4, nb=nb, bk=bk: e.matmul(
                                PB(bk), lhsT=actT[sl][ft][:, t4 * 128:(t4 + 1) * 128], rhs=w2b[b][:, ft, nb * 512:(nb + 1) * 512],
                                start=(ft == 0), stop=(ft == 1)), reads=[("actT", sl, ft), ("w2", b)], writes=[("pb", bk)])
                        op("dve", lambda e, tt=tt, nb=nb, bk=bk: e.tensor_tensor(out=x1[:, tt, nb * 512:(nb + 1) * 512],
                                                                                 in0=PB(bk), in1=x1[:, tt, nb * 512:(nb + 1) * 512],
                                                                                 op=ALU.add),
                           reads=[("pb", bk), ("x2", tt, nb)], writes=[("x2", tt, nb)], fast=True)
                        if nb == 1:
                            yield

            def drain(g_):
                for _ in g_:
                    pass

            units = [(e_i, tb) for e_i in range(16) for tb in range(4)]
            load_expert(0)
            load_expert(1)
            drain(stageA(units[0][0], units[0][1], 0))
            for u, (e_i, tb) in enumerate(units):
                gb = stageB(e_i, tb, u % 2)
                if u + 1 < len(units):
                    ne, ntb = units[u + 1]
                    if ne == 0:
                        for tt in range(4 * ntb, 4 * ntb + 4):
                            prep_tile(tt)
                    ga_ = stageA(ne, ntb, (u + 1) % 2)
                    drain(ga_)
                drain(gb)
                if tb == 3 and e_i + 2 < 16:
                    load_expert(e_i + 2)
            sch.barrier()
            if "x2" in dbg:
                op("sp", lambda e: e.dma_start(out=dbg["x2"].rearrange("(t p) c -> p t c", p=128), in_=x1), writes=["dbg_x2"], dma=True)
                op("sp", None, reads=["dbg_x2"])

            fa = MultiAlloc([(R_W, R_W + 64 * K)])
            ss6 = fa([128, NT], F32)
            rs6 = fa([128, NT], F32)
            junk6 = fa([128, D], BF16)
            yo = [fa([128, D], F32) for _ in range(2)]
            op("dve", lambda e: e.memset(ss6, 0.0), writes=["ss6"])
            for tt in range(NT):
                b = tt % 2
                op("act", lambda e, tt=tt: e.activation(out=junk6, in_=x1[:, tt, :], func=AF.Square, accum_out=ss6[:, tt:tt + 1]),
                   reads=["ss6"], writes=["junk6", ("ss6", tt)])
                op("act", lambda e, tt=tt: e.activation(out=rs6[:, tt:tt + 1], in_=ss6[:, tt:tt + 1], func=AF.Sqrt, bias=epsc,
                                                        scale=1.0 / D), reads=[("ss6", tt), "epsc"], writes=[("rs6", tt)])
                op("dve", lambda e, tt=tt: e.reciprocal(out=rs6[:, tt:tt + 1], in_=rs6[:, tt:tt + 1]), reads=[("rs6", tt)],
                   writes=[("rs6", tt)])
                op("dve", lambda e, tt=tt, b=b: e.scalar_tensor_tensor(out=yo[b], in0=x1[:, tt, :], scalar=rs6[:, tt:tt + 1], in1=fng,
                                                                       op0=ALU.mult, op1=ALU.mult),
                   reads=[("rs6", tt), "fng"], writes=[("yo", b)])
                op("sp", lambda e, tt=tt, b=b: e.dma_start(out=out_d[tt * 128:(tt + 1) * 128, :], in_=yo[b]), reads=[("yo", b)],
                   writes=[("out", tt)], dma=True)
            op("sp", None, reads=[("out", tt) for tt in range(NT)])


        body()
        sch.barrier()
        DEBUG["stats_pre"] = {e: len(sch.ops[e]) for e in Sched.ENGS}
        with nc.Block() as block:
            sch.emit(nc, block, engsem, dmasem)
        DEBUG["stats"] = sch.stats
    return nc


_NC_CACHE = {}


def kernel(**inputs):
    dbg = tuple(DEBUG.get("outputs", ()))
    key = (dbg, DEBUG.get("stop_after"))
    if key not in _NC_CACHE:
        _NC_CACHE[key] = build_nc(dbg, DEBUG.get("stop_after"))
    nc = _NC_CACHE[key]
    n = 8
    x = np.ascontiguousarray(inputs["x"], dtype=np.float32)
    posn = np.ascontiguousarray(inputs["positions"], dtype=np.int32)
    f32 = lambda a: np.ascontiguousarray(a, dtype=np.float32)
    inv = (10000.0 ** (-np.arange(32, dtype=np.float32) / np.float32(32))).astype(np.float32).reshape(1, 32)
    shared = {
        "norm1_g": f32(inputs["norm1_g"][0].reshape(8, 128).T),
        "w_in": f32(inputs["w_in"][0]),
        "conv_w": f32(inputs["conv_w"][0].reshape(4, 12, 128).transpose(2, 1, 0).reshape(128, 48)),
        "inv_freq": inv,
        "a_log": f32(inputs["a_log"][0].reshape(1, 8)),
        "dt_bias": f32(inputs["dt_bias"][0].reshape(1, 8)),
        "a_norm_g": f32(inputs["a_norm_g"][0].reshape(1, 64)),
        "b_gate": f32(inputs["b_gate"][0].reshape(16, 128).T),
        "norm2_g": f32(inputs["norm2_g"][0].reshape(8, 128).T),
        "final_norm_g": f32(inputs["final_norm_g"].reshape(1, D)),
        "w_proj_a": f32(inputs["w_proj_a"][0]),
        "w_proj_b": f32(inputs["w_proj_b"][0]),
        "w_out": f32(inputs["w_out"][0]),
        "w_router": f32(np.concatenate([inputs["w_router_group"][0], inputs["w_router_expert"][0]], axis=1)),
        "b_router": f32(np.concatenate([inputs["b_router_group"][0], inputs["b_router_expert"][0]], axis=0).reshape(1, 20)),
        "w_exp_gate": f32(inputs["w_exp_gate"][0]),
        "w_exp_up": f32(inputs["w_exp_up"][0]),
        "w_exp_down": f32(inputs["w_exp_down"][0]),
    }
    in_maps = []
    for c in range(n):
        m = dict(shared)
        m["x"] = x[c]
        m["positions"] = np.ascontiguousarray(posn[c].reshape(NT, 128).T)
        in_maps.append(m)
    res = run_bass_kernel_spmd(nc, in_maps, core_ids=list(range(n)))
    DEBUG["results"] = res.results
    return np.stack([r["out"] for r in res.results], axis=0)
```

```python
import math
from contextlib import ExitStack
import numpy as np
import concourse.bass as bass
import concourse.mybir as mybir
from concourse.bass_utils import run_bass_kernel_spmd

F32 = mybir.dt.float32
BF16 = mybir.dt.bfloat16
I32 = mybir.dt.int32
AF = mybir.ActivationFunctionType
ALU = mybir.AluOpType
AX = mybir.AxisListType

S = 2048
D = 1024
NT = S // 128
D_IN = 5464
EPS = 1e-6
N_DMA_SEMS = 24
NEG = -30000.0
NBIS = 12
TWO_PI = 2.0 * math.pi

C_AQ, C_AK, C_AV, C_AZ = 0, 512, 1024, 1536
C_BETA, C_ALPHA = 2048, 2056
C_BQ, C_BK, C_BV = 2064, 2576, 2704
C_IQ, C_IK, C_IW = 2832, 3344, 3408
C_GA, C_GB = 3416, 4440

DEBUG = {}
STRICT_SAME_ENGINE = True


class Sched:
    ENGS = ("pe", "act", "dve", "pool", "sp")

    def __init__(self):
        self.ops = {e: [] for e in self.ENGS}
        self.last_w = {}
        self.readers = {}
        self.dma_rr = 0
        self.dma_count = [0] * N_DMA_SEMS

    def op(self, eng, fn, reads=(), writes=(), dma=False, fast=False):
        deps = set()
        raw = set()
        for k in reads:
            t = self.last_w.get(k)
            if t is not None:
                deps.add(t)
                raw.add(t)
        for k in writes:
            t = self.last_w.get(k)
            if t is not None:
                deps.add(t)
            for t in self.readers.get(k, {}).values():
                deps.add(t)
        idx = len(self.ops[eng])
        if dma:
            si = self.dma_rr
            self.dma_rr = (self.dma_rr + 1) % N_DMA_SEMS
            prev = self.dma_count[si]
            if prev > 0:
                deps.add(("dma", si, prev))
            self.dma_count[si] = prev + 1
            tok = ("dma", si, prev + 1)
            rkey = ("dma", si)
        else:
            tok = ("eng", eng, idx)
            rkey = eng
            if STRICT_SAME_ENGINE:
                deps = {t for t in deps if not (t[0] == "eng" and t[1] == eng) or eng != "pe"}
            else:
                deps = {t for t in deps if not (t[0] == "eng" and t[1] == eng)
                        or (t in raw and eng != "pe" and not fast and idx - t[2] <= 8)}
        self.ops[eng].append(dict(fn=fn, deps=deps, signal=False, dma=(tok if dma else None)))
        for k in writes:
            self.last_w[k] = tok
            self.readers[k] = {}
        for k in reads:
            if k in writes:
                continue
            self.readers.setdefault(k, {})[rkey] = tok
        return tok

    def barrier(self):
        toks = set()
        for e in self.ENGS:
            j = len(self.ops[e]) - 1
            while j >= 0 and (self.ops[e][j]["fn"] is None or self.ops[e][j]["dma"] is not None):
                j -= 1
            if j >= 0:
                toks.add(("eng", e, j))
        for si in range(N_DMA_SEMS):
            if self.dma_count[si] > 0:
                toks.add(("dma", si, self.dma_count[si]))
        for e in self.ENGS:
            deps = {t for t in toks if not (t[0] == "eng" and t[1] == e and (e == "pe" or not STRICT_SAME_ENGINE))}
            self.ops[e].append(dict(fn=None, deps=deps, signal=False, dma=None))
        self.last_w = {}
        self.readers = {}

    def emit(self, nc, block, engsem, dmasem):
        for e in self.ENGS:
            for o in self.ops[e]:
                for t in o["deps"]:
                    if t[0] == "eng":
                        self.ops[t[1]][t[2]]["signal"] = True
        sigcount = {}
        for e in self.ENGS:
            c = 0
            lst = []
            for o in self.ops[e]:
                if o["signal"]:
                    c += 1
                lst.append(c)
            sigcount[e] = lst
        self.stats = {e: (len(self.ops[e]), sigcount[e][-1] if sigcount[e] else 0) for e in self.ENGS}

        def run(e, eng):
            waited = {}
            for o in self.ops[e]:
                need = {}
                for t in o["deps"]:
                    if t[0] == "eng":
                        key = ("eng", t[1])
                        val = sigcount[t[1]][t[2]]
                    else:
                        key = ("dma", t[1])
                        val = 16 * t[2]
                    if val > need.get(key, 0):
                        need[key] = val
                for key, val in need.items():
                    if waited.get(key, 0) >= val:
                        continue
                    waited[key] = val
                    sem = engsem[key[1]] if key[0] == "eng" else dmasem[key[1]]
                    eng.wait_ge(sem, val)
                if o["fn"] is None:
                    continue
                inst = o["fn"](eng)
                if o["dma"] is not None:
                    inst.then_inc(dmasem[o["dma"][1]], 16)
                elif o["signal"]:
                    inst.then_inc(engsem[e], 1)

        @block.tensor
        def _(eng):
            run("pe", eng)

        @block.scalar
        def _(eng):
            run("act", eng)

        @block.vector
        def _(eng):
            run("dve", eng)

        @block.gpsimd
        def _(eng):
            run("pool", eng)

        @block.sync
        def _(eng):
            run("sp", eng)


DT_SIZE = {F32: 4, BF16: 2, I32: 4}


def build_nc(debug=(), stop_after=None):
    nc = bass.Bass("TRN2", target_bir_lowering=False)

    def din(name, shape, dt=F32):
        return nc.dram_tensor(name, list(shape), dt, kind="ExternalInput").ap()

    x_d = din("x", [S, D])
    pos_d = din("positions", [128, NT], I32)
    g1_d = din("norm1_g", [128, 8])
    w_in_d = din("w_in", [D, D_IN])
    convw_d = din("conv_w", [128, 48])
    invf_d = din("inv_freq", [1, 32])
    alog_d = din("a_log", [1, 8])
    dtb_d = din("dt_bias", [1, 8])
    ang_d = din("a_norm_g", [1, 64])
    bgate_d = din("b_gate", [128, 16])
    g2_d = din("norm2_g", [128, 8])
    fng_d = din("final_norm_g", [1, D])
    wpa_d = din("w_proj_a", [512, D])
    wpb_d = din("w_proj_b", [512, D])
    wout_d = din("w_out", [D, D])
    wr_d = din("w_router", [D, 20])
    br_d = din("b_router", [1, 20])
    w1_d = din("w_exp_gate", [16, D, 256])
    w3_d = din("w_exp_up", [16, D, 256])
    w2_d = din("w_exp_down", [16, 256, D])
    out_d = nc.dram_tensor("out", [S, D], F32, kind="ExternalOutput").ap()
    dbg = {}
    for name, shape, dt in debug:
        dbg[name] = nc.dram_tensor("dbg_" + name, list(shape), dt, kind="ExternalOutput").ap()
    w_in_v = w_in_d.rearrange("(k p) c -> p k c", p=128)

    sch = Sched()
    op = sch.op
    es = ExitStack()
    with es:
        ARENA_BYTES = 207 * 1024
        arena = es.enter_context(nc.sbuf_tensor("arena", [128, ARENA_BYTES // 4], F32))

        def view(off, shape, dt):
            n = 1
            for s_ in shape[1:]:
                n *= s_
            size = n * DT_SIZE[dt]
            assert off % 4 == 0 and size % 4 == 0 and off + size <= ARENA_BYTES, (off, size)
            ap = arena[:, off // 4:(off + size) // 4]
            if dt != F32:
                ap = ap.bitcast(dt)
            if len(shape) == 3:
                ap = ap.rearrange("p (a b) -> p a b", a=shape[1])
            elif len(shape) == 4:
                ap = ap.rearrange("p (a b c) -> p a b c", a=shape[1], b=shape[2])
            return ap

        class Alloc:
            def __init__(self, base, limit):
                self.off = base
                self.limit = limit

            def __call__(self, shape, dt):
                n = 1
                for s_ in shape[1:]:
                    n *= s_
                size = (n * DT_SIZE[dt] + 63) // 64 * 64
                self.off = (self.off + 63) // 64 * 64
                v = view(self.off, shape, dt)
                self.off += size
                assert self.off <= self.limit, (self.off, self.limit)
                return v

        pbank = [es.enter_context(nc.psum_tensor("pb%d" % i, [128, 512], F32)) for i in range(8)]
        engsem = {e: es.enter_context(nc.semaphore("sem_" + e)) for e in Sched.ENGS}
        dmasem = [es.enter_context(nc.semaphore("dsem%d" % i)) for i in range(N_DMA_SEMS)]

        def PB(i):
            return pbank[i][:]

        def PBb(i):
            return pbank[i][:].bitcast(BF16)

        def body():
            K = 1024
            ca = Alloc(0, 9 * K)
            ident_f = ca([128, 128], F32)
            ident_b = ca([128, 128], BF16)
            ucs_f = ca([128, 128], F32)
            mc0_f = ca([128, 128], F32)
            mc1_f = ca([128, 128], F32)
            maskneg_f = ca([128, 128], F32)
            strict_b = ca([128, 128], BF16)
            g1col = ca([128, 8], F32)
            epsc = ca([128, 1], F32)
            cw = ca([128, 48], F32)
            invf = ca([128, 32], F32)
            dtb = ca([128, 8], F32)
            negA = ca([128, 8], F32)
            angb = ca([128, 64], F32)
            posi = ca([128, NT], I32)
            posf = ca([128, NT], F32)
            cs = ca([128, NT, 64], F32)
            assert ca.off <= 9 * K, ca.off

            op("pool", lambda e: e.memset(ident_f, 1.0), writes=["ident_f"])
            op("pool", lambda e: e.affine_select(out=ident_f, in_=ident_f, pattern=[[-1, 128]], compare_op=ALU.is_equal,
                                                 fill=0.0, base=0, channel_multiplier=1), writes=["ident_f"])
            op("pool", lambda e: e.tensor_copy(out=ident_b, in_=ident_f), reads=["ident_f"], writes=["ident_b"])
            op("pool", lambda e: e.memset(ucs_f, 1.0), writes=["ucs_f"])
            op("pool", lambda e: e.affine_select(out=ucs_f, in_=ucs_f, pattern=[[1, 128]], compare_op=ALU.is_ge,
                                                 fill=0.0, base=0, channel_multiplier=-1), writes=["ucs_f"])
            op("pool", lambda e: e.memset(ucs_f[0:64, 64:128], 0.0), writes=["ucs_f"])
            op("pool", lambda e: e.memset(mc0_f, 0.0), writes=["mc0_f"])
            op("pool", lambda e: e.memset(mc0_f[0:64, :], 1.0), writes=["mc0_f"])
            op("pool", lambda e: e.memset(mc1_f, 0.0), writes=["mc1_f"])
            op("pool", lambda e: e.memset(mc1_f[64:128, :], 1.0), writes=["mc1_f"])
            op("pool", lambda e: e.memset(maskneg_f, 0.0), writes=["maskneg_f"])
            op("pool", lambda e: e.affine_select(out=maskneg_f, in_=maskneg_f, pattern=[[-1, 128]], compare_op=ALU.is_ge,
                                                 fill=NEG, base=0, channel_multiplier=1), writes=["maskneg_f"])
            op("pool", lambda e: e.memset(maskneg_f[64:128, 0:64], NEG), writes=["maskneg_f"])
            op("pool", lambda e: e.memset(strict_b, 1.0), writes=["strict_b"])
            op("pool", lambda e: e.affine_select(out=strict_b, in_=strict_b, pattern=[[-1, 128]], compare_op=ALU.is_gt,
                                                 fill=0.0, base=0, channel_multiplier=1), writes=["strict_b"])
            op("pool", lambda e: e.memset(strict_b[64:128, 0:64], 0.0), writes=["strict_b"])
            op("dve", lambda e: e.memset(epsc, EPS), writes=["epsc"])
            op("sp", lambda e: e.dma_start(out=g1col, in_=g1_d), writes=["g1col"], dma=True)
            op("sp", lambda e: e.dma_start(out=cw, in_=convw_d), writes=["cw"], dma=True)
            op("sp", lambda e: e.dma_start(out=invf, in_=invf_d.partition_broadcast(128)), writes=["invf"], dma=True)
            op("sp", lambda e: e.dma_start(out=dtb, in_=dtb_d.partition_broadcast(128)), writes=["dtb"], dma=True)
            op("sp", lambda e: e.dma_start(out=negA, in_=alog_d.partition_broadcast(128)), writes=["negA"], dma=True)
            op("sp", lambda e: e.dma_start(out=angb, in_=ang_d.partition_broadcast(128)), writes=["angb"], dma=True)
            op("sp", lambda e: e.dma_start(out=posi, in_=pos_d), writes=["posi"], dma=True)
            op("act", lambda e: e.activation(out=negA, in_=negA, func=AF.Exp), reads=["negA"], writes=["negA"])
            op("dve", lambda e: e.tensor_scalar(out=negA, in0=negA, scalar1=-1.0, scalar2=None, op0=ALU.mult),
               reads=["negA"], writes=["negA"])

            R_A = 9 * K
            R_W = R_A + 32 * K
            R_Z = R_W + 16 * K
            R_Q = R_Z + 50 * K
            R_S = R_Q + 48 * K
            hT = view(R_A, [128, 8, S], BF16)
            wstage = view(R_W, [128, 8, 256], F32)
            wbf = [view(R_W + 8 * K + i * 4 * K, [128, 8, 256], BF16) for i in range(2)]
            zqkvT = view(R_Z, [128, 12, S + 4], BF16)
            bqT = view(R_Z, [128, 4, S], BF16)
            iqT = view(R_Z + 16 * K, [128, 4, S], BF16)
            azs = view(R_Z + 32 * K, [128, NT, 512], BF16)
            qkv_tok = view(R_Q, [128, NT, 1536], BF16)
            sa = Alloc(R_S, ARENA_BYTES)
            kz = [[sa([128, S], BF16) for _ in range(2)] for _ in range(2)]
            ikT2 = sa([128, S], BF16)
            bv_tok = sa([128, NT, 130], BF16)
            ab_tok = sa([128, NT, 16], F32)
            iw_tok = sa([128, NT, 8], F32)
            diagw = sa([128, 48, 128], BF16)
            R_WORK = sa.off

            def rope_tables():
                wa = Alloc(R_Q, R_Q + 48 * K)
                ang = wa([128, NT, 32], F32)
                tmp = wa([128, NT, 32], F32)
                ki = wa([128, NT, 32], I32)
                op("dve", lambda e: e.tensor_copy(out=posf, in_=posi), reads=["posi"], writes=["posf"])
                op("dve", lambda e: e.tensor_tensor(out=ang, in0=posf.unsqueeze(2).to_broadcast([128, NT, 32]),
                                                    in1=invf.unsqueeze(1).to_broadcast([128, NT, 32]), op=ALU.mult),
                   reads=["posf", "invf"], writes=["ang"])
                for which, shift in ((1, 0.0), (0, math.pi / 2.0)):
                    dst = cs[:, :, which * 32:(which + 1) * 32]
                    op("dve", lambda e, shift=shift: e.tensor_scalar(out=tmp, in0=ang, scalar1=shift, scalar2=None, op0=ALU.add),
                       reads=["ang"], writes=["rt_tmp"])
                    op("dve", lambda e: e.tensor_scalar(out=ki, in0=tmp, scalar1=1.0 / TWO_PI, scalar2=None, op0=ALU.mult),
                       reads=["rt_tmp"], writes=["rt_ki"])
                    op("dve", lambda e, dst=dst: e.tensor_copy(out=dst, in_=ki), reads=["rt_ki"], writes=["cs"])
                    op("dve", lambda e, dst=dst: e.scalar_tensor_tensor(out=dst, in0=dst, scalar=-TWO_PI, in1=tmp,
                                                                       op0=ALU.mult, op1=ALU.add),
                       reads=["cs", "rt_tmp"], writes=["cs"])
                    op("dve", lambda e, dst=dst: e.tensor_scalar(out=dst, in0=dst, scalar1=math.pi, scalar2=-math.pi,
                                                                op0=ALU.min, op1=ALU.max), reads=["cs"], writes=["cs"])
                    op("act", lambda e, dst=dst: e.activation(out=dst, in_=dst, func=AF.Sin), reads=["cs"], writes=["cs"])

            rope_tables()

            def phase1(hT_dst, keyp):
                wa = Alloc(R_Q + 16 * K, R_Q + 48 * K)
                xt = [wa([128, D], F32) for _ in range(2)]
                hb = [wa([128, D], BF16) for _ in range(2)]
                junk = wa([128, D], BF16)
                ss1 = wa([128, NT], F32)
                rstd1 = wa([128, NT], F32)
                op("dve", lambda e: e.memset(ss1, 0.0), writes=[keyp + "ss1"])
                for tt in range(NT):
                    b = tt % 2
                    op("sp", lambda e, tt=tt, b=b: e.dma_start(out=xt[b], in_=x_d[tt * 128:(tt + 1) * 128, :]),
                       writes=[(keyp + "xt", b)], dma=True)
                    op("act", lambda e, tt=tt, b=b: e.activation(out=junk, in_=xt[b], func=AF.Square,
                                                                 accum_out=ss1[:, tt:tt + 1]),
                       reads=[(keyp + "xt", b), keyp + "ss1"], writes=[keyp + "junk", (keyp + "ss1", tt)])
                    op("act", lambda e, tt=tt: e.activation(out=rstd1[:, tt:tt + 1], in_=ss1[:, tt:tt + 1], func=AF.Sqrt,
                                                            bias=epsc, scale=1.0 / D),
                       reads=[(keyp + "ss1", tt), "epsc"], writes=[(keyp + "rstd1", tt)])
                    op("dve", lambda e, tt=tt: e.reciprocal(out=rstd1[:, tt:tt + 1], in_=rstd1[:, tt:tt + 1]),
                       reads=[(keyp + "rstd1", tt)], writes=[(keyp + "rstd1", tt)])
                    op("dve", lambda e, tt=tt, b=b: e.tensor_scalar(out=hb[b], in0=xt[b], scalar1=rstd1[:, tt:tt + 1],
                                                                    scalar2=None, op0=ALU.mult),
                       reads=[(keyp + "xt", b), (keyp + "rstd1", tt)], writes=[(keyp + "hb", b)])
                    pbv = PBb(tt % 2)
                    for k in range(8):
                        op("pe", lambda e, k=k, b=b, pbv=pbv: e.transpose(out=pbv[:, k * 128:(k + 1) * 128],
                                                                          in_=hb[b][:, k * 128:(k + 1) * 128], identity=ident_b),
                           reads=[(keyp + "hb", b), "ident_b"], writes=[("pb", tt % 2)])
                    op("act", lambda e, tt=tt, pbv=pbv: e.copy(out=hT_dst[:, :, tt * 128:(tt + 1) * 128],
                                                               in_=pbv.rearrange("p (k t) -> p k t", k=8)),
                       reads=[("pb", tt % 2)], writes=[("hT", tt)])

            phase1(hT, "p1")
            if stop_after == "p1":
                sch.barrier()
                return
            ALL_HT = [("hT", tt) for tt in range(NT)]

            wchunk_i = [0]

            def load_w(ranges):
                i = wchunk_i[0]
                wchunk_i[0] += 1
                b = i % 2
                off = 0
                for (c0, w) in ranges:
                    op("sp", lambda e, c0=c0, w=w, off=off: e.dma_start(out=wstage[:, :, off:off + w],
                                                                        in_=w_in_v[:, :, c0:c0 + w]),
                       writes=["wstage"], dma=True)
                    off += w
                tot = off
                op("pool", lambda e, b=b, tot=tot: e.tensor_tensor(out=wbf[b][:, :, 0:tot], in0=wstage[:, :, 0:tot],
                                                                   in1=g1col.unsqueeze(2).to_broadcast([128, 8, tot]),
                                                                   op=ALU.mult),
                   reads=["wstage", "g1col"], writes=[("wbf", b)])
                return wbf[b], ("wbf", b), tot

            for ci in range(48):
                op("pool", lambda e, ci=ci: e.tensor_scalar(out=diagw[:, ci, :], in0=ident_f, scalar1=cw[:, ci:ci + 1],
                                                            scalar2=None, op0=ALU.mult),
                   reads=["ident_f", "cw"], writes=[("diagw", ci)])
            op("pool", lambda e: e.memset(zqkvT[:, :, 0:4], 0.0), writes=["zpad"])

            cva = Alloc(R_WORK, ARENA_BYTES)
            convtmp = [cva([128, 512], BF16) for _ in range(2)]
            evq = [0]

            def evac_copy(out, in_, reads, writes):
                evq[0] += 1
                if evq[0] % 2 == 0:
                    op("act", lambda e: e.copy(out=out, in_=in_), reads=reads, writes=writes)
                else:
                    op("dve", lambda e: e.tensor_copy(out=out, in_=in_), reads=reads, writes=writes)

            pbi = [0]

            def g1_proj(c, wt, wkey, ct):
                for tb in range(4):
                    bk = 2 + (pbi[0] % 2)
                    pbi[0] += 1
                    for k in range(8):
                        op("pe", lambda e, k=k, tb=tb, bk=bk: e.matmul(
                            PB(bk), lhsT=wt[:, k, ct * 128:(ct + 1) * 128], rhs=hT[:, k, tb * 512:(tb + 1) * 512],
                            start=(k == 0), stop=(k == 7)),
                           reads=[wkey] + ALL_HT[tb * 4:tb * 4 + 4], writes=[("pb", bk)])
                    evac_copy(zqkvT[:, c, 4 + tb * 512:4 + (tb + 1) * 512], PB(bk), [("pb", bk)], [("zq", c, tb)])

            def g1_conv(c):
                for tb in range(4):
                    bk = 4 + (tb % 2)
                    for j in range(4):
                        op("pe", lambda e, tb=tb, j=j, bk=bk: e.matmul(
                            PB(bk), lhsT=diagw[:, c * 4 + j, :], rhs=zqkvT[:, c, tb * 512 + j + 1:tb * 512 + j + 1 + 512],
                            start=(j == 0), stop=(j == 3)),
                           reads=[("diagw", c * 4 + j), ("zq", c, tb), "zpad"] + ([("zq", c, tb - 1)] if tb > 0 else []),
                           writes=[("pb", bk)])
                    ctb = tb % 2
                    op("act", lambda e, bk=bk, ctb=ctb: e.activation(out=convtmp[ctb], in_=PB(bk), func=AF.Silu),
                       reads=[("pb", bk)], writes=[("convtmp", ctb)])
                    tbk = 6 + (tb % 2)
                    for q in range(4):
                        op("pe", lambda e, q=q, ctb=ctb, tbk=tbk: e.transpose(out=PBb(tbk)[:, q * 128:(q + 1) * 128],
                                                                              in_=convtmp[ctb][:, q * 128:(q + 1) * 128],
                                                                              identity=ident_b),
                           reads=[("convtmp", ctb), "ident_b"], writes=[("pb", tbk)])
                    op("dve", lambda e, tb=tb, tbk=tbk: e.tensor_copy(
                        out=qkv_tok[:, tb * 4:(tb + 1) * 4, c * 128:(c + 1) * 128],
                        in_=PBb(tbk)[:, 0:512].rearrange("p (q t) -> p q t", q=4)),
                       reads=[("pb", tbk)], writes=[("qkv_tok", tb * 4 + q, c) for q in range(4)])

            prev_c = None
            nxt_w = load_w([(0, 256)])
            for chunk in range(6):
                wt, wkey, _ = nxt_w
                for ct in range(2):
                    c = chunk * 2 + ct
                    g1_proj(c, wt, wkey, ct)
                    if ct == 0:
                        nxt_w = load_w([((chunk + 1) * 256, 256)]) if chunk + 1 < 6 else load_w([(C_AZ, 256)])
                    if prev_c is not None:
                        g1_conv(prev_c)
                    prev_c = c
            g1_conv(prev_c)
            pending_w = [nxt_w]

            if "qkv_tok" in dbg:
                op("sp", lambda e: e.dma_start(out=dbg["qkv_tok"].rearrange("(t p) c -> p t c", p=128), in_=qkv_tok),
                   reads=[("qkv_tok", tt, c) for tt in range(NT) for c in range(12)], writes=["dbg_qkv_tok"], dma=True)
                op("sp", None, reads=["dbg_qkv_tok"])
            sch.barrier()
            if stop_after == "g1":
                return

            rwa = Alloc(cva.off, ARENA_BYTES)
            zr = [rwa([128, 256], F32) for _ in range(2)]
            rt = [rwa([128, 4, 32], F32) for _ in range(4)]
            roped = [rwa([128, 256], BF16) for _ in range(2)]
            op("pool", lambda e: e.memset(bv_tok, 1.0), writes=["bv_ones"])
            for a_ in range(2):
                for b_ in range(2):
                    op("pool", lambda e, a_=a_, b_=b_: e.memset(kz[a_][b_], 0.0), writes=["kz0"])

            def rope_ops(src, nh, dst_views, tt, rkey, wkeys, b):
                sv = src.rearrange("p (h d) -> p h d", h=nh)
                x1 = sv[:, :, 0:32]
                x2 = sv[:, :, 32:64]
                cc = cs[:, tt, 0:32].unsqueeze(1).to_broadcast([128, nh, 32])
                sn = cs[:, tt, 32:64].unsqueeze(1).to_broadcast([128, nh, 32])
                t = [r[:, 0:nh, :] for r in rt]
                op("dve", lambda e: e.tensor_tensor(out=t[0], in0=x1, in1=cc, op=ALU.mult), reads=[rkey, "cs"], writes=[("rt", 0)])
                op("pool", lambda e: e.tensor_tensor(out=t[1], in0=x2, in1=sn, op=ALU.mult), reads=[rkey, "cs"], writes=[("rt", 1)])
                op("pool", lambda e: e.tensor_tensor(out=t[2], in0=x2, in1=cc, op=ALU.mult), reads=[rkey, "cs"], writes=[("rt", 2)])
                op("dve", lambda e: e.tensor_tensor(out=t[3], in0=x1, in1=sn, op=ALU.mult), reads=[rkey, "cs"], writes=[("rt", 3)])
                for i, dv in enumerate(dst_views):
                    eng = "dve" if i % 2 == 0 else "pool"
                    op(eng, lambda e, dv=dv: e.tensor_tensor(out=dv[:, :, 0:32], in0=t[0], in1=t[1], op=ALU.subtract),
                       reads=[("rt", 0), ("rt", 1)], writes=wkeys)
                    op(eng, lambda e, dv=dv: e.tensor_tensor(out=dv[:, :, 32:64], in0=t[2], in1=t[3], op=ALU.add),
                       reads=[("rt", 2), ("rt", 3)], writes=wkeys)

            def tok_chunk(ranges, handler, sel=None, next_ranges=None):
                if pending_w[0] is not None:
                    wt, wkey, tot = pending_w[0]
                    pending_w[0] = None
                else:
                    wt, wkey, tot = load_w(ranges)
                lo, hi = (0, tot) if sel is None else sel
                pend = []
                for tt in range(NT):
                    if tt == 6 and next_ranges is not None:
                        pending_w[0] = load_w(next_ranges)
                    bk = 2 + (tt % 2)
                    for k in range(8):
                        op("pe", lambda e, k=k, tt=tt, bk=bk, wt=wt: e.matmul(
                            PB(bk)[:, 0:hi - lo], lhsT=hT[:, k, tt * 128:(tt + 1) * 128], rhs=wt[:, k, lo:hi],
                            start=(k == 0), stop=(k == 7)),
                           reads=[wkey, ("hT", tt)], writes=[("pb", bk)])
                    if tt >= 1:
                        pend.append(handler(tt - 1, 2 + ((tt - 1) % 2)))
                    if len(pend) >= 2:
                        p2 = pend.pop(0)
                        if p2 is not None:
                            p2()
                pend.append(handler(NT - 1, 2 + ((NT - 1) % 2)))
                for p2 in pend:
                    if p2 is not None:
                        p2()

            for j in range(2):
                def h_az(tt, bk, j=j):
                    op("act", lambda e: e.activation(out=azs[:, tt, j * 256:(j + 1) * 256], in_=PB(bk)[:, 0:256], func=AF.Silu),
                       reads=[("pb", bk)], writes=[("azs", tt, j)])
                tok_chunk([(C_AZ + j * 256, 256)], h_az, next_ranges=[(C_AZ + 256, 256)] if j == 0 else [(C_BQ, 256)])

            if stop_after == "u1":
                return
            for (c0, dstT, nm) in ((C_BQ, bqT, "bqT"), (C_IQ, iqT, "iqT")):
                for j in range(2):
                    def h_q(tt, bk, j=j, dstT=dstT, nm=nm):
                        b = tt % 2
                        op("act", lambda e: e.copy(out=zr[b], in_=PB(bk)[:, 0:256]), reads=[("pb", bk)], writes=[("zr", b)])
                        rope_ops(zr[b], 4, [roped[b].rearrange("p (h d) -> p h d", h=4)], tt, ("zr", b), [("roped", b)], b)
                        tbk = 6 + b

                        def part2():
                            for q in range(2):
                                op("pe", lambda e, q=q: e.transpose(out=PBb(tbk)[:, q * 128:(q + 1) * 128],
                                                                    in_=roped[b][:, q * 128:(q + 1) * 128], identity=ident_b),
                                   reads=[("roped", b), "ident_b"], writes=[("pb", tbk)])
                            op("act", lambda e: e.copy(out=dstT[:, 2 * j:2 * j + 2, tt * 128:(tt + 1) * 128],
                                                       in_=PBb(tbk)[:, 0:256].rearrange("p (q t) -> p q t", q=2)),
                               reads=[("pb", tbk)], writes=[(nm, tt, j)])
                        return part2
                    nr = [(c0 + 256, 256)] if j == 0 else ([(C_IQ, 256)] if c0 == C_BQ else [(C_BK, 256)])
                    tok_chunk([(c0 + j * 256, 256)], h_q, next_ranges=nr)

            if stop_after == "u23":
                return
            def h_kv(tt, bk):
                b = tt % 2
                op("act", lambda e: e.copy(out=zr[b], in_=PB(bk)[:, 0:256]), reads=[("pb", bk)], writes=[("zr", b)])
                rv = roped[b].rearrange("p (h d) -> p h d", h=4)
                rope_ops(zr[b][:, 0:128], 2, [rv[:, 0:2, :]], tt, ("zr", b), [("roped", b)], b)
                op("pool", lambda e: e.tensor_copy(out=rv[:, 2, :], in_=rv[:, 1, :]), reads=[("roped", b)], writes=[("roped", b)])
                op("pool", lambda e: e.tensor_copy(out=rv[:, 3, :], in_=rv[:, 0, :]), reads=[("roped", b)], writes=[("roped", b)])
                def part2():
                    tbk = 6 + b
                    for q in range(2):
                        op("pe", lambda e, q=q: e.transpose(out=PBb(tbk)[:, q * 128:(q + 1) * 128],
                                                            in_=roped[b][:, q * 128:(q + 1) * 128], identity=ident_b),
                           reads=[("roped", b), "ident_b"], writes=[("pb", tbk)])
                    ts_ = slice(tt * 128, (tt + 1) * 128)
                    op("act", lambda e: e.copy(out=kz[0][0][0:64, ts_], in_=PBb(tbk)[0:64, 0:128]), reads=[("pb", tbk), "kz0"],
                       writes=[("bkT", tt)])
                    op("act", lambda e: e.copy(out=kz[1][1][64:128, ts_], in_=PBb(tbk)[64:128, 0:128]), reads=[("pb", tbk)],
                       writes=[("bkT", tt)])
                    op("act", lambda e: e.copy(out=kz[1][0][0:64, ts_], in_=PBb(tbk)[0:64, 128:256]), reads=[("pb", tbk)],
                       writes=[("bkT", tt)])
                    op("act", lambda e: e.copy(out=kz[0][1][64:128, ts_], in_=PBb(tbk)[64:128, 128:256]), reads=[("pb", tbk)],
                       writes=[("bkT", tt)])

                op("dve", lambda e: e.tensor_copy(out=bv_tok[:, tt, 0:64], in_=zr[b][:, 128:192]), reads=[("zr", b), "bv_ones"],
                   writes=[("bv", tt)])
                op("dve", lambda e: e.tensor_copy(out=bv_tok[:, tt, 65:129], in_=zr[b][:, 192:256]), reads=[("zr", b)],
                   writes=[("bv", tt)])
                return part2
            tok_chunk([(C_BK, 256)], h_kv, next_ranges=[(C_IW + 8 - 256, 256)])

            if stop_after == "u4a":
                return
            IW_SCALE = (8 ** -0.5) * (64 ** -0.5)

            def h_small(tt, bk):
                b = tt % 2
                op("act", lambda e: e.copy(out=zr[b][:, 0:72], in_=PB(bk)[:, 0:72]), reads=[("pb", bk)], writes=[("zr", b)])
                rv = roped[b].rearrange("p (h d) -> p h d", h=4)
                rope_ops(zr[b][:, 0:64], 1, [rv[:, 0:1, :], rv[:, 1:2, :]], tt, ("zr", b), [("roped", b)], b)
                def part2():
                    tbk = 6 + b
                    op("pe", lambda e: e.transpose(out=PBb(tbk)[:, 0:128], in_=roped[b][:, 0:128], identity=ident_b),
                       reads=[("roped", b), "ident_b"], writes=[("pb", tbk)])
                    op("act", lambda e: e.copy(out=ikT2[:, tt * 128:(tt + 1) * 128], in_=PBb(tbk)[:, 0:128]),
                       reads=[("pb", tbk)], writes=[("ikT", tt)])

                op("dve", lambda e: e.tensor_scalar(out=iw_tok[:, tt, :], in0=zr[b][:, 64:72], scalar1=IW_SCALE, scalar2=None,
                                                    op0=ALU.mult), reads=[("zr", b)], writes=[("iw", tt)])
                return part2
            tok_chunk([(C_IW + 8 - 256, 256)], h_small, sel=(184, 256), next_ranges=[(C_BETA, 256)])

            def h_ab(tt, bk):
                op("act", lambda e: e.copy(out=ab_tok[:, tt, :], in_=PB(bk)[:, 0:16]), reads=[("pb", bk)], writes=[("ab", tt)])
            tok_chunk([(C_BETA, 256)], h_ab, sel=(0, 16))

            for nm, t_, shape in (("bqT", bqT, None), ("iqT", iqT, None)):
                if nm in dbg:
                    op("sp", lambda e, nm=nm, t_=t_: e.dma_start(out=dbg[nm].rearrange("(a p) t -> p a t", p=128), in_=t_),
                       reads=[(nm, tt, j) for tt in range(NT) for j in range(2)], writes=["dbg_" + nm], dma=True)
                    op("sp", None, reads=["dbg_" + nm])
            if "misc" in dbg:
                sch.barrier()
                mt = view(R_W, [128, NT, 154], F32)
                op("dve", lambda e: e.tensor_copy(out=mt[:, :, 0:16], in_=ab_tok), reads=[("ab", tt) for tt in range(NT)], writes=["mt"])
                op("dve", lambda e: e.tensor_copy(out=mt[:, :, 16:24], in_=iw_tok), reads=[("iw", tt) for tt in range(NT)], writes=["mt"])
                op("dve", lambda e: e.tensor_copy(out=mt[:, :, 24:154], in_=bv_tok), reads=[("bv", tt) for tt in range(NT)], writes=["mt"])
                op("sp", lambda e: e.dma_start(out=dbg["misc"].rearrange("(t p) c -> p t c", p=128), in_=mt), reads=["mt"],
                   writes=["dbg_misc"], dma=True)
                op("sp", None, reads=["dbg_misc"])
            sch.barrier()
            if stop_after == "p2":
                return

            class MultiAlloc:
                def __init__(self, regions):
                    self.regs = [[a, b] for a, b in regions]

                def __call__(self, shape, dt):
                    n = 1
                    for s_ in shape[1:]:
                        n *= s_
                    size = (n * DT_SIZE[dt] + 63) // 64 * 64
                    for r in self.regs:
                        r[0] = (r[0] + 63) // 64 * 64
                        if r[0] + size <= r[1]:
                            v = view(r[0], shape, dt)
                            r[0] += size
                            return v
                    raise AssertionError(("MultiAlloc out of space", shape, self.regs))

            def dump(name, ap, reads):
                if name in dbg:
                    op("sp", lambda e: e.dma_start(out=dbg[name], in_=ap), reads=reads, writes=["dbg_" + name], dma=True)
                    op("sp", None, reads=["dbg_" + name])

            o_aT = view(R_A, [128, 4, S], BF16)
            diagw_off = R_WORK - 12 * K
            ga = MultiAlloc([(R_W, R_W + 16 * K), (R_A + 16 * K, R_A + 32 * K), (diagw_off, ARENA_BYTES)])
            g_all = ga([128, NT, 8], F32)
            bet = ga([128, NT, 8], F32)
            gs = ga([128, 24], F32)
            eG = ga([128, 8], F32)
            eGlmG = ga([128, 8], F32)
            scs = [ga([128, 4], F32) for _ in range(2)]
            g_bc = ga([128, 8, 128], F32)
            sq = ga([128, 1024], F32)
            ssn = ga([128, 16], F32)
            rn = ga([128, 16], F32)
            cq = ga([128, 8], F32)
            cqd = ga([128, 8], F32)
            cbk = ga([128, 8], F32)
            ckd = ga([128, 8], F32)
            negbeta = ga([128, 8], F32)
            qn = ga([128, 512], BF16)
            qd = ga([128, 512], BF16)
            kn = ga([128, 512], BF16)
            rhsk = ga([128, 512], BF16)
            kdec = ga([128, 512], BF16)
            rhsv = ga([128, 512], BF16)
            qnT = ga([128, 4, 128], BF16)
            qdT = ga([128, 4, 128], BF16)
            knT = ga([128, 4, 128], BF16)
            Dm = ga([128, 8, 128], BF16)
            Ds = ga([128, 8, 128], BF16)
            Mm = [ga([128, 8, 128], BF16) for _ in range(2)]
            Nm = [ga([128, 8, 128], BF16) for _ in range(2)]
            Pm = [ga([128, 8, 128], BF16) for _ in range(2)]
            qkm = ga([128, 8, 128], BF16)
            qkT_sb = ga([128, 8, 128], BF16)
            u_c = ga([128, 2, 512], F32)
            w_tok = ga([128, 512], BF16)
            wT_sb = ga([128, 4, 128], BF16)
            vn_b = ga([128, 512], BF16)
            Sst = ga([128, 4, 128], F32)
            Stmp = ga([128, 4, 128], F32)
            S_bd = ga([128, 4, 128], BF16)
            bdmask = ga([128, 4, 128], BF16)
            o_c = ga([128, 512], F32)
            qkT_c1 = ga([128, 8, 64], BF16)
            kdec_c1 = ga([128, 512], BF16)
            az_c = ga([128, 512], BF16)
            ss2 = ga([128, 8], F32)
            r2 = ga([128, 8], F32)
            oa_b = ga([128, 512], BF16)

            def bc8(v):
                return v.unsqueeze(2).to_broadcast([128, 8, 64])

            ABK = [("ab", tt) for tt in range(NT)]
            op("act", lambda e: e.activation(out=bet, in_=ab_tok[:, :, 0:8], func=AF.Sigmoid), reads=["ab_all"], writes=["bet"])
            op("dve", lambda e: e.tensor_tensor(out=g_all, in0=ab_tok[:, :, 8:16], in1=dtb.unsqueeze(1).to_broadcast([128, NT, 8]),
                                                op=ALU.add), reads=["ab_all", "dtb"], writes=["g_all"])
            op("act", lambda e: e.activation(out=g_all, in_=g_all, func=AF.Exp), reads=["g_all"], writes=["g_all"])
            op("act", lambda e: e.activation(out=g_all, in_=g_all, func=AF.Ln, bias=1.0), reads=["g_all"], writes=["g_all"])
            op("dve", lambda e: e.tensor_tensor(out=g_all, in0=g_all, in1=negA.unsqueeze(1).to_broadcast([128, NT, 8]),
                                                op=ALU.mult), reads=["g_all", "negA"], writes=["g_all"])
            op("dve", lambda e: e.memset(Sst, 0.0), writes=["S"])
            op("dve", lambda e: e.memset(S_bd, 0.0), writes=["S_bd"])
            op("pool", lambda e: e.memset(bdmask, 0.0), writes=["bdmask"])
            op("pool", lambda e: e.memset(bdmask[0:64, :, 0:64], 1.0), writes=["bdmask"])
            op("pool", lambda e: e.memset(bdmask[64:128, :, 64:128], 1.0), writes=["bdmask"])
            if "g" in dbg:
                op("sp", lambda e: e.dma_start(out=dbg["g"].rearrange("(t p) c -> p t c", p=128), in_=g_all), reads=["g_all"],
                   writes=["dbg_g"], dma=True)
                op("sp", None, reads=["dbg_g"])

            if stop_after == "gdn_pre":
                return
            for tt in range(NT):
                op("pe", lambda e, tt=tt: e.matmul(PB(0)[:, 0:8], lhsT=ucs_f, rhs=g_all[:, tt, :], start=True, stop=True),
                   reads=["g_all"], writes=[("pb", 0)])
                op("pe", lambda e, tt=tt: e.matmul(PB(0)[:, 8:16], lhsT=mc0_f, rhs=g_all[:, tt, :], start=True, stop=True),
                   reads=["g_all"], writes=[("pb", 0)])
                op("pe", lambda e, tt=tt: e.matmul(PB(0)[:, 16:24], lhsT=mc1_f, rhs=g_all[:, tt, :], start=True, stop=True),
                   reads=["g_all"], writes=[("pb", 0)])
                op("act", lambda e: e.copy(out=gs, in_=PB(0)[:, 0:24]), reads=[("pb", 0)], writes=["gs"])
                op("act", lambda e: e.activation(out=eG, in_=gs[:, 0:8], func=AF.Exp), reads=["gs"], writes=["eG"])
                op("dve", lambda e: e.tensor_tensor(out=eGlmG[0:64, :], in0=gs[0:64, 8:16], in1=gs[0:64, 0:8], op=ALU.subtract),
                   reads=["gs"], writes=["eGlmG"])
                op("dve", lambda e: e.tensor_tensor(out=eGlmG[64:128, :], in0=gs[64:128, 16:24], in1=gs[64:128, 0:8],
                                                    op=ALU.subtract), reads=["gs"], writes=["eGlmG"])
                op("act", lambda e: e.activation(out=eGlmG, in_=eGlmG, func=AF.Exp), reads=["eGlmG"], writes=["eGlmG"])
                for hf in range(2):
                    c0 = 8 + 8 * hf
                    op("act", lambda e, hf=hf, c0=c0: e.activation(out=scs[hf][0:64, :], in_=gs[0:64, c0:c0 + 8:2], func=AF.Exp),
                       reads=["gs"], writes=[("scs", hf)])
                    op("act", lambda e, hf=hf, c0=c0: e.activation(out=scs[hf][64:128, :], in_=gs[64:128, c0 + 1:c0 + 8:2],
                                                                   func=AF.Exp), reads=["gs"], writes=[("scs", hf)])
                op("dve", lambda e, tt=tt: e.tensor_scalar(out=g_bc, in0=g_all[:, tt, :].unsqueeze(2).to_broadcast([128, 8, 128]),
                                                           scalar1=-1.0, scalar2=None, op0=ALU.mult),
                   reads=["g_all"], writes=["g_bc"])
                if stop_after == "gdn_a":
                    return
                QK = [("qkv_tok", tt, c) for c in range(8)]
                VV = [("qkv_tok", tt, c) for c in range(8, 12)]
                op("dve", lambda e, tt=tt: e.tensor_tensor(out=sq, in0=qkv_tok[:, tt, 0:1024], in1=qkv_tok[:, tt, 0:1024],
                                                           op=ALU.mult), reads=["qkv_all"], writes=["sq"])
                op("dve", lambda e: e.tensor_reduce(out=ssn, in_=sq.rearrange("p (h d) -> p h d", h=16), axis=AX.X, op=ALU.add),
                   reads=["sq"], writes=["ssn"])
                op("act", lambda e: e.activation(out=rn, in_=ssn, func=AF.Ln, bias=epsc, scale=1.0), reads=["ssn", "epsc"],
                   writes=["rn"])
                op("act", lambda e: e.activation(out=rn, in_=rn, func=AF.Exp, scale=-0.5), reads=["rn"], writes=["rn"])
                op("dve", lambda e: e.tensor_scalar(out=cq, in0=rn[:, 0:8], scalar1=0.125, scalar2=None, op0=ALU.mult),
                   reads=["rn"], writes=["cq"])
                op("dve", lambda e: e.tensor_tensor(out=cqd, in0=cq, in1=eG, op=ALU.mult), reads=["cq", "eG"], writes=["cqd"])
                op("dve", lambda e, tt=tt: e.tensor_tensor(out=cbk, in0=rn[:, 8:16], in1=bet[:, tt, :], op=ALU.mult),
                   reads=["rn", "bet"], writes=["cbk"])
                op("dve", lambda e: e.tensor_tensor(out=cbk, in0=cbk, in1=eG, op=ALU.mult), reads=["cbk", "eG"], writes=["cbk"])
                op("dve", lambda e: e.tensor_tensor(out=ckd, in0=rn[:, 8:16], in1=eGlmG, op=ALU.mult), reads=["rn", "eGlmG"],
                   writes=["ckd"])
                op("dve", lambda e, tt=tt: e.tensor_scalar(out=negbeta, in0=bet[:, tt, :], scalar1=-1.0, scalar2=None,
                                                           op0=ALU.mult), reads=["bet"], writes=["negbeta"])
                qv = qkv_tok[:, tt, 0:512].rearrange("p (h d) -> p h d", h=8)
                kv = qkv_tok[:, tt, 512:1024].rearrange("p (h d) -> p h d", h=8)
                vv = qkv_tok[:, tt, 1024:1536].rearrange("p (h d) -> p h d", h=8)

                def v3(t_):
                    return t_.rearrange("p (h d) -> p h d", h=8)
                op("dve", lambda e, qv=qv: e.tensor_tensor(out=v3(qn), in0=qv, in1=bc8(cq), op=ALU.mult),
                   reads=["qkv_all", "cq"], writes=["qn"])
                op("pool", lambda e, qv=qv: e.tensor_tensor(out=v3(qd), in0=qv, in1=bc8(cqd), op=ALU.mult),
                   reads=["qkv_all", "cqd"], writes=["qd"])
                op("dve", lambda e, kv=kv: e.tensor_tensor(out=v3(kn), in0=kv, in1=bc8(rn[:, 8:16]), op=ALU.mult),
                   reads=["qkv_all", "rn"], writes=["kn"])
                op("pool", lambda e, kv=kv: e.tensor_tensor(out=v3(rhsk), in0=kv, in1=bc8(cbk), op=ALU.mult),
                   reads=["qkv_all", "cbk"], writes=["rhsk"])
                op("pool", lambda e, kv=kv: e.tensor_tensor(out=v3(kdec), in0=kv, in1=bc8(ckd), op=ALU.mult),
                   reads=["qkv_all", "ckd"], writes=["kdec"])
                op("dve", lambda e, vv=vv, tt=tt: e.tensor_tensor(out=v3(rhsv), in0=vv, in1=bc8(bet[:, tt, :]), op=ALU.mult),
                   reads=["qkv_all", "bet"], writes=["rhsv"])
                if stop_after == "gdn_b":
                    return
                for (src, skey, dst, dkey, bank, coff, eng) in ((qn, "qn", qnT, "qnT", 6, 0, "act"), (qd, "qd", qdT, "qdT", 7, 0, "dve"),
                                                                (kn, "kn", knT, "knT", 0, 0, "act")):
                    for q in range(4):
                        op("pe", lambda e, src=src, bank=bank, coff=coff, q=q: e.transpose(
                            out=PBb(bank)[:, coff + q * 128:coff + (q + 1) * 128], in_=src[:, q * 128:(q + 1) * 128], identity=ident_b),
                           reads=[skey, "ident_b"], writes=[("pb", bank)])
                    if eng == "act":
                        op("act", lambda e, dst=dst, bank=bank, coff=coff: e.copy(
                            out=dst, in_=PBb(bank)[:, coff:coff + 512].rearrange("p (q t) -> p q t", q=4)),
                           reads=[("pb", bank)], writes=[dkey])
                    else:
                        op("dve", lambda e, dst=dst, bank=bank, coff=coff: e.tensor_copy(
                            out=dst, in_=PBb(bank)[:, coff:coff + 512].rearrange("p (q t) -> p q t", q=4)),
                           reads=[("pb", bank)], writes=[dkey])
                    if stop_after == "gdn_c_" + skey:
                        return
                if tt == 0:
                    dump("qn0", qn, ["qn"]); dump("kn0", kn, ["kn"]); dump("rhsv0", rhsv, ["rhsv"]); dump("rhsk0", rhsk, ["rhsk"])
                    dump("kdec0", kdec, ["kdec"]); dump("qd0", qd, ["qd"]); dump("gs0", gs, ["gs"])
                    dump("knT0", knT.rearrange("p a t -> p (a t)"), ["knT"])
                    dump("rn0", rn, ["rn"]); dump("cq0", cq, ["cq"]); dump("cqd0", cqd, ["cqd"]); dump("cbk0", cbk, ["cbk"])
                    dump("ckd0", ckd, ["ckd"]); dump("eG0", eG, ["eG"]); dump("ssn0", ssn, ["ssn"])
                if stop_after == "gdn_c":
                    return
                def group_gen(hg, bA, bB, bC, bT):
                    hs_list = list(range(4))
                    grp = slice(4 * hg, 4 * hg + 4)
                    for hs in hs_list:
                        h = 4 * hg + hs
                        hp, par = h // 2, h % 2
                        rows = slice(par * 64, par * 64 + 64)
                        cs_ = slice(hs * 128, hs * 128 + 128)
                        op("pe", lambda e, hp=hp, rows=rows, cs_=cs_, par=par: e.matmul(
                            PB(bA)[:, cs_], lhsT=knT[rows, hp, :], rhs=knT[rows, hp, :], start=True, stop=True,
                            tile_position=(par * 64, 0)), reads=["knT"], writes=[("pb", bA)])
                        op("pe", lambda e, hp=hp, rows=rows, cs_=cs_, par=par: e.matmul(
                            PB(bB)[:, cs_], lhsT=qnT[rows, hp, :], rhs=knT[rows, hp, :], start=True, stop=True,
                            tile_position=(par * 64, 0)), reads=["knT", "qnT"], writes=[("pb", bB)])
                        op("pe", lambda e, h=h, cs_=cs_: e.matmul(PB(bC)[:, cs_], lhsT=g_bc[:, h, :], rhs=ucs_f, start=True, stop=False),
                           reads=["g_bc", "ucs_f"], writes=[("pb", bC)])
                        op("pe", lambda e, cs_=cs_: e.matmul(PB(bC)[:, cs_], lhsT=ident_f, rhs=maskneg_f, start=False, stop=True),
                           reads=["ident_f", "maskneg_f"], writes=[("pb", bC)])
                    yield
                    for hs in hs_list:
                        h = 4 * hg + hs
                        cs_ = slice(hs * 128, hs * 128 + 128)
                        op("act", lambda e, h=h, cs_=cs_: e.activation(out=Dm[:, h, :], in_=PB(bC)[:, cs_], func=AF.Exp,
                                                                       bias=gs[:, h:h + 1], scale=1.0),
                           reads=[("pb", bC), "gs"], writes=[("Dm", h)])
                        op("pool", lambda e, h=h: e.tensor_tensor(out=Ds[:, h, :], in0=Dm[:, h, :], in1=strict_b, op=ALU.mult),
                           reads=[("Dm", h), "strict_b"], writes=[("Ds", h)])
                        op("dve", lambda e, h=h, cs_=cs_: e.scalar_tensor_tensor(out=Mm[0][:, h, :], in0=PB(bA)[:, cs_],
                                                                                 scalar=negbeta[:, h:h + 1], in1=Ds[:, h, :],
                                                                                 op0=ALU.mult, op1=ALU.mult),
                           reads=[("pb", bA), "negbeta", ("Ds", h)], writes=[("M", 0, hg)])
                        op("dve", lambda e, h=h, cs_=cs_: e.tensor_tensor(out=qkm[:, h, :], in0=PB(bB)[:, cs_], in1=Dm[:, h, :],
                                                                          op=ALU.mult),
                           reads=[("pb", bB), ("Dm", h)], writes=[("qkm", hg)])
                    yield
                    for hs in hs_list:
                        h = 4 * hg + hs
                        cs_ = slice(hs * 128, hs * 128 + 128)
                        op("pe", lambda e, h=h, cs_=cs_: e.transpose(out=PBb(bT)[:, cs_], in_=Mm[0][:, h, :], identity=ident_b),
                           reads=[("M", 0, hg), "ident_b"], writes=[("pb", bT)])
                    op("act", lambda e: e.copy(out=Nm[0][:, grp, :], in_=PBb(bT)[:, 0:512].rearrange("p (q t) -> p q t", q=4)),
                       reads=[("pb", bT)], writes=[("N", 0, hg)])
                    op("pool", lambda e: e.tensor_tensor(out=Pm[0][:, grp, :], in0=Nm[0][:, grp, :],
                                                         in1=ident_b.unsqueeze(1).to_broadcast([128, 4, 128]), op=ALU.add),
                       reads=[("N", 0, hg), "ident_b"], writes=[("P", 0, hg)])
                    yield
                    for hs in hs_list:
                        h = 4 * hg + hs
                        cs2 = slice(hs * 128, hs * 128 + 128)
                        op("pe", lambda e, h=h, cs2=cs2: e.transpose(out=PBb(bT)[:, cs2], in_=qkm[:, h, :], identity=ident_b),
                           reads=[("qkm", hg), "ident_b"], writes=[("pb", bT)])
                    op("dve", lambda e: e.tensor_copy(out=qkT_sb[:, grp, :], in_=PBb(bT)[:, 0:512].rearrange("p (q t) -> p q t", q=4)),
                       reads=[("pb", bT)], writes=[("qkT", hg)])
                    yield
                    for lv in range(1, 6):
                        cur, nxt = (lv - 1) % 2, lv % 2
                        for hs in hs_list:
                            h = 4 * hg + hs
                            cs_ = slice(hs * 128, hs * 128 + 128)
                            op("pe", lambda e, h=h, cs_=cs_, cur=cur: e.matmul(PB(bA)[:, cs_], lhsT=Nm[cur][:, h, :], rhs=Mm[cur][:, h, :],
                                                                               start=True, stop=True),
                               reads=[("N", cur, hg), ("M", cur, hg)], writes=[("pb", bA)])
                        if lv < 5:
                            for hs in hs_list:
                                h = 4 * hg + hs
                                cs_ = slice(hs * 128, hs * 128 + 128)
                                op("pe", lambda e, h=h, cs_=cs_, cur=cur: e.matmul(PB(bB)[:, cs_], lhsT=Mm[cur][:, h, :],
                                                                                   rhs=Nm[cur][:, h, :], start=True, stop=True),
                                   reads=[("N", cur, hg), ("M", cur, hg)], writes=[("pb", bB)])
                        yield
                        op("act", lambda e, nxt=nxt: e.copy(out=Mm[nxt][:, grp, :], in_=PB(bA).rearrange("p (q t) -> p q t", q=4)),
                           reads=[("pb", bA)], writes=[("M", nxt, hg)])
                        if lv < 5:
                            op("dve", lambda e, nxt=nxt: e.tensor_copy(out=Nm[nxt][:, grp, :],
                                                                       in_=PB(bB).rearrange("p (q t) -> p q t", q=4)),
                               reads=[("pb", bB)], writes=[("N", nxt, hg)])
                        for hs in hs_list:
                            h = 4 * hg + hs
                            cs_ = slice(hs * 128, hs * 128 + 128)
                            op("pe", lambda e, h=h, cs_=cs_, cur=cur, nxt=nxt: e.matmul(PB(bC)[:, cs_], lhsT=Mm[nxt][:, h, :],
                                                                                        rhs=Pm[cur][:, h, :], start=True, stop=True),
                               reads=[("M", nxt, hg), ("P", cur, hg)], writes=[("pb", bC)])
                        yield
                        op("dve", lambda e, cur=cur, nxt=nxt: e.tensor_tensor(
                            out=Pm[nxt][:, grp, :], in0=Pm[cur][:, grp, :], in1=PB(bC).rearrange("p (q t) -> p q t", q=4), op=ALU.add),
                           reads=[("pb", bC), ("P", cur, hg)], writes=[("P", nxt, hg)])

                gens = [group_gen(0, 3, 4, 5, 6), group_gen(1, 0, 1, 2, 7)]
                while gens:
                    for g_ in list(gens):
                        try:
                            next(g_)
                        except StopIteration:
                            gens.remove(g_)
                Pf = Pm[1]
                PK = [("P", 1, 0), ("P", 1, 1)]
                if tt == 0:
                    dump("D0", Dm.rearrange("p a t -> p (a t)"), [("Dm", h) for h in range(8)])
                    dump("M0", Mm[0].rearrange("p a t -> p (a t)"), [("M", 0, 0), ("M", 0, 1)])
                    dump("N0", Nm[0].rearrange("p a t -> p (a t)"), [("N", 0, 0), ("N", 0, 1)])
                    dump("P0", Pm[1].rearrange("p a t -> p (a t)"), [("P", 1, 0), ("P", 1, 1)])
                    dump("qkT0", qkT_sb.rearrange("p a t -> p (a t)"), [("qkT", 0), ("qkT", 1)])
                if stop_after == "gdn_e":
                    return
                for hf in range(2):
                    ub = 7 if hf == 0 else 0
                    for h in range(8):
                        op("pe", lambda e, h=h, hf=hf, ub=ub: e.matmul(PB(ub)[0:64, h * 64:(h + 1) * 64],
                                                                       lhsT=Pf[:, h, hf * 64:(hf + 1) * 64],
                                                                       rhs=rhsv[:, h * 64:(h + 1) * 64], start=True, stop=True),
                           reads=PK + ["rhsv"], writes=[("pb", ub)])
                    op("act", lambda e, hf=hf, ub=ub: e.copy(out=u_c[0:64, hf, :], in_=PB(ub)[0:64, :]), reads=[("pb", ub)],
                       writes=[("u_c", hf)])
                for h in range(8):
                    op("pe", lambda e, h=h: e.matmul(PB(1)[:, h * 64:(h + 1) * 64], lhsT=Pf[:, h, :], rhs=rhsk[:, h * 64:(h + 1) * 64],
                                                     start=True, stop=True), reads=PK + ["rhsk"], writes=[("pb", 1)])
                op("act", lambda e: e.copy(out=w_tok, in_=PB(1)), reads=[("pb", 1)], writes=["w_tok"])
                for q in range(4):
                    op("pe", lambda e, q=q: e.transpose(out=PBb(6)[:, q * 128:(q + 1) * 128], in_=w_tok[:, q * 128:(q + 1) * 128],
                                                        identity=ident_b), reads=["w_tok", "ident_b"], writes=[("pb", 6)])
                op("dve", lambda e: e.tensor_copy(out=wT_sb, in_=PBb(6)[:, 0:512].rearrange("p (q t) -> p q t", q=4)),
                   reads=[("pb", 6)], writes=["wT_sb"])
                op("sp", lambda e: e.dma_start(out=qkT_c1[0:64, :, :], in_=qkT_sb[64:128, :, 64:128]),
                   reads=[("qkT", 0), ("qkT", 1)], writes=["qkT_c1"], dma=True)
                op("sp", lambda e: e.dma_start(out=kdec_c1[0:64, :], in_=kdec[64:128, :]), reads=["kdec"], writes=["kdec_c1"],
                   dma=True)
                op("sp", lambda e, tt=tt: e.dma_start(out=az_c[0:64, :], in_=azs[64:128, tt, :]), reads=["azs_all"], writes=["az_c"],
                   dma=True)
                if tt == 0:
                    dump("u0", u_c.rearrange("p a t -> p (a t)"), [("u_c", 0), ("u_c", 1)])
                    dump("wT0", wT_sb.rearrange("p a t -> p (a t)"), ["wT_sb"])
                if stop_after == "gdn_f":
                    return
                for hf in range(2):
                    tcs = slice(hf * 64, hf * 64 + 64)
                    ck = 2 * tt + hf
                    if hf == 0:
                        qk_x, qk_keys = qkT_sb[0:64, :, 0:64], [("qkT", 0), ("qkT", 1)]
                        kd_x, kd_keys = kdec[0:64, :], ["kdec"]
                    else:
                        qk_x, qk_keys = qkT_c1[0:64, :, :], ["qkT_c1"]
                        kd_x, kd_keys = kdec_c1[0:64, :], ["kdec_c1"]
                    for hp in range(4):
                        op("pe", lambda e, hp=hp, tcs=tcs: e.matmul(PB(1)[0:64, hp * 128:(hp + 1) * 128], lhsT=wT_sb[:, hp, tcs],
                                                                    rhs=S_bd[:, hp, :], start=True, stop=True),
                           reads=["wT_sb", "S_bd"], writes=[("pb", 1)])
                    op("dve", lambda e, hf=hf: e.tensor_tensor(out=vn_b[0:64, :], in0=u_c[0:64, hf, :], in1=PB(1)[0:64, :],
                                                               op=ALU.subtract), reads=[("u_c", hf), ("pb", 1)], writes=["vn_b"])
                    for h in range(8):
                        hp, par = h // 2, h % 2
                        op("pe", lambda e, h=h, hp=hp, par=par, tcs=tcs: e.matmul(
                            PB(2)[0:64, h * 64:(h + 1) * 64], lhsT=qdT[:, hp, tcs], rhs=S_bd[:, hp, par * 64:(par + 1) * 64],
                            start=True, stop=False), reads=["qdT", "S_bd"], writes=[("pb", 2)])
                        op("pe", lambda e, h=h, qk_x=qk_x: e.matmul(
                            PB(2)[0:64, h * 64:(h + 1) * 64], lhsT=qk_x[:, h, :], rhs=vn_b[0:64, h * 64:(h + 1) * 64],
                            start=False, stop=True), reads=qk_keys + ["vn_b"], writes=[("pb", 2)])
                    for hp in range(4):
                        op("pe", lambda e, hp=hp, kd_x=kd_x: e.matmul(PB(7)[:, hp * 128:(hp + 1) * 128],
                                                                      lhsT=kd_x[:, hp * 128:(hp + 1) * 128],
                                                                      rhs=vn_b[0:64, hp * 128:(hp + 1) * 128], start=True, stop=True),
                           reads=kd_keys + ["vn_b"], writes=[("pb", 7)])
                    op("act", lambda e: e.copy(out=o_c[0:64, :], in_=PB(2)[0:64, :]), reads=[("pb", 2)], writes=["o_c"])
                    op("pool", lambda e, hf=hf: e.tensor_tensor(out=Stmp, in0=Sst,
                                                                in1=scs[hf].unsqueeze(2).to_broadcast([128, 4, 128]), op=ALU.mult),
                       reads=["S", ("scs", hf)], writes=["Stmp"])
                    op("dve", lambda e: e.tensor_tensor(out=Sst, in0=Stmp, in1=PB(7).rearrange("p (a d) -> p a d", a=4), op=ALU.add),
                       reads=["Stmp", ("pb", 7)], writes=["S"])
                    op("pool", lambda e: e.tensor_tensor(out=S_bd, in0=Sst, in1=bdmask, op=ALU.mult), reads=["S", "bdmask"],
                       writes=["S_bd"])
                    if "o_raw" in dbg:
                        op("sp", lambda e, ck=ck: e.dma_start(out=dbg["o_raw"][ck * 64:(ck + 1) * 64, :], in_=o_c[0:64, :]),
                           reads=["o_c"], writes=["dbg_o_raw"], dma=True)
                    sqh = sq[0:64, 0:512]
                    op("dve", lambda e: e.tensor_tensor(out=sqh, in0=o_c[0:64, :], in1=o_c[0:64, :], op=ALU.mult), reads=["o_c"],
                       writes=["sq"])
                    op("dve", lambda e: e.tensor_reduce(out=ss2[0:64, :], in_=sqh.rearrange("p (h d) -> p h d", h=8), axis=AX.X,
                                                        op=ALU.add), reads=["sq"], writes=["ss2"])
                    op("act", lambda e: e.activation(out=r2[0:64, :], in_=ss2[0:64, :], func=AF.Ln, bias=epsc[0:64, :],
                                                     scale=1.0 / 64), reads=["ss2", "epsc"], writes=["r2"])
                    op("act", lambda e: e.activation(out=r2[0:64, :], in_=r2[0:64, :], func=AF.Exp, scale=-0.5), reads=["r2"],
                       writes=["r2"])
                    op("dve", lambda e: e.tensor_tensor(out=v3(sqh), in0=v3(o_c[0:64, :]),
                                                        in1=r2[0:64, :].unsqueeze(2).to_broadcast([64, 8, 64]), op=ALU.mult),
                       reads=["o_c", "r2"], writes=["sq"])
                    op("pool", lambda e: e.tensor_tensor(out=v3(sqh), in0=v3(sqh),
                                                         in1=angb[0:64, :].unsqueeze(1).to_broadcast([64, 8, 64]), op=ALU.mult),
                       reads=["sq", "angb"], writes=["sq"])
                    az_x = azs[0:64, tt, :] if hf == 0 else az_c[0:64, :]
                    op("pool", lambda e, az_x=az_x: e.tensor_tensor(out=oa_b[0:64, :], in0=sqh, in1=az_x, op=ALU.mult),
                       reads=["sq", "az_c"], writes=["oa_b"])
                    for q in range(4):
                        op("pe", lambda e, q=q: e.transpose(out=PBb(6)[:, q * 64:(q + 1) * 64], in_=oa_b[0:64, q * 128:(q + 1) * 128],
                                                            identity=ident_b[0:64, 0:64]), reads=["oa_b", "ident_b"],
                           writes=[("pb", 6)])
                    op("act", lambda e, ck=ck: e.copy(out=o_aT[:, :, ck * 64:(ck + 1) * 64],
                                                      in_=PBb(6)[:, 0:256].rearrange("p (q t) -> p q t", q=4)),
                       reads=[("pb", 6)], writes=[("o_aT", ck)])
            if "o_raw" in dbg:
                op("sp", None, reads=["dbg_o_raw"])
            if "o_aT" in dbg:
                op("sp", lambda e: e.dma_start(out=dbg["o_aT"].rearrange("(a p) t -> p a t", p=128), in_=o_aT),
                   reads=[("o_aT", ck) for ck in range(2 * NT)], writes=["dbg_o_aT"], dma=True)
                op("sp", None, reads=["dbg_o_aT"])

            sch.barrier()
            if stop_after == "gdn":
                return

            o_bT = view(R_A + 16 * K, [128, 4, S], BF16)
            da = MultiAlloc([(R_Q, R_Q + 48 * K), (R_W, R_W + 16 * K)])
            scoreb = [da([128, S], F32) for _ in range(2)]
            rl = [da([128, 512], F32) for _ in range(2)]
            maskbb = [da([128, S], BF16) for _ in range(2)]
            thr_t = [da([128, 1], F32) for _ in range(2)]
            PTt = [[da([128, 512], BF16) for _ in range(2)] for _ in range(2)]
            I4 = da([128, 512], BF16)
            lo_t = da([128, 1], F32)
            hi_t = da([128, 1], F32)
            W0 = da([128, 1], F32)
            mid_t = da([128, 1], F32)
            tsel = da([128, 1], F32)
            Wk = da([128, NBIS], F32)
            cnt = da([128, NBIS], F32)
            pow2 = da([128, NBIS], F32)
            ob = da([128, 520], F32)
            rden = da([128, 8], F32)
            ob_b = da([128, 512], BF16)
            for q in range(4):
                op("pool", lambda e, q=q: e.tensor_copy(out=I4[:, q * 128:(q + 1) * 128], in_=ident_b), reads=["ident_b"], writes=["I4"])
            for k in range(NBIS):
                op("pool", lambda e, k=k: e.memset(pow2[:, k:k + 1], 2.0 ** (-(k + 1))), writes=["pow2"])

            def scores_part(tt, sb):
                L = (tt + 1) * 128
                nkb = (L + 511) // 512
                qs = slice(tt * 128, (tt + 1) * 128)
                score = scoreb[sb]
                maskb = maskbb[sb]
                for h in range(8):
                    hp, par = h // 2, h % 2
                    rows = slice(par * 64, par * 64 + 64)
                    for kb in range(nkb):
                        w = min(512, L - kb * 512)
                        bank = kb % 2
                        ks = slice(kb * 512, kb * 512 + w)
                        op("pe", lambda e, hp=hp, par=par, rows=rows, w=w, bank=bank, ks=ks: e.matmul(
                            PB(bank)[:, 0:w], lhsT=iqT[rows, hp, qs], rhs=ikT2[rows, ks], start=True, stop=True,
                            tile_position=(par * 64, 0)), reads=["iqT", "ikT2"], writes=[("pb", bank)])
                        op("act", lambda e, w=w, bank=bank: e.activation(out=rl[bank][:, 0:w], in_=PB(bank)[:, 0:w], func=AF.Relu),
                           reads=[("pb", bank)], writes=[("rl", bank)])
                        if h == 0:
                            op("dve", lambda e, w=w, bank=bank, ks=ks: e.tensor_scalar(
                                out=score[:, ks], in0=rl[bank][:, 0:w], scalar1=iw_tok[:, tt, 0:1], scalar2=None, op0=ALU.mult),
                               reads=[("rl", bank), "iw_tok"], writes=[("score", sb, kb)])
                        else:
                            op("dve", lambda e, w=w, bank=bank, ks=ks, h=h: e.scalar_tensor_tensor(
                                out=score[:, ks], in0=rl[bank][:, 0:w], scalar=iw_tok[:, tt, h:h + 1], in1=score[:, ks],
                                op0=ALU.mult, op1=ALU.add), reads=[("rl", bank), "iw_tok", ("score", sb, kb)],
                               writes=[("score", sb, kb)], fast=(w >= 256))
                SK = [("score", sb, kb) for kb in range(nkb)]
                if tt >= 2:
                    op("dve", lambda e: e.tensor_reduce(out=hi_t, in_=score[:, 0:L], axis=AX.X, op=ALU.max), reads=SK, writes=["hi"])
                    op("dve", lambda e: e.tensor_reduce(out=lo_t, in_=score[:, 0:L], axis=AX.X, op=ALU.min), reads=SK, writes=["lo"])
                op("dve", lambda e: e.memset(score[0:64, L - 64:L], -1.0e30), reads=SK, writes=SK)
                if tt >= 2:
                    op("dve", lambda e: e.tensor_tensor(out=W0, in0=hi_t, in1=lo_t, op=ALU.subtract), reads=["hi", "lo"], writes=["W0"])
                    op("dve", lambda e: e.tensor_scalar(out=Wk, in0=pow2, scalar1=W0[:, 0:1], scalar2=None, op0=ALU.mult),
                       reads=["W0", "pow2"], writes=["Wk"])
                    op("dve", lambda e: e.memset(cnt, 0.0), writes=["cnt"])
                    op("dve", lambda e: e.tensor_tensor(out=mid_t, in0=lo_t, in1=Wk[:, 0:1], op=ALU.add), reads=["lo", "Wk"], writes=["mid"])
                    for k in range(NBIS):
                        op("dve", lambda e, k=k: e.tensor_scalar(out=maskb[:, 0:L], in0=score[:, 0:L], scalar1=mid_t[:, 0:1],
                                                                 scalar2=0.0, op0=ALU.is_gt, op1=ALU.add, accum_out=cnt[:, k:k + 1]),
                           reads=SK + ["mid", "cnt"], writes=[("maskb", sb), ("cntk", k)])
                        op("dve", lambda e, k=k: e.tensor_scalar(out=tsel, in0=cnt[:, k:k + 1], scalar1=255.5, scalar2=0.5,
                                                                 op0=ALU.is_gt, op1=ALU.subtract), reads=[("cntk", k)], writes=["tsel"])
                        op("dve", lambda e, k=k: e.scalar_tensor_tensor(out=mid_t, in0=tsel, scalar=Wk[:, k:k + 1], in1=mid_t,
                                                                        op0=ALU.mult, op1=ALU.add),
                           reads=["tsel", "Wk", "mid"], writes=["mid"])
                    op("dve", lambda e: e.scalar_tensor_tensor(out=thr_t[sb], in0=Wk[:, NBIS - 1:NBIS], scalar=-0.5, in1=mid_t,
                                                               op0=ALU.mult, op1=ALU.add), reads=["Wk", "mid"], writes=[("thr", sb)])
                else:
                    op("dve", lambda e: e.memset(thr_t[sb], -1.0e29), writes=[("thr", sb)])
                op("dve", lambda e: e.tensor_scalar(out=maskb[:, 0:L], in0=score[:, 0:L], scalar1=thr_t[sb][:, 0:1], scalar2=NEG,
                                                    op0=ALU.is_le, op1=ALU.mult), reads=SK + [("thr", sb)], writes=[("maskb", sb)])
                if "thr" in dbg:
                    op("sp", lambda e: e.dma_start(out=dbg["thr"][tt * 128:(tt + 1) * 128, :], in_=thr_t[sb]), reads=[("thr", sb)],
                       writes=["dbg_thr"], dma=True)
                if "score" in dbg and tt == NT - 1:
                    op("sp", lambda e: e.dma_start(out=dbg["score"], in_=score), reads=SK, writes=["dbg_score"], dma=True)

            def attn_part(tt, sb):
                qs = slice(tt * 128, (tt + 1) * 128)
                maskb = maskbb[sb]
                for kb in range(tt + 1):
                    kcs = slice(kb * 128, (kb + 1) * 128)
                    for g2 in range(2):
                        bank = 2 + g2 + 2 * (kb % 2)
                        pt = PTt[g2][kb % 2]
                        op("pe", lambda e, bank=bank, kcs=kcs: e.matmul(PB(bank), lhsT=maskb[:, kcs], rhs=I4, start=True, stop=False),
                           reads=[("maskb", sb), "I4"], writes=[("pb", bank)])
                        for s_ in range(4):
                            h = 4 * g2 + s_
                            hp, par = h // 2, h % 2
                            kT = kz[g2][par]
                            op("pe", lambda e, bank=bank, s_=s_, kT=kT, kcs=kcs, hp=hp: e.matmul(
                                PB(bank)[:, s_ * 128:(s_ + 1) * 128], lhsT=kT[:, kcs], rhs=bqT[:, hp, qs], start=False, stop=(s_ == 3)),
                               reads=["bkT", "bqT"], writes=[("pb", bank)])
                        op("act", lambda e, bank=bank, pt=pt: e.activation(out=pt, in_=PB(bank), func=AF.Exp, scale=0.125),
                           reads=[("pb", bank)], writes=[("PT", g2, kb % 2)])
                        for s_ in range(4):
                            op("pe", lambda e, g2=g2, s_=s_, pt=pt, kb=kb: e.matmul(
                                PB(6 + g2)[:, s_ * 65:(s_ + 1) * 65], lhsT=pt[:, s_ * 128:(s_ + 1) * 128],
                                rhs=bv_tok[:, kb, g2 * 65:(g2 + 1) * 65], start=(kb == 0 and s_ == 0), stop=(kb == tt and s_ == 3)),
                               reads=[("PT", g2, kb % 2), "bv_tok"], writes=[("pb", 6 + g2)])
                op("act", lambda e: e.copy(out=ob[:, 0:260], in_=PB(6)[:, 0:260]), reads=[("pb", 6)], writes=["ob"])
                op("act", lambda e: e.copy(out=ob[:, 260:520], in_=PB(7)[:, 0:260]), reads=[("pb", 7)], writes=["ob"])
                obv = ob.rearrange("p (s e) -> p s e", e=65)
                op("dve", lambda e: e.reciprocal(out=rden, in_=obv[:, :, 64]), reads=["ob"], writes=["rden"])
                op("dve", lambda e: e.tensor_tensor(out=ob_b.rearrange("p (h d) -> p h d", h=8), in0=obv[:, :, 0:64],
                                                    in1=rden.unsqueeze(2).to_broadcast([128, 8, 64]), op=ALU.mult),
                   reads=["ob", "rden"], writes=["ob_b"])
                for q in range(4):
                    op("pe", lambda e, q=q: e.transpose(out=PBb(0)[:, q * 128:(q + 1) * 128], in_=ob_b[:, q * 128:(q + 1) * 128],
                                                        identity=ident_b), reads=["ob_b", "ident_b"], writes=[("pb", 0)])
                op("act", lambda e: e.copy(out=o_bT[:, :, qs], in_=PBb(0)[:, 0:512].rearrange("p (q t) -> p q t", q=4)),
                   reads=[("pb", 0)], writes=[("o_bT", tt)])

            scores_part(0, 0)
            for tt in range(NT):
                if tt + 1 < NT:
                    scores_part(tt + 1, (tt + 1) % 2)
                attn_part(tt, tt % 2)
            if "thr" in dbg:
                op("sp", None, reads=["dbg_thr"])
            if "score" in dbg:
                op("sp", None, reads=["dbg_score"])
            if "o_bT" in dbg:
                op("sp", lambda e: e.dma_start(out=dbg["o_bT"].rearrange("(a p) t -> p a t", p=128), in_=o_bT),
                   reads=[("o_bT", tt) for tt in range(NT)], writes=["dbg_o_bT"], dma=True)
                op("sp", None, reads=["dbg_o_bT"])

            sch.barrier()
            if stop_after == "dsa":
                return

            pa = MultiAlloc([(R_W, ARENA_BYTES)])
            hT2 = pa([128, 8, S], BF16)
            mergedT = pa([128, 8, S], BF16)
            x1 = pa([128, NT, D], F32)
            bgate = pa([128, 16], F32)
            g2col = pa([128, 8], F32)
            fng = pa([128, D], F32)
            wst2 = pa([128, 8, 256], F32)
            wg_bf = [pa([128, 8, 256], BF16) for _ in range(2)]
            wp_bf = [pa([128, 4, 256], BF16) for _ in range(2)]
            ga_s = pa([128, 512], BF16)
            gb_s = pa([128, 512], BF16)
            t1 = pa([128, 512], BF16)
            t2 = pa([128, 512], BF16)
            xt4 = [pa([128, D], F32) for _ in range(2)]
            op("sp", lambda e: e.dma_start(out=bgate, in_=bgate_d), writes=["bgate"], dma=True)
            op("sp", lambda e: e.dma_start(out=g2col, in_=g2_d), writes=["g2col"], dma=True)
            op("sp", lambda e: e.dma_start(out=fng, in_=fng_d.partition_broadcast(128)), writes=["fng"], dma=True)

            def phase1b():
                xa = Alloc(R_W + 64 * K, R_W + 128 * K)
                xt = [xa([128, D], F32) for _ in range(2)]
                hb = [xa([128, D], BF16) for _ in range(2)]
                junk = xa([128, D], BF16)
                ssx = xa([128, NT], F32)
                rsx = xa([128, NT], F32)
                op("dve", lambda e: e.memset(ssx, 0.0), writes=["ssx"])
                for tt in range(NT):
                    b = tt % 2
                    op("sp", lambda e, tt=tt, b=b: e.dma_start(out=xt[b], in_=x_d[tt * 128:(tt + 1) * 128, :]), writes=[("xt", b)], dma=True)
                    op("act", lambda e, tt=tt, b=b: e.activation(out=junk, in_=xt[b], func=AF.Square, accum_out=ssx[:, tt:tt + 1]),
                       reads=[("xt", b), "ssx"], writes=["junk", ("ssx", tt)])
                    op("act", lambda e, tt=tt: e.activation(out=rsx[:, tt:tt + 1], in_=ssx[:, tt:tt + 1], func=AF.Sqrt, bias=epsc,
                                                            scale=1.0 / D), reads=[("ssx", tt), "epsc"], writes=[("rsx", tt)])
                    op("dve", lambda e, tt=tt: e.reciprocal(out=rsx[:, tt:tt + 1], in_=rsx[:, tt:tt + 1]), reads=[("rsx", tt)],
                       writes=[("rsx", tt)])
                    op("dve", lambda e, tt=tt, b=b: e.tensor_scalar(out=hb[b], in0=xt[b], scalar1=rsx[:, tt:tt + 1], scalar2=None,
                                                                    op0=ALU.mult), reads=[("xt", b), ("rsx", tt)], writes=[("hb", b)])
                    bk = tt % 2
                    for k in range(8):
                        op("pe", lambda e, k=k, b=b, bk=bk: e.transpose(out=PBb(bk)[:, k * 128:(k + 1) * 128],
                                                                        in_=hb[b][:, k * 128:(k + 1) * 128], identity=ident_b),
                           reads=[("hb", b), "ident_b"], writes=[("pb", bk)])
                    op("act", lambda e, tt=tt, bk=bk: e.copy(out=hT2[:, :, tt * 128:(tt + 1) * 128],
                                                             in_=PBb(bk).rearrange("p (k t) -> p k t", k=8)),
                       reads=[("pb", bk)], writes=[("hT2", tt)])
            phase1b()
            sch.barrier()
            HT2 = [("hT2", tt) for tt in range(NT)]

            wpa_v = wpa_d.rearrange("(k p) c -> p k c", p=128)
            wpb_v = wpb_d.rearrange("(k p) c -> p k c", p=128)
            wout_v = wout_d.rearrange("(k p) c -> p k c", p=128)
            wi4 = [0]

            wst2b = xt4[0].rearrange("p (k c) -> p k c", k=4)
            wst2c = xt4[1].rearrange("p (k c) -> p k c", k=4)
            wi5 = [0]

            def load_gate(c0):
                i = wi4[0]
                wi4[0] += 1
                b = i % 2
                op("sp", lambda e: e.dma_start(out=wst2, in_=w_in_v[:, :, c0:c0 + 256]), writes=["wst2"], dma=True)
                op("pool", lambda e, b=b: e.tensor_tensor(out=wg_bf[b], in0=wst2, in1=g1col.unsqueeze(2).to_broadcast([128, 8, 256]),
                                                          op=ALU.mult), reads=["wst2", "g1col"], writes=[("wg", b)])
                return wg_bf[b], ("wg", b)

            def load_proj(src_v, c0):
                i = wi5[0]
                wi5[0] += 1
                b = i % 2
                st = wst2b if b == 0 else wst2c
                op("sp", lambda e: e.dma_start(out=st, in_=src_v[:, :, c0:c0 + 256]), writes=[("wstp", b)], dma=True)
                op("pool", lambda e, b=b: e.tensor_copy(out=wp_bf[b], in_=st), reads=[("wstp", b)], writes=[("wp", b)])
                return wp_bf[b], ("wp", b)

            p4u = [0]
            for j in range(4):
                wga, kga = load_gate(C_GA + j * 256)
                wgb, kgb = load_gate(C_GB + j * 256)
                wpa, kpa = load_proj(wpa_v, j * 256)
                wpb, kpb = load_proj(wpb_v, j * 256)
                for ct in range(2):
                    c = 2 * j + ct
                    ccs = slice(ct * 128, (ct + 1) * 128)
                    for tb in range(4):
                        tcs = slice(tb * 512, (tb + 1) * 512)
                        hk = HT2[tb * 4:tb * 4 + 4]
                        bA, bB, bC, bD = (2, 3, 4, 5) if (p4u[0] % 2 == 0) else (0, 1, 6, 7)
                        p4u[0] += 1
                        for k in range(8):
                            op("pe", lambda e, k=k, ccs=ccs, tcs=tcs, wga=wga, bA=bA: e.matmul(PB(bA), lhsT=wga[:, k, ccs], rhs=hT2[:, k, tcs],
                                                                                         start=(k == 0), stop=(k == 7)),
                               reads=[kga] + hk, writes=[("pb", bA)])
                        op("act", lambda e, c=c, bA=bA: e.activation(out=ga_s, in_=PB(bA), func=AF.Sigmoid, bias=bgate[:, c:c + 1], scale=1.0),
                           reads=[("pb", bA), "bgate"], writes=["ga_s"])
                        for k in range(8):
                            op("pe", lambda e, k=k, ccs=ccs, tcs=tcs, wgb=wgb, bB=bB: e.matmul(PB(bB), lhsT=wgb[:, k, ccs], rhs=hT2[:, k, tcs],
                                                                                         start=(k == 0), stop=(k == 7)),
                               reads=[kgb] + hk, writes=[("pb", bB)])
                        op("act", lambda e, c=c, bB=bB: e.activation(out=gb_s, in_=PB(bB), func=AF.Sigmoid, bias=bgate[:, 8 + c:9 + c], scale=1.0),
                           reads=[("pb", bB), "bgate"], writes=["gb_s"])
                        for hp in range(4):
                            op("pe", lambda e, hp=hp, ccs=ccs, tcs=tcs, wpa=wpa, bC=bC: e.matmul(PB(bC), lhsT=wpa[:, hp, ccs], rhs=o_aT[:, hp, tcs],
                                                                                           start=(hp == 0), stop=(hp == 3)),
                               reads=[kpa, "o_aT"], writes=[("pb", bC)])
                        for hp in range(4):
                            op("pe", lambda e, hp=hp, ccs=ccs, tcs=tcs, wpb=wpb, bD=bD: e.matmul(PB(bD), lhsT=wpb[:, hp, ccs], rhs=o_bT[:, hp, tcs],
                                                                                           start=(hp == 0), stop=(hp == 3)),
                               reads=[kpb, "o_bT"], writes=[("pb", bD)])
                        op("dve", lambda e, bC=bC: e.tensor_tensor(out=t1, in0=PB(bC), in1=ga_s, op=ALU.mult), reads=[("pb", bC), "ga_s"],
                           writes=["t1"])
                        op("dve", lambda e, bD=bD: e.tensor_tensor(out=t2, in0=PB(bD), in1=gb_s, op=ALU.mult), reads=[("pb", bD), "gb_s"],
                           writes=["t2"])
                        op("pool", lambda e, c=c, tcs=tcs: e.tensor_tensor(out=mergedT[:, c, tcs], in0=t1, in1=t2, op=ALU.add),
                           reads=["t1", "t2"], writes=[("mergedT", c, tb)])
            if "mergedT" in dbg:
                op("sp", lambda e: e.dma_start(out=dbg["mergedT"].rearrange("(a p) t -> p a t", p=128), in_=mergedT),
                   reads=[("mergedT", c, tb) for c in range(8) for tb in range(4)], writes=["dbg_mergedT"], dma=True)
                op("sp", None, reads=["dbg_mergedT"])
            sch.barrier()
            wout_bf = view(R_A, [128, 8, D], BF16)
            for j in range(4):
                op("sp", lambda e, j=j: e.dma_start(out=wst2, in_=wout_v[:, :, j * 256:(j + 1) * 256]), writes=["wst2"], dma=True)
                op("pool", lambda e, j=j: e.tensor_copy(out=wout_bf[:, :, j * 256:(j + 1) * 256], in_=wst2), reads=["wst2"],
                   writes=[("wout", j)])
            WOUT = [("wout", j) for j in range(4)]
            for tt in range(NT):
                b = tt % 2
                op("sp", lambda e, tt=tt, b=b: e.dma_start(out=xt4[b], in_=x_d[tt * 128:(tt + 1) * 128, :]), writes=[("xt4", b)], dma=True)
                for nb in range(2):
                    bk = 2 + 2 * b + nb
                    for c in range(8):
                        op("pe", lambda e, c=c, tt=tt, nb=nb, bk=bk: e.matmul(PB(bk), lhsT=mergedT[:, c, tt * 128:(tt + 1) * 128],
                                                                               rhs=wout_bf[:, c, nb * 512:(nb + 1) * 512],
                                                                               start=(c == 0), stop=(c == 7)),
                           reads=WOUT + ["mergedT_all"], writes=[("pb", bk)])
                    op("dve", lambda e, tt=tt, nb=nb, bk=bk, b=b: e.tensor_tensor(out=x1[:, tt, nb * 512:(nb + 1) * 512], in0=PB(bk),
                                                                                   in1=xt4[b][:, nb * 512:(nb + 1) * 512], op=ALU.add),
                       reads=[("pb", bk), ("xt4", b)], writes=[("x1", tt)])
            if "x1" in dbg:
                op("sp", lambda e: e.dma_start(out=dbg["x1"].rearrange("(t p) c -> p t c", p=128), in_=x1),
                   reads=[("x1", tt) for tt in range(NT)], writes=["dbg_x1"], dma=True)
                op("sp", None, reads=["dbg_x1"])
            sch.barrier()
            if stop_after == "p4":
                return

            h2T = view(R_W, [128, 8, S], BF16)
            ma = MultiAlloc([(R_W + 32 * K, R_W + 64 * K), (R_A, R_A + 32 * K)])
            tail_off = None
            hb2 = [ma([128, D], BF16) for _ in range(2)]
            junk2 = ma([128, D], BF16)
            ss5 = ma([128, NT], F32)
            rs5 = ma([128, NT], F32)
            wr_st = ma([128, 8, 20], F32)
            wr_bf = ma([128, 8, 20], BF16)
            brow = ma([128, 20], F32)
            lg = ma([128, 20], F32)
            sm = {n_: ma([128, 4], F32) for n_ in ("goh", "gex", "elg", "oh1", "msk", "oh2", "wsel")}
            sc1 = {n_: ma([128, 1], F32) for n_ in ("gmax", "ngmax", "gsum", "ggate", "m1", "m2", "d21", "e21", "den", "w1", "w2")}
            tmp44 = ma([128, 4, 4], F32)
            comb_b = ma([128, 16], BF16)
            combT = ma([128, S], BF16)
            sel16 = ma([128, 16, 128], BF16)
            est = ma([128, 8, 256], F32)
            w1b = [ma([128, 8, 256], BF16) for _ in range(2)]
            w3b = [ma([128, 8, 256], BF16) for _ in range(2)]
            w2b = [ma([128, 2, D], BF16) for _ in range(2)]
            sg = [[ma([128, 512], BF16) for _ in range(2)] for _ in range(2)]
            cbt = [ma([128, 512], BF16) for _ in range(2)]
            tu = [[ma([128, 512], BF16) for _ in range(2)] for _ in range(2)]
            actT = [[ma([128, 512], BF16) for _ in range(2)] for _ in range(2)]
            op("sp", lambda e: e.dma_start(out=wr_st, in_=wr_d.rearrange("(k p) c -> p k c", p=128)), writes=["wr_st"], dma=True)
            op("sp", lambda e: e.dma_start(out=brow, in_=br_d.partition_broadcast(128)), writes=["brow"], dma=True)
            op("pool", lambda e: e.tensor_tensor(out=wr_bf, in0=wr_st, in1=g2col.unsqueeze(2).to_broadcast([128, 8, 20]), op=ALU.mult),
               reads=["wr_st", "g2col"], writes=["wr_bf"])
            op("pool", lambda e: e.memset(sel16[0:16, :, :], 1.0), writes=["sel16"])
            op("pool", lambda e: e.affine_select(out=sel16[0:16, :, :], in_=sel16[0:16, :, :], pattern=[[-1, 16], [0, 128]],
                                                 compare_op=ALU.is_equal, fill=0.0, base=0, channel_multiplier=1), writes=["sel16"])
            op("dve", lambda e: e.memset(ss5, 0.0), writes=["ss5"])
            def prep_tile(tt):
                b = tt % 2
                bk = tt % 2
                op("act", lambda e, tt=tt: e.activation(out=junk2, in_=x1[:, tt, :], func=AF.Square, accum_out=ss5[:, tt:tt + 1]),
                   reads=["x1_all", "ss5"], writes=["junk2", ("ss5", tt)])
                op("act", lambda e, tt=tt: e.activation(out=rs5[:, tt:tt + 1], in_=ss5[:, tt:tt + 1], func=AF.Ln, bias=epsc,
                                                        scale=1.0 / D), reads=[("ss5", tt), "epsc"], writes=[("rs5", tt)])
                op("act", lambda e, tt=tt: e.activation(out=rs5[:, tt:tt + 1], in_=rs5[:, tt:tt + 1], func=AF.Exp, scale=-0.5),
                   reads=[("rs5", tt)], writes=[("rs5", tt)])
                op("dve", lambda e, tt=tt, b=b: e.tensor_scalar(out=hb2[b], in0=x1[:, tt, :], scalar1=rs5[:, tt:tt + 1], scalar2=None,
                                                                op0=ALU.mult), reads=["x1_all", ("rs5", tt)], writes=[("hb2", b)])
                for k in range(8):
                    op("pe", lambda e, k=k, b=b, bk=bk: e.transpose(out=PBb(bk)[:, k * 128:(k + 1) * 128],
                                                                    in_=hb2[b][:, k * 128:(k + 1) * 128], identity=ident_b),
                       reads=[("hb2", b), "ident_b"], writes=[("pb", bk)])
                op("act", lambda e, tt=tt, bk=bk: e.copy(out=h2T[:, :, tt * 128:(tt + 1) * 128],
                                                         in_=PBb(bk).rearrange("p (k t) -> p k t", k=8)),
                   reads=[("pb", bk)], writes=[("h2T", tt)])
                for k in range(8):
                    op("pe", lambda e, k=k, tt=tt: e.matmul(PB(2)[:, 0:20], lhsT=h2T[:, k, tt * 128:(tt + 1) * 128], rhs=wr_bf[:, k, :],
                                                            start=(k == 0), stop=(k == 7)),
                       reads=[("h2T", tt), "wr_bf"], writes=[("pb", 2)])
                R = []

                def rop(fn, rd, wr):
                    op("dve", fn, reads=rd, writes=wr)
                rop(lambda e: e.tensor_tensor(out=lg, in0=PB(2)[:, 0:20], in1=brow, op=ALU.add), [("pb", 2), "brow"], ["lg"])
                elv = lg[:, 4:20].rearrange("p (g x) -> p g x", g=4)
                rop(lambda e: e.tensor_reduce(out=sc1["gmax"], in_=lg[:, 0:4], axis=AX.X, op=ALU.max), ["lg"], ["gmax"])
                rop(lambda e: e.tensor_scalar(out=sm["goh"], in0=lg[:, 0:4], scalar1=sc1["gmax"][:, 0:1], scalar2=None,
                                              op0=ALU.is_equal), ["lg", "gmax"], ["goh"])
                rop(lambda e: e.tensor_scalar(out=sc1["ngmax"], in0=sc1["gmax"], scalar1=-1.0, scalar2=None, op0=ALU.mult),
                    ["gmax"], ["ngmax"])
                op("act", lambda e: e.activation(out=sm["gex"], in_=lg[:, 0:4], func=AF.Exp, bias=sc1["ngmax"][:, 0:1], scale=1.0),
                   reads=["lg", "ngmax"], writes=["gex"])
                rop(lambda e: e.tensor_reduce(out=sc1["gsum"], in_=sm["gex"], axis=AX.X, op=ALU.add), ["gex"], ["gsum"])
                rop(lambda e: e.reciprocal(out=sc1["ggate"], in_=sc1["gsum"]), ["gsum"], ["ggate"])
                rop(lambda e: e.tensor_tensor(out=tmp44, in0=elv, in1=sm["goh"].unsqueeze(2).to_broadcast([128, 4, 4]), op=ALU.mult),
                    ["lg", "goh"], ["tmp44"])
                rop(lambda e: e.tensor_reduce(out=sm["elg"], in_=tmp44.rearrange("p g x -> p x g"), axis=AX.X, op=ALU.add),
                    ["tmp44"], ["elg"])
                rop(lambda e: e.tensor_reduce(out=sc1["m1"], in_=sm["elg"], axis=AX.X, op=ALU.max), ["elg"], ["m1"])
                rop(lambda e: e.tensor_scalar(out=sm["oh1"], in0=sm["elg"], scalar1=sc1["m1"][:, 0:1], scalar2=None, op0=ALU.is_equal),
                    ["elg", "m1"], ["oh1"])
                rop(lambda e: e.scalar_tensor_tensor(out=sm["msk"], in0=sm["oh1"], scalar=-1.0e30, in1=sm["elg"], op0=ALU.mult,
                                                     op1=ALU.add), ["oh1", "elg"], ["msk"])
                rop(lambda e: e.tensor_reduce(out=sc1["m2"], in_=sm["msk"], axis=AX.X, op=ALU.max), ["msk"], ["m2"])
                rop(lambda e: e.tensor_scalar(out=sm["oh2"], in0=sm["msk"], scalar1=sc1["m2"][:, 0:1], scalar2=None, op0=ALU.is_equal),
                    ["msk", "m2"], ["oh2"])
                rop(lambda e: e.tensor_tensor(out=sc1["d21"], in0=sc1["m2"], in1=sc1["m1"], op=ALU.subtract), ["m1", "m2"], ["d21"])
                op("act", lambda e: e.activation(out=sc1["e21"], in_=sc1["d21"], func=AF.Exp), reads=["d21"], writes=["e21"])
                rop(lambda e: e.tensor_scalar(out=sc1["den"], in0=sc1["e21"], scalar1=1.0, scalar2=None, op0=ALU.add), ["e21"], ["den"])
                rop(lambda e: e.reciprocal(out=sc1["den"], in_=sc1["den"]), ["den"], ["den"])
                rop(lambda e: e.tensor_tensor(out=sc1["w1"], in0=sc1["ggate"], in1=sc1["den"], op=ALU.mult), ["ggate", "den"], ["w1"])
                rop(lambda e: e.tensor_tensor(out=sc1["w2"], in0=sc1["w1"], in1=sc1["e21"], op=ALU.mult), ["w1", "e21"], ["w2"])
                rop(lambda e: e.tensor_scalar(out=sm["wsel"], in0=sm["oh1"], scalar1=sc1["w1"][:, 0:1], scalar2=None, op0=ALU.mult),
                    ["oh1", "w1"], ["wsel"])
                rop(lambda e: e.scalar_tensor_tensor(out=sm["wsel"], in0=sm["oh2"], scalar=sc1["w2"][:, 0:1], in1=sm["wsel"],
                                                     op0=ALU.mult, op1=ALU.add), ["oh2", "w2", "wsel"], ["wsel"])
                rop(lambda e: e.tensor_tensor(out=comb_b.rearrange("p (g x) -> p g x", g=4),
                                              in0=sm["goh"].unsqueeze(2).to_broadcast([128, 4, 4]),
                                              in1=sm["wsel"].unsqueeze(1).to_broadcast([128, 4, 4]), op=ALU.mult),
                    ["goh", "wsel"], ["comb_b"])
                if "comb" in dbg:
                    op("sp", lambda e, tt=tt: e.dma_start(out=dbg["comb"][tt * 128:(tt + 1) * 128, :], in_=comb_b), reads=["comb_b"],
                       writes=["dbg_comb"], dma=True)
                op("pe", lambda e: e.transpose(out=PBb(3)[0:16, 0:128], in_=comb_b, identity=ident_b), reads=["comb_b", "ident_b"],
                   writes=[("pb", 3)])
                op("act", lambda e, tt=tt: e.copy(out=combT[0:16, tt * 128:(tt + 1) * 128], in_=PBb(3)[0:16, 0:128]),
                   reads=[("pb", 3)], writes=[("combT", tt)])
            for tt in range(4):
                prep_tile(tt)
            H2T = [("h2T", tt) for tt in range(NT)]
            CT = [("combT", tt) for tt in range(NT)]

            def load_expert(e_i):
                b = e_i % 2
                for (src, dst, nm, fold) in ((w1_d, w1b[b], "w1", True), (w3_d, w3b[b], "w3", True)):
                    op("sp", lambda e, src=src: e.dma_start(out=est, in_=src[e_i].rearrange("(k p) f -> p k f", p=128)),
                       writes=["est"], dma=True)
                    op("pool", lambda e, dst=dst: e.tensor_tensor(out=dst, in0=est, in1=g2col.unsqueeze(2).to_broadcast([128, 8, 256]),
                                                                  op=ALU.mult), reads=["est", "g2col"], writes=[(nm, b)])
                op("sp", lambda e: e.dma_start(out=est.rearrange("p k f -> p (k f)").rearrange("p (a c) -> p a c", a=2),
                                               in_=w2_d[e_i].rearrange("(a p) c -> p a c", p=128)), writes=["est"], dma=True)
                op("pool", lambda e: e.tensor_copy(out=w2b[b], in_=est.rearrange("p k f -> p (k f)").rearrange("p (a c) -> p a c", a=2)),
                   reads=["est"], writes=[("w2", b)])

            def stageA(e_i, tb, sl):
                b = e_i % 2
                tcs = slice(tb * 512, (tb + 1) * 512)
                hk = H2T[tb * 4:tb * 4 + 4]
                cbk_ = 6
                op("pe", lambda e: e.matmul(PB(cbk_), lhsT=sel16[0:16, e_i, :], rhs=combT[0:16, tcs], start=True, stop=True),
                   reads=["sel16"] + CT[tb * 4:tb * 4 + 4], writes=[("pb", cbk_)])
                op("act", lambda e: e.copy(out=cbt[sl], in_=PB(cbk_)), reads=[("pb", cbk_)], writes=[("cbt", sl)])
                for ft in range(2):
                    fcs = slice(ft * 128, (ft + 1) * 128)
                    for k in range(8):
                        op("pe", lambda e, k=k, fcs=fcs, ft=ft: e.matmul(PB(2 + ft), lhsT=w1b[b][:, k, fcs], rhs=h2T[:, k, tcs],
                                                                         start=(k == 0), stop=(k == 7)),
                           reads=[("w1", b)] + hk, writes=[("pb", 2 + ft)])
                    op("act", lambda e, ft=ft: e.activation(out=sg[sl][ft], in_=PB(2 + ft), func=AF.Silu), reads=[("pb", 2 + ft)],
                       writes=[("sg", sl, ft)])
                    yield
                    for k in range(8):
                        op("pe", lambda e, k=k, fcs=fcs, ft=ft: e.matmul(PB(4 + ft), lhsT=w3b[b][:, k, fcs], rhs=h2T[:, k, tcs],
                                                                         start=(k == 0), stop=(k == 7)),
                           reads=[("w3", b)] + hk, writes=[("pb", 4 + ft)])
                    op("dve", lambda e, ft=ft: e.tensor_tensor(out=tu[sl][ft], in0=PB(4 + ft), in1=sg[sl][ft], op=ALU.mult),
                       reads=[("pb", 4 + ft), ("sg", sl, ft)], writes=[("tu", sl, ft)], fast=True)
                    op("pool", lambda e, ft=ft: e.tensor_tensor(out=actT[sl][ft], in0=tu[sl][ft], in1=cbt[sl], op=ALU.mult),
                       reads=[("tu", sl, ft), ("cbt", sl)], writes=[("actT", sl, ft)])
                    yield

            ybank = [0]

            def stageB(e_i, tb, sl):
                b = e_i % 2
                for t4 in range(4):
                    tt = tb * 4 + t4
                    for nb in range(2):
                        bk = (0, 1, 7)[ybank[0] % 3]
                        ybank[0] += 1
                        for ft in range(2):
                            op("pe", lambda e, ft=ft, t4=t4, nb=nb, bk=bk: e.matmul(
                                PB(bk), lhsT=actT[sl][ft][:, t4 * 128:(t4 + 1) * 128], rhs=w2b[b][:, ft, nb * 512:(nb + 1) * 512],
                                start=(ft == 0), stop=(ft == 1)), reads=[("actT", sl, ft), ("w2", b)], writes=[("pb", bk)])
                        op("dve", lambda e, tt=tt, nb=nb, bk=bk: e.tensor_tensor(out=x1[:, tt, nb * 512:(nb + 1) * 512],
                                                                                 in0=PB(bk), in1=x1[:, tt, nb * 512:(nb + 1) * 512],
                                                                                 op=ALU.add),
                           reads=[("pb", bk), ("x2", tt, nb)], writes=[("x2", tt, nb)], fast=True)
                        if nb == 1:
                            yield

            def drain(g_):
                for _ in g_:
                    pass

            units = [(e_i, tb) for e_i in range(16) for tb in range(4)]
            load_expert(0)
            load_expert(1)
            drain(stageA(units[0][0], units[0][1], 0))
            for u, (e_i, tb) in enumerate(units):
                gb = stageB(e_i, tb, u % 2)
                if u + 1 < len(units):
                    ne, ntb = units[u + 1]
                    if ne == 0:
                        for tt in range(4 * ntb, 4 * ntb + 4):
                            prep_tile(tt)
                    ga_ = stageA(ne, ntb, (u + 1) % 2)
                    drain(ga_)
                drain(gb)
                if tb == 3 and e_i + 2 < 16:
                    load_expert(e_i + 2)
            sch.barrier()
            if "x2" in dbg:
                op("sp", lambda e: e.dma_start(out=dbg["x2"].rearrange("(t p) c -> p t c", p=128), in_=x1), writes=["dbg_x2"], dma=True)
                op("sp", None, reads=["dbg_x2"])

            fa = MultiAlloc([(R_W, R_W + 64 * K)])
            ss6 = fa([128, NT], F32)
            rs6 = fa([128, NT], F32)
            junk6 = fa([128, D], BF16)
            yo = [fa([128, D], F32) for _ in range(2)]
            op("dve", lambda e: e.memset(ss6, 0.0), writes=["ss6"])
            for tt in range(NT):
                b = tt % 2
                op("act", lambda e, tt=tt: e.activation(out=junk6, in_=x1[:, tt, :], func=AF.Square, accum_out=ss6[:, tt:tt + 1]),
                   reads=["ss6"], writes=["junk6", ("ss6", tt)])
                op("act", lambda e, tt=tt: e.activation(out=rs6[:, tt:tt + 1], in_=ss6[:, tt:tt + 1], func=AF.Sqrt, bias=epsc,
                                                        scale=1.0 / D), reads=[("ss6", tt), "epsc"], writes=[("rs6", tt)])
                op("dve", lambda e, tt=tt: e.reciprocal(out=rs6[:, tt:tt + 1], in_=rs6[:, tt:tt + 1]), reads=[("rs6", tt)],
                   writes=[("rs6", tt)])
                op("dve", lambda e, tt=tt, b=b: e.scalar_tensor_tensor(out=yo[b], in0=x1[:, tt, :], scalar=rs6[:, tt:tt + 1], in1=fng,
                                                                       op0=ALU.mult, op1=ALU.mult),
                   reads=[("rs6", tt), "fng"], writes=[("yo", b)])
                op("sp", lambda e, tt=tt, b=b: e.dma_start(out=out_d[tt * 128:(tt + 1) * 128, :], in_=yo[b]), reads=[("yo", b)],
                   writes=[("out", tt)], dma=True)
            op("sp", None, reads=[("out", tt) for tt in range(NT)])


        body()
        sch.barrier()
        DEBUG["stats_pre"] = {e: len(sch.ops[e]) for e in Sched.ENGS}
        with nc.Block() as block:
            sch.emit(nc, block, engsem, dmasem)
        DEBUG["stats"] = sch.stats
    return nc


_NC_CACHE = {}


def kernel(**inputs):
    dbg = tuple(DEBUG.get("outputs", ()))
    key = (dbg, DEBUG.get("stop_after"))
    if key not in _NC_CACHE:
        _NC_CACHE[key] = build_nc(dbg, DEBUG.get("stop_after"))
    nc = _NC_CACHE[key]
    n = 8
    x = np.ascontiguousarray(inputs["x"], dtype=np.float32)
    posn = np.ascontiguousarray(inputs["positions"], dtype=np.int32)
    f32 = lambda a: np.ascontiguousarray(a, dtype=np.float32)
    inv = (10000.0 ** (-np.arange(32, dtype=np.float32) / np.float32(32))).astype(np.float32).reshape(1, 32)
    shared = {
        "norm1_g": f32(inputs["norm1_g"][0].reshape(8, 128).T),
        "w_in": f32(inputs["w_in"][0]),
        "conv_w": f32(inputs["conv_w"][0].reshape(4, 12, 128).transpose(2, 1, 0).reshape(128, 48)),
        "inv_freq": inv,
        "a_log": f32(inputs["a_log"][0].reshape(1, 8)),
        "dt_bias": f32(inputs["dt_bias"][0].reshape(1, 8)),
        "a_norm_g": f32(inputs["a_norm_g"][0].reshape(1, 64)),
        "b_gate": f32(inputs["b_gate"][0].reshape(16, 128).T),
        "norm2_g": f32(inputs["norm2_g"][0].reshape(8, 128).T),
        "final_norm_g": f32(inputs["final_norm_g"].reshape(1, D)),
        "w_proj_a": f32(inputs["w_proj_a"][0]),
        "w_proj_b": f32(inputs["w_proj_b"][0]),
        "w_out": f32(inputs["w_out"][0]),
        "w_router": f32(np.concatenate([inputs["w_router_group"][0], inputs["w_router_expert"][0]], axis=1)),
        "b_router": f32(np.concatenate([inputs["b_router_group"][0], inputs["b_router_expert"][0]], axis=0).reshape(1, 20)),
        "w_exp_gate": f32(inputs["w_exp_gate"][0]),
        "w_exp_up": f32(inputs["w_exp_up"][0]),
        "w_exp_down": f32(inputs["w_exp_down"][0]),
    }
    in_maps = []
    for c in range(n):
        m = dict(shared)
        m["x"] = x[c]
        m["positions"] = np.ascontiguousarray(posn[c].reshape(NT, 128).T)
        in_maps.append(m)
    res = run_bass_kernel_spmd(nc, in_maps, core_ids=list(range(n)))
    DEBUG["results"] = res.results
    return np.stack([r["out"] for r in res.results], axis=0)
```

```python
import math
from contextlib import ExitStack
import numpy as np
import concourse.bass as bass
import concourse.mybir as mybir
from concourse.bass_utils import run_bass_kernel_spmd

F32 = mybir.dt.float32
BF16 = mybir.dt.bfloat16
I32 = mybir.dt.int32
AF = mybir.ActivationFunctionType
ALU = mybir.AluOpType
AX = mybir.AxisListType

S = 2048
D = 1024
NT = S // 128
D_IN = 5464
EPS = 1e-6
N_DMA_SEMS = 24
NEG = -30000.0
NBIS = 12
TWO_PI = 2.0 * math.pi

C_AQ, C_AK, C_AV, C_AZ = 0, 512, 1024, 1536
C_BETA, C_ALPHA = 2048, 2056
C_BQ, C_BK, C_BV = 2064, 2576, 2704
C_IQ, C_IK, C_IW = 2832, 3344, 3408
C_GA, C_GB = 3416, 4440

DEBUG = {}
STRICT_SAME_ENGINE = True


class Sched:
    ENGS = ("pe", "act", "dve", "pool", "sp")

    def __init__(self):
        self.ops = {e: [] for e in self.ENGS}
        self.last_w = {}
        self.readers = {}
        self.dma_rr = 0
        self.dma_count = [0] * N_DMA_SEMS

    def op(self, eng, fn, reads=(), writes=(), dma=False, fast=False):
        deps = set()
        raw = set()
        for k in reads:
            t = self.last_w.get(k)
            if t is not None:
                deps.add(t)
                raw.add(t)
        for k in writes:
            t = self.last_w.get(k)
            if t is not None:
                deps.add(t)
            for t in self.readers.get(k, {}).values():
                deps.add(t)
        idx = len(self.ops[eng])
        if dma:
            si = self.dma_rr
            self.dma_rr = (self.dma_rr + 1) % N_DMA_SEMS
            prev = self.dma_count[si]
            if prev > 0:
                deps.add(("dma", si, prev))
            self.dma_count[si] = prev + 1
            tok = ("dma", si, prev + 1)
            rkey = ("dma", si)
        else:
            tok = ("eng", eng, idx)
            rkey = eng
            if STRICT_SAME_ENGINE:
                deps = {t for t in deps if not (t[0] == "eng" and t[1] == eng) or eng != "pe"}
            else:
                deps = {t for t in deps if not (t[0] == "eng" and t[1] == eng)
                        or (t in raw and eng != "pe" and not fast and idx - t[2] <= 8)}
        self.ops[eng].append(dict(fn=fn, deps=deps, signal=False, dma=(tok if dma else None)))
        for k in writes:
            self.last_w[k] = tok
            self.readers[k] = {}
        for k in reads:
            if k in writes:
                continue
            self.readers.setdefault(k, {})[rkey] = tok
        return tok

    def barrier(self):
        toks = set()
        for e in self.ENGS:
            j = len(self.ops[e]) - 1
            while j >= 0 and (self.ops[e][j]["fn"] is None or self.ops[e][j]["dma"] is not None):
                j -= 1
            if j >= 0:
                toks.add(("eng", e, j))
        for si in range(N_DMA_SEMS):
            if self.dma_count[si] > 0:
                toks.add(("dma", si, self.dma_count[si]))
        for e in self.ENGS:
            deps = {t for t in toks if not (t[0] == "eng" and t[1] == e and (e == "pe" or not STRICT_SAME_ENGINE))}
            self.ops[e].append(dict(fn=None, deps=deps, signal=False, dma=None))
        self.last_w = {}
        self.readers = {}

    def emit(self, nc, block, engsem, dmasem):
        for e in self.ENGS:
            for o in self.ops[e]:
                for t in o["deps"]:
                    if t[0] == "eng":
                        self.ops[t[1]][t[2]]["signal"] = True
        sigcount = {}
        for e in self.ENGS:
            c = 0
            lst = []
            for o in self.ops[e]:
                if o["signal"]:
                    c += 1
                lst.append(c)
            sigcount[e] = lst
        self.stats = {e: (len(self.ops[e]), sigcount[e][-1] if sigcount[e] else 0) for e in self.ENGS}

        def run(e, eng):
            waited = {}
            for o in self.ops[e]:
                need = {}
                for t in o["deps"]:
                    if t[0] == "eng":
                        key = ("eng", t[1])
                        val = sigcount[t[1]][t[2]]
                    else:
                        key = ("dma", t[1])
                        val = 16 * t[2]
                    if val > need.get(key, 0):
                        need[key] = val
                for key, val in need.items():
                    if waited.get(key, 0) >= val:
                        continue
                    waited[key] = val
                    sem = engsem[key[1]] if key[0] == "eng" else dmasem[key[1]]
                    eng.wait_ge(sem, val)
                if o["fn"] is None:
                    continue
                inst = o["fn"](eng)
                if o["dma"] is not None:
                    inst.then_inc(dmasem[o["dma"][1]], 16)
                elif o["signal"]:
                    inst.then_inc(engsem[e], 1)

        @block.tensor
        def _(eng):
            run("pe", eng)

        @block.scalar
        def _(eng):
            run("act", eng)

        @block.vector
        def _(eng):
            run("dve", eng)

        @block.gpsimd
        def _(eng):
            run("pool", eng)

        @block.sync
        def _(eng):
            run("sp", eng)


DT_SIZE = {F32: 4, BF16: 2, I32: 4}


def build_nc(debug=(), stop_after=None):
    nc = bass.Bass("TRN2", target_bir_lowering=False)

    def din(name, shape, dt=F32):
        return nc.dram_tensor(name, list(shape), dt, kind="ExternalInput").ap()

    x_d = din("x", [S, D])
    pos_d = din("positions", [128, NT], I32)
    g1_d = din("norm1_g", [128, 8])
    w_in_d = din("w_in", [D, D_IN])
    convw_d = din("conv_w", [128, 48])
    invf_d = din("inv_freq", [1, 32])
    alog_d = din("a_log", [1, 8])
    dtb_d = din("dt_bias", [1, 8])
    ang_d = din("a_norm_g", [1, 64])
    bgate_d = din("b_gate", [128, 16])
    g2_d = din("norm2_g", [128, 8])
    fng_d = din("final_norm_g", [1, D])
    wpa_d = din("w_proj_a", [512, D])
    wpb_d = din("w_proj_b", [512, D])
    wout_d = din("w_out", [D, D])
    wr_d = din("w_router", [D, 20])
    br_d = din("b_router", [1, 20])
    w1_d = din("w_exp_gate", [16, D, 256])
    w3_d = din("w_exp_up", [16, D, 256])
    w2_d = din("w_exp_down", [16, 256, D])
    out_d = nc.dram_tensor("out", [S, D], F32, kind="ExternalOutput").ap()
    dbg = {}
    for name, shape, dt in debug:
        dbg[name] = nc.dram_tensor("dbg_" + name, list(shape), dt, kind="ExternalOutput").ap()
    w_in_v = w_in_d.rearrange("(k p) c -> p k c", p=128)

    sch = Sched()
    op = sch.op
    es = ExitStack()
    with es:
        ARENA_BYTES = 207 * 1024
        arena = es.enter_context(nc.sbuf_tensor("arena", [128, ARENA_BYTES // 4], F32))

        def view(off, shape, dt):
            n = 1
            for s_ in shape[1:]:
                n *= s_
            size = n * DT_SIZE[dt]
            assert off % 4 == 0 and size % 4 == 0 and off + size <= ARENA_BYTES, (off, size)
            ap = arena[:, off // 4:(off + size) // 4]
            if dt != F32:
                ap = ap.bitcast(dt)
            if len(shape) == 3:
                ap = ap.rearrange("p (a b) -> p a b", a=shape[1])
            elif len(shape) == 4:
                ap = ap.rearrange("p (a b c) -> p a b c", a=shape[1], b=shape[2])
            return ap

        class Alloc:
            def __init__(self, base, limit):
                self.off = base
                self.limit = limit

            def __call__(self, shape, dt):
                n = 1
                for s_ in shape[1:]:
                    n *= s_
                size = (n * DT_SIZE[dt] + 63) // 64 * 64
                self.off = (self.off + 63) // 64 * 64
                v = view(self.off, shape, dt)
                self.off += size
                assert self.off <= self.limit, (self.off, self.limit)
                return v

        pbank = [es.enter_context(nc.psum_tensor("pb%d" % i, [128, 512], F32)) for i in range(8)]
        engsem = {e: es.enter_context(nc.semaphore("sem_" + e)) for e in Sched.ENGS}
        dmasem = [es.enter_context(nc.semaphore("dsem%d" % i)) for i in range(N_DMA_SEMS)]

        def PB(i):
            return pbank[i][:]

        def PBb(i):
            return pbank[i][:].bitcast(BF16)

        def body():
            K = 1024
            ca = Alloc(0, 9 * K)
            ident_f = ca([128, 128], F32)
            ident_b = ca([128, 128], BF16)
            ucs_f = ca([128, 128], F32)
            mc0_f = ca([128, 128], F32)
            mc1_f = ca([128, 128], F32)
            maskneg_f = ca([128, 128], F32)
            strict_b = ca([128, 128], BF16)
            g1col = ca([128, 8], F32)
            epsc = ca([128, 1], F32)
            cw = ca([128, 48], F32)
            invf = ca([128, 32], F32)
            dtb = ca([128, 8], F32)
            negA = ca([128, 8], F32)
            angb = ca([128, 64], F32)
            posi = ca([128, NT], I32)
            posf = ca([128, NT], F32)
            cs = ca([128, NT, 64], F32)
            assert ca.off <= 9 * K, ca.off

            op("pool", lambda e: e.memset(ident_f, 1.0), writes=["ident_f"])
            op("pool", lambda e: e.affine_select(out=ident_f, in_=ident_f, pattern=[[-1, 128]], compare_op=ALU.is_equal,
                                                 fill=0.0, base=0, channel_multiplier=1), writes=["ident_f"])
            op("pool", lambda e: e.tensor_copy(out=ident_b, in_=ident_f), reads=["ident_f"], writes=["ident_b"])
            op("pool", lambda e: e.memset(ucs_f, 1.0), writes=["ucs_f"])
            op("pool", lambda e: e.affine_select(out=ucs_f, in_=ucs_f, pattern=[[1, 128]], compare_op=ALU.is_ge,
                                                 fill=0.0, base=0, channel_multiplier=-1), writes=["ucs_f"])
            op("pool", lambda e: e.memset(ucs_f[0:64, 64:128], 0.0), writes=["ucs_f"])
            op("pool", lambda e: e.memset(mc0_f, 0.0), writes=["mc0_f"])
            op("pool", lambda e: e.memset(mc0_f[0:64, :], 1.0), writes=["mc0_f"])
            op("pool", lambda e: e.memset(mc1_f, 0.0), writes=["mc1_f"])
            op("pool", lambda e: e.memset(mc1_f[64:128, :], 1.0), writes=["mc1_f"])
            op("pool", lambda e: e.memset(maskneg_f, 0.0), writes=["maskneg_f"])
            op("pool", lambda e: e.affine_select(out=maskneg_f, in_=maskneg_f, pattern=[[-1, 128]], compare_op=ALU.is_ge,
                                                 fill=NEG, base=0, channel_multiplier=1), writes=["maskneg_f"])
            op("pool", lambda e: e.memset(maskneg_f[64:128, 0:64], NEG), writes=["maskneg_f"])
            op("pool", lambda e: e.memset(strict_b, 1.0), writes=["strict_b"])
            op("pool", lambda e: e.affine_select(out=strict_b, in_=strict_b, pattern=[[-1, 128]], compare_op=ALU.is_gt,
                                                 fill=0.0, base=0, channel_multiplier=1), writes=["strict_b"])
            op("pool", lambda e: e.memset(strict_b[64:128, 0:64], 0.0), writes=["strict_b"])
            op("dve", lambda e: e.memset(epsc, EPS), writes=["epsc"])
            op("sp", lambda e: e.dma_start(out=g1col, in_=g1_d), writes=["g1col"], dma=True)
            op("sp", lambda e: e.dma_start(out=cw, in_=convw_d), writes=["cw"], dma=True)
            op("sp", lambda e: e.dma_start(out=invf, in_=invf_d.partition_broadcast(128)), writes=["invf"], dma=True)
            op("sp", lambda e: e.dma_start(out=dtb, in_=dtb_d.partition_broadcast(128)), writes=["dtb"], dma=True)
            op("sp", lambda e: e.dma_start(out=negA, in_=alog_d.partition_broadcast(128)), writes=["negA"], dma=True)
            op("sp", lambda e: e.dma_start(out=angb, in_=ang_d.partition_broadcast(128)), writes=["angb"], dma=True)
            op("sp", lambda e: e.dma_start(out=posi, in_=pos_d), writes=["posi"], dma=True)
            op("act", lambda e: e.activation(out=negA, in_=negA, func=AF.Exp), reads=["negA"], writes=["negA"])
            op("dve", lambda e: e.tensor_scalar(out=negA, in0=negA, scalar1=-1.0, scalar2=None, op0=ALU.mult),
               reads=["negA"], writes=["negA"])

            R_A = 9 * K
            R_W = R_A + 32 * K
            R_Z = R_W + 16 * K
            R_Q = R_Z + 50 * K
            R_S = R_Q + 48 * K
            hT = view(R_A, [128, 8, S], BF16)
            wstage = view(R_W, [128, 8, 256], F32)
            wbf = [view(R_W + 8 * K + i * 4 * K, [128, 8, 256], BF16) for i in range(2)]
            zqkvT = view(R_Z, [128, 12, S + 4], BF16)
            bqT = view(R_Z, [128, 4, S], BF16)
            iqT = view(R_Z + 16 * K, [128, 4, S], BF16)
            azs = view(R_Z + 32 * K, [128, NT, 512], BF16)
            qkv_tok = view(R_Q, [128, NT, 1536], BF16)
            sa = Alloc(R_S, ARENA_BYTES)
            kz = [[sa([128, S], BF16) for _ in range(2)] for _ in range(2)]
            ikT2 = sa([128, S], BF16)
            bv_tok = sa([128, NT, 130], BF16)
            ab_tok = sa([128, NT, 16], F32)
            iw_tok = sa([128, NT, 8], F32)
            diagw = sa([128, 48, 128], BF16)
            R_WORK = sa.off

            def rope_tables():
                wa = Alloc(R_Q, R_Q + 48 * K)
                ang = wa([128, NT, 32], F32)
                tmp = wa([128, NT, 32], F32)
                ki = wa([128, NT, 32], I32)
                op("dve", lambda e: e.tensor_copy(out=posf, in_=posi), reads=["posi"], writes=["posf"])
                op("dve", lambda e: e.tensor_tensor(out=ang, in0=posf.unsqueeze(2).to_broadcast([128, NT, 32]),
                                                    in1=invf.unsqueeze(1).to_broadcast([128, NT, 32]), op=ALU.mult),
                   reads=["posf", "invf"], writes=["ang"])
                for which, shift in ((1, 0.0), (0, math.pi / 2.0)):
                    dst = cs[:, :, which * 32:(which + 1) * 32]
                    op("dve", lambda e, shift=shift: e.tensor_scalar(out=tmp, in0=ang, scalar1=shift, scalar2=None, op0=ALU.add),
                       reads=["ang"], writes=["rt_tmp"])
                    op("dve", lambda e: e.tensor_scalar(out=ki, in0=tmp, scalar1=1.0 / TWO_PI, scalar2=None, op0=ALU.mult),
                       reads=["rt_tmp"], writes=["rt_ki"])
                    op("dve", lambda e, dst=dst: e.tensor_copy(out=dst, in_=ki), reads=["rt_ki"], writes=["cs"])
                    op("dve", lambda e, dst=dst: e.scalar_tensor_tensor(out=dst, in0=dst, scalar=-TWO_PI, in1=tmp,
                                                                       op0=ALU.mult, op1=ALU.add),
                       reads=["cs", "rt_tmp"], writes=["cs"])
                    op("dve", lambda e, dst=dst: e.tensor_scalar(out=dst, in0=dst, scalar1=math.pi, scalar2=-math.pi,
                                                                op0=ALU.min, op1=ALU.max), reads=["cs"], writes=["cs"])
                    op("act", lambda e, dst=dst: e.activation(out=dst, in_=dst, func=AF.Sin), reads=["cs"], writes=["cs"])

            rope_tables()

            def phase1(hT_dst, keyp):
                wa = Alloc(R_Q + 16 * K, R_Q + 48 * K)
                xt = [wa([128, D], F32) for _ in range(2)]
                hb = [wa([128, D], BF16) for _ in range(2)]
                junk = wa([128, D], BF16)
                ss1 = wa([128, NT], F32)
                rstd1 = wa([128, NT], F32)
                op("dve", lambda e: e.memset(ss1, 0.0), writes=[keyp + "ss1"])
                for tt in range(NT):
                    b = tt % 2
                    op("sp", lambda e, tt=tt, b=b: e.dma_start(out=xt[b], in_=x_d[tt * 128:(tt + 1) * 128, :]),
                       writes=[(keyp + "xt", b)], dma=True)
                    op("act", lambda e, tt=tt, b=b: e.activation(out=junk, in_=xt[b], func=AF.Square,
                                                                 accum_out=ss1[:, tt:tt + 1]),
                       reads=[(keyp + "xt", b), keyp + "ss1"], writes=[keyp + "junk", (keyp + "ss1", tt)])
                    op("act", lambda e, tt=tt: e.activation(out=rstd1[:, tt:tt + 1], in_=ss1[:, tt:tt + 1], func=AF.Sqrt,
                                                            bias=epsc, scale=1.0 / D),
                       reads=[(keyp + "ss1", tt), "epsc"], writes=[(keyp + "rstd1", tt)])
                    op("dve", lambda e, tt=tt: e.reciprocal(out=rstd1[:, tt:tt + 1], in_=rstd1[:, tt:tt + 1]),
                       reads=[(keyp + "rstd1", tt)], writes=[(keyp + "rstd1", tt)])
                    op("dve", lambda e, tt=tt, b=b: e.tensor_scalar(out=hb[b], in0=xt[b], scalar1=rstd1[:, tt:tt + 1],
                                                                    scalar2=None, op0=ALU.mult),
                       reads=[(keyp + "xt", b), (keyp + "rstd1", tt)], writes=[(keyp + "hb", b)])
                    pbv = PBb(tt % 2)
                    for k in range(8):
                        op("pe", lambda e, k=k, b=b, pbv=pbv: e.transpose(out=pbv[:, k * 128:(k + 1) * 128],
                                                                          in_=hb[b][:, k * 128:(k + 1) * 128], identity=ident_b),
                           reads=[(keyp + "hb", b), "ident_b"], writes=[("pb", tt % 2)])
                    op("act", lambda e, tt=tt, pbv=pbv: e.copy(out=hT_dst[:, :, tt * 128:(tt + 1) * 128],
                                                               in_=pbv.rearrange("p (k t) -> p k t", k=8)),
                       reads=[("pb", tt % 2)], writes=[("hT", tt)])

            phase1(hT, "p1")
            if stop_after == "p1":
                sch.barrier()
                return
            ALL_HT = [("hT", tt) for tt in range(NT)]

            wchunk_i = [0]

            def load_w(ranges):
                i = wchunk_i[0]
                wchunk_i[0] += 1
                b = i % 2
                off = 0
                for (c0, w) in ranges:
                    op("sp", lambda e, c0=c0, w=w, off=off: e.dma_start(out=wstage[:, :, off:off + w],
                                                                        in_=w_in_v[:, :, c0:c0 + w]),
                       writes=["wstage"], dma=True)
                    off += w
                tot = off
                op("pool", lambda e, b=b, tot=tot: e.tensor_tensor(out=wbf[b][:, :, 0:tot], in0=wstage[:, :, 0:tot],
                                                                   in1=g1col.unsqueeze(2).to_broadcast([128, 8, tot]),
                                                                   op=ALU.mult),
                   reads=["wstage", "g1col"], writes=[("wbf", b)])
                return wbf[b], ("wbf", b), tot

            for ci in range(48):
                op("pool", lambda e, ci=ci: e.tensor_scalar(out=diagw[:, ci, :], in0=ident_f, scalar1=cw[:, ci:ci + 1],
                                                            scalar2=None, op0=ALU.mult),
                   reads=["ident_f", "cw"], writes=[("diagw", ci)])
            op("pool", lambda e: e.memset(zqkvT[:, :, 0:4], 0.0), writes=["zpad"])

            cva = Alloc(R_WORK, ARENA_BYTES)
            convtmp = [cva([128, 512], BF16) for _ in range(2)]
            evq = [0]

            def evac_copy(out, in_, reads, writes):
                evq[0] += 1
                if evq[0] % 2 == 0:
                    op("act", lambda e: e.copy(out=out, in_=in_), reads=reads, writes=writes)
                else:
                    op("dve", lambda e: e.tensor_copy(out=out, in_=in_), reads=reads, writes=writes)

            pbi = [0]

            def g1_proj(c, wt, wkey, ct):
                for tb in range(4):
                    bk = 2 + (pbi[0] % 2)
                    pbi[0] += 1
                    for k in range(8):
                        op("pe", lambda e, k=k, tb=tb, bk=bk: e.matmul(
                            PB(bk), lhsT=wt[:, k, ct * 128:(ct + 1) * 128], rhs=hT[:, k, tb * 512:(tb + 1) * 512],
                            start=(k == 0), stop=(k == 7)),
                           reads=[wkey] + ALL_HT[tb * 4:tb * 4 + 4], writes=[("pb", bk)])
                    evac_copy(zqkvT[:, c, 4 + tb * 512:4 + (tb + 1) * 512], PB(bk), [("pb", bk)], [("zq", c, tb)])

            def g1_conv(c):
                for tb in range(4):
                    bk = 4 + (tb % 2)
                    for j in range(4):
                        op("pe", lambda e, tb=tb, j=j, bk=bk: e.matmul(
                            PB(bk), lhsT=diagw[:, c * 4 + j, :], rhs=zqkvT[:, c, tb * 512 + j + 1:tb * 512 + j + 1 + 512],
                            start=(j == 0), stop=(j == 3)),
                           reads=[("diagw", c * 4 + j), ("zq", c, tb), "zpad"] + ([("zq", c, tb - 1)] if tb > 0 else []),
                           writes=[("pb", bk)])
                    ctb = tb % 2
                    op("act", lambda e, bk=bk, ctb=ctb: e.activation(out=convtmp[ctb], in_=PB(bk), func=AF.Silu),
                       reads=[("pb", bk)], writes=[("convtmp", ctb)])
                    tbk = 6 + (tb % 2)
                    for q in range(4):
                        op("pe", lambda e, q=q, ctb=ctb, tbk=tbk: e.transpose(out=PBb(tbk)[:, q * 128:(q + 1) * 128],
                                                                              in_=convtmp[ctb][:, q * 128:(q + 1) * 128],
                                                                              identity=ident_b),
                           reads=[("convtmp", ctb), "ident_b"], writes=[("pb", tbk)])
                    op("dve", lambda e, tb=tb, tbk=tbk: e.tensor_copy(
                        out=qkv_tok[:, tb * 4:(tb + 1) * 4, c * 128:(c + 1) * 128],
                        in_=PBb(tbk)[:, 0:512].rearrange("p (q t) -> p q t", q=4)),
                       reads=[("pb", tbk)], writes=[("qkv_tok", tb * 4 + q, c) for q in range(4)])

            prev_c = None
            nxt_w = load_w([(0, 256)])
            for chunk in range(6):
                wt, wkey, _ = nxt_w
                for ct in range(2):
                    c = chunk * 2 + ct
                    g1_proj(c, wt, wkey, ct)
                    if ct == 0:
                        nxt_w = load_w([((chunk + 1) * 256, 256)]) if chunk + 1 < 6 else load_w([(C_AZ, 256)])
                    if prev_c is not None:
                        g1_conv(prev_c)
                    prev_c = c
            g1_conv(prev_c)
            pending_w = [nxt_w]

            if "qkv_tok" in dbg:
                op("sp", lambda e: e.dma_start(out=dbg["qkv_tok"].rearrange("(t p) c -> p t c", p=128), in_=qkv_tok),
                   reads=[("qkv_tok", tt, c) for tt in range(NT) for c in range(12)], writes=["dbg_qkv_tok"], dma=True)
                op("sp", None, reads=["dbg_qkv_tok"])
            sch.barrier()
            if stop_after == "g1":
                return

            rwa = Alloc(cva.off, ARENA_BYTES)
            zr = [rwa([128, 256], F32) for _ in range(2)]
            rt = [rwa([128, 4, 32], F32) for _ in range(4)]
            roped = [rwa([128, 256], BF16) for _ in range(2)]
            op("pool", lambda e: e.memset(bv_tok, 1.0), writes=["bv_ones"])
            for a_ in range(2):
                for b_ in range(2):
                    op("pool", lambda e, a_=a_, b_=b_: e.memset(kz[a_][b_], 0.0), writes=["kz0"])

            def rope_ops(src, nh, dst_views, tt, rkey, wkeys, b):
                sv = src.rearrange("p (h d) -> p h d", h=nh)
                x1 = sv[:, :, 0:32]
                x2 = sv[:, :, 32:64]
                cc = cs[:, tt, 0:32].unsqueeze(1).to_broadcast([128, nh, 32])
                sn = cs[:, tt, 32:64].unsqueeze(1).to_broadcast([128, nh, 32])
                t = [r[:, 0:nh, :] for r in rt]
                op("dve", lambda e: e.tensor_tensor(out=t[0], in0=x1, in1=cc, op=ALU.mult), reads=[rkey, "cs"], writes=[("rt", 0)])
                op("pool", lambda e: e.tensor_tensor(out=t[1], in0=x2, in1=sn, op=ALU.mult), reads=[rkey, "cs"], writes=[("rt", 1)])
                op("pool", lambda e: e.tensor_tensor(out=t[2], in0=x2, in1=cc, op=ALU.mult), reads=[rkey, "cs"], writes=[("rt", 2)])
                op("dve", lambda e: e.tensor_tensor(out=t[3], in0=x1, in1=sn, op=ALU.mult), reads=[rkey, "cs"], writes=[("rt", 3)])
                for i, dv in enumerate(dst_views):
                    eng = "dve" if i % 2 == 0 else "pool"
                    op(eng, lambda e, dv=dv: e.tensor_tensor(out=dv[:, :, 0:32], in0=t[0], in1=t[1], op=ALU.subtract),
                       reads=[("rt", 0), ("rt", 1)], writes=wkeys)
                    op(eng, lambda e, dv=dv: e.tensor_tensor(out=dv[:, :, 32:64], in0=t[2], in1=t[3], op=ALU.add),
                       reads=[("rt", 2), ("rt", 3)], writes=wkeys)

            def tok_chunk(ranges, handler, sel=None, next_ranges=None):
                if pending_w[0] is not None:
                    wt, wkey, tot = pending_w[0]
                    pending_w[0] = None
                else:
                    wt, wkey, tot = load_w(ranges)
                lo, hi = (0, tot) if sel is None else sel
                pend = []
                for tt in range(NT):
                    if tt == 6 and next_ranges is not None:
                        pending_w[0] = load_w(next_ranges)
                    bk = 2 + (tt % 2)
                    for k in range(8):
                        op("pe", lambda e, k=k, tt=tt, bk=bk, wt=wt: e.matmul(
                            PB(bk)[:, 0:hi - lo], lhsT=hT[:, k, tt * 128:(tt + 1) * 128], rhs=wt[:, k, lo:hi],
                            start=(k == 0), stop=(k == 7)),
                           reads=[wkey, ("hT", tt)], writes=[("pb", bk)])
                    if tt >= 1:
                        pend.append(handler(tt - 1, 2 + ((tt - 1) % 2)))
                    if len(pend) >= 2:
                        p2 = pend.pop(0)
                        if p2 is not None:
                            p2()
                pend.append(handler(NT - 1, 2 + ((NT - 1) % 2)))
                for p2 in pend:
                    if p2 is not None:
                        p2()

            for j in range(2):
                def h_az(tt, bk, j=j):
                    op("act", lambda e: e.activation(out=azs[:, tt, j * 256:(j + 1) * 256], in_=PB(bk)[:, 0:256], func=AF.Silu),
                       reads=[("pb", bk)], writes=[("azs", tt, j)])
                tok_chunk([(C_AZ + j * 256, 256)], h_az, next_ranges=[(C_AZ + 256, 256)] if j == 0 else [(C_BQ, 256)])

            if stop_after == "u1":
                return
            for (c0, dstT, nm) in ((C_BQ, bqT, "bqT"), (C_IQ, iqT, "iqT")):
                for j in range(2):
                    def h_q(tt, bk, j=j, dstT=dstT, nm=nm):
                        b = tt % 2
                        op("act", lambda e: e.copy(out=zr[b], in_=PB(bk)[:, 0:256]), reads=[("pb", bk)], writes=[("zr", b)])
                        rope_ops(zr[b], 4, [roped[b].rearrange("p (h d) -> p h d", h=4)], tt, ("zr", b), [("roped", b)], b)
                        tbk = 6 + b

                        def part2():
                            for q in range(2):
                                op("pe", lambda e, q=q: e.transpose(out=PBb(tbk)[:, q * 128:(q + 1) * 128],
                                                                    in_=roped[b][:, q * 128:(q + 1) * 128], identity=ident_b),
                                   reads=[("roped", b), "ident_b"], writes=[("pb", tbk)])
                            op("act", lambda e: e.copy(out=dstT[:, 2 * j:2 * j + 2, tt * 128:(tt + 1) * 128],
                                                       in_=PBb(tbk)[:, 0:256].rearrange("p (q t) -> p q t", q=2)),
                               reads=[("pb", tbk)], writes=[(nm, tt, j)])
                        return part2
                    nr = [(c0 + 256, 256)] if j == 0 else ([(C_IQ, 256)] if c0 == C_BQ else [(C_BK, 256)])
                    tok_chunk([(c0 + j * 256, 256)], h_q, next_ranges=nr)

            if stop_after == "u23":
                return
            def h_kv(tt, bk):
                b = tt % 2
                op("act", lambda e: e.copy(out=zr[b], in_=PB(bk)[:, 0:256]), reads=[("pb", bk)], writes=[("zr", b)])
                rv = roped[b].rearrange("p (h d) -> p h d", h=4)
                rope_ops(zr[b][:, 0:128], 2, [rv[:, 0:2, :]], tt, ("zr", b), [("roped", b)], b)
                op("pool", lambda e: e.tensor_copy(out=rv[:, 2, :], in_=rv[:, 1, :]), reads=[("roped", b)], writes=[("roped", b)])
                op("pool", lambda e: e.tensor_copy(out=rv[:, 3, :], in_=rv[:, 0, :]), reads=[("roped", b)], writes=[("roped", b)])
                def part2():
                    tbk = 6 + b
                    for q in range(2):
                        op("pe", lambda e, q=q: e.transpose(out=PBb(tbk)[:, q * 128:(q + 1) * 128],
                                                            in_=roped[b][:, q * 128:(q + 1) * 128], identity=ident_b),
                           reads=[("roped", b), "ident_b"], writes=[("pb", tbk)])
                    ts_ = slice(tt * 128, (tt + 1) * 128)
                    op("act", lambda e: e.copy(out=kz[0][0][0:64, ts_], in_=PBb(tbk)[0:64, 0:128]), reads=[("pb", tbk), "kz0"],
                       writes=[("bkT", tt)])
                    op("act", lambda e: e.copy(out=kz[1][1][64:128, ts_], in_=PBb(tbk)[64:128, 0:128]), reads=[("pb", tbk)],
                       writes=[("bkT", tt)])
                    op("act", lambda e: e.copy(out=kz[1][0][0:64, ts_], in_=PBb(tbk)[0:64, 128:256]), reads=[("pb", tbk)],
                       writes=[("bkT", tt)])
                    op("act", lambda e: e.copy(out=kz[0][1][64:128, ts_], in_=PBb(tbk)[64:128, 128:256]), reads=[("pb", tbk)],
                       writes=[("bkT", tt)])

                op("dve", lambda e: e.tensor_copy(out=bv_tok[:, tt, 0:64], in_=zr[b][:, 128:192]), reads=[("zr", b), "bv_ones"],
                   writes=[("bv", tt)])
                op("dve", lambda e: e.tensor_copy(out=bv_tok[:, tt, 65:129], in_=zr[b][:, 192:256]), reads=[("zr", b)],
                   writes=[("bv", tt)])
                return part2
            tok_chunk([(C_BK, 256)], h_kv, next_ranges=[(C_IW + 8 - 256, 256)])

            if stop_after == "u4a":
                return
            IW_SCALE = (8 ** -0.5) * (64 ** -0.5)

            def h_small(tt, bk):
                b = tt % 2
                op("act", lambda e: e.copy(out=zr[b][:, 0:72], in_=PB(bk)[:, 0:72]), reads=[("pb", bk)], writes=[("zr", b)])
                rv = roped[b].rearrange("p (h d) -> p h d", h=4)
                rope_ops(zr[b][:, 0:64], 1, [rv[:, 0:1, :], rv[:, 1:2, :]], tt, ("zr", b), [("roped", b)], b)
                def part2():
                    tbk = 6 + b
                    op("pe", lambda e: e.transpose(out=PBb(tbk)[:, 0:128], in_=roped[b][:, 0:128], identity=ident_b),
                       reads=[("roped", b), "ident_b"], writes=[("pb", tbk)])
                    op("act", lambda e: e.copy(out=ikT2[:, tt * 128:(tt + 1) * 128], in_=PBb(tbk)[:, 0:128]),
                       reads=[("pb", tbk)], writes=[("ikT", tt)])

                op("dve", lambda e: e.tensor_scalar(out=iw_tok[:, tt, :], in0=zr[b][:, 64:72], scalar1=IW_SCALE, scalar2=None,
                                                    op0=ALU.mult), reads=[("zr", b)], writes=[("iw", tt)])
                return part2
            tok_chunk([(C_IW + 8 - 256, 256)], h_small, sel=(184, 256), next_ranges=[(C_BETA, 256)])

            def h_ab(tt, bk):
                op("act", lambda e: e.copy(out=ab_tok[:, tt, :], in_=PB(bk)[:, 0:16]), reads=[("pb", bk)], writes=[("ab", tt)])
            tok_chunk([(C_BETA, 256)], h_ab, sel=(0, 16))

            for nm, t_, shape in (("bqT", bqT, None), ("iqT", iqT, None)):
                if nm in dbg:
                    op("sp", lambda e, nm=nm, t_=t_: e.dma_start(out=dbg[nm].rearrange("(a p) t -> p a t", p=128), in_=t_),
                       reads=[(nm, tt, j) for tt in range(NT) for j in range(2)], writes=["dbg_" + nm], dma=True)
                    op("sp", None, reads=["dbg_" + nm])
            if "misc" in dbg:
                sch.barrier()
                mt = view(R_W, [128, NT, 154], F32)
                op("dve", lambda e: e.tensor_copy(out=mt[:, :, 0:16], in_=ab_tok), reads=[("ab", tt) for tt in range(NT)], writes=["mt"])
                op("dve", lambda e: e.tensor_copy(out=mt[:, :, 16:24], in_=iw_tok), reads=[("iw", tt) for tt in range(NT)], writes=["mt"])
                op("dve", lambda e: e.tensor_copy(out=mt[:, :, 24:154], in_=bv_tok), reads=[("bv", tt) for tt in range(NT)], writes=["mt"])
                op("sp", lambda e: e.dma_start(out=dbg["misc"].rearrange("(t p) c -> p t c", p=128), in_=mt), reads=["mt"],
                   writes=["dbg_misc"], dma=True)
                op("sp", None, reads=["dbg_misc"])
            sch.barrier()
            if stop_after == "p2":
                return

            class MultiAlloc:
                def __init__(self, regions):
                    self.regs = [[a, b] for a, b in regions]

                def __call__(self, shape, dt):
                    n = 1
                    for s_ in shape[1:]:
                        n *= s_
                    size = (n * DT_SIZE[dt] + 63) // 64 * 64
                    for r in self.regs:
                        r[0] = (r[0] + 63) // 64 * 64
                        if r[0] + size <= r[1]:
                            v = view(r[0], shape, dt)
                            r[0] += size
                            return v
                    raise AssertionError(("MultiAlloc out of space", shape, self.regs))

            def dump(name, ap, reads):
                if name in dbg:
                    op("sp", lambda e: e.dma_start(out=dbg[name], in_=ap), reads=reads, writes=["dbg_" + name], dma=True)
                    op("sp", None, reads=["dbg_" + name])

            o_aT = view(R_A, [128, 4, S], BF16)
            diagw_off = R_WORK - 12 * K
            ga = MultiAlloc([(R_W, R_W + 16 * K), (R_A + 16 * K, R_A + 32 * K), (diagw_off, ARENA_BYTES)])
            g_all = ga([128, NT, 8], F32)
            bet = ga([128, NT, 8], F32)
            gs = ga([128, 24], F32)
            eG = ga([128, 8], F32)
            eGlmG = ga([128, 8], F32)
            scs = [ga([128, 4], F32) for _ in range(2)]
            g_bc = ga([128, 8, 128], F32)
            sq = ga([128, 1024], F32)
            ssn = ga([128, 16], F32)
            rn = ga([128, 16], F32)
            cq = ga([128, 8], F32)
            cqd = ga([128, 8], F32)
            cbk = ga([128, 8], F32)
            ckd = ga([128, 8], F32)
            negbeta = ga([128, 8], F32)
            qn = ga([128, 512], BF16)
            qd = ga([128, 512], BF16)
            kn = ga([128, 512], BF16)
            rhsk = ga([128, 512], BF16)
            kdec = ga([128, 512], BF16)
            rhsv = ga([128, 512], BF16)
            qnT = ga([128, 4, 128], BF16)
            qdT = ga([128, 4, 128], BF16)
            knT = ga([128, 4, 128], BF16)
            Dm = ga([128, 8, 128], BF16)
            Ds = ga([128, 8, 128], BF16)
            Mm = [ga([128, 8, 128], BF16) for _ in range(2)]
            Nm = [ga([128, 8, 128], BF16) for _ in range(2)]
            Pm = [ga([128, 8, 128], BF16) for _ in range(2)]
            qkm = ga([128, 8, 128], BF16)
            qkT_sb = ga([128, 8, 128], BF16)
            u_c = ga([128, 2, 512], F32)
            w_tok = ga([128, 512], BF16)
            wT_sb = ga([128, 4, 128], BF16)
            vn_b = ga([128, 512], BF16)
            Sst = ga([128, 4, 128], F32)
            Stmp = ga([128, 4, 128], F32)
            S_bd = ga([128, 4, 128], BF16)
            bdmask = ga([128, 4, 128], BF16)
            o_c = ga([128, 512], F32)
            qkT_c1 = ga([128, 8, 64], BF16)
            kdec_c1 = ga([128, 512], BF16)
            az_c = ga([128, 512], BF16)
            ss2 = ga([128, 8], F32)
            r2 = ga([128, 8], F32)
            oa_b = ga([128, 512], BF16)

            def bc8(v):
                return v.unsqueeze(2).to_broadcast([128, 8, 64])

            ABK = [("ab", tt) for tt in range(NT)]
            op("act", lambda e: e.activation(out=bet, in_=ab_tok[:, :, 0:8], func=AF.Sigmoid), reads=["ab_all"], writes=["bet"])
            op("dve", lambda e: e.tensor_tensor(out=g_all, in0=ab_tok[:, :, 8:16], in1=dtb.unsqueeze(1).to_broadcast([128, NT, 8]),
                                                op=ALU.add), reads=["ab_all", "dtb"], writes=["g_all"])
            op("act", lambda e: e.activation(out=g_all, in_=g_all, func=AF.Exp), reads=["g_all"], writes=["g_all"])
            op("act", lambda e: e.activation(out=g_all, in_=g_all, func=AF.Ln, bias=1.0), reads=["g_all"], writes=["g_all"])
            op("dve", lambda e: e.tensor_tensor(out=g_all, in0=g_all, in1=negA.unsqueeze(1).to_broadcast([128, NT, 8]),
                                                op=ALU.mult), reads=["g_all", "negA"], writes=["g_all"])
            op("dve", lambda e: e.memset(Sst, 0.0), writes=["S"])
            op("dve", lambda e: e.memset(S_bd, 0.0), writes=["S_bd"])
            op("pool", lambda e: e.memset(bdmask, 0.0), writes=["bdmask"])
            op("pool", lambda e: e.memset(bdmask[0:64, :, 0:64], 1.0), writes=["bdmask"])
            op("pool", lambda e: e.memset(bdmask[64:128, :, 64:128], 1.0), writes=["bdmask"])
            if "g" in dbg:
                op("sp", lambda e: e.dma_start(out=dbg["g"].rearrange("(t p) c -> p t c", p=128), in_=g_all), reads=["g_all"],
                   writes=["dbg_g"], dma=True)
                op("sp", None, reads=["dbg_g"])

            if stop_after == "gdn_pre":
                return
            for tt in range(NT):
                op("pe", lambda e, tt=tt: e.matmul(PB(0)[:, 0:8], lhsT=ucs_f, rhs=g_all[:, tt, :], start=True, stop=True),
                   reads=["g_all"], writes=[("pb", 0)])
                op("pe", lambda e, tt=tt: e.matmul(PB(0)[:, 8:16], lhsT=mc0_f, rhs=g_all[:, tt, :], start=True, stop=True),
                   reads=["g_all"], writes=[("pb", 0)])
                op("pe", lambda e, tt=tt: e.matmul(PB(0)[:, 16:24], lhsT=mc1_f, rhs=g_all[:, tt, :], start=True, stop=True),
                   reads=["g_all"], writes=[("pb", 0)])
                op("act", lambda e: e.copy(out=gs, in_=PB(0)[:, 0:24]), reads=[("pb", 0)], writes=["gs"])
                op("act", lambda e: e.activation(out=eG, in_=gs[:, 0:8], func=AF.Exp), reads=["gs"], writes=["eG"])
                op("dve", lambda e: e.tensor_tensor(out=eGlmG[0:64, :], in0=gs[0:64, 8:16], in1=gs[0:64, 0:8], op=ALU.subtract),
                   reads=["gs"], writes=["eGlmG"])
                op("dve", lambda e: e.tensor_tensor(out=eGlmG[64:128, :], in0=gs[64:128, 16:24], in1=gs[64:128, 0:8],
                                                    op=ALU.subtract), reads=["gs"], writes=["eGlmG"])
                op("act", lambda e: e.activation(out=eGlmG, in_=eGlmG, func=AF.Exp), reads=["eGlmG"], writes=["eGlmG"])
                for hf in range(2):
                    c0 = 8 + 8 * hf
                    op("act", lambda e, hf=hf, c0=c0: e.activation(out=scs[hf][0:64, :], in_=gs[0:64, c0:c0 + 8:2], func=AF.Exp),
                       reads=["gs"], writes=[("scs", hf)])
                    op("act", lambda e, hf=hf, c0=c0: e.activation(out=scs[hf][64:128, :], in_=gs[64:128, c0 + 1:c0 + 8:2],
                                                                   func=AF.Exp), reads=["gs"], writes=[("scs", hf)])
                op("dve", lambda e, tt=tt: e.tensor_scalar(out=g_bc, in0=g_all[:, tt, :].unsqueeze(2).to_broadcast([128, 8, 128]),
                                                           scalar1=-1.0, scalar2=None, op0=ALU.mult),
                   reads=["g_all"], writes=["g_bc"])
                if stop_after == "gdn_a":
                    return
                QK = [("qkv_tok", tt, c) for c in range(8)]
                VV = [("qkv_tok", tt, c) for c in range(8, 12)]
                op("dve", lambda e, tt=tt: e.tensor_tensor(out=sq, in0=qkv_tok[:, tt, 0:1024], in1=qkv_tok[:, tt, 0:1024],
                                                           op=ALU.mult), reads=["qkv_all"], writes=["sq"])
                op("dve", lambda e: e.tensor_reduce(out=ssn, in_=sq.rearrange("p (h d) -> p h d", h=16), axis=AX.X, op=ALU.add),
                   reads=["sq"], writes=["ssn"])
                op("act", lambda e: e.activation(out=rn, in_=ssn, func=AF.Ln, bias=epsc, scale=1.0), reads=["ssn", "epsc"],
                   writes=["rn"])
                op("act", lambda e: e.activation(out=rn, in_=rn, func=AF.Exp, scale=-0.5), reads=["rn"], writes=["rn"])
                op("dve", lambda e: e.tensor_scalar(out=cq, in0=rn[:, 0:8], scalar1=0.125, scalar2=None, op0=ALU.mult),
                   reads=["rn"], writes=["cq"])
                op("dve", lambda e: e.tensor_tensor(out=cqd, in0=cq, in1=eG, op=ALU.mult), reads=["cq", "eG"], writes=["cqd"])
                op("dve", lambda e, tt=tt: e.tensor_tensor(out=cbk, in0=rn[:, 8:16], in1=bet[:, tt, :], op=ALU.mult),
                   reads=["rn", "bet"], writes=["cbk"])
                op("dve", lambda e: e.tensor_tensor(out=cbk, in0=cbk, in1=eG, op=ALU.mult), reads=["cbk", "eG"], writes=["cbk"])
                op("dve", lambda e: e.tensor_tensor(out=ckd, in0=rn[:, 8:16], in1=eGlmG, op=ALU.mult), reads=["rn", "eGlmG"],
                   writes=["ckd"])
                op("dve", lambda e, tt=tt: e.tensor_scalar(out=negbeta, in0=bet[:, tt, :], scalar1=-1.0, scalar2=None,
                                                           op0=ALU.mult), reads=["bet"], writes=["negbeta"])
                qv = qkv_tok[:, tt, 0:512].rearrange("p (h d) -> p h d", h=8)
                kv = qkv_tok[:, tt, 512:1024].rearrange("p (h d) -> p h d", h=8)
                vv = qkv_tok[:, tt, 1024:1536].rearrange("p (h d) -> p h d", h=8)

                def v3(t_):
                    return t_.rearrange("p (h d) -> p h d", h=8)
                op("dve", lambda e, qv=qv: e.tensor_tensor(out=v3(qn), in0=qv, in1=bc8(cq), op=ALU.mult),
                   reads=["qkv_all", "cq"], writes=["qn"])
                op("pool", lambda e, qv=qv: e.tensor_tensor(out=v3(qd), in0=qv, in1=bc8(cqd), op=ALU.mult),
                   reads=["qkv_all", "cqd"], writes=["qd"])
                op("dve", lambda e, kv=kv: e.tensor_tensor(out=v3(kn), in0=kv, in1=bc8(rn[:, 8:16]), op=ALU.mult),
                   reads=["qkv_all", "rn"], writes=["kn"])
                op("pool", lambda e, kv=kv: e.tensor_tensor(out=v3(rhsk), in0=kv, in1=bc8(cbk), op=ALU.mult),
                   reads=["qkv_all", "cbk"], writes=["rhsk"])
                op("pool", lambda e, kv=kv: e.tensor_tensor(out=v3(kdec), in0=kv, in1=bc8(ckd), op=ALU.mult),
                   reads=["qkv_all", "ckd"], writes=["kdec"])
                op("dve", lambda e, vv=vv, tt=tt: e.tensor_tensor(out=v3(rhsv), in0=vv, in1=bc8(bet[:, tt, :]), op=ALU.mult),
                   reads=["qkv_all", "bet"], writes=["rhsv"])
                if stop_after == "gdn_b":
                    return
                for (src, skey, dst, dkey, bank, coff, eng) in ((qn, "qn", qnT, "qnT", 6, 0, "act"), (qd, "qd", qdT, "qdT", 7, 0, "dve"),
                                                                (kn, "kn", knT, "knT", 0, 0, "act")):
                    for q in range(4):
                        op("pe", lambda e, src=src, bank=bank, coff=coff, q=q: e.transpose(
                            out=PBb(bank)[:, coff + q * 128:coff + (q + 1) * 128], in_=src[:, q * 128:(q + 1) * 128], identity=ident_b),
                           reads=[skey, "ident_b"], writes=[("pb", bank)])
                    if eng == "act":
                        op("act", lambda e, dst=dst, bank=bank, coff=coff: e.copy(
                            out=dst, in_=PBb(bank)[:, coff:coff + 512].rearrange("p (q t) -> p q t", q=4)),
                           reads=[("pb", bank)], writes=[dkey])
                    else:
                        op("dve", lambda e, dst=dst, bank=bank, coff=coff: e.tensor_copy(
                            out=dst, in_=PBb(bank)[:, coff:coff + 512].rearrange("p (q t) -> p q t", q=4)),
                           reads=[("pb", bank)], writes=[dkey])
                    if stop_after == "gdn_c_" + skey:
                        return
                if tt == 0:
                    dump("qn0", qn, ["qn"]); dump("kn0", kn, ["kn"]); dump("rhsv0", rhsv, ["rhsv"]); dump("rhsk0", rhsk, ["rhsk"])
                    dump("kdec0", kdec, ["kdec"]); dump("qd0", qd, ["qd"]); dump("gs0", gs, ["gs"])
                    dump("knT0", knT.rearrange("p a t -> p (a t)"), ["knT"])
                    dump("rn0", rn, ["rn"]); dump("cq0", cq, ["cq"]); dump("cqd0", cqd, ["cqd"]); dump("cbk0", cbk, ["cbk"])
                    dump("ckd0", ckd, ["ckd"]); dump("eG0", eG, ["eG"]); dump("ssn0", ssn, ["ssn"])
                if stop_after == "gdn_c":
                    return
                def group_gen(hg, bA, bB, bC, bT):
                    hs_list = list(range(4))
                    grp = slice(4 * hg, 4 * hg + 4)
                    for hs in hs_list:
                        h = 4 * hg + hs
                        hp, par = h // 2, h % 2
                        rows = slice(par * 64, par * 64 + 64)
                        cs_ = slice(hs * 128, hs * 128 + 128)
                        op("pe", lambda e, hp=hp, rows=rows, cs_=cs_, par=par: e.matmul(
                            PB(bA)[:, cs_], lhsT=knT[rows, hp, :], rhs=knT[rows, hp, :], start=True, stop=True,
                            tile_position=(par * 64, 0)), reads=["knT"], writes=[("pb", bA)])
                        op("pe", lambda e, hp=hp, rows=rows, cs_=cs_, par=par: e.matmul(
                            PB(bB)[:, cs_], lhsT=qnT[rows, hp, :], rhs=knT[rows, hp, :], start=True, stop=True,
                            tile_position=(par * 64, 0)), reads=["knT", "qnT"], writes=[("pb", bB)])
                        op("pe", lambda e, h=h, cs_=cs_: e.matmul(PB(bC)[:, cs_], lhsT=g_bc[:, h, :], rhs=ucs_f, start=True, stop=False),
                           reads=["g_bc", "ucs_f"], writes=[("pb", bC)])
                        op("pe", lambda e, cs_=cs_: e.matmul(PB(bC)[:, cs_], lhsT=ident_f, rhs=maskneg_f, start=False, stop=True),
                           reads=["ident_f", "maskneg_f"], writes=[("pb", bC)])
                    yield
                    for hs in hs_list:
                        h = 4 * hg + hs
                        cs_ = slice(hs * 128, hs * 128 + 128)
                        op("act", lambda e, h=h, cs_=cs_: e.activation(out=Dm[:, h, :], in_=PB(bC)[:, cs_], func=AF.Exp,
                                                                       bias=gs[:, h:h + 1], scale=1.0),
                           reads=[("pb", bC), "gs"], writes=[("Dm", h)])
                        op("dve", lambda e, h=h: e.tensor_tensor(out=Ds[:, h, :], in0=Dm[:, h, :], in1=strict_b, op=ALU.mult),
                           reads=[("Dm", h), "strict_b"], writes=[("Ds", h)])
                        op("dve", lambda e, h=h, cs_=cs_: e.scalar_tensor_tensor(out=Mm[0][:, h, :], in0=PB(bA)[:, cs_],
                                                                                 scalar=negbeta[:, h:h + 1], in1=Ds[:, h, :],
                                                                                 op0=ALU.mult, op1=ALU.mult),
                           reads=[("pb", bA), "negbeta", ("Ds", h)], writes=[("M", 0, hg)])
                        op("dve", lambda e, h=h, cs_=cs_: e.tensor_tensor(out=qkm[:, h, :], in0=PB(bB)[:, cs_], in1=Dm[:, h, :],
                                                                          op=ALU.mult),
                           reads=[("pb", bB), ("Dm", h)], writes=[("qkm", hg)])
                    yield
                    for hs in hs_list:
                        h = 4 * hg + hs
                        cs_ = slice(hs * 128, hs * 128 + 128)
                        op("pe", lambda e, h=h, cs_=cs_: e.transpose(out=PBb(bT)[:, cs_], in_=Mm[0][:, h, :], identity=ident_b),
                           reads=[("M", 0, hg), "ident_b"], writes=[("pb", bT)])
                    op("act", lambda e: e.copy(out=Nm[0][:, grp, :], in_=PBb(bT)[:, 0:512].rearrange("p (q t) -> p q t", q=4)),
                       reads=[("pb", bT)], writes=[("N", 0, hg)])
                    op("pool", lambda e: e.tensor_tensor(out=Pm[0][:, grp, :], in0=Nm[0][:, grp, :],
                                                         in1=ident_b.unsqueeze(1).to_broadcast([128, 4, 128]), op=ALU.add),
                       reads=[("N", 0, hg), "ident_b"], writes=[("P", 0, hg)])
                    yield
                    for hs in hs_list:
                        h = 4 * hg + hs
                        cs2 = slice(hs * 128, hs * 128 + 128)
                        op("pe", lambda e, h=h, cs2=cs2: e.transpose(out=PBb(bT)[:, cs2], in_=qkm[:, h, :], identity=ident_b),
                           reads=[("qkm", hg), "ident_b"], writes=[("pb", bT)])
                    op("dve", lambda e: e.tensor_copy(out=qkT_sb[:, grp, :], in_=PBb(bT)[:, 0:512].rearrange("p (q t) -> p q t", q=4)),
                       reads=[("pb", bT)], writes=[("qkT", hg)])
                    yield
                    for lv in range(1, 6):
                        cur, nxt = (lv - 1) % 2, lv % 2
                        for hs in hs_list:
                            h = 4 * hg + hs
                            cs_ = slice(hs * 128, hs * 128 + 128)
                            op("pe", lambda e, h=h, cs_=cs_, cur=cur: e.matmul(PB(bA)[:, cs_], lhsT=Nm[cur][:, h, :], rhs=Mm[cur][:, h, :],
                                                                               start=True, stop=True),
                               reads=[("N", cur, hg), ("M", cur, hg)], writes=[("pb", bA)])
                        if lv < 5:
                            for hs in hs_list:
                                h = 4 * hg + hs
                                cs_ = slice(hs * 128, hs * 128 + 128)
                                op("pe", lambda e, h=h, cs_=cs_, cur=cur: e.matmul(PB(bB)[:, cs_], lhsT=Mm[cur][:, h, :],
                                                                                   rhs=Nm[cur][:, h, :], start=True, stop=True),
                                   reads=[("N", cur, hg), ("M", cur, hg)], writes=[("pb", bB)])
                        yield
                        op("act", lambda e, nxt=nxt: e.copy(out=Mm[nxt][:, grp, :], in_=PB(bA).rearrange("p (q t) -> p q t", q=4)),
                           reads=[("pb", bA)], writes=[("M", nxt, hg)])
                        if lv < 5:
                            op("dve", lambda e, nxt=nxt: e.tensor_copy(out=Nm[nxt][:, grp, :],
                                                                       in_=PB(bB).rearrange("p (q t) -> p q t", q=4)),
                               reads=[("pb", bB)], writes=[("N", nxt, hg)])
                        for hs in hs_list:
                            h = 4 * hg + hs
                            cs_ = slice(hs * 128, hs * 128 + 128)
                            op("pe", lambda e, h=h, cs_=cs_, cur=cur, nxt=nxt: e.matmul(PB(bC)[:, cs_], lhsT=Mm[nxt][:, h, :],
                                                                                        rhs=Pm[cur][:, h, :], start=True, stop=True),
                               reads=[("M", nxt, hg), ("P", cur, hg)], writes=[("pb", bC)])
                        yield
                        op("dve", lambda e, cur=cur, nxt=nxt: e.tensor_tensor(
                            out=Pm[nxt][:, grp, :], in0=Pm[cur][:, grp, :], in1=PB(bC).rearrange("p (q t) -> p q t", q=4), op=ALU.add),
                           reads=[("pb", bC), ("P", cur, hg)], writes=[("P", nxt, hg)])

                gens = [group_gen(0, 3, 4, 5, 6), group_gen(1, 0, 1, 2, 7)]
                while gens:
                    for g_ in list(gens):
                        try:
                            next(g_)
                        except StopIteration:
                            gens.remove(g_)
                Pf = Pm[1]
                PK = [("P", 1, 0), ("P", 1, 1)]
                if tt == 0:
                    dump("D0", Dm.rearrange("p a t -> p (a t)"), [("Dm", h) for h in range(8)])
                    dump("M0", Mm[0].rearrange("p a t -> p (a t)"), [("M", 0, 0), ("M", 0, 1)])
                    dump("N0", Nm[0].rearrange("p a t -> p (a t)"), [("N", 0, 0), ("N", 0, 1)])
                    dump("P0", Pm[1].rearrange("p a t -> p (a t)"), [("P", 1, 0), ("P", 1, 1)])
                    dump("qkT0", qkT_sb.rearrange("p a t -> p (a t)"), [("qkT", 0), ("qkT", 1)])
                if stop_after == "gdn_e":
                    return
                for hf in range(2):
                    ub = 7 if hf == 0 else 0
                    for h in range(8):
                        op("pe", lambda e, h=h, hf=hf, ub=ub: e.matmul(PB(ub)[0:64, h * 64:(h + 1) * 64],
                                                                       lhsT=Pf[:, h, hf * 64:(hf + 1) * 64],
                                                                       rhs=rhsv[:, h * 64:(h + 1) * 64], start=True, stop=True),
                           reads=PK + ["rhsv"], writes=[("pb", ub)])
                    op("act", lambda e, hf=hf, ub=ub: e.copy(out=u_c[0:64, hf, :], in_=PB(ub)[0:64, :]), reads=[("pb", ub)],
                       writes=[("u_c", hf)])
                for h in range(8):
                    op("pe", lambda e, h=h: e.matmul(PB(1)[:, h * 64:(h + 1) * 64], lhsT=Pf[:, h, :], rhs=rhsk[:, h * 64:(h + 1) * 64],
                                                     start=True, stop=True), reads=PK + ["rhsk"], writes=[("pb", 1)])
                op("act", lambda e: e.copy(out=w_tok, in_=PB(1)), reads=[("pb", 1)], writes=["w_tok"])
                for q in range(4):
                    op("pe", lambda e, q=q: e.transpose(out=PBb(6)[:, q * 128:(q + 1) * 128], in_=w_tok[:, q * 128:(q + 1) * 128],
                                                        identity=ident_b), reads=["w_tok", "ident_b"], writes=[("pb", 6)])
                op("dve", lambda e: e.tensor_copy(out=wT_sb, in_=PBb(6)[:, 0:512].rearrange("p (q t) -> p q t", q=4)),
                   reads=[("pb", 6)], writes=["wT_sb"])
                op("sp", lambda e: e.dma_start(out=qkT_c1[0:64, :, :], in_=qkT_sb[64:128, :, 64:128]),
                   reads=[("qkT", 0), ("qkT", 1)], writes=["qkT_c1"], dma=True)
                op("sp", lambda e: e.dma_start(out=kdec_c1[0:64, :], in_=kdec[64:128, :]), reads=["kdec"], writes=["kdec_c1"],
                   dma=True)
                op("sp", lambda e, tt=tt: e.dma_start(out=az_c[0:64, :], in_=azs[64:128, tt, :]), reads=["azs_all"], writes=["az_c"],
                   dma=True)
                if tt == 0:
                    dump("u0", u_c.rearrange("p a t -> p (a t)"), [("u_c", 0), ("u_c", 1)])
                    dump("wT0", wT_sb.rearrange("p a t -> p (a t)"), ["wT_sb"])
                if stop_after == "gdn_f":
                    return
                for hf in range(2):
                    tcs = slice(hf * 64, hf * 64 + 64)
                    ck = 2 * tt + hf
                    if hf == 0:
                        qk_x, qk_keys = qkT_sb[0:64, :, 0:64], [("qkT", 0), ("qkT", 1)]
                        kd_x, kd_keys = kdec[0:64, :], ["kdec"]
                    else:
                        qk_x, qk_keys = qkT_c1[0:64, :, :], ["qkT_c1"]
                        kd_x, kd_keys = kdec_c1[0:64, :], ["kdec_c1"]
                    for hp in range(4):
                        op("pe", lambda e, hp=hp, tcs=tcs: e.matmul(PB(1)[0:64, hp * 128:(hp + 1) * 128], lhsT=wT_sb[:, hp, tcs],
                                                                    rhs=S_bd[:, hp, :], start=True, stop=True),
                           reads=["wT_sb", "S_bd"], writes=[("pb", 1)])
                    op("dve", lambda e, hf=hf: e.tensor_tensor(out=vn_b[0:64, :], in0=u_c[0:64, hf, :], in1=PB(1)[0:64, :],
                                                               op=ALU.subtract), reads=[("u_c", hf), ("pb", 1)], writes=["vn_b"])
                    for h in range(8):
                        hp, par = h // 2, h % 2
                        op("pe", lambda e, h=h, hp=hp, par=par, tcs=tcs: e.matmul(
                            PB(2)[0:64, h * 64:(h + 1) * 64], lhsT=qdT[:, hp, tcs], rhs=S_bd[:, hp, par * 64:(par + 1) * 64],
                            start=True, stop=False), reads=["qdT", "S_bd"], writes=[("pb", 2)])
                        op("pe", lambda e, h=h, qk_x=qk_x: e.matmul(
                            PB(2)[0:64, h * 64:(h + 1) * 64], lhsT=qk_x[:, h, :], rhs=vn_b[0:64, h * 64:(h + 1) * 64],
                            start=False, stop=True), reads=qk_keys + ["vn_b"], writes=[("pb", 2)])
                    for hp in range(4):
                        op("pe", lambda e, hp=hp, kd_x=kd_x: e.matmul(PB(7)[:, hp * 128:(hp + 1) * 128],
                                                                      lhsT=kd_x[:, hp * 128:(hp + 1) * 128],
                                                                      rhs=vn_b[0:64, hp * 128:(hp + 1) * 128], start=True, stop=True),
                           reads=kd_keys + ["vn_b"], writes=[("pb", 7)])
                    op("act", lambda e: e.copy(out=o_c[0:64, :], in_=PB(2)[0:64, :]), reads=[("pb", 2)], writes=["o_c"])
                    op("pool", lambda e, hf=hf: e.tensor_tensor(out=Stmp, in0=Sst,
                                                                in1=scs[hf].unsqueeze(2).to_broadcast([128, 4, 128]), op=ALU.mult),
                       reads=["S", ("scs", hf)], writes=["Stmp"])
                    op("dve", lambda e: e.tensor_tensor(out=Sst, in0=Stmp, in1=PB(7).rearrange("p (a d) -> p a d", a=4), op=ALU.add),
                       reads=["Stmp", ("pb", 7)], writes=["S"])
                    op("pool", lambda e: e.tensor_tensor(out=S_bd, in0=Sst, in1=bdmask, op=ALU.mult), reads=["S", "bdmask"],
                       writes=["S_bd"])
                    if "o_raw" in dbg:
                        op("sp", lambda e, ck=ck: e.dma_start(out=dbg["o_raw"][ck * 64:(ck + 1) * 64, :], in_=o_c[0:64, :]),
                           reads=["o_c"], writes=["dbg_o_raw"], dma=True)
                    sqh = sq[0:64, 0:512]
                    op("dve", lambda e: e.tensor_tensor(out=sqh, in0=o_c[0:64, :], in1=o_c[0:64, :], op=ALU.mult), reads=["o_c"],
                       writes=["sq"])
                    op("dve", lambda e: e.tensor_reduce(out=ss2[0:64, :], in_=sqh.rearrange("p (h d) -> p h d", h=8), axis=AX.X,
                                                        op=ALU.add), reads=["sq"], writes=["ss2"])
                    op("act", lambda e: e.activation(out=r2[0:64, :], in_=ss2[0:64, :], func=AF.Ln, bias=epsc[0:64, :],
                                                     scale=1.0 / 64), reads=["ss2", "epsc"], writes=["r2"])
                    op("act", lambda e: e.activation(out=r2[0:64, :], in_=r2[0:64, :], func=AF.Exp, scale=-0.5), reads=["r2"],
                       writes=["r2"])
                    op("dve", lambda e: e.tensor_tensor(out=v3(sqh), in0=v3(o_c[0:64, :]),
                                                        in1=r2[0:64, :].unsqueeze(2).to_broadcast([64, 8, 64]), op=ALU.mult),
                       reads=["o_c", "r2"], writes=["sq"])
                    op("pool", lambda e: e.tensor_tensor(out=v3(sqh), in0=v3(sqh),
                                                         in1=angb[0:64, :].unsqueeze(1).to_broadcast([64, 8, 64]), op=ALU.mult),
                       reads=["sq", "angb"], writes=["sq"])
                    az_x = azs[0:64, tt, :] if hf == 0 else az_c[0:64, :]
                    op("pool", lambda e, az_x=az_x: e.tensor_tensor(out=oa_b[0:64, :], in0=sqh, in1=az_x, op=ALU.mult),
                       reads=["sq", "az_c"], writes=["oa_b"])
                    for q in range(4):
                        op("pe", lambda e, q=q: e.transpose(out=PBb(6)[:, q * 64:(q + 1) * 64], in_=oa_b[0:64, q * 128:(q + 1) * 128],
                                                            identity=ident_b[0:64, 0:64]), reads=["oa_b", "ident_b"],
                           writes=[("pb", 6)])
                    op("act", lambda e, ck=ck: e.copy(out=o_aT[:, :, ck * 64:(ck + 1) * 64],
                                                      in_=PBb(6)[:, 0:256].rearrange("p (q t) -> p q t", q=4)),
                       reads=[("pb", 6)], writes=[("o_aT", ck)])
            if "o_raw" in dbg:
                op("sp", None, reads=["dbg_o_raw"])
            if "o_aT" in dbg:
                op("sp", lambda e: e.dma_start(out=dbg["o_aT"].rearrange("(a p) t -> p a t", p=128), in_=o_aT),
                   reads=[("o_aT", ck) for ck in range(2 * NT)], writes=["dbg_o_aT"], dma=True)
                op("sp", None, reads=["dbg_o_aT"])

            sch.barrier()
            if stop_after == "gdn":
                return

            o_bT = view(R_A + 16 * K, [128, 4, S], BF16)
            da = MultiAlloc([(R_Q, R_Q + 48 * K), (R_W, R_W + 16 * K)])
            scoreb = [da([128, S], F32) for _ in range(2)]
            rl = [da([128, 512], F32) for _ in range(2)]
            maskbb = [da([128, S], BF16) for _ in range(2)]
            thr_t = [da([128, 1], F32) for _ in range(2)]
            PTt = [[da([128, 512], BF16) for _ in range(2)] for _ in range(2)]
            I4 = da([128, 512], BF16)
            lo_t = da([128, 1], F32)
            hi_t = da([128, 1], F32)
            W0 = da([128, 1], F32)
            mid_t = da([128, 1], F32)
            tsel = da([128, 1], F32)
            Wk = da([128, NBIS], F32)
            cnt = da([128, NBIS], F32)
            pow2 = da([128, NBIS], F32)
            ob = da([128, 520], F32)
            rden = da([128, 8], F32)
            ob_b = da([128, 512], BF16)
            for q in range(4):
                op("pool", lambda e, q=q: e.tensor_copy(out=I4[:, q * 128:(q + 1) * 128], in_=ident_b), reads=["ident_b"], writes=["I4"])
            for k in range(NBIS):
                op("pool", lambda e, k=k: e.memset(pow2[:, k:k + 1], 2.0 ** (-(k + 1))), writes=["pow2"])

            xbk = [0]

            def scores_part(tt, sb):
                L = (tt + 1) * 128
                nkb = (L + 511) // 512
                qs = slice(tt * 128, (tt + 1) * 128)
                score = scoreb[sb]
                maskb = maskbb[sb]
                for h in range(8):
                    hp, par = h // 2, h % 2
                    rows = slice(par * 64, par * 64 + 64)
                    for kb in range(nkb):
                        w = min(512, L - kb * 512)
                        bank = xbk[0] % 2
                        xbk[0] += 1
                        ks = slice(kb * 512, kb * 512 + w)
                        op("pe", lambda e, hp=hp, par=par, rows=rows, w=w, bank=bank, ks=ks: e.matmul(
                            PB(bank)[:, 0:w], lhsT=iqT[rows, hp, qs], rhs=ikT2[rows, ks], start=True, stop=True,
                            tile_position=(par * 64, 0)), reads=["iqT", "ikT2"], writes=[("pb", bank)])
                        op("act", lambda e, w=w, bank=bank: e.activation(out=rl[bank][:, 0:w], in_=PB(bank)[:, 0:w], func=AF.Relu),
                           reads=[("pb", bank)], writes=[("rl", bank)])
                        if h == 0:
                            op("dve", lambda e, w=w, bank=bank, ks=ks: e.tensor_scalar(
                                out=score[:, ks], in0=rl[bank][:, 0:w], scalar1=iw_tok[:, tt, 0:1], scalar2=None, op0=ALU.mult),
                               reads=[("rl", bank), "iw_tok"], writes=[("score", sb, kb)])
                        else:
                            op("dve", lambda e, w=w, bank=bank, ks=ks, h=h: e.scalar_tensor_tensor(
                                out=score[:, ks], in0=rl[bank][:, 0:w], scalar=iw_tok[:, tt, h:h + 1], in1=score[:, ks],
                                op0=ALU.mult, op1=ALU.add), reads=[("rl", bank), "iw_tok", ("score", sb, kb)],
                               writes=[("score", sb, kb)], fast=(w >= 256))
                SK = [("score", sb, kb) for kb in range(nkb)]
                if tt >= 2:
                    op("dve", lambda e: e.tensor_reduce(out=hi_t, in_=score[:, 0:L], axis=AX.X, op=ALU.max), reads=SK, writes=["hi"])
                    op("dve", lambda e: e.tensor_reduce(out=lo_t, in_=score[:, 0:L], axis=AX.X, op=ALU.min), reads=SK, writes=["lo"])
                op("dve", lambda e: e.memset(score[0:64, L - 64:L], -1.0e30), reads=SK, writes=SK)
                if tt >= 2:
                    op("dve", lambda e: e.tensor_tensor(out=W0, in0=hi_t, in1=lo_t, op=ALU.subtract), reads=["hi", "lo"], writes=["W0"])
                    op("dve", lambda e: e.tensor_scalar(out=Wk, in0=pow2, scalar1=W0[:, 0:1], scalar2=None, op0=ALU.mult),
                       reads=["W0", "pow2"], writes=["Wk"])
                    op("dve", lambda e: e.memset(cnt, 0.0), writes=["cnt"])
                    op("dve", lambda e: e.tensor_tensor(out=mid_t, in0=lo_t, in1=Wk[:, 0:1], op=ALU.add), reads=["lo", "Wk"], writes=["mid"])
                    for k in range(NBIS):
                        op("dve", lambda e, k=k: e.tensor_scalar(out=maskb[:, 0:L], in0=score[:, 0:L], scalar1=mid_t[:, 0:1],
                                                                 scalar2=0.0, op0=ALU.is_gt, op1=ALU.add, accum_out=cnt[:, k:k + 1]),
                           reads=SK + ["mid", "cnt"], writes=[("maskb", sb), ("cntk", k)])
                        op("dve", lambda e, k=k: e.tensor_scalar(out=tsel, in0=cnt[:, k:k + 1], scalar1=255.5, scalar2=0.5,
                                                                 op0=ALU.is_gt, op1=ALU.subtract), reads=[("cntk", k)], writes=["tsel"])
                        op("dve", lambda e, k=k: e.scalar_tensor_tensor(out=mid_t, in0=tsel, scalar=Wk[:, k:k + 1], in1=mid_t,
                                                                        op0=ALU.mult, op1=ALU.add),
                           reads=["tsel", "Wk", "mid"], writes=["mid"])
                    op("dve", lambda e: e.scalar_tensor_tensor(out=thr_t[sb], in0=Wk[:, NBIS - 1:NBIS], scalar=-0.5, in1=mid_t,
                                                               op0=ALU.mult, op1=ALU.add), reads=["Wk", "mid"], writes=[("thr", sb)])
                else:
                    op("dve", lambda e: e.memset(thr_t[sb], -1.0e29), writes=[("thr", sb)])
                op("dve", lambda e: e.tensor_scalar(out=maskb[:, 0:L], in0=score[:, 0:L], scalar1=thr_t[sb][:, 0:1], scalar2=NEG,
                                                    op0=ALU.is_le, op1=ALU.mult), reads=SK + [("thr", sb)], writes=[("maskb", sb)])
                if "thr" in dbg:
                    op("sp", lambda e: e.dma_start(out=dbg["thr"][tt * 128:(tt + 1) * 128, :], in_=thr_t[sb]), reads=[("thr", sb)],
                       writes=["dbg_thr"], dma=True)
                if "score" in dbg and tt == NT - 1:
                    op("sp", lambda e: e.dma_start(out=dbg["score"], in_=score), reads=SK, writes=["dbg_score"], dma=True)

            def attn_part(tt, sb):
                qs = slice(tt * 128, (tt + 1) * 128)
                maskb = maskbb[sb]
                for kb in range(tt + 1):
                    kcs = slice(kb * 128, (kb + 1) * 128)
                    for g2 in range(2):
                        bank = 2 + g2 + 2 * (kb % 2)
                        pt = PTt[g2][kb % 2]
                        op("pe", lambda e, bank=bank, kcs=kcs: e.matmul(PB(bank), lhsT=maskb[:, kcs], rhs=I4, start=True, stop=False),
                           reads=[("maskb", sb), "I4"], writes=[("pb", bank)])
                        for s_ in range(4):
                            h = 4 * g2 + s_
                            hp, par = h // 2, h % 2
                            kT = kz[g2][par]
                            op("pe", lambda e, bank=bank, s_=s_, kT=kT, kcs=kcs, hp=hp: e.matmul(
                                PB(bank)[:, s_ * 128:(s_ + 1) * 128], lhsT=kT[:, kcs], rhs=bqT[:, hp, qs], start=False, stop=(s_ == 3)),
                               reads=["bkT", "bqT"], writes=[("pb", bank)])
                        op("act", lambda e, bank=bank, pt=pt: e.activation(out=pt, in_=PB(bank), func=AF.Exp, scale=0.125),
                           reads=[("pb", bank)], writes=[("PT", g2, kb % 2)])
                        for s_ in range(4):
                            op("pe", lambda e, g2=g2, s_=s_, pt=pt, kb=kb: e.matmul(
                                PB(6 + g2)[:, s_ * 65:(s_ + 1) * 65], lhsT=pt[:, s_ * 128:(s_ + 1) * 128],
                                rhs=bv_tok[:, kb, g2 * 65:(g2 + 1) * 65], start=(kb == 0 and s_ == 0), stop=(kb == tt and s_ == 3)),
                               reads=[("PT", g2, kb % 2), "bv_tok"], writes=[("pb", 6 + g2)])
                op("act", lambda e: e.copy(out=ob[:, 0:260], in_=PB(6)[:, 0:260]), reads=[("pb", 6)], writes=["ob"])
                op("act", lambda e: e.copy(out=ob[:, 260:520], in_=PB(7)[:, 0:260]), reads=[("pb", 7)], writes=["ob"])
                obv = ob.rearrange("p (s e) -> p s e", e=65)
                op("dve", lambda e: e.reciprocal(out=rden, in_=obv[:, :, 64]), reads=["ob"], writes=["rden"])
                op("dve", lambda e: e.tensor_tensor(out=ob_b.rearrange("p (h d) -> p h d", h=8), in0=obv[:, :, 0:64],
                                                    in1=rden.unsqueeze(2).to_broadcast([128, 8, 64]), op=ALU.mult),
                   reads=["ob", "rden"], writes=["ob_b"])
                for q in range(4):
                    op("pe", lambda e, q=q: e.transpose(out=PBb(0)[:, q * 128:(q + 1) * 128], in_=ob_b[:, q * 128:(q + 1) * 128],
                                                        identity=ident_b), reads=["ob_b", "ident_b"], writes=[("pb", 0)])
                op("act", lambda e: e.copy(out=o_bT[:, :, qs], in_=PBb(0)[:, 0:512].rearrange("p (q t) -> p q t", q=4)),
                   reads=[("pb", 0)], writes=[("o_bT", tt)])

            scores_part(0, 0)
            for tt in range(NT):
                if tt + 1 < NT:
                    scores_part(tt + 1, (tt + 1) % 2)
                attn_part(tt, tt % 2)
            if "thr" in dbg:
                op("sp", None, reads=["dbg_thr"])
            if "score" in dbg:
                op("sp", None, reads=["dbg_score"])
            if "o_bT" in dbg:
                op("sp", lambda e: e.dma_start(out=dbg["o_bT"].rearrange("(a p) t -> p a t", p=128), in_=o_bT),
                   reads=[("o_bT", tt) for tt in range(NT)], writes=["dbg_o_bT"], dma=True)
                op("sp", None, reads=["dbg_o_bT"])

            sch.barrier()
            if stop_after == "dsa":
                return

            pa = MultiAlloc([(R_W, ARENA_BYTES)])
            hT2 = pa([128, 8, S], BF16)
            mergedT = pa([128, 8, S], BF16)
            x1 = pa([128, NT, D], F32)
            bgate = pa([128, 16], F32)
            g2col = pa([128, 8], F32)
            fng = pa([128, D], F32)
            wst2 = pa([128, 8, 256], F32)
            wg_bf = [pa([128, 8, 256], BF16) for _ in range(2)]
            wp_bf = [pa([128, 4, 256], BF16) for _ in range(2)]
            ga_s = pa([128, 512], BF16)
            gb_s = pa([128, 512], BF16)
            t1 = pa([128, 512], BF16)
            t2 = pa([128, 512], BF16)
            xt4 = [pa([128, D], F32) for _ in range(2)]
            op("sp", lambda e: e.dma_start(out=bgate, in_=bgate_d), writes=["bgate"], dma=True)
            op("sp", lambda e: e.dma_start(out=g2col, in_=g2_d), writes=["g2col"], dma=True)
            op("sp", lambda e: e.dma_start(out=fng, in_=fng_d.partition_broadcast(128)), writes=["fng"], dma=True)

            def phase1b():
                xa = Alloc(R_W + 64 * K, R_W + 128 * K)
                xt = [xa([128, D], F32) for _ in range(2)]
                hb = [xa([128, D], BF16) for _ in range(2)]
                junk = xa([128, D], BF16)
                ssx = xa([128, NT], F32)
                rsx = xa([128, NT], F32)
                op("dve", lambda e: e.memset(ssx, 0.0), writes=["ssx"])
                for tt in range(NT):
                    b = tt % 2
                    op("sp", lambda e, tt=tt, b=b: e.dma_start(out=xt[b], in_=x_d[tt * 128:(tt + 1) * 128, :]), writes=[("xt", b)], dma=True)
                    op("act", lambda e, tt=tt, b=b: e.activation(out=junk, in_=xt[b], func=AF.Square, accum_out=ssx[:, tt:tt + 1]),
                       reads=[("xt", b), "ssx"], writes=["junk", ("ssx", tt)])
                    op("act", lambda e, tt=tt: e.activation(out=rsx[:, tt:tt + 1], in_=ssx[:, tt:tt + 1], func=AF.Sqrt, bias=epsc,
                                                            scale=1.0 / D), reads=[("ssx", tt), "epsc"], writes=[("rsx", tt)])
                    op("dve", lambda e, tt=tt: e.reciprocal(out=rsx[:, tt:tt + 1], in_=rsx[:, tt:tt + 1]), reads=[("rsx", tt)],
                       writes=[("rsx", tt)])
                    op("dve", lambda e, tt=tt, b=b: e.tensor_scalar(out=hb[b], in0=xt[b], scalar1=rsx[:, tt:tt + 1], scalar2=None,
                                                                    op0=ALU.mult), reads=[("xt", b), ("rsx", tt)], writes=[("hb", b)])
                    bk = tt % 2
                    for k in range(8):
                        op("pe", lambda e, k=k, b=b, bk=bk: e.transpose(out=PBb(bk)[:, k * 128:(k + 1) * 128],
                                                                        in_=hb[b][:, k * 128:(k + 1) * 128], identity=ident_b),
                           reads=[("hb", b), "ident_b"], writes=[("pb", bk)])
                    op("act", lambda e, tt=tt, bk=bk: e.copy(out=hT2[:, :, tt * 128:(tt + 1) * 128],
                                                             in_=PBb(bk).rearrange("p (k t) -> p k t", k=8)),
                       reads=[("pb", bk)], writes=[("hT2", tt)])
            phase1b()
            sch.barrier()
            HT2 = [("hT2", tt) for tt in range(NT)]

            wpa_v = wpa_d.rearrange("(k p) c -> p k c", p=128)
            wpb_v = wpb_d.rearrange("(k p) c -> p k c", p=128)
            wout_v = wout_d.rearrange("(k p) c -> p k c", p=128)
            wi4 = [0]

            wst2b = xt4[0].rearrange("p (k c) -> p k c", k=4)
            wst2c = xt4[1].rearrange("p (k c) -> p k c", k=4)
            wi5 = [0]

            def load_gate(c0):
                i = wi4[0]
                wi4[0] += 1
                b = i % 2
                op("sp", lambda e: e.dma_start(out=wst2, in_=w_in_v[:, :, c0:c0 + 256]), writes=["wst2"], dma=True)
                op("pool", lambda e, b=b: e.tensor_tensor(out=wg_bf[b], in0=wst2, in1=g1col.unsqueeze(2).to_broadcast([128, 8, 256]),
                                                          op=ALU.mult), reads=["wst2", "g1col"], writes=[("wg", b)])
                return wg_bf[b], ("wg", b)

            def load_proj(src_v, c0):
                i = wi5[0]
                wi5[0] += 1
                b = i % 2
                st = wst2b if b == 0 else wst2c
                op("sp", lambda e: e.dma_start(out=st, in_=src_v[:, :, c0:c0 + 256]), writes=[("wstp", b)], dma=True)
                op("pool", lambda e, b=b: e.tensor_copy(out=wp_bf[b], in_=st), reads=[("wstp", b)], writes=[("wp", b)])
                return wp_bf[b], ("wp", b)

            p4u = [0]
            for j in range(4):
                wga, kga = load_gate(C_GA + j * 256)
                wgb, kgb = load_gate(C_GB + j * 256)
                wpa, kpa = load_proj(wpa_v, j * 256)
                wpb, kpb = load_proj(wpb_v, j * 256)
                for ct in range(2):
                    c = 2 * j + ct
                    ccs = slice(ct * 128, (ct + 1) * 128)
                    for tb in range(4):
                        tcs = slice(tb * 512, (tb + 1) * 512)
                        hk = HT2[tb * 4:tb * 4 + 4]
                        bA, bB, bC, bD = (2, 3, 4, 5) if (p4u[0] % 2 == 0) else (0, 1, 6, 7)
                        p4u[0] += 1
                        for k in range(8):
                            op("pe", lambda e, k=k, ccs=ccs, tcs=tcs, wga=wga, bA=bA: e.matmul(PB(bA), lhsT=wga[:, k, ccs], rhs=hT2[:, k, tcs],
                                                                                         start=(k == 0), stop=(k == 7)),
                               reads=[kga] + hk, writes=[("pb", bA)])
                        op("act", lambda e, c=c, bA=bA: e.activation(out=ga_s, in_=PB(bA), func=AF.Sigmoid, bias=bgate[:, c:c + 1], scale=1.0),
                           reads=[("pb", bA), "bgate"], writes=["ga_s"])
                        for k in range(8):
                            op("pe", lambda e, k=k, ccs=ccs, tcs=tcs, wgb=wgb, bB=bB: e.matmul(PB(bB), lhsT=wgb[:, k, ccs], rhs=hT2[:, k, tcs],
                                                                                         start=(k == 0), stop=(k == 7)),
                               reads=[kgb] + hk, writes=[("pb", bB)])
                        op("act", lambda e, c=c, bB=bB: e.activation(out=gb_s, in_=PB(bB), func=AF.Sigmoid, bias=bgate[:, 8 + c:9 + c], scale=1.0),
                           reads=[("pb", bB), "bgate"], writes=["gb_s"])
                        for hp in range(4):
                            op("pe", lambda e, hp=hp, ccs=ccs, tcs=tcs, wpa=wpa, bC=bC: e.matmul(PB(bC), lhsT=wpa[:, hp, ccs], rhs=o_aT[:, hp, tcs],
                                                                                           start=(hp == 0), stop=(hp == 3)),
                               reads=[kpa, "o_aT"], writes=[("pb", bC)])
                        for hp in range(4):
                            op("pe", lambda e, hp=hp, ccs=ccs, tcs=tcs, wpb=wpb, bD=bD: e.matmul(PB(bD), lhsT=wpb[:, hp, ccs], rhs=o_bT[:, hp, tcs],
                                                                                           start=(hp == 0), stop=(hp == 3)),
                               reads=[kpb, "o_bT"], writes=[("pb", bD)])
                        op("dve", lambda e, bC=bC: e.tensor_tensor(out=t1, in0=PB(bC), in1=ga_s, op=ALU.mult), reads=[("pb", bC), "ga_s"],
                           writes=["t1"])
                        op("dve", lambda e, bD=bD: e.tensor_tensor(out=t2, in0=PB(bD), in1=gb_s, op=ALU.mult), reads=[("pb", bD), "gb_s"],
                           writes=["t2"])
                        op("pool", lambda e, c=c, tcs=tcs: e.tensor_tensor(out=mergedT[:, c, tcs], in0=t1, in1=t2, op=ALU.add),
                           reads=["t1", "t2"], writes=[("mergedT", c, tb)])
            if "mergedT" in dbg:
                op("sp", lambda e: e.dma_start(out=dbg["mergedT"].rearrange("(a p) t -> p a t", p=128), in_=mergedT),
                   reads=[("mergedT", c, tb) for c in range(8) for tb in range(4)], writes=["dbg_mergedT"], dma=True)
                op("sp", None, reads=["dbg_mergedT"])
            sch.barrier()
            wout_bf = view(R_A, [128, 8, D], BF16)
            for j in range(4):
                op("sp", lambda e, j=j: e.dma_start(out=wst2, in_=wout_v[:, :, j * 256:(j + 1) * 256]), writes=["wst2"], dma=True)
                op("pool", lambda e, j=j: e.tensor_copy(out=wout_bf[:, :, j * 256:(j + 1) * 256], in_=wst2), reads=["wst2"],
                   writes=[("wout", j)])
            WOUT = [("wout", j) for j in range(4)]
            for tt in range(NT):
                b = tt % 2
                op("sp", lambda e, tt=tt, b=b: e.dma_start(out=xt4[b], in_=x_d[tt * 128:(tt + 1) * 128, :]), writes=[("xt4", b)], dma=True)
                for nb in range(2):
                    bk = 2 + 2 * b + nb
                    for c in range(8):
                        op("pe", lambda e, c=c, tt=tt, nb=nb, bk=bk: e.matmul(PB(bk), lhsT=mergedT[:, c, tt * 128:(tt + 1) * 128],
                                                                               rhs=wout_bf[:, c, nb * 512:(nb + 1) * 512],
                                                                               start=(c == 0), stop=(c == 7)),
                           reads=WOUT + ["mergedT_all"], writes=[("pb", bk)])
                    op("dve", lambda e, tt=tt, nb=nb, bk=bk, b=b: e.tensor_tensor(out=x1[:, tt, nb * 512:(nb + 1) * 512], in0=PB(bk),
                                                                                   in1=xt4[b][:, nb * 512:(nb + 1) * 512], op=ALU.add),
                       reads=[("pb", bk), ("xt4", b)], writes=[("x1", tt)])
            if "x1" in dbg:
                op("sp", lambda e: e.dma_start(out=dbg["x1"].rearrange("(t p) c -> p t c", p=128), in_=x1),
                   reads=[("x1", tt) for tt in range(NT)], writes=["dbg_x1"], dma=True)
                op("sp", None, reads=["dbg_x1"])
            sch.barrier()
            if stop_after == "p4":
                return

            h2T = view(R_W, [128, 8, S], BF16)
            ma = MultiAlloc([(R_W + 32 * K, R_W + 64 * K), (R_A, R_A + 32 * K)])
            tail_off = None
            hb2 = [ma([128, D], BF16) for _ in range(2)]
            junk2 = ma([128, D], BF16)
            ss5 = ma([128, NT], F32)
            rs5 = ma([128, NT], F32)
            wr_st = ma([128, 8, 20], F32)
            wr_bf = ma([128, 8, 20], BF16)
            brow = ma([128, 20], F32)
            lg = ma([128, 20], F32)
            sm = {n_: ma([128, 4], F32) for n_ in ("goh", "gex", "elg", "oh1", "msk", "oh2", "wsel")}
            sc1 = {n_: ma([128, 1], F32) for n_ in ("gmax", "ngmax", "gsum", "ggate", "m1", "m2", "d21", "e21", "den", "w1", "w2")}
            tmp44 = ma([128, 4, 4], F32)
            comb_b = ma([128, 16], BF16)
            combT = ma([128, S], BF16)
            sel16 = ma([128, 16, 128], BF16)
            est = ma([128, 8, 256], F32)
            w1b = [ma([128, 8, 256], BF16) for _ in range(2)]
            w3b = [ma([128, 8, 256], BF16) for _ in range(2)]
            w2b = [ma([128, 2, D], BF16) for _ in range(2)]
            sg = [[ma([128, 512], BF16) for _ in range(2)] for _ in range(2)]
            cbt = [ma([128, 512], BF16) for _ in range(2)]
            tu = [[ma([128, 512], BF16) for _ in range(2)] for _ in range(2)]
            actT = [[ma([128, 512], BF16) for _ in range(2)] for _ in range(2)]
            op("sp", lambda e: e.dma_start(out=wr_st, in_=wr_d.rearrange("(k p) c -> p k c", p=128)), writes=["wr_st"], dma=True)
            op("sp", lambda e: e.dma_start(out=brow, in_=br_d.partition_broadcast(128)), writes=["brow"], dma=True)
            op("pool", lambda e: e.tensor_tensor(out=wr_bf, in0=wr_st, in1=g2col.unsqueeze(2).to_broadcast([128, 8, 20]), op=ALU.mult),
               reads=["wr_st", "g2col"], writes=["wr_bf"])
            op("pool", lambda e: e.memset(sel16[0:16, :, :], 1.0), writes=["sel16"])
            op("pool", lambda e: e.affine_select(out=sel16[0:16, :, :], in_=sel16[0:16, :, :], pattern=[[-1, 16], [0, 128]],
                                                 compare_op=ALU.is_equal, fill=0.0, base=0, channel_multiplier=1), writes=["sel16"])
            op("dve", lambda e: e.memset(ss5, 0.0), writes=["ss5"])
            def prep_tile(tt):
                b = tt % 2
                bk = tt % 2
                op("act", lambda e, tt=tt: e.activation(out=junk2, in_=x1[:, tt, :], func=AF.Square, accum_out=ss5[:, tt:tt + 1]),
                   reads=["x1_all", "ss5"], writes=["junk2", ("ss5", tt)])
                op("act", lambda e, tt=tt: e.activation(out=rs5[:, tt:tt + 1], in_=ss5[:, tt:tt + 1], func=AF.Ln, bias=epsc,
                                                        scale=1.0 / D), reads=[("ss5", tt), "epsc"], writes=[("rs5", tt)])
                op("act", lambda e, tt=tt: e.activation(out=rs5[:, tt:tt + 1], in_=rs5[:, tt:tt + 1], func=AF.Exp, scale=-0.5),
                   reads=[("rs5", tt)], writes=[("rs5", tt)])
                op("dve", lambda e, tt=tt, b=b: e.tensor_scalar(out=hb2[b], in0=x1[:, tt, :], scalar1=rs5[:, tt:tt + 1], scalar2=None,
                                                                op0=ALU.mult), reads=["x1_all", ("rs5", tt)], writes=[("hb2", b)])
                for k in range(8):
                    op("pe", lambda e, k=k, b=b, bk=bk: e.transpose(out=PBb(bk)[:, k * 128:(k + 1) * 128],
                                                                    in_=hb2[b][:, k * 128:(k + 1) * 128], identity=ident_b),
                       reads=[("hb2", b), "ident_b"], writes=[("pb", bk)])
                op("act", lambda e, tt=tt, bk=bk: e.copy(out=h2T[:, :, tt * 128:(tt + 1) * 128],
                                                         in_=PBb(bk).rearrange("p (k t) -> p k t", k=8)),
                   reads=[("pb", bk)], writes=[("h2T", tt)])
                for k in range(8):
                    op("pe", lambda e, k=k, tt=tt: e.matmul(PB(2)[:, 0:20], lhsT=h2T[:, k, tt * 128:(tt + 1) * 128], rhs=wr_bf[:, k, :],
                                                            start=(k == 0), stop=(k == 7)),
                       reads=[("h2T", tt), "wr_bf"], writes=[("pb", 2)])
                R = []

                def rop(fn, rd, wr):
                    op("dve", fn, reads=rd, writes=wr)
                rop(lambda e: e.tensor_tensor(out=lg, in0=PB(2)[:, 0:20], in1=brow, op=ALU.add), [("pb", 2), "brow"], ["lg"])
                elv = lg[:, 4:20].rearrange("p (g x) -> p g x", g=4)
                rop(lambda e: e.tensor_reduce(out=sc1["gmax"], in_=lg[:, 0:4], axis=AX.X, op=ALU.max), ["lg"], ["gmax"])
                rop(lambda e: e.tensor_scalar(out=sm["goh"], in0=lg[:, 0:4], scalar1=sc1["gmax"][:, 0:1], scalar2=None,
                                              op0=ALU.is_equal), ["lg", "gmax"], ["goh"])
                rop(lambda e: e.tensor_scalar(out=sc1["ngmax"], in0=sc1["gmax"], scalar1=-1.0, scalar2=None, op0=ALU.mult),
                    ["gmax"], ["ngmax"])
                op("act", lambda e: e.activation(out=sm["gex"], in_=lg[:, 0:4], func=AF.Exp, bias=sc1["ngmax"][:, 0:1], scale=1.0),
                   reads=["lg", "ngmax"], writes=["gex"])
                rop(lambda e: e.tensor_reduce(out=sc1["gsum"], in_=sm["gex"], axis=AX.X, op=ALU.add), ["gex"], ["gsum"])
                rop(lambda e: e.reciprocal(out=sc1["ggate"], in_=sc1["gsum"]), ["gsum"], ["ggate"])
                rop(lambda e: e.tensor_tensor(out=tmp44, in0=elv, in1=sm["goh"].unsqueeze(2).to_broadcast([128, 4, 4]), op=ALU.mult),
                    ["lg", "goh"], ["tmp44"])
                rop(lambda e: e.tensor_reduce(out=sm["elg"], in_=tmp44.rearrange("p g x -> p x g"), axis=AX.X, op=ALU.add),
                    ["tmp44"], ["elg"])
                rop(lambda e: e.tensor_reduce(out=sc1["m1"], in_=sm["elg"], axis=AX.X, op=ALU.max), ["elg"], ["m1"])
                rop(lambda e: e.tensor_scalar(out=sm["oh1"], in0=sm["elg"], scalar1=sc1["m1"][:, 0:1], scalar2=None, op0=ALU.is_equal),
                    ["elg", "m1"], ["oh1"])
                rop(lambda e: e.scalar_tensor_tensor(out=sm["msk"], in0=sm["oh1"], scalar=-1.0e30, in1=sm["elg"], op0=ALU.mult,
                                                     op1=ALU.add), ["oh1", "elg"], ["msk"])
                rop(lambda e: e.tensor_reduce(out=sc1["m2"], in_=sm["msk"], axis=AX.X, op=ALU.max), ["msk"], ["m2"])
                rop(lambda e: e.tensor_scalar(out=sm["oh2"], in0=sm["msk"], scalar1=sc1["m2"][:, 0:1], scalar2=None, op0=ALU.is_equal),
                    ["msk", "m2"], ["oh2"])
                rop(lambda e: e.tensor_tensor(out=sc1["d21"], in0=sc1["m2"], in1=sc1["m1"], op=ALU.subtract), ["m1", "m2"], ["d21"])
                op("act", lambda e: e.activation(out=sc1["e21"], in_=sc1["d21"], func=AF.Exp), reads=["d21"], writes=["e21"])
                rop(lambda e: e.tensor_scalar(out=sc1["den"], in0=sc1["e21"], scalar1=1.0, scalar2=None, op0=ALU.add), ["e21"], ["den"])
                rop(lambda e: e.reciprocal(out=sc1["den"], in_=sc1["den"]), ["den"], ["den"])
                rop(lambda e: e.tensor_tensor(out=sc1["w1"], in0=sc1["ggate"], in1=sc1["den"], op=ALU.mult), ["ggate", "den"], ["w1"])
                rop(lambda e: e.tensor_tensor(out=sc1["w2"], in0=sc1["w1"], in1=sc1["e21"], op=ALU.mult), ["w1", "e21"], ["w2"])
                rop(lambda e: e.tensor_scalar(out=sm["wsel"], in0=sm["oh1"], scalar1=sc1["w1"][:, 0:1], scalar2=None, op0=ALU.mult),
                    ["oh1", "w1"], ["wsel"])
                rop(lambda e: e.scalar_tensor_tensor(out=sm["wsel"], in0=sm["oh2"], scalar=sc1["w2"][:, 0:1], in1=sm["wsel"],
                                                     op0=ALU.mult, op1=ALU.add), ["oh2", "w2", "wsel"], ["wsel"])
                rop(lambda e: e.tensor_tensor(out=comb_b.rearrange("p (g x) -> p g x", g=4),
                                              in0=sm["goh"].unsqueeze(2).to_broadcast([128, 4, 4]),
                                              in1=sm["wsel"].unsqueeze(1).to_broadcast([128, 4, 4]), op=ALU.mult),
                    ["goh", "wsel"], ["comb_b"])
                if "comb" in dbg:
                    op("sp", lambda e, tt=tt: e.dma_start(out=dbg["comb"][tt * 128:(tt + 1) * 128, :], in_=comb_b), reads=["comb_b"],
                       writes=["dbg_comb"], dma=True)
                op("pe", lambda e: e.transpose(out=PBb(3)[0:16, 0:128], in_=comb_b, identity=ident_b), reads=["comb_b", "ident_b"],
                   writes=[("pb", 3)])
                op("act", lambda e, tt=tt: e.copy(out=combT[0:16, tt * 128:(tt + 1) * 128], in_=PBb(3)[0:16, 0:128]),
                   reads=[("pb", 3)], writes=[("combT", tt)])
            for tt in range(4):
                prep_tile(tt)
            H2T = [("h2T", tt) for tt in range(NT)]
            CT = [("combT", tt) for tt in range(NT)]

            def load_expert(e_i):
                b = e_i % 2
                for (src, dst, nm, fold) in ((w1_d, w1b[b], "w1", True), (w3_d, w3b[b], "w3", True)):
                    op("sp", lambda e, src=src: e.dma_start(out=est, in_=src[e_i].rearrange("(k p) f -> p k f", p=128)),
                       writes=["est"], dma=True)
                    op("pool", lambda e, dst=dst: e.tensor_tensor(out=dst, in0=est, in1=g2col.unsqueeze(2).to_broadcast([128, 8, 256]),
                                                                  op=ALU.mult), reads=["est", "g2col"], writes=[(nm, b)])
                op("sp", lambda e: e.dma_start(out=est.rearrange("p k f -> p (k f)").rearrange("p (a c) -> p a c", a=2),
                                               in_=w2_d[e_i].rearrange("(a p) c -> p a c", p=128)), writes=["est"], dma=True)
                op("pool", lambda e: e.tensor_copy(out=w2b[b], in_=est.rearrange("p k f -> p (k f)").rearrange("p (a c) -> p a c", a=2)),
                   reads=["est"], writes=[("w2", b)])

            def stageA(e_i, tb, sl):
                b = e_i % 2
                tcs = slice(tb * 512, (tb + 1) * 512)
                hk = H2T[tb * 4:tb * 4 + 4]
                cbk_ = 6
                op("pe", lambda e: e.matmul(PB(cbk_), lhsT=sel16[0:16, e_i, :], rhs=combT[0:16, tcs], start=True, stop=True),
                   reads=["sel16"] + CT[tb * 4:tb * 4 + 4], writes=[("pb", cbk_)])
                op("act", lambda e: e.copy(out=cbt[sl], in_=PB(cbk_)), reads=[("pb", cbk_)], writes=[("cbt", sl)])
                for ft in range(2):
                    fcs = slice(ft * 128, (ft + 1) * 128)
                    for k in range(8):
                        op("pe", lambda e, k=k, fcs=fcs, ft=ft: e.matmul(PB(2 + ft), lhsT=w1b[b][:, k, fcs], rhs=h2T[:, k, tcs],
                                                                         start=(k == 0), stop=(k == 7)),
                           reads=[("w1", b)] + hk, writes=[("pb", 2 + ft)])
                    op("act", lambda e, ft=ft: e.activation(out=sg[sl][ft], in_=PB(2 + ft), func=AF.Silu), reads=[("pb", 2 + ft)],
                       writes=[("sg", sl, ft)])
                    yield
                    for k in range(8):
                        op("pe", lambda e, k=k, fcs=fcs, ft=ft: e.matmul(PB(4 + ft), lhsT=w3b[b][:, k, fcs], rhs=h2T[:, k, tcs],
                                                                         start=(k == 0), stop=(k == 7)),
                           reads=[("w3", b)] + hk, writes=[("pb", 4 + ft)])
                    op("dve", lambda e, ft=ft: e.tensor_tensor(out=tu[sl][ft], in0=PB(4 + ft), in1=sg[sl][ft], op=ALU.mult),
                       reads=[("pb", 4 + ft), ("sg", sl, ft)], writes=[("tu", sl, ft)], fast=True)
                    op("pool", lambda e, ft=ft: e.tensor_tensor(out=actT[sl][ft], in0=tu[sl][ft], in1=cbt[sl], op=ALU.mult),
                       reads=[("tu", sl, ft), ("cbt", sl)], writes=[("actT", sl, ft)])
                    yield

            ybank = [0]

            def stageB(e_i, tb, sl):
                b = e_i % 2
                for t4 in range(4):
                    tt = tb * 4 + t4
                    for nb in range(2):
                        bk = (0, 1, 7)[ybank[0] % 3]
                        ybank[0] += 1
                        for ft in range(2):
                            op("pe", lambda e, ft=ft, t4=t4, nb=nb, bk=bk: e.matmul(
                                PB(bk), lhsT=actT[sl][ft][:, t4 * 128:(t4 + 1) * 128], rhs=w2b[b][:, ft, nb * 512:(nb + 1) * 512],
                                start=(ft == 0), stop=(ft == 1)), reads=[("actT", sl, ft), ("w2", b)], writes=[("pb", bk)])
                        op("dve", lambda e, tt=tt, nb=nb, bk=bk: e.tensor_tensor(out=x1[:, tt, nb * 512:(nb + 1) * 512],
                                                                                 in0=PB(bk), in1=x1[:, tt, nb * 512:(nb + 1) * 512],
                                                                                 op=ALU.add),
                           reads=[("pb", bk), ("x2", tt, nb)], writes=[("x2", tt, nb)], fast=True)
                        if nb == 1:
                            yield

            def drain(g_):
                for _ in g_:
                    pass

            units = [(e_i, tb) for e_i in range(16) for tb in range(4)]
            load_expert(0)
            load_expert(1)
            drain(stageA(units[0][0], units[0][1], 0))
            for u, (e_i, tb) in enumerate(units):
                gb = stageB(e_i, tb, u % 2)
                if u + 1 < len(units):
                    ne, ntb = units[u + 1]
                    if ne == 0:
                        for tt in range(4 * ntb, 4 * ntb + 4):
                            prep_tile(tt)
                    ga_ = stageA(ne, ntb, (u + 1) % 2)
                    drain(ga_)
                drain(gb)
                if tb == 3 and e_i + 2 < 16:
                    load_expert(e_i + 2)
            sch.barrier()
            if "x2" in dbg:
                op("sp", lambda e: e.dma_start(out=dbg["x2"].rearrange("(t p) c -> p t c", p=128), in_=x1), writes=["dbg_x2"], dma=True)
                op("sp", None, reads=["dbg_x2"])

            fa = MultiAlloc([(R_W, R_W + 64 * K)])
            ss6 = fa([128, NT], F32)
            rs6 = fa([128, NT], F32)
            junk6 = fa([128, D], BF16)
            yo = [fa([128, D], F32) for _ in range(2)]
            op("dve", lambda e: e.memset(ss6, 0.0), writes=["ss6"])
            for tt in range(NT):
                b = tt % 2
                op("act", lambda e, tt=tt: e.activation(out=junk6, in_=x1[:, tt, :], func=AF.Square, accum_out=ss6[:, tt:tt + 1]),
                   reads=["ss6"], writes=["junk6", ("ss6", tt)])
                op("act", lambda e, tt=tt: e.activation(out=rs6[:, tt:tt + 1], in_=ss6[:, tt:tt + 1], func=AF.Sqrt, bias=epsc,
                                                        scale=1.0 / D), reads=[("ss6", tt), "epsc"], writes=[("rs6", tt)])
                op("dve", lambda e, tt=tt: e.reciprocal(out=rs6[:, tt:tt + 1], in_=rs6[:, tt:tt + 1]), reads=[("rs6", tt)],
                   writes=[("rs6", tt)])
                op("dve", lambda e, tt=tt, b=b: e.scalar_tensor_tensor(out=yo[b], in0=x1[:, tt, :], scalar=rs6[:, tt:tt + 1], in1=fng,
                                                                       op0=ALU.mult, op1=ALU.mult),
                   reads=[("rs6", tt), "fng"], writes=[("yo", b)])
                op("sp", lambda e, tt=tt, b=b: e.dma_start(out=out_d[tt * 128:(tt + 1) * 128, :], in_=yo[b]), reads=[("yo", b)],
                   writes=[("out", tt)], dma=True)
            op("sp", None, reads=[("out", tt) for tt in range(NT)])


        body()
        sch.barrier()
        DEBUG["stats_pre"] = {e: len(sch.ops[e]) for e in Sched.ENGS}
        with nc.Block() as block:
            sch.emit(nc, block, engsem, dmasem)
        DEBUG["stats"] = sch.stats
    return nc


_NC_CACHE = {}


def kernel(**inputs):
    dbg = tuple(DEBUG.get("outputs", ()))
    key = (dbg, DEBUG.get("stop_after"))
    if key not in _NC_CACHE:
        _NC_CACHE[key] = build_nc(dbg, DEBUG.get("stop_after"))
    nc = _NC_CACHE[key]
    n = 8
    x = np.ascontiguousarray(inputs["x"], dtype=np.float32)
    posn = np.ascontiguousarray(inputs["positions"], dtype=np.int32)
    f32 = lambda a: np.ascontiguousarray(a, dtype=np.float32)
    inv = (10000.0 ** (-np.arange(32, dtype=np.float32) / np.float32(32))).astype(np.float32).reshape(1, 32)
    shared = {
        "norm1_g": f32(inputs["norm1_g"][0].reshape(8, 128).T),
        "w_in": f32(inputs["w_in"][0]),
        "conv_w": f32(inputs["conv_w"][0].reshape(4, 12, 128).transpose(2, 1, 0).reshape(128, 48)),
        "inv_freq": inv,
        "a_log": f32(inputs["a_log"][0].reshape(1, 8)),
        "dt_bias": f32(inputs["dt_bias"][0].reshape(1, 8)),
        "a_norm_g": f32(inputs["a_norm_g"][0].reshape(1, 64)),
        "b_gate": f32(inputs["b_gate"][0].reshape(16, 128).T),
        "norm2_g": f32(inputs["norm2_g"][0].reshape(8, 128).T),
        "final_norm_g": f32(inputs["final_norm_g"].reshape(1, D)),
        "w_proj_a": f32(inputs["w_proj_a"][0]),
        "w_proj_b": f32(inputs["w_proj_b"][0]),
        "w_out": f32(inputs["w_out"][0]),
        "w_router": f32(np.concatenate([inputs["w_router_group"][0], inputs["w_router_expert"][0]], axis=1)),
        "b_router": f32(np.concatenate([inputs["b_router_group"][0], inputs["b_router_expert"][0]], axis=0).reshape(1, 20)),
        "w_exp_gate": f32(inputs["w_exp_gate"][0]),
        "w_exp_up": f32(inputs["w_exp_up"][0]),
        "w_exp_down": f32(inputs["w_exp_down"][0]),
    }
    in_maps = []
    for c in range(n):
        m = dict(shared)
        m["x"] = x[c]
        m["positions"] = np.ascontiguousarray(posn[c].reshape(NT, 128).T)
        in_maps.append(m)
    res = run_bass_kernel_spmd(nc, in_maps, core_ids=list(range(n)))
    DEBUG["results"] = res.results
    return np.stack([r["out"] for r in res.results], axis=0)
```

```python
import math
from contextlib import ExitStack
import numpy as np
import concourse.bass as bass
import concourse.mybir as mybir
from concourse.bass_utils import run_bass_kernel_spmd

F32 = mybir.dt.float32
BF16 = mybir.dt.bfloat16
I32 = mybir.dt.int32
AF = mybir.ActivationFunctionType
ALU = mybir.AluOpType
AX = mybir.AxisListType

S = 2048
D = 1024
NT = S // 128
D_IN = 5464
EPS = 1e-6
N_DMA_SEMS = 24
NEG = -30000.0
NBIS = 12
TWO_PI = 2.0 * math.pi

C_AQ, C_AK, C_AV, C_AZ = 0, 512, 1024, 1536
C_BETA, C_ALPHA = 2048, 2056
C_BQ, C_BK, C_BV = 2064, 2576, 2704
C_IQ, C_IK, C_IW = 2832, 3344, 3408
C_GA, C_GB = 3416, 4440

DEBUG = {}
STRICT_SAME_ENGINE = True


class Sched:
    ENGS = ("pe", "act", "dve", "pool", "sp")

    def __init__(self):
        self.ops = {e: [] for e in self.ENGS}
        self.last_w = {}
        self.readers = {}
        self.dma_rr = 0
        self.dma_count = [0] * N_DMA_SEMS

    def op(self, eng, fn, reads=(), writes=(), dma=False, fast=False):
        deps = set()
        raw = set()
        for k in reads:
            t = self.last_w.get(k)
            if t is not None:
                deps.add(t)
                raw.add(t)
        for k in writes:
            t = self.last_w.get(k)
            if t is not None:
                deps.add(t)
            for t in self.readers.get(k, {}).values():
                deps.add(t)
        idx = len(self.ops[eng])
        if dma:
            si = self.dma_rr
            self.dma_rr = (self.dma_rr + 1) % N_DMA_SEMS
            prev = self.dma_count[si]
            if prev > 0:
                deps.add(("dma", si, prev))
            self.dma_count[si] = prev + 1
            tok = ("dma", si, prev + 1)
            rkey = ("dma", si)
        else:
            tok = ("eng", eng, idx)
            rkey = eng
            if STRICT_SAME_ENGINE:
                deps = {t for t in deps if not (t[0] == "eng" and t[1] == eng) or eng != "pe"}
            else:
                deps = {t for t in deps if not (t[0] == "eng" and t[1] == eng)
                        or (t in raw and eng != "pe" and not fast and idx - t[2] <= 8)}
        self.ops[eng].append(dict(fn=fn, deps=deps, signal=False, dma=(tok if dma else None)))
        for k in writes:
            self.last_w[k] = tok
            self.readers[k] = {}
        for k in reads:
            if k in writes:
                continue
            self.readers.setdefault(k, {})[rkey] = tok
        return tok

    def barrier(self):
        toks = set()
        for e in self.ENGS:
            j = len(self.ops[e]) - 1
            while j >= 0 and (self.ops[e][j]["fn"] is None or self.ops[e][j]["dma"] is not None):
                j -= 1
            if j >= 0:
                toks.add(("eng", e, j))
        for si in range(N_DMA_SEMS):
            if self.dma_count[si] > 0:
                toks.add(("dma", si, self.dma_count[si]))
        for e in self.ENGS:
            deps = {t for t in toks if not (t[0] == "eng" and t[1] == e and (e == "pe" or not STRICT_SAME_ENGINE))}
            self.ops[e].append(dict(fn=None, deps=deps, signal=False, dma=None))
        self.last_w = {}
        self.readers = {}

    def emit(self, nc, block, engsem, dmasem):
        for e in self.ENGS:
            for o in self.ops[e]:
                for t in o["deps"]:
                    if t[0] == "eng":
                        self.ops[t[1]][t[2]]["signal"] = True
        sigcount = {}
        for e in self.ENGS:
            c = 0
            lst = []
            for o in self.ops[e]:
                if o["signal"]:
                    c += 1
                lst.append(c)
            sigcount[e] = lst
        self.stats = {e: (len(self.ops[e]), sigcount[e][-1] if sigcount[e] else 0) for e in self.ENGS}

        def run(e, eng):
            waited = {}
            for o in self.ops[e]:
                need = {}
                for t in o["deps"]:
                    if t[0] == "eng":
                        key = ("eng", t[1])
                        val = sigcount[t[1]][t[2]]
                    else:
                        key = ("dma", t[1])
                        val = 16 * t[2]
                    if val > need.get(key, 0):
                        need[key] = val
                for key, val in need.items():
                    if waited.get(key, 0) >= val:
                        continue
                    waited[key] = val
                    sem = engsem[key[1]] if key[0] == "eng" else dmasem[key[1]]
                    eng.wait_ge(sem, val)
                if o["fn"] is None:
                    continue
                inst = o["fn"](eng)
                if o["dma"] is not None:
                    inst.then_inc(dmasem[o["dma"][1]], 16)
                elif o["signal"]:
                    inst.then_inc(engsem[e], 1)

        @block.tensor
        def _(eng):
            run("pe", eng)

        @block.scalar
        def _(eng):
            run("act", eng)

        @block.vector
        def _(eng):
            run("dve", eng)

        @block.gpsimd
        def _(eng):
            run("pool", eng)

        @block.sync
        def _(eng):
            run("sp", eng)


DT_SIZE = {F32: 4, BF16: 2, I32: 4}


def build_nc(debug=(), stop_after=None):
    nc = bass.Bass("TRN2", target_bir_lowering=False)

    def din(name, shape, dt=F32):
        return nc.dram_tensor(name, list(shape), dt, kind="ExternalInput").ap()

    x_d = din("x", [S, D])
    pos_d = din("positions", [128, NT], I32)
    g1_d = din("norm1_g", [128, 8])
    w_in_d = din("w_in", [D, D_IN])
    convw_d = din("conv_w", [128, 48])
    invf_d = din("inv_freq", [1, 32])
    alog_d = din("a_log", [1, 8])
    dtb_d = din("dt_bias", [1, 8])
    ang_d = din("a_norm_g", [1, 64])
    bgate_d = din("b_gate", [128, 16])
    g2_d = din("norm2_g", [128, 8])
    fng_d = din("final_norm_g", [1, D])
    wpa_d = din("w_proj_a", [512, D])
    wpb_d = din("w_proj_b", [512, D])
    wout_d = din("w_out", [D, D])
    wr_d = din("w_router", [D, 20])
    br_d = din("b_router", [1, 20])
    w1_d = din("w_exp_gate", [16, D, 256])
    w3_d = din("w_exp_up", [16, D, 256])
    w2_d = din("w_exp_down", [16, 256, D])
    out_d = nc.dram_tensor("out", [S, D], F32, kind="ExternalOutput").ap()
    dbg = {}
    for name, shape, dt in debug:
        dbg[name] = nc.dram_tensor("dbg_" + name, list(shape), dt, kind="ExternalOutput").ap()
    w_in_v = w_in_d.rearrange("(k p) c -> p k c", p=128)

    sch = Sched()
    op = sch.op
    es = ExitStack()
    with es:
        ARENA_BYTES = 207 * 1024
        arena = es.enter_context(nc.sbuf_tensor("arena", [128, ARENA_BYTES // 4], F32))

        def view(off, shape, dt):
            n = 1
            for s_ in shape[1:]:
                n *= s_
            size = n * DT_SIZE[dt]
            assert off % 4 == 0 and size % 4 == 0 and off + size <= ARENA_BYTES, (off, size)
            ap = arena[:, off // 4:(off + size) // 4]
            if dt != F32:
                ap = ap.bitcast(dt)
            if len(shape) == 3:
                ap = ap.rearrange("p (a b) -> p a b", a=shape[1])
            elif len(shape) == 4:
                ap = ap.rearrange("p (a b c) -> p a b c", a=shape[1], b=shape[2])
            return ap

        class Alloc:
            def __init__(self, base, limit):
                self.off = base
                self.limit = limit

            def __call__(self, shape, dt):
                n = 1
                for s_ in shape[1:]:
                    n *= s_
                size = (n * DT_SIZE[dt] + 63) // 64 * 64
                self.off = (self.off + 63) // 64 * 64
                v = view(self.off, shape, dt)
                self.off += size
                assert self.off <= self.limit, (self.off, self.limit)
                return v

        pbank = [es.enter_context(nc.psum_tensor("pb%d" % i, [128, 512], F32)) for i in range(8)]
        engsem = {e: es.enter_context(nc.semaphore("sem_" + e)) for e in Sched.ENGS}
        dmasem = [es.enter_context(nc.semaphore("dsem%d" % i)) for i in range(N_DMA_SEMS)]

        def PB(i):
            return pbank[i][:]

        def PBb(i):
            return pbank[i][:].bitcast(BF16)

        def body():
            K = 1024
            ca = Alloc(0, 9 * K)
            ident_f = ca([128, 128], F32)
            ident_b = ca([128, 128], BF16)
            ucs_f = ca([128, 128], F32)
            mc0_f = ca([128, 128], F32)
            mc1_f = ca([128, 128], F32)
            maskneg_f = ca([128, 128], F32)
            strict_b = ca([128, 128], BF16)
            g1col = ca([128, 8], F32)
            epsc = ca([128, 1], F32)
            cw = ca([128, 48], F32)
            invf = ca([128, 32], F32)
            dtb = ca([128, 8], F32)
            negA = ca([128, 8], F32)
            angb = ca([128, 64], F32)
            posi = ca([128, NT], I32)
            posf = ca([128, NT], F32)
            cs = ca([128, NT, 64], F32)
            assert ca.off <= 9 * K, ca.off

            op("pool", lambda e: e.memset(ident_f, 1.0), writes=["ident_f"])
            op("pool", lambda e: e.affine_select(out=ident_f, in_=ident_f, pattern=[[-1, 128]], compare_op=ALU.is_equal,
                                                 fill=0.0, base=0, channel_multiplier=1), writes=["ident_f"])
            op("pool", lambda e: e.tensor_copy(out=ident_b, in_=ident_f), reads=["ident_f"], writes=["ident_b"])
            op("pool", lambda e: e.memset(ucs_f, 1.0), writes=["ucs_f"])
            op("pool", lambda e: e.affine_select(out=ucs_f, in_=ucs_f, pattern=[[1, 128]], compare_op=ALU.is_ge,
                                                 fill=0.0, base=0, channel_multiplier=-1), writes=["ucs_f"])
            op("pool", lambda e: e.memset(ucs_f[0:64, 64:128], 0.0), writes=["ucs_f"])
            op("pool", lambda e: e.memset(mc0_f, 0.0), writes=["mc0_f"])
            op("pool", lambda e: e.memset(mc0_f[0:64, :], 1.0), writes=["mc0_f"])
            op("pool", lambda e: e.memset(mc1_f, 0.0), writes=["mc1_f"])
            op("pool", lambda e: e.memset(mc1_f[64:128, :], 1.0), writes=["mc1_f"])
            op("pool", lambda e: e.memset(maskneg_f, 0.0), writes=["maskneg_f"])
            op("pool", lambda e: e.affine_select(out=maskneg_f, in_=maskneg_f, pattern=[[-1, 128]], compare_op=ALU.is_ge,
                                                 fill=NEG, base=0, channel_multiplier=1), writes=["maskneg_f"])
            op("pool", lambda e: e.memset(maskneg_f[64:128, 0:64], NEG), writes=["maskneg_f"])
            op("pool", lambda e: e.memset(strict_b, 1.0), writes=["strict_b"])
            op("pool", lambda e: e.affine_select(out=strict_b, in_=strict_b, pattern=[[-1, 128]], compare_op=ALU.is_gt,
                                                 fill=0.0, base=0, channel_multiplier=1), writes=["strict_b"])
            op("pool", lambda e: e.memset(strict_b[64:128, 0:64], 0.0), writes=["strict_b"])
            op("dve", lambda e: e.memset(epsc, EPS), writes=["epsc"])
            op("sp", lambda e: e.dma_start(out=g1col, in_=g1_d), writes=["g1col"], dma=True)
            op("sp", lambda e: e.dma_start(out=cw, in_=convw_d), writes=["cw"], dma=True)
            op("sp", lambda e: e.dma_start(out=invf, in_=invf_d.partition_broadcast(128)), writes=["invf"], dma=True)
            op("sp", lambda e: e.dma_start(out=dtb, in_=dtb_d.partition_broadcast(128)), writes=["dtb"], dma=True)
            op("sp", lambda e: e.dma_start(out=negA, in_=alog_d.partition_broadcast(128)), writes=["negA"], dma=True)
            op("sp", lambda e: e.dma_start(out=angb, in_=ang_d.partition_broadcast(128)), writes=["angb"], dma=True)
            op("sp", lambda e: e.dma_start(out=posi, in_=pos_d), writes=["posi"], dma=True)
            op("act", lambda e: e.activation(out=negA, in_=negA, func=AF.Exp), reads=["negA"], writes=["negA"])
            op("dve", lambda e: e.tensor_scalar(out=negA, in0=negA, scalar1=-1.0, scalar2=None, op0=ALU.mult),
               reads=["negA"], writes=["negA"])

            R_A = 9 * K
            R_W = R_A + 32 * K
            R_Z = R_W + 16 * K
            R_Q = R_Z + 50 * K
            R_S = R_Q + 48 * K
            hT = view(R_A, [128, 8, S], BF16)
            wstage = view(R_W, [128, 8, 256], F32)
            wbf = [view(R_W + 8 * K + i * 4 * K, [128, 8, 256], BF16) for i in range(2)]
            zqkvT = view(R_Z, [128, 12, S + 4], BF16)
            bqT = view(R_Z, [128, 4, S], BF16)
            iqT = view(R_Z + 16 * K, [128, 4, S], BF16)
            azs = view(R_Z + 32 * K, [128, NT, 512], BF16)
            qkv_tok = view(R_Q, [128, NT, 1536], BF16)
            sa = Alloc(R_S, ARENA_BYTES)
            kz = [[sa([128, S], BF16) for _ in range(2)] for _ in range(2)]
            ikT2 = sa([128, S], BF16)
            bv_tok = sa([128, NT, 130], BF16)
            ab_tok = sa([128, NT, 16], F32)
            iw_tok = sa([128, NT, 8], F32)
            diagw = sa([128, 48, 128], BF16)
            R_WORK = sa.off

            def rope_tables():
                wa = Alloc(R_Q, R_Q + 48 * K)
                ang = wa([128, NT, 32], F32)
                tmp = wa([128, NT, 32], F32)
                ki = wa([128, NT, 32], I32)
                op("dve", lambda e: e.tensor_copy(out=posf, in_=posi), reads=["posi"], writes=["posf"])
                op("dve", lambda e: e.tensor_tensor(out=ang, in0=posf.unsqueeze(2).to_broadcast([128, NT, 32]),
                                                    in1=invf.unsqueeze(1).to_broadcast([128, NT, 32]), op=ALU.mult),
                   reads=["posf", "invf"], writes=["ang"])
                for which, shift in ((1, 0.0), (0, math.pi / 2.0)):
                    dst = cs[:, :, which * 32:(which + 1) * 32]
                    op("dve", lambda e, shift=shift: e.tensor_scalar(out=tmp, in0=ang, scalar1=shift, scalar2=None, op0=ALU.add),
                       reads=["ang"], writes=["rt_tmp"])
                    op("dve", lambda e: e.tensor_scalar(out=ki, in0=tmp, scalar1=1.0 / TWO_PI, scalar2=None, op0=ALU.mult),
                       reads=["rt_tmp"], writes=["rt_ki"])
                    op("dve", lambda e, dst=dst: e.tensor_copy(out=dst, in_=ki), reads=["rt_ki"], writes=["cs"])
                    op("dve", lambda e, dst=dst: e.scalar_tensor_tensor(out=dst, in0=dst, scalar=-TWO_PI, in1=tmp,
                                                                       op0=ALU.mult, op1=ALU.add),
                       reads=["cs", "rt_tmp"], writes=["cs"])
                    op("dve", lambda e, dst=dst: e.tensor_scalar(out=dst, in0=dst, scalar1=math.pi, scalar2=-math.pi,
                                                                op0=ALU.min, op1=ALU.max), reads=["cs"], writes=["cs"])
                    op("act", lambda e, dst=dst: e.activation(out=dst, in_=dst, func=AF.Sin), reads=["cs"], writes=["cs"])

            rope_tables()

            def phase1(hT_dst, keyp):
                wa = Alloc(R_Q + 16 * K, R_Q + 48 * K)
                xt = [wa([128, D], F32) for _ in range(2)]
                hb = [wa([128, D], BF16) for _ in range(2)]
                junk = wa([128, D], BF16)
                ss1 = wa([128, NT], F32)
                rstd1 = wa([128, NT], F32)
                op("dve", lambda e: e.memset(ss1, 0.0), writes=[keyp + "ss1"])
                for tt in range(NT):
                    b = tt % 2
                    op("sp", lambda e, tt=tt, b=b: e.dma_start(out=xt[b], in_=x_d[tt * 128:(tt + 1) * 128, :]),
                       writes=[(keyp + "xt", b)], dma=True)
                    op("act", lambda e, tt=tt, b=b: e.activation(out=junk, in_=xt[b], func=AF.Square,
                                                                 accum_out=ss1[:, tt:tt + 1]),
                       reads=[(keyp + "xt", b), keyp + "ss1"], writes=[keyp + "junk", (keyp + "ss1", tt)])
                    op("act", lambda e, tt=tt: e.activation(out=rstd1[:, tt:tt + 1], in_=ss1[:, tt:tt + 1], func=AF.Sqrt,
                                                            bias=epsc, scale=1.0 / D),
                       reads=[(keyp + "ss1", tt), "epsc"], writes=[(keyp + "rstd1", tt)])
                    op("dve", lambda e, tt=tt: e.reciprocal(out=rstd1[:, tt:tt + 1], in_=rstd1[:, tt:tt + 1]),
                       reads=[(keyp + "rstd1", tt)], writes=[(keyp + "rstd1", tt)])
                    op("dve", lambda e, tt=tt, b=b: e.tensor_scalar(out=hb[b], in0=xt[b], scalar1=rstd1[:, tt:tt + 1],
                                                                    scalar2=None, op0=ALU.mult),
                       reads=[(keyp + "xt", b), (keyp + "rstd1", tt)], writes=[(keyp + "hb", b)])
                    pbv = PBb(tt % 2)
                    for k in range(8):
                        op("pe", lambda e, k=k, b=b, pbv=pbv: e.transpose(out=pbv[:, k * 128:(k + 1) * 128],
                                                                          in_=hb[b][:, k * 128:(k + 1) * 128], identity=ident_b),
                           reads=[(keyp + "hb", b), "ident_b"], writes=[("pb", tt % 2)])
                    op("act", lambda e, tt=tt, pbv=pbv: e.copy(out=hT_dst[:, :, tt * 128:(tt + 1) * 128],
                                                               in_=pbv.rearrange("p (k t) -> p k t", k=8)),
                       reads=[("pb", tt % 2)], writes=[("hT", tt)])

            phase1(hT, "p1")
            if stop_after == "p1":
                sch.barrier()
                return
            ALL_HT = [("hT", tt) for tt in range(NT)]

            wchunk_i = [0]

            def load_w(ranges):
                i = wchunk_i[0]
                wchunk_i[0] += 1
                b = i % 2
                off = 0
                for (c0, w) in ranges:
                    op("sp", lambda e, c0=c0, w=w, off=off: e.dma_start(out=wstage[:, :, off:off + w],
                                                                        in_=w_in_v[:, :, c0:c0 + w]),
                       writes=["wstage"], dma=True)
                    off += w
                tot = off
                op("pool", lambda e, b=b, tot=tot: e.tensor_tensor(out=wbf[b][:, :, 0:tot], in0=wstage[:, :, 0:tot],
                                                                   in1=g1col.unsqueeze(2).to_broadcast([128, 8, tot]),
                                                                   op=ALU.mult),
                   reads=["wstage", "g1col"], writes=[("wbf", b)])
                return wbf[b], ("wbf", b), tot

            for ci in range(48):
                op("pool", lambda e, ci=ci: e.tensor_scalar(out=diagw[:, ci, :], in0=ident_f, scalar1=cw[:, ci:ci + 1],
                                                            scalar2=None, op0=ALU.mult),
                   reads=["ident_f", "cw"], writes=[("diagw", ci)])
            op("pool", lambda e: e.memset(zqkvT[:, :, 0:4], 0.0), writes=["zpad"])

            cva = Alloc(R_WORK, ARENA_BYTES)
            convtmp = [cva([128, 512], BF16) for _ in range(2)]
            evq = [0]

            def evac_copy(out, in_, reads, writes):
                evq[0] += 1
                if evq[0] % 2 == 0:
                    op("act", lambda e: e.copy(out=out, in_=in_), reads=reads, writes=writes)
                else:
                    op("dve", lambda e: e.tensor_copy(out=out, in_=in_), reads=reads, writes=writes)

            pbi = [0]

            def g1_proj(c, wt, wkey, ct):
                for tb in range(4):
                    bk = 2 + (pbi[0] % 2)
                    pbi[0] += 1
                    for k in range(8):
                        op("pe", lambda e, k=k, tb=tb, bk=bk: e.matmul(
                            PB(bk), lhsT=wt[:, k, ct * 128:(ct + 1) * 128], rhs=hT[:, k, tb * 512:(tb + 1) * 512],
                            start=(k == 0), stop=(k == 7)),
                           reads=[wkey] + ALL_HT[tb * 4:tb * 4 + 4], writes=[("pb", bk)])
                    evac_copy(zqkvT[:, c, 4 + tb * 512:4 + (tb + 1) * 512], PB(bk), [("pb", bk)], [("zq", c, tb)])

            def g1_conv(c):
                for tb in range(4):
                    bk = 4 + (tb % 2)
                    for j in range(4):
                        op("pe", lambda e, tb=tb, j=j, bk=bk: e.matmul(
                            PB(bk), lhsT=diagw[:, c * 4 + j, :], rhs=zqkvT[:, c, tb * 512 + j + 1:tb * 512 + j + 1 + 512],
                            start=(j == 0), stop=(j == 3)),
                           reads=[("diagw", c * 4 + j), ("zq", c, tb), "zpad"] + ([("zq", c, tb - 1)] if tb > 0 else []),
                           writes=[("pb", bk)])
                    ctb = tb % 2
                    op("act", lambda e, bk=bk, ctb=ctb: e.activation(out=convtmp[ctb], in_=PB(bk), func=AF.Silu),
                       reads=[("pb", bk)], writes=[("convtmp", ctb)])
                    tbk = 6 + (tb % 2)
                    for q in range(4):
                        op("pe", lambda e, q=q, ctb=ctb, tbk=tbk: e.transpose(out=PBb(tbk)[:, q * 128:(q + 1) * 128],
                                                                              in_=convtmp[ctb][:, q * 128:(q + 1) * 128],
                                                                              identity=ident_b),
                           reads=[("convtmp", ctb), "ident_b"], writes=[("pb", tbk)])
                    op("dve", lambda e, tb=tb, tbk=tbk: e.tensor_copy(
                        out=qkv_tok[:, tb * 4:(tb + 1) * 4, c * 128:(c + 1) * 128],
                        in_=PBb(tbk)[:, 0:512].rearrange("p (q t) -> p q t", q=4)),
                       reads=[("pb", tbk)], writes=[("qkv_tok", tb * 4 + q, c) for q in range(4)])

            prev_c = None
            nxt_w = load_w([(0, 256)])
            for chunk in range(6):
                wt, wkey, _ = nxt_w
                for ct in range(2):
                    c = chunk * 2 + ct
                    g1_proj(c, wt, wkey, ct)
                    if ct == 0:
                        nxt_w = load_w([((chunk + 1) * 256, 256)]) if chunk + 1 < 6 else load_w([(C_AZ, 256)])
                    if prev_c is not None:
                        g1_conv(prev_c)
                    prev_c = c
            g1_conv(prev_c)
            pending_w = [nxt_w]

            if "qkv_tok" in dbg:
                op("sp", lambda e: e.dma_start(out=dbg["qkv_tok"].rearrange("(t p) c -> p t c", p=128), in_=qkv_tok),
                   reads=[("qkv_tok", tt, c) for tt in range(NT) for c in range(12)], writes=["dbg_qkv_tok"], dma=True)
                op("sp", None, reads=["dbg_qkv_tok"])
            sch.barrier()
            if stop_after == "g1":
                return

            rwa = Alloc(cva.off, ARENA_BYTES)
            zr = [rwa([128, 256], F32) for _ in range(2)]
            rt = [rwa([128, 4, 32], F32) for _ in range(4)]
            roped = [rwa([128, 256], BF16) for _ in range(2)]
            op("pool", lambda e: e.memset(bv_tok, 1.0), writes=["bv_ones"])
            for a_ in range(2):
                for b_ in range(2):
                    op("pool", lambda e, a_=a_, b_=b_: e.memset(kz[a_][b_], 0.0), writes=["kz0"])

            def rope_ops(src, nh, dst_views, tt, rkey, wkeys, b):
                sv = src.rearrange("p (h d) -> p h d", h=nh)
                x1 = sv[:, :, 0:32]
                x2 = sv[:, :, 32:64]
                cc = cs[:, tt, 0:32].unsqueeze(1).to_broadcast([128, nh, 32])
                sn = cs[:, tt, 32:64].unsqueeze(1).to_broadcast([128, nh, 32])
                t = [r[:, 0:nh, :] for r in rt]
                op("dve", lambda e: e.tensor_tensor(out=t[0], in0=x1, in1=cc, op=ALU.mult), reads=[rkey, "cs"], writes=[("rt", 0)])
                op("pool", lambda e: e.tensor_tensor(out=t[1], in0=x2, in1=sn, op=ALU.mult), reads=[rkey, "cs"], writes=[("rt", 1)])
                op("pool", lambda e: e.tensor_tensor(out=t[2], in0=x2, in1=cc, op=ALU.mult), reads=[rkey, "cs"], writes=[("rt", 2)])
                op("dve", lambda e: e.tensor_tensor(out=t[3], in0=x1, in1=sn, op=ALU.mult), reads=[rkey, "cs"], writes=[("rt", 3)])
                for i, dv in enumerate(dst_views):
                    eng = "dve" if i % 2 == 0 else "pool"
                    op(eng, lambda e, dv=dv: e.tensor_tensor(out=dv[:, :, 0:32], in0=t[0], in1=t[1], op=ALU.subtract),
                       reads=[("rt", 0), ("rt", 1)], writes=wkeys)
                    op(eng, lambda e, dv=dv: e.tensor_tensor(out=dv[:, :, 32:64], in0=t[2], in1=t[3], op=ALU.add),
                       reads=[("rt", 2), ("rt", 3)], writes=wkeys)

            def tok_chunk(ranges, handler, sel=None, next_ranges=None):
                if pending_w[0] is not None:
                    wt, wkey, tot = pending_w[0]
                    pending_w[0] = None
                else:
                    wt, wkey, tot = load_w(ranges)
                lo, hi = (0, tot) if sel is None else sel
                pend = []
                for tt in range(NT):
                    if tt == 6 and next_ranges is not None:
                        pending_w[0] = load_w(next_ranges)
                    bk = 2 + (tt % 2)
                    for k in range(8):
                        op("pe", lambda e, k=k, tt=tt, bk=bk, wt=wt: e.matmul(
                            PB(bk)[:, 0:hi - lo], lhsT=hT[:, k, tt * 128:(tt + 1) * 128], rhs=wt[:, k, lo:hi],
                            start=(k == 0), stop=(k == 7)),
                           reads=[wkey, ("hT", tt)], writes=[("pb", bk)])
                    if tt >= 1:
                        pend.append(handler(tt - 1, 2 + ((tt - 1) % 2)))
                    if len(pend) >= 2:
                        p2 = pend.pop(0)
                        if p2 is not None:
                            p2()
                pend.append(handler(NT - 1, 2 + ((NT - 1) % 2)))
                for p2 in pend:
                    if p2 is not None:
                        p2()

            for j in range(2):
                def h_az(tt, bk, j=j):
                    op("act", lambda e: e.activation(out=azs[:, tt, j * 256:(j + 1) * 256], in_=PB(bk)[:, 0:256], func=AF.Silu),
                       reads=[("pb", bk)], writes=[("azs", tt, j)])
                tok_chunk([(C_AZ + j * 256, 256)], h_az, next_ranges=[(C_AZ + 256, 256)] if j == 0 else [(C_BQ, 256)])

            if stop_after == "u1":
                return
            for (c0, dstT, nm) in ((C_BQ, bqT, "bqT"), (C_IQ, iqT, "iqT")):
                for j in range(2):
                    def h_q(tt, bk, j=j, dstT=dstT, nm=nm):
                        b = tt % 2
                        op("act", lambda e: e.copy(out=zr[b], in_=PB(bk)[:, 0:256]), reads=[("pb", bk)], writes=[("zr", b)])
                        rope_ops(zr[b], 4, [roped[b].rearrange("p (h d) -> p h d", h=4)], tt, ("zr", b), [("roped", b)], b)
                        tbk = 6 + b

                        def part2():
                            for q in range(2):
                                op("pe", lambda e, q=q: e.transpose(out=PBb(tbk)[:, q * 128:(q + 1) * 128],
                                                                    in_=roped[b][:, q * 128:(q + 1) * 128], identity=ident_b),
                                   reads=[("roped", b), "ident_b"], writes=[("pb", tbk)])
                            op("act", lambda e: e.copy(out=dstT[:, 2 * j:2 * j + 2, tt * 128:(tt + 1) * 128],
                                                       in_=PBb(tbk)[:, 0:256].rearrange("p (q t) -> p q t", q=2)),
                               reads=[("pb", tbk)], writes=[(nm, tt, j)])
                        return part2
                    nr = [(c0 + 256, 256)] if j == 0 else ([(C_IQ, 256)] if c0 == C_BQ else [(C_BK, 256)])
                    tok_chunk([(c0 + j * 256, 256)], h_q, next_ranges=nr)

            if stop_after == "u23":
                return
            def h_kv(tt, bk):
                b = tt % 2
                op("act", lambda e: e.copy(out=zr[b], in_=PB(bk)[:, 0:256]), reads=[("pb", bk)], writes=[("zr", b)])
                rv = roped[b].rearrange("p (h d) -> p h d", h=4)
                rope_ops(zr[b][:, 0:128], 2, [rv[:, 0:2, :]], tt, ("zr", b), [("roped", b)], b)
                op("pool", lambda e: e.tensor_copy(out=rv[:, 2, :], in_=rv[:, 1, :]), reads=[("roped", b)], writes=[("roped", b)])
                op("pool", lambda e: e.tensor_copy(out=rv[:, 3, :], in_=rv[:, 0, :]), reads=[("roped", b)], writes=[("roped", b)])
                def part2():
                    tbk = 6 + b
                    for q in range(2):
                        op("pe", lambda e, q=q: e.transpose(out=PBb(tbk)[:, q * 128:(q + 1) * 128],
                                                            in_=roped[b][:, q * 128:(q + 1) * 128], identity=ident_b),
                           reads=[("roped", b), "ident_b"], writes=[("pb", tbk)])
                    ts_ = slice(tt * 128, (tt + 1) * 128)
                    op("act", lambda e: e.copy(out=kz[0][0][0:64, ts_], in_=PBb(tbk)[0:64, 0:128]), reads=[("pb", tbk), "kz0"],
                       writes=[("bkT", tt)])
                    op("act", lambda e: e.copy(out=kz[1][1][64:128, ts_], in_=PBb(tbk)[64:128, 0:128]), reads=[("pb", tbk)],
                       writes=[("bkT", tt)])
                    op("act", lambda e: e.copy(out=kz[1][0][0:64, ts_], in_=PBb(tbk)[0:64, 128:256]), reads=[("pb", tbk)],
                       writes=[("bkT", tt)])
                    op("act", lambda e: e.copy(out=kz[0][1][64:128, ts_], in_=PBb(tbk)[64:128, 128:256]), reads=[("pb", tbk)],
                       writes=[("bkT", tt)])

                op("dve", lambda e: e.tensor_copy(out=bv_tok[:, tt, 0:64], in_=zr[b][:, 128:192]), reads=[("zr", b), "bv_ones"],
                   writes=[("bv", tt)])
                op("dve", lambda e: e.tensor_copy(out=bv_tok[:, tt, 65:129], in_=zr[b][:, 192:256]), reads=[("zr", b)],
                   writes=[("bv", tt)])
                return part2
            tok_chunk([(C_BK, 256)], h_kv, next_ranges=[(C_IW + 8 - 256, 256)])

            if stop_after == "u4a":
                return
            IW_SCALE = (8 ** -0.5) * (64 ** -0.5)

            def h_small(tt, bk):
                b = tt % 2
                op("act", lambda e: e.copy(out=zr[b][:, 0:72], in_=PB(bk)[:, 0:72]), reads=[("pb", bk)], writes=[("zr", b)])
                rv = roped[b].rearrange("p (h d) -> p h d", h=4)
                rope_ops(zr[b][:, 0:64], 1, [rv[:, 0:1, :], rv[:, 1:2, :]], tt, ("zr", b), [("roped", b)], b)
                def part2():
                    tbk = 6 + b
                    op("pe", lambda e: e.transpose(out=PBb(tbk)[:, 0:128], in_=roped[b][:, 0:128], identity=ident_b),
                       reads=[("roped", b), "ident_b"], writes=[("pb", tbk)])
                    op("act", lambda e: e.copy(out=ikT2[:, tt * 128:(tt + 1) * 128], in_=PBb(tbk)[:, 0:128]),
                       reads=[("pb", tbk)], writes=[("ikT", tt)])

                op("dve", lambda e: e.tensor_scalar(out=iw_tok[:, tt, :], in0=zr[b][:, 64:72], scalar1=IW_SCALE, scalar2=None,
                                                    op0=ALU.mult), reads=[("zr", b)], writes=[("iw", tt)])
                return part2
            tok_chunk([(C_IW + 8 - 256, 256)], h_small, sel=(184, 256), next_ranges=[(C_BETA, 256)])

            def h_ab(tt, bk):
                op("act", lambda e: e.copy(out=ab_tok[:, tt, :], in_=PB(bk)[:, 0:16]), reads=[("pb", bk)], writes=[("ab", tt)])
            tok_chunk([(C_BETA, 256)], h_ab, sel=(0, 16))

            for nm, t_, shape in (("bqT", bqT, None), ("iqT", iqT, None)):
                if nm in dbg:
                    op("sp", lambda e, nm=nm, t_=t_: e.dma_start(out=dbg[nm].rearrange("(a p) t -> p a t", p=128), in_=t_),
                       reads=[(nm, tt, j) for tt in range(NT) for j in range(2)], writes=["dbg_" + nm], dma=True)
                    op("sp", None, reads=["dbg_" + nm])
            if "misc" in dbg:
                sch.barrier()
                mt = view(R_W, [128, NT, 154], F32)
                op("dve", lambda e: e.tensor_copy(out=mt[:, :, 0:16], in_=ab_tok), reads=[("ab", tt) for tt in range(NT)], writes=["mt"])
                op("dve", lambda e: e.tensor_copy(out=mt[:, :, 16:24], in_=iw_tok), reads=[("iw", tt) for tt in range(NT)], writes=["mt"])
                op("dve", lambda e: e.tensor_copy(out=mt[:, :, 24:154], in_=bv_tok), reads=[("bv", tt) for tt in range(NT)], writes=["mt"])
                op("sp", lambda e: e.dma_start(out=dbg["misc"].rearrange("(t p) c -> p t c", p=128), in_=mt), reads=["mt"],
                   writes=["dbg_misc"], dma=True)
                op("sp", None, reads=["dbg_misc"])
            sch.barrier()
            if stop_after == "p2":
                return

            class MultiAlloc:
                def __init__(self, regions):
                    self.regs = [[a, b] for a, b in regions]

                def __call__(self, shape, dt):
                    n = 1
                    for s_ in shape[1:]:
                        n *= s_
                    size = (n * DT_SIZE[dt] + 63) // 64 * 64
                    for r in self.regs:
                        r[0] = (r[0] + 63) // 64 * 64
                        if r[0] + size <= r[1]:
                            v = view(r[0], shape, dt)
                            r[0] += size
                            return v
                    raise AssertionError(("MultiAlloc out of space", shape, self.regs))

            def dump(name, ap, reads):
                if name in dbg:
                    op("sp", lambda e: e.dma_start(out=dbg[name], in_=ap), reads=reads, writes=["dbg_" + name], dma=True)
                    op("sp", None, reads=["dbg_" + name])

            o_aT = view(R_A, [128, 4, S], BF16)
            diagw_off = R_WORK - 12 * K
            ga = MultiAlloc([(R_W, R_W + 16 * K), (R_A + 16 * K, R_A + 32 * K), (diagw_off, ARENA_BYTES)])
            g_all = ga([128, NT, 8], F32)
            bet = ga([128, NT, 8], F32)
            gs = ga([128, 24], F32)
            eG = ga([128, 8], F32)
            eGlmG = ga([128, 8], F32)
            scs = [ga([128, 4], F32) for _ in range(2)]
            g_bc = ga([128, 8, 128], F32)
            sq = ga([128, 1024], F32)
            ssn = ga([128, 16], F32)
            rn = ga([128, 16], F32)
            cq = ga([128, 8], F32)
            cqd = ga([128, 8], F32)
            cbk = ga([128, 8], F32)
            ckd = ga([128, 8], F32)
            negbeta = ga([128, 8], F32)
            qn = ga([128, 512], BF16)
            qd = ga([128, 512], BF16)
            kn = ga([128, 512], BF16)
            rhsk = ga([128, 512], BF16)
            kdec = ga([128, 512], BF16)
            rhsv = ga([128, 512], BF16)
            qnT = ga([128, 4, 128], BF16)
            qdT = ga([128, 4, 128], BF16)
            knT = ga([128, 4, 128], BF16)
            Dm = ga([128, 8, 128], BF16)
            Ds = ga([128, 8, 128], BF16)
            Mm = [ga([128, 8, 128], BF16) for _ in range(2)]
            Nm = [ga([128, 8, 128], BF16) for _ in range(2)]
            Pm = [ga([128, 8, 128], BF16) for _ in range(2)]
            qkm = ga([128, 8, 128], BF16)
            qkT_sb = ga([128, 8, 128], BF16)
            u_c = ga([128, 2, 512], F32)
            w_tok = ga([128, 512], BF16)
            wT_sb = ga([128, 4, 128], BF16)
            vn_b = ga([128, 512], BF16)
            Sst = ga([128, 4, 128], F32)
            Stmp = ga([128, 4, 128], F32)
            S_bd = ga([128, 4, 128], BF16)
            bdmask = ga([128, 4, 128], BF16)
            o_c = ga([128, 512], F32)
            qkT_c1 = ga([128, 8, 64], BF16)
            kdec_c1 = ga([128, 512], BF16)
            az_c = ga([128, 512], BF16)
            ss2 = ga([128, 8], F32)
            r2 = ga([128, 8], F32)
            oa_b = ga([128, 512], BF16)

            def bc8(v):
                return v.unsqueeze(2).to_broadcast([128, 8, 64])

            ABK = [("ab", tt) for tt in range(NT)]
            op("act", lambda e: e.activation(out=bet, in_=ab_tok[:, :, 0:8], func=AF.Sigmoid), reads=["ab_all"], writes=["bet"])
            op("dve", lambda e: e.tensor_tensor(out=g_all, in0=ab_tok[:, :, 8:16], in1=dtb.unsqueeze(1).to_broadcast([128, NT, 8]),
                                                op=ALU.add), reads=["ab_all", "dtb"], writes=["g_all"])
            op("act", lambda e: e.activation(out=g_all, in_=g_all, func=AF.Exp), reads=["g_all"], writes=["g_all"])
            op("act", lambda e: e.activation(out=g_all, in_=g_all, func=AF.Ln, bias=1.0), reads=["g_all"], writes=["g_all"])
            op("dve", lambda e: e.tensor_tensor(out=g_all, in0=g_all, in1=negA.unsqueeze(1).to_broadcast([128, NT, 8]),
                                                op=ALU.mult), reads=["g_all", "negA"], writes=["g_all"])
            op("dve", lambda e: e.memset(Sst, 0.0), writes=["S"])
            op("dve", lambda e: e.memset(S_bd, 0.0), writes=["S_bd"])
            op("pool", lambda e: e.memset(bdmask, 0.0), writes=["bdmask"])
            op("pool", lambda e: e.memset(bdmask[0:64, :, 0:64], 1.0), writes=["bdmask"])
            op("pool", lambda e: e.memset(bdmask[64:128, :, 64:128], 1.0), writes=["bdmask"])
            if "g" in dbg:
                op("sp", lambda e: e.dma_start(out=dbg["g"].rearrange("(t p) c -> p t c", p=128), in_=g_all), reads=["g_all"],
                   writes=["dbg_g"], dma=True)
                op("sp", None, reads=["dbg_g"])

            if stop_after == "gdn_pre":
                return
            for tt in range(NT):
                op("pe", lambda e, tt=tt: e.matmul(PB(0)[:, 0:8], lhsT=ucs_f, rhs=g_all[:, tt, :], start=True, stop=True),
                   reads=["g_all"], writes=[("pb", 0)])
                op("pe", lambda e, tt=tt: e.matmul(PB(0)[:, 8:16], lhsT=mc0_f, rhs=g_all[:, tt, :], start=True, stop=True),
                   reads=["g_all"], writes=[("pb", 0)])
                op("pe", lambda e, tt=tt: e.matmul(PB(0)[:, 16:24], lhsT=mc1_f, rhs=g_all[:, tt, :], start=True, stop=True),
                   reads=["g_all"], writes=[("pb", 0)])
                op("act", lambda e: e.copy(out=gs, in_=PB(0)[:, 0:24]), reads=[("pb", 0)], writes=["gs"])
                op("act", lambda e: e.activation(out=eG, in_=gs[:, 0:8], func=AF.Exp), reads=["gs"], writes=["eG"])
                op("dve", lambda e: e.tensor_tensor(out=eGlmG[0:64, :], in0=gs[0:64, 8:16], in1=gs[0:64, 0:8], op=ALU.subtract),
                   reads=["gs"], writes=["eGlmG"])
                op("dve", lambda e: e.tensor_tensor(out=eGlmG[64:128, :], in0=gs[64:128, 16:24], in1=gs[64:128, 0:8],
                                                    op=ALU.subtract), reads=["gs"], writes=["eGlmG"])
                op("act", lambda e: e.activation(out=eGlmG, in_=eGlmG, func=AF.Exp), reads=["eGlmG"], writes=["eGlmG"])
                for hf in range(2):
                    c0 = 8 + 8 * hf
                    op("act", lambda e, hf=hf, c0=c0: e.activation(out=scs[hf][0:64, :], in_=gs[0:64, c0:c0 + 8:2], func=AF.Exp),
                       reads=["gs"], writes=[("scs", hf)])
                    op("act", lambda e, hf=hf, c0=c0: e.activation(out=scs[hf][64:128, :], in_=gs[64:128, c0 + 1:c0 + 8:2],
                                                                   func=AF.Exp), reads=["gs"], writes=[("scs", hf)])
                op("dve", lambda e, tt=tt: e.tensor_scalar(out=g_bc, in0=g_all[:, tt, :].unsqueeze(2).to_broadcast([128, 8, 128]),
                                                           scalar1=-1.0, scalar2=None, op0=ALU.mult),
                   reads=["g_all"], writes=["g_bc"])
                if stop_after == "gdn_a":
                    return
                QK = [("qkv_tok", tt, c) for c in range(8)]
                VV = [("qkv_tok", tt, c) for c in range(8, 12)]
                op("dve", lambda e, tt=tt: e.tensor_tensor(out=sq, in0=qkv_tok[:, tt, 0:1024], in1=qkv_tok[:, tt, 0:1024],
                                                           op=ALU.mult), reads=["qkv_all"], writes=["sq"])
                op("dve", lambda e: e.tensor_reduce(out=ssn, in_=sq.rearrange("p (h d) -> p h d", h=16), axis=AX.X, op=ALU.add),
                   reads=["sq"], writes=["ssn"])
                op("act", lambda e: e.activation(out=rn, in_=ssn, func=AF.Ln, bias=epsc, scale=1.0), reads=["ssn", "epsc"],
                   writes=["rn"])
                op("act", lambda e: e.activation(out=rn, in_=rn, func=AF.Exp, scale=-0.5), reads=["rn"], writes=["rn"])
                op("dve", lambda e: e.tensor_scalar(out=cq, in0=rn[:, 0:8], scalar1=0.125, scalar2=None, op0=ALU.mult),
                   reads=["rn"], writes=["cq"])
                op("dve", lambda e: e.tensor_tensor(out=cqd, in0=cq, in1=eG, op=ALU.mult), reads=["cq", "eG"], writes=["cqd"])
                op("dve", lambda e, tt=tt: e.tensor_tensor(out=cbk, in0=rn[:, 8:16], in1=bet[:, tt, :], op=ALU.mult),
                   reads=["rn", "bet"], writes=["cbk"])
                op("dve", lambda e: e.tensor_tensor(out=cbk, in0=cbk, in1=eG, op=ALU.mult), reads=["cbk", "eG"], writes=["cbk"])
                op("dve", lambda e: e.tensor_tensor(out=ckd, in0=rn[:, 8:16], in1=eGlmG, op=ALU.mult), reads=["rn", "eGlmG"],
                   writes=["ckd"])
                op("dve", lambda e, tt=tt: e.tensor_scalar(out=negbeta, in0=bet[:, tt, :], scalar1=-1.0, scalar2=None,
                                                           op0=ALU.mult), reads=["bet"], writes=["negbeta"])
                qv = qkv_tok[:, tt, 0:512].rearrange("p (h d) -> p h d", h=8)
                kv = qkv_tok[:, tt, 512:1024].rearrange("p (h d) -> p h d", h=8)
                vv = qkv_tok[:, tt, 1024:1536].rearrange("p (h d) -> p h d", h=8)

                def v3(t_):
                    return t_.rearrange("p (h d) -> p h d", h=8)
                op("dve", lambda e, qv=qv: e.tensor_tensor(out=v3(qn), in0=qv, in1=bc8(cq), op=ALU.mult),
                   reads=["qkv_all", "cq"], writes=["qn"])
                op("dve", lambda e, qv=qv: e.tensor_tensor(out=v3(qd), in0=qv, in1=bc8(cqd), op=ALU.mult),
                   reads=["qkv_all", "cqd"], writes=["qd"])
                op("dve", lambda e, kv=kv: e.tensor_tensor(out=v3(kn), in0=kv, in1=bc8(rn[:, 8:16]), op=ALU.mult),
                   reads=["qkv_all", "rn"], writes=["kn"])
                op("dve", lambda e, kv=kv: e.tensor_tensor(out=v3(rhsk), in0=kv, in1=bc8(cbk), op=ALU.mult),
                   reads=["qkv_all", "cbk"], writes=["rhsk"])
                op("dve", lambda e, kv=kv: e.tensor_tensor(out=v3(kdec), in0=kv, in1=bc8(ckd), op=ALU.mult),
                   reads=["qkv_all", "ckd"], writes=["kdec"])
                op("dve", lambda e, vv=vv, tt=tt: e.tensor_tensor(out=v3(rhsv), in0=vv, in1=bc8(bet[:, tt, :]), op=ALU.mult),
                   reads=["qkv_all", "bet"], writes=["rhsv"])
                if stop_after == "gdn_b":
                    return
                for (src, skey, dst, dkey, bank, coff, eng) in ((qn, "qn", qnT, "qnT", 6, 0, "act"), (qd, "qd", qdT, "qdT", 7, 0, "dve"),
                                                                (kn, "kn", knT, "knT", 0, 0, "act")):
                    for q in range(4):
                        op("pe", lambda e, src=src, bank=bank, coff=coff, q=q: e.transpose(
                            out=PBb(bank)[:, coff + q * 128:coff + (q + 1) * 128], in_=src[:, q * 128:(q + 1) * 128], identity=ident_b),
                           reads=[skey, "ident_b"], writes=[("pb", bank)])
                    if eng == "act":
                        op("act", lambda e, dst=dst, bank=bank, coff=coff: e.copy(
                            out=dst, in_=PBb(bank)[:, coff:coff + 512].rearrange("p (q t) -> p q t", q=4)),
                           reads=[("pb", bank)], writes=[dkey])
                    else:
                        op("dve", lambda e, dst=dst, bank=bank, coff=coff: e.tensor_copy(
                            out=dst, in_=PBb(bank)[:, coff:coff + 512].rearrange("p (q t) -> p q t", q=4)),
                           reads=[("pb", bank)], writes=[dkey])
                    if stop_after == "gdn_c_" + skey:
                        return
                if tt == 0:
                    dump("qn0", qn, ["qn"]); dump("kn0", kn, ["kn"]); dump("rhsv0", rhsv, ["rhsv"]); dump("rhsk0", rhsk, ["rhsk"])
                    dump("kdec0", kdec, ["kdec"]); dump("qd0", qd, ["qd"]); dump("gs0", gs, ["gs"])
                    dump("knT0", knT.rearrange("p a t -> p (a t)"), ["knT"])
                    dump("rn0", rn, ["rn"]); dump("cq0", cq, ["cq"]); dump("cqd0", cqd, ["cqd"]); dump("cbk0", cbk, ["cbk"])
                    dump("ckd0", ckd, ["ckd"]); dump("eG0", eG, ["eG"]); dump("ssn0", ssn, ["ssn"])
                if stop_after == "gdn_c":
                    return
                def group_gen(hg, bA, bB, bC, bT):
                    hs_list = list(range(4))
                    grp = slice(4 * hg, 4 * hg + 4)
                    for hs in hs_list:
                        h = 4 * hg + hs
                        hp, par = h // 2, h % 2
                        rows = slice(par * 64, par * 64 + 64)
                        cs_ = slice(hs * 128, hs * 128 + 128)
                        op("pe", lambda e, hp=hp, rows=rows, cs_=cs_, par=par: e.matmul(
                            PB(bA)[:, cs_], lhsT=knT[rows, hp, :], rhs=knT[rows, hp, :], start=True, stop=True,
                            tile_position=(par * 64, 0)), reads=["knT"], writes=[("pb", bA)])
                        op("pe", lambda e, hp=hp, rows=rows, cs_=cs_, par=par: e.matmul(
                            PB(bB)[:, cs_], lhsT=qnT[rows, hp, :], rhs=knT[rows, hp, :], start=True, stop=True,
                            tile_position=(par * 64, 0)), reads=["knT", "qnT"], writes=[("pb", bB)])
                        op("pe", lambda e, h=h, cs_=cs_: e.matmul(PB(bC)[:, cs_], lhsT=g_bc[:, h, :], rhs=ucs_f, start=True, stop=False),
                           reads=["g_bc", "ucs_f"], writes=[("pb", bC)])
                        op("pe", lambda e, cs_=cs_: e.matmul(PB(bC)[:, cs_], lhsT=ident_f, rhs=maskneg_f, start=False, stop=True),
                           reads=["ident_f", "maskneg_f"], writes=[("pb", bC)])
                    yield
                    for hs in hs_list:
                        h = 4 * hg + hs
                        cs_ = slice(hs * 128, hs * 128 + 128)
                        op("act", lambda e, h=h, cs_=cs_: e.activation(out=Dm[:, h, :], in_=PB(bC)[:, cs_], func=AF.Exp,
                                                                       bias=gs[:, h:h + 1], scale=1.0),
                           reads=[("pb", bC), "gs"], writes=[("Dm", h)])
                        op("dve", lambda e, h=h: e.tensor_tensor(out=Ds[:, h, :], in0=Dm[:, h, :], in1=strict_b, op=ALU.mult),
                           reads=[("Dm", h), "strict_b"], writes=[("Ds", h)])
                        op("dve", lambda e, h=h, cs_=cs_: e.scalar_tensor_tensor(out=Mm[0][:, h, :], in0=PB(bA)[:, cs_],
                                                                                 scalar=negbeta[:, h:h + 1], in1=Ds[:, h, :],
                                                                                 op0=ALU.mult, op1=ALU.mult),
                           reads=[("pb", bA), "negbeta", ("Ds", h)], writes=[("M", 0, hg)])
                        op("dve", lambda e, h=h, cs_=cs_: e.tensor_tensor(out=qkm[:, h, :], in0=PB(bB)[:, cs_], in1=Dm[:, h, :],
                                                                          op=ALU.mult),
                           reads=[("pb", bB), ("Dm", h)], writes=[("qkm", hg)])
                    yield
                    for hs in hs_list:
                        h = 4 * hg + hs
                        cs_ = slice(hs * 128, hs * 128 + 128)
                        op("pe", lambda e, h=h, cs_=cs_: e.transpose(out=PBb(bT)[:, cs_], in_=Mm[0][:, h, :], identity=ident_b),
                           reads=[("M", 0, hg), "ident_b"], writes=[("pb", bT)])
                    op("act", lambda e: e.copy(out=Nm[0][:, grp, :], in_=PBb(bT)[:, 0:512].rearrange("p (q t) -> p q t", q=4)),
                       reads=[("pb", bT)], writes=[("N", 0, hg)])
                    op("pool", lambda e: e.tensor_tensor(out=Pm[0][:, grp, :], in0=Nm[0][:, grp, :],
                                                         in1=ident_b.unsqueeze(1).to_broadcast([128, 4, 128]), op=ALU.add),
                       reads=[("N", 0, hg), "ident_b"], writes=[("P", 0, hg)])
                    yield
                    for hs in hs_list:
                        h = 4 * hg + hs
                        cs2 = slice(hs * 128, hs * 128 + 128)
                        op("pe", lambda e, h=h, cs2=cs2: e.transpose(out=PBb(bT)[:, cs2], in_=qkm[:, h, :], identity=ident_b),
                           reads=[("qkm", hg), "ident_b"], writes=[("pb", bT)])
                    op("dve", lambda e: e.tensor_copy(out=qkT_sb[:, grp, :], in_=PBb(bT)[:, 0:512].rearrange("p (q t) -> p q t", q=4)),
                       reads=[("pb", bT)], writes=[("qkT", hg)])
                    yield
                    for lv in range(1, 6):
                        cur, nxt = (lv - 1) % 2, lv % 2
                        for hs in hs_list:
                            h = 4 * hg + hs
                            cs_ = slice(hs * 128, hs * 128 + 128)
                            op("pe", lambda e, h=h, cs_=cs_, cur=cur: e.matmul(PB(bA)[:, cs_], lhsT=Nm[cur][:, h, :], rhs=Mm[cur][:, h, :],
                                                                               start=True, stop=True),
                               reads=[("N", cur, hg), ("M", cur, hg)], writes=[("pb", bA)])
                        if lv < 5:
                            for hs in hs_list:
                                h = 4 * hg + hs
                                cs_ = slice(hs * 128, hs * 128 + 128)
                                op("pe", lambda e, h=h, cs_=cs_, cur=cur: e.matmul(PB(bB)[:, cs_], lhsT=Mm[cur][:, h, :],
                                                                                   rhs=Nm[cur][:, h, :], start=True, stop=True),
                                   reads=[("N", cur, hg), ("M", cur, hg)], writes=[("pb", bB)])
                        yield
                        op("act", lambda e, nxt=nxt: e.copy(out=Mm[nxt][:, grp, :], in_=PB(bA).rearrange("p (q t) -> p q t", q=4)),
                           reads=[("pb", bA)], writes=[("M", nxt, hg)])
                        if lv < 5:
                            op("dve", lambda e, nxt=nxt: e.tensor_copy(out=Nm[nxt][:, grp, :],
                                                                       in_=PB(bB).rearrange("p (q t) -> p q t", q=4)),
                               reads=[("pb", bB)], writes=[("N", nxt, hg)])
                        for hs in hs_list:
                            h = 4 * hg + hs
                            cs_ = slice(hs * 128, hs * 128 + 128)
                            op("pe", lambda e, h=h, cs_=cs_, cur=cur, nxt=nxt: e.matmul(PB(bC)[:, cs_], lhsT=Mm[nxt][:, h, :],
                                                                                        rhs=Pm[cur][:, h, :], start=True, stop=True),
                               reads=[("M", nxt, hg), ("P", cur, hg)], writes=[("pb", bC)])
                        yield
                        op("dve", lambda e, cur=cur, nxt=nxt: e.tensor_tensor(
                            out=Pm[nxt][:, grp, :], in0=Pm[cur][:, grp, :], in1=PB(bC).rearrange("p (q t) -> p q t", q=4), op=ALU.add),
                           reads=[("pb", bC), ("P", cur, hg)], writes=[("P", nxt, hg)])

                gens = [group_gen(0, 3, 4, 5, 6), group_gen(1, 0, 1, 2, 7)]
                while gens:
                    for g_ in list(gens):
                        try:
                            next(g_)
                        except StopIteration:
                            gens.remove(g_)
                Pf = Pm[1]
                PK = [("P", 1, 0), ("P", 1, 1)]
                if tt == 0:
                    dump("D0", Dm.rearrange("p a t -> p (a t)"), [("Dm", h) for h in range(8)])
                    dump("M0", Mm[0].rearrange("p a t -> p (a t)"), [("M", 0, 0), ("M", 0, 1)])
                    dump("N0", Nm[0].rearrange("p a t -> p (a t)"), [("N", 0, 0), ("N", 0, 1)])
                    dump("P0", Pm[1].rearrange("p a t -> p (a t)"), [("P", 1, 0), ("P", 1, 1)])
                    dump("qkT0", qkT_sb.rearrange("p a t -> p (a t)"), [("qkT", 0), ("qkT", 1)])
                if stop_after == "gdn_e":
                    return
                for hf in range(2):
                    ub = 7 if hf == 0 else 0
                    for h in range(8):
                        op("pe", lambda e, h=h, hf=hf, ub=ub: e.matmul(PB(ub)[0:64, h * 64:(h + 1) * 64],
                                                                       lhsT=Pf[:, h, hf * 64:(hf + 1) * 64],
                                                                       rhs=rhsv[:, h * 64:(h + 1) * 64], start=True, stop=True),
                           reads=PK + ["rhsv"], writes=[("pb", ub)])
                    op("act", lambda e, hf=hf, ub=ub: e.copy(out=u_c[0:64, hf, :], in_=PB(ub)[0:64, :]), reads=[("pb", ub)],
                       writes=[("u_c", hf)])
                for h in range(8):
                    op("pe", lambda e, h=h: e.matmul(PB(1)[:, h * 64:(h + 1) * 64], lhsT=Pf[:, h, :], rhs=rhsk[:, h * 64:(h + 1) * 64],
                                                     start=True, stop=True), reads=PK + ["rhsk"], writes=[("pb", 1)])
                op("act", lambda e: e.copy(out=w_tok, in_=PB(1)), reads=[("pb", 1)], writes=["w_tok"])
                for q in range(4):
                    op("pe", lambda e, q=q: e.transpose(out=PBb(6)[:, q * 128:(q + 1) * 128], in_=w_tok[:, q * 128:(q + 1) * 128],
                                                        identity=ident_b), reads=["w_tok", "ident_b"], writes=[("pb", 6)])
                op("dve", lambda e: e.tensor_copy(out=wT_sb, in_=PBb(6)[:, 0:512].rearrange("p (q t) -> p q t", q=4)),
                   reads=[("pb", 6)], writes=["wT_sb"])
                op("sp", lambda e: e.dma_start(out=qkT_c1[0:64, :, :], in_=qkT_sb[64:128, :, 64:128]),
                   reads=[("qkT", 0), ("qkT", 1)], writes=["qkT_c1"], dma=True)
                op("sp", lambda e: e.dma_start(out=kdec_c1[0:64, :], in_=kdec[64:128, :]), reads=["kdec"], writes=["kdec_c1"],
                   dma=True)
                op("sp", lambda e, tt=tt: e.dma_start(out=az_c[0:64, :], in_=azs[64:128, tt, :]), reads=["azs_all"], writes=["az_c"],
                   dma=True)
                if tt == 0:
                    dump("u0", u_c.rearrange("p a t -> p (a t)"), [("u_c", 0), ("u_c", 1)])
                    dump("wT0", wT_sb.rearrange("p a t -> p (a t)"), ["wT_sb"])
                if stop_after == "gdn_f":
                    return
                for hf in range(2):
                    tcs = slice(hf * 64, hf * 64 + 64)
                    ck = 2 * tt + hf
                    if hf == 0:
                        qk_x, qk_keys = qkT_sb[0:64, :, 0:64], [("qkT", 0), ("qkT", 1)]
                        kd_x, kd_keys = kdec[0:64, :], ["kdec"]
                    else:
                        qk_x, qk_keys = qkT_c1[0:64, :, :], ["qkT_c1"]
                        kd_x, kd_keys = kdec_c1[0:64, :], ["kdec_c1"]
                    for hp in range(4):
                        op("pe", lambda e, hp=hp, tcs=tcs: e.matmul(PB(1)[0:64, hp * 128:(hp + 1) * 128], lhsT=wT_sb[:, hp, tcs],
                                                                    rhs=S_bd[:, hp, :], start=True, stop=True),
                           reads=["wT_sb", "S_bd"], writes=[("pb", 1)])
                    op("dve", lambda e, hf=hf: e.tensor_tensor(out=vn_b[0:64, :], in0=u_c[0:64, hf, :], in1=PB(1)[0:64, :],
                                                               op=ALU.subtract), reads=[("u_c", hf), ("pb", 1)], writes=["vn_b"])
                    for h in range(8):
                        hp, par = h // 2, h % 2
                        op("pe", lambda e, h=h, hp=hp, par=par, tcs=tcs: e.matmul(
                            PB(2)[0:64, h * 64:(h + 1) * 64], lhsT=qdT[:, hp, tcs], rhs=S_bd[:, hp, par * 64:(par + 1) * 64],
                            start=True, stop=False), reads=["qdT", "S_bd"], writes=[("pb", 2)])
                        op("pe", lambda e, h=h, qk_x=qk_x: e.matmul(
                            PB(2)[0:64, h * 64:(h + 1) * 64], lhsT=qk_x[:, h, :], rhs=vn_b[0:64, h * 64:(h + 1) * 64],
                            start=False, stop=True), reads=qk_keys + ["vn_b"], writes=[("pb", 2)])
                    for hp in range(4):
                        op("pe", lambda e, hp=hp, kd_x=kd_x: e.matmul(PB(7)[:, hp * 128:(hp + 1) * 128],
                                                                      lhsT=kd_x[:, hp * 128:(hp + 1) * 128],
                                                                      rhs=vn_b[0:64, hp * 128:(hp + 1) * 128], start=True, stop=True),
                           reads=kd_keys + ["vn_b"], writes=[("pb", 7)])
                    op("act", lambda e: e.copy(out=o_c[0:64, :], in_=PB(2)[0:64, :]), reads=[("pb", 2)], writes=["o_c"])
                    op("pool", lambda e, hf=hf: e.tensor_tensor(out=Stmp, in0=Sst,
                                                                in1=scs[hf].unsqueeze(2).to_broadcast([128, 4, 128]), op=ALU.mult),
                       reads=["S", ("scs", hf)], writes=["Stmp"])
                    op("dve", lambda e: e.tensor_tensor(out=Sst, in0=Stmp, in1=PB(7).rearrange("p (a d) -> p a d", a=4), op=ALU.add),
                       reads=["Stmp", ("pb", 7)], writes=["S"])
                    op("pool", lambda e: e.tensor_tensor(out=S_bd, in0=Sst, in1=bdmask, op=ALU.mult), reads=["S", "bdmask"],
                       writes=["S_bd"])
                    if "o_raw" in dbg:
                        op("sp", lambda e, ck=ck: e.dma_start(out=dbg["o_raw"][ck * 64:(ck + 1) * 64, :], in_=o_c[0:64, :]),
                           reads=["o_c"], writes=["dbg_o_raw"], dma=True)
                    sqh = sq[0:64, 0:512]
                    op("dve", lambda e: e.tensor_tensor(out=sqh, in0=o_c[0:64, :], in1=o_c[0:64, :], op=ALU.mult), reads=["o_c"],
                       writes=["sq"])
                    op("dve", lambda e: e.tensor_reduce(out=ss2[0:64, :], in_=sqh.rearrange("p (h d) -> p h d", h=8), axis=AX.X,
                                                        op=ALU.add), reads=["sq"], writes=["ss2"])
                    op("act", lambda e: e.activation(out=r2[0:64, :], in_=ss2[0:64, :], func=AF.Ln, bias=epsc[0:64, :],
                                                     scale=1.0 / 64), reads=["ss2", "epsc"], writes=["r2"])
                    op("act", lambda e: e.activation(out=r2[0:64, :], in_=r2[0:64, :], func=AF.Exp, scale=-0.5), reads=["r2"],
                       writes=["r2"])
                    op("dve", lambda e: e.tensor_tensor(out=v3(sqh), in0=v3(o_c[0:64, :]),
                                                        in1=r2[0:64, :].unsqueeze(2).to_broadcast([64, 8, 64]), op=ALU.mult),
                       reads=["o_c", "r2"], writes=["sq"])
                    op("pool", lambda e: e.tensor_tensor(out=v3(sqh), in0=v3(sqh),
                                                         in1=angb[0:64, :].unsqueeze(1).to_broadcast([64, 8, 64]), op=ALU.mult),
                       reads=["sq", "angb"], writes=["sq"])
                    az_x = azs[0:64, tt, :] if hf == 0 else az_c[0:64, :]
                    op("pool", lambda e, az_x=az_x: e.tensor_tensor(out=oa_b[0:64, :], in0=sqh, in1=az_x, op=ALU.mult),
                       reads=["sq", "az_c"], writes=["oa_b"])
                    for q in range(4):
                        op("pe", lambda e, q=q: e.transpose(out=PBb(6)[:, q * 64:(q + 1) * 64], in_=oa_b[0:64, q * 128:(q + 1) * 128],
                                                            identity=ident_b[0:64, 0:64]), reads=["oa_b", "ident_b"],
                           writes=[("pb", 6)])
                    op("act", lambda e, ck=ck: e.copy(out=o_aT[:, :, ck * 64:(ck + 1) * 64],
                                                      in_=PBb(6)[:, 0:256].rearrange("p (q t) -> p q t", q=4)),
                       reads=[("pb", 6)], writes=[("o_aT", ck)])
            if "o_raw" in dbg:
                op("sp", None, reads=["dbg_o_raw"])
            if "o_aT" in dbg:
                op("sp", lambda e: e.dma_start(out=dbg["o_aT"].rearrange("(a p) t -> p a t", p=128), in_=o_aT),
                   reads=[("o_aT", ck) for ck in range(2 * NT)], writes=["dbg_o_aT"], dma=True)
                op("sp", None, reads=["dbg_o_aT"])

            sch.barrier()
            if stop_after == "gdn":
                return

            o_bT = view(R_A + 16 * K, [128, 4, S], BF16)
            da = MultiAlloc([(R_Q, R_Q + 48 * K), (R_W, R_W + 16 * K)])
            scoreb = [da([128, S], F32) for _ in range(2)]
            rl = [da([128, 512], F32) for _ in range(2)]
            maskbb = [da([128, S], BF16) for _ in range(2)]
            thr_t = [da([128, 1], F32) for _ in range(2)]
            PTt = [[da([128, 512], BF16) for _ in range(2)] for _ in range(2)]
            I4 = da([128, 512], BF16)
            lo_t = da([128, 1], F32)
            hi_t = da([128, 1], F32)
            W0 = da([128, 1], F32)
            mid_t = da([128, 1], F32)
            tsel = da([128, 1], F32)
            Wk = da([128, NBIS], F32)
            cnt = da([128, NBIS], F32)
            pow2 = da([128, NBIS], F32)
            ob = da([128, 520], F32)
            rden = da([128, 8], F32)
            ob_b = da([128, 512], BF16)
            for q in range(4):
                op("pool", lambda e, q=q: e.tensor_copy(out=I4[:, q * 128:(q + 1) * 128], in_=ident_b), reads=["ident_b"], writes=["I4"])
            for k in range(NBIS):
                op("pool", lambda e, k=k: e.memset(pow2[:, k:k + 1], 2.0 ** (-(k + 1))), writes=["pow2"])

            xbk = [0]

            def scores_part(tt, sb):
                L = (tt + 1) * 128
                nkb = (L + 511) // 512
                qs = slice(tt * 128, (tt + 1) * 128)
                score = scoreb[sb]
                maskb = maskbb[sb]
                for h in range(8):
                    hp, par = h // 2, h % 2
                    rows = slice(par * 64, par * 64 + 64)
                    for kb in range(nkb):
                        w = min(512, L - kb * 512)
                        bank = xbk[0] % 2
                        xbk[0] += 1
                        ks = slice(kb * 512, kb * 512 + w)
                        op("pe", lambda e, hp=hp, par=par, rows=rows, w=w, bank=bank, ks=ks: e.matmul(
                            PB(bank)[:, 0:w], lhsT=iqT[rows, hp, qs], rhs=ikT2[rows, ks], start=True, stop=True,
                            tile_position=(par * 64, 0)), reads=["iqT", "ikT2"], writes=[("pb", bank)])
                        op("act", lambda e, w=w, bank=bank: e.activation(out=rl[bank][:, 0:w], in_=PB(bank)[:, 0:w], func=AF.Relu),
                           reads=[("pb", bank)], writes=[("rl", bank)])
                        if h == 0:
                            op("dve", lambda e, w=w, bank=bank, ks=ks: e.tensor_scalar(
                                out=score[:, ks], in0=rl[bank][:, 0:w], scalar1=iw_tok[:, tt, 0:1], scalar2=None, op0=ALU.mult),
                               reads=[("rl", bank), "iw_tok"], writes=[("score", sb, kb)])
                        else:
                            op("dve", lambda e, w=w, bank=bank, ks=ks, h=h: e.scalar_tensor_tensor(
                                out=score[:, ks], in0=rl[bank][:, 0:w], scalar=iw_tok[:, tt, h:h + 1], in1=score[:, ks],
                                op0=ALU.mult, op1=ALU.add), reads=[("rl", bank), "iw_tok", ("score", sb, kb)],
                               writes=[("score", sb, kb)], fast=(w >= 256))
                SK = [("score", sb, kb) for kb in range(nkb)]
                if tt >= 2:
                    op("dve", lambda e: e.tensor_reduce(out=hi_t, in_=score[:, 0:L], axis=AX.X, op=ALU.max), reads=SK, writes=["hi"])
                    op("dve", lambda e: e.tensor_reduce(out=lo_t, in_=score[:, 0:L], axis=AX.X, op=ALU.min), reads=SK, writes=["lo"])
                op("dve", lambda e: e.memset(score[0:64, L - 64:L], -1.0e30), reads=SK, writes=SK)
                if tt >= 2:
                    op("dve", lambda e: e.tensor_tensor(out=W0, in0=hi_t, in1=lo_t, op=ALU.subtract), reads=["hi", "lo"], writes=["W0"])
                    op("dve", lambda e: e.tensor_scalar(out=Wk, in0=pow2, scalar1=W0[:, 0:1], scalar2=None, op0=ALU.mult),
                       reads=["W0", "pow2"], writes=["Wk"])
                    op("dve", lambda e: e.memset(cnt, 0.0), writes=["cnt"])
                    op("dve", lambda e: e.tensor_tensor(out=mid_t, in0=lo_t, in1=Wk[:, 0:1], op=ALU.add), reads=["lo", "Wk"], writes=["mid"])
                    for k in range(NBIS):
                        op("dve", lambda e, k=k: e.tensor_scalar(out=maskb[:, 0:L], in0=score[:, 0:L], scalar1=mid_t[:, 0:1],
                                                                 scalar2=0.0, op0=ALU.is_gt, op1=ALU.add, accum_out=cnt[:, k:k + 1]),
                           reads=SK + ["mid", "cnt"], writes=[("maskb", sb), ("cntk", k)])
                        op("dve", lambda e, k=k: e.tensor_scalar(out=tsel, in0=cnt[:, k:k + 1], scalar1=255.5, scalar2=0.5,
                                                                 op0=ALU.is_gt, op1=ALU.subtract), reads=[("cntk", k)], writes=["tsel"])
                        op("dve", lambda e, k=k: e.scalar_tensor_tensor(out=mid_t, in0=tsel, scalar=Wk[:, k:k + 1], in1=mid_t,
                                                                        op0=ALU.mult, op1=ALU.add),
                           reads=["tsel", "Wk", "mid"], writes=["mid"])
                    op("dve", lambda e: e.scalar_tensor_tensor(out=thr_t[sb], in0=Wk[:, NBIS - 1:NBIS], scalar=-0.5, in1=mid_t,
                                                               op0=ALU.mult, op1=ALU.add), reads=["Wk", "mid"], writes=[("thr", sb)])
                else:
                    op("dve", lambda e: e.memset(thr_t[sb], -1.0e29), writes=[("thr", sb)])
                op("dve", lambda e: e.tensor_scalar(out=maskb[:, 0:L], in0=score[:, 0:L], scalar1=thr_t[sb][:, 0:1], scalar2=NEG,
                                                    op0=ALU.is_le, op1=ALU.mult), reads=SK + [("thr", sb)], writes=[("maskb", sb)])
                if "thr" in dbg:
                    op("sp", lambda e: e.dma_start(out=dbg["thr"][tt * 128:(tt + 1) * 128, :], in_=thr_t[sb]), reads=[("thr", sb)],
                       writes=["dbg_thr"], dma=True)
                if "score" in dbg and tt == NT - 1:
                    op("sp", lambda e: e.dma_start(out=dbg["score"], in_=score), reads=SK, writes=["dbg_score"], dma=True)

            def attn_part(tt, sb):
                qs = slice(tt * 128, (tt + 1) * 128)
                maskb = maskbb[sb]
                for kb in range(tt + 1):
                    kcs = slice(kb * 128, (kb + 1) * 128)
                    for g2 in range(2):
                        bank = 2 + g2 + 2 * (kb % 2)
                        pt = PTt[g2][kb % 2]
                        op("pe", lambda e, bank=bank, kcs=kcs: e.matmul(PB(bank), lhsT=maskb[:, kcs], rhs=I4, start=True, stop=False),
                           reads=[("maskb", sb), "I4"], writes=[("pb", bank)])
                        for s_ in range(4):
                            h = 4 * g2 + s_
                            hp, par = h // 2, h % 2
                            kT = kz[g2][par]
                            op("pe", lambda e, bank=bank, s_=s_, kT=kT, kcs=kcs, hp=hp: e.matmul(
                                PB(bank)[:, s_ * 128:(s_ + 1) * 128], lhsT=kT[:, kcs], rhs=bqT[:, hp, qs], start=False, stop=(s_ == 3)),
                               reads=["bkT", "bqT"], writes=[("pb", bank)])
                        op("act", lambda e, bank=bank, pt=pt: e.activation(out=pt, in_=PB(bank), func=AF.Exp, scale=0.125),
                           reads=[("pb", bank)], writes=[("PT", g2, kb % 2)])
                        for s_ in range(4):
                            op("pe", lambda e, g2=g2, s_=s_, pt=pt, kb=kb: e.matmul(
                                PB(6 + g2)[:, s_ * 65:(s_ + 1) * 65], lhsT=pt[:, s_ * 128:(s_ + 1) * 128],
                                rhs=bv_tok[:, kb, g2 * 65:(g2 + 1) * 65], start=(kb == 0 and s_ == 0), stop=(kb == tt and s_ == 3)),
                               reads=[("PT", g2, kb % 2), "bv_tok"], writes=[("pb", 6 + g2)])
                op("act", lambda e: e.copy(out=ob[:, 0:260], in_=PB(6)[:, 0:260]), reads=[("pb", 6)], writes=["ob"])
                op("act", lambda e: e.copy(out=ob[:, 260:520], in_=PB(7)[:, 0:260]), reads=[("pb", 7)], writes=["ob"])
                obv = ob.rearrange("p (s e) -> p s e", e=65)
                op("dve", lambda e: e.reciprocal(out=rden, in_=obv[:, :, 64]), reads=["ob"], writes=["rden"])
                op("dve", lambda e: e.tensor_tensor(out=ob_b.rearrange("p (h d) -> p h d", h=8), in0=obv[:, :, 0:64],
                                                    in1=rden.unsqueeze(2).to_broadcast([128, 8, 64]), op=ALU.mult),
                   reads=["ob", "rden"], writes=["ob_b"])
                for q in range(4):
                    op("pe", lambda e, q=q: e.transpose(out=PBb(0)[:, q * 128:(q + 1) * 128], in_=ob_b[:, q * 128:(q + 1) * 128],
                                                        identity=ident_b), reads=["ob_b", "ident_b"], writes=[("pb", 0)])
                op("act", lambda e: e.copy(out=o_bT[:, :, qs], in_=PBb(0)[:, 0:512].rearrange("p (q t) -> p q t", q=4)),
                   reads=[("pb", 0)], writes=[("o_bT", tt)])

            scores_part(0, 0)
            for tt in range(NT):
                if tt + 1 < NT:
                    scores_part(tt + 1, (tt + 1) % 2)
                attn_part(tt, tt % 2)
            if "thr" in dbg:
                op("sp", None, reads=["dbg_thr"])
            if "score" in dbg:
                op("sp", None, reads=["dbg_score"])
            if "o_bT" in dbg:
                op("sp", lambda e: e.dma_start(out=dbg["o_bT"].rearrange("(a p) t -> p a t", p=128), in_=o_bT),
                   reads=[("o_bT", tt) for tt in range(NT)], writes=["dbg_o_bT"], dma=True)
                op("sp", None, reads=["dbg_o_bT"])

            sch.barrier()
            if stop_after == "dsa":
                return

            pa = MultiAlloc([(R_W, ARENA_BYTES)])
            hT2 = pa([128, 8, S], BF16)
            mergedT = pa([128, 8, S], BF16)
            x1 = pa([128, NT, D], F32)
            bgate = pa([128, 16], F32)
            g2col = pa([128, 8], F32)
            fng = pa([128, D], F32)
            wst2 = pa([128, 8, 256], F32)
            wg_bf = [pa([128, 8, 256], BF16) for _ in range(2)]
            wp_bf = [pa([128, 4, 256], BF16) for _ in range(2)]
            ga_s = pa([128, 512], BF16)
            gb_s = pa([128, 512], BF16)
            t1 = pa([128, 512], BF16)
            t2 = pa([128, 512], BF16)
            xt4 = [pa([128, D], F32) for _ in range(2)]
            op("sp", lambda e: e.dma_start(out=bgate, in_=bgate_d), writes=["bgate"], dma=True)
            op("sp", lambda e: e.dma_start(out=g2col, in_=g2_d), writes=["g2col"], dma=True)
            op("sp", lambda e: e.dma_start(out=fng, in_=fng_d.partition_broadcast(128)), writes=["fng"], dma=True)

            def phase1b():
                xa = Alloc(R_W + 64 * K, R_W + 128 * K)
                xt = [xa([128, D], F32) for _ in range(2)]
                hb = [xa([128, D], BF16) for _ in range(2)]
                junk = xa([128, D], BF16)
                ssx = xa([128, NT], F32)
                rsx = xa([128, NT], F32)
                op("dve", lambda e: e.memset(ssx, 0.0), writes=["ssx"])
                for tt in range(NT):
                    b = tt % 2
                    op("sp", lambda e, tt=tt, b=b: e.dma_start(out=xt[b], in_=x_d[tt * 128:(tt + 1) * 128, :]), writes=[("xt", b)], dma=True)
                    op("act", lambda e, tt=tt, b=b: e.activation(out=junk, in_=xt[b], func=AF.Square, accum_out=ssx[:, tt:tt + 1]),
                       reads=[("xt", b), "ssx"], writes=["junk", ("ssx", tt)])
                    op("act", lambda e, tt=tt: e.activation(out=rsx[:, tt:tt + 1], in_=ssx[:, tt:tt + 1], func=AF.Sqrt, bias=epsc,
                                                            scale=1.0 / D), reads=[("ssx", tt), "epsc"], writes=[("rsx", tt)])
                    op("dve", lambda e, tt=tt: e.reciprocal(out=rsx[:, tt:tt + 1], in_=rsx[:, tt:tt + 1]), reads=[("rsx", tt)],
                       writes=[("rsx", tt)])
                    op("dve", lambda e, tt=tt, b=b: e.tensor_scalar(out=hb[b], in0=xt[b], scalar1=rsx[:, tt:tt + 1], scalar2=None,
                                                                    op0=ALU.mult), reads=[("xt", b), ("rsx", tt)], writes=[("hb", b)])
                    bk = tt % 2
                    for k in range(8):
                        op("pe", lambda e, k=k, b=b, bk=bk: e.transpose(out=PBb(bk)[:, k * 128:(k + 1) * 128],
                                                                        in_=hb[b][:, k * 128:(k + 1) * 128], identity=ident_b),
                           reads=[("hb", b), "ident_b"], writes=[("pb", bk)])
                    op("act", lambda e, tt=tt, bk=bk: e.copy(out=hT2[:, :, tt * 128:(tt + 1) * 128],
                                                             in_=PBb(bk).rearrange("p (k t) -> p k t", k=8)),
                       reads=[("pb", bk)], writes=[("hT2", tt)])
            phase1b()
            sch.barrier()
            HT2 = [("hT2", tt) for tt in range(NT)]

            wpa_v = wpa_d.rearrange("(k p) c -> p k c", p=128)
            wpb_v = wpb_d.rearrange("(k p) c -> p k c", p=128)
            wout_v = wout_d.rearrange("(k p) c -> p k c", p=128)
            wi4 = [0]

            wst2b = xt4[0].rearrange("p (k c) -> p k c", k=4)
            wst2c = xt4[1].rearrange("p (k c) -> p k c", k=4)
            wi5 = [0]

            def load_gate(c0):
                i = wi4[0]
                wi4[0] += 1
                b = i % 2
                op("sp", lambda e: e.dma_start(out=wst2, in_=w_in_v[:, :, c0:c0 + 256]), writes=["wst2"], dma=True)
                op("pool", lambda e, b=b: e.tensor_tensor(out=wg_bf[b], in0=wst2, in1=g1col.unsqueeze(2).to_broadcast([128, 8, 256]),
                                                          op=ALU.mult), reads=["wst2", "g1col"], writes=[("wg", b)])
                return wg_bf[b], ("wg", b)

            def load_proj(src_v, c0):
                i = wi5[0]
                wi5[0] += 1
                b = i % 2
                st = wst2b if b == 0 else wst2c
                op("sp", lambda e: e.dma_start(out=st, in_=src_v[:, :, c0:c0 + 256]), writes=[("wstp", b)], dma=True)
                op("pool", lambda e, b=b: e.tensor_copy(out=wp_bf[b], in_=st), reads=[("wstp", b)], writes=[("wp", b)])
                return wp_bf[b], ("wp", b)

            p4u = [0]
            for j in range(4):
                wga, kga = load_gate(C_GA + j * 256)
                wgb, kgb = load_gate(C_GB + j * 256)
                wpa, kpa = load_proj(wpa_v, j * 256)
                wpb, kpb = load_proj(wpb_v, j * 256)
                for ct in range(2):
                    c = 2 * j + ct
                    ccs = slice(ct * 128, (ct + 1) * 128)
                    for tb in range(4):
                        tcs = slice(tb * 512, (tb + 1) * 512)
                        hk = HT2[tb * 4:tb * 4 + 4]
                        bA, bB, bC, bD = (2, 3, 4, 5) if (p4u[0] % 2 == 0) else (0, 1, 6, 7)
                        p4u[0] += 1
                        for k in range(8):
                            op("pe", lambda e, k=k, ccs=ccs, tcs=tcs, wga=wga, bA=bA: e.matmul(PB(bA), lhsT=wga[:, k, ccs], rhs=hT2[:, k, tcs],
                                                                                         start=(k == 0), stop=(k == 7)),
                               reads=[kga] + hk, writes=[("pb", bA)])
                        op("act", lambda e, c=c, bA=bA: e.activation(out=ga_s, in_=PB(bA), func=AF.Sigmoid, bias=bgate[:, c:c + 1], scale=1.0),
                           reads=[("pb", bA), "bgate"], writes=["ga_s"])
                        for k in range(8):
                            op("pe", lambda e, k=k, ccs=ccs, tcs=tcs, wgb=wgb, bB=bB: e.matmul(PB(bB), lhsT=wgb[:, k, ccs], rhs=hT2[:, k, tcs],
                                                                                         start=(k == 0), stop=(k == 7)),
                               reads=[kgb] + hk, writes=[("pb", bB)])
                        op("act", lambda e, c=c, bB=bB: e.activation(out=gb_s, in_=PB(bB), func=AF.Sigmoid, bias=bgate[:, 8 + c:9 + c], scale=1.0),
                           reads=[("pb", bB), "bgate"], writes=["gb_s"])
                        for hp in range(4):
                            op("pe", lambda e, hp=hp, ccs=ccs, tcs=tcs, wpa=wpa, bC=bC: e.matmul(PB(bC), lhsT=wpa[:, hp, ccs], rhs=o_aT[:, hp, tcs],
                                                                                           start=(hp == 0), stop=(hp == 3)),
                               reads=[kpa, "o_aT"], writes=[("pb", bC)])
                        for hp in range(4):
                            op("pe", lambda e, hp=hp, ccs=ccs, tcs=tcs, wpb=wpb, bD=bD: e.matmul(PB(bD), lhsT=wpb[:, hp, ccs], rhs=o_bT[:, hp, tcs],
                                                                                           start=(hp == 0), stop=(hp == 3)),
                               reads=[kpb, "o_bT"], writes=[("pb", bD)])
                        op("dve", lambda e, bC=bC: e.tensor_tensor(out=t1, in0=PB(bC), in1=ga_s, op=ALU.mult), reads=[("pb", bC), "ga_s"],
                           writes=["t1"])
                        op("dve", lambda e, bD=bD: e.tensor_tensor(out=t2, in0=PB(bD), in1=gb_s, op=ALU.mult), reads=[("pb", bD), "gb_s"],
                           writes=["t2"])
                        op("pool", lambda e, c=c, tcs=tcs: e.tensor_tensor(out=mergedT[:, c, tcs], in0=t1, in1=t2, op=ALU.add),
                           reads=["t1", "t2"], writes=[("mergedT", c, tb)])
            if "mergedT" in dbg:
                op("sp", lambda e: e.dma_start(out=dbg["mergedT"].rearrange("(a p) t -> p a t", p=128), in_=mergedT),
                   reads=[("mergedT", c, tb) for c in range(8) for tb in range(4)], writes=["dbg_mergedT"], dma=True)
                op("sp", None, reads=["dbg_mergedT"])
            sch.barrier()
            wout_bf = view(R_A, [128, 8, D], BF16)
            for j in range(4):
                op("sp", lambda e, j=j: e.dma_start(out=wst2, in_=wout_v[:, :, j * 256:(j + 1) * 256]), writes=["wst2"], dma=True)
                op("pool", lambda e, j=j: e.tensor_copy(out=wout_bf[:, :, j * 256:(j + 1) * 256], in_=wst2), reads=["wst2"],
                   writes=[("wout", j)])
            WOUT = [("wout", j) for j in range(4)]
            for tt in range(NT):
                b = tt % 2
                op("sp", lambda e, tt=tt, b=b: e.dma_start(out=xt4[b], in_=x_d[tt * 128:(tt + 1) * 128, :]), writes=[("xt4", b)], dma=True)
                for nb in range(2):
                    bk = 2 + 2 * b + nb
                    for c in range(8):
                        op("pe", lambda e, c=c, tt=tt, nb=nb, bk=bk: e.matmul(PB(bk), lhsT=mergedT[:, c, tt * 128:(tt + 1) * 128],
                                                                               rhs=wout_bf[:, c, nb * 512:(nb + 1) * 512],
                                                                               start=(c == 0), stop=(c == 7)),
                           reads=WOUT + ["mergedT_all"], writes=[("pb", bk)])
                    op("dve", lambda e, tt=tt, nb=nb, bk=bk, b=b: e.tensor_tensor(out=x1[:, tt, nb * 512:(nb + 1) * 512], in0=PB(bk),
                                                                                   in1=xt4[b][:, nb * 512:(nb + 1) * 512], op=ALU.add),
                       reads=[("pb", bk), ("xt4", b)], writes=[("x1", tt)])
            if "x1" in dbg:
                op("sp", lambda e: e.dma_start(out=dbg["x1"].rearrange("(t p) c -> p t c", p=128), in_=x1),
                   reads=[("x1", tt) for tt in range(NT)], writes=["dbg_x1"], dma=True)
                op("sp", None, reads=["dbg_x1"])
            sch.barrier()
            if stop_after == "p4":
                return

            h2T = view(R_W, [128, 8, S], BF16)
            ma = MultiAlloc([(R_W + 32 * K, R_W + 64 * K), (R_A, R_A + 32 * K)])
            tail_off = None
            hb2 = [ma([128, D], BF16) for _ in range(2)]
            junk2 = ma([128, D], BF16)
            ss5 = ma([128, NT], F32)
            rs5 = ma([128, NT], F32)
            wr_st = ma([128, 8, 20], F32)
            wr_bf = ma([128, 8, 20], BF16)
            brow = ma([128, 20], F32)
            lg = ma([128, 20], F32)
            sm = {n_: ma([128, 4], F32) for n_ in ("goh", "gex", "elg", "oh1", "msk", "oh2", "wsel")}
            sc1 = {n_: ma([128, 1], F32) for n_ in ("gmax", "ngmax", "gsum", "ggate", "m1", "m2", "d21", "e21", "den", "w1", "w2")}
            tmp44 = ma([128, 4, 4], F32)
            comb_b = ma([128, 16], BF16)
            combT = ma([128, S], BF16)
            sel16 = ma([128, 16, 128], BF16)
            est = ma([128, 8, 256], F32)
            w1b = [ma([128, 8, 256], BF16) for _ in range(2)]
            w3b = [ma([128, 8, 256], BF16) for _ in range(2)]
            w2b = [ma([128, 2, D], BF16) for _ in range(2)]
            sg = [[ma([128, 512], BF16) for _ in range(2)] for _ in range(2)]
            cbt = [ma([128, 512], BF16) for _ in range(2)]
            tu = [[ma([128, 512], BF16) for _ in range(2)] for _ in range(2)]
            actT = [[ma([128, 512], BF16) for _ in range(2)] for _ in range(2)]
            op("sp", lambda e: e.dma_start(out=wr_st, in_=wr_d.rearrange("(k p) c -> p k c", p=128)), writes=["wr_st"], dma=True)
            op("sp", lambda e: e.dma_start(out=brow, in_=br_d.partition_broadcast(128)), writes=["brow"], dma=True)
            op("pool", lambda e: e.tensor_tensor(out=wr_bf, in0=wr_st, in1=g2col.unsqueeze(2).to_broadcast([128, 8, 20]), op=ALU.mult),
               reads=["wr_st", "g2col"], writes=["wr_bf"])
            op("pool", lambda e: e.memset(sel16[0:16, :, :], 1.0), writes=["sel16"])
            op("pool", lambda e: e.affine_select(out=sel16[0:16, :, :], in_=sel16[0:16, :, :], pattern=[[-1, 16], [0, 128]],
                                                 compare_op=ALU.is_equal, fill=0.0, base=0, channel_multiplier=1), writes=["sel16"])
            op("dve", lambda e: e.memset(ss5, 0.0), writes=["ss5"])
            def prep_tile(tt):
                b = tt % 2
                bk = tt % 2
                op("act", lambda e, tt=tt: e.activation(out=junk2, in_=x1[:, tt, :], func=AF.Square, accum_out=ss5[:, tt:tt + 1]),
                   reads=["x1_all", "ss5"], writes=["junk2", ("ss5", tt)])
                op("act", lambda e, tt=tt: e.activation(out=rs5[:, tt:tt + 1], in_=ss5[:, tt:tt + 1], func=AF.Ln, bias=epsc,
                                                        scale=1.0 / D), reads=[("ss5", tt), "epsc"], writes=[("rs5", tt)])
                op("act", lambda e, tt=tt: e.activation(out=rs5[:, tt:tt + 1], in_=rs5[:, tt:tt + 1], func=AF.Exp, scale=-0.5),
                   reads=[("rs5", tt)], writes=[("rs5", tt)])
                op("dve", lambda e, tt=tt, b=b: e.tensor_scalar(out=hb2[b], in0=x1[:, tt, :], scalar1=rs5[:, tt:tt + 1], scalar2=None,
                                                                op0=ALU.mult), reads=["x1_all", ("rs5", tt)], writes=[("hb2", b)])
                for k in range(8):
                    op("pe", lambda e, k=k, b=b, bk=bk: e.transpose(out=PBb(bk)[:, k * 128:(k + 1) * 128],
                                                                    in_=hb2[b][:, k * 128:(k + 1) * 128], identity=ident_b),
                       reads=[("hb2", b), "ident_b"], writes=[("pb", bk)])
                op("act", lambda e, tt=tt, bk=bk: e.copy(out=h2T[:, :, tt * 128:(tt + 1) * 128],
                                                         in_=PBb(bk).rearrange("p (k t) -> p k t", k=8)),
                   reads=[("pb", bk)], writes=[("h2T", tt)])
                for k in range(8):
                    op("pe", lambda e, k=k, tt=tt: e.matmul(PB(2)[:, 0:20], lhsT=h2T[:, k, tt * 128:(tt + 1) * 128], rhs=wr_bf[:, k, :],
                                                            start=(k == 0), stop=(k == 7)),
                       reads=[("h2T", tt), "wr_bf"], writes=[("pb", 2)])
                R = []

                def rop(fn, rd, wr):
                    op("dve", fn, reads=rd, writes=wr)
                rop(lambda e: e.tensor_tensor(out=lg, in0=PB(2)[:, 0:20], in1=brow, op=ALU.add), [("pb", 2), "brow"], ["lg"])
                elv = lg[:, 4:20].rearrange("p (g x) -> p g x", g=4)
                rop(lambda e: e.tensor_reduce(out=sc1["gmax"], in_=lg[:, 0:4], axis=AX.X, op=ALU.max), ["lg"], ["gmax"])
                rop(lambda e: e.tensor_scalar(out=sm["goh"], in0=lg[:, 0:4], scalar1=sc1["gmax"][:, 0:1], scalar2=None,
                                              op0=ALU.is_equal), ["lg", "gmax"], ["goh"])
                rop(lambda e: e.tensor_scalar(out=sc1["ngmax"], in0=sc1["gmax"], scalar1=-1.0, scalar2=None, op0=ALU.mult),
                    ["gmax"], ["ngmax"])
                op("act", lambda e: e.activation(out=sm["gex"], in_=lg[:, 0:4], func=AF.Exp, bias=sc1["ngmax"][:, 0:1], scale=1.0),
                   reads=["lg", "ngmax"], writes=["gex"])
                rop(lambda e: e.tensor_reduce(out=sc1["gsum"], in_=sm["gex"], axis=AX.X, op=ALU.add), ["gex"], ["gsum"])
                rop(lambda e: e.reciprocal(out=sc1["ggate"], in_=sc1["gsum"]), ["gsum"], ["ggate"])
                rop(lambda e: e.tensor_tensor(out=tmp44, in0=elv, in1=sm["goh"].unsqueeze(2).to_broadcast([128, 4, 4]), op=ALU.mult),
                    ["lg", "goh"], ["tmp44"])
                rop(lambda e: e.tensor_reduce(out=sm["elg"], in_=tmp44.rearrange("p g x -> p x g"), axis=AX.X, op=ALU.add),
                    ["tmp44"], ["elg"])
                rop(lambda e: e.tensor_reduce(out=sc1["m1"], in_=sm["elg"], axis=AX.X, op=ALU.max), ["elg"], ["m1"])
                rop(lambda e: e.tensor_scalar(out=sm["oh1"], in0=sm["elg"], scalar1=sc1["m1"][:, 0:1], scalar2=None, op0=ALU.is_equal),
                    ["elg", "m1"], ["oh1"])
                rop(lambda e: e.scalar_tensor_tensor(out=sm["msk"], in0=sm["oh1"], scalar=-1.0e30, in1=sm["elg"], op0=ALU.mult,
                                                     op1=ALU.add), ["oh1", "elg"], ["msk"])
                rop(lambda e: e.tensor_reduce(out=sc1["m2"], in_=sm["msk"], axis=AX.X, op=ALU.max), ["msk"], ["m2"])
                rop(lambda e: e.tensor_scalar(out=sm["oh2"], in0=sm["msk"], scalar1=sc1["m2"][:, 0:1], scalar2=None, op0=ALU.is_equal),
                    ["msk", "m2"], ["oh2"])
                rop(lambda e: e.tensor_tensor(out=sc1["d21"], in0=sc1["m2"], in1=sc1["m1"], op=ALU.subtract), ["m1", "m2"], ["d21"])
                op("act", lambda e: e.activation(out=sc1["e21"], in_=sc1["d21"], func=AF.Exp), reads=["d21"], writes=["e21"])
                rop(lambda e: e.tensor_scalar(out=sc1["den"], in0=sc1["e21"], scalar1=1.0, scalar2=None, op0=ALU.add), ["e21"], ["den"])
                rop(lambda e: e.reciprocal(out=sc1["den"], in_=sc1["den"]), ["den"], ["den"])
                rop(lambda e: e.tensor_tensor(out=sc1["w1"], in0=sc1["ggate"], in1=sc1["den"], op=ALU.mult), ["ggate", "den"], ["w1"])
                rop(lambda e: e.tensor_tensor(out=sc1["w2"], in0=sc1["w1"], in1=sc1["e21"], op=ALU.mult), ["w1", "e21"], ["w2"])
                rop(lambda e: e.tensor_scalar(out=sm["wsel"], in0=sm["oh1"], scalar1=sc1["w1"][:, 0:1], scalar2=None, op0=ALU.mult),
                    ["oh1", "w1"], ["wsel"])
                rop(lambda e: e.scalar_tensor_tensor(out=sm["wsel"], in0=sm["oh2"], scalar=sc1["w2"][:, 0:1], in1=sm["wsel"],
                                                     op0=ALU.mult, op1=ALU.add), ["oh2", "w2", "wsel"], ["wsel"])
                rop(lambda e: e.tensor_tensor(out=comb_b.rearrange("p (g x) -> p g x", g=4),
                                              in0=sm["goh"].unsqueeze(2).to_broadcast([128, 4, 4]),
                                              in1=sm["wsel"].unsqueeze(1).to_broadcast([128, 4, 4]), op=ALU.mult),
                    ["goh", "wsel"], ["comb_b"])
                if "comb" in dbg:
                    op("sp", lambda e, tt=tt: e.dma_start(out=dbg["comb"][tt * 128:(tt + 1) * 128, :], in_=comb_b), reads=["comb_b"],
                       writes=["dbg_comb"], dma=True)
                op("pe", lambda e: e.transpose(out=PBb(3)[0:16, 0:128], in_=comb_b, identity=ident_b), reads=["comb_b", "ident_b"],
                   writes=[("pb", 3)])
                op("act", lambda e, tt=tt: e.copy(out=combT[0:16, tt * 128:(tt + 1) * 128], in_=PBb(3)[0:16, 0:128]),
                   reads=[("pb", 3)], writes=[("combT", tt)])
            for tt in range(4):
                prep_tile(tt)
            H2T = [("h2T", tt) for tt in range(NT)]
            CT = [("combT", tt) for tt in range(NT)]

            def load_expert(e_i):
                b = e_i % 2
                for (src, dst, nm, fold) in ((w1_d, w1b[b], "w1", True), (w3_d, w3b[b], "w3", True)):
                    op("sp", lambda e, src=src: e.dma_start(out=est, in_=src[e_i].rearrange("(k p) f -> p k f", p=128)),
                       writes=["est"], dma=True)
                    op("pool", lambda e, dst=dst: e.tensor_tensor(out=dst, in0=est, in1=g2col.unsqueeze(2).to_broadcast([128, 8, 256]),
                                                                  op=ALU.mult), reads=["est", "g2col"], writes=[(nm, b)])
                op("sp", lambda e: e.dma_start(out=est.rearrange("p k f -> p (k f)").rearrange("p (a c) -> p a c", a=2),
                                               in_=w2_d[e_i].rearrange("(a p) c -> p a c", p=128)), writes=["est"], dma=True)
                op("pool", lambda e: e.tensor_copy(out=w2b[b], in_=est.rearrange("p k f -> p (k f)").rearrange("p (a c) -> p a c", a=2)),
                   reads=["est"], writes=[("w2", b)])

            def stageA(e_i, tb, sl):
                b = e_i % 2
                tcs = slice(tb * 512, (tb + 1) * 512)
                hk = H2T[tb * 4:tb * 4 + 4]
                cbk_ = 6
                op("pe", lambda e: e.matmul(PB(cbk_), lhsT=sel16[0:16, e_i, :], rhs=combT[0:16, tcs], start=True, stop=True),
                   reads=["sel16"] + CT[tb * 4:tb * 4 + 4], writes=[("pb", cbk_)])
                op("act", lambda e: e.copy(out=cbt[sl], in_=PB(cbk_)), reads=[("pb", cbk_)], writes=[("cbt", sl)])
                for ft in range(2):
                    fcs = slice(ft * 128, (ft + 1) * 128)
                    for k in range(8):
                        op("pe", lambda e, k=k, fcs=fcs, ft=ft: e.matmul(PB(2 + ft), lhsT=w1b[b][:, k, fcs], rhs=h2T[:, k, tcs],
                                                                         start=(k == 0), stop=(k == 7)),
                           reads=[("w1", b)] + hk, writes=[("pb", 2 + ft)])
                    op("act", lambda e, ft=ft: e.activation(out=sg[sl][ft], in_=PB(2 + ft), func=AF.Silu), reads=[("pb", 2 + ft)],
                       writes=[("sg", sl, ft)])
                    yield
                    for k in range(8):
                        op("pe", lambda e, k=k, fcs=fcs, ft=ft: e.matmul(PB(4 + ft), lhsT=w3b[b][:, k, fcs], rhs=h2T[:, k, tcs],
                                                                         start=(k == 0), stop=(k == 7)),
                           reads=[("w3", b)] + hk, writes=[("pb", 4 + ft)])
                    op("dve", lambda e, ft=ft: e.tensor_tensor(out=tu[sl][ft], in0=PB(4 + ft), in1=sg[sl][ft], op=ALU.mult),
                       reads=[("pb", 4 + ft), ("sg", sl, ft)], writes=[("tu", sl, ft)], fast=True)
                    op("pool", lambda e, ft=ft: e.tensor_tensor(out=actT[sl][ft], in0=tu[sl][ft], in1=cbt[sl], op=ALU.mult),
                       reads=[("tu", sl, ft), ("cbt", sl)], writes=[("actT", sl, ft)])
                    yield

            ybank = [0]

            def stageB(e_i, tb, sl):
                b = e_i % 2
                for t4 in range(4):
                    tt = tb * 4 + t4
                    for nb in range(2):
                        bk = (0, 1, 7)[ybank[0] % 3]
                        ybank[0] += 1
                        for ft in range(2):
                            op("pe", lambda e, ft=ft, t4=t4, nb=nb, bk=bk: e.matmul(
                                PB(bk), lhsT=actT[sl][ft][:, t4 * 128:(t4 + 1) * 128], rhs=w2b[b][:, ft, nb * 512:(nb + 1) * 512],
                                start=(ft == 0), stop=(ft == 1)), reads=[("actT", sl, ft), ("w2", b)], writes=[("pb", bk)])
                        op("dve", lambda e, tt=tt, nb=nb, bk=bk: e.tensor_tensor(out=x1[:, tt, nb * 512:(nb + 1) * 512],
                                                                                 in0=PB(bk), in1=x1[:, tt, nb * 512:(nb + 1) * 512],
                                                                                 op=ALU.add),
                           reads=[("pb", bk), ("x2", tt, nb)], writes=[("x2", tt, nb)], fast=True)
                        if nb == 1:
                            yield

            def drain(g_):
                for _ in g_:
                    pass

            units = [(e_i, tb) for e_i in range(16) for tb in range(4)]
            load_expert(0)
            load_expert(1)
            drain(stageA(units[0][0], units[0][1], 0))
            for u, (e_i, tb) in enumerate(units):
                gb = stageB(e_i, tb, u % 2)
                if u + 1 < len(units):
                    ne, ntb = units[u + 1]
                    if ne == 0:
                        for tt in range(4 * ntb, 4 * ntb + 4):
                            prep_tile(tt)
                    ga_ = stageA(ne, ntb, (u + 1) % 2)
                    drain(ga_)
                drain(gb)
                if tb == 3 and e_i + 2 < 16:
                    load_expert(e_i + 2)
            sch.barrier()
            if "x2" in dbg:
                op("sp", lambda e: e.dma_start(out=dbg["x2"].rearrange("(t p) c -> p t c", p=128), in_=x1), writes=["dbg_x2"], dma=True)
                op("sp", None, reads=["dbg_x2"])

            fa = MultiAlloc([(R_W, R_W + 64 * K)])
            ss6 = fa([128, NT], F32)
            rs6 = fa([128, NT], F32)
            junk6 = fa([128, D], BF16)
            yo = [fa([128, D], F32) for _ in range(2)]
            op("dve", lambda e: e.memset(ss6, 0.0), writes=["ss6"])
            for tt in range(NT):
                b = tt % 2
                op("act", lambda e, tt=tt: e.activation(out=junk6, in_=x1[:, tt, :], func=AF.Square, accum_out=ss6[:, tt:tt + 1]),
                   reads=["ss6"], writes=["junk6", ("ss6", tt)])
                op("act", lambda e, tt=tt: e.activation(out=rs6[:, tt:tt + 1], in_=ss6[:, tt:tt + 1], func=AF.Sqrt, bias=epsc,
                                                        scale=1.0 / D), reads=[("ss6", tt), "epsc"], writes=[("rs6", tt)])
                op("dve", lambda e, tt=tt: e.reciprocal(out=rs6[:, tt:tt + 1], in_=rs6[:, tt:tt + 1]), reads=[("rs6", tt)],
                   writes=[("rs6", tt)])
                op("dve", lambda e, tt=tt, b=b: e.scalar_tensor_tensor(out=yo[b], in0=x1[:, tt, :], scalar=rs6[:, tt:tt + 1], in1=fng,
                                                                       op0=ALU.mult, op1=ALU.mult),
                   reads=[("rs6", tt), "fng"], writes=[("yo", b)])
                op("sp", lambda e, tt=tt, b=b: e.dma_start(out=out_d[tt * 128:(tt + 1) * 128, :], in_=yo[b]), reads=[("yo", b)],
                   writes=[("out", tt)], dma=True)
            op("sp", None, reads=[("out", tt) for tt in range(NT)])


        body()
        sch.barrier()
        DEBUG["stats_pre"] = {e: len(sch.ops[e]) for e in Sched.ENGS}
        with nc.Block() as block:
            sch.emit(nc, block, engsem, dmasem)
        DEBUG["stats"] = sch.stats
    return nc


_NC_CACHE = {}


def kernel(**inputs):
    dbg = tuple(DEBUG.get("outputs", ()))
    key = (dbg, DEBUG.get("stop_after"))
    if key not in _NC_CACHE:
        _NC_CACHE[key] = build_nc(dbg, DEBUG.get("stop_after"))
    nc = _NC_CACHE[key]
    n = 8
    x = np.ascontiguousarray(inputs["x"], dtype=np.float32)
    posn = np.ascontiguousarray(inputs["positions"], dtype=np.int32)
    f32 = lambda a: np.ascontiguousarray(a, dtype=np.float32)
    inv = (10000.0 ** (-np.arange(32, dtype=np.float32) / np.float32(32))).astype(np.float32).reshape(1, 32)
    shared = {
        "norm1_g": f32(inputs["norm1_g"][0].reshape(8, 128).T),
        "w_in": f32(inputs["w_in"][0]),
        "conv_w": f32(inputs["conv_w"][0].reshape(4, 12, 128).transpose(2, 1, 0).reshape(128, 48)),
        "inv_freq": inv,
        "a_log": f32(inputs["a_log"][0].reshape(1, 8)),
        "dt_bias": f32(inputs["dt_bias"][0].reshape(1, 8)),
        "a_norm_g": f32(inputs["a_norm_g"][0].reshape(1, 64)),
        "b_gate": f32(inputs["b_gate"][0].reshape(16, 128).T),
        "norm2_g": f32(inputs["norm2_g"][0].reshape(8, 128).T),
        "final_norm_g": f32(inputs["final_norm_g"].reshape(1, D)),
        "w_proj_a": f32(inputs["w_proj_a"][0]),
        "w_proj_b": f32(inputs["w_proj_b"][0]),
        "w_out": f32(inputs["w_out"][0]),
        "w_router": f32(np.concatenate([inputs["w_router_group"][0], inputs["w_router_expert"][0]], axis=1)),
        "b_router": f32(np.concatenate([inputs["b_router_group"][0], inputs["b_router_expert"][0]], axis=0).reshape(1, 20)),
        "w_exp_gate": f32(inputs["w_exp_gate"][0]),
        "w_exp_up": f32(inputs["w_exp_up"][0]),
        "w_exp_down": f32(inputs["w_exp_down"][0]),
    }
    in_maps = []
    for c in range(n):
        m = dict(shared)
        m["x"] = x[c]
        m["positions"] = np.ascontiguousarray(posn[c].reshape(NT, 128).T)
        in_maps.append(m)
    res = run_bass_kernel_spmd(nc, in_maps, core_ids=list(range(n)))
    DEBUG["results"] = res.results
    return np.stack([r["out"] for r in res.results], axis=0)
```

```python
import math
from contextlib import ExitStack
import numpy as np
import concourse.bass as bass
import concourse.mybir as mybir
from concourse.bass_utils import run_bass_kernel_spmd

F32 = mybir.dt.float32
BF16 = mybir.dt.bfloat16
I32 = mybir.dt.int32
AF = mybir.ActivationFunctionType
ALU = mybir.AluOpType
AX = mybir.AxisListType

S = 2048
D = 1024
NT = S // 128
D_IN = 5464
EPS = 1e-6
N_DMA_SEMS = 24
NEG = -30000.0
NBIS = 12
TWO_PI = 2.0 * math.pi

C_AQ, C_AK, C_AV, C_AZ = 0, 512, 1024, 1536
C_BETA, C_ALPHA = 2048, 2056
C_BQ, C_BK, C_BV = 2064, 2576, 2704
C_IQ, C_IK, C_IW = 2832, 3344, 3408
C_GA, C_GB = 3416, 4440

DEBUG = {}
STRICT_SAME_ENGINE = True


class Sched:
    ENGS = ("pe", "act", "dve", "pool", "sp")

    def __init__(self):
        self.ops = {e: [] for e in self.ENGS}
        self.last_w = {}
        self.readers = {}
        self.dma_rr = 0
        self.dma_count = [0] * N_DMA_SEMS

    def op(self, eng, fn, reads=(), writes=(), dma=False, fast=False):
        deps = set()
        raw = set()
        for k in reads:
            t = self.last_w.get(k)
            if t is not None:
                deps.add(t)
                raw.add(t)
        for k in writes:
            t = self.last_w.get(k)
            if t is not None:
                deps.add(t)
            for t in self.readers.get(k, {}).values():
                deps.add(t)
        idx = len(self.ops[eng])
        if dma:
            si = self.dma_rr
            self.dma_rr = (self.dma_rr + 1) % N_DMA_SEMS
            prev = self.dma_count[si]
            if prev > 0:
                deps.add(("dma", si, prev))
            self.dma_count[si] = prev + 1
            tok = ("dma", si, prev + 1)
            rkey = ("dma", si)
        else:
            tok = ("eng", eng, idx)
            rkey = eng
            if STRICT_SAME_ENGINE:
                deps = {t for t in deps if not (t[0] == "eng" and t[1] == eng) or eng != "pe"}
            else:
                deps = {t for t in deps if not (t[0] == "eng" and t[1] == eng)
                        or (t in raw and eng != "pe" and not fast and idx - t[2] <= 8)}
        self.ops[eng].append(dict(fn=fn, deps=deps, signal=False, dma=(tok if dma else None)))
        for k in writes:
            self.last_w[k] = tok
            self.readers[k] = {}
        for k in reads:
            if k in writes:
                continue
            self.readers.setdefault(k, {})[rkey] = tok
        return tok

    def barrier(self):
        toks = set()
        for e in self.ENGS:
            j = len(self.ops[e]) - 1
            while j >= 0 and (self.ops[e][j]["fn"] is None or self.ops[e][j]["dma"] is not None):
                j -= 1
            if j >= 0:
                toks.add(("eng", e, j))
        for si in range(N_DMA_SEMS):
            if self.dma_count[si] > 0:
                toks.add(("dma", si, self.dma_count[si]))
        for e in self.ENGS:
            deps = {t for t in toks if not (t[0] == "eng" and t[1] == e and (e == "pe" or not STRICT_SAME_ENGINE))}
            self.ops[e].append(dict(fn=None, deps=deps, signal=False, dma=None))
        self.last_w = {}
        self.readers = {}

    def emit(self, nc, block, engsem, dmasem):
        for e in self.ENGS:
            for o in self.ops[e]:
                for t in o["deps"]:
                    if t[0] == "eng":
                        self.ops[t[1]][t[2]]["signal"] = True
        sigcount = {}
        for e in self.ENGS:
            c = 0
            lst = []
            for o in self.ops[e]:
                if o["signal"]:
                    c += 1
                lst.append(c)
            sigcount[e] = lst
        self.stats = {e: (len(self.ops[e]), sigcount[e][-1] if sigcount[e] else 0) for e in self.ENGS}

        def run(e, eng):
            waited = {}
            for o in self.ops[e]:
                need = {}
                for t in o["deps"]:
                    if t[0] == "eng":
                        key = ("eng", t[1])
                        val = sigcount[t[1]][t[2]]
                    else:
                        key = ("dma", t[1])
                        val = 16 * t[2]
                    if val > need.get(key, 0):
                        need[key] = val
                for key, val in need.items():
                    if waited.get(key, 0) >= val:
                        continue
                    waited[key] = val
                    sem = engsem[key[1]] if key[0] == "eng" else dmasem[key[1]]
                    eng.wait_ge(sem, val)
                if o["fn"] is None:
                    continue
                inst = o["fn"](eng)
                if o["dma"] is not None:
                    inst.then_inc(dmasem[o["dma"][1]], 16)
                elif o["signal"]:
                    inst.then_inc(engsem[e], 1)

        @block.tensor
        def _(eng):
            run("pe", eng)

        @block.scalar
        def _(eng):
            run("act", eng)

        @block.vector
        def _(eng):
            run("dve", eng)

        @block.gpsimd
        def _(eng):
            run("pool", eng)

        @block.sync
        def _(eng):
            run("sp", eng)


DT_SIZE = {F32: 4, BF16: 2, I32: 4}


def build_nc(debug=(), stop_after=None):
    nc = bass.Bass("TRN2", target_bir_lowering=False)

    def din(name, shape, dt=F32):
        return nc.dram_tensor(name, list(shape), dt, kind="ExternalInput").ap()

    x_d = din("x", [S, D])
    pos_d = din("positions", [128, NT], I32)
    g1_d = din("norm1_g", [128, 8])
    w_in_d = din("w_in", [D, D_IN])
    convw_d = din("conv_w", [128, 48])
    invf_d = din("inv_freq", [1, 32])
    alog_d = din("a_log", [1, 8])
    dtb_d = din("dt_bias", [1, 8])
    ang_d = din("a_norm_g", [1, 64])
    bgate_d = din("b_gate", [128, 16])
    g2_d = din("norm2_g", [128, 8])
    fng_d = din("final_norm_g", [1, D])
    wpa_d = din("w_proj_a", [512, D])
    wpb_d = din("w_proj_b", [512, D])
    wout_d = din("w_out", [D, D])
    wr_d = din("w_router", [D, 20])
    br_d = din("b_router", [1, 20])
    w1_d = din("w_exp_gate", [16, D, 256])
    w3_d = din("w_exp_up", [16, D, 256])
    w2_d = din("w_exp_down", [16, 256, D])
    out_d = nc.dram_tensor("out", [S, D], F32, kind="ExternalOutput").ap()
    dbg = {}
    for name, shape, dt in debug:
        dbg[name] = nc.dram_tensor("dbg_" + name, list(shape), dt, kind="ExternalOutput").ap()
    w_in_v = w_in_d.rearrange("(k p) c -> p k c", p=128)

    sch = Sched()
    op = sch.op
    es = ExitStack()
    with es:
        ARENA_BYTES = 207 * 1024
        arena = es.enter_context(nc.sbuf_tensor("arena", [128, ARENA_BYTES // 4], F32))

        def view(off, shape, dt):
            n = 1
            for s_ in shape[1:]:
                n *= s_
            size = n * DT_SIZE[dt]
            assert off % 4 == 0 and size % 4 == 0 and off + size <= ARENA_BYTES, (off, size)
            ap = arena[:, off // 4:(off + size) // 4]
            if dt != F32:
                ap = ap.bitcast(dt)
            if len(shape) == 3:
                ap = ap.rearrange("p (a b) -> p a b", a=shape[1])
            elif len(shape) == 4:
                ap = ap.rearrange("p (a b c) -> p a b c", a=shape[1], b=shape[2])
            return ap

        class Alloc:
            def __init__(self, base, limit):
                self.off = base
                self.limit = limit

            def __call__(self, shape, dt):
                n = 1
                for s_ in shape[1:]:
                    n *= s_
                size = (n * DT_SIZE[dt] + 63) // 64 * 64
                self.off = (self.off + 63) // 64 * 64
                v = view(self.off, shape, dt)
                self.off += size
                assert self.off <= self.limit, (self.off, self.limit)
                return v

        pbank = [es.enter_context(nc.psum_tensor("pb%d" % i, [128, 512], F32)) for i in range(8)]
        engsem = {e: es.enter_context(nc.semaphore("sem_" + e)) for e in Sched.ENGS}
        dmasem = [es.enter_context(nc.semaphore("dsem%d" % i)) for i in range(N_DMA_SEMS)]

        def PB(i):
            return pbank[i][:]

        def PBb(i):
            return pbank[i][:].bitcast(BF16)

        def body():
            K = 1024
            ca = Alloc(0, 9 * K)
            ident_f = ca([128, 128], F32)
            ident_b = ca([128, 128], BF16)
            ucs_f = ca([128, 128], F32)
            mc0_f = ca([128, 128], F32)
            mc1_f = ca([128, 128], F32)
            maskneg_f = ca([128, 128], F32)
            strict_b = ca([128, 128], BF16)
            g1col = ca([128, 8], F32)
            epsc = ca([128, 1], F32)
            cw = ca([128, 48], F32)
            invf = ca([128, 32], F32)
            dtb = ca([128, 8], F32)
            negA = ca([128, 8], F32)
            angb = ca([128, 64], F32)
            posi = ca([128, NT], I32)
            posf = ca([128, NT], F32)
            cs = ca([128, NT, 64], F32)
            assert ca.off <= 9 * K, ca.off

            op("pool", lambda e: e.memset(ident_f, 1.0), writes=["ident_f"])
            op("pool", lambda e: e.affine_select(out=ident_f, in_=ident_f, pattern=[[-1, 128]], compare_op=ALU.is_equal,
                                                 fill=0.0, base=0, channel_multiplier=1), writes=["ident_f"])
            op("pool", lambda e: e.tensor_copy(out=ident_b, in_=ident_f), reads=["ident_f"], writes=["ident_b"])
            op("pool", lambda e: e.memset(ucs_f, 1.0), writes=["ucs_f"])
            op("pool", lambda e: e.affine_select(out=ucs_f, in_=ucs_f, pattern=[[1, 128]], compare_op=ALU.is_ge,
                                                 fill=0.0, base=0, channel_multiplier=-1), writes=["ucs_f"])
            op("pool", lambda e: e.memset(ucs_f[0:64, 64:128], 0.0), writes=["ucs_f"])
            op("pool", lambda e: e.memset(mc0_f, 0.0), writes=["mc0_f"])
            op("pool", lambda e: e.memset(mc0_f[0:64, :], 1.0), writes=["mc0_f"])
            op("pool", lambda e: e.memset(mc1_f, 0.0), writes=["mc1_f"])
            op("pool", lambda e: e.memset(mc1_f[64:128, :], 1.0), writes=["mc1_f"])
            op("pool", lambda e: e.memset(maskneg_f, 0.0), writes=["maskneg_f"])
            op("pool", lambda e: e.affine_select(out=maskneg_f, in_=maskneg_f, pattern=[[-1, 128]], compare_op=ALU.is_ge,
                                                 fill=NEG, base=0, channel_multiplier=1), writes=["maskneg_f"])
            op("pool", lambda e: e.memset(maskneg_f[64:128, 0:64], NEG), writes=["maskneg_f"])
            op("pool", lambda e: e.memset(strict_b, 1.0), writes=["strict_b"])
            op("pool", lambda e: e.affine_select(out=strict_b, in_=strict_b, pattern=[[-1, 128]], compare_op=ALU.is_gt,
                                                 fill=0.0, base=0, channel_multiplier=1), writes=["strict_b"])
            op("pool", lambda e: e.memset(strict_b[64:128, 0:64], 0.0), writes=["strict_b"])
            op("dve", lambda e: e.memset(epsc, EPS), writes=["epsc"])
            op("sp", lambda e: e.dma_start(out=g1col, in_=g1_d), writes=["g1col"], dma=True)
            op("sp", lambda e: e.dma_start(out=cw, in_=convw_d), writes=["cw"], dma=True)
            op("sp", lambda e: e.dma_start(out=invf, in_=invf_d.partition_broadcast(128)), writes=["invf"], dma=True)
            op("sp", lambda e: e.dma_start(out=dtb, in_=dtb_d.partition_broadcast(128)), writes=["dtb"], dma=True)
            op("sp", lambda e: e.dma_start(out=negA, in_=alog_d.partition_broadcast(128)), writes=["negA"], dma=True)
            op("sp", lambda e: e.dma_start(out=angb, in_=ang_d.partition_broadcast(128)), writes=["angb"], dma=True)
            op("sp", lambda e: e.dma_start(out=posi, in_=pos_d), writes=["posi"], dma=True)
            op("act", lambda e: e.activation(out=negA, in_=negA, func=AF.Exp), reads=["negA"], writes=["negA"])
            op("dve", lambda e: e.tensor_scalar(out=negA, in0=negA, scalar1=-1.0, scalar2=None, op0=ALU.mult),
               reads=["negA"], writes=["negA"])

            R_A = 9 * K
            R_W = R_A + 32 * K
            R_Z = R_W + 16 * K
            R_Q = R_Z + 50 * K
            R_S = R_Q + 48 * K
            hT = view(R_A, [128, 8, S], BF16)
            wstage = view(R_W, [128, 8, 256], F32)
            wbf = [view(R_W + 8 * K + i * 4 * K, [128, 8, 256], BF16) for i in range(2)]
            zqkvT = view(R_Z, [128, 12, S + 4], BF16)
            bqT = view(R_Z, [128, 4, S], BF16)
            iqT = view(R_Z + 16 * K, [128, 4, S], BF16)
            azs = view(R_Z + 32 * K, [128, NT, 512], BF16)
            qkv_tok = view(R_Q, [128, NT, 1536], BF16)
            sa = Alloc(R_S, ARENA_BYTES)
            kz = [[sa([128, S], BF16) for _ in range(2)] for _ in range(2)]
            ikT2 = sa([128, S], BF16)
            bv_tok = sa([128, NT, 130], BF16)
            ab_tok = sa([128, NT, 16], F32)
            iw_tok = sa([128, NT, 8], F32)
            diagw = sa([128, 48, 128], BF16)
            R_WORK = sa.off

            def rope_tables():
                wa = Alloc(R_Q, R_Q + 48 * K)
                ang = wa([128, NT, 32], F32)
                tmp = wa([128, NT, 32], F32)
                ki = wa([128, NT, 32], I32)
                op("dve", lambda e: e.tensor_copy(out=posf, in_=posi), reads=["posi"], writes=["posf"])
                op("dve", lambda e: e.tensor_tensor(out=ang, in0=posf.unsqueeze(2).to_broadcast([128, NT, 32]),
                                                    in1=invf.unsqueeze(1).to_broadcast([128, NT, 32]), op=ALU.mult),
                   reads=["posf", "invf"], writes=["ang"])
                for which, shift in ((1, 0.0), (0, math.pi / 2.0)):
                    dst = cs[:, :, which * 32:(which + 1) * 32]
                    op("dve", lambda e, shift=shift: e.tensor_scalar(out=tmp, in0=ang, scalar1=shift, scalar2=None, op0=ALU.add),
                       reads=["ang"], writes=["rt_tmp"])
                    op("dve", lambda e: e.tensor_scalar(out=ki, in0=tmp, scalar1=1.0 / TWO_PI, scalar2=None, op0=ALU.mult),
                       reads=["rt_tmp"], writes=["rt_ki"])
                    op("dve", lambda e, dst=dst: e.tensor_copy(out=dst, in_=ki), reads=["rt_ki"], writes=["cs"])
                    op("dve", lambda e, dst=dst: e.scalar_tensor_tensor(out=dst, in0=dst, scalar=-TWO_PI, in1=tmp,
                                                                       op0=ALU.mult, op1=ALU.add),
                       reads=["cs", "rt_tmp"], writes=["cs"])
                    op("dve", lambda e, dst=dst: e.tensor_scalar(out=dst, in0=dst, scalar1=math.pi, scalar2=-math.pi,
                                                                op0=ALU.min, op1=ALU.max), reads=["cs"], writes=["cs"])
                    op("act", lambda e, dst=dst: e.activation(out=dst, in_=dst, func=AF.Sin), reads=["cs"], writes=["cs"])

            rope_tables()

            def phase1(hT_dst, keyp):
                wa = Alloc(R_Q + 16 * K, R_Q + 48 * K)
                xt = [wa([128, D], F32) for _ in range(2)]
                hb = [wa([128, D], BF16) for _ in range(2)]
                junk = wa([128, D], BF16)
                ss1 = wa([128, NT], F32)
                rstd1 = wa([128, NT], F32)
                op("dve", lambda e: e.memset(ss1, 0.0), writes=[keyp + "ss1"])
                for tt in range(NT):
                    b = tt % 2
                    op("sp", lambda e, tt=tt, b=b: e.dma_start(out=xt[b], in_=x_d[tt * 128:(tt + 1) * 128, :]),
                       writes=[(keyp + "xt", b)], dma=True)
                    op("act", lambda e, tt=tt, b=b: e.activation(out=junk, in_=xt[b], func=AF.Square,
                                                                 accum_out=ss1[:, tt:tt + 1]),
                       reads=[(keyp + "xt", b), keyp + "ss1"], writes=[keyp + "junk", (keyp + "ss1", tt)])
                    op("act", lambda e, tt=tt: e.activation(out=rstd1[:, tt:tt + 1], in_=ss1[:, tt:tt + 1], func=AF.Sqrt,
                                                            bias=epsc, scale=1.0 / D),
                       reads=[(keyp + "ss1", tt), "epsc"], writes=[(keyp + "rstd1", tt)])
                    op("dve", lambda e, tt=tt: e.reciprocal(out=rstd1[:, tt:tt + 1], in_=rstd1[:, tt:tt + 1]),
                       reads=[(keyp + "rstd1", tt)], writes=[(keyp + "rstd1", tt)])
                    op("dve", lambda e, tt=tt, b=b: e.tensor_scalar(out=hb[b], in0=xt[b], scalar1=rstd1[:, tt:tt + 1],
                                                                    scalar2=None, op0=ALU.mult),
                       reads=[(keyp + "xt", b), (keyp + "rstd1", tt)], writes=[(keyp + "hb", b)])
                    pbv = PBb(tt % 2)
                    for k in range(8):
                        op("pe", lambda e, k=k, b=b, pbv=pbv: e.transpose(out=pbv[:, k * 128:(k + 1) * 128],
                                                                          in_=hb[b][:, k * 128:(k + 1) * 128], identity=ident_b),
                           reads=[(keyp + "hb", b), "ident_b"], writes=[("pb", tt % 2)])
                    op("act", lambda e, tt=tt, pbv=pbv: e.copy(out=hT_dst[:, :, tt * 128:(tt + 1) * 128],
                                                               in_=pbv.rearrange("p (k t) -> p k t", k=8)),
                       reads=[("pb", tt % 2)], writes=[("hT", tt)])

            phase1(hT, "p1")
            if stop_after == "p1":
                sch.barrier()
                return
            ALL_HT = [("hT", tt) for tt in range(NT)]

            wchunk_i = [0]

            def load_w(ranges):
                i = wchunk_i[0]
                wchunk_i[0] += 1
                b = i % 2
                off = 0
                for (c0, w) in ranges:
                    op("sp", lambda e, c0=c0, w=w, off=off: e.dma_start(out=wstage[:, :, off:off + w],
                                                                        in_=w_in_v[:, :, c0:c0 + w]),
                       writes=["wstage"], dma=True)
                    off += w
                tot = off
                op("pool", lambda e, b=b, tot=tot: e.tensor_tensor(out=wbf[b][:, :, 0:tot], in0=wstage[:, :, 0:tot],
                                                                   in1=g1col.unsqueeze(2).to_broadcast([128, 8, tot]),
                                                                   op=ALU.mult),
                   reads=["wstage", "g1col"], writes=[("wbf", b)])
                return wbf[b], ("wbf", b), tot

            for ci in range(48):
                op("pool", lambda e, ci=ci: e.tensor_scalar(out=diagw[:, ci, :], in0=ident_f, scalar1=cw[:, ci:ci + 1],
                                                            scalar2=None, op0=ALU.mult),
                   reads=["ident_f", "cw"], writes=[("diagw", ci)])
            op("pool", lambda e: e.memset(zqkvT[:, :, 0:4], 0.0), writes=["zpad"])

            cva = Alloc(R_WORK, ARENA_BYTES)
            convtmp = [cva([128, 512], BF16) for _ in range(2)]
            evq = [0]

            def evac_copy(out, in_, reads, writes):
                evq[0] += 1
                if evq[0] % 2 == 0:
                    op("act", lambda e: e.copy(out=out, in_=in_), reads=reads, writes=writes)
                else:
                    op("dve", lambda e: e.tensor_copy(out=out, in_=in_), reads=reads, writes=writes)

            pbi = [0]

            def g1_proj(c, wt, wkey, ct):
                for tb in range(4):
                    bk = 2 + (pbi[0] % 2)
                    pbi[0] += 1
                    for k in range(8):
                        op("pe", lambda e, k=k, tb=tb, bk=bk: e.matmul(
                            PB(bk), lhsT=wt[:, k, ct * 128:(ct + 1) * 128], rhs=hT[:, k, tb * 512:(tb + 1) * 512],
                            start=(k == 0), stop=(k == 7)),
                           reads=[wkey] + ALL_HT[tb * 4:tb * 4 + 4], writes=[("pb", bk)])
                    evac_copy(zqkvT[:, c, 4 + tb * 512:4 + (tb + 1) * 512], PB(bk), [("pb", bk)], [("zq", c, tb)])

            def g1_conv(c):
                for tb in range(4):
                    bk = 4 + (tb % 2)
                    for j in range(4):
                        op("pe", lambda e, tb=tb, j=j, bk=bk: e.matmul(
                            PB(bk), lhsT=diagw[:, c * 4 + j, :], rhs=zqkvT[:, c, tb * 512 + j + 1:tb * 512 + j + 1 + 512],
                            start=(j == 0), stop=(j == 3)),
                           reads=[("diagw", c * 4 + j), ("zq", c, tb), "zpad"] + ([("zq", c, tb - 1)] if tb > 0 else []),
                           writes=[("pb", bk)])
                    ctb = tb % 2
                    op("act", lambda e, bk=bk, ctb=ctb: e.activation(out=convtmp[ctb], in_=PB(bk), func=AF.Silu),
                       reads=[("pb", bk)], writes=[("convtmp", ctb)])
                    tbk = 6 + (tb % 2)
                    for q in range(4):
                        op("pe", lambda e, q=q, ctb=ctb, tbk=tbk: e.transpose(out=PBb(tbk)[:, q * 128:(q + 1) * 128],
                                                                              in_=convtmp[ctb][:, q * 128:(q + 1) * 128],
                                                                              identity=ident_b),
                           reads=[("convtmp", ctb), "ident_b"], writes=[("pb", tbk)])
                    op("dve", lambda e, tb=tb, tbk=tbk: e.tensor_copy(
                        out=qkv_tok[:, tb * 4:(tb + 1) * 4, c * 128:(c + 1) * 128],
                        in_=PBb(tbk)[:, 0:512].rearrange("p (q t) -> p q t", q=4)),
                       reads=[("pb", tbk)], writes=[("qkv_tok", tb * 4 + q, c) for q in range(4)])

            prev_c = None
            nxt_w = load_w([(0, 256)])
            for chunk in range(6):
                wt, wkey, _ = nxt_w
                for ct in range(2):
                    c = chunk * 2 + ct
                    g1_proj(c, wt, wkey, ct)
                    if ct == 0:
                        nxt_w = load_w([((chunk + 1) * 256, 256)]) if chunk + 1 < 6 else load_w([(C_AZ, 256)])
                    if prev_c is not None:
                        g1_conv(prev_c)
                    prev_c = c
            g1_conv(prev_c)
            pending_w = [nxt_w]

            if "qkv_tok" in dbg:
                op("sp", lambda e: e.dma_start(out=dbg["qkv_tok"].rearrange("(t p) c -> p t c", p=128), in_=qkv_tok),
                   reads=[("qkv_tok", tt, c) for tt in range(NT) for c in range(12)], writes=["dbg_qkv_tok"], dma=True)
                op("sp", None, reads=["dbg_qkv_tok"])
            sch.barrier()
            if stop_after == "g1":
                return

            rwa = Alloc(cva.off, ARENA_BYTES)
            zr = [rwa([128, 256], F32) for _ in range(2)]
            rt = [rwa([128, 4, 32], F32) for _ in range(4)]
            roped = [rwa([128, 256], BF16) for _ in range(2)]
            op("pool", lambda e: e.memset(bv_tok, 1.0), writes=["bv_ones"])
            for a_ in range(2):
                for b_ in range(2):
                    op("pool", lambda e, a_=a_, b_=b_: e.memset(kz[a_][b_], 0.0), writes=["kz0"])

            def rope_ops(src, nh, dst_views, tt, rkey, wkeys, b):
                sv = src.rearrange("p (h d) -> p h d", h=nh)
                x1 = sv[:, :, 0:32]
                x2 = sv[:, :, 32:64]
                cc = cs[:, tt, 0:32].unsqueeze(1).to_broadcast([128, nh, 32])
                sn = cs[:, tt, 32:64].unsqueeze(1).to_broadcast([128, nh, 32])
                t = [r[:, 0:nh, :] for r in rt]
                op("dve", lambda e: e.tensor_tensor(out=t[0], in0=x1, in1=cc, op=ALU.mult), reads=[rkey, "cs"], writes=[("rt", 0)])
                op("pool", lambda e: e.tensor_tensor(out=t[1], in0=x2, in1=sn, op=ALU.mult), reads=[rkey, "cs"], writes=[("rt", 1)])
                op("pool", lambda e: e.tensor_tensor(out=t[2], in0=x2, in1=cc, op=ALU.mult), reads=[rkey, "cs"], writes=[("rt", 2)])
                op("dve", lambda e: e.tensor_tensor(out=t[3], in0=x1, in1=sn, op=ALU.mult), reads=[rkey, "cs"], writes=[("rt", 3)])
                for i, dv in enumerate(dst_views):
                    eng = "dve" if i % 2 == 0 else "pool"
                    op(eng, lambda e, dv=dv: e.tensor_tensor(out=dv[:, :, 0:32], in0=t[0], in1=t[1], op=ALU.subtract),
                       reads=[("rt", 0), ("rt", 1)], writes=wkeys)
                    op(eng, lambda e, dv=dv: e.tensor_tensor(out=dv[:, :, 32:64], in0=t[2], in1=t[3], op=ALU.add),
                       reads=[("rt", 2), ("rt", 3)], writes=wkeys)

            def tok_chunk(ranges, handler, sel=None, next_ranges=None):
                if pending_w[0] is not None:
                    wt, wkey, tot = pending_w[0]
                    pending_w[0] = None
                else:
                    wt, wkey, tot = load_w(ranges)
                lo, hi = (0, tot) if sel is None else sel
                pend = []
                for tt in range(NT):
                    if tt == 6 and next_ranges is not None:
                        pending_w[0] = load_w(next_ranges)
                    bk = 2 + (tt % 2)
                    for k in range(8):
                        op("pe", lambda e, k=k, tt=tt, bk=bk, wt=wt: e.matmul(
                            PB(bk)[:, 0:hi - lo], lhsT=hT[:, k, tt * 128:(tt + 1) * 128], rhs=wt[:, k, lo:hi],
                            start=(k == 0), stop=(k == 7)),
                           reads=[wkey, ("hT", tt)], writes=[("pb", bk)])
                    if tt >= 1:
                        pend.append(handler(tt - 1, 2 + ((tt - 1) % 2)))
                    if len(pend) >= 2:
                        p2 = pend.pop(0)
                        if p2 is not None:
                            p2()
                pend.append(handler(NT - 1, 2 + ((NT - 1) % 2)))
                for p2 in pend:
                    if p2 is not None:
                        p2()

            for j in range(2):
                def h_az(tt, bk, j=j):
                    op("act", lambda e: e.activation(out=azs[:, tt, j * 256:(j + 1) * 256], in_=PB(bk)[:, 0:256], func=AF.Silu),
                       reads=[("pb", bk)], writes=[("azs", tt, j)])
                tok_chunk([(C_AZ + j * 256, 256)], h_az, next_ranges=[(C_AZ + 256, 256)] if j == 0 else [(C_BQ, 256)])

            if stop_after == "u1":
                return
            for (c0, dstT, nm) in ((C_BQ, bqT, "bqT"), (C_IQ, iqT, "iqT")):
                for j in range(2):
                    def h_q(tt, bk, j=j, dstT=dstT, nm=nm):
                        b = tt % 2
                        op("act", lambda e: e.copy(out=zr[b], in_=PB(bk)[:, 0:256]), reads=[("pb", bk)], writes=[("zr", b)])
                        rope_ops(zr[b], 4, [roped[b].rearrange("p (h d) -> p h d", h=4)], tt, ("zr", b), [("roped", b)], b)
                        tbk = 6 + b

                        def part2():
                            for q in range(2):
                                op("pe", lambda e, q=q: e.transpose(out=PBb(tbk)[:, q * 128:(q + 1) * 128],
                                                                    in_=roped[b][:, q * 128:(q + 1) * 128], identity=ident_b),
                                   reads=[("roped", b), "ident_b"], writes=[("pb", tbk)])
                            op("act", lambda e: e.copy(out=dstT[:, 2 * j:2 * j + 2, tt * 128:(tt + 1) * 128],
                                                       in_=PBb(tbk)[:, 0:256].rearrange("p (q t) -> p q t", q=2)),
                               reads=[("pb", tbk)], writes=[(nm, tt, j)])
                        return part2
                    nr = [(c0 + 256, 256)] if j == 0 else ([(C_IQ, 256)] if c0 == C_BQ else [(C_BK, 256)])
                    tok_chunk([(c0 + j * 256, 256)], h_q, next_ranges=nr)

            if stop_after == "u23":
                return
            def h_kv(tt, bk):
                b = tt % 2
                op("act", lambda e: e.copy(out=zr[b], in_=PB(bk)[:, 0:256]), reads=[("pb", bk)], writes=[("zr", b)])
                rv = roped[b].rearrange("p (h d) -> p h d", h=4)
                rope_ops(zr[b][:, 0:128], 2, [rv[:, 0:2, :]], tt, ("zr", b), [("roped", b)], b)
                op("pool", lambda e: e.tensor_copy(out=rv[:, 2, :], in_=rv[:, 1, :]), reads=[("roped", b)], writes=[("roped", b)])
                op("pool", lambda e: e.tensor_copy(out=rv[:, 3, :], in_=rv[:, 0, :]), reads=[("roped", b)], writes=[("roped", b)])
                def part2():
                    tbk = 6 + b
                    for q in range(2):
                        op("pe", lambda e, q=q: e.transpose(out=PBb(tbk)[:, q * 128:(q + 1) * 128],
                                                            in_=roped[b][:, q * 128:(q + 1) * 128], identity=ident_b),
                           reads=[("roped", b), "ident_b"], writes=[("pb", tbk)])
                    ts_ = slice(tt * 128, (tt + 1) * 128)
                    op("act", lambda e: e.copy(out=kz[0][0][0:64, ts_], in_=PBb(tbk)[0:64, 0:128]), reads=[("pb", tbk), "kz0"],
                       writes=[("bkT", tt)])
                    op("act", lambda e: e.copy(out=kz[1][1][64:128, ts_], in_=PBb(tbk)[64:128, 0:128]), reads=[("pb", tbk)],
                       writes=[("bkT", tt)])
                    op("act", lambda e: e.copy(out=kz[1][0][0:64, ts_], in_=PBb(tbk)[0:64, 128:256]), reads=[("pb", tbk)],
                       writes=[("bkT", tt)])
                    op("act", lambda e: e.copy(out=kz[0][1][64:128, ts_], in_=PBb(tbk)[64:128, 128:256]), reads=[("pb", tbk)],
                       writes=[("bkT", tt)])

                op("dve", lambda e: e.tensor_copy(out=bv_tok[:, tt, 0:64], in_=zr[b][:, 128:192]), reads=[("zr", b), "bv_ones"],
                   writes=[("bv", tt)])
                op("dve", lambda e: e.tensor_copy(out=bv_tok[:, tt, 65:129], in_=zr[b][:, 192:256]), reads=[("zr", b)],
                   writes=[("bv", tt)])
                return part2
            tok_chunk([(C_BK, 256)], h_kv, next_ranges=[(C_IW + 8 - 256, 256)])

            if stop_after == "u4a":
                return
            IW_SCALE = (8 ** -0.5) * (64 ** -0.5)

            def h_small(tt, bk):
                b = tt % 2
                op("act", lambda e: e.copy(out=zr[b][:, 0:72], in_=PB(bk)[:, 0:72]), reads=[("pb", bk)], writes=[("zr", b)])
                rv = roped[b].rearrange("p (h d) -> p h d", h=4)
                rope_ops(zr[b][:, 0:64], 1, [rv[:, 0:1, :], rv[:, 1:2, :]], tt, ("zr", b), [("roped", b)], b)
                def part2():
                    tbk = 6 + b
                    op("pe", lambda e: e.transpose(out=PBb(tbk)[:, 0:128], in_=roped[b][:, 0:128], identity=ident_b),
                       reads=[("roped", b), "ident_b"], writes=[("pb", tbk)])
                    op("act", lambda e: e.copy(out=ikT2[:, tt * 128:(tt + 1) * 128], in_=PBb(tbk)[:, 0:128]),
                       reads=[("pb", tbk)], writes=[("ikT", tt)])

                op("dve", lambda e: e.tensor_scalar(out=iw_tok[:, tt, :], in0=zr[b][:, 64:72], scalar1=IW_SCALE, scalar2=None,
                                                    op0=ALU.mult), reads=[("zr", b)], writes=[("iw", tt)])
                return part2
            tok_chunk([(C_IW + 8 - 256, 256)], h_small, sel=(184, 256), next_ranges=[(C_BETA, 256)])

            def h_ab(tt, bk):
                op("act", lambda e: e.copy(out=ab_tok[:, tt, :], in_=PB(bk)[:, 0:16]), reads=[("pb", bk)], writes=[("ab", tt)])
            tok_chunk([(C_BETA, 256)], h_ab, sel=(0, 16))

            for nm, t_, shape in (("bqT", bqT, None), ("iqT", iqT, None)):
                if nm in dbg:
                    op("sp", lambda e, nm=nm, t_=t_: e.dma_start(out=dbg[nm].rearrange("(a p) t -> p a t", p=128), in_=t_),
                       reads=[(nm, tt, j) for tt in range(NT) for j in range(2)], writes=["dbg_" + nm], dma=True)
                    op("sp", None, reads=["dbg_" + nm])
            if "misc" in dbg:
                sch.barrier()
                mt = view(R_W, [128, NT, 154], F32)
                op("dve", lambda e: e.tensor_copy(out=mt[:, :, 0:16], in_=ab_tok), reads=[("ab", tt) for tt in range(NT)], writes=["mt"])
                op("dve", lambda e: e.tensor_copy(out=mt[:, :, 16:24], in_=iw_tok), reads=[("iw", tt) for tt in range(NT)], writes=["mt"])
                op("dve", lambda e: e.tensor_copy(out=mt[:, :, 24:154], in_=bv_tok), reads=[("bv", tt) for tt in range(NT)], writes=["mt"])
                op("sp", lambda e: e.dma_start(out=dbg["misc"].rearrange("(t p) c -> p t c", p=128), in_=mt), reads=["mt"],
                   writes=["dbg_misc"], dma=True)
                op("sp", None, reads=["dbg_misc"])
            sch.barrier()
            if stop_after == "p2":
                return

            class MultiAlloc:
                def __init__(self, regions):
                    self.regs = [[a, b] for a, b in regions]

                def __call__(self, shape, dt):
                    n = 1
                    for s_ in shape[1:]:
                        n *= s_
                    size = (n * DT_SIZE[dt] + 63) // 64 * 64
                    for r in self.regs:
                        r[0] = (r[0] + 63) // 64 * 64
                        if r[0] + size <= r[1]:
                            v = view(r[0], shape, dt)
                            r[0] += size
                            return v
                    raise AssertionError(("MultiAlloc out of space", shape, self.regs))

            def dump(name, ap, reads):
                if name in dbg:
                    op("sp", lambda e: e.dma_start(out=dbg[name], in_=ap), reads=reads, writes=["dbg_" + name], dma=True)
                    op("sp", None, reads=["dbg_" + name])

            o_aT = view(R_A, [128, 4, S], BF16)
            diagw_off = R_WORK - 12 * K
            ga = MultiAlloc([(R_W, R_W + 16 * K), (R_A + 16 * K, R_A + 32 * K), (diagw_off, ARENA_BYTES)])
            g_all = ga([128, NT, 8], F32)
            bet = ga([128, NT, 8], F32)
            gs = ga([128, 24], F32)
            eG = ga([128, 8], F32)
            eGlmG = ga([128, 8], F32)
            scs = [ga([128, 4], F32) for _ in range(2)]
            g_bc = ga([128, 8, 128], F32)
            sq = ga([128, 1024], F32)
            ssn = ga([128, 16], F32)
            rn = ga([128, 16], F32)
            cq = ga([128, 8], F32)
            cqd = ga([128, 8], F32)
            cbk = ga([128, 8], F32)
            ckd = ga([128, 8], F32)
            negbeta = ga([128, 8], F32)
            qn = ga([128, 512], BF16)
            qd = ga([128, 512], BF16)
            kn = ga([128, 512], BF16)
            rhsk = ga([128, 512], BF16)
            kdec = ga([128, 512], BF16)
            rhsv = ga([128, 512], BF16)
            qnT = ga([128, 4, 128], BF16)
            qdT = ga([128, 4, 128], BF16)
            knT = ga([128, 4, 128], BF16)
            Dm = ga([128, 8, 128], BF16)
            Ds = ga([128, 8, 128], BF16)
            Mm = [ga([128, 8, 128], BF16) for _ in range(2)]
            Nm = [ga([128, 8, 128], BF16) for _ in range(2)]
            Pm = [ga([128, 8, 128], BF16) for _ in range(2)]
            qkm = ga([128, 8, 128], BF16)
            qkT_sb = ga([128, 8, 128], BF16)
            u_c = ga([128, 2, 512], F32)
            w_tok = ga([128, 512], BF16)
            wT_sb = ga([128, 4, 128], BF16)
            vn_b = ga([128, 512], BF16)
            Sst = ga([128, 4, 128], F32)
            Stmp = ga([128, 4, 128], F32)
            S_bd = ga([128, 4, 128], BF16)
            bdmask = ga([128, 4, 128], BF16)
            o_c = ga([128, 512], F32)
            qkT_c1 = ga([128, 8, 64], BF16)
            kdec_c1 = ga([128, 512], BF16)
            az_c = ga([128, 512], BF16)
            ss2 = ga([128, 8], F32)
            r2 = ga([128, 8], F32)
            oa_b = ga([128, 512], BF16)

            def bc8(v):
                return v.unsqueeze(2).to_broadcast([128, 8, 64])

            ABK = [("ab", tt) for tt in range(NT)]
            op("act", lambda e: e.activation(out=bet, in_=ab_tok[:, :, 0:8], func=AF.Sigmoid), reads=["ab_all"], writes=["bet"])
            op("dve", lambda e: e.tensor_tensor(out=g_all, in0=ab_tok[:, :, 8:16], in1=dtb.unsqueeze(1).to_broadcast([128, NT, 8]),
                                                op=ALU.add), reads=["ab_all", "dtb"], writes=["g_all"])
            op("act", lambda e: e.activation(out=g_all, in_=g_all, func=AF.Exp), reads=["g_all"], writes=["g_all"])
            op("act", lambda e: e.activation(out=g_all, in_=g_all, func=AF.Ln, bias=1.0), reads=["g_all"], writes=["g_all"])
            op("dve", lambda e: e.tensor_tensor(out=g_all, in0=g_all, in1=negA.unsqueeze(1).to_broadcast([128, NT, 8]),
                                                op=ALU.mult), reads=["g_all", "negA"], writes=["g_all"])
            op("dve", lambda e: e.memset(Sst, 0.0), writes=["S"])
            op("dve", lambda e: e.memset(S_bd, 0.0), writes=["S_bd"])
            op("pool", lambda e: e.memset(bdmask, 0.0), writes=["bdmask"])
            op("pool", lambda e: e.memset(bdmask[0:64, :, 0:64], 1.0), writes=["bdmask"])
            op("pool", lambda e: e.memset(bdmask[64:128, :, 64:128], 1.0), writes=["bdmask"])
            if "g" in dbg:
                op("sp", lambda e: e.dma_start(out=dbg["g"].rearrange("(t p) c -> p t c", p=128), in_=g_all), reads=["g_all"],
                   writes=["dbg_g"], dma=True)
                op("sp", None, reads=["dbg_g"])

            if stop_after == "gdn_pre":
                return
            for tt in range(NT):
                op("pe", lambda e, tt=tt: e.matmul(PB(0)[:, 0:8], lhsT=ucs_f, rhs=g_all[:, tt, :], start=True, stop=True),
                   reads=["g_all"], writes=[("pb", 0)])
                op("pe", lambda e, tt=tt: e.matmul(PB(0)[:, 8:16], lhsT=mc0_f, rhs=g_all[:, tt, :], start=True, stop=True),
                   reads=["g_all"], writes=[("pb", 0)])
                op("pe", lambda e, tt=tt: e.matmul(PB(0)[:, 16:24], lhsT=mc1_f, rhs=g_all[:, tt, :], start=True, stop=True),
                   reads=["g_all"], writes=[("pb", 0)])
                op("act", lambda e: e.copy(out=gs, in_=PB(0)[:, 0:24]), reads=[("pb", 0)], writes=["gs"])
                op("act", lambda e: e.activation(out=eG, in_=gs[:, 0:8], func=AF.Exp), reads=["gs"], writes=["eG"])
                op("dve", lambda e: e.tensor_tensor(out=eGlmG[0:64, :], in0=gs[0:64, 8:16], in1=gs[0:64, 0:8], op=ALU.subtract),
                   reads=["gs"], writes=["eGlmG"])
                op("dve", lambda e: e.tensor_tensor(out=eGlmG[64:128, :], in0=gs[64:128, 16:24], in1=gs[64:128, 0:8],
                                                    op=ALU.subtract), reads=["gs"], writes=["eGlmG"])
                op("act", lambda e: e.activation(out=eGlmG, in_=eGlmG, func=AF.Exp), reads=["eGlmG"], writes=["eGlmG"])
                for hf in range(2):
                    c0 = 8 + 8 * hf
                    op("act", lambda e, hf=hf, c0=c0: e.activation(out=scs[hf][0:64, :], in_=gs[0:64, c0:c0 + 8:2], func=AF.Exp),
                       reads=["gs"], writes=[("scs", hf)])
                    op("act", lambda e, hf=hf, c0=c0: e.activation(out=scs[hf][64:128, :], in_=gs[64:128, c0 + 1:c0 + 8:2],
                                                                   func=AF.Exp), reads=["gs"], writes=[("scs", hf)])
                op("dve", lambda e, tt=tt: e.tensor_scalar(out=g_bc, in0=g_all[:, tt, :].unsqueeze(2).to_broadcast([128, 8, 128]),
                                                           scalar1=-1.0, scalar2=None, op0=ALU.mult),
                   reads=["g_all"], writes=["g_bc"])
                if stop_after == "gdn_a":
                    return
                QK = [("qkv_tok", tt, c) for c in range(8)]
                VV = [("qkv_tok", tt, c) for c in range(8, 12)]
                op("dve", lambda e, tt=tt: e.tensor_tensor(out=sq, in0=qkv_tok[:, tt, 0:1024], in1=qkv_tok[:, tt, 0:1024],
                                                           op=ALU.mult), reads=["qkv_all"], writes=["sq"])
                op("dve", lambda e: e.tensor_reduce(out=ssn, in_=sq.rearrange("p (h d) -> p h d", h=16), axis=AX.X, op=ALU.add),
                   reads=["sq"], writes=["ssn"])
                op("act", lambda e: e.activation(out=rn, in_=ssn, func=AF.Ln, bias=epsc, scale=1.0), reads=["ssn", "epsc"],
                   writes=["rn"])
                op("act", lambda e: e.activation(out=rn, in_=rn, func=AF.Exp, scale=-0.5), reads=["rn"], writes=["rn"])
                op("dve", lambda e: e.tensor_scalar(out=cq, in0=rn[:, 0:8], scalar1=0.125, scalar2=None, op0=ALU.mult),
                   reads=["rn"], writes=["cq"])
                op("dve", lambda e: e.tensor_tensor(out=cqd, in0=cq, in1=eG, op=ALU.mult), reads=["cq", "eG"], writes=["cqd"])
                op("dve", lambda e, tt=tt: e.tensor_tensor(out=cbk, in0=rn[:, 8:16], in1=bet[:, tt, :], op=ALU.mult),
                   reads=["rn", "bet"], writes=["cbk"])
                op("dve", lambda e: e.tensor_tensor(out=cbk, in0=cbk, in1=eG, op=ALU.mult), reads=["cbk", "eG"], writes=["cbk"])
                op("dve", lambda e: e.tensor_tensor(out=ckd, in0=rn[:, 8:16], in1=eGlmG, op=ALU.mult), reads=["rn", "eGlmG"],
                   writes=["ckd"])
                op("dve", lambda e, tt=tt: e.tensor_scalar(out=negbeta, in0=bet[:, tt, :], scalar1=-1.0, scalar2=None,
                                                           op0=ALU.mult), reads=["bet"], writes=["negbeta"])
                qv = qkv_tok[:, tt, 0:512].rearrange("p (h d) -> p h d", h=8)
                kv = qkv_tok[:, tt, 512:1024].rearrange("p (h d) -> p h d", h=8)
                vv = qkv_tok[:, tt, 1024:1536].rearrange("p (h d) -> p h d", h=8)

                def v3(t_):
                    return t_.rearrange("p (h d) -> p h d", h=8)
                op("dve", lambda e, qv=qv: e.tensor_tensor(out=v3(qn), in0=qv, in1=bc8(cq), op=ALU.mult),
                   reads=["qkv_all", "cq"], writes=["qn"])
                op("dve", lambda e, qv=qv: e.tensor_tensor(out=v3(qd), in0=qv, in1=bc8(cqd), op=ALU.mult),
                   reads=["qkv_all", "cqd"], writes=["qd"])
                op("dve", lambda e, kv=kv: e.tensor_tensor(out=v3(kn), in0=kv, in1=bc8(rn[:, 8:16]), op=ALU.mult),
                   reads=["qkv_all", "rn"], writes=["kn"])
                op("dve", lambda e, kv=kv: e.tensor_tensor(out=v3(rhsk), in0=kv, in1=bc8(cbk), op=ALU.mult),
                   reads=["qkv_all", "cbk"], writes=["rhsk"])
                op("dve", lambda e, kv=kv: e.tensor_tensor(out=v3(kdec), in0=kv, in1=bc8(ckd), op=ALU.mult),
                   reads=["qkv_all", "ckd"], writes=["kdec"])
                op("dve", lambda e, vv=vv, tt=tt: e.tensor_tensor(out=v3(rhsv), in0=vv, in1=bc8(bet[:, tt, :]), op=ALU.mult),
                   reads=["qkv_all", "bet"], writes=["rhsv"])
                if stop_after == "gdn_b":
                    return
                for (src, skey, dst, dkey, bank, coff, eng) in ((qn, "qn", qnT, "qnT", 6, 0, "act"), (qd, "qd", qdT, "qdT", 7, 0, "dve"),
                                                                (kn, "kn", knT, "knT", 0, 0, "act")):
                    for q in range(4):
                        op("pe", lambda e, src=src, bank=bank, coff=coff, q=q: e.transpose(
                            out=PBb(bank)[:, coff + q * 128:coff + (q + 1) * 128], in_=src[:, q * 128:(q + 1) * 128], identity=ident_b),
                           reads=[skey, "ident_b"], writes=[("pb", bank)])
                    if eng == "act":
                        op("act", lambda e, dst=dst, bank=bank, coff=coff: e.copy(
                            out=dst, in_=PBb(bank)[:, coff:coff + 512].rearrange("p (q t) -> p q t", q=4)),
                           reads=[("pb", bank)], writes=[dkey])
                    else:
                        op("dve", lambda e, dst=dst, bank=bank, coff=coff: e.tensor_copy(
                            out=dst, in_=PBb(bank)[:, coff:coff + 512].rearrange("p (q t) -> p q t", q=4)),
                           reads=[("pb", bank)], writes=[dkey])
                    if stop_after == "gdn_c_" + skey:
                        return
                if tt == 0:
                    dump("qn0", qn, ["qn"]); dump("kn0", kn, ["kn"]); dump("rhsv0", rhsv, ["rhsv"]); dump("rhsk0", rhsk, ["rhsk"])
                    dump("kdec0", kdec, ["kdec"]); dump("qd0", qd, ["qd"]); dump("gs0", gs, ["gs"])
                    dump("knT0", knT.rearrange("p a t -> p (a t)"), ["knT"])
                    dump("rn0", rn, ["rn"]); dump("cq0", cq, ["cq"]); dump("cqd0", cqd, ["cqd"]); dump("cbk0", cbk, ["cbk"])
                    dump("ckd0", ckd, ["ckd"]); dump("eG0", eG, ["eG"]); dump("ssn0", ssn, ["ssn"])
                if stop_after == "gdn_c":
                    return
                def group_gen(hg, bA, bB, bC, bT):
                    hs_list = list(range(4))
                    grp = slice(4 * hg, 4 * hg + 4)
                    for hs in hs_list:
                        h = 4 * hg + hs
                        hp, par = h // 2, h % 2
                        rows = slice(par * 64, par * 64 + 64)
                        cs_ = slice(hs * 128, hs * 128 + 128)
                        op("pe", lambda e, hp=hp, rows=rows, cs_=cs_, par=par: e.matmul(
                            PB(bA)[:, cs_], lhsT=knT[rows, hp, :], rhs=knT[rows, hp, :], start=True, stop=True,
                            tile_position=(par * 64, 0)), reads=["knT"], writes=[("pb", bA)])
                        op("pe", lambda e, hp=hp, rows=rows, cs_=cs_, par=par: e.matmul(
                            PB(bB)[:, cs_], lhsT=qnT[rows, hp, :], rhs=knT[rows, hp, :], start=True, stop=True,
                            tile_position=(par * 64, 0)), reads=["knT", "qnT"], writes=[("pb", bB)])
                        op("pe", lambda e, h=h, cs_=cs_: e.matmul(PB(bC)[:, cs_], lhsT=g_bc[:, h, :], rhs=ucs_f, start=True, stop=False),
                           reads=["g_bc", "ucs_f"], writes=[("pb", bC)])
                        op("pe", lambda e, cs_=cs_: e.matmul(PB(bC)[:, cs_], lhsT=ident_f, rhs=maskneg_f, start=False, stop=True),
                           reads=["ident_f", "maskneg_f"], writes=[("pb", bC)])
                    yield
                    for hs in hs_list:
                        h = 4 * hg + hs
                        cs_ = slice(hs * 128, hs * 128 + 128)
                        op("act", lambda e, h=h, cs_=cs_: e.activation(out=Dm[:, h, :], in_=PB(bC)[:, cs_], func=AF.Exp,
                                                                       bias=gs[:, h:h + 1], scale=1.0),
                           reads=[("pb", bC), "gs"], writes=[("Dm", h)])
                        op("dve", lambda e, h=h: e.tensor_tensor(out=Ds[:, h, :], in0=Dm[:, h, :], in1=strict_b, op=ALU.mult),
                           reads=[("Dm", h), "strict_b"], writes=[("Ds", h)])
                        op("dve", lambda e, h=h, cs_=cs_: e.scalar_tensor_tensor(out=Mm[0][:, h, :], in0=PB(bA)[:, cs_],
                                                                                 scalar=negbeta[:, h:h + 1], in1=Ds[:, h, :],
                                                                                 op0=ALU.mult, op1=ALU.mult),
                           reads=[("pb", bA), "negbeta", ("Ds", h)], writes=[("M", 0, hg)])
                        op("dve", lambda e, h=h, cs_=cs_: e.tensor_tensor(out=qkm[:, h, :], in0=PB(bB)[:, cs_], in1=Dm[:, h, :],
                                                                          op=ALU.mult),
                           reads=[("pb", bB), ("Dm", h)], writes=[("qkm", hg)])
                    yield
                    for hs in hs_list:
                        h = 4 * hg + hs
                        cs_ = slice(hs * 128, hs * 128 + 128)
                        op("pe", lambda e, h=h, cs_=cs_: e.transpose(out=PBb(bT)[:, cs_], in_=Mm[0][:, h, :], identity=ident_b),
                           reads=[("M", 0, hg), "ident_b"], writes=[("pb", bT)])
                    op("act", lambda e: e.copy(out=Nm[0][:, grp, :], in_=PBb(bT)[:, 0:512].rearrange("p (q t) -> p q t", q=4)),
                       reads=[("pb", bT)], writes=[("N", 0, hg)])
                    op("pool", lambda e: e.tensor_tensor(out=Pm[0][:, grp, :], in0=Nm[0][:, grp, :],
                                                         in1=ident_b.unsqueeze(1).to_broadcast([128, 4, 128]), op=ALU.add),
                       reads=[("N", 0, hg), "ident_b"], writes=[("P", 0, hg)])
                    yield
                    for hs in hs_list:
                        h = 4 * hg + hs
                        cs2 = slice(hs * 128, hs * 128 + 128)
                        op("pe", lambda e, h=h, cs2=cs2: e.transpose(out=PBb(bT)[:, cs2], in_=qkm[:, h, :], identity=ident_b),
                           reads=[("qkm", hg), "ident_b"], writes=[("pb", bT)])
                    op("dve", lambda e: e.tensor_copy(out=qkT_sb[:, grp, :], in_=PBb(bT)[:, 0:512].rearrange("p (q t) -> p q t", q=4)),
                       reads=[("pb", bT)], writes=[("qkT", hg)])
                    yield
                    for lv in range(1, 6):
                        cur, nxt = (lv - 1) % 2, lv % 2
                        for hs in hs_list:
                            h = 4 * hg + hs
                            cs_ = slice(hs * 128, hs * 128 + 128)
                            op("pe", lambda e, h=h, cs_=cs_, cur=cur: e.matmul(PB(bA)[:, cs_], lhsT=Nm[cur][:, h, :], rhs=Mm[cur][:, h, :],
                                                                               start=True, stop=True),
                               reads=[("N", cur, hg), ("M", cur, hg)], writes=[("pb", bA)])
                        if lv < 5:
                            for hs in hs_list:
                                h = 4 * hg + hs
                                cs_ = slice(hs * 128, hs * 128 + 128)
                                op("pe", lambda e, h=h, cs_=cs_, cur=cur: e.matmul(PB(bB)[:, cs_], lhsT=Mm[cur][:, h, :],
                                                                                   rhs=Nm[cur][:, h, :], start=True, stop=True),
                                   reads=[("N", cur, hg), ("M", cur, hg)], writes=[("pb", bB)])
                        yield
                        op("act", lambda e, nxt=nxt: e.copy(out=Mm[nxt][:, grp, :], in_=PB(bA).rearrange("p (q t) -> p q t", q=4)),
                           reads=[("pb", bA)], writes=[("M", nxt, hg)])
                        if lv < 5:
                            op("dve", lambda e, nxt=nxt: e.tensor_copy(out=Nm[nxt][:, grp, :],
                                                                       in_=PB(bB).rearrange("p (q t) -> p q t", q=4)),
                               reads=[("pb", bB)], writes=[("N", nxt, hg)])
                        for hs in hs_list:
                            h = 4 * hg + hs
                            cs_ = slice(hs * 128, hs * 128 + 128)
                            op("pe", lambda e, h=h, cs_=cs_, cur=cur, nxt=nxt: e.matmul(PB(bC)[:, cs_], lhsT=Mm[nxt][:, h, :],
                                                                                        rhs=Pm[cur][:, h, :], start=True, stop=True),
                               reads=[("M", nxt, hg), ("P", cur, hg)], writes=[("pb", bC)])
                        yield
                        op("dve", lambda e, cur=cur, nxt=nxt: e.tensor_tensor(
                            out=Pm[nxt][:, grp, :], in0=Pm[cur][:, grp, :], in1=PB(bC).rearrange("p (q t) -> p q t", q=4), op=ALU.add),
                           reads=[("pb", bC), ("P", cur, hg)], writes=[("P", nxt, hg)])

                gens = [group_gen(0, 3, 4, 5, 6), group_gen(1, 0, 1, 2, 7)]
                while gens:
                    for g_ in list(gens):
                        try:
                            next(g_)
                        except StopIteration:
                            gens.remove(g_)
                Pf = Pm[1]
                PK = [("P", 1, 0), ("P", 1, 1)]
                if tt == 0:
                    dump("D0", Dm.rearrange("p a t -> p (a t)"), [("Dm", h) for h in range(8)])
                    dump("M0", Mm[0].rearrange("p a t -> p (a t)"), [("M", 0, 0), ("M", 0, 1)])
                    dump("N0", Nm[0].rearrange("p a t -> p (a t)"), [("N", 0, 0), ("N", 0, 1)])
                    dump("P0", Pm[1].rearrange("p a t -> p (a t)"), [("P", 1, 0), ("P", 1, 1)])
                    dump("qkT0", qkT_sb.rearrange("p a t -> p (a t)"), [("qkT", 0), ("qkT", 1)])
                if stop_after == "gdn_e":
                    return
                for hf in range(2):
                    ub = 7 if hf == 0 else 0
                    for h in range(8):
                        op("pe", lambda e, h=h, hf=hf, ub=ub: e.matmul(PB(ub)[0:64, h * 64:(h + 1) * 64],
                                                                       lhsT=Pf[:, h, hf * 64:(hf + 1) * 64],
                                                                       rhs=rhsv[:, h * 64:(h + 1) * 64], start=True, stop=True),
                           reads=PK + ["rhsv"], writes=[("pb", ub)])
                    op("act", lambda e, hf=hf, ub=ub: e.copy(out=u_c[0:64, hf, :], in_=PB(ub)[0:64, :]), reads=[("pb", ub)],
                       writes=[("u_c", hf)])
                for h in range(8):
                    op("pe", lambda e, h=h: e.matmul(PB(1)[:, h * 64:(h + 1) * 64], lhsT=Pf[:, h, :], rhs=rhsk[:, h * 64:(h + 1) * 64],
                                                     start=True, stop=True), reads=PK + ["rhsk"], writes=[("pb", 1)])
                op("act", lambda e: e.copy(out=w_tok, in_=PB(1)), reads=[("pb", 1)], writes=["w_tok"])
                for q in range(4):
                    op("pe", lambda e, q=q: e.transpose(out=PBb(6)[:, q * 128:(q + 1) * 128], in_=w_tok[:, q * 128:(q + 1) * 128],
                                                        identity=ident_b), reads=["w_tok", "ident_b"], writes=[("pb", 6)])
                op("dve", lambda e: e.tensor_copy(out=wT_sb, in_=PBb(6)[:, 0:512].rearrange("p (q t) -> p q t", q=4)),
                   reads=[("pb", 6)], writes=["wT_sb"])
                op("sp", lambda e: e.dma_start(out=qkT_c1[0:64, :, :], in_=qkT_sb[64:128, :, 64:128]),
                   reads=[("qkT", 0), ("qkT", 1)], writes=["qkT_c1"], dma=True)
                op("sp", lambda e: e.dma_start(out=kdec_c1[0:64, :], in_=kdec[64:128, :]), reads=["kdec"], writes=["kdec_c1"],
                   dma=True)
                op("sp", lambda e, tt=tt: e.dma_start(out=az_c[0:64, :], in_=azs[64:128, tt, :]), reads=["azs_all"], writes=["az_c"],
                   dma=True)
                if tt == 0:
                    dump("u0", u_c.rearrange("p a t -> p (a t)"), [("u_c", 0), ("u_c", 1)])
                    dump("wT0", wT_sb.rearrange("p a t -> p (a t)"), ["wT_sb"])
                if stop_after == "gdn_f":
                    return
                for hf in range(2):
                    tcs = slice(hf * 64, hf * 64 + 64)
                    ck = 2 * tt + hf
                    if hf == 0:
                        qk_x, qk_keys = qkT_sb[0:64, :, 0:64], [("qkT", 0), ("qkT", 1)]
                        kd_x, kd_keys = kdec[0:64, :], ["kdec"]
                    else:
                        qk_x, qk_keys = qkT_c1[0:64, :, :], ["qkT_c1"]
                        kd_x, kd_keys = kdec_c1[0:64, :], ["kdec_c1"]
                    for hp in range(4):
                        op("pe", lambda e, hp=hp, tcs=tcs: e.matmul(PB(1)[0:64, hp * 128:(hp + 1) * 128], lhsT=wT_sb[:, hp, tcs],
                                                                    rhs=S_bd[:, hp, :], start=True, stop=True),
                           reads=["wT_sb", "S_bd"], writes=[("pb", 1)])
                    op("dve", lambda e, hf=hf: e.tensor_tensor(out=vn_b[0:64, :], in0=u_c[0:64, hf, :], in1=PB(1)[0:64, :],
                                                               op=ALU.subtract), reads=[("u_c", hf), ("pb", 1)], writes=["vn_b"])
                    for h in range(8):
                        hp, par = h // 2, h % 2
                        op("pe", lambda e, h=h, hp=hp, par=par, tcs=tcs: e.matmul(
                            PB(2)[0:64, h * 64:(h + 1) * 64], lhsT=qdT[:, hp, tcs], rhs=S_bd[:, hp, par * 64:(par + 1) * 64],
                            start=True, stop=False), reads=["qdT", "S_bd"], writes=[("pb", 2)])
                        op("pe", lambda e, h=h, qk_x=qk_x: e.matmul(
                            PB(2)[0:64, h * 64:(h + 1) * 64], lhsT=qk_x[:, h, :], rhs=vn_b[0:64, h * 64:(h + 1) * 64],
                            start=False, stop=True), reads=qk_keys + ["vn_b"], writes=[("pb", 2)])
                    for hp in range(4):
                        op("pe", lambda e, hp=hp, kd_x=kd_x: e.matmul(PB(7)[:, hp * 128:(hp + 1) * 128],
                                                                      lhsT=kd_x[:, hp * 128:(hp + 1) * 128],
                                                                      rhs=vn_b[0:64, hp * 128:(hp + 1) * 128], start=True, stop=True),
                           reads=kd_keys + ["vn_b"], writes=[("pb", 7)])
                    op("act", lambda e: e.copy(out=o_c[0:64, :], in_=PB(2)[0:64, :]), reads=[("pb", 2)], writes=["o_c"])
                    op("pool", lambda e, hf=hf: e.tensor_tensor(out=Stmp, in0=Sst,
                                                                in1=scs[hf].unsqueeze(2).to_broadcast([128, 4, 128]), op=ALU.mult),
                       reads=["S", ("scs", hf)], writes=["Stmp"])
                    op("dve", lambda e: e.tensor_tensor(out=Sst, in0=Stmp, in1=PB(7).rearrange("p (a d) -> p a d", a=4), op=ALU.add),
                       reads=["Stmp", ("pb", 7)], writes=["S"])
                    op("pool", lambda e: e.tensor_tensor(out=S_bd, in0=Sst, in1=bdmask, op=ALU.mult), reads=["S", "bdmask"],
                       writes=["S_bd"])
                    if "o_raw" in dbg:
                        op("sp", lambda e, ck=ck: e.dma_start(out=dbg["o_raw"][ck * 64:(ck + 1) * 64, :], in_=o_c[0:64, :]),
                           reads=["o_c"], writes=["dbg_o_raw"], dma=True)
                    sqh = sq[0:64, 0:512]
                    op("dve", lambda e: e.tensor_tensor(out=sqh, in0=o_c[0:64, :], in1=o_c[0:64, :], op=ALU.mult), reads=["o_c"],
                       writes=["sq"])
                    op("dve", lambda e: e.tensor_reduce(out=ss2[0:64, :], in_=sqh.rearrange("p (h d) -> p h d", h=8), axis=AX.X,
                                                        op=ALU.add), reads=["sq"], writes=["ss2"])
                    op("act", lambda e: e.activation(out=r2[0:64, :], in_=ss2[0:64, :], func=AF.Ln, bias=epsc[0:64, :],
                                                     scale=1.0 / 64), reads=["ss2", "epsc"], writes=["r2"])
                    op("act", lambda e: e.activation(out=r2[0:64, :], in_=r2[0:64, :], func=AF.Exp, scale=-0.5), reads=["r2"],
                       writes=["r2"])
                    op("dve", lambda e: e.tensor_tensor(out=v3(sqh), in0=v3(o_c[0:64, :]),
                                                        in1=r2[0:64, :].unsqueeze(2).to_broadcast([64, 8, 64]), op=ALU.mult),
                       reads=["o_c", "r2"], writes=["sq"])
                    op("dve", lambda e: e.tensor_tensor(out=v3(sqh), in0=v3(sqh),
                                                        in1=angb[0:64, :].unsqueeze(1).to_broadcast([64, 8, 64]), op=ALU.mult),
                       reads=["sq", "angb"], writes=["sq"])
                    az_x = azs[0:64, tt, :] if hf == 0 else az_c[0:64, :]
                    op("dve", lambda e, az_x=az_x: e.tensor_tensor(out=oa_b[0:64, :], in0=sqh, in1=az_x, op=ALU.mult),
                       reads=["sq", "az_c"], writes=["oa_b"])
                    for q in range(4):
                        op("pe", lambda e, q=q: e.transpose(out=PBb(6)[:, q * 64:(q + 1) * 64], in_=oa_b[0:64, q * 128:(q + 1) * 128],
                                                            identity=ident_b[0:64, 0:64]), reads=["oa_b", "ident_b"],
                           writes=[("pb", 6)])
                    op("act", lambda e, ck=ck: e.copy(out=o_aT[:, :, ck * 64:(ck + 1) * 64],
                                                      in_=PBb(6)[:, 0:256].rearrange("p (q t) -> p q t", q=4)),
                       reads=[("pb", 6)], writes=[("o_aT", ck)])
            if "o_raw" in dbg:
                op("sp", None, reads=["dbg_o_raw"])
            if "o_aT" in dbg:
                op("sp", lambda e: e.dma_start(out=dbg["o_aT"].rearrange("(a p) t -> p a t", p=128), in_=o_aT),
                   reads=[("o_aT", ck) for ck in range(2 * NT)], writes=["dbg_o_aT"], dma=True)
                op("sp", None, reads=["dbg_o_aT"])

            sch.barrier()
            if stop_after == "gdn":
                return

            o_bT = view(R_A + 16 * K, [128, 4, S], BF16)
            da = MultiAlloc([(R_Q, R_Q + 48 * K), (R_W, R_W + 16 * K)])
            scoreb = [da([128, S], F32) for _ in range(2)]
            rl = [da([128, 512], F32) for _ in range(2)]
            maskbb = [da([128, S], BF16) for _ in range(2)]
            thr_t = [da([128, 1], F32) for _ in range(2)]
            PTt = [[da([128, 512], BF16) for _ in range(2)] for _ in range(2)]
            I4 = da([128, 512], BF16)
            lo_t = da([128, 1], F32)
            hi_t = da([128, 1], F32)
            W0 = da([128, 1], F32)
            mid_t = da([128, 1], F32)
            tsel = da([128, 1], F32)
            Wk = da([128, NBIS], F32)
            cnt = da([128, NBIS], F32)
            pow2 = da([128, NBIS], F32)
            ob = da([128, 520], F32)
            rden = da([128, 8], F32)
            ob_b = da([128, 512], BF16)
            for q in range(4):
                op("pool", lambda e, q=q: e.tensor_copy(out=I4[:, q * 128:(q + 1) * 128], in_=ident_b), reads=["ident_b"], writes=["I4"])
            for k in range(NBIS):
                op("pool", lambda e, k=k: e.memset(pow2[:, k:k + 1], 2.0 ** (-(k + 1))), writes=["pow2"])

            xbk = [0]

            def scores_part(tt, sb):
                L = (tt + 1) * 128
                nkb = (L + 511) // 512
                qs = slice(tt * 128, (tt + 1) * 128)
                score = scoreb[sb]
                maskb = maskbb[sb]
                for h in range(8):
                    hp, par = h // 2, h % 2
                    rows = slice(par * 64, par * 64 + 64)
                    for kb in range(nkb):
                        w = min(512, L - kb * 512)
                        bank = xbk[0] % 2
                        xbk[0] += 1
                        ks = slice(kb * 512, kb * 512 + w)
                        op("pe", lambda e, hp=hp, par=par, rows=rows, w=w, bank=bank, ks=ks: e.matmul(
                            PB(bank)[:, 0:w], lhsT=iqT[rows, hp, qs], rhs=ikT2[rows, ks], start=True, stop=True,
                            tile_position=(par * 64, 0)), reads=["iqT", "ikT2"], writes=[("pb", bank)])
                        op("act", lambda e, w=w, bank=bank: e.activation(out=rl[bank][:, 0:w], in_=PB(bank)[:, 0:w], func=AF.Relu),
                           reads=[("pb", bank)], writes=[("rl", bank)])
                        if h == 0:
                            op("dve", lambda e, w=w, bank=bank, ks=ks: e.tensor_scalar(
                                out=score[:, ks], in0=rl[bank][:, 0:w], scalar1=iw_tok[:, tt, 0:1], scalar2=None, op0=ALU.mult),
                               reads=[("rl", bank), "iw_tok"], writes=[("score", sb, kb)])
                        else:
                            op("dve", lambda e, w=w, bank=bank, ks=ks, h=h: e.scalar_tensor_tensor(
                                out=score[:, ks], in0=rl[bank][:, 0:w], scalar=iw_tok[:, tt, h:h + 1], in1=score[:, ks],
                                op0=ALU.mult, op1=ALU.add), reads=[("rl", bank), "iw_tok", ("score", sb, kb)],
                               writes=[("score", sb, kb)], fast=(w >= 256))
                SK = [("score", sb, kb) for kb in range(nkb)]
                if tt >= 2:
                    op("dve", lambda e: e.tensor_reduce(out=hi_t, in_=score[:, 0:L], axis=AX.X, op=ALU.max), reads=SK, writes=["hi"])
                    op("dve", lambda e: e.tensor_reduce(out=lo_t, in_=score[:, 0:L], axis=AX.X, op=ALU.min), reads=SK, writes=["lo"])
                op("dve", lambda e: e.memset(score[0:64, L - 64:L], -1.0e30), reads=SK, writes=SK)
                if tt >= 2:
                    op("dve", lambda e: e.tensor_tensor(out=W0, in0=hi_t, in1=lo_t, op=ALU.subtract), reads=["hi", "lo"], writes=["W0"])
                    op("dve", lambda e: e.tensor_scalar(out=Wk, in0=pow2, scalar1=W0[:, 0:1], scalar2=None, op0=ALU.mult),
                       reads=["W0", "pow2"], writes=["Wk"])
                    op("dve", lambda e: e.memset(cnt, 0.0), writes=["cnt"])
                    op("dve", lambda e: e.tensor_tensor(out=mid_t, in0=lo_t, in1=Wk[:, 0:1], op=ALU.add), reads=["lo", "Wk"], writes=["mid"])
                    for k in range(NBIS):
                        op("dve", lambda e, k=k: e.tensor_scalar(out=maskb[:, 0:L], in0=score[:, 0:L], scalar1=mid_t[:, 0:1],
                                                                 scalar2=0.0, op0=ALU.is_gt, op1=ALU.add, accum_out=cnt[:, k:k + 1]),
                           reads=SK + ["mid", "cnt"], writes=[("maskb", sb), ("cntk", k)])
                        op("dve", lambda e, k=k: e.tensor_scalar(out=tsel, in0=cnt[:, k:k + 1], scalar1=255.5, scalar2=0.5,
                                                                 op0=ALU.is_gt, op1=ALU.subtract), reads=[("cntk", k)], writes=["tsel"])
                        op("dve", lambda e, k=k: e.scalar_tensor_tensor(out=mid_t, in0=tsel, scalar=Wk[:, k:k + 1], in1=mid_t,
                                                                        op0=ALU.mult, op1=ALU.add),
                           reads=["tsel", "Wk", "mid"], writes=["mid"])
                    op("dve", lambda e: e.scalar_tensor_tensor(out=thr_t[sb], in0=Wk[:, NBIS - 1:NBIS], scalar=-0.5, in1=mid_t,
                                                               op0=ALU.mult, op1=ALU.add), reads=["Wk", "mid"], writes=[("thr", sb)])
                else:
                    op("dve", lambda e: e.memset(thr_t[sb], -1.0e29), writes=[("thr", sb)])
                op("dve", lambda e: e.tensor_scalar(out=maskb[:, 0:L], in0=score[:, 0:L], scalar1=thr_t[sb][:, 0:1], scalar2=NEG,
                                                    op0=ALU.is_le, op1=ALU.mult), reads=SK + [("thr", sb)], writes=[("maskb", sb)])
                if "thr" in dbg:
                    op("sp", lambda e: e.dma_start(out=dbg["thr"][tt * 128:(tt + 1) * 128, :], in_=thr_t[sb]), reads=[("thr", sb)],
                       writes=["dbg_thr"], dma=True)
                if "score" in dbg and tt == NT - 1:
                    op("sp", lambda e: e.dma_start(out=dbg["score"], in_=score), reads=SK, writes=["dbg_score"], dma=True)

            def attn_part(tt, sb):
                qs = slice(tt * 128, (tt + 1) * 128)
                maskb = maskbb[sb]
                for kb in range(tt + 1):
                    kcs = slice(kb * 128, (kb + 1) * 128)
                    for g2 in range(2):
                        bank = 2 + g2 + 2 * (kb % 2)
                        pt = PTt[g2][kb % 2]
                        op("pe", lambda e, bank=bank, kcs=kcs: e.matmul(PB(bank), lhsT=maskb[:, kcs], rhs=I4, start=True, stop=False),
                           reads=[("maskb", sb), "I4"], writes=[("pb", bank)])
                        for s_ in range(4):
                            h = 4 * g2 + s_
                            hp, par = h // 2, h % 2
                            kT = kz[g2][par]
                            op("pe", lambda e, bank=bank, s_=s_, kT=kT, kcs=kcs, hp=hp: e.matmul(
                                PB(bank)[:, s_ * 128:(s_ + 1) * 128], lhsT=kT[:, kcs], rhs=bqT[:, hp, qs], start=False, stop=(s_ == 3)),
                               reads=["bkT", "bqT"], writes=[("pb", bank)])
                        op("act", lambda e, bank=bank, pt=pt: e.activation(out=pt, in_=PB(bank), func=AF.Exp, scale=0.125),
                           reads=[("pb", bank)], writes=[("PT", g2, kb % 2)])
                        for s_ in range(4):
                            op("pe", lambda e, g2=g2, s_=s_, pt=pt, kb=kb: e.matmul(
                                PB(6 + g2)[:, s_ * 65:(s_ + 1) * 65], lhsT=pt[:, s_ * 128:(s_ + 1) * 128],
                                rhs=bv_tok[:, kb, g2 * 65:(g2 + 1) * 65], start=(kb == 0 and s_ == 0), stop=(kb == tt and s_ == 3)),
                               reads=[("PT", g2, kb % 2), "bv_tok"], writes=[("pb", 6 + g2)])
                op("act", lambda e: e.copy(out=ob[:, 0:260], in_=PB(6)[:, 0:260]), reads=[("pb", 6)], writes=["ob"])
                op("act", lambda e: e.copy(out=ob[:, 260:520], in_=PB(7)[:, 0:260]), reads=[("pb", 7)], writes=["ob"])
                obv = ob.rearrange("p (s e) -> p s e", e=65)
                op("dve", lambda e: e.reciprocal(out=rden, in_=obv[:, :, 64]), reads=["ob"], writes=["rden"])
                op("dve", lambda e: e.tensor_tensor(out=ob_b.rearrange("p (h d) -> p h d", h=8), in0=obv[:, :, 0:64],
                                                    in1=rden.unsqueeze(2).to_broadcast([128, 8, 64]), op=ALU.mult),
                   reads=["ob", "rden"], writes=["ob_b"])
                for q in range(4):
                    op("pe", lambda e, q=q: e.transpose(out=PBb(0)[:, q * 128:(q + 1) * 128], in_=ob_b[:, q * 128:(q + 1) * 128],
                                                        identity=ident_b), reads=["ob_b", "ident_b"], writes=[("pb", 0)])
                op("act", lambda e: e.copy(out=o_bT[:, :, qs], in_=PBb(0)[:, 0:512].rearrange("p (q t) -> p q t", q=4)),
                   reads=[("pb", 0)], writes=[("o_bT", tt)])

            scores_part(0, 0)
            for tt in range(NT):
                if tt + 1 < NT:
                    scores_part(tt + 1, (tt + 1) % 2)
                attn_part(tt, tt % 2)
            if "thr" in dbg:
                op("sp", None, reads=["dbg_thr"])
            if "score" in dbg:
                op("sp", None, reads=["dbg_score"])
            if "o_bT" in dbg:
                op("sp", lambda e: e.dma_start(out=dbg["o_bT"].rearrange("(a p) t -> p a t", p=128), in_=o_bT),
                   reads=[("o_bT", tt) for tt in range(NT)], writes=["dbg_o_bT"], dma=True)
                op("sp", None, reads=["dbg_o_bT"])

            sch.barrier()
            if stop_after == "dsa":
                return

            pa = MultiAlloc([(R_W, ARENA_BYTES)])
            hT2 = pa([128, 8, S], BF16)
            mergedT = pa([128, 8, S], BF16)
            x1 = pa([128, NT, D], F32)
            bgate = pa([128, 16], F32)
            g2col = pa([128, 8], F32)
            fng = pa([128, D], F32)
            wst2 = pa([128, 8, 256], F32)
            wg_bf = [pa([128, 8, 256], BF16) for _ in range(2)]
            wp_bf = [pa([128, 4, 256], BF16) for _ in range(2)]
            ga_s = pa([128, 512], BF16)
            gb_s = pa([128, 512], BF16)
            t1 = pa([128, 512], BF16)
            t2 = pa([128, 512], BF16)
            xt4 = [pa([128, D], F32) for _ in range(2)]
            op("sp", lambda e: e.dma_start(out=bgate, in_=bgate_d), writes=["bgate"], dma=True)
            op("sp", lambda e: e.dma_start(out=g2col, in_=g2_d), writes=["g2col"], dma=True)
            op("sp", lambda e: e.dma_start(out=fng, in_=fng_d.partition_broadcast(128)), writes=["fng"], dma=True)

            def phase1b():
                xa = Alloc(R_W + 64 * K, R_W + 128 * K)
                xt = [xa([128, D], F32) for _ in range(2)]
                hb = [xa([128, D], BF16) for _ in range(2)]
                junk = xa([128, D], BF16)
                ssx = xa([128, NT], F32)
                rsx = xa([128, NT], F32)
                op("dve", lambda e: e.memset(ssx, 0.0), writes=["ssx"])
                for tt in range(NT):
                    b = tt % 2
                    op("sp", lambda e, tt=tt, b=b: e.dma_start(out=xt[b], in_=x_d[tt * 128:(tt + 1) * 128, :]), writes=[("xt", b)], dma=True)
                    op("act", lambda e, tt=tt, b=b: e.activation(out=junk, in_=xt[b], func=AF.Square, accum_out=ssx[:, tt:tt + 1]),
                       reads=[("xt", b), "ssx"], writes=["junk", ("ssx", tt)])
                    op("act", lambda e, tt=tt: e.activation(out=rsx[:, tt:tt + 1], in_=ssx[:, tt:tt + 1], func=AF.Sqrt, bias=epsc,
                                                            scale=1.0 / D), reads=[("ssx", tt), "epsc"], writes=[("rsx", tt)])
                    op("dve", lambda e, tt=tt: e.reciprocal(out=rsx[:, tt:tt + 1], in_=rsx[:, tt:tt + 1]), reads=[("rsx", tt)],
                       writes=[("rsx", tt)])
                    op("dve", lambda e, tt=tt, b=b: e.tensor_scalar(out=hb[b], in0=xt[b], scalar1=rsx[:, tt:tt + 1], scalar2=None,
                                                                    op0=ALU.mult), reads=[("xt", b), ("rsx", tt)], writes=[("hb", b)])
                    bk = tt % 2
                    for k in range(8):
                        op("pe", lambda e, k=k, b=b, bk=bk: e.transpose(out=PBb(bk)[:, k * 128:(k + 1) * 128],
                                                                        in_=hb[b][:, k * 128:(k + 1) * 128], identity=ident_b),
                           reads=[("hb", b), "ident_b"], writes=[("pb", bk)])
                    op("act", lambda e, tt=tt, bk=bk: e.copy(out=hT2[:, :, tt * 128:(tt + 1) * 128],
                                                             in_=PBb(bk).rearrange("p (k t) -> p k t", k=8)),
                       reads=[("pb", bk)], writes=[("hT2", tt)])
            phase1b()
            sch.barrier()
            HT2 = [("hT2", tt) for tt in range(NT)]

            wpa_v = wpa_d.rearrange("(k p) c -> p k c", p=128)
            wpb_v = wpb_d.rearrange("(k p) c -> p k c", p=128)
            wout_v = wout_d.rearrange("(k p) c -> p k c", p=128)
            wi4 = [0]

            wst2b = xt4[0].rearrange("p (k c) -> p k c", k=4)
            wst2c = xt4[1].rearrange("p (k c) -> p k c", k=4)
            wi5 = [0]

            def load_gate(c0):
                i = wi4[0]
                wi4[0] += 1
                b = i % 2
                op("sp", lambda e: e.dma_start(out=wst2, in_=w_in_v[:, :, c0:c0 + 256]), writes=["wst2"], dma=True)
                op("pool", lambda e, b=b: e.tensor_tensor(out=wg_bf[b], in0=wst2, in1=g1col.unsqueeze(2).to_broadcast([128, 8, 256]),
                                                          op=ALU.mult), reads=["wst2", "g1col"], writes=[("wg", b)])
                return wg_bf[b], ("wg", b)

            def load_proj(src_v, c0):
                i = wi5[0]
                wi5[0] += 1
                b = i % 2
                st = wst2b if b == 0 else wst2c
                op("sp", lambda e: e.dma_start(out=st, in_=src_v[:, :, c0:c0 + 256]), writes=[("wstp", b)], dma=True)
                op("pool", lambda e, b=b: e.tensor_copy(out=wp_bf[b], in_=st), reads=[("wstp", b)], writes=[("wp", b)])
                return wp_bf[b], ("wp", b)

            p4u = [0]
            for j in range(4):
                wga, kga = load_gate(C_GA + j * 256)
                wgb, kgb = load_gate(C_GB + j * 256)
                wpa, kpa = load_proj(wpa_v, j * 256)
                wpb, kpb = load_proj(wpb_v, j * 256)
                for ct in range(2):
                    c = 2 * j + ct
                    ccs = slice(ct * 128, (ct + 1) * 128)
                    for tb in range(4):
                        tcs = slice(tb * 512, (tb + 1) * 512)
                        hk = HT2[tb * 4:tb * 4 + 4]
                        bA, bB, bC, bD = (2, 3, 4, 5) if (p4u[0] % 2 == 0) else (0, 1, 6, 7)
                        p4u[0] += 1
                        for k in range(8):
                            op("pe", lambda e, k=k, ccs=ccs, tcs=tcs, wga=wga, bA=bA: e.matmul(PB(bA), lhsT=wga[:, k, ccs], rhs=hT2[:, k, tcs],
                                                                                         start=(k == 0), stop=(k == 7)),
                               reads=[kga] + hk, writes=[("pb", bA)])
                        op("act", lambda e, c=c, bA=bA: e.activation(out=ga_s, in_=PB(bA), func=AF.Sigmoid, bias=bgate[:, c:c + 1], scale=1.0),
                           reads=[("pb", bA), "bgate"], writes=["ga_s"])
                        for k in range(8):
                            op("pe", lambda e, k=k, ccs=ccs, tcs=tcs, wgb=wgb, bB=bB: e.matmul(PB(bB), lhsT=wgb[:, k, ccs], rhs=hT2[:, k, tcs],
                                                                                         start=(k == 0), stop=(k == 7)),
                               reads=[kgb] + hk, writes=[("pb", bB)])
                        op("act", lambda e, c=c, bB=bB: e.activation(out=gb_s, in_=PB(bB), func=AF.Sigmoid, bias=bgate[:, 8 + c:9 + c], scale=1.0),
                           reads=[("pb", bB), "bgate"], writes=["gb_s"])
                        for hp in range(4):
                            op("pe", lambda e, hp=hp, ccs=ccs, tcs=tcs, wpa=wpa, bC=bC: e.matmul(PB(bC), lhsT=wpa[:, hp, ccs], rhs=o_aT[:, hp, tcs],
                                                                                           start=(hp == 0), stop=(hp == 3)),
                               reads=[kpa, "o_aT"], writes=[("pb", bC)])
                        for hp in range(4):
                            op("pe", lambda e, hp=hp, ccs=ccs, tcs=tcs, wpb=wpb, bD=bD: e.matmul(PB(bD), lhsT=wpb[:, hp, ccs], rhs=o_bT[:, hp, tcs],
                                                                                           start=(hp == 0), stop=(hp == 3)),
                               reads=[kpb, "o_bT"], writes=[("pb", bD)])
                        op("dve", lambda e, bC=bC: e.tensor_tensor(out=t1, in0=PB(bC), in1=ga_s, op=ALU.mult), reads=[("pb", bC), "ga_s"],
                           writes=["t1"])
                        op("dve", lambda e, bD=bD: e.tensor_tensor(out=t2, in0=PB(bD), in1=gb_s, op=ALU.mult), reads=[("pb", bD), "gb_s"],
                           writes=["t2"])
                        op("pool", lambda e, c=c, tcs=tcs: e.tensor_tensor(out=mergedT[:, c, tcs], in0=t1, in1=t2, op=ALU.add),
                           reads=["t1", "t2"], writes=[("mergedT", c, tb)])
            if "mergedT" in dbg:
                op("sp", lambda e: e.dma_start(out=dbg["mergedT"].rearrange("(a p) t -> p a t", p=128), in_=mergedT),
                   reads=[("mergedT", c, tb) for c in range(8) for tb in range(4)], writes=["dbg_mergedT"], dma=True)
                op("sp", None, reads=["dbg_mergedT"])
            sch.barrier()
            wout_bf = view(R_A, [128, 8, D], BF16)
            for j in range(4):
                op("sp", lambda e, j=j: e.dma_start(out=wst2, in_=wout_v[:, :, j * 256:(j + 1) * 256]), writes=["wst2"], dma=True)
                op("pool", lambda e, j=j: e.tensor_copy(out=wout_bf[:, :, j * 256:(j + 1) * 256], in_=wst2), reads=["wst2"],
                   writes=[("wout", j)])
            WOUT = [("wout", j) for j in range(4)]
            for tt in range(NT):
                b = tt % 2
                op("sp", lambda e, tt=tt, b=b: e.dma_start(out=xt4[b], in_=x_d[tt * 128:(tt + 1) * 128, :]), writes=[("xt4", b)], dma=True)
                for nb in range(2):
                    bk = 2 + 2 * b + nb
                    for c in range(8):
                        op("pe", lambda e, c=c, tt=tt, nb=nb, bk=bk: e.matmul(PB(bk), lhsT=mergedT[:, c, tt * 128:(tt + 1) * 128],
                                                                               rhs=wout_bf[:, c, nb * 512:(nb + 1) * 512],
                                                                               start=(c == 0), stop=(c == 7)),
                           reads=WOUT + ["mergedT_all"], writes=[("pb", bk)])
                    op("dve", lambda e, tt=tt, nb=nb, bk=bk, b=b: e.tensor_tensor(out=x1[:, tt, nb * 512:(nb + 1) * 512], in0=PB(bk),
                                                                                   in1=xt4[b][:, nb * 512:(nb + 1) * 512], op=ALU.add),
                       reads=[("pb", bk), ("xt4", b)], writes=[("x1", tt)])
            if "x1" in dbg:
                op("sp", lambda e: e.dma_start(out=dbg["x1"].rearrange("(t p) c -> p t c", p=128), in_=x1),
                   reads=[("x1", tt) for tt in range(NT)], writes=["dbg_x1"], dma=True)
                op("sp", None, reads=["dbg_x1"])
            sch.barrier()
            if stop_after == "p4":
                return

            h2T = view(R_W, [128, 8, S], BF16)
            ma = MultiAlloc([(R_W + 32 * K, R_W + 64 * K), (R_A, R_A + 32 * K)])
            tail_off = None
            hb2 = [ma([128, D], BF16) for _ in range(2)]
            junk2 = ma([128, D], BF16)
            ss5 = ma([128, NT], F32)
            rs5 = ma([128, NT], F32)
            wr_st = ma([128, 8, 20], F32)
            wr_bf = ma([128, 8, 20], BF16)
            brow = ma([128, 20], F32)
            lg = ma([128, 20], F32)
            sm = {n_: ma([128, 4], F32) for n_ in ("goh", "gex", "elg", "oh1", "msk", "oh2", "wsel")}
            sc1 = {n_: ma([128, 1], F32) for n_ in ("gmax", "ngmax", "gsum", "ggate", "m1", "m2", "d21", "e21", "den", "w1", "w2")}
            tmp44 = ma([128, 4, 4], F32)
            comb_b = ma([128, 16], BF16)
            combT = ma([128, S], BF16)
            sel16 = ma([128, 16, 128], BF16)
            est = ma([128, 8, 256], F32)
            w1b = [ma([128, 8, 256], BF16) for _ in range(2)]
            w3b = [ma([128, 8, 256], BF16) for _ in range(2)]
            w2b = [ma([128, 2, D], BF16) for _ in range(2)]
            sg = [[ma([128, 512], BF16) for _ in range(2)] for _ in range(2)]
            cbt = [ma([128, 512], BF16) for _ in range(2)]
            tu = [[ma([128, 512], BF16) for _ in range(2)] for _ in range(2)]
            actT = [[ma([128, 512], BF16) for _ in range(2)] for _ in range(2)]
            op("sp", lambda e: e.dma_start(out=wr_st, in_=wr_d.rearrange("(k p) c -> p k c", p=128)), writes=["wr_st"], dma=True)
            op("sp", lambda e: e.dma_start(out=brow, in_=br_d.partition_broadcast(128)), writes=["brow"], dma=True)
            op("pool", lambda e: e.tensor_tensor(out=wr_bf, in0=wr_st, in1=g2col.unsqueeze(2).to_broadcast([128, 8, 20]), op=ALU.mult),
               reads=["wr_st", "g2col"], writes=["wr_bf"])
            op("pool", lambda e: e.memset(sel16[0:16, :, :], 1.0), writes=["sel16"])
            op("pool", lambda e: e.affine_select(out=sel16[0:16, :, :], in_=sel16[0:16, :, :], pattern=[[-1, 16], [0, 128]],
                                                 compare_op=ALU.is_equal, fill=0.0, base=0, channel_multiplier=1), writes=["sel16"])
            op("dve", lambda e: e.memset(ss5, 0.0), writes=["ss5"])
            def prep_tile(tt):
                b = tt % 2
                bk = tt % 2
                op("act", lambda e, tt=tt: e.activation(out=junk2, in_=x1[:, tt, :], func=AF.Square, accum_out=ss5[:, tt:tt + 1]),
                   reads=["x1_all", "ss5"], writes=["junk2", ("ss5", tt)])
                op("act", lambda e, tt=tt: e.activation(out=rs5[:, tt:tt + 1], in_=ss5[:, tt:tt + 1], func=AF.Ln, bias=epsc,
                                                        scale=1.0 / D), reads=[("ss5", tt), "epsc"], writes=[("rs5", tt)])
                op("act", lambda e, tt=tt: e.activation(out=rs5[:, tt:tt + 1], in_=rs5[:, tt:tt + 1], func=AF.Exp, scale=-0.5),
                   reads=[("rs5", tt)], writes=[("rs5", tt)])
                op("dve", lambda e, tt=tt, b=b: e.tensor_scalar(out=hb2[b], in0=x1[:, tt, :], scalar1=rs5[:, tt:tt + 1], scalar2=None,
                                                                op0=ALU.mult), reads=["x1_all", ("rs5", tt)], writes=[("hb2", b)])
                for k in range(8):
                    op("pe", lambda e, k=k, b=b, bk=bk: e.transpose(out=PBb(bk)[:, k * 128:(k + 1) * 128],
                                                                    in_=hb2[b][:, k * 128:(k + 1) * 128], identity=ident_b),
                       reads=[("hb2", b), "ident_b"], writes=[("pb", bk)])
                op("act", lambda e, tt=tt, bk=bk: e.copy(out=h2T[:, :, tt * 128:(tt + 1) * 128],
                                                         in_=PBb(bk).rearrange("p (k t) -> p k t", k=8)),
                   reads=[("pb", bk)], writes=[("h2T", tt)])
                for k in range(8):
                    op("pe", lambda e, k=k, tt=tt: e.matmul(PB(2)[:, 0:20], lhsT=h2T[:, k, tt * 128:(tt + 1) * 128], rhs=wr_bf[:, k, :],
                                                            start=(k == 0), stop=(k == 7)),
                       reads=[("h2T", tt), "wr_bf"], writes=[("pb", 2)])
                R = []

                def rop(fn, rd, wr):
                    op("dve", fn, reads=rd, writes=wr)
                rop(lambda e: e.tensor_tensor(out=lg, in0=PB(2)[:, 0:20], in1=brow, op=ALU.add), [("pb", 2), "brow"], ["lg"])
                elv = lg[:, 4:20].rearrange("p (g x) -> p g x", g=4)
                rop(lambda e: e.tensor_reduce(out=sc1["gmax"], in_=lg[:, 0:4], axis=AX.X, op=ALU.max), ["lg"], ["gmax"])
                rop(lambda e: e.tensor_scalar(out=sm["goh"], in0=lg[:, 0:4], scalar1=sc1["gmax"][:, 0:1], scalar2=None,
                                              op0=ALU.is_equal), ["lg", "gmax"], ["goh"])
                rop(lambda e: e.tensor_scalar(out=sc1["ngmax"], in0=sc1["gmax"], scalar1=-1.0, scalar2=None, op0=ALU.mult),
                    ["gmax"], ["ngmax"])
                op("act", lambda e: e.activation(out=sm["gex"], in_=lg[:, 0:4], func=AF.Exp, bias=sc1["ngmax"][:, 0:1], scale=1.0),
                   reads=["lg", "ngmax"], writes=["gex"])
                rop(lambda e: e.tensor_reduce(out=sc1["gsum"], in_=sm["gex"], axis=AX.X, op=ALU.add), ["gex"], ["gsum"])
                rop(lambda e: e.reciprocal(out=sc1["ggate"], in_=sc1["gsum"]), ["gsum"], ["ggate"])
                rop(lambda e: e.tensor_tensor(out=tmp44, in0=elv, in1=sm["goh"].unsqueeze(2).to_broadcast([128, 4, 4]), op=ALU.mult),
                    ["lg", "goh"], ["tmp44"])
                rop(lambda e: e.tensor_reduce(out=sm["elg"], in_=tmp44.rearrange("p g x -> p x g"), axis=AX.X, op=ALU.add),
                    ["tmp44"], ["elg"])
                rop(lambda e: e.tensor_reduce(out=sc1["m1"], in_=sm["elg"], axis=AX.X, op=ALU.max), ["elg"], ["m1"])
                rop(lambda e: e.tensor_scalar(out=sm["oh1"], in0=sm["elg"], scalar1=sc1["m1"][:, 0:1], scalar2=None, op0=ALU.is_equal),
                    ["elg", "m1"], ["oh1"])
                rop(lambda e: e.scalar_tensor_tensor(out=sm["msk"], in0=sm["oh1"], scalar=-1.0e30, in1=sm["elg"], op0=ALU.mult,
                                                     op1=ALU.add), ["oh1", "elg"], ["msk"])
                rop(lambda e: e.tensor_reduce(out=sc1["m2"], in_=sm["msk"], axis=AX.X, op=ALU.max), ["msk"], ["m2"])
                rop(lambda e: e.tensor_scalar(out=sm["oh2"], in0=sm["msk"], scalar1=sc1["m2"][:, 0:1], scalar2=None, op0=ALU.is_equal),
                    ["msk", "m2"], ["oh2"])
                rop(lambda e: e.tensor_tensor(out=sc1["d21"], in0=sc1["m2"], in1=sc1["m1"], op=ALU.subtract), ["m1", "m2"], ["d21"])
                op("act", lambda e: e.activation(out=sc1["e21"], in_=sc1["d21"], func=AF.Exp), reads=["d21"], writes=["e21"])
                rop(lambda e: e.tensor_scalar(out=sc1["den"], in0=sc1["e21"], scalar1=1.0, scalar2=None, op0=ALU.add), ["e21"], ["den"])
                rop(lambda e: e.reciprocal(out=sc1["den"], in_=sc1["den"]), ["den"], ["den"])
                rop(lambda e: e.tensor_tensor(out=sc1["w1"], in0=sc1["ggate"], in1=sc1["den"], op=ALU.mult), ["ggate", "den"], ["w1"])
                rop(lambda e: e.tensor_tensor(out=sc1["w2"], in0=sc1["w1"], in1=sc1["e21"], op=ALU.mult), ["w1", "e21"], ["w2"])
                rop(lambda e: e.tensor_scalar(out=sm["wsel"], in0=sm["oh1"], scalar1=sc1["w1"][:, 0:1], scalar2=None, op0=ALU.mult),
                    ["oh1", "w1"], ["wsel"])
                rop(lambda e: e.scalar_tensor_tensor(out=sm["wsel"], in0=sm["oh2"], scalar=sc1["w2"][:, 0:1], in1=sm["wsel"],
                                                     op0=ALU.mult, op1=ALU.add), ["oh2", "w2", "wsel"], ["wsel"])
                rop(lambda e: e.tensor_tensor(out=comb_b.rearrange("p (g x) -> p g x", g=4),
                                              in0=sm["goh"].unsqueeze(2).to_broadcast([128, 4, 4]),
                                              in1=sm["wsel"].unsqueeze(1).to_broadcast([128, 4, 4]), op=ALU.mult),
                    ["goh", "wsel"], ["comb_b"])
                if "comb" in dbg:
                    op("sp", lambda e, tt=tt: e.dma_start(out=dbg["comb"][tt * 128:(tt + 1) * 128, :], in_=comb_b), reads=["comb_b"],
                       writes=["dbg_comb"], dma=True)
                op("pe", lambda e: e.transpose(out=PBb(3)[0:16, 0:128], in_=comb_b, identity=ident_b), reads=["comb_b", "ident_b"],
                   writes=[("pb", 3)])
                op("act", lambda e, tt=tt: e.copy(out=combT[0:16, tt * 128:(tt + 1) * 128], in_=PBb(3)[0:16, 0:128]),
                   reads=[("pb", 3)], writes=[("combT", tt)])
            for tt in range(4):
                prep_tile(tt)
            H2T = [("h2T", tt) for tt in range(NT)]
            CT = [("combT", tt) for tt in range(NT)]

            def load_expert(e_i):
                b = e_i % 2
                for (src, dst, nm, fold) in ((w1_d, w1b[b], "w1", True), (w3_d, w3b[b], "w3", True)):
                    op("sp", lambda e, src=src: e.dma_start(out=est, in_=src[e_i].rearrange("(k p) f -> p k f", p=128)),
                       writes=["est"], dma=True)
                    op("pool", lambda e, dst=dst: e.tensor_tensor(out=dst, in0=est, in1=g2col.unsqueeze(2).to_broadcast([128, 8, 256]),
                                                                  op=ALU.mult), reads=["est", "g2col"], writes=[(nm, b)])
                op("sp", lambda e: e.dma_start(out=est.rearrange("p k f -> p (k f)").rearrange("p (a c) -> p a c", a=2),
                                               in_=w2_d[e_i].rearrange("(a p) c -> p a c", p=128)), writes=["est"], dma=True)
                op("pool", lambda e: e.tensor_copy(out=w2b[b], in_=est.rearrange("p k f -> p (k f)").rearrange("p (a c) -> p a c", a=2)),
                   reads=["est"], writes=[("w2", b)])

            def stageA(e_i, tb, sl):
                b = e_i % 2
                tcs = slice(tb * 512, (tb + 1) * 512)
                hk = H2T[tb * 4:tb * 4 + 4]
                cbk_ = 6
                op("pe", lambda e: e.matmul(PB(cbk_), lhsT=sel16[0:16, e_i, :], rhs=combT[0:16, tcs], start=True, stop=True),
                   reads=["sel16"] + CT[tb * 4:tb * 4 + 4], writes=[("pb", cbk_)])
                op("act", lambda e: e.copy(out=cbt[sl], in_=PB(cbk_)), reads=[("pb", cbk_)], writes=[("cbt", sl)])
                for ft in range(2):
                    fcs = slice(ft * 128, (ft + 1) * 128)
                    for k in range(8):
                        op("pe", lambda e, k=k, fcs=fcs, ft=ft: e.matmul(PB(2 + ft), lhsT=w1b[b][:, k, fcs], rhs=h2T[:, k, tcs],
                                                                         start=(k == 0), stop=(k == 7)),
                           reads=[("w1", b)] + hk, writes=[("pb", 2 + ft)])
                    op("act", lambda e, ft=ft: e.activation(out=sg[sl][ft], in_=PB(2 + ft), func=AF.Silu), reads=[("pb", 2 + ft)],
                       writes=[("sg", sl, ft)])
                    yield
                    for k in range(8):
                        op("pe", lambda e, k=k, fcs=fcs, ft=ft: e.matmul(PB(4 + ft), lhsT=w3b[b][:, k, fcs], rhs=h2T[:, k, tcs],
                                                                         start=(k == 0), stop=(k == 7)),
                           reads=[("w3", b)] + hk, writes=[("pb", 4 + ft)])
                    op("dve", lambda e, ft=ft: e.tensor_tensor(out=tu[sl][ft], in0=PB(4 + ft), in1=sg[sl][ft], op=ALU.mult),
                       reads=[("pb", 4 + ft), ("sg", sl, ft)], writes=[("tu", sl, ft)], fast=True)
                    op("pool", lambda e, ft=ft: e.tensor_tensor(out=actT[sl][ft], in0=tu[sl][ft], in1=cbt[sl], op=ALU.mult),
                       reads=[("tu", sl, ft), ("cbt", sl)], writes=[("actT", sl, ft)])
                    yield

            ybank = [0]

            def stageB(e_i, tb, sl):
                b = e_i % 2
                for t4 in range(4):
                    tt = tb * 4 + t4
                    for nb in range(2):
                        bk = (0, 1, 7)[ybank[0] % 3]
                        ybank[0] += 1
                        for ft in range(2):
                            op("pe", lambda e, ft=ft, t4=t4, nb=nb, bk=bk: e.matmul(
                                PB(bk), lhsT=actT[sl][ft][:, t4 * 128:(t4 + 1) * 128], rhs=w2b[b][:, ft, nb * 512:(nb + 1) * 512],
                                start=(ft == 0), stop=(ft == 1)), reads=[("actT", sl, ft), ("w2", b)], writes=[("pb", bk)])
                        op("dve", lambda e, tt=tt, nb=nb, bk=bk: e.tensor_tensor(out=x1[:, tt, nb * 512:(nb + 1) * 512],
                                                                                 in0=PB(bk), in1=x1[:, tt, nb * 512:(nb + 1) * 512],
                                                                                 op=ALU.add),
                           reads=[("pb", bk), ("x2", tt, nb)], writes=[("x2", tt, nb)], fast=True)
                        if nb == 1:
                            yield

            def drain(g_):
                for _ in g_:
                    pass

            units = [(e_i, tb) for e_i in range(16) for tb in range(4)]
            load_expert(0)
            load_expert(1)
            drain(stageA(units[0][0], units[0][1], 0))
            for u, (e_i, tb) in enumerate(units):
                gb = stageB(e_i, tb, u % 2)
                if u + 1 < len(units):
                    ne, ntb = units[u + 1]
                    if ne == 0:
                        for tt in range(4 * ntb, 4 * ntb + 4):
                            prep_tile(tt)
                    ga_ = stageA(ne, ntb, (u + 1) % 2)
                    drain(ga_)
                drain(gb)
                if tb == 3 and e_i + 2 < 16:
                    load_expert(e_i + 2)
            sch.barrier()
            if "x2" in dbg:
                op("sp", lambda e: e.dma_start(out=dbg["x2"].rearrange("(t p) c -> p t c", p=128), in_=x1), writes=["dbg_x2"], dma=True)
                op("sp", None, reads=["dbg_x2"])

            fa = MultiAlloc([(R_W, R_W + 64 * K)])
            ss6 = fa([128, NT], F32)
            rs6 = fa([128, NT], F32)
            junk6 = fa([128, D], BF16)
            yo = [fa([128, D], F32) for _ in range(2)]
            op("dve", lambda e: e.memset(ss6, 0.0), writes=["ss6"])
            for tt in range(NT):
                b = tt % 2
                op("act", lambda e, tt=tt: e.activation(out=junk6, in_=x1[:, tt, :], func=AF.Square, accum_out=ss6[:, tt:tt + 1]),
                   reads=["ss6"], writes=["junk6", ("ss6", tt)])
                op("act", lambda e, tt=tt: e.activation(out=rs6[:, tt:tt + 1], in_=ss6[:, tt:tt + 1], func=AF.Sqrt, bias=epsc,
                                                        scale=1.0 / D), reads=[("ss6", tt), "epsc"], writes=[("rs6", tt)])
                op("dve", lambda e, tt=tt: e.reciprocal(out=rs6[:, tt:tt + 1], in_=rs6[:, tt:tt + 1]), reads=[("rs6", tt)],
                   writes=[("rs6", tt)])
                op("dve", lambda e, tt=tt, b=b: e.scalar_tensor_tensor(out=yo[b], in0=x1[:, tt, :], scalar=rs6[:, tt:tt + 1], in1=fng,
                                                                       op0=ALU.mult, op1=ALU.mult),
                   reads=[("rs6", tt), "fng"], writes=[("yo", b)])
                op("sp", lambda e, tt=tt, b=b: e.dma_start(out=out_d[tt * 128:(tt + 1) * 128, :], in_=yo[b]), reads=[("yo", b)],
                   writes=[("out", tt)], dma=True)
            op("sp", None, reads=[("out", tt) for tt in range(NT)])


        body()
        sch.barrier()
        DEBUG["stats_pre"] = {e: len(sch.ops[e]) for e in Sched.ENGS}
        with nc.Block() as block:
            sch.emit(nc, block, engsem, dmasem)
        DEBUG["stats"] = sch.stats
    return nc


_NC_CACHE = {}


def kernel(**inputs):
    dbg = tuple(DEBUG.get("outputs", ()))
    key = (dbg, DEBUG.get("stop_after"))
    if key not in _NC_CACHE:
        _NC_CACHE[key] = build_nc(dbg, DEBUG.get("stop_after"))
    nc = _NC_CACHE[key]
    n = 8
    x = np.ascontiguousarray(inputs["x"], dtype=np.float32)
    posn = np.ascontiguousarray(inputs["positions"], dtype=np.int32)
    f32 = lambda a: np.ascontiguousarray(a, dtype=np.float32)
    inv = (10000.0 ** (-np.arange(32, dtype=np.float32) / np.float32(32))).astype(np.float32).reshape(1, 32)
    shared = {
        "norm1_g": f32(inputs["norm1_g"][0].reshape(8, 128).T),
        "w_in": f32(inputs["w_in"][0]),
        "conv_w": f32(inputs["conv_w"][0].reshape(4, 12, 128).transpose(2, 1, 0).reshape(128, 48)),
        "inv_freq": inv,
        "a_log": f32(inputs["a_log"][0].reshape(1, 8)),
        "dt_bias": f32(inputs["dt_bias"][0].reshape(1, 8)),
        "a_norm_g": f32(inputs["a_norm_g"][0].reshape(1, 64)),
        "b_gate": f32(inputs["b_gate"][0].reshape(16, 128).T),
        "norm2_g": f32(inputs["norm2_g"][0].reshape(8, 128).T),
        "final_norm_g": f32(inputs["final_norm_g"].reshape(1, D)),
        "w_proj_a": f32(inputs["w_proj_a"][0]),
        "w_proj_b": f32(inputs["w_proj_b"][0]),
        "w_out": f32(inputs["w_out"][0]),
        "w_router": f32(np.concatenate([inputs["w_router_group"][0], inputs["w_router_expert"][0]], axis=1)),
        "b_router": f32(np.concatenate([inputs["b_router_group"][0], inputs["b_router_expert"][0]], axis=0).reshape(1, 20)),
        "w_exp_gate": f32(inputs["w_exp_gate"][0]),
        "w_exp_up": f32(inputs["w_exp_up"][0]),
        "w_exp_down": f32(inputs["w_exp_down"][0]),
    }
    in_maps = []
    for c in range(n):
        m = dict(shared)
        m["x"] = x[c]
        m["positions"] = np.ascontiguousarray(posn[c].reshape(NT, 128).T)
        in_maps.append(m)
    res = run_bass_kernel_spmd(nc, in_maps, core_ids=list(range(n)))
    DEBUG["results"] = res.results
    return np.stack([r["out"] for r in res.results], axis=0)
```

```python
import math
from contextlib import ExitStack
import numpy as np
import concourse.bass as bass
import concourse.mybir as mybir
from concourse.bass_utils import run_bass_kernel_spmd

F32 = mybir.dt.float32
BF16 = mybir.dt.bfloat16
I32 = mybir.dt.int32
AF = mybir.ActivationFunctionType
ALU = mybir.AluOpType
AX = mybir.AxisListType

S = 2048
D = 1024
NT = S // 128
D_IN = 5464
EPS = 1e-6
N_DMA_SEMS = 24
NEG = -30000.0
NBIS = 12
TWO_PI = 2.0 * math.pi

C_AQ, C_AK, C_AV, C_AZ = 0, 512, 1024, 1536
C_BETA, C_ALPHA = 2048, 2056
C_BQ, C_BK, C_BV = 2064, 2576, 2704
C_IQ, C_IK, C_IW = 2832, 3344, 3408
C_GA, C_GB = 3416, 4440

DEBUG = {}
STRICT_SAME_ENGINE = True


class Sched:
    ENGS = ("pe", "act", "dve", "pool", "sp")

    def __init__(self):
        self.ops = {e: [] for e in self.ENGS}
        self.last_w = {}
        self.readers = {}
        self.dma_rr = 0
        self.dma_count = [0] * N_DMA_SEMS

    def op(self, eng, fn, reads=(), writes=(), dma=False, fast=False):
        deps = set()
        raw = set()
        for k in reads:
            t = self.last_w.get(k)
            if t is not None:
                deps.add(t)
                raw.add(t)
        for k in writes:
            t = self.last_w.get(k)
            if t is not None:
                deps.add(t)
            for t in self.readers.get(k, {}).values():
                deps.add(t)
        idx = len(self.ops[eng])
        if dma:
            si = self.dma_rr
            self.dma_rr = (self.dma_rr + 1) % N_DMA_SEMS
            prev = self.dma_count[si]
            if prev > 0:
                deps.add(("dma", si, prev))
            self.dma_count[si] = prev + 1
            tok = ("dma", si, prev + 1)
            rkey = ("dma", si)
        else:
            tok = ("eng", eng, idx)
            rkey = eng
            if STRICT_SAME_ENGINE:
                deps = {t for t in deps if not (t[0] == "eng" and t[1] == eng) or eng != "pe"}
            else:
                deps = {t for t in deps if not (t[0] == "eng" and t[1] == eng)
                        or (t in raw and eng != "pe" and not fast and idx - t[2] <= 8)}
        self.ops[eng].append(dict(fn=fn, deps=deps, signal=False, dma=(tok if dma else None)))
        for k in writes:
            self.last_w[k] = tok
            self.readers[k] = {}
        for k in reads:
            if k in writes:
                continue
            self.readers.setdefault(k, {})[rkey] = tok
        return tok

    def barrier(self):
        toks = set()
        for e in self.ENGS:
            j = len(self.ops[e]) - 1
            while j >= 0 and (self.ops[e][j]["fn"] is None or self.ops[e][j]["dma"] is not None):
                j -= 1
            if j >= 0:
                toks.add(("eng", e, j))
        for si in range(N_DMA_SEMS):
            if self.dma_count[si] > 0:
                toks.add(("dma", si, self.dma_count[si]))
        for e in self.ENGS:
            deps = {t for t in toks if not (t[0] == "eng" and t[1] == e and (e == "pe" or not STRICT_SAME_ENGINE))}
            self.ops[e].append(dict(fn=None, deps=deps, signal=False, dma=None))
        self.last_w = {}
        self.readers = {}

    def emit(self, nc, block, engsem, dmasem):
        for e in self.ENGS:
            for o in self.ops[e]:
                for t in o["deps"]:
                    if t[0] == "eng":
                        self.ops[t[1]][t[2]]["signal"] = True
        sigcount = {}
        for e in self.ENGS:
            c = 0
            lst = []
            for o in self.ops[e]:
                if o["signal"]:
                    c += 1
                lst.append(c)
            sigcount[e] = lst
        self.stats = {e: (len(self.ops[e]), sigcount[e][-1] if sigcount[e] else 0) for e in self.ENGS}

        def run(e, eng):
            waited = {}
            for o in self.ops[e]:
                need = {}
                for t in o["deps"]:
                    if t[0] == "eng":
                        key = ("eng", t[1])
                        val = sigcount[t[1]][t[2]]
                    else:
                        key = ("dma", t[1])
                        val = 16 * t[2]
                    if val > need.get(key, 0):
                        need[key] = val
                for key, val in need.items():
                    if waited.get(key, 0) >= val:
                        continue
                    waited[key] = val
                    sem = engsem[key[1]] if key[0] == "eng" else dmasem[key[1]]
                    eng.wait_ge(sem, val)
                if o["fn"] is None:
                    continue
                inst = o["fn"](eng)
                if o["dma"] is not None:
                    inst.then_inc(dmasem[o["dma"][1]], 16)
                elif o["signal"]:
                    inst.then_inc(engsem[e], 1)

        @block.tensor
        def _(eng):
            run("pe", eng)

        @block.scalar
        def _(eng):
            run("act", eng)

        @block.vector
        def _(eng):
            run("dve", eng)

        @block.gpsimd
        def _(eng):
            run("pool", eng)

        @block.sync
        def _(eng):
            run("sp", eng)


DT_SIZE = {F32: 4, BF16: 2, I32: 4}


def build_nc(debug=(), stop_after=None):
    nc = bass.Bass("TRN2", target_bir_lowering=False)

    def din(name, shape, dt=F32):
        return nc.dram_tensor(name, list(shape), dt, kind="ExternalInput").ap()

    x_d = din("x", [S, D])
    pos_d = din("positions", [128, NT], I32)
    g1_d = din("norm1_g", [128, 8])
    w_in_d = din("w_in", [D, D_IN])
    convw_d = din("conv_w", [128, 48])
    invf_d = din("inv_freq", [1, 32])
    alog_d = din("a_log", [1, 8])
    dtb_d = din("dt_bias", [1, 8])
    ang_d = din("a_norm_g", [1, 64])
    bgate_d = din("b_gate", [128, 16])
    g2_d = din("norm2_g", [128, 8])
    fng_d = din("final_norm_g", [1, D])
    wpa_d = din("w_proj_a", [512, D])
    wpb_d = din("w_proj_b", [512, D])
    wout_d = din("w_out", [D, D])
    wr_d = din("w_router", [D, 20])
    br_d = din("b_router", [1, 20])
    w1_d = din("w_exp_gate", [16, D, 256])
    w3_d = din("w_exp_up", [16, D, 256])
    w2_d = din("w_exp_down", [16, 256, D])
    out_d = nc.dram_tensor("out", [S, D], F32, kind="ExternalOutput").ap()
    dbg = {}
    for name, shape, dt in debug:
        dbg[name] = nc.dram_tensor("dbg_" + name, list(shape), dt, kind="ExternalOutput").ap()
    w_in_v = w_in_d.rearrange("(k p) c -> p k c", p=128)

    sch = Sched()
    op = sch.op
    es = ExitStack()
    with es:
        ARENA_BYTES = 207 * 1024
        arena = es.enter_context(nc.sbuf_tensor("arena", [128, ARENA_BYTES // 4], F32))

        def view(off, shape, dt):
            n = 1
            for s_ in shape[1:]:
                n *= s_
            size = n * DT_SIZE[dt]
            assert off % 4 == 0 and size % 4 == 0 and off + size <= ARENA_BYTES, (off, size)
            ap = arena[:, off // 4:(off + size) // 4]
            if dt != F32:
                ap = ap.bitcast(dt)
            if len(shape) == 3:
                ap = ap.rearrange("p (a b) -> p a b", a=shape[1])
            elif len(shape) == 4:
                ap = ap.rearrange("p (a b c) -> p a b c", a=shape[1], b=shape[2])
            return ap

        class Alloc:
            def __init__(self, base, limit):
                self.off = base
                self.limit = limit

            def __call__(self, shape, dt):
                n = 1
                for s_ in shape[1:]:
                    n *= s_
                size = (n * DT_SIZE[dt] + 63) // 64 * 64
                self.off = (self.off + 63) // 64 * 64
                v = view(self.off, shape, dt)
                self.off += size
                assert self.off <= self.limit, (self.off, self.limit)
                return v

        pbank = [es.enter_context(nc.psum_tensor("pb%d" % i, [128, 512], F32)) for i in range(8)]
        engsem = {e: es.enter_context(nc.semaphore("sem_" + e)) for e in Sched.ENGS}
        dmasem = [es.enter_context(nc.semaphore("dsem%d" % i)) for i in range(N_DMA_SEMS)]

        def PB(i):
            return pbank[i][:]

        def PBb(i):
            return pbank[i][:].bitcast(BF16)

        def body():
            K = 1024
            ca = Alloc(0, 9 * K)
            ident_f = ca([128, 128], F32)
            ident_b = ca([128, 128], BF16)
            ucs_f = ca([128, 128], F32)
            mc0_f = ca([128, 128], F32)
            mc1_f = ca([128, 128], F32)
            maskneg_f = ca([128, 128], F32)
            strict_b = ca([128, 128], BF16)
            g1col = ca([128, 8], F32)
            epsc = ca([128, 1], F32)
            cw = ca([128, 48], F32)
            invf = ca([128, 32], F32)
            dtb = ca([128, 8], F32)
            negA = ca([128, 8], F32)
            angb = ca([128, 64], F32)
            posi = ca([128, NT], I32)
            posf = ca([128, NT], F32)
            cs = ca([128, NT, 64], F32)
            assert ca.off <= 9 * K, ca.off

            op("pool", lambda e: e.memset(ident_f, 1.0), writes=["ident_f"])
            op("pool", lambda e: e.affine_select(out=ident_f, in_=ident_f, pattern=[[-1, 128]], compare_op=ALU.is_equal,
                                                 fill=0.0, base=0, channel_multiplier=1), writes=["ident_f"])
            op("pool", lambda e: e.tensor_copy(out=ident_b, in_=ident_f), reads=["ident_f"], writes=["ident_b"])
            op("pool", lambda e: e.memset(ucs_f, 1.0), writes=["ucs_f"])
            op("pool", lambda e: e.affine_select(out=ucs_f, in_=ucs_f, pattern=[[1, 128]], compare_op=ALU.is_ge,
                                                 fill=0.0, base=0, channel_multiplier=-1), writes=["ucs_f"])
            op("pool", lambda e: e.memset(ucs_f[0:64, 64:128], 0.0), writes=["ucs_f"])
            op("pool", lambda e: e.memset(mc0_f, 0.0), writes=["mc0_f"])
            op("pool", lambda e: e.memset(mc0_f[0:64, :], 1.0), writes=["mc0_f"])
            op("pool", lambda e: e.memset(mc1_f, 0.0), writes=["mc1_f"])
            op("pool", lambda e: e.memset(mc1_f[64:128, :], 1.0), writes=["mc1_f"])
            op("pool", lambda e: e.memset(maskneg_f, 0.0), writes=["maskneg_f"])
            op("pool", lambda e: e.affine_select(out=maskneg_f, in_=maskneg_f, pattern=[[-1, 128]], compare_op=ALU.is_ge,
                                                 fill=NEG, base=0, channel_multiplier=1), writes=["maskneg_f"])
            op("pool", lambda e: e.memset(maskneg_f[64:128, 0:64], NEG), writes=["maskneg_f"])
            op("pool", lambda e: e.memset(strict_b, 1.0), writes=["strict_b"])
            op("pool", lambda e: e.affine_select(out=strict_b, in_=strict_b, pattern=[[-1, 128]], compare_op=ALU.is_gt,
                                                 fill=0.0, base=0, channel_multiplier=1), writes=["strict_b"])
            op("pool", lambda e: e.memset(strict_b[64:128, 0:64], 0.0), writes=["strict_b"])
            op("dve", lambda e: e.memset(epsc, EPS), writes=["epsc"])
            op("sp", lambda e: e.dma_start(out=g1col, in_=g1_d), writes=["g1col"], dma=True)
            op("sp", lambda e: e.dma_start(out=cw, in_=convw_d), writes=["cw"], dma=True)
            op("sp", lambda e: e.dma_start(out=invf, in_=invf_d.partition_broadcast(128)), writes=["invf"], dma=True)
            op("sp", lambda e: e.dma_start(out=dtb, in_=dtb_d.partition_broadcast(128)), writes=["dtb"], dma=True)
            op("sp", lambda e: e.dma_start(out=negA, in_=alog_d.partition_broadcast(128)), writes=["negA"], dma=True)
            op("sp", lambda e: e.dma_start(out=angb, in_=ang_d.partition_broadcast(128)), writes=["angb"], dma=True)
            op("sp", lambda e: e.dma_start(out=posi, in_=pos_d), writes=["posi"], dma=True)
            op("act", lambda e: e.activation(out=negA, in_=negA, func=AF.Exp), reads=["negA"], writes=["negA"])
            op("dve", lambda e: e.tensor_scalar(out=negA, in0=negA, scalar1=-1.0, scalar2=None, op0=ALU.mult),
               reads=["negA"], writes=["negA"])

            R_A = 9 * K
            R_W = R_A + 32 * K
            R_Z = R_W + 16 * K
            R_Q = R_Z + 50 * K
            R_S = R_Q + 48 * K
            hT = view(R_A, [128, 8, S], BF16)
            wstage = view(R_W, [128, 8, 256], F32)
            wbf = [view(R_W + 8 * K + i * 4 * K, [128, 8, 256], BF16) for i in range(2)]
            zqkvT = view(R_Z, [128, 12, S + 4], BF16)
            bqT = view(R_Z, [128, 4, S], BF16)
            iqT = view(R_Z + 16 * K, [128, 4, S], BF16)
            azs = view(R_Z + 32 * K, [128, NT, 512], BF16)
            qkv_tok = view(R_Q, [128, NT, 1536], BF16)
            sa = Alloc(R_S, ARENA_BYTES)
            kz = [[sa([128, S], BF16) for _ in range(2)] for _ in range(2)]
            ikT2 = sa([128, S], BF16)
            bv_tok = sa([128, NT, 130], BF16)
            ab_tok = sa([128, NT, 16], F32)
            iw_tok = sa([128, NT, 8], F32)
            diagw = sa([128, 48, 128], BF16)
            R_WORK = sa.off

            def rope_tables():
                wa = Alloc(R_Q, R_Q + 48 * K)
                ang = wa([128, NT, 32], F32)
                tmp = wa([128, NT, 32], F32)
                ki = wa([128, NT, 32], I32)
                op("dve", lambda e: e.tensor_copy(out=posf, in_=posi), reads=["posi"], writes=["posf"])
                op("dve", lambda e: e.tensor_tensor(out=ang, in0=posf.unsqueeze(2).to_broadcast([128, NT, 32]),
                                                    in1=invf.unsqueeze(1).to_broadcast([128, NT, 32]), op=ALU.mult),
                   reads=["posf", "invf"], writes=["ang"])
                for which, shift in ((1, 0.0), (0, math.pi / 2.0)):
                    dst = cs[:, :, which * 32:(which + 1) * 32]
                    op("dve", lambda e, shift=shift: e.tensor_scalar(out=tmp, in0=ang, scalar1=shift, scalar2=None, op0=ALU.add),
                       reads=["ang"], writes=["rt_tmp"])
                    op("dve", lambda e: e.tensor_scalar(out=ki, in0=tmp, scalar1=1.0 / TWO_PI, scalar2=None, op0=ALU.mult),
                       reads=["rt_tmp"], writes=["rt_ki"])
                    op("dve", lambda e, dst=dst: e.tensor_copy(out=dst, in_=ki), reads=["rt_ki"], writes=["cs"])
                    op("dve", lambda e, dst=dst: e.scalar_tensor_tensor(out=dst, in0=dst, scalar=-TWO_PI, in1=tmp,
                                                                       op0=ALU.mult, op1=ALU.add),
                       reads=["cs", "rt_tmp"], writes=["cs"])
                    op("dve", lambda e, dst=dst: e.tensor_scalar(out=dst, in0=dst, scalar1=math.pi, scalar2=-math.pi,
                                                                op0=ALU.min, op1=ALU.max), reads=["cs"], writes=["cs"])
                    op("act", lambda e, dst=dst: e.activation(out=dst, in_=dst, func=AF.Sin), reads=["cs"], writes=["cs"])

            rope_tables()

            def phase1(hT_dst, keyp):
                wa = Alloc(R_Q + 16 * K, R_Q + 48 * K)
                xt = [wa([128, D], F32) for _ in range(2)]
                hb = [wa([128, D], BF16) for _ in range(2)]
                junk = wa([128, D], BF16)
                ss1 = wa([128, NT], F32)
                rstd1 = wa([128, NT], F32)
                op("dve", lambda e: e.memset(ss1, 0.0), writes=[keyp + "ss1"])
                for tt in range(NT):
                    b = tt % 2
                    op("sp", lambda e, tt=tt, b=b: e.dma_start(out=xt[b], in_=x_d[tt * 128:(tt + 1) * 128, :]),
                       writes=[(keyp + "xt", b)], dma=True)
                    op("act", lambda e, tt=tt, b=b: e.activation(out=junk, in_=xt[b], func=AF.Square,
                                                                 accum_out=ss1[:, tt:tt + 1]),
                       reads=[(keyp + "xt", b), keyp + "ss1"], writes=[keyp + "junk", (keyp + "ss1", tt)])
                    op("act", lambda e, tt=tt: e.activation(out=rstd1[:, tt:tt + 1], in_=ss1[:, tt:tt + 1], func=AF.Sqrt,
                                                            bias=epsc, scale=1.0 / D),
                       reads=[(keyp + "ss1", tt), "epsc"], writes=[(keyp + "rstd1", tt)])
                    op("dve", lambda e, tt=tt: e.reciprocal(out=rstd1[:, tt:tt + 1], in_=rstd1[:, tt:tt + 1]),
                       reads=[(keyp + "rstd1", tt)], writes=[(keyp + "rstd1", tt)])
                    op("dve", lambda e, tt=tt, b=b: e.tensor_scalar(out=hb[b], in0=xt[b], scalar1=rstd1[:, tt:tt + 1],
                                                                    scalar2=None, op0=ALU.mult),
                       reads=[(keyp + "xt", b), (keyp + "rstd1", tt)], writes=[(keyp + "hb", b)])
                    pbv = PBb(tt % 2)
                    for k in range(8):
                        op("pe", lambda e, k=k, b=b, pbv=pbv: e.transpose(out=pbv[:, k * 128:(k + 1) * 128],
                                                                          in_=hb[b][:, k * 128:(k + 1) * 128], identity=ident_b),
                           reads=[(keyp + "hb", b), "ident_b"], writes=[("pb", tt % 2)])
                    op("act", lambda e, tt=tt, pbv=pbv: e.copy(out=hT_dst[:, :, tt * 128:(tt + 1) * 128],
                                                               in_=pbv.rearrange("p (k t) -> p k t", k=8)),
                       reads=[("pb", tt % 2)], writes=[("hT", tt)])

            phase1(hT, "p1")
            if stop_after == "p1":
                sch.barrier()
                return
            ALL_HT = [("hT", tt) for tt in range(NT)]

            wchunk_i = [0]

            def load_w(ranges):
                i = wchunk_i[0]
                wchunk_i[0] += 1
                b = i % 2
                off = 0
                for (c0, w) in ranges:
                    op("sp", lambda e, c0=c0, w=w, off=off: e.dma_start(out=wstage[:, :, off:off + w],
                                                                        in_=w_in_v[:, :, c0:c0 + w]),
                       writes=["wstage"], dma=True)
                    off += w
                tot = off
                op("pool", lambda e, b=b, tot=tot: e.tensor_tensor(out=wbf[b][:, :, 0:tot], in0=wstage[:, :, 0:tot],
                                                                   in1=g1col.unsqueeze(2).to_broadcast([128, 8, tot]),
                                                                   op=ALU.mult),
                   reads=["wstage", "g1col"], writes=[("wbf", b)])
                return wbf[b], ("wbf", b), tot

            for ci in range(48):
                op("pool", lambda e, ci=ci: e.tensor_scalar(out=diagw[:, ci, :], in0=ident_f, scalar1=cw[:, ci:ci + 1],
                                                            scalar2=None, op0=ALU.mult),
                   reads=["ident_f", "cw"], writes=[("diagw", ci)])
            op("pool", lambda e: e.memset(zqkvT[:, :, 0:4], 0.0), writes=["zpad"])

            cva = Alloc(R_WORK, ARENA_BYTES)
            convtmp = [cva([128, 512], BF16) for _ in range(2)]
            evq = [0]

            def evac_copy(out, in_, reads, writes):
                evq[0] += 1
                if evq[0] % 2 == 0:
                    op("act", lambda e: e.copy(out=out, in_=in_), reads=reads, writes=writes)
                else:
                    op("dve", lambda e: e.tensor_copy(out=out, in_=in_), reads=reads, writes=writes)

            pbi = [0]

            def g1_proj(c, wt, wkey, ct):
                for tb in range(4):
                    bk = 2 + (pbi[0] % 2)
                    pbi[0] += 1
                    for k in range(8):
                        op("pe", lambda e, k=k, tb=tb, bk=bk: e.matmul(
                            PB(bk), lhsT=wt[:, k, ct * 128:(ct + 1) * 128], rhs=hT[:, k, tb * 512:(tb + 1) * 512],
                            start=(k == 0), stop=(k == 7)),
                           reads=[wkey] + ALL_HT[tb * 4:tb * 4 + 4], writes=[("pb", bk)])
                    evac_copy(zqkvT[:, c, 4 + tb * 512:4 + (tb + 1) * 512], PB(bk), [("pb", bk)], [("zq", c, tb)])

            def g1_conv(c):
                for tb in range(4):
                    bk = 4 + (tb % 2)
                    for j in range(4):
                        op("pe", lambda e, tb=tb, j=j, bk=bk: e.matmul(
                            PB(bk), lhsT=diagw[:, c * 4 + j, :], rhs=zqkvT[:, c, tb * 512 + j + 1:tb * 512 + j + 1 + 512],
                            start=(j == 0), stop=(j == 3)),
                           reads=[("diagw", c * 4 + j), ("zq", c, tb), "zpad"] + ([("zq", c, tb - 1)] if tb > 0 else []),
                           writes=[("pb", bk)])
                    ctb = tb % 2
                    op("act", lambda e, bk=bk, ctb=ctb: e.activation(out=convtmp[ctb], in_=PB(bk), func=AF.Silu),
                       reads=[("pb", bk)], writes=[("convtmp", ctb)])
                    tbk = 6 + (tb % 2)
                    for q in range(4):
                        op("pe", lambda e, q=q, ctb=ctb, tbk=tbk: e.transpose(out=PBb(tbk)[:, q * 128:(q + 1) * 128],
                                                                              in_=convtmp[ctb][:, q * 128:(q + 1) * 128],
                                                                              identity=ident_b),
                           reads=[("convtmp", ctb), "ident_b"], writes=[("pb", tbk)])
                    op("dve", lambda e, tb=tb, tbk=tbk: e.tensor_copy(
                        out=qkv_tok[:, tb * 4:(tb + 1) * 4, c * 128:(c + 1) * 128],
                        in_=PBb(tbk)[:, 0:512].rearrange("p (q t) -> p q t", q=4)),
                       reads=[("pb", tbk)], writes=[("qkv_tok", tb * 4 + q, c) for q in range(4)])

            prev_c = None
            nxt_w = load_w([(0, 256)])
            for chunk in range(6):
                wt, wkey, _ = nxt_w
                for ct in range(2):
                    c = chunk * 2 + ct
                    g1_proj(c, wt, wkey, ct)
                    if ct == 0:
                        nxt_w = load_w([((chunk + 1) * 256, 256)]) if chunk + 1 < 6 else load_w([(C_AZ, 256)])
                    if prev_c is not None:
                        g1_conv(prev_c)
                    prev_c = c
            g1_conv(prev_c)
            pending_w = [nxt_w]

            if "qkv_tok" in dbg:
                op("sp", lambda e: e.dma_start(out=dbg["qkv_tok"].rearrange("(t p) c -> p t c", p=128), in_=qkv_tok),
                   reads=[("qkv_tok", tt, c) for tt in range(NT) for c in range(12)], writes=["dbg_qkv_tok"], dma=True)
                op("sp", None, reads=["dbg_qkv_tok"])
            sch.barrier()
            if stop_after == "g1":
                return

            rwa = Alloc(cva.off, ARENA_BYTES)
            zr = [rwa([128, 256], F32) for _ in range(2)]
            rt = [rwa([128, 4, 32], F32) for _ in range(4)]
            roped = [rwa([128, 256], BF16) for _ in range(2)]
            op("pool", lambda e: e.memset(bv_tok, 1.0), writes=["bv_ones"])
            for a_ in range(2):
                for b_ in range(2):
                    op("pool", lambda e, a_=a_, b_=b_: e.memset(kz[a_][b_], 0.0), writes=["kz0"])

            def rope_ops(src, nh, dst_views, tt, rkey, wkeys, b):
                sv = src.rearrange("p (h d) -> p h d", h=nh)
                x1 = sv[:, :, 0:32]
                x2 = sv[:, :, 32:64]
                cc = cs[:, tt, 0:32].unsqueeze(1).to_broadcast([128, nh, 32])
                sn = cs[:, tt, 32:64].unsqueeze(1).to_broadcast([128, nh, 32])
                t = [r[:, 0:nh, :] for r in rt]
                op("dve", lambda e: e.tensor_tensor(out=t[0], in0=x1, in1=cc, op=ALU.mult), reads=[rkey, "cs"], writes=[("rt", 0)])
                op("pool", lambda e: e.tensor_tensor(out=t[1], in0=x2, in1=sn, op=ALU.mult), reads=[rkey, "cs"], writes=[("rt", 1)])
                op("pool", lambda e: e.tensor_tensor(out=t[2], in0=x2, in1=cc, op=ALU.mult), reads=[rkey, "cs"], writes=[("rt", 2)])
                op("dve", lambda e: e.tensor_tensor(out=t[3], in0=x1, in1=sn, op=ALU.mult), reads=[rkey, "cs"], writes=[("rt", 3)])
                for i, dv in enumerate(dst_views):
                    eng = "dve" if i % 2 == 0 else "pool"
                    op(eng, lambda e, dv=dv: e.tensor_tensor(out=dv[:, :, 0:32], in0=t[0], in1=t[1], op=ALU.subtract),
                       reads=[("rt", 0), ("rt", 1)], writes=wkeys)
                    op(eng, lambda e, dv=dv: e.tensor_tensor(out=dv[:, :, 32:64], in0=t[2], in1=t[3], op=ALU.add),
                       reads=[("rt", 2), ("rt", 3)], writes=wkeys)

            def tok_chunk(ranges, handler, sel=None, next_ranges=None):
                if pending_w[0] is not None:
                    wt, wkey, tot = pending_w[0]
                    pending_w[0] = None
                else:
                    wt, wkey, tot = load_w(ranges)
                lo, hi = (0, tot) if sel is None else sel
                pend = []
                for tt in range(NT):
                    if tt == 6 and next_ranges is not None:
                        pending_w[0] = load_w(next_ranges)
                    bk = 2 + (tt % 2)
                    for k in range(8):
                        op("pe", lambda e, k=k, tt=tt, bk=bk, wt=wt: e.matmul(
                            PB(bk)[:, 0:hi - lo], lhsT=hT[:, k, tt * 128:(tt + 1) * 128], rhs=wt[:, k, lo:hi],
                            start=(k == 0), stop=(k == 7)),
                           reads=[wkey, ("hT", tt)], writes=[("pb", bk)])
                    if tt >= 1:
                        pend.append(handler(tt - 1, 2 + ((tt - 1) % 2)))
                    if len(pend) >= 2:
                        p2 = pend.pop(0)
                        if p2 is not None:
                            p2()
                pend.append(handler(NT - 1, 2 + ((NT - 1) % 2)))
                for p2 in pend:
                    if p2 is not None:
                        p2()

            for j in range(2):
                def h_az(tt, bk, j=j):
                    op("act", lambda e: e.activation(out=azs[:, tt, j * 256:(j + 1) * 256], in_=PB(bk)[:, 0:256], func=AF.Silu),
                       reads=[("pb", bk)], writes=[("azs", tt, j)])
                tok_chunk([(C_AZ + j * 256, 256)], h_az, next_ranges=[(C_AZ + 256, 256)] if j == 0 else [(C_BQ, 256)])

            if stop_after == "u1":
                return
            for (c0, dstT, nm) in ((C_BQ, bqT, "bqT"), (C_IQ, iqT, "iqT")):
                for j in range(2):
                    def h_q(tt, bk, j=j, dstT=dstT, nm=nm):
                        b = tt % 2
                        op("act", lambda e: e.copy(out=zr[b], in_=PB(bk)[:, 0:256]), reads=[("pb", bk)], writes=[("zr", b)])
                        rope_ops(zr[b], 4, [roped[b].rearrange("p (h d) -> p h d", h=4)], tt, ("zr", b), [("roped", b)], b)
                        tbk = 6 + b

                        def part2():
                            for q in range(2):
                                op("pe", lambda e, q=q: e.transpose(out=PBb(tbk)[:, q * 128:(q + 1) * 128],
                                                                    in_=roped[b][:, q * 128:(q + 1) * 128], identity=ident_b),
                                   reads=[("roped", b), "ident_b"], writes=[("pb", tbk)])
                            op("act", lambda e: e.copy(out=dstT[:, 2 * j:2 * j + 2, tt * 128:(tt + 1) * 128],
                                                       in_=PBb(tbk)[:, 0:256].rearrange("p (q t) -> p q t", q=2)),
                               reads=[("pb", tbk)], writes=[(nm, tt, j)])
                        return part2
                    nr = [(c0 + 256, 256)] if j == 0 else ([(C_IQ, 256)] if c0 == C_BQ else [(C_BK, 256)])
                    tok_chunk([(c0 + j * 256, 256)], h_q, next_ranges=nr)

            if stop_after == "u23":
                return
            def h_kv(tt, bk):
                b = tt % 2
                op("act", lambda e: e.copy(out=zr[b], in_=PB(bk)[:, 0:256]), reads=[("pb", bk)], writes=[("zr", b)])
                rv = roped[b].rearrange("p (h d) -> p h d", h=4)
                rope_ops(zr[b][:, 0:128], 2, [rv[:, 0:2, :]], tt, ("zr", b), [("roped", b)], b)
                op("pool", lambda e: e.tensor_copy(out=rv[:, 2, :], in_=rv[:, 1, :]), reads=[("roped", b)], writes=[("roped", b)])
                op("pool", lambda e: e.tensor_copy(out=rv[:, 3, :], in_=rv[:, 0, :]), reads=[("roped", b)], writes=[("roped", b)])
                def part2():
                    tbk = 6 + b
                    for q in range(2):
                        op("pe", lambda e, q=q: e.transpose(out=PBb(tbk)[:, q * 128:(q + 1) * 128],
                                                            in_=roped[b][:, q * 128:(q + 1) * 128], identity=ident_b),
                           reads=[("roped", b), "ident_b"], writes=[("pb", tbk)])
                    ts_ = slice(tt * 128, (tt + 1) * 128)
                    op("act", lambda e: e.copy(out=kz[0][0][0:64, ts_], in_=PBb(tbk)[0:64, 0:128]), reads=[("pb", tbk), "kz0"],
                       writes=[("bkT", tt)])
                    op("act", lambda e: e.copy(out=kz[1][1][64:128, ts_], in_=PBb(tbk)[64:128, 0:128]), reads=[("pb", tbk)],
                       writes=[("bkT", tt)])
                    op("act", lambda e: e.copy(out=kz[1][0][0:64, ts_], in_=PBb(tbk)[0:64, 128:256]), reads=[("pb", tbk)],
                       writes=[("bkT", tt)])
                    op("act", lambda e: e.copy(out=kz[0][1][64:128, ts_], in_=PBb(tbk)[64:128, 128:256]), reads=[("pb", tbk)],
                       writes=[("bkT", tt)])

                op("dve", lambda e: e.tensor_copy(out=bv_tok[:, tt, 0:64], in_=zr[b][:, 128:192]), reads=[("zr", b), "bv_ones"],
                   writes=[("bv", tt)])
                op("dve", lambda e: e.tensor_copy(out=bv_tok[:, tt, 65:129], in_=zr[b][:, 192:256]), reads=[("zr", b)],
                   writes=[("bv", tt)])
                return part2
            tok_chunk([(C_BK, 256)], h_kv, next_ranges=[(C_IW + 8 - 256, 256)])

            if stop_after == "u4a":
                return
            IW_SCALE = (8 ** -0.5) * (64 ** -0.5)

            def h_small(tt, bk):
                b = tt % 2
                op("act", lambda e: e.copy(out=zr[b][:, 0:72], in_=PB(bk)[:, 0:72]), reads=[("pb", bk)], writes=[("zr", b)])
                rv = roped[b].rearrange("p (h d) -> p h d", h=4)
                rope_ops(zr[b][:, 0:64], 1, [rv[:, 0:1, :], rv[:, 1:2, :]], tt, ("zr", b), [("roped", b)], b)
                def part2():
                    tbk = 6 + b
                    op("pe", lambda e: e.transpose(out=PBb(tbk)[:, 0:128], in_=roped[b][:, 0:128], identity=ident_b),
                       reads=[("roped", b), "ident_b"], writes=[("pb", tbk)])
                    op("act", lambda e: e.copy(out=ikT2[:, tt * 128:(tt + 1) * 128], in_=PBb(tbk)[:, 0:128]),
                       reads=[("pb", tbk)], writes=[("ikT", tt)])

                op("dve", lambda e: e.tensor_scalar(out=iw_tok[:, tt, :], in0=zr[b][:, 64:72], scalar1=IW_SCALE, scalar2=None,
                                                    op0=ALU.mult), reads=[("zr", b)], writes=[("iw", tt)])
                return part2
            tok_chunk([(C_IW + 8 - 256, 256)], h_small, sel=(184, 256), next_ranges=[(C_BETA, 256)])

            def h_ab(tt, bk):
                op("act", lambda e: e.copy(out=ab_tok[:, tt, :], in_=PB(bk)[:, 0:16]), reads=[("pb", bk)], writes=[("ab", tt)])
            tok_chunk([(C_BETA, 256)], h_ab, sel=(0, 16))

            for nm, t_, shape in (("bqT", bqT, None), ("iqT", iqT, None)):
                if nm in dbg:
                    op("sp", lambda e, nm=nm, t_=t_: e.dma_start(out=dbg[nm].rearrange("(a p) t -> p a t", p=128), in_=t_),
                       reads=[(nm, tt, j) for tt in range(NT) for j in range(2)], writes=["dbg_" + nm], dma=True)
                    op("sp", None, reads=["dbg_" + nm])
            if "misc" in dbg:
                sch.barrier()
                mt = view(R_W, [128, NT, 154], F32)
                op("dve", lambda e: e.tensor_copy(out=mt[:, :, 0:16], in_=ab_tok), reads=[("ab", tt) for tt in range(NT)], writes=["mt"])
                op("dve", lambda e: e.tensor_copy(out=mt[:, :, 16:24], in_=iw_tok), reads=[("iw", tt) for tt in range(NT)], writes=["mt"])
                op("dve", lambda e: e.tensor_copy(out=mt[:, :, 24:154], in_=bv_tok), reads=[("bv", tt) for tt in range(NT)], writes=["mt"])
                op("sp", lambda e: e.dma_start(out=dbg["misc"].rearrange("(t p) c -> p t c", p=128), in_=mt), reads=["mt"],
                   writes=["dbg_misc"], dma=True)
                op("sp", None, reads=["dbg_misc"])
            sch.barrier()
            if stop_after == "p2":
                return

            class MultiAlloc:
                def __init__(self, regions):
                    self.regs = [[a, b] for a, b in regions]

                def __call__(self, shape, dt):
                    n = 1
                    for s_ in shape[1:]:
                        n *= s_
                    size = (n * DT_SIZE[dt] + 63) // 64 * 64
                    for r in self.regs:
                        r[0] = (r[0] + 63) // 64 * 64
                        if r[0] + size <= r[1]:
                            v = view(r[0], shape, dt)
                            r[0] += size
                            return v
                    raise AssertionError(("MultiAlloc out of space", shape, self.regs))

            def dump(name, ap, reads):
                if name in dbg:
                    op("sp", lambda e: e.dma_start(out=dbg[name], in_=ap), reads=reads, writes=["dbg_" + name], dma=True)
                    op("sp", None, reads=["dbg_" + name])

            o_aT = view(R_A, [128, 4, S], BF16)
            diagw_off = R_WORK - 12 * K
            ga = MultiAlloc([(R_W, R_W + 16 * K), (R_A + 16 * K, R_A + 32 * K), (diagw_off, ARENA_BYTES)])
            g_all = ga([128, NT, 8], F32)
            bet = ga([128, NT, 8], F32)
            gs = ga([128, 24], F32)
            eG = ga([128, 8], F32)
            eGlmG = ga([128, 8], F32)
            scs = [ga([128, 4], F32) for _ in range(2)]
            g_bc = ga([128, 8, 128], F32)
            sq = ga([128, 1024], F32)
            ssn = ga([128, 16], F32)
            rn = ga([128, 16], F32)
            cq = ga([128, 8], F32)
            cqd = ga([128, 8], F32)
            cbk = ga([128, 8], F32)
            ckd = ga([128, 8], F32)
            negbeta = ga([128, 8], F32)
            qn = ga([128, 512], BF16)
            qd = ga([128, 512], BF16)
            kn = ga([128, 512], BF16)
            rhsk = ga([128, 512], BF16)
            kdec = ga([128, 512], BF16)
            rhsv = ga([128, 512], BF16)
            qnT = ga([128, 4, 128], BF16)
            qdT = ga([128, 4, 128], BF16)
            knT = ga([128, 4, 128], BF16)
            Dm = ga([128, 8, 128], BF16)
            Ds = ga([128, 8, 128], BF16)
            Mm = [ga([128, 8, 128], BF16) for _ in range(2)]
            Nm = [ga([128, 8, 128], BF16) for _ in range(2)]
            Pm = [ga([128, 8, 128], BF16) for _ in range(2)]
            qkm = ga([128, 8, 128], BF16)
            qkT_sb = ga([128, 8, 128], BF16)
            u_c = ga([128, 2, 512], F32)
            w_tok = ga([128, 512], BF16)
            wT_sb = ga([128, 4, 128], BF16)
            vn_b = ga([128, 512], BF16)
            Sst = ga([128, 4, 128], F32)
            Stmp = ga([128, 4, 128], F32)
            S_bd = ga([128, 4, 128], BF16)
            bdmask = ga([128, 4, 128], BF16)
            o_c = ga([128, 512], F32)
            qkT_c1 = ga([128, 8, 64], BF16)
            kdec_c1 = ga([128, 512], BF16)
            az_c = ga([128, 512], BF16)
            ss2 = ga([128, 8], F32)
            r2 = ga([128, 8], F32)
            oa_b = ga([128, 512], BF16)

            def bc8(v):
                return v.unsqueeze(2).to_broadcast([128, 8, 64])

            ABK = [("ab", tt) for tt in range(NT)]
            op("act", lambda e: e.activation(out=bet, in_=ab_tok[:, :, 0:8], func=AF.Sigmoid), reads=["ab_all"], writes=["bet"])
            op("dve", lambda e: e.tensor_tensor(out=g_all, in0=ab_tok[:, :, 8:16], in1=dtb.unsqueeze(1).to_broadcast([128, NT, 8]),
                                                op=ALU.add), reads=["ab_all", "dtb"], writes=["g_all"])
            op("act", lambda e: e.activation(out=g_all, in_=g_all, func=AF.Exp), reads=["g_all"], writes=["g_all"])
            op("act", lambda e: e.activation(out=g_all, in_=g_all, func=AF.Ln, bias=1.0), reads=["g_all"], writes=["g_all"])
            op("dve", lambda e: e.tensor_tensor(out=g_all, in0=g_all, in1=negA.unsqueeze(1).to_broadcast([128, NT, 8]),
                                                op=ALU.mult), reads=["g_all", "negA"], writes=["g_all"])
            op("dve", lambda e: e.memset(Sst, 0.0), writes=["S"])
            op("dve", lambda e: e.memset(S_bd, 0.0), writes=["S_bd"])
            op("pool", lambda e: e.memset(bdmask, 0.0), writes=["bdmask"])
            op("pool", lambda e: e.memset(bdmask[0:64, :, 0:64], 1.0), writes=["bdmask"])
            op("pool", lambda e: e.memset(bdmask[64:128, :, 64:128], 1.0), writes=["bdmask"])
            if "g" in dbg:
                op("sp", lambda e: e.dma_start(out=dbg["g"].rearrange("(t p) c -> p t c", p=128), in_=g_all), reads=["g_all"],
                   writes=["dbg_g"], dma=True)
                op("sp", None, reads=["dbg_g"])

            if stop_after == "gdn_pre":
                return
            for tt in range(NT):
                op("pe", lambda e, tt=tt: e.matmul(PB(0)[:, 0:8], lhsT=ucs_f, rhs=g_all[:, tt, :], start=True, stop=True),
                   reads=["g_all"], writes=[("pb", 0)])
                op("pe", lambda e, tt=tt: e.matmul(PB(0)[:, 8:16], lhsT=mc0_f, rhs=g_all[:, tt, :], start=True, stop=True),
                   reads=["g_all"], writes=[("pb", 0)])
                op("pe", lambda e, tt=tt: e.matmul(PB(0)[:, 16:24], lhsT=mc1_f, rhs=g_all[:, tt, :], start=True, stop=True),
                   reads=["g_all"], writes=[("pb", 0)])
                op("act", lambda e: e.copy(out=gs, in_=PB(0)[:, 0:24]), reads=[("pb", 0)], writes=["gs"])
                op("act", lambda e: e.activation(out=eG, in_=gs[:, 0:8], func=AF.Exp), reads=["gs"], writes=["eG"])
                op("dve", lambda e: e.tensor_tensor(out=eGlmG[0:64, :], in0=gs[0:64, 8:16], in1=gs[0:64, 0:8], op=ALU.subtract),
                   reads=["gs"], writes=["eGlmG"])
                op("dve", lambda e: e.tensor_tensor(out=eGlmG[64:128, :], in0=gs[64:128, 16:24], in1=gs[64:128, 0:8],
                                                    op=ALU.subtract), reads=["gs"], writes=["eGlmG"])
                op("act", lambda e: e.activation(out=eGlmG, in_=eGlmG, func=AF.Exp), reads=["eGlmG"], writes=["eGlmG"])
                for hf in range(2):
                    c0 = 8 + 8 * hf
                    op("act", lambda e, hf=hf, c0=c0: e.activation(out=scs[hf][0:64, :], in_=gs[0:64, c0:c0 + 8:2], func=AF.Exp),
                       reads=["gs"], writes=[("scs", hf)])
                    op("act", lambda e, hf=hf, c0=c0: e.activation(out=scs[hf][64:128, :], in_=gs[64:128, c0 + 1:c0 + 8:2],
                                                                   func=AF.Exp), reads=["gs"], writes=[("scs", hf)])
                op("dve", lambda e, tt=tt: e.tensor_scalar(out=g_bc, in0=g_all[:, tt, :].unsqueeze(2).to_broadcast([128, 8, 128]),
                                                           scalar1=-1.0, scalar2=None, op0=ALU.mult),
                   reads=["g_all"], writes=["g_bc"])
                if stop_after == "gdn_a":
                    return
                QK = [("qkv_tok", tt, c) for c in range(8)]
                VV = [("qkv_tok", tt, c) for c in range(8, 12)]
                op("dve", lambda e, tt=tt: e.tensor_tensor(out=sq, in0=qkv_tok[:, tt, 0:1024], in1=qkv_tok[:, tt, 0:1024],
                                                           op=ALU.mult), reads=["qkv_all"], writes=["sq"])
                op("dve", lambda e: e.tensor_reduce(out=ssn, in_=sq.rearrange("p (h d) -> p h d", h=16), axis=AX.X, op=ALU.add),
                   reads=["sq"], writes=["ssn"])
                op("act", lambda e: e.activation(out=rn, in_=ssn, func=AF.Ln, bias=epsc, scale=1.0), reads=["ssn", "epsc"],
                   writes=["rn"])
                op("act", lambda e: e.activation(out=rn, in_=rn, func=AF.Exp, scale=-0.5), reads=["rn"], writes=["rn"])
                op("dve", lambda e: e.tensor_scalar(out=cq, in0=rn[:, 0:8], scalar1=0.125, scalar2=None, op0=ALU.mult),
                   reads=["rn"], writes=["cq"])
                op("dve", lambda e: e.tensor_tensor(out=cqd, in0=cq, in1=eG, op=ALU.mult), reads=["cq", "eG"], writes=["cqd"])
                op("dve", lambda e, tt=tt: e.tensor_tensor(out=cbk, in0=rn[:, 8:16], in1=bet[:, tt, :], op=ALU.mult),
                   reads=["rn", "bet"], writes=["cbk"])
                op("dve", lambda e: e.tensor_tensor(out=cbk, in0=cbk, in1=eG, op=ALU.mult), reads=["cbk", "eG"], writes=["cbk"])
                op("dve", lambda e: e.tensor_tensor(out=ckd, in0=rn[:, 8:16], in1=eGlmG, op=ALU.mult), reads=["rn", "eGlmG"],
                   writes=["ckd"])
                op("dve", lambda e, tt=tt: e.tensor_scalar(out=negbeta, in0=bet[:, tt, :], scalar1=-1.0, scalar2=None,
                                                           op0=ALU.mult), reads=["bet"], writes=["negbeta"])
                qv = qkv_tok[:, tt, 0:512].rearrange("p (h d) -> p h d", h=8)
                kv = qkv_tok[:, tt, 512:1024].rearrange("p (h d) -> p h d", h=8)
                vv = qkv_tok[:, tt, 1024:1536].rearrange("p (h d) -> p h d", h=8)

                def v3(t_):
                    return t_.rearrange("p (h d) -> p h d", h=8)
                op("dve", lambda e, qv=qv: e.tensor_tensor(out=v3(qn), in0=qv, in1=bc8(cq), op=ALU.mult),
                   reads=["qkv_all", "cq"], writes=["qn"])
                op("dve", lambda e, qv=qv: e.tensor_tensor(out=v3(qd), in0=qv, in1=bc8(cqd), op=ALU.mult),
                   reads=["qkv_all", "cqd"], writes=["qd"])
                op("dve", lambda e, kv=kv: e.tensor_tensor(out=v3(kn), in0=kv, in1=bc8(rn[:, 8:16]), op=ALU.mult),
                   reads=["qkv_all", "rn"], writes=["kn"])
                op("dve", lambda e, kv=kv: e.tensor_tensor(out=v3(rhsk), in0=kv, in1=bc8(cbk), op=ALU.mult),
                   reads=["qkv_all", "cbk"], writes=["rhsk"])
                op("dve", lambda e, kv=kv: e.tensor_tensor(out=v3(kdec), in0=kv, in1=bc8(ckd), op=ALU.mult),
                   reads=["qkv_all", "ckd"], writes=["kdec"])
                op("dve", lambda e, vv=vv, tt=tt: e.tensor_tensor(out=v3(rhsv), in0=vv, in1=bc8(bet[:, tt, :]), op=ALU.mult),
                   reads=["qkv_all", "bet"], writes=["rhsv"])
                if stop_after == "gdn_b":
                    return
                for (src, skey, dst, dkey, bank, coff, eng) in ((qn, "qn", qnT, "qnT", 6, 0, "act"), (qd, "qd", qdT, "qdT", 7, 0, "dve"),
                                                                (kn, "kn", knT, "knT", 0, 0, "act")):
                    for q in range(4):
                        op("pe", lambda e, src=src, bank=bank, coff=coff, q=q: e.transpose(
                            out=PBb(bank)[:, coff + q * 128:coff + (q + 1) * 128], in_=src[:, q * 128:(q + 1) * 128], identity=ident_b),
                           reads=[skey, "ident_b"], writes=[("pb", bank)])
                    if eng == "act":
                        op("act", lambda e, dst=dst, bank=bank, coff=coff: e.copy(
                            out=dst, in_=PBb(bank)[:, coff:coff + 512].rearrange("p (q t) -> p q t", q=4)),
                           reads=[("pb", bank)], writes=[dkey])
                    else:
                        op("dve", lambda e, dst=dst, bank=bank, coff=coff: e.tensor_copy(
                            out=dst, in_=PBb(bank)[:, coff:coff + 512].rearrange("p (q t) -> p q t", q=4)),
                           reads=[("pb", bank)], writes=[dkey])
                    if stop_after == "gdn_c_" + skey:
                        return
                if tt == 0:
                    dump("qn0", qn, ["qn"]); dump("kn0", kn, ["kn"]); dump("rhsv0", rhsv, ["rhsv"]); dump("rhsk0", rhsk, ["rhsk"])
                    dump("kdec0", kdec, ["kdec"]); dump("qd0", qd, ["qd"]); dump("gs0", gs, ["gs"])
                    dump("knT0", knT.rearrange("p a t -> p (a t)"), ["knT"])
                    dump("rn0", rn, ["rn"]); dump("cq0", cq, ["cq"]); dump("cqd0", cqd, ["cqd"]); dump("cbk0", cbk, ["cbk"])
                    dump("ckd0", ckd, ["ckd"]); dump("eG0", eG, ["eG"]); dump("ssn0", ssn, ["ssn"])
                if stop_after == "gdn_c":
                    return
                def group_gen(hg, bA, bB, bC, bT):
                    hs_list = list(range(4))
                    grp = slice(4 * hg, 4 * hg + 4)
                    for hs in hs_list:
                        h = 4 * hg + hs
                        hp, par = h // 2, h % 2
                        rows = slice(par * 64, par * 64 + 64)
                        cs_ = slice(hs * 128, hs * 128 + 128)
                        op("pe", lambda e, hp=hp, rows=rows, cs_=cs_, par=par: e.matmul(
                            PB(bA)[:, cs_], lhsT=knT[rows, hp, :], rhs=knT[rows, hp, :], start=True, stop=True,
                            tile_position=(par * 64, 0)), reads=["knT"], writes=[("pb", bA)])
                        op("pe", lambda e, hp=hp, rows=rows, cs_=cs_, par=par: e.matmul(
                            PB(bB)[:, cs_], lhsT=qnT[rows, hp, :], rhs=knT[rows, hp, :], start=True, stop=True,
                            tile_position=(par * 64, 0)), reads=["knT", "qnT"], writes=[("pb", bB)])
                        op("pe", lambda e, h=h, cs_=cs_: e.matmul(PB(bC)[:, cs_], lhsT=g_bc[:, h, :], rhs=ucs_f, start=True, stop=False),
                           reads=["g_bc", "ucs_f"], writes=[("pb", bC)])
                        op("pe", lambda e, cs_=cs_: e.matmul(PB(bC)[:, cs_], lhsT=ident_f, rhs=maskneg_f, start=False, stop=True),
                           reads=["ident_f", "maskneg_f"], writes=[("pb", bC)])
                    yield
                    for hs in hs_list:
                        h = 4 * hg + hs
                        cs_ = slice(hs * 128, hs * 128 + 128)
                        op("act", lambda e, h=h, cs_=cs_: e.activation(out=Dm[:, h, :], in_=PB(bC)[:, cs_], func=AF.Exp,
                                                                       bias=gs[:, h:h + 1], scale=1.0),
                           reads=[("pb", bC), "gs"], writes=[("Dm", h)])
                        op("dve", lambda e, h=h: e.tensor_tensor(out=Ds[:, h, :], in0=Dm[:, h, :], in1=strict_b, op=ALU.mult),
                           reads=[("Dm", h), "strict_b"], writes=[("Ds", h)])
                        op("dve", lambda e, h=h, cs_=cs_: e.scalar_tensor_tensor(out=Mm[0][:, h, :], in0=PB(bA)[:, cs_],
                                                                                 scalar=negbeta[:, h:h + 1], in1=Ds[:, h, :],
                                                                                 op0=ALU.mult, op1=ALU.mult),
                           reads=[("pb", bA), "negbeta", ("Ds", h)], writes=[("M", 0, hg)])
                        op("dve", lambda e, h=h, cs_=cs_: e.tensor_tensor(out=qkm[:, h, :], in0=PB(bB)[:, cs_], in1=Dm[:, h, :],
                                                                          op=ALU.mult),
                           reads=[("pb", bB), ("Dm", h)], writes=[("qkm", hg)])
                    yield
                    for hs in hs_list:
                        h = 4 * hg + hs
                        cs_ = slice(hs * 128, hs * 128 + 128)
                        op("pe", lambda e, h=h, cs_=cs_: e.transpose(out=PBb(bT)[:, cs_], in_=Mm[0][:, h, :], identity=ident_b),
                           reads=[("M", 0, hg), "ident_b"], writes=[("pb", bT)])
                    op("act", lambda e: e.copy(out=Nm[0][:, grp, :], in_=PBb(bT)[:, 0:512].rearrange("p (q t) -> p q t", q=4)),
                       reads=[("pb", bT)], writes=[("N", 0, hg)])
                    op("dve", lambda e: e.tensor_tensor(out=Pm[0][:, grp, :], in0=Nm[0][:, grp, :],
                                                        in1=ident_b.unsqueeze(1).to_broadcast([128, 4, 128]), op=ALU.add),
                       reads=[("N", 0, hg), "ident_b"], writes=[("P", 0, hg)])
                    yield
                    for hs in hs_list:
                        h = 4 * hg + hs
                        cs2 = slice(hs * 128, hs * 128 + 128)
                        op("pe", lambda e, h=h, cs2=cs2: e.transpose(out=PBb(bT)[:, cs2], in_=qkm[:, h, :], identity=ident_b),
                           reads=[("qkm", hg), "ident_b"], writes=[("pb", bT)])
                    op("dve", lambda e: e.tensor_copy(out=qkT_sb[:, grp, :], in_=PBb(bT)[:, 0:512].rearrange("p (q t) -> p q t", q=4)),
                       reads=[("pb", bT)], writes=[("qkT", hg)])
                    yield
                    for lv in range(1, 6):
                        cur, nxt = (lv - 1) % 2, lv % 2
                        for hs in hs_list:
                            h = 4 * hg + hs
                            cs_ = slice(hs * 128, hs * 128 + 128)
                            op("pe", lambda e, h=h, cs_=cs_, cur=cur: e.matmul(PB(bA)[:, cs_], lhsT=Nm[cur][:, h, :], rhs=Mm[cur][:, h, :],
                                                                               start=True, stop=True),
                               reads=[("N", cur, hg), ("M", cur, hg)], writes=[("pb", bA)])
                        if lv < 5:
                            for hs in hs_list:
                                h = 4 * hg + hs
                                cs_ = slice(hs * 128, hs * 128 + 128)
                                op("pe", lambda e, h=h, cs_=cs_, cur=cur: e.matmul(PB(bB)[:, cs_], lhsT=Mm[cur][:, h, :],
                                                                                   rhs=Nm[cur][:, h, :], start=True, stop=True),
                                   reads=[("N", cur, hg), ("M", cur, hg)], writes=[("pb", bB)])
                        yield
                        op("act", lambda e, nxt=nxt: e.copy(out=Mm[nxt][:, grp, :], in_=PB(bA).rearrange("p (q t) -> p q t", q=4)),
                           reads=[("pb", bA)], writes=[("M", nxt, hg)])
                        if lv < 5:
                            op("dve", lambda e, nxt=nxt: e.tensor_copy(out=Nm[nxt][:, grp, :],
                                                                       in_=PB(bB).rearrange("p (q t) -> p q t", q=4)),
                               reads=[("pb", bB)], writes=[("N", nxt, hg)])
                        for hs in hs_list:
                            h = 4 * hg + hs
                            cs_ = slice(hs * 128, hs * 128 + 128)
                            op("pe", lambda e, h=h, cs_=cs_, cur=cur, nxt=nxt: e.matmul(PB(bC)[:, cs_], lhsT=Mm[nxt][:, h, :],
                                                                                        rhs=Pm[cur][:, h, :], start=True, stop=True),
                               reads=[("M", nxt, hg), ("P", cur, hg)], writes=[("pb", bC)])
                        yield
                        op("dve", lambda e, cur=cur, nxt=nxt: e.tensor_tensor(
                            out=Pm[nxt][:, grp, :], in0=Pm[cur][:, grp, :], in1=PB(bC).rearrange("p (q t) -> p q t", q=4), op=ALU.add),
                           reads=[("pb", bC), ("P", cur, hg)], writes=[("P", nxt, hg)])

                gens = [group_gen(0, 3, 4, 5, 6), group_gen(1, 0, 1, 2, 7)]
                while gens:
                    for g_ in list(gens):
                        try:
                            next(g_)
                        except StopIteration:
                            gens.remove(g_)
                Pf = Pm[1]
                PK = [("P", 1, 0), ("P", 1, 1)]
                if tt == 0:
                    dump("D0", Dm.rearrange("p a t -> p (a t)"), [("Dm", h) for h in range(8)])
                    dump("M0", Mm[0].rearrange("p a t -> p (a t)"), [("M", 0, 0), ("M", 0, 1)])
                    dump("N0", Nm[0].rearrange("p a t -> p (a t)"), [("N", 0, 0), ("N", 0, 1)])
                    dump("P0", Pm[1].rearrange("p a t -> p (a t)"), [("P", 1, 0), ("P", 1, 1)])
                    dump("qkT0", qkT_sb.rearrange("p a t -> p (a t)"), [("qkT", 0), ("qkT", 1)])
                if stop_after == "gdn_e":
                    return
                for hf in range(2):
                    ub = 7 if hf == 0 else 0
                    for h in range(8):
                        op("pe", lambda e, h=h, hf=hf, ub=ub: e.matmul(PB(ub)[0:64, h * 64:(h + 1) * 64],
                                                                       lhsT=Pf[:, h, hf * 64:(hf + 1) * 64],
                                                                       rhs=rhsv[:, h * 64:(h + 1) * 64], start=True, stop=True),
                           reads=PK + ["rhsv"], writes=[("pb", ub)])
                    op("act", lambda e, hf=hf, ub=ub: e.copy(out=u_c[0:64, hf, :], in_=PB(ub)[0:64, :]), reads=[("pb", ub)],
                       writes=[("u_c", hf)])
                for h in range(8):
                    op("pe", lambda e, h=h: e.matmul(PB(1)[:, h * 64:(h + 1) * 64], lhsT=Pf[:, h, :], rhs=rhsk[:, h * 64:(h + 1) * 64],
                                                     start=True, stop=True), reads=PK + ["rhsk"], writes=[("pb", 1)])
                op("act", lambda e: e.copy(out=w_tok, in_=PB(1)), reads=[("pb", 1)], writes=["w_tok"])
                for q in range(4):
                    op("pe", lambda e, q=q: e.transpose(out=PBb(6)[:, q * 128:(q + 1) * 128], in_=w_tok[:, q * 128:(q + 1) * 128],
                                                        identity=ident_b), reads=["w_tok", "ident_b"], writes=[("pb", 6)])
                op("dve", lambda e: e.tensor_copy(out=wT_sb, in_=PBb(6)[:, 0:512].rearrange("p (q t) -> p q t", q=4)),
                   reads=[("pb", 6)], writes=["wT_sb"])
                op("sp", lambda e: e.dma_start(out=qkT_c1[0:64, :, :], in_=qkT_sb[64:128, :, 64:128]),
                   reads=[("qkT", 0), ("qkT", 1)], writes=["qkT_c1"], dma=True)
                op("sp", lambda e: e.dma_start(out=kdec_c1[0:64, :], in_=kdec[64:128, :]), reads=["kdec"], writes=["kdec_c1"],
                   dma=True)
                op("sp", lambda e, tt=tt: e.dma_start(out=az_c[0:64, :], in_=azs[64:128, tt, :]), reads=["azs_all"], writes=["az_c"],
                   dma=True)
                if tt == 0:
                    dump("u0", u_c.rearrange("p a t -> p (a t)"), [("u_c", 0), ("u_c", 1)])
                    dump("wT0", wT_sb.rearrange("p a t -> p (a t)"), ["wT_sb"])
                if stop_after == "gdn_f":
                    return
                for hf in range(2):
                    tcs = slice(hf * 64, hf * 64 + 64)
                    ck = 2 * tt + hf
                    if hf == 0:
                        qk_x, qk_keys = qkT_sb[0:64, :, 0:64], [("qkT", 0), ("qkT", 1)]
                        kd_x, kd_keys = kdec[0:64, :], ["kdec"]
                    else:
                        qk_x, qk_keys = qkT_c1[0:64, :, :], ["qkT_c1"]
                        kd_x, kd_keys = kdec_c1[0:64, :], ["kdec_c1"]
                    for hp in range(4):
                        op("pe", lambda e, hp=hp, tcs=tcs: e.matmul(PB(1)[0:64, hp * 128:(hp + 1) * 128], lhsT=wT_sb[:, hp, tcs],
                                                                    rhs=S_bd[:, hp, :], start=True, stop=True),
                           reads=["wT_sb", "S_bd"], writes=[("pb", 1)])
                    op("dve", lambda e, hf=hf: e.tensor_tensor(out=vn_b[0:64, :], in0=u_c[0:64, hf, :], in1=PB(1)[0:64, :],
                                                               op=ALU.subtract), reads=[("u_c", hf), ("pb", 1)], writes=["vn_b"])
                    for h in range(8):
                        hp, par = h // 2, h % 2
                        op("pe", lambda e, h=h, hp=hp, par=par, tcs=tcs: e.matmul(
                            PB(2)[0:64, h * 64:(h + 1) * 64], lhsT=qdT[:, hp, tcs], rhs=S_bd[:, hp, par * 64:(par + 1) * 64],
                            start=True, stop=False), reads=["qdT", "S_bd"], writes=[("pb", 2)])
                        op("pe", lambda e, h=h, qk_x=qk_x: e.matmul(
                            PB(2)[0:64, h * 64:(h + 1) * 64], lhsT=qk_x[:, h, :], rhs=vn_b[0:64, h * 64:(h + 1) * 64],
                            start=False, stop=True), reads=qk_keys + ["vn_b"], writes=[("pb", 2)])
                    for hp in range(4):
                        op("pe", lambda e, hp=hp, kd_x=kd_x: e.matmul(PB(7)[:, hp * 128:(hp + 1) * 128],
                                                                      lhsT=kd_x[:, hp * 128:(hp + 1) * 128],
                                                                      rhs=vn_b[0:64, hp * 128:(hp + 1) * 128], start=True, stop=True),
                           reads=kd_keys + ["vn_b"], writes=[("pb", 7)])
                    op("act", lambda e: e.copy(out=o_c[0:64, :], in_=PB(2)[0:64, :]), reads=[("pb", 2)], writes=["o_c"])
                    op("pool", lambda e, hf=hf: e.tensor_tensor(out=Stmp, in0=Sst,
                                                                in1=scs[hf].unsqueeze(2).to_broadcast([128, 4, 128]), op=ALU.mult),
                       reads=["S", ("scs", hf)], writes=["Stmp"])
                    op("dve", lambda e: e.tensor_tensor(out=Sst, in0=Stmp, in1=PB(7).rearrange("p (a d) -> p a d", a=4), op=ALU.add),
                       reads=["Stmp", ("pb", 7)], writes=["S"])
                    op("pool", lambda e: e.tensor_tensor(out=S_bd, in0=Sst, in1=bdmask, op=ALU.mult), reads=["S", "bdmask"],
                       writes=["S_bd"])
                    if "o_raw" in dbg:
                        op("sp", lambda e, ck=ck: e.dma_start(out=dbg["o_raw"][ck * 64:(ck + 1) * 64, :], in_=o_c[0:64, :]),
                           reads=["o_c"], writes=["dbg_o_raw"], dma=True)
                    sqh = sq[0:64, 0:512]
                    op("dve", lambda e: e.tensor_tensor(out=sqh, in0=o_c[0:64, :], in1=o_c[0:64, :], op=ALU.mult), reads=["o_c"],
                       writes=["sq"])
                    op("dve", lambda e: e.tensor_reduce(out=ss2[0:64, :], in_=sqh.rearrange("p (h d) -> p h d", h=8), axis=AX.X,
                                                        op=ALU.add), reads=["sq"], writes=["ss2"])
                    op("act", lambda e: e.activation(out=r2[0:64, :], in_=ss2[0:64, :], func=AF.Ln, bias=epsc[0:64, :],
                                                     scale=1.0 / 64), reads=["ss2", "epsc"], writes=["r2"])
                    op("act", lambda e: e.activation(out=r2[0:64, :], in_=r2[0:64, :], func=AF.Exp, scale=-0.5), reads=["r2"],
                       writes=["r2"])
                    op("dve", lambda e: e.tensor_tensor(out=v3(sqh), in0=v3(o_c[0:64, :]),
                                                        in1=r2[0:64, :].unsqueeze(2).to_broadcast([64, 8, 64]), op=ALU.mult),
                       reads=["o_c", "r2"], writes=["sq"])
                    op("dve", lambda e: e.tensor_tensor(out=v3(sqh), in0=v3(sqh),
                                                        in1=angb[0:64, :].unsqueeze(1).to_broadcast([64, 8, 64]), op=ALU.mult),
                       reads=["sq", "angb"], writes=["sq"])
                    az_x = azs[0:64, tt, :] if hf == 0 else az_c[0:64, :]
                    op("dve", lambda e, az_x=az_x: e.tensor_tensor(out=oa_b[0:64, :], in0=sqh, in1=az_x, op=ALU.mult),
                       reads=["sq", "az_c"], writes=["oa_b"])
                    for q in range(4):
                        op("pe", lambda e, q=q: e.transpose(out=PBb(6)[:, q * 64:(q + 1) * 64], in_=oa_b[0:64, q * 128:(q + 1) * 128],
                                                            identity=ident_b[0:64, 0:64]), reads=["oa_b", "ident_b"],
                           writes=[("pb", 6)])
                    op("act", lambda e, ck=ck: e.copy(out=o_aT[:, :, ck * 64:(ck + 1) * 64],
                                                      in_=PBb(6)[:, 0:256].rearrange("p (q t) -> p q t", q=4)),
                       reads=[("pb", 6)], writes=[("o_aT", ck)])
            if "o_raw" in dbg:
                op("sp", None, reads=["dbg_o_raw"])
            if "o_aT" in dbg:
                op("sp", lambda e: e.dma_start(out=dbg["o_aT"].rearrange("(a p) t -> p a t", p=128), in_=o_aT),
                   reads=[("o_aT", ck) for ck in range(2 * NT)], writes=["dbg_o_aT"], dma=True)
                op("sp", None, reads=["dbg_o_aT"])

            sch.barrier()
            if stop_after == "gdn":
                return

            o_bT = view(R_A + 16 * K, [128, 4, S], BF16)
            da = MultiAlloc([(R_Q, R_Q + 48 * K), (R_W, R_W + 16 * K)])
            scoreb = [da([128, S], F32) for _ in range(2)]
            rl = [da([128, 512], F32) for _ in range(2)]
            maskbb = [da([128, S], BF16) for _ in range(2)]
            thr_t = [da([128, 1], F32) for _ in range(2)]
            PTt = [[da([128, 512], BF16) for _ in range(2)] for _ in range(2)]
            I4 = da([128, 512], BF16)
            lo_t = da([128, 1], F32)
            hi_t = da([128, 1], F32)
            W0 = da([128, 1], F32)
            mid_t = da([128, 1], F32)
            tsel = da([128, 1], F32)
            Wk = da([128, NBIS], F32)
            cnt = da([128, NBIS], F32)
            pow2 = da([128, NBIS], F32)
            ob = da([128, 520], F32)
            rden = da([128, 8], F32)
            ob_b = da([128, 512], BF16)
            for q in range(4):
                op("pool", lambda e, q=q: e.tensor_copy(out=I4[:, q * 128:(q + 1) * 128], in_=ident_b), reads=["ident_b"], writes=["I4"])
            for k in range(NBIS):
                op("pool", lambda e, k=k: e.memset(pow2[:, k:k + 1], 2.0 ** (-(k + 1))), writes=["pow2"])

            xbk = [0]

            def scores_part(tt, sb):
                L = (tt + 1) * 128
                nkb = (L + 511) // 512
                qs = slice(tt * 128, (tt + 1) * 128)
                score = scoreb[sb]
                maskb = maskbb[sb]
                for h in range(8):
                    hp, par = h // 2, h % 2
                    rows = slice(par * 64, par * 64 + 64)
                    for kb in range(nkb):
                        w = min(512, L - kb * 512)
                        bank = xbk[0] % 2
                        xbk[0] += 1
                        ks = slice(kb * 512, kb * 512 + w)
                        op("pe", lambda e, hp=hp, par=par, rows=rows, w=w, bank=bank, ks=ks: e.matmul(
                            PB(bank)[:, 0:w], lhsT=iqT[rows, hp, qs], rhs=ikT2[rows, ks], start=True, stop=True,
                            tile_position=(par * 64, 0)), reads=["iqT", "ikT2"], writes=[("pb", bank)])
                        op("act", lambda e, w=w, bank=bank: e.activation(out=rl[bank][:, 0:w], in_=PB(bank)[:, 0:w], func=AF.Relu),
                           reads=[("pb", bank)], writes=[("rl", bank)])
                        if h == 0:
                            op("dve", lambda e, w=w, bank=bank, ks=ks: e.tensor_scalar(
                                out=score[:, ks], in0=rl[bank][:, 0:w], scalar1=iw_tok[:, tt, 0:1], scalar2=None, op0=ALU.mult),
                               reads=[("rl", bank), "iw_tok"], writes=[("score", sb, kb)])
                        else:
                            op("dve", lambda e, w=w, bank=bank, ks=ks, h=h: e.scalar_tensor_tensor(
                                out=score[:, ks], in0=rl[bank][:, 0:w], scalar=iw_tok[:, tt, h:h + 1], in1=score[:, ks],
                                op0=ALU.mult, op1=ALU.add), reads=[("rl", bank), "iw_tok", ("score", sb, kb)],
                               writes=[("score", sb, kb)], fast=(w >= 256))
                SK = [("score", sb, kb) for kb in range(nkb)]
                if tt >= 2:
                    op("dve", lambda e: e.tensor_reduce(out=hi_t, in_=score[:, 0:L], axis=AX.X, op=ALU.max), reads=SK, writes=["hi"])
                    op("dve", lambda e: e.tensor_reduce(out=lo_t, in_=score[:, 0:L], axis=AX.X, op=ALU.min), reads=SK, writes=["lo"])
                op("dve", lambda e: e.memset(score[0:64, L - 64:L], -1.0e30), reads=SK, writes=SK)
                if tt >= 2:
                    op("dve", lambda e: e.tensor_tensor(out=W0, in0=hi_t, in1=lo_t, op=ALU.subtract), reads=["hi", "lo"], writes=["W0"])
                    op("dve", lambda e: e.tensor_scalar(out=Wk, in0=pow2, scalar1=W0[:, 0:1], scalar2=None, op0=ALU.mult),
                       reads=["W0", "pow2"], writes=["Wk"])
                    op("dve", lambda e: e.memset(cnt, 0.0), writes=["cnt"])
                    op("dve", lambda e: e.tensor_tensor(out=mid_t, in0=lo_t, in1=Wk[:, 0:1], op=ALU.add), reads=["lo", "Wk"], writes=["mid"])
                    for k in range(NBIS):
                        op("dve", lambda e, k=k: e.tensor_scalar(out=maskb[:, 0:L], in0=score[:, 0:L], scalar1=mid_t[:, 0:1],
                                                                 scalar2=0.0, op0=ALU.is_gt, op1=ALU.add, accum_out=cnt[:, k:k + 1]),
                           reads=SK + ["mid", "cnt"], writes=[("maskb", sb), ("cntk", k)])
                        op("dve", lambda e, k=k: e.tensor_scalar(out=tsel, in0=cnt[:, k:k + 1], scalar1=255.5, scalar2=0.5,
                                                                 op0=ALU.is_gt, op1=ALU.subtract), reads=[("cntk", k)], writes=["tsel"])
                        op("dve", lambda e, k=k: e.scalar_tensor_tensor(out=mid_t, in0=tsel, scalar=Wk[:, k:k + 1], in1=mid_t,
                                                                        op0=ALU.mult, op1=ALU.add),
                           reads=["tsel", "Wk", "mid"], writes=["mid"])
                    op("dve", lambda e: e.scalar_tensor_tensor(out=thr_t[sb], in0=Wk[:, NBIS - 1:NBIS], scalar=-0.5, in1=mid_t,
                                                               op0=ALU.mult, op1=ALU.add), reads=["Wk", "mid"], writes=[("thr", sb)])
                else:
                    op("dve", lambda e: e.memset(thr_t[sb], -1.0e29), writes=[("thr", sb)])
                op("dve", lambda e: e.tensor_scalar(out=maskb[:, 0:L], in0=score[:, 0:L], scalar1=thr_t[sb][:, 0:1], scalar2=NEG,
                                                    op0=ALU.is_le, op1=ALU.mult), reads=SK + [("thr", sb)], writes=[("maskb", sb)])
                if "thr" in dbg:
                    op("sp", lambda e: e.dma_start(out=dbg["thr"][tt * 128:(tt + 1) * 128, :], in_=thr_t[sb]), reads=[("thr", sb)],
                       writes=["dbg_thr"], dma=True)
                if "score" in dbg and tt == NT - 1:
                    op("sp", lambda e: e.dma_start(out=dbg["score"], in_=score), reads=SK, writes=["dbg_score"], dma=True)

            def attn_part(tt, sb):
                qs = slice(tt * 128, (tt + 1) * 128)
                maskb = maskbb[sb]
                for kb in range(tt + 1):
                    kcs = slice(kb * 128, (kb + 1) * 128)
                    for g2 in range(2):
                        bank = 2 + g2 + 2 * (kb % 2)
                        pt = PTt[g2][kb % 2]
                        op("pe", lambda e, bank=bank, kcs=kcs: e.matmul(PB(bank), lhsT=maskb[:, kcs], rhs=I4, start=True, stop=False),
                           reads=[("maskb", sb), "I4"], writes=[("pb", bank)])
                        for s_ in range(4):
                            h = 4 * g2 + s_
                            hp, par = h // 2, h % 2
                            kT = kz[g2][par]
                            op("pe", lambda e, bank=bank, s_=s_, kT=kT, kcs=kcs, hp=hp: e.matmul(
                                PB(bank)[:, s_ * 128:(s_ + 1) * 128], lhsT=kT[:, kcs], rhs=bqT[:, hp, qs], start=False, stop=(s_ == 3)),
                               reads=["bkT", "bqT"], writes=[("pb", bank)])
                        op("act", lambda e, bank=bank, pt=pt: e.activation(out=pt, in_=PB(bank), func=AF.Exp, scale=0.125),
                           reads=[("pb", bank)], writes=[("PT", g2, kb % 2)])
                        for s_ in range(4):
                            op("pe", lambda e, g2=g2, s_=s_, pt=pt, kb=kb: e.matmul(
                                PB(6 + g2)[:, s_ * 65:(s_ + 1) * 65], lhsT=pt[:, s_ * 128:(s_ + 1) * 128],
                                rhs=bv_tok[:, kb, g2 * 65:(g2 + 1) * 65], start=(kb == 0 and s_ == 0), stop=(kb == tt and s_ == 3)),
                               reads=[("PT", g2, kb % 2), "bv_tok"], writes=[("pb", 6 + g2)])
                op("act", lambda e: e.copy(out=ob[:, 0:260], in_=PB(6)[:, 0:260]), reads=[("pb", 6)], writes=["ob"])
                op("act", lambda e: e.copy(out=ob[:, 260:520], in_=PB(7)[:, 0:260]), reads=[("pb", 7)], writes=["ob"])
                obv = ob.rearrange("p (s e) -> p s e", e=65)
                op("dve", lambda e: e.reciprocal(out=rden, in_=obv[:, :, 64]), reads=["ob"], writes=["rden"])
                op("dve", lambda e: e.tensor_tensor(out=ob_b.rearrange("p (h d) -> p h d", h=8), in0=obv[:, :, 0:64],
                                                    in1=rden.unsqueeze(2).to_broadcast([128, 8, 64]), op=ALU.mult),
                   reads=["ob", "rden"], writes=["ob_b"])
                for q in range(4):
                    op("pe", lambda e, q=q: e.transpose(out=PBb(0)[:, q * 128:(q + 1) * 128], in_=ob_b[:, q * 128:(q + 1) * 128],
                                                        identity=ident_b), reads=["ob_b", "ident_b"], writes=[("pb", 0)])
                op("act", lambda e: e.copy(out=o_bT[:, :, qs], in_=PBb(0)[:, 0:512].rearrange("p (q t) -> p q t", q=4)),
                   reads=[("pb", 0)], writes=[("o_bT", tt)])

            scores_part(0, 0)
            for tt in range(NT):
                if tt + 1 < NT:
                    scores_part(tt + 1, (tt + 1) % 2)
                attn_part(tt, tt % 2)
            if "thr" in dbg:
                op("sp", None, reads=["dbg_thr"])
            if "score" in dbg:
                op("sp", None, reads=["dbg_score"])
            if "o_bT" in dbg:
                op("sp", lambda e: e.dma_start(out=dbg["o_bT"].rearrange("(a p) t -> p a t", p=128), in_=o_bT),
                   reads=[("o_bT", tt) for tt in range(NT)], writes=["dbg_o_bT"], dma=True)
                op("sp", None, reads=["dbg_o_bT"])

            sch.barrier()
            if stop_after == "dsa":
                return

            pa = MultiAlloc([(R_W, ARENA_BYTES)])
            hT2 = pa([128, 8, S], BF16)
            mergedT = pa([128, 8, S], BF16)
            x1 = pa([128, NT, D], F32)
            bgate = pa([128, 16], F32)
            g2col = pa([128, 8], F32)
            fng = pa([128, D], F32)
            wst2 = pa([128, 8, 256], F32)
            wg_bf = [pa([128, 8, 256], BF16) for _ in range(2)]
            wp_bf = [pa([128, 4, 256], BF16) for _ in range(2)]
            ga_s = pa([128, 512], BF16)
            gb_s = pa([128, 512], BF16)
            t1 = pa([128, 512], BF16)
            t2 = pa([128, 512], BF16)
            xt4 = [pa([128, D], F32) for _ in range(2)]
            op("sp", lambda e: e.dma_start(out=bgate, in_=bgate_d), writes=["bgate"], dma=True)
            op("sp", lambda e: e.dma_start(out=g2col, in_=g2_d), writes=["g2col"], dma=True)
            op("sp", lambda e: e.dma_start(out=fng, in_=fng_d.partition_broadcast(128)), writes=["fng"], dma=True)

            def phase1b():
                xa = Alloc(R_W + 64 * K, R_W + 128 * K)
                xt = [xa([128, D], F32) for _ in range(2)]
                hb = [xa([128, D], BF16) for _ in range(2)]
                junk = xa([128, D], BF16)
                ssx = xa([128, NT], F32)
                rsx = xa([128, NT], F32)
                op("dve", lambda e: e.memset(ssx, 0.0), writes=["ssx"])
                for tt in range(NT):
                    b = tt % 2
                    op("sp", lambda e, tt=tt, b=b: e.dma_start(out=xt[b], in_=x_d[tt * 128:(tt + 1) * 128, :]), writes=[("xt", b)], dma=True)
                    op("act", lambda e, tt=tt, b=b: e.activation(out=junk, in_=xt[b], func=AF.Square, accum_out=ssx[:, tt:tt + 1]),
                       reads=[("xt", b), "ssx"], writes=["junk", ("ssx", tt)])
                    op("act", lambda e, tt=tt: e.activation(out=rsx[:, tt:tt + 1], in_=ssx[:, tt:tt + 1], func=AF.Sqrt, bias=epsc,
                                                            scale=1.0 / D), reads=[("ssx", tt), "epsc"], writes=[("rsx", tt)])
                    op("dve", lambda e, tt=tt: e.reciprocal(out=rsx[:, tt:tt + 1], in_=rsx[:, tt:tt + 1]), reads=[("rsx", tt)],
                       writes=[("rsx", tt)])
                    op("dve", lambda e, tt=tt, b=b: e.tensor_scalar(out=hb[b], in0=xt[b], scalar1=rsx[:, tt:tt + 1], scalar2=None,
                                                                    op0=ALU.mult), reads=[("xt", b), ("rsx", tt)], writes=[("hb", b)])
                    bk = tt % 2
                    for k in range(8):
                        op("pe", lambda e, k=k, b=b, bk=bk: e.transpose(out=PBb(bk)[:, k * 128:(k + 1) * 128],
                                                                        in_=hb[b][:, k * 128:(k + 1) * 128], identity=ident_b),
                           reads=[("hb", b), "ident_b"], writes=[("pb", bk)])
                    op("act", lambda e, tt=tt, bk=bk: e.copy(out=hT2[:, :, tt * 128:(tt + 1) * 128],
                                                             in_=PBb(bk).rearrange("p (k t) -> p k t", k=8)),
                       reads=[("pb", bk)], writes=[("hT2", tt)])
            phase1b()
            sch.barrier()
            HT2 = [("hT2", tt) for tt in range(NT)]

            wpa_v = wpa_d.rearrange("(k p) c -> p k c", p=128)
            wpb_v = wpb_d.rearrange("(k p) c -> p k c", p=128)
            wout_v = wout_d.rearrange("(k p) c -> p k c", p=128)
            wi4 = [0]

            wst2b = xt4[0].rearrange("p (k c) -> p k c", k=4)
            wst2c = xt4[1].rearrange("p (k c) -> p k c", k=4)
            wi5 = [0]

            def load_gate(c0):
                i = wi4[0]
                wi4[0] += 1
                b = i % 2
                op("sp", lambda e: e.dma_start(out=wst2, in_=w_in_v[:, :, c0:c0 + 256]), writes=["wst2"], dma=True)
                op("pool", lambda e, b=b: e.tensor_tensor(out=wg_bf[b], in0=wst2, in1=g1col.unsqueeze(2).to_broadcast([128, 8, 256]),
                                                          op=ALU.mult), reads=["wst2", "g1col"], writes=[("wg", b)])
                return wg_bf[b], ("wg", b)

            def load_proj(src_v, c0):
                i = wi5[0]
                wi5[0] += 1
                b = i % 2
                st = wst2b if b == 0 else wst2c
                op("sp", lambda e: e.dma_start(out=st, in_=src_v[:, :, c0:c0 + 256]), writes=[("wstp", b)], dma=True)
                op("pool", lambda e, b=b: e.tensor_copy(out=wp_bf[b], in_=st), reads=[("wstp", b)], writes=[("wp", b)])
                return wp_bf[b], ("wp", b)

            p4u = [0]
            for j in range(4):
                wga, kga = load_gate(C_GA + j * 256)
                wgb, kgb = load_gate(C_GB + j * 256)
                wpa, kpa = load_proj(wpa_v, j * 256)
                wpb, kpb = load_proj(wpb_v, j * 256)
                for ct in range(2):
                    c = 2 * j + ct
                    ccs = slice(ct * 128, (ct + 1) * 128)
                    for tb in range(4):
                        tcs = slice(tb * 512, (tb + 1) * 512)
                        hk = HT2[tb * 4:tb * 4 + 4]
                        bA, bB, bC, bD = (2, 3, 4, 5) if (p4u[0] % 2 == 0) else (0, 1, 6, 7)
                        p4u[0] += 1
                        for k in range(8):
                            op("pe", lambda e, k=k, ccs=ccs, tcs=tcs, wga=wga, bA=bA: e.matmul(PB(bA), lhsT=wga[:, k, ccs], rhs=hT2[:, k, tcs],
                                                                                         start=(k == 0), stop=(k == 7)),
                               reads=[kga] + hk, writes=[("pb", bA)])
                        op("act", lambda e, c=c, bA=bA: e.activation(out=ga_s, in_=PB(bA), func=AF.Sigmoid, bias=bgate[:, c:c + 1], scale=1.0),
                           reads=[("pb", bA), "bgate"], writes=["ga_s"])
                        for k in range(8):
                            op("pe", lambda e, k=k, ccs=ccs, tcs=tcs, wgb=wgb, bB=bB: e.matmul(PB(bB), lhsT=wgb[:, k, ccs], rhs=hT2[:, k, tcs],
                                                                                         start=(k == 0), stop=(k == 7)),
                               reads=[kgb] + hk, writes=[("pb", bB)])
                        op("act", lambda e, c=c, bB=bB: e.activation(out=gb_s, in_=PB(bB), func=AF.Sigmoid, bias=bgate[:, 8 + c:9 + c], scale=1.0),
                           reads=[("pb", bB), "bgate"], writes=["gb_s"])
                        for hp in range(4):
                            op("pe", lambda e, hp=hp, ccs=ccs, tcs=tcs, wpa=wpa, bC=bC: e.matmul(PB(bC), lhsT=wpa[:, hp, ccs], rhs=o_aT[:, hp, tcs],
                                                                                           start=(hp == 0), stop=(hp == 3)),
                               reads=[kpa, "o_aT"], writes=[("pb", bC)])
                        for hp in range(4):
                            op("pe", lambda e, hp=hp, ccs=ccs, tcs=tcs, wpb=wpb, bD=bD: e.matmul(PB(bD), lhsT=wpb[:, hp, ccs], rhs=o_bT[:, hp, tcs],
                                                                                           start=(hp == 0), stop=(hp == 3)),
                               reads=[kpb, "o_bT"], writes=[("pb", bD)])
                        op("dve", lambda e, bC=bC: e.tensor_tensor(out=t1, in0=PB(bC), in1=ga_s, op=ALU.mult), reads=[("pb", bC), "ga_s"],
                           writes=["t1"])
                        op("dve", lambda e, bD=bD: e.tensor_tensor(out=t2, in0=PB(bD), in1=gb_s, op=ALU.mult), reads=[("pb", bD), "gb_s"],
                           writes=["t2"])
                        op("pool", lambda e, c=c, tcs=tcs: e.tensor_tensor(out=mergedT[:, c, tcs], in0=t1, in1=t2, op=ALU.add),
                           reads=["t1", "t2"], writes=[("mergedT", c, tb)])
            if "mergedT" in dbg:
                op("sp", lambda e: e.dma_start(out=dbg["mergedT"].rearrange("(a p) t -> p a t", p=128), in_=mergedT),
                   reads=[("mergedT", c, tb) for c in range(8) for tb in range(4)], writes=["dbg_mergedT"], dma=True)
                op("sp", None, reads=["dbg_mergedT"])
            sch.barrier()
            wout_bf = view(R_A, [128, 8, D], BF16)
            for j in range(4):
                op("sp", lambda e, j=j: e.dma_start(out=wst2, in_=wout_v[:, :, j * 256:(j + 1) * 256]), writes=["wst2"], dma=True)
                op("pool", lambda e, j=j: e.tensor_copy(out=wout_bf[:, :, j * 256:(j + 1) * 256], in_=wst2), reads=["wst2"],
                   writes=[("wout", j)])
            WOUT = [("wout", j) for j in range(4)]
            for tt in range(NT):
                b = tt % 2
                op("sp", lambda e, tt=tt, b=b: e.dma_start(out=xt4[b], in_=x_d[tt * 128:(tt + 1) * 128, :]), writes=[("xt4", b)], dma=True)
                for nb in range(2):
                    bk = 2 + 2 * b + nb
                    for c in range(8):
                        op("pe", lambda e, c=c, tt=tt, nb=nb, bk=bk: e.matmul(PB(bk), lhsT=mergedT[:, c, tt * 128:(tt + 1) * 128],
                                                                               rhs=wout_bf[:, c, nb * 512:(nb + 1) * 512],
                                                                               start=(c == 0), stop=(c == 7)),
                           reads=WOUT + ["mergedT_all"], writes=[("pb", bk)])
                    op("dve", lambda e, tt=tt, nb=nb, bk=bk, b=b: e.tensor_tensor(out=x1[:, tt, nb * 512:(nb + 1) * 512], in0=PB(bk),
                                                                                   in1=xt4[b][:, nb * 512:(nb + 1) * 512], op=ALU.add),
                       reads=[("pb", bk), ("xt4", b)], writes=[("x1", tt)])
            if "x1" in dbg:
                op("sp", lambda e: e.dma_start(out=dbg["x1"].rearrange("(t p) c -> p t c", p=128), in_=x1),
                   reads=[("x1", tt) for tt in range(NT)], writes=["dbg_x1"], dma=True)
                op("sp", None, reads=["dbg_x1"])
            sch.barrier()
            if stop_after == "p4":
                return

            h2T = view(R_W, [128, 8, S], BF16)
            ma = MultiAlloc([(R_W + 32 * K, R_W + 64 * K), (R_A, R_A + 32 * K)])
            tail_off = None
            hb2 = [ma([128, D], BF16) for _ in range(2)]
            junk2 = ma([128, D], BF16)
            ss5 = ma([128, NT], F32)
            rs5 = ma([128, NT], F32)
            wr_st = ma([128, 8, 20], F32)
            wr_bf = ma([128, 8, 20], BF16)
            brow = ma([128, 20], F32)
            lg = ma([128, 20], F32)
            sm = {n_: ma([128, 4], F32) for n_ in ("goh", "gex", "elg", "oh1", "msk", "oh2", "wsel")}
            sc1 = {n_: ma([128, 1], F32) for n_ in ("gmax", "ngmax", "gsum", "ggate", "m1", "m2", "d21", "e21", "den", "w1", "w2")}
            tmp44 = ma([128, 4, 4], F32)
            comb_b = ma([128, 16], BF16)
            combT = ma([128, S], BF16)
            sel16 = ma([128, 16, 128], BF16)
            est = ma([128, 8, 256], F32)
            w1b = [ma([128, 8, 256], BF16) for _ in range(2)]
            w3b = [ma([128, 8, 256], BF16) for _ in range(2)]
            w2b = [ma([128, 2, D], BF16) for _ in range(2)]
            sg = [[ma([128, 512], BF16) for _ in range(2)] for _ in range(2)]
            cbt = [ma([128, 512], BF16) for _ in range(2)]
            tu = [[ma([128, 512], BF16) for _ in range(2)] for _ in range(2)]
            actT = [[ma([128, 512], BF16) for _ in range(2)] for _ in range(2)]
            op("sp", lambda e: e.dma_start(out=wr_st, in_=wr_d.rearrange("(k p) c -> p k c", p=128)), writes=["wr_st"], dma=True)
            op("sp", lambda e: e.dma_start(out=brow, in_=br_d.partition_broadcast(128)), writes=["brow"], dma=True)
            op("pool", lambda e: e.tensor_tensor(out=wr_bf, in0=wr_st, in1=g2col.unsqueeze(2).to_broadcast([128, 8, 20]), op=ALU.mult),
               reads=["wr_st", "g2col"], writes=["wr_bf"])
            op("pool", lambda e: e.memset(sel16[0:16, :, :], 1.0), writes=["sel16"])
            op("pool", lambda e: e.affine_select(out=sel16[0:16, :, :], in_=sel16[0:16, :, :], pattern=[[-1, 16], [0, 128]],
                                                 compare_op=ALU.is_equal, fill=0.0, base=0, channel_multiplier=1), writes=["sel16"])
            op("dve", lambda e: e.memset(ss5, 0.0), writes=["ss5"])
            def prep_tile(tt):
                b = tt % 2
                bk = tt % 2
                op("act", lambda e, tt=tt: e.activation(out=junk2, in_=x1[:, tt, :], func=AF.Square, accum_out=ss5[:, tt:tt + 1]),
                   reads=["x1_all", "ss5"], writes=["junk2", ("ss5", tt)])
                op("act", lambda e, tt=tt: e.activation(out=rs5[:, tt:tt + 1], in_=ss5[:, tt:tt + 1], func=AF.Ln, bias=epsc,
                                                        scale=1.0 / D), reads=[("ss5", tt), "epsc"], writes=[("rs5", tt)])
                op("act", lambda e, tt=tt: e.activation(out=rs5[:, tt:tt + 1], in_=rs5[:, tt:tt + 1], func=AF.Exp, scale=-0.5),
                   reads=[("rs5", tt)], writes=[("rs5", tt)])
                op("dve", lambda e, tt=tt, b=b: e.tensor_scalar(out=hb2[b], in0=x1[:, tt, :], scalar1=rs5[:, tt:tt + 1], scalar2=None,
                                                                op0=ALU.mult), reads=["x1_all", ("rs5", tt)], writes=[("hb2", b)])
                for k in range(8):
                    op("pe", lambda e, k=k, b=b, bk=bk: e.transpose(out=PBb(bk)[:, k * 128:(k + 1) * 128],
                                                                    in_=hb2[b][:, k * 128:(k + 1) * 128], identity=ident_b),
                       reads=[("hb2", b), "ident_b"], writes=[("pb", bk)])
                op("act", lambda e, tt=tt, bk=bk: e.copy(out=h2T[:, :, tt * 128:(tt + 1) * 128],
                                                         in_=PBb(bk).rearrange("p (k t) -> p k t", k=8)),
                   reads=[("pb", bk)], writes=[("h2T", tt)])
                for k in range(8):
                    op("pe", lambda e, k=k, tt=tt: e.matmul(PB(2)[:, 0:20], lhsT=h2T[:, k, tt * 128:(tt + 1) * 128], rhs=wr_bf[:, k, :],
                                                            start=(k == 0), stop=(k == 7)),
                       reads=[("h2T", tt), "wr_bf"], writes=[("pb", 2)])
                R = []

                def rop(fn, rd, wr):
                    op("dve", fn, reads=rd, writes=wr)
                rop(lambda e: e.tensor_tensor(out=lg, in0=PB(2)[:, 0:20], in1=brow, op=ALU.add), [("pb", 2), "brow"], ["lg"])
                elv = lg[:, 4:20].rearrange("p (g x) -> p g x", g=4)
                rop(lambda e: e.tensor_reduce(out=sc1["gmax"], in_=lg[:, 0:4], axis=AX.X, op=ALU.max), ["lg"], ["gmax"])
                rop(lambda e: e.tensor_scalar(out=sm["goh"], in0=lg[:, 0:4], scalar1=sc1["gmax"][:, 0:1], scalar2=None,
                                              op0=ALU.is_equal), ["lg", "gmax"], ["goh"])
                rop(lambda e: e.tensor_scalar(out=sc1["ngmax"], in0=sc1["gmax"], scalar1=-1.0, scalar2=None, op0=ALU.mult),
                    ["gmax"], ["ngmax"])
                op("act", lambda e: e.activation(out=sm["gex"], in_=lg[:, 0:4], func=AF.Exp, bias=sc1["ngmax"][:, 0:1], scale=1.0),
                   reads=["lg", "ngmax"], writes=["gex"])
                rop(lambda e: e.tensor_reduce(out=sc1["gsum"], in_=sm["gex"], axis=AX.X, op=ALU.add), ["gex"], ["gsum"])
                rop(lambda e: e.reciprocal(out=sc1["ggate"], in_=sc1["gsum"]), ["gsum"], ["ggate"])
                rop(lambda e: e.tensor_tensor(out=tmp44, in0=elv, in1=sm["goh"].unsqueeze(2).to_broadcast([128, 4, 4]), op=ALU.mult),
                    ["lg", "goh"], ["tmp44"])
                rop(lambda e: e.tensor_reduce(out=sm["elg"], in_=tmp44.rearrange("p g x -> p x g"), axis=AX.X, op=ALU.add),
                    ["tmp44"], ["elg"])
                rop(lambda e: e.tensor_reduce(out=sc1["m1"], in_=sm["elg"], axis=AX.X, op=ALU.max), ["elg"], ["m1"])
                rop(lambda e: e.tensor_scalar(out=sm["oh1"], in0=sm["elg"], scalar1=sc1["m1"][:, 0:1], scalar2=None, op0=ALU.is_equal),
                    ["elg", "m1"], ["oh1"])
                rop(lambda e: e.scalar_tensor_tensor(out=sm["msk"], in0=sm["oh1"], scalar=-1.0e30, in1=sm["elg"], op0=ALU.mult,
                                                     op1=ALU.add), ["oh1", "elg"], ["msk"])
                rop(lambda e: e.tensor_reduce(out=sc1["m2"], in_=sm["msk"], axis=AX.X, op=ALU.max), ["msk"], ["m2"])
                rop(lambda e: e.tensor_scalar(out=sm["oh2"], in0=sm["msk"], scalar1=sc1["m2"][:, 0:1], scalar2=None, op0=ALU.is_equal),
                    ["msk", "m2"], ["oh2"])
                rop(lambda e: e.tensor_tensor(out=sc1["d21"], in0=sc1["m2"], in1=sc1["m1"], op=ALU.subtract), ["m1", "m2"], ["d21"])
                op("act", lambda e: e.activation(out=sc1["e21"], in_=sc1["d21"], func=AF.Exp), reads=["d21"], writes=["e21"])
                rop(lambda e: e.tensor_scalar(out=sc1["den"], in0=sc1["e21"], scalar1=1.0, scalar2=None, op0=ALU.add), ["e21"], ["den"])
                rop(lambda e: e.reciprocal(out=sc1["den"], in_=sc1["den"]), ["den"], ["den"])
                rop(lambda e: e.tensor_tensor(out=sc1["w1"], in0=sc1["ggate"], in1=sc1["den"], op=ALU.mult), ["ggate", "den"], ["w1"])
                rop(lambda e: e.tensor_tensor(out=sc1["w2"], in0=sc1["w1"], in1=sc1["e21"], op=ALU.mult), ["w1", "e21"], ["w2"])
                rop(lambda e: e.tensor_scalar(out=sm["wsel"], in0=sm["oh1"], scalar1=sc1["w1"][:, 0:1], scalar2=None, op0=ALU.mult),
                    ["oh1", "w1"], ["wsel"])
                rop(lambda e: e.scalar_tensor_tensor(out=sm["wsel"], in0=sm["oh2"], scalar=sc1["w2"][:, 0:1], in1=sm["wsel"],
                                                     op0=ALU.mult, op1=ALU.add), ["oh2", "w2", "wsel"], ["wsel"])
                rop(lambda e: e.tensor_tensor(out=comb_b.rearrange("p (g x) -> p g x", g=4),
                                              in0=sm["goh"].unsqueeze(2).to_broadcast([128, 4, 4]),
                                              in1=sm["wsel"].unsqueeze(1).to_broadcast([128, 4, 4]), op=ALU.mult),
                    ["goh", "wsel"], ["comb_b"])
                if "comb" in dbg:
                    op("sp", lambda e, tt=tt: e.dma_start(out=dbg["comb"][tt * 128:(tt + 1) * 128, :], in_=comb_b), reads=["comb_b"],
                       writes=["dbg_comb"], dma=True)
                op("pe", lambda e: e.transpose(out=PBb(3)[0:16, 0:128], in_=comb_b, identity=ident_b), reads=["comb_b", "ident_b"],
                   writes=[("pb", 3)])
                op("act", lambda e, tt=tt: e.copy(out=combT[0:16, tt * 128:(tt + 1) * 128], in_=PBb(3)[0:16, 0:128]),
                   reads=[("pb", 3)], writes=[("combT", tt)])
            for tt in range(4):
                prep_tile(tt)
            H2T = [("h2T", tt) for tt in range(NT)]
            CT = [("combT", tt) for tt in range(NT)]

            def load_expert(e_i):
                b = e_i % 2
                for (src, dst, nm, fold) in ((w1_d, w1b[b], "w1", True), (w3_d, w3b[b], "w3", True)):
                    op("sp", lambda e, src=src: e.dma_start(out=est, in_=src[e_i].rearrange("(k p) f -> p k f", p=128)),
                       writes=["est"], dma=True)
                    op("pool", lambda e, dst=dst: e.tensor_tensor(out=dst, in0=est, in1=g2col.unsqueeze(2).to_broadcast([128, 8, 256]),
                                                                  op=ALU.mult), reads=["est", "g2col"], writes=[(nm, b)])
                op("sp", lambda e: e.dma_start(out=est.rearrange("p k f -> p (k f)").rearrange("p (a c) -> p a c", a=2),
                                               in_=w2_d[e_i].rearrange("(a p) c -> p a c", p=128)), writes=["est"], dma=True)
                op("pool", lambda e: e.tensor_copy(out=w2b[b], in_=est.rearrange("p k f -> p (k f)").rearrange("p (a c) -> p a c", a=2)),
                   reads=["est"], writes=[("w2", b)])

            def stageA(e_i, tb, sl):
                b = e_i % 2
                tcs = slice(tb * 512, (tb + 1) * 512)
                hk = H2T[tb * 4:tb * 4 + 4]
                cbk_ = 6
                op("pe", lambda e: e.matmul(PB(cbk_), lhsT=sel16[0:16, e_i, :], rhs=combT[0:16, tcs], start=True, stop=True),
                   reads=["sel16"] + CT[tb * 4:tb * 4 + 4], writes=[("pb", cbk_)])
                op("act", lambda e: e.copy(out=cbt[sl], in_=PB(cbk_)), reads=[("pb", cbk_)], writes=[("cbt", sl)])
                for ft in range(2):
                    fcs = slice(ft * 128, (ft + 1) * 128)
                    for k in range(8):
                        op("pe", lambda e, k=k, fcs=fcs, ft=ft: e.matmul(PB(2 + ft), lhsT=w1b[b][:, k, fcs], rhs=h2T[:, k, tcs],
                                                                         start=(k == 0), stop=(k == 7)),
                           reads=[("w1", b)] + hk, writes=[("pb", 2 + ft)])
                    op("act", lambda e, ft=ft: e.activation(out=sg[sl][ft], in_=PB(2 + ft), func=AF.Silu), reads=[("pb", 2 + ft)],
                       writes=[("sg", sl, ft)])
                    yield
                    for k in range(8):
                        op("pe", lambda e, k=k, fcs=fcs, ft=ft: e.matmul(PB(4 + ft), lhsT=w3b[b][:, k, fcs], rhs=h2T[:, k, tcs],
                                                                         start=(k == 0), stop=(k == 7)),
                           reads=[("w3", b)] + hk, writes=[("pb", 4 + ft)])
                    op("dve", lambda e, ft=ft: e.tensor_tensor(out=tu[sl][ft], in0=PB(4 + ft), in1=sg[sl][ft], op=ALU.mult),
                       reads=[("pb", 4 + ft), ("sg", sl, ft)], writes=[("tu", sl, ft)], fast=True)
                    op("pool", lambda e, ft=ft: e.tensor_tensor(out=actT[sl][ft], in0=tu[sl][ft], in1=cbt[sl], op=ALU.mult),
                       reads=[("tu", sl, ft), ("cbt", sl)], writes=[("actT", sl, ft)])
                    yield

            ybank = [0]

            def stageB(e_i, tb, sl):
                b = e_i % 2
                for t4 in range(4):
                    tt = tb * 4 + t4
                    for nb in range(2):
                        bk = (0, 1, 7)[ybank[0] % 3]
                        ybank[0] += 1
                        for ft in range(2):
                            op("pe", lambda e, ft=ft, t4=t4, nb=nb, bk=bk: e.matmul(
                                PB(bk), lhsT=actT[sl][ft][:, t4 * 128:(t4 + 1) * 128], rhs=w2b[b][:, ft, nb * 512:(nb + 1) * 512],
                                start=(ft == 0), stop=(ft == 1)), reads=[("actT", sl, ft), ("w2", b)], writes=[("pb", bk)])
                        op("dve", lambda e, tt=tt, nb=nb, bk=bk: e.tensor_tensor(out=x1[:, tt, nb * 512:(nb + 1) * 512],
                                                                                 in0=PB(bk), in1=x1[:, tt, nb * 512:(nb + 1) * 512],
                                                                                 op=ALU.add),
                           reads=[("pb", bk), ("x2", tt, nb)], writes=[("x2", tt, nb)], fast=True)
                        if nb == 1:
                            yield

            def drain(g_):
                for _ in g_:
                    pass

            units = [(e_i, tb) for e_i in range(16) for tb in range(4)]
            load_expert(0)
            load_expert(1)
            drain(stageA(units[0][0], units[0][1], 0))
            for u, (e_i, tb) in enumerate(units):
                gb = stageB(e_i, tb, u % 2)
                if u + 1 < len(units):
                    ne, ntb = units[u + 1]
                    if ne == 0:
                        for tt in range(4 * ntb, 4 * ntb + 4):
                            prep_tile(tt)
                    ga_ = stageA(ne, ntb, (u + 1) % 2)
                    drain(ga_)
                drain(gb)
                if tb == 3 and e_i + 2 < 16:
                    load_expert(e_i + 2)
            sch.barrier()
            if "x2" in dbg:
                op("sp", lambda e: e.dma_start(out=dbg["x2"].rearrange("(t p) c -> p t c", p=128), in_=x1), writes=["dbg_x2"], dma=True)
                op("sp", None, reads=["dbg_x2"])

            fa = MultiAlloc([(R_W, R_W + 64 * K)])
            ss6 = fa([128, NT], F32)
            rs6 = fa([128, NT], F32)
            junk6 = fa([128, D], BF16)
            yo = [fa([128, D], F32) for _ in range(2)]
            op("dve", lambda e: e.memset(ss6, 0.0), writes=["ss6"])
            for tt in range(NT):
                b = tt % 2
                op("act", lambda e, tt=tt: e.activation(out=junk6, in_=x1[:, tt, :], func=AF.Square, accum_out=ss6[:, tt:tt + 1]),
                   reads=["ss6"], writes=["junk6", ("ss6", tt)])
                op("act", lambda e, tt=tt: e.activation(out=rs6[:, tt:tt + 1], in_=ss6[:, tt:tt + 1], func=AF.Sqrt, bias=epsc,
                                                        scale=1.0 / D), reads=[("ss6", tt), "epsc"], writes=[("rs6", tt)])
                op("dve", lambda e, tt=tt: e.reciprocal(out=rs6[:, tt:tt + 1], in_=rs6[:, tt:tt + 1]), reads=[("rs6", tt)],
                   writes=[("rs6", tt)])
                op("dve", lambda e, tt=tt, b=b: e.scalar_tensor_tensor(out=yo[b], in0=x1[:, tt, :], scalar=rs6[:, tt:tt + 1], in1=fng,
                                                                       op0=ALU.mult, op1=ALU.mult),
                   reads=[("rs6", tt), "fng"], writes=[("yo", b)])
                op("sp", lambda e, tt=tt, b=b: e.dma_start(out=out_d[tt * 128:(tt + 1) * 128, :], in_=yo[b]), reads=[("yo", b)],
                   writes=[("out", tt)], dma=True)
            op("sp", None, reads=[("out", tt) for tt in range(NT)])


        body()
        sch.barrier()
        DEBUG["stats_pre"] = {e: len(sch.ops[e]) for e in Sched.ENGS}
        with nc.Block() as block:
            sch.emit(nc, block, engsem, dmasem)
        DEBUG["stats"] = sch.stats
    return nc


_NC_CACHE = {}


def kernel(**inputs):
    dbg = tuple(DEBUG.get("outputs", ()))
    key = (dbg, DEBUG.get("stop_after"))
    if key not in _NC_CACHE:
        _NC_CACHE[key] = build_nc(dbg, DEBUG.get("stop_after"))
    nc = _NC_CACHE[key]
    n = 8
    x = np.ascontiguousarray(inputs["x"], dtype=np.float32)
    posn = np.ascontiguousarray(inputs["positions"], dtype=np.int32)
    f32 = lambda a: np.ascontiguousarray(a, dtype=np.float32)
    inv = (10000.0 ** (-np.arange(32, dtype=np.float32) / np.float32(32))).astype(np.float32).reshape(1, 32)
    shared = {
        "norm1_g": f32(inputs["norm1_g"][0].reshape(8, 128).T),
        "w_in": f32(inputs["w_in"][0]),
        "conv_w": f32(inputs["conv_w"][0].reshape(4, 12, 128).transpose(2, 1, 0).reshape(128, 48)),
        "inv_freq": inv,
        "a_log": f32(inputs["a_log"][0].reshape(1, 8)),
        "dt_bias": f32(inputs["dt_bias"][0].reshape(1, 8)),
        "a_norm_g": f32(inputs["a_norm_g"][0].reshape(1, 64)),
        "b_gate": f32(inputs["b_gate"][0].reshape(16, 128).T),
        "norm2_g": f32(inputs["norm2_g"][0].reshape(8, 128).T),
        "final_norm_g": f32(inputs["final_norm_g"].reshape(1, D)),
        "w_proj_a": f32(inputs["w_proj_a"][0]),
        "w_proj_b": f32(inputs["w_proj_b"][0]),
        "w_out": f32(inputs["w_out"][0]),
        "w_router": f32(np.concatenate([inputs["w_router_group"][0], inputs["w_router_expert"][0]], axis=1)),
        "b_router": f32(np.concatenate([inputs["b_router_group"][0], inputs["b_router_expert"][0]], axis=0).reshape(1, 20)),
        "w_exp_gate": f32(inputs["w_exp_gate"][0]),
        "w_exp_up": f32(inputs["w_exp_up"][0]),
        "w_exp_down": f32(inputs["w_exp_down"][0]),
    }
    in_maps = []
    for c in range(n):
        m = dict(shared)
        m["x"] = x[c]
        m["positions"] = np.ascontiguousarray(posn[c].reshape(NT, 128).T)
        in_maps.append(m)
    res = run_bass_kernel_spmd(nc, in_maps, core_ids=list(range(n)))
    DEBUG["results"] = res.results
    return np.stack([r["out"] for r in res.results], axis=0)
```

```python
import math
from contextlib import ExitStack
import numpy as np
import concourse.bass as bass
import concourse.mybir as mybir
from concourse.bass_utils import run_bass_kernel_spmd

F32 = mybir.dt.float32
BF16 = mybir.dt.bfloat16
I32 = mybir.dt.int32
AF = mybir.ActivationFunctionType
ALU = mybir.AluOpType
AX = mybir.AxisListType

S = 2048
D = 1024
NT = S // 128
D_IN = 5464
EPS = 1e-6
N_DMA_SEMS = 24
NEG = -30000.0
NBIS = 12
TWO_PI = 2.0 * math.pi

C_AQ, C_AK, C_AV, C_AZ = 0, 512, 1024, 1536
C_BETA, C_ALPHA = 2048, 2056
C_BQ, C_BK, C_BV = 2064, 2576, 2704
C_IQ, C_IK, C_IW = 2832, 3344, 3408
C_GA, C_GB = 3416, 4440

DEBUG = {}
STRICT_SAME_ENGINE = True


class Sched:
    ENGS = ("pe", "act", "dve", "pool", "sp")

    def __init__(self):
        self.ops = {e: [] for e in self.ENGS}
        self.last_w = {}
        self.readers = {}
        self.dma_rr = 0
        self.dma_count = [0] * N_DMA_SEMS

    def op(self, eng, fn, reads=(), writes=(), dma=False, fast=False):
        deps = set()
        raw = set()
        for k in reads:
            t = self.last_w.get(k)
            if t is not None:
                deps.add(t)
                raw.add(t)
        for k in writes:
            t = self.last_w.get(k)
            if t is not None:
                deps.add(t)
            for t in self.readers.get(k, {}).values():
                deps.add(t)
        idx = len(self.ops[eng])
        if dma:
            si = self.dma_rr
            self.dma_rr = (self.dma_rr + 1) % N_DMA_SEMS
            prev = self.dma_count[si]
            if prev > 0:
                deps.add(("dma", si, prev))
            self.dma_count[si] = prev + 1
            tok = ("dma", si, prev + 1)
            rkey = ("dma", si)
        else:
            tok = ("eng", eng, idx)
            rkey = eng
            if STRICT_SAME_ENGINE:
                deps = {t for t in deps if not (t[0] == "eng" and t[1] == eng) or eng != "pe"}
            else:
                deps = {t for t in deps if not (t[0] == "eng" and t[1] == eng)
                        or (t in raw and eng != "pe" and not fast and idx - t[2] <= 8)}
        self.ops[eng].append(dict(fn=fn, deps=deps, signal=False, dma=(tok if dma else None)))
        for k in writes:
            self.last_w[k] = tok
            self.readers[k] = {}
        for k in reads:
            if k in writes:
                continue
            self.readers.setdefault(k, {})[rkey] = tok
        return tok

    def barrier(self):
        toks = set()
        for e in self.ENGS:
            j = len(self.ops[e]) - 1
            while j >= 0 and (self.ops[e][j]["fn"] is None or self.ops[e][j]["dma"] is not None):
                j -= 1
            if j >= 0:
                toks.add(("eng", e, j))
        for si in range(N_DMA_SEMS):
            if self.dma_count[si] > 0:
                toks.add(("dma", si, self.dma_count[si]))
        for e in self.ENGS:
            deps = {t for t in toks if not (t[0] == "eng" and t[1] == e and (e == "pe" or not STRICT_SAME_ENGINE))}
            self.ops[e].append(dict(fn=None, deps=deps, signal=False, dma=None))
        self.last_w = {}
        self.readers = {}

    def emit(self, nc, block, engsem, dmasem):
        for e in self.ENGS:
            for o in self.ops[e]:
                for t in o["deps"]:
                    if t[0] == "eng":
                        self.ops[t[1]][t[2]]["signal"] = True
        sigcount = {}
        for e in self.ENGS:
            c = 0
            lst = []
            for o in self.ops[e]:
                if o["signal"]:
                    c += 1
                lst.append(c)
            sigcount[e] = lst
        self.stats = {e: (len(self.ops[e]), sigcount[e][-1] if sigcount[e] else 0) for e in self.ENGS}

        def run(e, eng):
            waited = {}
            for o in self.ops[e]:
                need = {}
                for t in o["deps"]:
                    if t[0] == "eng":
                        key = ("eng", t[1])
                        val = sigcount[t[1]][t[2]]
                    else:
                        key = ("dma", t[1])
                        val = 16 * t[2]
                    if val > need.get(key, 0):
                        need[key] = val
                for key, val in need.items():
                    if waited.get(key, 0) >= val:
                        continue
                    waited[key] = val
                    sem = engsem[key[1]] if key[0] == "eng" else dmasem[key[1]]
                    eng.wait_ge(sem, val)
                if o["fn"] is None:
                    continue
                inst = o["fn"](eng)
                if o["dma"] is not None:
                    inst.then_inc(dmasem[o["dma"][1]], 16)
                elif o["signal"]:
                    inst.then_inc(engsem[e], 1)

        @block.tensor
        def _(eng):
            run("pe", eng)

        @block.scalar
        def _(eng):
            run("act", eng)

        @block.vector
        def _(eng):
            run("dve", eng)

        @block.gpsimd
        def _(eng):
            run("pool", eng)

        @block.sync
        def _(eng):
            run("sp", eng)


DT_SIZE = {F32: 4, BF16: 2, I32: 4}


def build_nc(debug=(), stop_after=None):
    nc = bass.Bass("TRN2", target_bir_lowering=False)

    def din(name, shape, dt=F32):
        return nc.dram_tensor(name, list(shape), dt, kind="ExternalInput").ap()

    x_d = din("x", [S, D])
    pos_d = din("positions", [128, NT], I32)
    g1_d = din("norm1_g", [128, 8])
    w_in_d = din("w_in", [D, D_IN])
    convw_d = din("conv_w", [128, 48])
    invf_d = din("inv_freq", [1, 32])
    alog_d = din("a_log", [1, 8])
    dtb_d = din("dt_bias", [1, 8])
    ang_d = din("a_norm_g", [1, 64])
    bgate_d = din("b_gate", [128, 16])
    g2_d = din("norm2_g", [128, 8])
    fng_d = din("final_norm_g", [1, D])
    wpa_d = din("w_proj_a", [512, D])
    wpb_d = din("w_proj_b", [512, D])
    wout_d = din("w_out", [D, D])
    wr_d = din("w_router", [D, 20])
    br_d = din("b_router", [1, 20])
    w1_d = din("w_exp_gate", [16, D, 256])
    w3_d = din("w_exp_up", [16, D, 256])
    w2_d = din("w_exp_down", [16, 256, D])
    out_d = nc.dram_tensor("out", [S, D], F32, kind="ExternalOutput").ap()
    dbg = {}
    for name, shape, dt in debug:
        dbg[name] = nc.dram_tensor("dbg_" + name, list(shape), dt, kind="ExternalOutput").ap()
    w_in_v = w_in_d.rearrange("(k p) c -> p k c", p=128)

    sch = Sched()
    op = sch.op
    es = ExitStack()
    with es:
        ARENA_BYTES = 207 * 1024
        arena = es.enter_context(nc.sbuf_tensor("arena", [128, ARENA_BYTES // 4], F32))

        def view(off, shape, dt):
            n = 1
            for s_ in shape[1:]:
                n *= s_
            size = n * DT_SIZE[dt]
            assert off % 4 == 0 and size % 4 == 0 and off + size <= ARENA_BYTES, (off, size)
            ap = arena[:, off // 4:(off + size) // 4]
            if dt != F32:
                ap = ap.bitcast(dt)
            if len(shape) == 3:
                ap = ap.rearrange("p (a b) -> p a b", a=shape[1])
            elif len(shape) == 4:
                ap = ap.rearrange("p (a b c) -> p a b c", a=shape[1], b=shape[2])
            return ap

        class Alloc:
            def __init__(self, base, limit):
                self.off = base
                self.limit = limit

            def __call__(self, shape, dt):
                n = 1
                for s_ in shape[1:]:
                    n *= s_
                size = (n * DT_SIZE[dt] + 63) // 64 * 64
                self.off = (self.off + 63) // 64 * 64
                v = view(self.off, shape, dt)
                self.off += size
                assert self.off <= self.limit, (self.off, self.limit)
                return v

        pbank = [es.enter_context(nc.psum_tensor("pb%d" % i, [128, 512], F32)) for i in range(8)]
        engsem = {e: es.enter_context(nc.semaphore("sem_" + e)) for e in Sched.ENGS}
        dmasem = [es.enter_context(nc.semaphore("dsem%d" % i)) for i in range(N_DMA_SEMS)]

        def PB(i):
            return pbank[i][:]

        def PBb(i):
            return pbank[i][:].bitcast(BF16)

        def body():
            K = 1024
            ca = Alloc(0, 9 * K)
            ident_f = ca([128, 128], F32)
            ident_b = ca([128, 128], BF16)
            ucs_f = ca([128, 128], F32)
            mc0_f = ca([128, 128], F32)
            mc1_f = ca([128, 128], F32)
            maskneg_f = ca([128, 128], F32)
            strict_b = ca([128, 128], BF16)
            g1col = ca([128, 8], F32)
            epsc = ca([128, 1], F32)
            cw = ca([128, 48], F32)
            invf = ca([128, 32], F32)
            dtb = ca([128, 8], F32)
            negA = ca([128, 8], F32)
            angb = ca([128, 64], F32)
            posi = ca([128, NT], I32)
            posf = ca([128, NT], F32)
            cs = ca([128, NT, 64], F32)
            assert ca.off <= 9 * K, ca.off

            op("pool", lambda e: e.memset(ident_f, 1.0), writes=["ident_f"])
            op("pool", lambda e: e.affine_select(out=ident_f, in_=ident_f, pattern=[[-1, 128]], compare_op=ALU.is_equal,
                                                 fill=0.0, base=0, channel_multiplier=1), writes=["ident_f"])
            op("pool", lambda e: e.tensor_copy(out=ident_b, in_=ident_f), reads=["ident_f"], writes=["ident_b"])
            op("pool", lambda e: e.memset(ucs_f, 1.0), writes=["ucs_f"])
            op("pool", lambda e: e.affine_select(out=ucs_f, in_=ucs_f, pattern=[[1, 128]], compare_op=ALU.is_ge,
                                                 fill=0.0, base=0, channel_multiplier=-1), writes=["ucs_f"])
            op("pool", lambda e: e.memset(ucs_f[0:64, 64:128], 0.0), writes=["ucs_f"])
            op("pool", lambda e: e.memset(mc0_f, 0.0), writes=["mc0_f"])
            op("pool", lambda e: e.memset(mc0_f[0:64, :], 1.0), writes=["mc0_f"])
            op("pool", lambda e: e.memset(mc1_f, 0.0), writes=["mc1_f"])
            op("pool", lambda e: e.memset(mc1_f[64:128, :], 1.0), writes=["mc1_f"])
            op("pool", lambda e: e.memset(maskneg_f, 0.0), writes=["maskneg_f"])
            op("pool", lambda e: e.affine_select(out=maskneg_f, in_=maskneg_f, pattern=[[-1, 128]], compare_op=ALU.is_ge,
                                                 fill=NEG, base=0, channel_multiplier=1), writes=["maskneg_f"])
            op("pool", lambda e: e.memset(maskneg_f[64:128, 0:64], NEG), writes=["maskneg_f"])
            op("pool", lambda e: e.memset(strict_b, 1.0), writes=["strict_b"])
            op("pool", lambda e: e.affine_select(out=strict_b, in_=strict_b, pattern=[[-1, 128]], compare_op=ALU.is_gt,
                                                 fill=0.0, base=0, channel_multiplier=1), writes=["strict_b"])
            op("pool", lambda e: e.memset(strict_b[64:128, 0:64], 0.0), writes=["strict_b"])
            op("dve", lambda e: e.memset(epsc, EPS), writes=["epsc"])
            op("sp", lambda e: e.dma_start(out=g1col, in_=g1_d), writes=["g1col"], dma=True)
            op("sp", lambda e: e.dma_start(out=cw, in_=convw_d), writes=["cw"], dma=True)
            op("sp", lambda e: e.dma_start(out=invf, in_=invf_d.partition_broadcast(128)), writes=["invf"], dma=True)
            op("sp", lambda e: e.dma_start(out=dtb, in_=dtb_d.partition_broadcast(128)), writes=["dtb"], dma=True)
            op("sp", lambda e: e.dma_start(out=negA, in_=alog_d.partition_broadcast(128)), writes=["negA"], dma=True)
            op("sp", lambda e: e.dma_start(out=angb, in_=ang_d.partition_broadcast(128)), writes=["angb"], dma=True)
            op("sp", lambda e: e.dma_start(out=posi, in_=pos_d), writes=["posi"], dma=True)
            op("act", lambda e: e.activation(out=negA, in_=negA, func=AF.Exp), reads=["negA"], writes=["negA"])
            op("dve", lambda e: e.tensor_scalar(out=negA, in0=negA, scalar1=-1.0, scalar2=None, op0=ALU.mult),
               reads=["negA"], writes=["negA"])

            R_A = 9 * K
            R_W = R_A + 32 * K
            R_Z = R_W + 16 * K
            R_Q = R_Z + 50 * K
            R_S = R_Q + 48 * K
            hT = view(R_A, [128, 8, S], BF16)
            wstage = view(R_W, [128, 8, 256], F32)
            wbf = [view(R_W + 8 * K + i * 4 * K, [128, 8, 256], BF16) for i in range(2)]
            zqkvT = view(R_Z, [128, 12, S + 4], BF16)
            bqT = view(R_Z, [128, 4, S], BF16)
            iqT = view(R_Z + 16 * K, [128, 4, S], BF16)
            azs = view(R_Z + 32 * K, [128, NT, 512], BF16)
            qkv_tok = view(R_Q, [128, NT, 1536], BF16)
            sa = Alloc(R_S, ARENA_BYTES)
            kz = [[sa([128, S], BF16) for _ in range(2)] for _ in range(2)]
            ikT2 = sa([128, S], BF16)
            bv_tok = sa([128, NT, 130], BF16)
            ab_tok = sa([128, NT, 16], F32)
            iw_tok = sa([128, NT, 8], F32)
            diagw = sa([128, 48, 128], BF16)
            R_WORK = sa.off

            def rope_tables():
                wa = Alloc(R_Q, R_Q + 48 * K)
                ang = wa([128, NT, 32], F32)
                tmp = wa([128, NT, 32], F32)
                ki = wa([128, NT, 32], I32)
                op("dve", lambda e: e.tensor_copy(out=posf, in_=posi), reads=["posi"], writes=["posf"])
                op("dve", lambda e: e.tensor_tensor(out=ang, in0=posf.unsqueeze(2).to_broadcast([128, NT, 32]),
                                                    in1=invf.unsqueeze(1).to_broadcast([128, NT, 32]), op=ALU.mult),
                   reads=["posf", "invf"], writes=["ang"])
                for which, shift in ((1, 0.0), (0, math.pi / 2.0)):
                    dst = cs[:, :, which * 32:(which + 1) * 32]
                    op("dve", lambda e, shift=shift: e.tensor_scalar(out=tmp, in0=ang, scalar1=shift, scalar2=None, op0=ALU.add),
                       reads=["ang"], writes=["rt_tmp"])
                    op("dve", lambda e: e.tensor_scalar(out=ki, in0=tmp, scalar1=1.0 / TWO_PI, scalar2=None, op0=ALU.mult),
                       reads=["rt_tmp"], writes=["rt_ki"])
                    op("dve", lambda e, dst=dst: e.tensor_copy(out=dst, in_=ki), reads=["rt_ki"], writes=["cs"])
                    op("dve", lambda e, dst=dst: e.scalar_tensor_tensor(out=dst, in0=dst, scalar=-TWO_PI, in1=tmp,
                                                                       op0=ALU.mult, op1=ALU.add),
                       reads=["cs", "rt_tmp"], writes=["cs"])
                    op("dve", lambda e, dst=dst: e.tensor_scalar(out=dst, in0=dst, scalar1=math.pi, scalar2=-math.pi,
                                                                op0=ALU.min, op1=ALU.max), reads=["cs"], writes=["cs"])
                    op("act", lambda e, dst=dst: e.activation(out=dst, in_=dst, func=AF.Sin), reads=["cs"], writes=["cs"])

            rope_tables()

            def phase1(hT_dst, keyp):
                wa = Alloc(R_Q + 16 * K, R_Q + 48 * K)
                xt = [wa([128, D], F32) for _ in range(2)]
                hb = [wa([128, D], BF16) for _ in range(2)]
                junk = wa([128, D], BF16)
                ss1 = wa([128, NT], F32)
                rstd1 = wa([128, NT], F32)
                op("dve", lambda e: e.memset(ss1, 0.0), writes=[keyp + "ss1"])
                for tt in range(NT):
                    b = tt % 2
                    op("sp", lambda e, tt=tt, b=b: e.dma_start(out=xt[b], in_=x_d[tt * 128:(tt + 1) * 128, :]),
                       writes=[(keyp + "xt", b)], dma=True)
                    op("act", lambda e, tt=tt, b=b: e.activation(out=junk, in_=xt[b], func=AF.Square,
                                                                 accum_out=ss1[:, tt:tt + 1]),
                       reads=[(keyp + "xt", b), keyp + "ss1"], writes=[keyp + "junk", (keyp + "ss1", tt)])
                    op("act", lambda e, tt=tt: e.activation(out=rstd1[:, tt:tt + 1], in_=ss1[:, tt:tt + 1], func=AF.Sqrt,
                                                            bias=epsc, scale=1.0 / D),
                       reads=[(keyp + "ss1", tt), "epsc"], writes=[(keyp + "rstd1", tt)])
                    op("dve", lambda e, tt=tt: e.reciprocal(out=rstd1[:, tt:tt + 1], in_=rstd1[:, tt:tt + 1]),
                       reads=[(keyp + "rstd1", tt)], writes=[(keyp + "rstd1", tt)])
                    op("dve", lambda e, tt=tt, b=b: e.tensor_scalar(out=hb[b], in0=xt[b], scalar1=rstd1[:, tt:tt + 1],
                                                                    scalar2=None, op0=ALU.mult),
                       reads=[(keyp + "xt", b), (keyp + "rstd1", tt)], writes=[(keyp + "hb", b)])
                    pbv = PBb(tt % 2)
                    for k in range(8):
                        op("pe", lambda e, k=k, b=b, pbv=pbv: e.transpose(out=pbv[:, k * 128:(k + 1) * 128],
                                                                          in_=hb[b][:, k * 128:(k + 1) * 128], identity=ident_b),
                           reads=[(keyp + "hb", b), "ident_b"], writes=[("pb", tt % 2)])
                    op("act", lambda e, tt=tt, pbv=pbv: e.copy(out=hT_dst[:, :, tt * 128:(tt + 1) * 128],
                                                               in_=pbv.rearrange("p (k t) -> p k t", k=8)),
                       reads=[("pb", tt % 2)], writes=[("hT", tt)])

            phase1(hT, "p1")
            if stop_after == "p1":
                sch.barrier()
                return
            ALL_HT = [("hT", tt) for tt in range(NT)]

            wchunk_i = [0]

            def load_w(ranges):
                i = wchunk_i[0]
                wchunk_i[0] += 1
                b = i % 2
                off = 0
                for (c0, w) in ranges:
                    op("sp", lambda e, c0=c0, w=w, off=off: e.dma_start(out=wstage[:, :, off:off + w],
                                                                        in_=w_in_v[:, :, c0:c0 + w]),
                       writes=["wstage"], dma=True)
                    off += w
                tot = off
                op("pool", lambda e, b=b, tot=tot: e.tensor_tensor(out=wbf[b][:, :, 0:tot], in0=wstage[:, :, 0:tot],
                                                                   in1=g1col.unsqueeze(2).to_broadcast([128, 8, tot]),
                                                                   op=ALU.mult),
                   reads=["wstage", "g1col"], writes=[("wbf", b)])
                return wbf[b], ("wbf", b), tot

            for ci in range(48):
                op("pool", lambda e, ci=ci: e.tensor_scalar(out=diagw[:, ci, :], in0=ident_f, scalar1=cw[:, ci:ci + 1],
                                                            scalar2=None, op0=ALU.mult),
                   reads=["ident_f", "cw"], writes=[("diagw", ci)])
            op("pool", lambda e: e.memset(zqkvT[:, :, 0:4], 0.0), writes=["zpad"])

            cva = Alloc(R_WORK, ARENA_BYTES)
            convtmp = [cva([128, 512], BF16) for _ in range(2)]
            evq = [0]

            def evac_copy(out, in_, reads, writes):
                evq[0] += 1
                if evq[0] % 2 == 0:
                    op("act", lambda e: e.copy(out=out, in_=in_), reads=reads, writes=writes)
                else:
                    op("dve", lambda e: e.tensor_copy(out=out, in_=in_), reads=reads, writes=writes)

            pbi = [0]

            def g1_proj(c, wt, wkey, ct):
                for tb in range(4):
                    bk = 2 + (pbi[0] % 2)
                    pbi[0] += 1
                    for k in range(8):
                        op("pe", lambda e, k=k, tb=tb, bk=bk: e.matmul(
                            PB(bk), lhsT=wt[:, k, ct * 128:(ct + 1) * 128], rhs=hT[:, k, tb * 512:(tb + 1) * 512],
                            start=(k == 0), stop=(k == 7)),
                           reads=[wkey] + ALL_HT[tb * 4:tb * 4 + 4], writes=[("pb", bk)])
                    evac_copy(zqkvT[:, c, 4 + tb * 512:4 + (tb + 1) * 512], PB(bk), [("pb", bk)], [("zq", c, tb)])

            def g1_conv(c):
                for tb in range(4):
                    bk = 4 + (tb % 2)
                    for j in range(4):
                        op("pe", lambda e, tb=tb, j=j, bk=bk: e.matmul(
                            PB(bk), lhsT=diagw[:, c * 4 + j, :], rhs=zqkvT[:, c, tb * 512 + j + 1:tb * 512 + j + 1 + 512],
                            start=(j == 0), stop=(j == 3)),
                           reads=[("diagw", c * 4 + j), ("zq", c, tb), "zpad"] + ([("zq", c, tb - 1)] if tb > 0 else []),
                           writes=[("pb", bk)])
                    ctb = tb % 2
                    op("act", lambda e, bk=bk, ctb=ctb: e.activation(out=convtmp[ctb], in_=PB(bk), func=AF.Silu),
                       reads=[("pb", bk)], writes=[("convtmp", ctb)])
                    tbk = 6 + (tb % 2)
                    for q in range(4):
                        op("pe", lambda e, q=q, ctb=ctb, tbk=tbk: e.transpose(out=PBb(tbk)[:, q * 128:(q + 1) * 128],
                                                                              in_=convtmp[ctb][:, q * 128:(q + 1) * 128],
                                                                              identity=ident_b),
                           reads=[("convtmp", ctb), "ident_b"], writes=[("pb", tbk)])
                    op("dve", lambda e, tb=tb, tbk=tbk: e.tensor_copy(
                        out=qkv_tok[:, tb * 4:(tb + 1) * 4, c * 128:(c + 1) * 128],
                        in_=PBb(tbk)[:, 0:512].rearrange("p (q t) -> p q t", q=4)),
                       reads=[("pb", tbk)], writes=[("qkv_tok", tb * 4 + q, c) for q in range(4)])

            prev_c = None
            nxt_w = load_w([(0, 256)])
            for chunk in range(6):
                wt, wkey, _ = nxt_w
                for ct in range(2):
                    c = chunk * 2 + ct
                    g1_proj(c, wt, wkey, ct)
                    if ct == 0:
                        nxt_w = load_w([((chunk + 1) * 256, 256)]) if chunk + 1 < 6 else load_w([(C_AZ, 256)])
                    if prev_c is not None:
                        g1_conv(prev_c)
                    prev_c = c
            g1_conv(prev_c)
            pending_w = [nxt_w]

            if "qkv_tok" in dbg:
                op("sp", lambda e: e.dma_start(out=dbg["qkv_tok"].rearrange("(t p) c -> p t c", p=128), in_=qkv_tok),
                   reads=[("qkv_tok", tt, c) for tt in range(NT) for c in range(12)], writes=["dbg_qkv_tok"], dma=True)
                op("sp", None, reads=["dbg_qkv_tok"])
            sch.barrier()
            if stop_after == "g1":
                return

            rwa = Alloc(cva.off, ARENA_BYTES)
            zr = [rwa([128, 256], F32) for _ in range(2)]
            rt = [rwa([128, 4, 32], F32) for _ in range(4)]
            roped = [rwa([128, 256], BF16) for _ in range(2)]
            op("pool", lambda e: e.memset(bv_tok, 1.0), writes=["bv_ones"])
            for a_ in range(2):
                for b_ in range(2):
                    op("pool", lambda e, a_=a_, b_=b_: e.memset(kz[a_][b_], 0.0), writes=["kz0"])

            def rope_ops(src, nh, dst_views, tt, rkey, wkeys, b):
                sv = src.rearrange("p (h d) -> p h d", h=nh)
                x1 = sv[:, :, 0:32]
                x2 = sv[:, :, 32:64]
                cc = cs[:, tt, 0:32].unsqueeze(1).to_broadcast([128, nh, 32])
                sn = cs[:, tt, 32:64].unsqueeze(1).to_broadcast([128, nh, 32])
                t = [r[:, 0:nh, :] for r in rt]
                op("dve", lambda e: e.tensor_tensor(out=t[0], in0=x1, in1=cc, op=ALU.mult), reads=[rkey, "cs"], writes=[("rt", 0)])
                op("dve", lambda e: e.tensor_tensor(out=t[1], in0=x2, in1=sn, op=ALU.mult), reads=[rkey, "cs"], writes=[("rt", 1)])
                op("dve", lambda e: e.tensor_tensor(out=t[2], in0=x2, in1=cc, op=ALU.mult), reads=[rkey, "cs"], writes=[("rt", 2)])
                op("dve", lambda e: e.tensor_tensor(out=t[3], in0=x1, in1=sn, op=ALU.mult), reads=[rkey, "cs"], writes=[("rt", 3)])
                for i, dv in enumerate(dst_views):
                    eng = "dve" if i % 2 == 0 else "pool"
                    op(eng, lambda e, dv=dv: e.tensor_tensor(out=dv[:, :, 0:32], in0=t[0], in1=t[1], op=ALU.subtract),
                       reads=[("rt", 0), ("rt", 1)], writes=wkeys)
                    op(eng, lambda e, dv=dv: e.tensor_tensor(out=dv[:, :, 32:64], in0=t[2], in1=t[3], op=ALU.add),
                       reads=[("rt", 2), ("rt", 3)], writes=wkeys)

            def tok_chunk(ranges, handler, sel=None, next_ranges=None):
                if pending_w[0] is not None:
                    wt, wkey, tot = pending_w[0]
                    pending_w[0] = None
                else:
                    wt, wkey, tot = load_w(ranges)
                lo, hi = (0, tot) if sel is None else sel
                pend = []
                for tt in range(NT):
                    if tt == 6 and next_ranges is not None:
                        pending_w[0] = load_w(next_ranges)
                    bk = 2 + (tt % 2)
                    for k in range(8):
                        op("pe", lambda e, k=k, tt=tt, bk=bk, wt=wt: e.matmul(
                            PB(bk)[:, 0:hi - lo], lhsT=hT[:, k, tt * 128:(tt + 1) * 128], rhs=wt[:, k, lo:hi],
                            start=(k == 0), stop=(k == 7)),
                           reads=[wkey, ("hT", tt)], writes=[("pb", bk)])
                    if tt >= 1:
                        pend.append(handler(tt - 1, 2 + ((tt - 1) % 2)))
                    if len(pend) >= 2:
                        p2 = pend.pop(0)
                        if p2 is not None:
                            p2()
                pend.append(handler(NT - 1, 2 + ((NT - 1) % 2)))
                for p2 in pend:
                    if p2 is not None:
                        p2()

            for j in range(2):
                def h_az(tt, bk, j=j):
                    op("act", lambda e: e.activation(out=azs[:, tt, j * 256:(j + 1) * 256], in_=PB(bk)[:, 0:256], func=AF.Silu),
                       reads=[("pb", bk)], writes=[("azs", tt, j)])
                tok_chunk([(C_AZ + j * 256, 256)], h_az, next_ranges=[(C_AZ + 256, 256)] if j == 0 else [(C_BQ, 256)])

            if stop_after == "u1":
                return
            for (c0, dstT, nm) in ((C_BQ, bqT, "bqT"), (C_IQ, iqT, "iqT")):
                for j in range(2):
                    def h_q(tt, bk, j=j, dstT=dstT, nm=nm):
                        b = tt % 2
                        op("act", lambda e: e.copy(out=zr[b], in_=PB(bk)[:, 0:256]), reads=[("pb", bk)], writes=[("zr", b)])
                        rope_ops(zr[b], 4, [roped[b].rearrange("p (h d) -> p h d", h=4)], tt, ("zr", b), [("roped", b)], b)
                        tbk = 6 + b

                        def part2():
                            for q in range(2):
                                op("pe", lambda e, q=q: e.transpose(out=PBb(tbk)[:, q * 128:(q + 1) * 128],
                                                                    in_=roped[b][:, q * 128:(q + 1) * 128], identity=ident_b),
                                   reads=[("roped", b), "ident_b"], writes=[("pb", tbk)])
                            op("act", lambda e: e.copy(out=dstT[:, 2 * j:2 * j + 2, tt * 128:(tt + 1) * 128],
                                                       in_=PBb(tbk)[:, 0:256].rearrange("p (q t) -> p q t", q=2)),
                               reads=[("pb", tbk)], writes=[(nm, tt, j)])
                        return part2
                    nr = [(c0 + 256, 256)] if j == 0 else ([(C_IQ, 256)] if c0 == C_BQ else [(C_BK, 256)])
                    tok_chunk([(c0 + j * 256, 256)], h_q, next_ranges=nr)

            if stop_after == "u23":
                return
            def h_kv(tt, bk):
                b = tt % 2
                op("act", lambda e: e.copy(out=zr[b], in_=PB(bk)[:, 0:256]), reads=[("pb", bk)], writes=[("zr", b)])
                rv = roped[b].rearrange("p (h d) -> p h d", h=4)
                rope_ops(zr[b][:, 0:128], 2, [rv[:, 0:2, :]], tt, ("zr", b), [("roped", b)], b)
                op("pool", lambda e: e.tensor_copy(out=rv[:, 2, :], in_=rv[:, 1, :]), reads=[("roped", b)], writes=[("roped", b)])
                op("pool", lambda e: e.tensor_copy(out=rv[:, 3, :], in_=rv[:, 0, :]), reads=[("roped", b)], writes=[("roped", b)])
                def part2():
                    tbk = 6 + b
                    for q in range(2):
                        op("pe", lambda e, q=q: e.transpose(out=PBb(tbk)[:, q * 128:(q + 1) * 128],
                                                            in_=roped[b][:, q * 128:(q + 1) * 128], identity=ident_b),
                           reads=[("roped", b), "ident_b"], writes=[("pb", tbk)])
                    ts_ = slice(tt * 128, (tt + 1) * 128)
                    op("act", lambda e: e.copy(out=kz[0][0][0:64, ts_], in_=PBb(tbk)[0:64, 0:128]), reads=[("pb", tbk), "kz0"],
                       writes=[("bkT", tt)])
                    op("act", lambda e: e.copy(out=kz[1][1][64:128, ts_], in_=PBb(tbk)[64:128, 0:128]), reads=[("pb", tbk)],
                       writes=[("bkT", tt)])
                    op("act", lambda e: e.copy(out=kz[1][0][0:64, ts_], in_=PBb(tbk)[0:64, 128:256]), reads=[("pb", tbk)],
                       writes=[("bkT", tt)])
                    op("act", lambda e: e.copy(out=kz[0][1][64:128, ts_], in_=PBb(tbk)[64:128, 128:256]), reads=[("pb", tbk)],
                       writes=[("bkT", tt)])

                op("dve", lambda e: e.tensor_copy(out=bv_tok[:, tt, 0:64], in_=zr[b][:, 128:192]), reads=[("zr", b), "bv_ones"],
                   writes=[("bv", tt)])
                op("dve", lambda e: e.tensor_copy(out=bv_tok[:, tt, 65:129], in_=zr[b][:, 192:256]), reads=[("zr", b)],
                   writes=[("bv", tt)])
                return part2
            tok_chunk([(C_BK, 256)], h_kv, next_ranges=[(C_IW + 8 - 256, 256)])

            if stop_after == "u4a":
                return
            IW_SCALE = (8 ** -0.5) * (64 ** -0.5)

            def h_small(tt, bk):
                b = tt % 2
                op("act", lambda e: e.copy(out=zr[b][:, 0:72], in_=PB(bk)[:, 0:72]), reads=[("pb", bk)], writes=[("zr", b)])
                rv = roped[b].rearrange("p (h d) -> p h d", h=4)
                rope_ops(zr[b][:, 0:64], 1, [rv[:, 0:1, :], rv[:, 1:2, :]], tt, ("zr", b), [("roped", b)], b)
                def part2():
                    tbk = 6 + b
                    op("pe", lambda e: e.transpose(out=PBb(tbk)[:, 0:128], in_=roped[b][:, 0:128], identity=ident_b),
                       reads=[("roped", b), "ident_b"], writes=[("pb", tbk)])
                    op("act", lambda e: e.copy(out=ikT2[:, tt * 128:(tt + 1) * 128], in_=PBb(tbk)[:, 0:128]),
                       reads=[("pb", tbk)], writes=[("ikT", tt)])

                op("dve", lambda e: e.tensor_scalar(out=iw_tok[:, tt, :], in0=zr[b][:, 64:72], scalar1=IW_SCALE, scalar2=None,
                                                    op0=ALU.mult), reads=[("zr", b)], writes=[("iw", tt)])
                return part2
            tok_chunk([(C_IW + 8 - 256, 256)], h_small, sel=(184, 256), next_ranges=[(C_BETA, 256)])

            def h_ab(tt, bk):
                op("act", lambda e: e.copy(out=ab_tok[:, tt, :], in_=PB(bk)[:, 0:16]), reads=[("pb", bk)], writes=[("ab", tt)])
            tok_chunk([(C_BETA, 256)], h_ab, sel=(0, 16))

            for nm, t_, shape in (("bqT", bqT, None), ("iqT", iqT, None)):
                if nm in dbg:
                    op("sp", lambda e, nm=nm, t_=t_: e.dma_start(out=dbg[nm].rearrange("(a p) t -> p a t", p=128), in_=t_),
                       reads=[(nm, tt, j) for tt in range(NT) for j in range(2)], writes=["dbg_" + nm], dma=True)
                    op("sp", None, reads=["dbg_" + nm])
            if "misc" in dbg:
                sch.barrier()
                mt = view(R_W, [128, NT, 154], F32)
                op("dve", lambda e: e.tensor_copy(out=mt[:, :, 0:16], in_=ab_tok), reads=[("ab", tt) for tt in range(NT)], writes=["mt"])
                op("dve", lambda e: e.tensor_copy(out=mt[:, :, 16:24], in_=iw_tok), reads=[("iw", tt) for tt in range(NT)], writes=["mt"])
                op("dve", lambda e: e.tensor_copy(out=mt[:, :, 24:154], in_=bv_tok), reads=[("bv", tt) for tt in range(NT)], writes=["mt"])
                op("sp", lambda e: e.dma_start(out=dbg["misc"].rearrange("(t p) c -> p t c", p=128), in_=mt), reads=["mt"],
                   writes=["dbg_misc"], dma=True)
                op("sp", None, reads=["dbg_misc"])
            sch.barrier()
            if stop_after == "p2":
                return

            class MultiAlloc:
                def __init__(self, regions):
                    self.regs = [[a, b] for a, b in regions]

                def __call__(self, shape, dt):
                    n = 1
                    for s_ in shape[1:]:
                        n *= s_
                    size = (n * DT_SIZE[dt] + 63) // 64 * 64
                    for r in self.regs:
                        r[0] = (r[0] + 63) // 64 * 64
                        if r[0] + size <= r[1]:
                            v = view(r[0], shape, dt)
                            r[0] += size
                            return v
                    raise AssertionError(("MultiAlloc out of space", shape, self.regs))

            def dump(name, ap, reads):
                if name in dbg:
                    op("sp", lambda e: e.dma_start(out=dbg[name], in_=ap), reads=reads, writes=["dbg_" + name], dma=True)
                    op("sp", None, reads=["dbg_" + name])

            o_aT = view(R_A, [128, 4, S], BF16)
            diagw_off = R_WORK - 12 * K
            ga = MultiAlloc([(R_W, R_W + 16 * K), (R_A + 16 * K, R_A + 32 * K), (diagw_off, ARENA_BYTES)])
            g_all = ga([128, NT, 8], F32)
            bet = ga([128, NT, 8], F32)
            gs = ga([128, 24], F32)
            eG = ga([128, 8], F32)
            eGlmG = ga([128, 8], F32)
            scs = [ga([128, 4], F32) for _ in range(2)]
            g_bc = ga([128, 8, 128], F32)
            sq = ga([128, 1024], F32)
            ssn = ga([128, 16], F32)
            rn = ga([128, 16], F32)
            cq = ga([128, 8], F32)
            cqd = ga([128, 8], F32)
            cbk = ga([128, 8], F32)
            ckd = ga([128, 8], F32)
            negbeta = ga([128, 8], F32)
            qn = ga([128, 512], BF16)
            qd = ga([128, 512], BF16)
            kn = ga([128, 512], BF16)
            rhsk = ga([128, 512], BF16)
            kdec = ga([128, 512], BF16)
            rhsv = ga([128, 512], BF16)
            qnT = ga([128, 4, 128], BF16)
            qdT = ga([128, 4, 128], BF16)
            knT = ga([128, 4, 128], BF16)
            Dm = ga([128, 8, 128], BF16)
            Ds = ga([128, 8, 128], BF16)
            Mm = [ga([128, 8, 128], BF16) for _ in range(2)]
            Nm = [ga([128, 8, 128], BF16) for _ in range(2)]
            Pm = [ga([128, 8, 128], BF16) for _ in range(2)]
            qkm = ga([128, 8, 128], BF16)
            qkT_sb = ga([128, 8, 128], BF16)
            u_c = ga([128, 2, 512], F32)
            w_tok = ga([128, 512], BF16)
            wT_sb = ga([128, 4, 128], BF16)
            vn_b = ga([128, 512], BF16)
            Sst = ga([128, 4, 128], F32)
            Stmp = ga([128, 4, 128], F32)
            S_bd = ga([128, 4, 128], BF16)
            bdmask = ga([128, 4, 128], BF16)
            o_c = ga([128, 512], F32)
            qkT_c1 = ga([128, 8, 64], BF16)
            kdec_c1 = ga([128, 512], BF16)
            az_c = ga([128, 512], BF16)
            ss2 = ga([128, 8], F32)
            r2 = ga([128, 8], F32)
            oa_b = ga([128, 512], BF16)

            def bc8(v):
                return v.unsqueeze(2).to_broadcast([128, 8, 64])

            ABK = [("ab", tt) for tt in range(NT)]
            op("act", lambda e: e.activation(out=bet, in_=ab_tok[:, :, 0:8], func=AF.Sigmoid), reads=["ab_all"], writes=["bet"])
            op("dve", lambda e: e.tensor_tensor(out=g_all, in0=ab_tok[:, :, 8:16], in1=dtb.unsqueeze(1).to_broadcast([128, NT, 8]),
                                                op=ALU.add), reads=["ab_all", "dtb"], writes=["g_all"])
            op("act", lambda e: e.activation(out=g_all, in_=g_all, func=AF.Exp), reads=["g_all"], writes=["g_all"])
            op("act", lambda e: e.activation(out=g_all, in_=g_all, func=AF.Ln, bias=1.0), reads=["g_all"], writes=["g_all"])
            op("dve", lambda e: e.tensor_tensor(out=g_all, in0=g_all, in1=negA.unsqueeze(1).to_broadcast([128, NT, 8]),
                                                op=ALU.mult), reads=["g_all", "negA"], writes=["g_all"])
            op("dve", lambda e: e.memset(Sst, 0.0), writes=["S"])
            op("dve", lambda e: e.memset(S_bd, 0.0), writes=["S_bd"])
            op("pool", lambda e: e.memset(bdmask, 0.0), writes=["bdmask"])
            op("pool", lambda e: e.memset(bdmask[0:64, :, 0:64], 1.0), writes=["bdmask"])
            op("pool", lambda e: e.memset(bdmask[64:128, :, 64:128], 1.0), writes=["bdmask"])
            if "g" in dbg:
                op("sp", lambda e: e.dma_start(out=dbg["g"].rearrange("(t p) c -> p t c", p=128), in_=g_all), reads=["g_all"],
                   writes=["dbg_g"], dma=True)
                op("sp", None, reads=["dbg_g"])

            if stop_after == "gdn_pre":
                return
            for tt in range(NT):
                op("pe", lambda e, tt=tt: e.matmul(PB(0)[:, 0:8], lhsT=ucs_f, rhs=g_all[:, tt, :], start=True, stop=True),
                   reads=["g_all"], writes=[("pb", 0)])
                op("pe", lambda e, tt=tt: e.matmul(PB(0)[:, 8:16], lhsT=mc0_f, rhs=g_all[:, tt, :], start=True, stop=True),
                   reads=["g_all"], writes=[("pb", 0)])
                op("pe", lambda e, tt=tt: e.matmul(PB(0)[:, 16:24], lhsT=mc1_f, rhs=g_all[:, tt, :], start=True, stop=True),
                   reads=["g_all"], writes=[("pb", 0)])
                op("act", lambda e: e.copy(out=gs, in_=PB(0)[:, 0:24]), reads=[("pb", 0)], writes=["gs"])
                op("act", lambda e: e.activation(out=eG, in_=gs[:, 0:8], func=AF.Exp), reads=["gs"], writes=["eG"])
                op("dve", lambda e: e.tensor_tensor(out=eGlmG[0:64, :], in0=gs[0:64, 8:16], in1=gs[0:64, 0:8], op=ALU.subtract),
                   reads=["gs"], writes=["eGlmG"])
                op("dve", lambda e: e.tensor_tensor(out=eGlmG[64:128, :], in0=gs[64:128, 16:24], in1=gs[64:128, 0:8],
                                                    op=ALU.subtract), reads=["gs"], writes=["eGlmG"])
                op("act", lambda e: e.activation(out=eGlmG, in_=eGlmG, func=AF.Exp), reads=["eGlmG"], writes=["eGlmG"])
                for hf in range(2):
                    c0 = 8 + 8 * hf
                    op("act", lambda e, hf=hf, c0=c0: e.activation(out=scs[hf][0:64, :], in_=gs[0:64, c0:c0 + 8:2], func=AF.Exp),
                       reads=["gs"], writes=[("scs", hf)])
                    op("act", lambda e, hf=hf, c0=c0: e.activation(out=scs[hf][64:128, :], in_=gs[64:128, c0 + 1:c0 + 8:2],
                                                                   func=AF.Exp), reads=["gs"], writes=[("scs", hf)])
                op("dve", lambda e, tt=tt: e.tensor_scalar(out=g_bc, in0=g_all[:, tt, :].unsqueeze(2).to_broadcast([128, 8, 128]),
                                                           scalar1=-1.0, scalar2=None, op0=ALU.mult),
                   reads=["g_all"], writes=["g_bc"])
                if stop_after == "gdn_a":
                    return
                QK = [("qkv_tok", tt, c) for c in range(8)]
                VV = [("qkv_tok", tt, c) for c in range(8, 12)]
                op("dve", lambda e, tt=tt: e.tensor_tensor(out=sq, in0=qkv_tok[:, tt, 0:1024], in1=qkv_tok[:, tt, 0:1024],
                                                           op=ALU.mult), reads=["qkv_all"], writes=["sq"])
                op("dve", lambda e: e.tensor_reduce(out=ssn, in_=sq.rearrange("p (h d) -> p h d", h=16), axis=AX.X, op=ALU.add),
                   reads=["sq"], writes=["ssn"])
                op("act", lambda e: e.activation(out=rn, in_=ssn, func=AF.Ln, bias=epsc, scale=1.0), reads=["ssn", "epsc"],
                   writes=["rn"])
                op("act", lambda e: e.activation(out=rn, in_=rn, func=AF.Exp, scale=-0.5), reads=["rn"], writes=["rn"])
                op("dve", lambda e: e.tensor_scalar(out=cq, in0=rn[:, 0:8], scalar1=0.125, scalar2=None, op0=ALU.mult),
                   reads=["rn"], writes=["cq"])
                op("dve", lambda e: e.tensor_tensor(out=cqd, in0=cq, in1=eG, op=ALU.mult), reads=["cq", "eG"], writes=["cqd"])
                op("dve", lambda e, tt=tt: e.tensor_tensor(out=cbk, in0=rn[:, 8:16], in1=bet[:, tt, :], op=ALU.mult),
                   reads=["rn", "bet"], writes=["cbk"])
                op("dve", lambda e: e.tensor_tensor(out=cbk, in0=cbk, in1=eG, op=ALU.mult), reads=["cbk", "eG"], writes=["cbk"])
                op("dve", lambda e: e.tensor_tensor(out=ckd, in0=rn[:, 8:16], in1=eGlmG, op=ALU.mult), reads=["rn", "eGlmG"],
                   writes=["ckd"])
                op("dve", lambda e, tt=tt: e.tensor_scalar(out=negbeta, in0=bet[:, tt, :], scalar1=-1.0, scalar2=None,
                                                           op0=ALU.mult), reads=["bet"], writes=["negbeta"])
                qv = qkv_tok[:, tt, 0:512].rearrange("p (h d) -> p h d", h=8)
                kv = qkv_tok[:, tt, 512:1024].rearrange("p (h d) -> p h d", h=8)
                vv = qkv_tok[:, tt, 1024:1536].rearrange("p (h d) -> p h d", h=8)

                def v3(t_):
                    return t_.rearrange("p (h d) -> p h d", h=8)
                op("dve", lambda e, qv=qv: e.tensor_tensor(out=v3(qn), in0=qv, in1=bc8(cq), op=ALU.mult),
                   reads=["qkv_all", "cq"], writes=["qn"])
                op("dve", lambda e, qv=qv: e.tensor_tensor(out=v3(qd), in0=qv, in1=bc8(cqd), op=ALU.mult),
                   reads=["qkv_all", "cqd"], writes=["qd"])
                op("dve", lambda e, kv=kv: e.tensor_tensor(out=v3(kn), in0=kv, in1=bc8(rn[:, 8:16]), op=ALU.mult),
                   reads=["qkv_all", "rn"], writes=["kn"])
                op("dve", lambda e, kv=kv: e.tensor_tensor(out=v3(rhsk), in0=kv, in1=bc8(cbk), op=ALU.mult),
                   reads=["qkv_all", "cbk"], writes=["rhsk"])
                op("dve", lambda e, kv=kv: e.tensor_tensor(out=v3(kdec), in0=kv, in1=bc8(ckd), op=ALU.mult),
                   reads=["qkv_all", "ckd"], writes=["kdec"])
                op("dve", lambda e, vv=vv, tt=tt: e.tensor_tensor(out=v3(rhsv), in0=vv, in1=bc8(bet[:, tt, :]), op=ALU.mult),
                   reads=["qkv_all", "bet"], writes=["rhsv"])
                if stop_after == "gdn_b":
                    return
                for (src, skey, dst, dkey, bank, coff, eng) in ((qn, "qn", qnT, "qnT", 6, 0, "act"), (qd, "qd", qdT, "qdT", 7, 0, "dve"),
                                                                (kn, "kn", knT, "knT", 0, 0, "act")):
                    for q in range(4):
                        op("pe", lambda e, src=src, bank=bank, coff=coff, q=q: e.transpose(
                            out=PBb(bank)[:, coff + q * 128:coff + (q + 1) * 128], in_=src[:, q * 128:(q + 1) * 128], identity=ident_b),
                           reads=[skey, "ident_b"], writes=[("pb", bank)])
                    if eng == "act":
                        op("act", lambda e, dst=dst, bank=bank, coff=coff: e.copy(
                            out=dst, in_=PBb(bank)[:, coff:coff + 512].rearrange("p (q t) -> p q t", q=4)),
                           reads=[("pb", bank)], writes=[dkey])
                    else:
                        op("dve", lambda e, dst=dst, bank=bank, coff=coff: e.tensor_copy(
                            out=dst, in_=PBb(bank)[:, coff:coff + 512].rearrange("p (q t) -> p q t", q=4)),
                           reads=[("pb", bank)], writes=[dkey])
                    if stop_after == "gdn_c_" + skey:
                        return
                if tt == 0:
                    dump("qn0", qn, ["qn"]); dump("kn0", kn, ["kn"]); dump("rhsv0", rhsv, ["rhsv"]); dump("rhsk0", rhsk, ["rhsk"])
                    dump("kdec0", kdec, ["kdec"]); dump("qd0", qd, ["qd"]); dump("gs0", gs, ["gs"])
                    dump("knT0", knT.rearrange("p a t -> p (a t)"), ["knT"])
                    dump("rn0", rn, ["rn"]); dump("cq0", cq, ["cq"]); dump("cqd0", cqd, ["cqd"]); dump("cbk0", cbk, ["cbk"])
                    dump("ckd0", ckd, ["ckd"]); dump("eG0", eG, ["eG"]); dump("ssn0", ssn, ["ssn"])
                if stop_after == "gdn_c":
                    return
                def group_gen(hg, bA, bB, bC, bT):
                    hs_list = list(range(4))
                    grp = slice(4 * hg, 4 * hg + 4)
                    for hs in hs_list:
                        h = 4 * hg + hs
                        hp, par = h // 2, h % 2
                        rows = slice(par * 64, par * 64 + 64)
                        cs_ = slice(hs * 128, hs * 128 + 128)
                        op("pe", lambda e, hp=hp, rows=rows, cs_=cs_, par=par: e.matmul(
                            PB(bA)[:, cs_], lhsT=knT[rows, hp, :], rhs=knT[rows, hp, :], start=True, stop=True,
                            tile_position=(par * 64, 0)), reads=["knT"], writes=[("pb", bA)])
                        op("pe", lambda e, hp=hp, rows=rows, cs_=cs_, par=par: e.matmul(
                            PB(bB)[:, cs_], lhsT=qnT[rows, hp, :], rhs=knT[rows, hp, :], start=True, stop=True,
                            tile_position=(par * 64, 0)), reads=["knT", "qnT"], writes=[("pb", bB)])
                        op("pe", lambda e, h=h, cs_=cs_: e.matmul(PB(bC)[:, cs_], lhsT=g_bc[:, h, :], rhs=ucs_f, start=True, stop=False),
                           reads=["g_bc", "ucs_f"], writes=[("pb", bC)])
                        op("pe", lambda e, cs_=cs_: e.matmul(PB(bC)[:, cs_], lhsT=ident_f, rhs=maskneg_f, start=False, stop=True),
                           reads=["ident_f", "maskneg_f"], writes=[("pb", bC)])
                    yield
                    for hs in hs_list:
                        h = 4 * hg + hs
                        cs_ = slice(hs * 128, hs * 128 + 128)
                        op("act", lambda e, h=h, cs_=cs_: e.activation(out=Dm[:, h, :], in_=PB(bC)[:, cs_], func=AF.Exp,
                                                                       bias=gs[:, h:h + 1], scale=1.0),
                           reads=[("pb", bC), "gs"], writes=[("Dm", h)])
                        op("dve", lambda e, h=h: e.tensor_tensor(out=Ds[:, h, :], in0=Dm[:, h, :], in1=strict_b, op=ALU.mult),
                           reads=[("Dm", h), "strict_b"], writes=[("Ds", h)])
                        op("dve", lambda e, h=h, cs_=cs_: e.scalar_tensor_tensor(out=Mm[0][:, h, :], in0=PB(bA)[:, cs_],
                                                                                 scalar=negbeta[:, h:h + 1], in1=Ds[:, h, :],
                                                                                 op0=ALU.mult, op1=ALU.mult),
                           reads=[("pb", bA), "negbeta", ("Ds", h)], writes=[("M", 0, hg)])
                        op("dve", lambda e, h=h, cs_=cs_: e.tensor_tensor(out=qkm[:, h, :], in0=PB(bB)[:, cs_], in1=Dm[:, h, :],
                                                                          op=ALU.mult),
                           reads=[("pb", bB), ("Dm", h)], writes=[("qkm", hg)])
                    yield
                    for hs in hs_list:
                        h = 4 * hg + hs
                        cs_ = slice(hs * 128, hs * 128 + 128)
                        op("pe", lambda e, h=h, cs_=cs_: e.transpose(out=PBb(bT)[:, cs_], in_=Mm[0][:, h, :], identity=ident_b),
                           reads=[("M", 0, hg), "ident_b"], writes=[("pb", bT)])
                    op("act", lambda e: e.copy(out=Nm[0][:, grp, :], in_=PBb(bT)[:, 0:512].rearrange("p (q t) -> p q t", q=4)),
                       reads=[("pb", bT)], writes=[("N", 0, hg)])
                    op("dve", lambda e: e.tensor_tensor(out=Pm[0][:, grp, :], in0=Nm[0][:, grp, :],
                                                        in1=ident_b.unsqueeze(1).to_broadcast([128, 4, 128]), op=ALU.add),
                       reads=[("N", 0, hg), "ident_b"], writes=[("P", 0, hg)])
                    yield
                    for hs in hs_list:
                        h = 4 * hg + hs
                        cs2 = slice(hs * 128, hs * 128 + 128)
                        op("pe", lambda e, h=h, cs2=cs2: e.transpose(out=PBb(bT)[:, cs2], in_=qkm[:, h, :], identity=ident_b),
                           reads=[("qkm", hg), "ident_b"], writes=[("pb", bT)])
                    op("dve", lambda e: e.tensor_copy(out=qkT_sb[:, grp, :], in_=PBb(bT)[:, 0:512].rearrange("p (q t) -> p q t", q=4)),
                       reads=[("pb", bT)], writes=[("qkT", hg)])
                    yield
                    for lv in range(1, 6):
                        cur, nxt = (lv - 1) % 2, lv % 2
                        for hs in hs_list:
                            h = 4 * hg + hs
                            cs_ = slice(hs * 128, hs * 128 + 128)
                            op("pe", lambda e, h=h, cs_=cs_, cur=cur: e.matmul(PB(bA)[:, cs_], lhsT=Nm[cur][:, h, :], rhs=Mm[cur][:, h, :],
                                                                               start=True, stop=True),
                               reads=[("N", cur, hg), ("M", cur, hg)], writes=[("pb", bA)])
                        if lv < 5:
                            for hs in hs_list:
                                h = 4 * hg + hs
                                cs_ = slice(hs * 128, hs * 128 + 128)
                                op("pe", lambda e, h=h, cs_=cs_, cur=cur: e.matmul(PB(bB)[:, cs_], lhsT=Mm[cur][:, h, :],
                                                                                   rhs=Nm[cur][:, h, :], start=True, stop=True),
                                   reads=[("N", cur, hg), ("M", cur, hg)], writes=[("pb", bB)])
                        yield
                        op("act", lambda e, nxt=nxt: e.copy(out=Mm[nxt][:, grp, :], in_=PB(bA).rearrange("p (q t) -> p q t", q=4)),
                           reads=[("pb", bA)], writes=[("M", nxt, hg)])
                        if lv < 5:
                            op("dve", lambda e, nxt=nxt: e.tensor_copy(out=Nm[nxt][:, grp, :],
                                                                       in_=PB(bB).rearrange("p (q t) -> p q t", q=4)),
                               reads=[("pb", bB)], writes=[("N", nxt, hg)])
                        for hs in hs_list:
                            h = 4 * hg + hs
                            cs_ = slice(hs * 128, hs * 128 + 128)
                            op("pe", lambda e, h=h, cs_=cs_, cur=cur, nxt=nxt: e.matmul(PB(bC)[:, cs_], lhsT=Mm[nxt][:, h, :],
                                                                                        rhs=Pm[cur][:, h, :], start=True, stop=True),
                               reads=[("M", nxt, hg), ("P", cur, hg)], writes=[("pb", bC)])
                        yield
                        op("dve", lambda e, cur=cur, nxt=nxt: e.tensor_tensor(
                            out=Pm[nxt][:, grp, :], in0=Pm[cur][:, grp, :], in1=PB(bC).rearrange("p (q t) -> p q t", q=4), op=ALU.add),
                           reads=[("pb", bC), ("P", cur, hg)], writes=[("P", nxt, hg)])

                gens = [group_gen(0, 3, 4, 5, 6), group_gen(1, 0, 1, 2, 7)]
                while gens:
                    for g_ in list(gens):
                        try:
                            next(g_)
                        except StopIteration:
                            gens.remove(g_)
                Pf = Pm[1]
                PK = [("P", 1, 0), ("P", 1, 1)]
                if tt == 0:
                    dump("D0", Dm.rearrange("p a t -> p (a t)"), [("Dm", h) for h in range(8)])
                    dump("M0", Mm[0].rearrange("p a t -> p (a t)"), [("M", 0, 0), ("M", 0, 1)])
                    dump("N0", Nm[0].rearrange("p a t -> p (a t)"), [("N", 0, 0), ("N", 0, 1)])
                    dump("P0", Pm[1].rearrange("p a t -> p (a t)"), [("P", 1, 0), ("P", 1, 1)])
                    dump("qkT0", qkT_sb.rearrange("p a t -> p (a t)"), [("qkT", 0), ("qkT", 1)])
                if stop_after == "gdn_e":
                    return
                for hf in range(2):
                    ub = 7 if hf == 0 else 0
                    for h in range(8):
                        op("pe", lambda e, h=h, hf=hf, ub=ub: e.matmul(PB(ub)[0:64, h * 64:(h + 1) * 64],
                                                                       lhsT=Pf[:, h, hf * 64:(hf + 1) * 64],
                                                                       rhs=rhsv[:, h * 64:(h + 1) * 64], start=True, stop=True),
                           reads=PK + ["rhsv"], writes=[("pb", ub)])
                    op("act", lambda e, hf=hf, ub=ub: e.copy(out=u_c[0:64, hf, :], in_=PB(ub)[0:64, :]), reads=[("pb", ub)],
                       writes=[("u_c", hf)])
                for h in range(8):
                    op("pe", lambda e, h=h: e.matmul(PB(1)[:, h * 64:(h + 1) * 64], lhsT=Pf[:, h, :], rhs=rhsk[:, h * 64:(h + 1) * 64],
                                                     start=True, stop=True), reads=PK + ["rhsk"], writes=[("pb", 1)])
                op("act", lambda e: e.copy(out=w_tok, in_=PB(1)), reads=[("pb", 1)], writes=["w_tok"])
                for q in range(4):
                    op("pe", lambda e, q=q: e.transpose(out=PBb(6)[:, q * 128:(q + 1) * 128], in_=w_tok[:, q * 128:(q + 1) * 128],
                                                        identity=ident_b), reads=["w_tok", "ident_b"], writes=[("pb", 6)])
                op("dve", lambda e: e.tensor_copy(out=wT_sb, in_=PBb(6)[:, 0:512].rearrange("p (q t) -> p q t", q=4)),
                   reads=[("pb", 6)], writes=["wT_sb"])
                op("sp", lambda e: e.dma_start(out=qkT_c1[0:64, :, :], in_=qkT_sb[64:128, :, 64:128]),
                   reads=[("qkT", 0), ("qkT", 1)], writes=["qkT_c1"], dma=True)
                op("sp", lambda e: e.dma_start(out=kdec_c1[0:64, :], in_=kdec[64:128, :]), reads=["kdec"], writes=["kdec_c1"],
                   dma=True)
                op("sp", lambda e, tt=tt: e.dma_start(out=az_c[0:64, :], in_=azs[64:128, tt, :]), reads=["azs_all"], writes=["az_c"],
                   dma=True)
                if tt == 0:
                    dump("u0", u_c.rearrange("p a t -> p (a t)"), [("u_c", 0), ("u_c", 1)])
                    dump("wT0", wT_sb.rearrange("p a t -> p (a t)"), ["wT_sb"])
                if stop_after == "gdn_f":
                    return
                for hf in range(2):
                    tcs = slice(hf * 64, hf * 64 + 64)
                    ck = 2 * tt + hf
                    if hf == 0:
                        qk_x, qk_keys = qkT_sb[0:64, :, 0:64], [("qkT", 0), ("qkT", 1)]
                        kd_x, kd_keys = kdec[0:64, :], ["kdec"]
                    else:
                        qk_x, qk_keys = qkT_c1[0:64, :, :], ["qkT_c1"]
                        kd_x, kd_keys = kdec_c1[0:64, :], ["kdec_c1"]
                    for hp in range(4):
                        op("pe", lambda e, hp=hp, tcs=tcs: e.matmul(PB(1)[0:64, hp * 128:(hp + 1) * 128], lhsT=wT_sb[:, hp, tcs],
                                                                    rhs=S_bd[:, hp, :], start=True, stop=True),
                           reads=["wT_sb", "S_bd"], writes=[("pb", 1)])
                    op("dve", lambda e, hf=hf: e.tensor_tensor(out=vn_b[0:64, :], in0=u_c[0:64, hf, :], in1=PB(1)[0:64, :],
                                                               op=ALU.subtract), reads=[("u_c", hf), ("pb", 1)], writes=["vn_b"])
                    for h in range(8):
                        hp, par = h // 2, h % 2
                        op("pe", lambda e, h=h, hp=hp, par=par, tcs=tcs: e.matmul(
                            PB(2)[0:64, h * 64:(h + 1) * 64], lhsT=qdT[:, hp, tcs], rhs=S_bd[:, hp, par * 64:(par + 1) * 64],
                            start=True, stop=False), reads=["qdT", "S_bd"], writes=[("pb", 2)])
                        op("pe", lambda e, h=h, qk_x=qk_x: e.matmul(
                            PB(2)[0:64, h * 64:(h + 1) * 64], lhsT=qk_x[:, h, :], rhs=vn_b[0:64, h * 64:(h + 1) * 64],
                            start=False, stop=True), reads=qk_keys + ["vn_b"], writes=[("pb", 2)])
                    for hp in range(4):
                        op("pe", lambda e, hp=hp, kd_x=kd_x: e.matmul(PB(7)[:, hp * 128:(hp + 1) * 128],
                                                                      lhsT=kd_x[:, hp * 128:(hp + 1) * 128],
                                                                      rhs=vn_b[0:64, hp * 128:(hp + 1) * 128], start=True, stop=True),
                           reads=kd_keys + ["vn_b"], writes=[("pb", 7)])
                    op("act", lambda e: e.copy(out=o_c[0:64, :], in_=PB(2)[0:64, :]), reads=[("pb", 2)], writes=["o_c"])
                    op("pool", lambda e, hf=hf: e.tensor_tensor(out=Stmp, in0=Sst,
                                                                in1=scs[hf].unsqueeze(2).to_broadcast([128, 4, 128]), op=ALU.mult),
                       reads=["S", ("scs", hf)], writes=["Stmp"])
                    op("dve", lambda e: e.tensor_tensor(out=Sst, in0=Stmp, in1=PB(7).rearrange("p (a d) -> p a d", a=4), op=ALU.add),
                       reads=["Stmp", ("pb", 7)], writes=["S"])
                    op("pool", lambda e: e.tensor_tensor(out=S_bd, in0=Sst, in1=bdmask, op=ALU.mult), reads=["S", "bdmask"],
                       writes=["S_bd"])
                    if "o_raw" in dbg:
                        op("sp", lambda e, ck=ck: e.dma_start(out=dbg["o_raw"][ck * 64:(ck + 1) * 64, :], in_=o_c[0:64, :]),
                           reads=["o_c"], writes=["dbg_o_raw"], dma=True)
                    sqh = sq[0:64, 0:512]
                    op("dve", lambda e: e.tensor_tensor(out=sqh, in0=o_c[0:64, :], in1=o_c[0:64, :], op=ALU.mult), reads=["o_c"],
                       writes=["sq"])
                    op("dve", lambda e: e.tensor_reduce(out=ss2[0:64, :], in_=sqh.rearrange("p (h d) -> p h d", h=8), axis=AX.X,
                                                        op=ALU.add), reads=["sq"], writes=["ss2"])
                    op("act", lambda e: e.activation(out=r2[0:64, :], in_=ss2[0:64, :], func=AF.Ln, bias=epsc[0:64, :],
                                                     scale=1.0 / 64), reads=["ss2", "epsc"], writes=["r2"])
                    op("act", lambda e: e.activation(out=r2[0:64, :], in_=r2[0:64, :], func=AF.Exp, scale=-0.5), reads=["r2"],
                       writes=["r2"])
                    op("dve", lambda e: e.tensor_tensor(out=v3(sqh), in0=v3(o_c[0:64, :]),
                                                        in1=r2[0:64, :].unsqueeze(2).to_broadcast([64, 8, 64]), op=ALU.mult),
                       reads=["o_c", "r2"], writes=["sq"])
                    op("dve", lambda e: e.tensor_tensor(out=v3(sqh), in0=v3(sqh),
                                                        in1=angb[0:64, :].unsqueeze(1).to_broadcast([64, 8, 64]), op=ALU.mult),
                       reads=["sq", "angb"], writes=["sq"])
                    az_x = azs[0:64, tt, :] if hf == 0 else az_c[0:64, :]
                    op("dve", lambda e, az_x=az_x: e.tensor_tensor(out=oa_b[0:64, :], in0=sqh, in1=az_x, op=ALU.mult),
                       reads=["sq", "az_c"], writes=["oa_b"])
                    for q in range(4):
                        op("pe", lambda e, q=q: e.transpose(out=PBb(6)[:, q * 64:(q + 1) * 64], in_=oa_b[0:64, q * 128:(q + 1) * 128],
                                                            identity=ident_b[0:64, 0:64]), reads=["oa_b", "ident_b"],
                           writes=[("pb", 6)])
                    op("act", lambda e, ck=ck: e.copy(out=o_aT[:, :, ck * 64:(ck + 1) * 64],
                                                      in_=PBb(6)[:, 0:256].rearrange("p (q t) -> p q t", q=4)),
                       reads=[("pb", 6)], writes=[("o_aT", ck)])
            if "o_raw" in dbg:
                op("sp", None, reads=["dbg_o_raw"])
            if "o_aT" in dbg:
                op("sp", lambda e: e.dma_start(out=dbg["o_aT"].rearrange("(a p) t -> p a t", p=128), in_=o_aT),
                   reads=[("o_aT", ck) for ck in range(2 * NT)], writes=["dbg_o_aT"], dma=True)
                op("sp", None, reads=["dbg_o_aT"])

            sch.barrier()
            if stop_after == "gdn":
                return

            o_bT = view(R_A + 16 * K, [128, 4, S], BF16)
            da = MultiAlloc([(R_Q, R_Q + 48 * K), (R_W, R_W + 16 * K)])
            scoreb = [da([128, S], F32) for _ in range(2)]
            rl = [da([128, 512], F32) for _ in range(2)]
            maskbb = [da([128, S], BF16) for _ in range(2)]
            thr_t = [da([128, 1], F32) for _ in range(2)]
            PTt = [[da([128, 512], BF16) for _ in range(2)] for _ in range(2)]
            I4 = da([128, 512], BF16)
            lo_t = da([128, 1], F32)
            hi_t = da([128, 1], F32)
            W0 = da([128, 1], F32)
            mid_t = da([128, 1], F32)
            tsel = da([128, 1], F32)
            Wk = da([128, NBIS], F32)
            cnt = da([128, NBIS], F32)
            pow2 = da([128, NBIS], F32)
            ob = da([128, 520], F32)
            rden = da([128, 8], F32)
            ob_b = da([128, 512], BF16)
            for q in range(4):
                op("pool", lambda e, q=q: e.tensor_copy(out=I4[:, q * 128:(q + 1) * 128], in_=ident_b), reads=["ident_b"], writes=["I4"])
            for k in range(NBIS):
                op("pool", lambda e, k=k: e.memset(pow2[:, k:k + 1], 2.0 ** (-(k + 1))), writes=["pow2"])

            xbk = [0]

            def scores_part(tt, sb):
                L = (tt + 1) * 128
                nkb = (L + 511) // 512
                qs = slice(tt * 128, (tt + 1) * 128)
                score = scoreb[sb]
                maskb = maskbb[sb]
                for h in range(8):
                    hp, par = h // 2, h % 2
                    rows = slice(par * 64, par * 64 + 64)
                    for kb in range(nkb):
                        w = min(512, L - kb * 512)
                        bank = xbk[0] % 2
                        xbk[0] += 1
                        ks = slice(kb * 512, kb * 512 + w)
                        op("pe", lambda e, hp=hp, par=par, rows=rows, w=w, bank=bank, ks=ks: e.matmul(
                            PB(bank)[:, 0:w], lhsT=iqT[rows, hp, qs], rhs=ikT2[rows, ks], start=True, stop=True,
                            tile_position=(par * 64, 0)), reads=["iqT", "ikT2"], writes=[("pb", bank)])
                        op("act", lambda e, w=w, bank=bank: e.activation(out=rl[bank][:, 0:w], in_=PB(bank)[:, 0:w], func=AF.Relu),
                           reads=[("pb", bank)], writes=[("rl", bank)])
                        if h == 0:
                            op("dve", lambda e, w=w, bank=bank, ks=ks: e.tensor_scalar(
                                out=score[:, ks], in0=rl[bank][:, 0:w], scalar1=iw_tok[:, tt, 0:1], scalar2=None, op0=ALU.mult),
                               reads=[("rl", bank), "iw_tok"], writes=[("score", sb, kb)])
                        else:
                            op("dve", lambda e, w=w, bank=bank, ks=ks, h=h: e.scalar_tensor_tensor(
                                out=score[:, ks], in0=rl[bank][:, 0:w], scalar=iw_tok[:, tt, h:h + 1], in1=score[:, ks],
                                op0=ALU.mult, op1=ALU.add), reads=[("rl", bank), "iw_tok", ("score", sb, kb)],
                               writes=[("score", sb, kb)], fast=(w >= 256))
                SK = [("score", sb, kb) for kb in range(nkb)]
                if tt >= 2:
                    op("dve", lambda e: e.tensor_reduce(out=hi_t, in_=score[:, 0:L], axis=AX.X, op=ALU.max), reads=SK, writes=["hi"])
                    op("dve", lambda e: e.tensor_reduce(out=lo_t, in_=score[:, 0:L], axis=AX.X, op=ALU.min), reads=SK, writes=["lo"])
                op("dve", lambda e: e.memset(score[0:64, L - 64:L], -1.0e30), reads=SK, writes=SK)
                if tt >= 2:
                    op("dve", lambda e: e.tensor_tensor(out=W0, in0=hi_t, in1=lo_t, op=ALU.subtract), reads=["hi", "lo"], writes=["W0"])
                    op("dve", lambda e: e.tensor_scalar(out=Wk, in0=pow2, scalar1=W0[:, 0:1], scalar2=None, op0=ALU.mult),
                       reads=["W0", "pow2"], writes=["Wk"])
                    op("dve", lambda e: e.memset(cnt, 0.0), writes=["cnt"])
                    op("dve", lambda e: e.tensor_tensor(out=mid_t, in0=lo_t, in1=Wk[:, 0:1], op=ALU.add), reads=["lo", "Wk"], writes=["mid"])
                    for k in range(NBIS):
                        op("dve", lambda e, k=k: e.tensor_scalar(out=maskb[:, 0:L], in0=score[:, 0:L], scalar1=mid_t[:, 0:1],
                                                                 scalar2=0.0, op0=ALU.is_gt, op1=ALU.add, accum_out=cnt[:, k:k + 1]),
                           reads=SK + ["mid", "cnt"], writes=[("maskb", sb), ("cntk", k)])
                        op("dve", lambda e, k=k: e.tensor_scalar(out=tsel, in0=cnt[:, k:k + 1], scalar1=255.5, scalar2=0.5,
                                                                 op0=ALU.is_gt, op1=ALU.subtract), reads=[("cntk", k)], writes=["tsel"])
                        op("dve", lambda e, k=k: e.scalar_tensor_tensor(out=mid_t, in0=tsel, scalar=Wk[:, k:k + 1], in1=mid_t,
                                                                        op0=ALU.mult, op1=ALU.add),
                           reads=["tsel", "Wk", "mid"], writes=["mid"])
                    op("dve", lambda e: e.scalar_tensor_tensor(out=thr_t[sb], in0=Wk[:, NBIS - 1:NBIS], scalar=-0.5, in1=mid_t,
                                                               op0=ALU.mult, op1=ALU.add), reads=["Wk", "mid"], writes=[("thr", sb)])
                else:
                    op("dve", lambda e: e.memset(thr_t[sb], -1.0e29), writes=[("thr", sb)])
                op("dve", lambda e: e.tensor_scalar(out=maskb[:, 0:L], in0=score[:, 0:L], scalar1=thr_t[sb][:, 0:1], scalar2=NEG,
                                                    op0=ALU.is_le, op1=ALU.mult), reads=SK + [("thr", sb)], writes=[("maskb", sb)])
                if "thr" in dbg:
                    op("sp", lambda e: e.dma_start(out=dbg["thr"][tt * 128:(tt + 1) * 128, :], in_=thr_t[sb]), reads=[("thr", sb)],
                       writes=["dbg_thr"], dma=True)
                if "score" in dbg and tt == NT - 1:
                    op("sp", lambda e: e.dma_start(out=dbg["score"], in_=score), reads=SK, writes=["dbg_score"], dma=True)

            def attn_part(tt, sb):
                qs = slice(tt * 128, (tt + 1) * 128)
                maskb = maskbb[sb]
                for kb in range(tt + 1):
                    kcs = slice(kb * 128, (kb + 1) * 128)
                    for g2 in range(2):
                        bank = 2 + g2 + 2 * (kb % 2)
                        pt = PTt[g2][kb % 2]
                        op("pe", lambda e, bank=bank, kcs=kcs: e.matmul(PB(bank), lhsT=maskb[:, kcs], rhs=I4, start=True, stop=False),
                           reads=[("maskb", sb), "I4"], writes=[("pb", bank)])
                        for s_ in range(4):
                            h = 4 * g2 + s_
                            hp, par = h // 2, h % 2
                            kT = kz[g2][par]
                            op("pe", lambda e, bank=bank, s_=s_, kT=kT, kcs=kcs, hp=hp: e.matmul(
                                PB(bank)[:, s_ * 128:(s_ + 1) * 128], lhsT=kT[:, kcs], rhs=bqT[:, hp, qs], start=False, stop=(s_ == 3)),
                               reads=["bkT", "bqT"], writes=[("pb", bank)])
                        op("act", lambda e, bank=bank, pt=pt: e.activation(out=pt, in_=PB(bank), func=AF.Exp, scale=0.125),
                           reads=[("pb", bank)], writes=[("PT", g2, kb % 2)])
                        for s_ in range(4):
                            op("pe", lambda e, g2=g2, s_=s_, pt=pt, kb=kb: e.matmul(
                                PB(6 + g2)[:, s_ * 65:(s_ + 1) * 65], lhsT=pt[:, s_ * 128:(s_ + 1) * 128],
                                rhs=bv_tok[:, kb, g2 * 65:(g2 + 1) * 65], start=(kb == 0 and s_ == 0), stop=(kb == tt and s_ == 3)),
                               reads=[("PT", g2, kb % 2), "bv_tok"], writes=[("pb", 6 + g2)])
                op("act", lambda e: e.copy(out=ob[:, 0:260], in_=PB(6)[:, 0:260]), reads=[("pb", 6)], writes=["ob"])
                op("act", lambda e: e.copy(out=ob[:, 260:520], in_=PB(7)[:, 0:260]), reads=[("pb", 7)], writes=["ob"])
                obv = ob.rearrange("p (s e) -> p s e", e=65)
                op("dve", lambda e: e.reciprocal(out=rden, in_=obv[:, :, 64]), reads=["ob"], writes=["rden"])
                op("dve", lambda e: e.tensor_tensor(out=ob_b.rearrange("p (h d) -> p h d", h=8), in0=obv[:, :, 0:64],
                                                    in1=rden.unsqueeze(2).to_broadcast([128, 8, 64]), op=ALU.mult),
                   reads=["ob", "rden"], writes=["ob_b"])
                for q in range(4):
                    op("pe", lambda e, q=q: e.transpose(out=PBb(0)[:, q * 128:(q + 1) * 128], in_=ob_b[:, q * 128:(q + 1) * 128],
                                                        identity=ident_b), reads=["ob_b", "ident_b"], writes=[("pb", 0)])
                op("act", lambda e: e.copy(out=o_bT[:, :, qs], in_=PBb(0)[:, 0:512].rearrange("p (q t) -> p q t", q=4)),
                   reads=[("pb", 0)], writes=[("o_bT", tt)])

            scores_part(0, 0)
            for tt in range(NT):
                if tt + 1 < NT:
                    scores_part(tt + 1, (tt + 1) % 2)
                attn_part(tt, tt % 2)
            if "thr" in dbg:
                op("sp", None, reads=["dbg_thr"])
            if "score" in dbg:
                op("sp", None, reads=["dbg_score"])
            if "o_bT" in dbg:
                op("sp", lambda e: e.dma_start(out=dbg["o_bT"].rearrange("(a p) t -> p a t", p=128), in_=o_bT),
                   reads=[("o_bT", tt) for tt in range(NT)], writes=["dbg_o_bT"], dma=True)
                op("sp", None, reads=["dbg_o_bT"])

            sch.barrier()
            if stop_after == "dsa":
                return

            pa = MultiAlloc([(R_W, ARENA_BYTES)])
            hT2 = pa([128, 8, S], BF16)
            mergedT = pa([128, 8, S], BF16)
            x1 = pa([128, NT, D], F32)
            bgate = pa([128, 16], F32)
            g2col = pa([128, 8], F32)
            fng = pa([128, D], F32)
            wst2 = pa([128, 8, 256], F32)
            wg_bf = [pa([128, 8, 256], BF16) for _ in range(2)]
            wp_bf = [pa([128, 4, 256], BF16) for _ in range(2)]
            ga_s = pa([128, 512], BF16)
            gb_s = pa([128, 512], BF16)
            t1 = pa([128, 512], BF16)
            t2 = pa([128, 512], BF16)
            xt4 = [pa([128, D], F32) for _ in range(2)]
            op("sp", lambda e: e.dma_start(out=bgate, in_=bgate_d), writes=["bgate"], dma=True)
            op("sp", lambda e: e.dma_start(out=g2col, in_=g2_d), writes=["g2col"], dma=True)
            op("sp", lambda e: e.dma_start(out=fng, in_=fng_d.partition_broadcast(128)), writes=["fng"], dma=True)

            def phase1b():
                xa = Alloc(R_W + 64 * K, R_W + 128 * K)
                xt = [xa([128, D], F32) for _ in range(2)]
                hb = [xa([128, D], BF16) for _ in range(2)]
                junk = xa([128, D], BF16)
                ssx = xa([128, NT], F32)
                rsx = xa([128, NT], F32)
                op("dve", lambda e: e.memset(ssx, 0.0), writes=["ssx"])
                for tt in range(NT):
                    b = tt % 2
                    op("sp", lambda e, tt=tt, b=b: e.dma_start(out=xt[b], in_=x_d[tt * 128:(tt + 1) * 128, :]), writes=[("xt", b)], dma=True)
                    op("act", lambda e, tt=tt, b=b: e.activation(out=junk, in_=xt[b], func=AF.Square, accum_out=ssx[:, tt:tt + 1]),
                       reads=[("xt", b), "ssx"], writes=["junk", ("ssx", tt)])
                    op("act", lambda e, tt=tt: e.activation(out=rsx[:, tt:tt + 1], in_=ssx[:, tt:tt + 1], func=AF.Sqrt, bias=epsc,
                                                            scale=1.0 / D), reads=[("ssx", tt), "epsc"], writes=[("rsx", tt)])
                    op("dve", lambda e, tt=tt: e.reciprocal(out=rsx[:, tt:tt + 1], in_=rsx[:, tt:tt + 1]), reads=[("rsx", tt)],
                       writes=[("rsx", tt)])
                    op("dve", lambda e, tt=tt, b=b: e.tensor_scalar(out=hb[b], in0=xt[b], scalar1=rsx[:, tt:tt + 1], scalar2=None,
                                                                    op0=ALU.mult), reads=[("xt", b), ("rsx", tt)], writes=[("hb", b)])
                    bk = tt % 2
                    for k in range(8):
                        op("pe", lambda e, k=k, b=b, bk=bk: e.transpose(out=PBb(bk)[:, k * 128:(k + 1) * 128],
                                                                        in_=hb[b][:, k * 128:(k + 1) * 128], identity=ident_b),
                           reads=[("hb", b), "ident_b"], writes=[("pb", bk)])
                    op("act", lambda e, tt=tt, bk=bk: e.copy(out=hT2[:, :, tt * 128:(tt + 1) * 128],
                                                             in_=PBb(bk).rearrange("p (k t) -> p k t", k=8)),
                       reads=[("pb", bk)], writes=[("hT2", tt)])
            phase1b()
            sch.barrier()
            HT2 = [("hT2", tt) for tt in range(NT)]

            wpa_v = wpa_d.rearrange("(k p) c -> p k c", p=128)
            wpb_v = wpb_d.rearrange("(k p) c -> p k c", p=128)
            wout_v = wout_d.rearrange("(k p) c -> p k c", p=128)
            wi4 = [0]

            wst2b = xt4[0].rearrange("p (k c) -> p k c", k=4)
            wst2c = xt4[1].rearrange("p (k c) -> p k c", k=4)
            wi5 = [0]

            def load_gate(c0):
                i = wi4[0]
                wi4[0] += 1
                b = i % 2
                op("sp", lambda e: e.dma_start(out=wst2, in_=w_in_v[:, :, c0:c0 + 256]), writes=["wst2"], dma=True)
                op("pool", lambda e, b=b: e.tensor_tensor(out=wg_bf[b], in0=wst2, in1=g1col.unsqueeze(2).to_broadcast([128, 8, 256]),
                                                          op=ALU.mult), reads=["wst2", "g1col"], writes=[("wg", b)])
                return wg_bf[b], ("wg", b)

            def load_proj(src_v, c0):
                i = wi5[0]
                wi5[0] += 1
                b = i % 2
                st = wst2b if b == 0 else wst2c
                op("sp", lambda e: e.dma_start(out=st, in_=src_v[:, :, c0:c0 + 256]), writes=[("wstp", b)], dma=True)
                op("pool", lambda e, b=b: e.tensor_copy(out=wp_bf[b], in_=st), reads=[("wstp", b)], writes=[("wp", b)])
                return wp_bf[b], ("wp", b)

            p4u = [0]
            for j in range(4):
                wga, kga = load_gate(C_GA + j * 256)
                wgb, kgb = load_gate(C_GB + j * 256)
                wpa, kpa = load_proj(wpa_v, j * 256)
                wpb, kpb = load_proj(wpb_v, j * 256)
                for ct in range(2):
                    c = 2 * j + ct
                    ccs = slice(ct * 128, (ct + 1) * 128)
                    for tb in range(4):
                        tcs = slice(tb * 512, (tb + 1) * 512)
                        hk = HT2[tb * 4:tb * 4 + 4]
                        bA, bB, bC, bD = (2, 3, 4, 5) if (p4u[0] % 2 == 0) else (0, 1, 6, 7)
                        p4u[0] += 1
                        for k in range(8):
                            op("pe", lambda e, k=k, ccs=ccs, tcs=tcs, wga=wga, bA=bA: e.matmul(PB(bA), lhsT=wga[:, k, ccs], rhs=hT2[:, k, tcs],
                                                                                         start=(k == 0), stop=(k == 7)),
                               reads=[kga] + hk, writes=[("pb", bA)])
                        op("act", lambda e, c=c, bA=bA: e.activation(out=ga_s, in_=PB(bA), func=AF.Sigmoid, bias=bgate[:, c:c + 1], scale=1.0),
                           reads=[("pb", bA), "bgate"], writes=["ga_s"])
                        for k in range(8):
                            op("pe", lambda e, k=k, ccs=ccs, tcs=tcs, wgb=wgb, bB=bB: e.matmul(PB(bB), lhsT=wgb[:, k, ccs], rhs=hT2[:, k, tcs],
                                                                                         start=(k == 0), stop=(k == 7)),
                               reads=[kgb] + hk, writes=[("pb", bB)])
                        op("act", lambda e, c=c, bB=bB: e.activation(out=gb_s, in_=PB(bB), func=AF.Sigmoid, bias=bgate[:, 8 + c:9 + c], scale=1.0),
                           reads=[("pb", bB), "bgate"], writes=["gb_s"])
                        for hp in range(4):
                            op("pe", lambda e, hp=hp, ccs=ccs, tcs=tcs, wpa=wpa, bC=bC: e.matmul(PB(bC), lhsT=wpa[:, hp, ccs], rhs=o_aT[:, hp, tcs],
                                                                                           start=(hp == 0), stop=(hp == 3)),
                               reads=[kpa, "o_aT"], writes=[("pb", bC)])
                        for hp in range(4):
                            op("pe", lambda e, hp=hp, ccs=ccs, tcs=tcs, wpb=wpb, bD=bD: e.matmul(PB(bD), lhsT=wpb[:, hp, ccs], rhs=o_bT[:, hp, tcs],
                                                                                           start=(hp == 0), stop=(hp == 3)),
                               reads=[kpb, "o_bT"], writes=[("pb", bD)])
                        op("dve", lambda e, bC=bC: e.tensor_tensor(out=t1, in0=PB(bC), in1=ga_s, op=ALU.mult), reads=[("pb", bC), "ga_s"],
                           writes=["t1"])
                        op("dve", lambda e, bD=bD: e.tensor_tensor(out=t2, in0=PB(bD), in1=gb_s, op=ALU.mult), reads=[("pb", bD), "gb_s"],
                           writes=["t2"])
                        op("pool", lambda e, c=c, tcs=tcs: e.tensor_tensor(out=mergedT[:, c, tcs], in0=t1, in1=t2, op=ALU.add),
                           reads=["t1", "t2"], writes=[("mergedT", c, tb)])
            if "mergedT" in dbg:
                op("sp", lambda e: e.dma_start(out=dbg["mergedT"].rearrange("(a p) t -> p a t", p=128), in_=mergedT),
                   reads=[("mergedT", c, tb) for c in range(8) for tb in range(4)], writes=["dbg_mergedT"], dma=True)
                op("sp", None, reads=["dbg_mergedT"])
            sch.barrier()
            wout_bf = view(R_A, [128, 8, D], BF16)
            for j in range(4):
                op("sp", lambda e, j=j: e.dma_start(out=wst2, in_=wout_v[:, :, j * 256:(j + 1) * 256]), writes=["wst2"], dma=True)
                op("pool", lambda e, j=j: e.tensor_copy(out=wout_bf[:, :, j * 256:(j + 1) * 256], in_=wst2), reads=["wst2"],
                   writes=[("wout", j)])
            WOUT = [("wout", j) for j in range(4)]
            for tt in range(NT):
                b = tt % 2
                op("sp", lambda e, tt=tt, b=b: e.dma_start(out=xt4[b], in_=x_d[tt * 128:(tt + 1) * 128, :]), writes=[("xt4", b)], dma=True)
                for nb in range(2):
                    bk = 2 + 2 * b + nb
                    for c in range(8):
                        op("pe", lambda e, c=c, tt=tt, nb=nb, bk=bk: e.matmul(PB(bk), lhsT=mergedT[:, c, tt * 128:(tt + 1) * 128],
                                                                               rhs=wout_bf[:, c, nb * 512:(nb + 1) * 512],
                                                                               start=(c == 0), stop=(c == 7)),
                           reads=WOUT + ["mergedT_all"], writes=[("pb", bk)])
                    op("dve", lambda e, tt=tt, nb=nb, bk=bk, b=b: e.tensor_tensor(out=x1[:, tt, nb * 512:(nb + 1) * 512], in0=PB(bk),
                                                                                   in1=xt4[b][:, nb * 512:(nb + 1) * 512], op=ALU.add),
                       reads=[("pb", bk), ("xt4", b)], writes=[("x1", tt)])
            if "x1" in dbg:
                op("sp", lambda e: e.dma_start(out=dbg["x1"].rearrange("(t p) c -> p t c", p=128), in_=x1),
                   reads=[("x1", tt) for tt in range(NT)], writes=["dbg_x1"], dma=True)
                op("sp", None, reads=["dbg_x1"])
            sch.barrier()
            if stop_after == "p4":
                return

            h2T = view(R_W, [128, 8, S], BF16)
            ma = MultiAlloc([(R_W + 32 * K, R_W + 64 * K), (R_A, R_A + 32 * K)])
            tail_off = None
            hb2 = [ma([128, D], BF16) for _ in range(2)]
            junk2 = ma([128, D], BF16)
            ss5 = ma([128, NT], F32)
            rs5 = ma([128, NT], F32)
            wr_st = ma([128, 8, 20], F32)
            wr_bf = ma([128, 8, 20], BF16)
            brow = ma([128, 20], F32)
            lg = ma([128, 20], F32)
            sm = {n_: ma([128, 4], F32) for n_ in ("goh", "gex", "elg", "oh1", "msk", "oh2", "wsel")}
            sc1 = {n_: ma([128, 1], F32) for n_ in ("gmax", "ngmax", "gsum", "ggate", "m1", "m2", "d21", "e21", "den", "w1", "w2")}
            tmp44 = ma([128, 4, 4], F32)
            comb_b = ma([128, 16], BF16)
            combT = ma([128, S], BF16)
            sel16 = ma([128, 16, 128], BF16)
            est = ma([128, 8, 256], F32)
            w1b = [ma([128, 8, 256], BF16) for _ in range(2)]
            w3b = [ma([128, 8, 256], BF16) for _ in range(2)]
            w2b = [ma([128, 2, D], BF16) for _ in range(2)]
            sg = [[ma([128, 512], BF16) for _ in range(2)] for _ in range(2)]
            cbt = [ma([128, 512], BF16) for _ in range(2)]
            tu = [[ma([128, 512], BF16) for _ in range(2)] for _ in range(2)]
            actT = [[ma([128, 512], BF16) for _ in range(2)] for _ in range(2)]
            op("sp", lambda e: e.dma_start(out=wr_st, in_=wr_d.rearrange("(k p) c -> p k c", p=128)), writes=["wr_st"], dma=True)
            op("sp", lambda e: e.dma_start(out=brow, in_=br_d.partition_broadcast(128)), writes=["brow"], dma=True)
            op("pool", lambda e: e.tensor_tensor(out=wr_bf, in0=wr_st, in1=g2col.unsqueeze(2).to_broadcast([128, 8, 20]), op=ALU.mult),
               reads=["wr_st", "g2col"], writes=["wr_bf"])
            op("pool", lambda e: e.memset(sel16[0:16, :, :], 1.0), writes=["sel16"])
            op("pool", lambda e: e.affine_select(out=sel16[0:16, :, :], in_=sel16[0:16, :, :], pattern=[[-1, 16], [0, 128]],
                                                 compare_op=ALU.is_equal, fill=0.0, base=0, channel_multiplier=1), writes=["sel16"])
            op("dve", lambda e: e.memset(ss5, 0.0), writes=["ss5"])
            def prep_tile(tt):
                b = tt % 2
                bk = tt % 2
                op("act", lambda e, tt=tt: e.activation(out=junk2, in_=x1[:, tt, :], func=AF.Square, accum_out=ss5[:, tt:tt + 1]),
                   reads=["x1_all", "ss5"], writes=["junk2", ("ss5", tt)])
                op("act", lambda e, tt=tt: e.activation(out=rs5[:, tt:tt + 1], in_=ss5[:, tt:tt + 1], func=AF.Ln, bias=epsc,
                                                        scale=1.0 / D), reads=[("ss5", tt), "epsc"], writes=[("rs5", tt)])
                op("act", lambda e, tt=tt: e.activation(out=rs5[:, tt:tt + 1], in_=rs5[:, tt:tt + 1], func=AF.Exp, scale=-0.5),
                   reads=[("rs5", tt)], writes=[("rs5", tt)])
                op("dve", lambda e, tt=tt, b=b: e.tensor_scalar(out=hb2[b], in0=x1[:, tt, :], scalar1=rs5[:, tt:tt + 1], scalar2=None,
                                                                op0=ALU.mult), reads=["x1_all", ("rs5", tt)], writes=[("hb2", b)])
                for k in range(8):
                    op("pe", lambda e, k=k, b=b, bk=bk: e.transpose(out=PBb(bk)[:, k * 128:(k + 1) * 128],
                                                                    in_=hb2[b][:, k * 128:(k + 1) * 128], identity=ident_b),
                       reads=[("hb2", b), "ident_b"], writes=[("pb", bk)])
                op("act", lambda e, tt=tt, bk=bk: e.copy(out=h2T[:, :, tt * 128:(tt + 1) * 128],
                                                         in_=PBb(bk).rearrange("p (k t) -> p k t", k=8)),
                   reads=[("pb", bk)], writes=[("h2T", tt)])
                for k in range(8):
                    op("pe", lambda e, k=k, tt=tt: e.matmul(PB(2)[:, 0:20], lhsT=h2T[:, k, tt * 128:(tt + 1) * 128], rhs=wr_bf[:, k, :],
                                                            start=(k == 0), stop=(k == 7)),
                       reads=[("h2T", tt), "wr_bf"], writes=[("pb", 2)])
                R = []

                def rop(fn, rd, wr):
                    op("dve", fn, reads=rd, writes=wr)
                rop(lambda e: e.tensor_tensor(out=lg, in0=PB(2)[:, 0:20], in1=brow, op=ALU.add), [("pb", 2), "brow"], ["lg"])
                elv = lg[:, 4:20].rearrange("p (g x) -> p g x", g=4)
                rop(lambda e: e.tensor_reduce(out=sc1["gmax"], in_=lg[:, 0:4], axis=AX.X, op=ALU.max), ["lg"], ["gmax"])
                rop(lambda e: e.tensor_scalar(out=sm["goh"], in0=lg[:, 0:4], scalar1=sc1["gmax"][:, 0:1], scalar2=None,
                                              op0=ALU.is_equal), ["lg", "gmax"], ["goh"])
                rop(lambda e: e.tensor_scalar(out=sc1["ngmax"], in0=sc1["gmax"], scalar1=-1.0, scalar2=None, op0=ALU.mult),
                    ["gmax"], ["ngmax"])
                op("act", lambda e: e.activation(out=sm["gex"], in_=lg[:, 0:4], func=AF.Exp, bias=sc1["ngmax"][:, 0:1], scale=1.0),
                   reads=["lg", "ngmax"], writes=["gex"])
                rop(lambda e: e.tensor_reduce(out=sc1["gsum"], in_=sm["gex"], axis=AX.X, op=ALU.add), ["gex"], ["gsum"])
                rop(lambda e: e.reciprocal(out=sc1["ggate"], in_=sc1["gsum"]), ["gsum"], ["ggate"])
                rop(lambda e: e.tensor_tensor(out=tmp44, in0=elv, in1=sm["goh"].unsqueeze(2).to_broadcast([128, 4, 4]), op=ALU.mult),
                    ["lg", "goh"], ["tmp44"])
                rop(lambda e: e.tensor_reduce(out=sm["elg"], in_=tmp44.rearrange("p g x -> p x g"), axis=AX.X, op=ALU.add),
                    ["tmp44"], ["elg"])
                rop(lambda e: e.tensor_reduce(out=sc1["m1"], in_=sm["elg"], axis=AX.X, op=ALU.max), ["elg"], ["m1"])
                rop(lambda e: e.tensor_scalar(out=sm["oh1"], in0=sm["elg"], scalar1=sc1["m1"][:, 0:1], scalar2=None, op0=ALU.is_equal),
                    ["elg", "m1"], ["oh1"])
                rop(lambda e: e.scalar_tensor_tensor(out=sm["msk"], in0=sm["oh1"], scalar=-1.0e30, in1=sm["elg"], op0=ALU.mult,
                                                     op1=ALU.add), ["oh1", "elg"], ["msk"])
                rop(lambda e: e.tensor_reduce(out=sc1["m2"], in_=sm["msk"], axis=AX.X, op=ALU.max), ["msk"], ["m2"])
                rop(lambda e: e.tensor_scalar(out=sm["oh2"], in0=sm["msk"], scalar1=sc1["m2"][:, 0:1], scalar2=None, op0=ALU.is_equal),
                    ["msk", "m2"], ["oh2"])
                rop(lambda e: e.tensor_tensor(out=sc1["d21"], in0=sc1["m2"], in1=sc1["m1"], op=ALU.subtract), ["m1", "m2"], ["d21"])
                op("act", lambda e: e.activation(out=sc1["e21"], in_=sc1["d21"], func=AF.Exp), reads=["d21"], writes=["e21"])
                rop(lambda e: e.tensor_scalar(out=sc1["den"], in0=sc1["e21"], scalar1=1.0, scalar2=None, op0=ALU.add), ["e21"], ["den"])
                rop(lambda e: e.reciprocal(out=sc1["den"], in_=sc1["den"]), ["den"], ["den"])
                rop(lambda e: e.tensor_tensor(out=sc1["w1"], in0=sc1["ggate"], in1=sc1["den"], op=ALU.mult), ["ggate", "den"], ["w1"])
                rop(lambda e: e.tensor_tensor(out=sc1["w2"], in0=sc1["w1"], in1=sc1["e21"], op=ALU.mult), ["w1", "e21"], ["w2"])
                rop(lambda e: e.tensor_scalar(out=sm["wsel"], in0=sm["oh1"], scalar1=sc1["w1"][:, 0:1], scalar2=None, op0=ALU.mult),
                    ["oh1", "w1"], ["wsel"])
                rop(lambda e: e.scalar_tensor_tensor(out=sm["wsel"], in0=sm["oh2"], scalar=sc1["w2"][:, 0:1], in1=sm["wsel"],
                                                     op0=ALU.mult, op1=ALU.add), ["oh2", "w2", "wsel"], ["wsel"])
                rop(lambda e: e.tensor_tensor(out=comb_b.rearrange("p (g x) -> p g x", g=4),
                                              in0=sm["goh"].unsqueeze(2).to_broadcast([128, 4, 4]),
                                              in1=sm["wsel"].unsqueeze(1).to_broadcast([128, 4, 4]), op=ALU.mult),
                    ["goh", "wsel"], ["comb_b"])
                if "comb" in dbg:
                    op("sp", lambda e, tt=tt: e.dma_start(out=dbg["comb"][tt * 128:(tt + 1) * 128, :], in_=comb_b), reads=["comb_b"],
                       writes=["dbg_comb"], dma=True)
                op("pe", lambda e: e.transpose(out=PBb(3)[0:16, 0:128], in_=comb_b, identity=ident_b), reads=["comb_b", "ident_b"],
                   writes=[("pb", 3)])
                op("act", lambda e, tt=tt: e.copy(out=combT[0:16, tt * 128:(tt + 1) * 128], in_=PBb(3)[0:16, 0:128]),
                   reads=[("pb", 3)], writes=[("combT", tt)])
            for tt in range(4):
                prep_tile(tt)
            H2T = [("h2T", tt) for tt in range(NT)]
            CT = [("combT", tt) for tt in range(NT)]

            def load_expert(e_i):
                b = e_i % 2
                for (src, dst, nm, fold) in ((w1_d, w1b[b], "w1", True), (w3_d, w3b[b], "w3", True)):
                    op("sp", lambda e, src=src: e.dma_start(out=est, in_=src[e_i].rearrange("(k p) f -> p k f", p=128)),
                       writes=["est"], dma=True)
                    op("pool", lambda e, dst=dst: e.tensor_tensor(out=dst, in0=est, in1=g2col.unsqueeze(2).to_broadcast([128, 8, 256]),
                                                                  op=ALU.mult), reads=["est", "g2col"], writes=[(nm, b)])
                op("sp", lambda e: e.dma_start(out=est.rearrange("p k f -> p (k f)").rearrange("p (a c) -> p a c", a=2),
                                               in_=w2_d[e_i].rearrange("(a p) c -> p a c", p=128)), writes=["est"], dma=True)
                op("pool", lambda e: e.tensor_copy(out=w2b[b], in_=est.rearrange("p k f -> p (k f)").rearrange("p (a c) -> p a c", a=2)),
                   reads=["est"], writes=[("w2", b)])

            def stageA(e_i, tb, sl):
                b = e_i % 2
                tcs = slice(tb * 512, (tb + 1) * 512)
                hk = H2T[tb * 4:tb * 4 + 4]
                cbk_ = 6
                op("pe", lambda e: e.matmul(PB(cbk_), lhsT=sel16[0:16, e_i, :], rhs=combT[0:16, tcs], start=True, stop=True),
                   reads=["sel16"] + CT[tb * 4:tb * 4 + 4], writes=[("pb", cbk_)])
                op("act", lambda e: e.copy(out=cbt[sl], in_=PB(cbk_)), reads=[("pb", cbk_)], writes=[("cbt", sl)])
                for ft in range(2):
                    fcs = slice(ft * 128, (ft + 1) * 128)
                    for k in range(8):
                        op("pe", lambda e, k=k, fcs=fcs, ft=ft: e.matmul(PB(2 + ft), lhsT=w1b[b][:, k, fcs], rhs=h2T[:, k, tcs],
                                                                         start=(k == 0), stop=(k == 7)),
                           reads=[("w1", b)] + hk, writes=[("pb", 2 + ft)])
                    op("act", lambda e, ft=ft: e.activation(out=sg[sl][ft], in_=PB(2 + ft), func=AF.Silu), reads=[("pb", 2 + ft)],
                       writes=[("sg", sl, ft)])
                    yield
                    for k in range(8):
                        op("pe", lambda e, k=k, fcs=fcs, ft=ft: e.matmul(PB(4 + ft), lhsT=w3b[b][:, k, fcs], rhs=h2T[:, k, tcs],
                                                                         start=(k == 0), stop=(k == 7)),
                           reads=[("w3", b)] + hk, writes=[("pb", 4 + ft)])
                    op("dve", lambda e, ft=ft: e.tensor_tensor(out=tu[sl][ft], in0=PB(4 + ft), in1=sg[sl][ft], op=ALU.mult),
                       reads=[("pb", 4 + ft), ("sg", sl, ft)], writes=[("tu", sl, ft)], fast=True)
                    op("pool", lambda e, ft=ft: e.tensor_tensor(out=actT[sl][ft], in0=tu[sl][ft], in1=cbt[sl], op=ALU.mult),
                       reads=[("tu", sl, ft), ("cbt", sl)], writes=[("actT", sl, ft)])
                    yield

            ybank = [0]

            def stageB(e_i, tb, sl):
                b = e_i % 2
                for t4 in range(4):
                    tt = tb * 4 + t4
                    for nb in range(2):
                        bk = (0, 1, 7)[ybank[0] % 3]
                        ybank[0] += 1
                        for ft in range(2):
                            op("pe", lambda e, ft=ft, t4=t4, nb=nb, bk=bk: e.matmul(
                                PB(bk), lhsT=actT[sl][ft][:, t4 * 128:(t4 + 1) * 128], rhs=w2b[b][:, ft, nb * 512:(nb + 1) * 512],
                                start=(ft == 0), stop=(ft == 1)), reads=[("actT", sl, ft), ("w2", b)], writes=[("pb", bk)])
                        op("dve", lambda e, tt=tt, nb=nb, bk=bk: e.tensor_tensor(out=x1[:, tt, nb * 512:(nb + 1) * 512],
                                                                                 in0=PB(bk), in1=x1[:, tt, nb * 512:(nb + 1) * 512],
                                                                                 op=ALU.add),
                           reads=[("pb", bk), ("x2", tt, nb)], writes=[("x2", tt, nb)], fast=True)
                        if nb == 1:
                            yield

            def drain(g_):
                for _ in g_:
                    pass

            units = [(e_i, tb) for e_i in range(16) for tb in range(4)]
            load_expert(0)
            load_expert(1)
            drain(stageA(units[0][0], units[0][1], 0))
            for u, (e_i, tb) in enumerate(units):
                gb = stageB(e_i, tb, u % 2)
                if u + 1 < len(units):
                    ne, ntb = units[u + 1]
                    if ne == 0:
                        for tt in range(4 * ntb, 4 * ntb + 4):
                            prep_tile(tt)
                    ga_ = stageA(ne, ntb, (u + 1) % 2)
                    drain(ga_)
                drain(gb)
                if tb == 3 and e_i + 2 < 16:
                    load_expert(e_i + 2)
            sch.barrier()
            if "x2" in dbg:
                op("sp", lambda e: e.dma_start(out=dbg["x2"].rearrange("(t p) c -> p t c", p=128), in_=x1), writes=["dbg_x2"], dma=True)
                op("sp", None, reads=["dbg_x2"])

            fa = MultiAlloc([(R_W, R_W + 64 * K)])
            ss6 = fa([128, NT], F32)
            rs6 = fa([128, NT], F32)
            junk6 = fa([128, D], BF16)
            yo = [fa([128, D], F32) for _ in range(2)]
            op("dve", lambda e: e.memset(ss6, 0.0), writes=["ss6"])
            for tt in range(NT):
                b = tt % 2
                op("act", lambda e, tt=tt: e.activation(out=junk6, in_=x1[:, tt, :], func=AF.Square, accum_out=ss6[:, tt:tt + 1]),
                   reads=["ss6"], writes=["junk6", ("ss6", tt)])
                op("act", lambda e, tt=tt: e.activation(out=rs6[:, tt:tt + 1], in_=ss6[:, tt:tt + 1], func=AF.Sqrt, bias=epsc,
                                                        scale=1.0 / D), reads=[("ss6", tt), "epsc"], writes=[("rs6", tt)])
                op("dve", lambda e, tt=tt: e.reciprocal(out=rs6[:, tt:tt + 1], in_=rs6[:, tt:tt + 1]), reads=[("rs6", tt)],
                   writes=[("rs6", tt)])
                op("dve", lambda e, tt=tt, b=b: e.scalar_tensor_tensor(out=yo[b], in0=x1[:, tt, :], scalar=rs6[:, tt:tt + 1], in1=fng,
                                                                       op0=ALU.mult, op1=ALU.mult),
                   reads=[("rs6", tt), "fng"], writes=[("yo", b)])
                op("sp", lambda e, tt=tt, b=b: e.dma_start(out=out_d[tt * 128:(tt + 1) * 128, :], in_=yo[b]), reads=[("yo", b)],
                   writes=[("out", tt)], dma=True)
            op("sp", None, reads=[("out", tt) for tt in range(NT)])


        body()
        sch.barrier()
        DEBUG["stats_pre"] = {e: len(sch.ops[e]) for e in Sched.ENGS}
        with nc.Block() as block:
            sch.emit(nc, block, engsem, dmasem)
        DEBUG["stats"] = sch.stats
    return nc


_NC_CACHE = {}


def kernel(**inputs):
    dbg = tuple(DEBUG.get("outputs", ()))
    key = (dbg, DEBUG.get("stop_after"))
    if key not in _NC_CACHE:
        _NC_CACHE[key] = build_nc(dbg, DEBUG.get("stop_after"))
    nc = _NC_CACHE[key]
    n = 8
    x = np.ascontiguousarray(inputs["x"], dtype=np.float32)
    posn = np.ascontiguousarray(inputs["positions"], dtype=np.int32)
    f32 = lambda a: np.ascontiguousarray(a, dtype=np.float32)
    inv = (10000.0 ** (-np.arange(32, dtype=np.float32) / np.float32(32))).astype(np.float32).reshape(1, 32)
    shared = {
        "norm1_g": f32(inputs["norm1_g"][0].reshape(8, 128).T),
        "w_in": f32(inputs["w_in"][0]),
        "conv_w": f32(inputs["conv_w"][0].reshape(4, 12, 128).transpose(2, 1, 0).reshape(128, 48)),
        "inv_freq": inv,
        "a_log": f32(inputs["a_log"][0].reshape(1, 8)),
        "dt_bias": f32(inputs["dt_bias"][0].reshape(1, 8)),
        "a_norm_g": f32(inputs["a_norm_g"][0].reshape(1, 64)),
        "b_gate": f32(inputs["b_gate"][0].reshape(16, 128).T),
        "norm2_g": f32(inputs["norm2_g"][0].reshape(8, 128).T),
        "final_norm_g": f32(inputs["final_norm_g"].reshape(1, D)),
        "w_proj_a": f32(inputs["w_proj_a"][0]),
        "w_proj_b": f32(inputs["w_proj_b"][0]),
        "w_out": f32(inputs["w_out"][0]),
        "w_router": f32(np.concatenate([inputs["w_router_group"][0], inputs["w_router_expert"][0]], axis=1)),
        "b_router": f32(np.concatenate([inputs["b_router_group"][0], inputs["b_router_expert"][0]], axis=0).reshape(1, 20)),
        "w_exp_gate": f32(inputs["w_exp_gate"][0]),
        "w_exp_up": f32(inputs["w_exp_up"][0]),
        "w_exp_down": f32(inputs["w_exp_down"][0]),
    }
    in_maps = []
    for c in range(n):
        m = dict(shared)
        m["x"] = x[c]
        m["positions"] = np.ascontiguousarray(posn[c].reshape(NT, 128).T)
        in_maps.append(m)
    res = run_bass_kernel_spmd(nc, in_maps, core_ids=list(range(n)))
    DEBUG["results"] = res.results
    return np.stack([r["out"] for r in res.results], axis=0)
```
